# Optimizing a Trainium2 kernel written in Bass

```python
import math
import jax
import jax.numpy as jnp
from jax import lax
import numpy as np

D_MODEL = 1024
BATCH = 16
SEQ = 2048
DEPTH = 2

N_EVEN = (DEPTH + 1) // 2
N_ODD = DEPTH // 2
BRANCH = D_MODEL // 2
S5_GROUP = 16
S5_GROUPS = BRANCH // S5_GROUP
S5_STATE = 64
S5_DT_MIN = 1e-3
S5_DT_MAX = 1e-1
ML_HEADS = 4
ML_HEAD_DIM = BRANCH // ML_HEADS
ML_CONV = 4
ML_CHUNK = 64
DIL_HEADS = 8
DIL_HEAD_DIM = BRANCH // DIL_HEADS
DIL_PAIRS = ((128, 1), (512, 4), (2048, 16))
DIFF_HEADS = 4
DIFF_HEAD_DIM = BRANCH // (2 * DIFF_HEADS)
Q_BLOCK = 128
ROPE_THETA = 10000.0
NORM_EPS = 1e-6
HEAD_NORM_EPS = 1e-5
F32 = jnp.float32

kernel_name = 'hybrid_s5_mlstm_dilated_diffattn'


def _rmsnorm(x, g, eps=NORM_EPS):
    xf = x.astype(F32)
    y = xf * lax.rsqrt(jnp.mean(xf * xf, axis=-1, keepdims=True) + eps)
    return (y * g.astype(F32)).astype(x.dtype)


def _rotary(x, pos):
    dh = x.shape[-1]
    L = x.shape[1]
    inv = ROPE_THETA ** (-jnp.arange(0, dh, 2, dtype=F32) / dh)
    ang = pos.astype(F32)[:, None] * inv[None, :]
    shape = (1, L) + (1,) * (x.ndim - 3) + (dh // 2,)
    cos = jnp.cos(ang).reshape(shape)
    sin = jnp.sin(ang).reshape(shape)
    xf = x.astype(F32)
    x1, x2 = xf[..., : dh // 2], xf[..., dh // 2:]
    return jnp.concatenate([x1 * cos - x2 * sin, x2 * cos + x1 * sin], axis=-1).astype(x.dtype)


def _complex_diag_scan(a_r, a_i, b_r, b_i):
    def combine(e1, e2):
        ar1, ai1, br1, bi1 = e1
        ar2, ai2, br2, bi2 = e2
        return (ar2 * ar1 - ai2 * ai1, ar2 * ai1 + ai2 * ar1,
                ar2 * br1 - ai2 * bi1 + br2, ar2 * bi1 + ai2 * br1 + bi2)
    _, _, x_r, x_i = lax.associative_scan(combine, (a_r, a_i, b_r, b_i), axis=0)
    return x_r, x_i


def _s5_branch(u, lam_re, lam_im, log_dt, b_re, b_im, c_re, c_im, d_skip, glu_w, glu_b):
    Bsz, L, _ = u.shape
    uf = u.astype(F32)
    ug = uf.reshape(Bsz, L, S5_GROUPS, S5_GROUP)
    lr, li = lam_re.astype(F32), lam_im.astype(F32)
    dt = jnp.exp(log_dt.astype(F32))[:, None]
    mag = jnp.exp(lr * dt)
    ab_r, ab_i = mag * jnp.cos(li * dt), mag * jnp.sin(li * dt)
    den = lr * lr + li * li
    f_r = ((ab_r - 1.0) * lr + ab_i * li) / den
    f_i = (ab_i * lr - (ab_r - 1.0) * li) / den
    br, bi = b_re.astype(F32), b_im.astype(F32)
    bb_r = f_r[..., None] * br - f_i[..., None] * bi
    bb_i = f_r[..., None] * bi + f_i[..., None] * br
    bu_r = jnp.einsum('blgc,gpc->blgp', ug, bb_r)
    bu_i = jnp.einsum('blgc,gpc->blgp', ug, bb_i)
    a_r = jnp.broadcast_to(ab_r, (L,) + ab_r.shape)
    a_i = jnp.broadcast_to(ab_i, (L,) + ab_i.shape)
    x_r, x_i = jax.vmap(_complex_diag_scan, in_axes=(None, None, 0, 0))(a_r, a_i, bu_r, bu_i)
    y = (jnp.einsum('blgp,gcp->blgc', x_r, c_re.astype(F32))
         - jnp.einsum('blgp,gcp->blgc', x_i, c_im.astype(F32)))
    y = y.reshape(Bsz, L, BRANCH) + d_skip.astype(F32) * uf
    g = jax.nn.gelu(y)
    out = g * jax.nn.sigmoid(g @ glu_w.astype(F32) + glu_b.astype(F32))
    return out.astype(u.dtype)


def _mlstm_chunkwise(q, k, v, log_i, log_f):
    Bsz, H, L, dh = q.shape
    nc = L // ML_CHUNK

    def chunks(t):
        t = t.reshape((Bsz, H, nc, ML_CHUNK) + t.shape[3:])
        return jnp.moveaxis(t, 2, 0)

    causal = jnp.tril(jnp.ones((ML_CHUNK, ML_CHUNK), dtype=bool))

    def step(carry, inp):
        C, n, m = carry
        qc, kc, vc, li, lf = inp
        b = jnp.cumsum(lf, axis=-1)
        logD = jnp.where(causal, b[..., :, None] - b[..., None, :] + li[..., None, :], -jnp.inf)
        inter = b + m[..., None]
        m_t = jnp.maximum(inter, jnp.max(logD, axis=-1))
        dmat = jnp.exp(logD - m_t[..., None])
        sc = jnp.exp(inter - m_t)
        s = jnp.einsum('bhtd,bhsd->bhts', qc, kc) * dmat
        num = sc[..., None] * jnp.einsum('bhtd,bhed->bhte', qc, C) + jnp.einsum('bhts,bhse->bhte', s, vc)
        dnm = sc * jnp.einsum('bhtd,bhd->bht', qc, n) + jnp.sum(s, axis=-1)
        h = num / jnp.maximum(jnp.abs(dnm), jnp.exp(-m_t))[..., None]
        b_last = b[..., -1]
        w_log = b_last[..., None] - b + li
        m_new = jnp.maximum(b_last + m, jnp.max(w_log, axis=-1))
        w = jnp.exp(w_log - m_new[..., None])
        decay = jnp.exp(b_last + m - m_new)
        C_new = decay[..., None, None] * C + jnp.einsum('bhs,bhse,bhsd->bhed', w, vc, kc)
        n_new = decay[..., None] * n + jnp.einsum('bhs,bhsd->bhd', w, kc)
        return (C_new, n_new, m_new), h

    init = (jnp.zeros((Bsz, H, dh, dh), F32), jnp.zeros((Bsz, H, dh), F32), jnp.zeros((Bsz, H), F32))
    _, h = lax.scan(step, init, tuple(map(chunks, (q, k, v, log_i, log_f))))
    return jnp.moveaxis(h, 0, 2).reshape(Bsz, H, L, dh)


def _mlstm_branch(xm, conv_w, conv_b, wq, wk, wv, gate_w, gate_b, norm_g, skip):
    Bsz, L, W = xm.shape
    xp = jnp.pad(xm, ((0, 0), (ML_CONV - 1, 0), (0, 0)))
    xc = conv_b + sum(xp[:, j:j + L] * conv_w[j] for j in range(ML_CONV))
    xc = jax.nn.silu(xc)
    xc_h = xc.reshape(Bsz, L, ML_HEADS, ML_HEAD_DIM)
    xm_h = xm.reshape(Bsz, L, ML_HEADS, ML_HEAD_DIM)
    q = jnp.einsum('blhd,hde->blhe', xc_h, wq)
    k = jnp.einsum('blhd,hde->blhe', xc_h, wk)
    v = jnp.einsum('blhd,hde->blhe', xm_h, wv)
    qkv = jnp.concatenate([q.reshape(Bsz, L, W), k.reshape(Bsz, L, W), v.reshape(Bsz, L, W)], axis=-1)
    gates = (qkv @ gate_w + gate_b).astype(F32)
    log_i = jnp.transpose(gates[..., :ML_HEADS], (0, 2, 1))
    log_f = jnp.transpose(jax.nn.log_sigmoid(gates[..., ML_HEADS:]), (0, 2, 1))

    def to_bhl(t):
        return jnp.transpose(t.astype(F32), (0, 2, 1, 3))

    h = _mlstm_chunkwise(to_bhl(q), to_bhl(k) * (ML_HEAD_DIM ** -0.5), to_bhl(v), log_i, log_f)
    mu = jnp.mean(h, axis=-1, keepdims=True)
    var = jnp.mean(jnp.square(h - mu), axis=-1, keepdims=True)
    h = (h - mu) * lax.rsqrt(var + HEAD_NORM_EPS)
    h = jnp.transpose(h, (0, 2, 1, 3)).reshape(Bsz, L, W) * norm_g.astype(F32)
    return (h + skip.astype(F32) * xc.astype(F32)).astype(xm.dtype)


def _strided_window_attention(q, k, v, window, dilation):
    Bsz, L, H, dh = q.shape
    n_back = window // dilation
    bs = n_back
    n_sub = -(-L // dilation)
    n_blk = -(-n_sub // bs)
    pad = dilation * n_blk * bs - L

    def to_blocks(t):
        t = jnp.pad(t, ((0, 0), (0, pad), (0, 0), (0, 0)))
        return t.reshape(Bsz, n_blk, bs, dilation, H, dh)

    def with_prev(t):
        prev = jnp.concatenate([jnp.zeros_like(t[:, :1]), t[:, :-1]], axis=1)
        return jnp.concatenate([prev, t], axis=2)

    qb = to_blocks(q)
    kw = with_prev(to_blocks(k))
    vw = with_prev(to_blocks(v)).astype(F32)
    s = jnp.einsum('bnirhd,bnjrhd->bnrhij', qb, kw).astype(F32) * (dh ** -0.5)
    i_rel = jnp.arange(bs)[:, None]
    j_rel = jnp.arange(2 * bs)[None, :]
    delta = bs + i_rel - j_rel
    band = (delta >= 0) & (delta <= n_back)
    valid = (jnp.arange(n_blk)[:, None, None] > 0) | (j_rel[None] >= bs)
    mask = band[None] & valid
    s = jnp.where(mask[None, :, None, None], s, -jnp.inf)
    m = jnp.max(s, axis=-1, keepdims=True)
    p = jnp.exp(s - m)
    den = jnp.sum(p, axis=-1)
    o = jnp.einsum('bnrhij,bnjrhd->bnirhd', p, vw) / jnp.transpose(den, (0, 1, 4, 2, 3))[..., None]
    lse = jnp.transpose(m[..., 0] + jnp.log(den), (0, 1, 4, 2, 3))
    o = o.reshape(Bsz, -1, H, dh)[:, :L]
    lse = lse.reshape(Bsz, -1, H)[:, :L]
    return o, lse


def _dilated_mixture(q, k, v):
    outs, lses = [], []
    for window, dilation in DIL_PAIRS:
        o, lse = _strided_window_attention(q, k, v, window, dilation)
        outs.append(o)
        lses.append(lse)
    wts = jax.nn.softmax(jnp.stack(lses, axis=0), axis=0)
    return jnp.sum(wts[..., None] * jnp.stack(outs, axis=0), axis=0)


def _diff_attention(q, k, v, lam):
    Bsz, L, H, _, dh = q.shape
    n_qb = L // Q_BLOCK
    k_pos = jnp.arange(L)
    vf = v.astype(F32)
    scale = dh ** -0.5

    def block(i):
        start = i * Q_BLOCK
        qb = lax.dynamic_slice_in_dim(q, start, Q_BLOCK, axis=1)
        s = jnp.einsum('bqhcd,bkhcd->bhcqk', qb, k).astype(F32) * scale
        q_pos = start + jnp.arange(Q_BLOCK)
        s = jnp.where(k_pos[None, :] <= q_pos[:, None], s, -jnp.inf)
        p = jax.nn.softmax(s, axis=-1)
        attn = p[:, :, 0] - lam * p[:, :, 1]
        return jnp.einsum('bhqk,bkhe->bqhe', attn, vf)

    o = lax.map(block, jnp.arange(n_qb))
    return jnp.moveaxis(o, 0, 1).reshape(Bsz, L, H, -1)


def _even_layer(z, w_in, lam_re, lam_im, log_dt, b_re, b_im, c_re, c_im, d_skip, glu_w, glu_b,
                conv_w, conv_b, wq, wk, wv, gate_w, gate_b, ml_norm, ml_skip, w_out):
    proj = z @ w_in
    s5_u, s5_z, ml_x, ml_z = jnp.split(proj, 4, axis=-1)
    a = _s5_branch(s5_u, lam_re, lam_im, log_dt, b_re, b_im, c_re, c_im, d_skip, glu_w, glu_b) * jax.nn.silu(s5_z)
    b = _mlstm_branch(ml_x, conv_w, conv_b, wq, wk, wv, gate_w, gate_b, ml_norm, ml_skip) * jax.nn.silu(ml_z)
    return jnp.concatenate([a, b], axis=-1) @ w_out


def _odd_layer(z, w_in, lq1, lk1, lq2, lk2, diff_norm, w_out, layer_idx):
    Bsz, L, _ = z.shape
    proj = z @ w_in
    cq, ck, cv, cz, dq, dk, dv, dz = jnp.split(proj, 8, axis=-1)
    pos = jnp.arange(L)

    def heads_c(t):
        return t.reshape(Bsz, L, DIL_HEADS, DIL_HEAD_DIM)

    def heads_d(t):
        return t.reshape(Bsz, L, DIFF_HEADS, 2, DIFF_HEAD_DIM)

    c_o = _dilated_mixture(_rotary(heads_c(cq), pos), _rotary(heads_c(ck), pos), heads_c(cv))
    c_out = c_o.reshape(Bsz, L, BRANCH).astype(z.dtype) * jax.nn.silu(cz)
    lam_init = 0.8 - 0.6 * math.exp(-0.3 * layer_idx)
    lam = (jnp.exp(jnp.sum(lq1.astype(F32) * lk1.astype(F32)))
           - jnp.exp(jnp.sum(lq2.astype(F32) * lk2.astype(F32))) + lam_init)
    d_o = _diff_attention(_rotary(heads_d(dq), pos), _rotary(heads_d(dk), pos),
                          dv.reshape(Bsz, L, DIFF_HEADS, 2 * DIFF_HEAD_DIM), lam)
    d_o = _rmsnorm(d_o, diff_norm, HEAD_NORM_EPS) * (1.0 - lam_init)
    d_out = d_o.reshape(Bsz, L, BRANCH).astype(z.dtype) * jax.nn.silu(dz)
    return jnp.concatenate([c_out, d_out], axis=-1) @ w_out


def setup_inputs(seed: int = 0) -> dict:
    key = jax.random.key(seed)
    ks = iter(jax.random.split(key, 40))

    def nrm(shape, std):
        return std * jax.random.normal(next(ks), shape, F32)

    G, P = S5_GROUPS, S5_STATE
    NE, NO = N_EVEN, N_ODD
    gate_b = jnp.concatenate([
        nrm((NE, ML_HEADS), 0.1),
        jnp.linspace(3.0, 6.0, ML_HEADS, dtype=F32)[None, :] + nrm((NE, ML_HEADS), 0.1)], axis=-1)
    return {
        'x': nrm((BATCH, SEQ, D_MODEL), 1.0),
        'pre_norm': 1.0 + nrm((DEPTH, D_MODEL), 0.1),
        'post_norm': 1.0 + nrm((DEPTH, D_MODEL), 0.1),
        'w_in_ab': nrm((NE, D_MODEL, 4 * BRANCH), D_MODEL ** -0.5),
        's5_lambda_re': -0.5 + nrm((NE, G, P), 0.02),
        's5_lambda_im': jnp.pi * jnp.arange(P, dtype=F32) + nrm((NE, G, P), 0.02),
        's5_log_dt': jax.random.uniform(next(ks), (NE, G), F32, math.log(S5_DT_MIN), math.log(S5_DT_MAX)),
        's5_b_re': nrm((NE, G, P, S5_GROUP), (2 * S5_GROUP) ** -0.5),
        's5_b_im': nrm((NE, G, P, S5_GROUP), (2 * S5_GROUP) ** -0.5),
        's5_c_re': nrm((NE, G, S5_GROUP, P), P ** -0.5),
        's5_c_im': nrm((NE, G, S5_GROUP, P), P ** -0.5),
        's5_d': nrm((NE, BRANCH), 1.0),
        's5_glu_w': nrm((NE, BRANCH, BRANCH), BRANCH ** -0.5),
        's5_glu_b': nrm((NE, BRANCH), 0.02),
        'ml_conv_w': nrm((NE, ML_CONV, BRANCH), ML_CONV ** -0.5),
        'ml_conv_b': nrm((NE, BRANCH), 0.02),
        'ml_wq': nrm((NE, ML_HEADS, ML_HEAD_DIM, ML_HEAD_DIM), ML_HEAD_DIM ** -0.5),
        'ml_wk': nrm((NE, ML_HEADS, ML_HEAD_DIM, ML_HEAD_DIM), ML_HEAD_DIM ** -0.5),
        'ml_wv': nrm((NE, ML_HEADS, ML_HEAD_DIM, ML_HEAD_DIM), ML_HEAD_DIM ** -0.5),
        'ml_gate_w': nrm((NE, 3 * BRANCH, 2 * ML_HEADS), 0.1 * (3 * BRANCH) ** -0.5),
        'ml_gate_b': gate_b,
        'ml_norm': 1.0 + nrm((NE, BRANCH), 0.1),
        'ml_skip': 1.0 + nrm((NE, BRANCH), 0.1),
        'w_out_ab': nrm((NE, 2 * BRANCH, D_MODEL), (2 * BRANCH) ** -0.5),
        'w_in_cd': nrm((NO, D_MODEL, 8 * BRANCH), D_MODEL ** -0.5),
        'diff_lq1': nrm((NO, DIFF_HEAD_DIM), 0.1),
        'diff_lk1': nrm((NO, DIFF_HEAD_DIM), 0.1),
        'diff_lq2': nrm((NO, DIFF_HEAD_DIM), 0.1),
        'diff_lk2': nrm((NO, DIFF_HEAD_DIM), 0.1),
        'diff_norm': 1.0 + nrm((NO, 2 * DIFF_HEAD_DIM), 0.1),
        'w_out_cd': nrm((NO, 2 * BRANCH, D_MODEL), (2 * BRANCH) ** -0.5),
    }


def reference(x, pre_norm, post_norm, w_in_ab, s5_lambda_re, s5_lambda_im, s5_log_dt, s5_b_re, s5_b_im,
              s5_c_re, s5_c_im, s5_d, s5_glu_w, s5_glu_b, ml_conv_w, ml_conv_b, ml_wq, ml_wk, ml_wv,
              ml_gate_w, ml_gate_b, ml_norm, ml_skip, w_out_ab, w_in_cd, diff_lq1, diff_lk1, diff_lq2,
              diff_lk2, diff_norm, w_out_cd):
    h = x
    for l in range(DEPTH):
        z = _rmsnorm(h, pre_norm[l])
        i = l // 2
        if l % 2 == 0:
            y = _even_layer(z, w_in_ab[i], s5_lambda_re[i], s5_lambda_im[i], s5_log_dt[i], s5_b_re[i],
                            s5_b_im[i], s5_c_re[i], s5_c_im[i], s5_d[i], s5_glu_w[i], s5_glu_b[i],
                            ml_conv_w[i], ml_conv_b[i], ml_wq[i], ml_wk[i], ml_wv[i], ml_gate_w[i],
                            ml_gate_b[i], ml_norm[i], ml_skip[i], w_out_ab[i])
        else:
            y = _odd_layer(z, w_in_cd[i], diff_lq1[i], diff_lk1[i], diff_lq2[i], diff_lk2[i],
                           diff_norm[i], w_out_cd[i], l)
        h = h + _rmsnorm(y, post_norm[l])
    return h
```

```python
import contextlib
import math
import numpy as np
import ml_dtypes
import concourse.bass as bass
import concourse.mybir as mybir
from concourse.bass_utils import run_bass_kernel_spmd

F32 = mybir.dt.float32
BF16 = mybir.dt.bfloat16
I32 = mybir.dt.int32
AF = mybir.ActivationFunctionType
ALU = mybir.AluOpType
AX = mybir.AxisListType

D = 1024
S = 2048
BR = 512
NCORES = 8
NORM_EPS = 1e-6
HEAD_EPS = 1e-5


class Ctx:
    def __init__(self, nc, stack, n_dma_sems=48, same_engine_sync=True):
        self.nc = nc
        self.eng = {"pe": nc.tensor, "act": nc.scalar, "dve": nc.vector,
                    "pool": nc.gpsimd, "sp": nc.sync}
        self.sem = {}
        self.cnt = {}
        for k in ("pe", "act", "dve", "pool"):
            self.sem[k] = stack.enter_context(nc.semaphore("s_" + k))
            self.cnt[k] = 0
        self.dma_sems = []
        for i in range(n_dma_sems):
            k = "dma%d" % i
            self.sem[k] = stack.enter_context(nc.semaphore("s_" + k))
            self.cnt[k] = 0
            self.dma_sems.append(k)
        self.dma_rr = 0
        self.waited = {k: {} for k in self.eng}
        self.last_w = {}
        self.readers = {}
        self.same_engine_sync = same_engine_sync
        self.n_instr = 0
        self.n_wait = 0

    def _deps(self, r, w):
        deps = []
        for x in r:
            if x in self.last_w:
                deps.append(self.last_w[x])
            if x.startswith("ps"):
                deps.extend(self.readers.get(x, ()))
        for x in w:
            if x in self.last_w:
                deps.append(self.last_w[x])
            deps.extend(self.readers.get(x, ()))
        return deps

    def _wait(self, e, deps):
        need = {}
        for (k, v) in deps:
            if k == e and (e == "pe" or not self.same_engine_sync):
                continue
            if need.get(k, 0) < v:
                need[k] = v
        for k, v in need.items():
            if self.waited[e].get(k, 0) >= v:
                continue
            self.eng[e].wait_ge(self.sem[k], v)
            self.waited[e][k] = v
            self.n_wait += 1

    def _commit(self, tok, r, w):
        for x in w:
            self.last_w[x] = tok
            self.readers[x] = []
        for x in r:
            if x in w:
                continue
            self.readers.setdefault(x, []).append(tok)

    def op(self, e, fn, r=(), w=()):
        self._wait(e, self._deps(r, w))
        ins = fn(self.eng[e])
        self.cnt[e] += 1
        ins.then_inc(self.sem[e], 1)
        self._commit((e, self.cnt[e]), r, w)
        self.n_instr += 1
        return ins

    def dma(self, out, in_, r=(), w=(), q="sp", **kw):
        k = self.dma_sems[self.dma_rr]
        self.dma_rr = (self.dma_rr + 1) % len(self.dma_sems)
        deps = self._deps(r, w)
        if self.cnt[k] > 0:
            deps.append((k, self.cnt[k]))
        self._wait(q, deps)
        ins = self.eng[q].dma_start(out=out, in_=in_, **kw)
        self.cnt[k] += 16
        ins.then_inc(self.sem[k], 16)
        self._commit((k, self.cnt[k]), r, w)
        self.n_instr += 1
        return ins

    def barrier(self):
        deps = [(k, v) for k, v in self.cnt.items() if v > 0]
        for e in self.eng:
            self._wait(e, deps)

    def finish(self, res):
        deps = [self.last_w[x] for x in res if x in self.last_w]
        self._wait("sp", deps)


class K:
    def __init__(self, nseq=2, export=(), phases=None, seqlen=S):
        self.nseq = nseq
        self.S = seqlen
        self.NT = nseq * seqlen
        self.export = set(export)
        self.phases = phases
        self.nc = bass.Bass("TRN2", target_bir_lowering=False)
        self.inputs = {}
        self.outputs = {}
        self.s5_main_enabled = True
        self._uid = 0

    def sbt(self, name, shape, dt):
        self._uid += 1
        return self.nc.sbuf_tensor("%s_u%d" % (name, self._uid), list(shape), dt)

    def din(self, name, shape, dt=F32):
        ap = self.nc.dram_tensor(name, list(shape), dt, kind="ExternalInput").ap()
        self.inputs[name] = ap
        return ap

    def dscr(self, name, shape, dt):
        kind = "ExternalOutput" if name in self.export else "Internal"
        ap = self.nc.dram_tensor(name, list(shape), dt, kind=kind).ap()
        if kind == "ExternalOutput":
            self.outputs[name] = ap
        return ap

    @contextlib.contextmanager
    def scope(self):
        with contextlib.ExitStack() as st:
            yield st
            self.c.barrier()

    def build(self):
        nc = self.nc
        NT = self.NT
        with contextlib.ExitStack() as st:
            self.c = Ctx(nc, st)
            self.ps = [st.enter_context(nc.psum_tensor("ps%d" % i, [128, 512], F32)) for i in range(8)]
            self.x = self.din("x", [NT, D])
            self.ident_d = self.din("ident", [128, 128], BF16)
            self.pre0 = self.din("pre0", [128, 8])
            self.w_in_ab = self.din("w_in_ab", [D, 4 * BR])
            for nm in ("s5_lr", "s5_li", "s5_ldt"):
                setattr(self, nm, self.din(nm, [64, 32]))
            for nm in ("s5_br", "s5_bi", "s5_cr", "s5_ci"):
                setattr(self, nm, self.din(nm, [64, 32, 16]))
            self.tv_d = self.din("tv", [64, 16])
            self.kv_d = self.din("kv", [64, 256])
            self.toepmask_d = self.din("toepmask", [128, 128])
            self.identf_d = self.din("identf", [128, 128])
            self.s5d_rep = self.din("s5d_rep", [128, BR])
            self.glub_rep = self.din("glub_rep", [128, BR])
            self.glu_w = self.din("glu_w", [BR, BR])
            self.ml_cw = self.din("ml_cw", [128, 4, 4]); self.ml_cb = self.din("ml_cb", [128, 4])
            self.ml_mn = self.din("ml_mn", [128, 4]); self.ml_msk = self.din("ml_msk", [128, 4])
            self.ml_gbi = self.din("ml_gbi", [4, 1]); self.ml_gbf = self.din("ml_gbf", [4, 1])
            self.ml_maskS = self.din("ml_maskS", [128, 128])
            self.ml_gw = self.din("ml_gw", [128, 12, 8])
            self.ml_wq = self.din("ml_wq", [128, 4, 128]); self.ml_wk = self.din("ml_wk", [128, 4, 128]); self.ml_wv = self.din("ml_wv", [128, 4, 128])
            self.bT = self.dscr("bT", [BR, NT], BF16)
            self.w_out_ab = self.din("w_out_ab", [D, D]); self.post0_rep = self.din("post0_rep", [128, D])
            self.out = self.nc.dram_tensor("out", [NT, D], F32, kind="ExternalOutput").ap()
            self.outputs["out"] = self.out
            self.h1src = self.out
            if self.phases is not None and "l0o" not in self.phases:
                self.h1src = self.din("h1_in", [NT, D])
            self.pre1 = self.din("pre1", [128, 8]); self.w_in_cd = self.din("w_in_cd", [D, 8 * BR])
            self.rope_cos = self.din("rope_cos", [128, self.S]); self.rope_sin = self.din("rope_sin", [128, self.S])
            self.rope_rm = self.din("rope_rm", [128, 128], BF16)
            for nm in ("cqT", "ckT", "dqT", "dkT", "czT", "dzT"):
                setattr(self, nm, self.dscr(nm, [BR, NT], BF16))
            for nm in ("cv_tok", "dv_tok", "oc_tok", "od_tok"):
                setattr(self, nm, self.dscr(nm, [NT, BR], BF16))
            self.dil_masks = self.din("dil_masks", [128, 9, 512], BF16); self.diff_masks = self.din("diff_masks", [128, 4, 512], BF16)
            self.diff_lqk = self.din("diff_lqk", [1, 4, 64]); self.diffnorm_rep = self.din("diffnorm_rep", [128, 128])
            self.w_out_cd = self.din("w_out_cd", [D, D]); self.post1_rep = self.din("post1_rep", [128, D])
            self.rotc = self.dscr("rotc", [64, 32, 256], F32)
            self.rots = self.dscr("rots", [64, 32, 256], F32)
            self.rhot = self.dscr("rhot", [64, 32, 256], F32)
            self.toep_x = self.dscr("toep_x", [128, 32, 128], BF16)
            self.wii_x = self.dscr("wii_x", [128, 32, 2, 64], BF16)
            self.wiv_x = self.dscr("wiv_x", [64, 2, 32, 128], BF16)
            self.a_tok = self.dscr("a_tok", [NT, BR], BF16)
            self.u_tok = self.dscr("u_tok", [NT, BR], BF16)
            self.sz_tok = self.dscr("sz_tok", [NT, BR], BF16)
            self.xmT = self.dscr("xmT", [BR, NT], BF16)
            self.mzT = self.dscr("mzT", [BR, NT], BF16)
            self.ident = st.enter_context(nc.sbuf_tensor("identb", [128, 128], BF16))
            self.c.dma(self.ident[:], self.ident_d, w=["ident"])
            ph = self.phases
            fin = []
            if ph is None or "l0p" in ph:
                self.phase_l0_proj()
                fin += ["u_tok", "sz_tok", "xmT", "mzT"]
            if ph is None or "s5" in ph:
                self.phase_s5()
                fin += ["a_tok", "rotc", "rots", "rhot", "toep_x", "wii_x", "wiv_x"]
            if ph is None or "ml" in ph:
                self.phase_ml()
                fin += ["bT"]
            if ph is None or "l0o" in ph:
                self.phase_l0_out()
                fin += ["h1_%d" % b for b in range(NT // 128)]
            if ph is None or "l1p" in ph:
                self.phase_l1_proj()
                fin += ["cqT", "ckT", "dqT", "dkT", "czT", "dzT", "cv_tok", "dv_tok"]
            if ph is None or "adil" in ph:
                self.phase_attn("dil")
                fin += ["oc_tok"]
            if ph is None or "adiff" in ph:
                self.phase_attn("diff")
                fin += ["od_tok"]
            if ph is None or "l1o" in ph:
                self.phase_l1_out()
                fin += ["h1_%d" % b for b in range(NT // 128)]
            self.c.finish(fin)
            self.c.barrier()
        return nc

    def rmsnorm_T(self, st, xsrc_rows, nblk, zT, zkey, tagp, ps_tr):
        raise NotImplementedError

    def phase_l0_proj(self):
        nc, c = self.nc, self.c
        NT = self.NT
        ngrp = NT // 512
        with self.scope() as st:
            T = lambda name, shape, dt: st.enter_context(self.sbt(name, shape, dt))
            win = T("win0", [128, 8, 4 * BR], BF16)
            g0 = T("g0", [128, 8], F32)
            stage = [T("wst%d" % i, [128, 4 * BR], F32) for i in range(2)]
            c.dma(g0[:], self.pre0, w=["g0"])
            for dc in range(8):
                sk = "wst%d" % (dc % 2)
                c.dma(stage[dc % 2][:], self.w_in_ab[dc * 128:(dc + 1) * 128, :], w=[sk])
                c.op("act", lambda e: e.activation(out=win[:, dc, :], in_=stage[dc % 2][:], func=AF.Copy,
                                                   scale=g0[:, dc:dc + 1]), r=[sk, "g0"], w=["win0"])
            NXB = 6
            xt = [T("xt%d" % i, [128, D], F32) for i in range(NXB)]
            junk = T("junk", [128, D], F32)
            ss = T("ss", [128, 4], F32)
            rstd = T("rstd", [128, 4], F32)
            zb = [T("zb%d" % i, [128, D], BF16) for i in range(2)]
            zT = [T("zT%d" % i, [128, 8, 512], BF16) for i in range(2)]
            uo = [T("uo%d" % i, [128, BR], BF16) for i in range(2)]
            so = [T("so%d" % i, [128, BR], BF16) for i in range(2)]
            xmo = [T("xmo%d" % i, [128, 4, 512], BF16) for i in range(2)]
            mzo = [T("mzo%d" % i, [128, 4, 512], BF16) for i in range(2)]
            nblk = NT // 128

            def load_x(b):
                if b < nblk:
                    c.dma(xt[b % NXB][:], self.x[b * 128:(b + 1) * 128, :], w=["xt%d" % (b % NXB)])
            for b in range(4):
                load_x(b)
            for g in range(ngrp):
                zk = "zT%d" % (g % 2)
                for j in range(4):
                    b = g * 4 + j
                    xk = "xt%d" % (b % NXB)
                    c.op("act", lambda e: e.activation(out=junk[:], in_=xt[b % NXB][:], func=AF.Square,
                                                       accum_out=ss[:, j:j + 1]), r=[xk], w=["junk", "ss"])
                c.op("act", lambda e: e.activation(out=rstd[:], in_=ss[:], func=AF.Ln, scale=1.0 / D, bias=NORM_EPS),
                     r=["ss"], w=["rstd"])
                c.op("act", lambda e: e.activation(out=rstd[:], in_=rstd[:], func=AF.Exp, scale=-0.5),
                     r=["rstd"], w=["rstd"])
                for j in range(4):
                    b = g * 4 + j
                    xk = "xt%d" % (b % NXB)
                    zbk = "zb%d" % (b % 2)
                    pst = self.ps[b % 2]
                    pk = "ps%d" % (b % 2)
                    c.op("dve", lambda e: e.tensor_scalar(out=zb[b % 2][:], in0=xt[b % NXB][:], scalar1=rstd[:, j:j + 1],
                                                          scalar2=None, op0=ALU.mult), r=[xk, "rstd"], w=[zbk])
                    load_x(b + 4)
                    ptv = pst[:].bitcast(BF16).rearrange("p (a b) -> p a b", a=8)
                    for dc in range(8):
                        c.op("pe", lambda e: e.transpose(out=ptv[:, dc, :], in_=zb[b % 2][:, dc * 128:(dc + 1) * 128],
                                                         identity=self.ident[:]), r=[zbk, "ident"], w=[pk])
                    c.op("dve", lambda e: e.tensor_copy(out=zT[g % 2][:, :, j * 128:(j + 1) * 128], in_=ptv),
                         r=[pk], w=[zk])
                    for dc in range(8):
                        c.op("pe", lambda e: e.matmul(out=self.ps[2][:], lhsT=zT[g % 2][:, dc, j * 128:(j + 1) * 128],
                                                      rhs=win[:, dc, 0:BR], start=(dc == 0), stop=(dc == 7)),
                             r=[zk, "win0"], w=["ps2"])
                    c.op("act", lambda e: e.copy(out=uo[b % 2][:], in_=self.ps[2][:]), r=["ps2"], w=["uo%d" % (b % 2)])
                    c.dma(self.u_tok[b * 128:(b + 1) * 128, :], uo[b % 2][:], r=["uo%d" % (b % 2)], w=["u_tok"])
                    for dc in range(8):
                        c.op("pe", lambda e: e.matmul(out=self.ps[3][:], lhsT=zT[g % 2][:, dc, j * 128:(j + 1) * 128],
                                                      rhs=win[:, dc, BR:2 * BR], start=(dc == 0), stop=(dc == 7)),
                             r=[zk, "win0"], w=["ps3"])
                    c.op("act", lambda e: e.activation(out=so[b % 2][:], in_=self.ps[3][:], func=AF.Silu),
                         r=["ps3"], w=["so%d" % (b % 2)])
                    c.dma(self.sz_tok[b * 128:(b + 1) * 128, :], so[b % 2][:], r=["so%d" % (b % 2)], w=["sz_tok"])
                for fc in range(8):
                    pb = 4 + fc % 4
                    for dc in range(8):
                        c.op("pe", lambda e: e.matmul(out=self.ps[pb][:], lhsT=win[:, dc, 2 * BR + fc * 128:2 * BR + (fc + 1) * 128],
                                                      rhs=zT[g % 2][:, dc, :], start=(dc == 0), stop=(dc == 7)),
                             r=[zk, "win0"], w=["ps%d" % pb])
                    if fc < 4:
                        c.op("dve", lambda e: e.tensor_copy(out=xmo[g % 2][:, fc, :], in_=self.ps[pb][:]),
                             r=["ps%d" % pb], w=["xmo%d" % (g % 2)])
                    else:
                        c.op("act", lambda e: e.activation(out=mzo[g % 2][:, fc - 4, :], in_=self.ps[pb][:], func=AF.Silu),
                             r=["ps%d" % pb], w=["mzo%d" % (g % 2)])
                c.dma(self.xmT.rearrange("(c p) t -> p c t", p=128)[:, :, g * 512:(g + 1) * 512], xmo[g % 2][:],
                      r=["xmo%d" % (g % 2)], w=["xmT"])
                c.dma(self.mzT.rearrange("(c p) t -> p c t", p=128)[:, :, g * 512:(g + 1) * 512], mzo[g % 2][:],
                      r=["mzo%d" % (g % 2)], w=["mzT"])
        c.barrier()

    def tt(self, e, out, a, b, op, r, w):
        return self.c.op(e, lambda en: en.tensor_tensor(out=out, in0=a, in1=b, op=op), r=r, w=w)

    def ts(self, e, out, a, s1, op0, r, w, s2=None, op1=None):
        if op1 is None:
            return self.c.op(e, lambda en: en.tensor_scalar(out=out, in0=a, scalar1=s1, scalar2=None, op0=op0), r=r, w=w)
        return self.c.op(e, lambda en: en.tensor_scalar(out=out, in0=a, scalar1=s1, scalar2=s2, op0=op0, op1=op1), r=r, w=w)

    def stt(self, e, out, a, s, b, op0, op1, r, w):
        return self.c.op(e, lambda en: en.scalar_tensor_tensor(out=out, in0=a, scalar=s, in1=b, op0=op0, op1=op1), r=r, w=w)

    def actf(self, out, in_, func, r, w, **kw):
        return self.c.op("act", lambda en: en.activation(out=out, in_=in_, func=func, **kw), r=r, w=w)

    def cp(self, e, out, in_, r, w):
        if e == "act":
            return self.c.op("act", lambda en: en.copy(out=out, in_=in_), r=r, w=w)
        return self.c.op(e, lambda en: en.tensor_copy(out=out, in_=in_), r=r, w=w)

    def sincos(self, ang, akey, sin_o, cos_o, skey, ckey, tf, ti, red_o=None):
        C1 = 6.28125
        C2 = 2 * math.pi - C1
        for (off, out, okey) in ((0.0, sin_o, skey), (math.pi / 2, cos_o, ckey)):
            if out is None:
                continue
            self.ts("dve", tf, ang, 1.0 / (2 * math.pi), ALU.mult, r=[akey], w=["sc_tf"], s2=off / (2 * math.pi), op1=ALU.add)
            self.cp("dve", ti, tf, r=["sc_tf"], w=["sc_ti"])
            self.cp("dve", tf, ti, r=["sc_ti"], w=["sc_tf"])
            self.stt("dve", out, tf, -C1, ang, ALU.mult, ALU.add, r=["sc_tf", akey], w=[okey])
            self.stt("dve", out, tf, -C2, out, ALU.mult, ALU.add, r=["sc_tf", okey], w=[okey])
            if off != 0.0:
                self.ts("dve", out, out, off, ALU.add, r=[okey], w=[okey])
            self.ts("dve", out, out, math.pi, ALU.min, r=[okey], w=[okey], s2=-math.pi, op1=ALU.max)
            if red_o is not None and off == 0.0:
                self.cp("dve", red_o[0], out, r=[okey], w=[red_o[1]])
            self.actf(out, out, AF.Sin, r=[okey], w=[okey])

    def s5_precompute(self, st, Toep, Wii, WivR, WivI):
        nc, c = self.nc, self.c
        so = contextlib.ExitStack()
        TO = lambda name, shape, dt=F32: so.enter_context(self.sbt(name, shape, dt))
        thr = TO("p_thr", [64, 32]); rho8 = TO("p_rho8", [64, 32]); kv = TO("p_kv", [64, 256])
        tfs = TO("p_tfs", [64, 32]); tis = TO("p_tis", [64, 32], I32)
        with self.scope() as sp:
            T = lambda name, shape, dt=F32: sp.enter_context(self.sbt(name, shape, dt))
            lr = T("p_lr", [64, 32]); li = T("p_li", [64, 32]); ldt = T("p_ldt", [64, 32])
            br = T("p_br", [64, 32, 16]); bi = T("p_bi", [64, 32, 16])
            cr = T("p_cr", [64, 32, 16]); ci = T("p_ci", [64, 32, 16])
            tv = T("p_tv", [64, 16])
            msk = T("p_msk", [128, 128]); idf = T("p_idf", [128, 128])
            for (t, d, key) in ((lr, self.s5_lr, "p_lr"), (li, self.s5_li, "p_li"), (ldt, self.s5_ldt, "p_ldt"),
                                (br, self.s5_br, "p_br"), (bi, self.s5_bi, "p_bi"), (cr, self.s5_cr, "p_cr"),
                                (ci, self.s5_ci, "p_ci"), (tv, self.tv_d, "p_tv"), (kv, self.kv_d, "p_kv"),
                                (msk, self.toepmask_d, "p_msk"), (idf, self.identf_d, "p_idf")):
                c.dma(t[:], d, w=[key])
            dt = T("p_dt", [64, 32]); lrdt = T("p_lrdt", [64, 32]); th = T("p_th", [64, 32])
            s0 = T("p_s0", [64, 32]); c0 = T("p_c0", [64, 32]); mag = T("p_mag", [64, 32])
            self.actf(dt[:], ldt[:], AF.Exp, r=["p_ldt"], w=["p_dt"])
            self.tt("dve", lrdt[:], lr[:], dt[:], ALU.mult, r=["p_lr", "p_dt"], w=["p_lrdt"])
            self.tt("dve", th[:], li[:], dt[:], ALU.mult, r=["p_li", "p_dt"], w=["p_th"])
            self.sincos(th[:], "p_th", s0[:], c0[:], "p_s0", "p_c0", tfs[:], tis[:], red_o=(thr[:], "p_thr"))
            self.actf(mag[:], lrdt[:], AF.Exp, r=["p_lrdt"], w=["p_mag"])
            abr = T("p_abr", [64, 32]); abi = T("p_abi", [64, 32]); am1 = T("p_am1", [64, 32])
            self.tt("dve", abr[:], mag[:], c0[:], ALU.mult, r=["p_mag", "p_c0"], w=["p_abr"])
            self.tt("dve", abi[:], mag[:], s0[:], ALU.mult, r=["p_mag", "p_s0"], w=["p_abi"])
            self.ts("dve", am1[:], abr[:], -1.0, ALU.add, r=["p_abr"], w=["p_am1"])
            den = T("p_den", [64, 32]); t1 = T("p_t1", [64, 32]); t2 = T("p_t2", [64, 32])
            fr = T("p_fr", [64, 32]); fi = T("p_fi", [64, 32])
            self.tt("dve", den[:], lr[:], lr[:], ALU.mult, r=["p_lr"], w=["p_den"])
            self.tt("dve", t1[:], li[:], li[:], ALU.mult, r=["p_li"], w=["p_t1"])
            self.tt("dve", den[:], den[:], t1[:], ALU.add, r=["p_den", "p_t1"], w=["p_den"])
            c.op("dve", lambda e: e.reciprocal(out=den[:], in_=den[:]), r=["p_den"], w=["p_den"])
            self.tt("dve", t1[:], am1[:], lr[:], ALU.mult, r=["p_am1", "p_lr"], w=["p_t1"])
            self.tt("dve", t2[:], abi[:], li[:], ALU.mult, r=["p_abi", "p_li"], w=["p_t2"])
            self.tt("dve", t1[:], t1[:], t2[:], ALU.add, r=["p_t1", "p_t2"], w=["p_t1"])
            self.tt("dve", fr[:], t1[:], den[:], ALU.mult, r=["p_t1", "p_den"], w=["p_fr"])
            self.tt("dve", t1[:], abi[:], lr[:], ALU.mult, r=["p_abi", "p_lr"], w=["p_t1"])
            self.tt("dve", t2[:], am1[:], li[:], ALU.mult, r=["p_am1", "p_li"], w=["p_t2"])
            self.tt("dve", t1[:], t1[:], t2[:], ALU.subtract, r=["p_t1", "p_t2"], w=["p_t1"])
            self.tt("dve", fi[:], t1[:], den[:], ALU.mult, r=["p_t1", "p_den"], w=["p_fi"])
            Bbr = T("p_Bbr", [64, 32, 16]); Bbi = T("p_Bbi", [64, 32, 16])
            u1 = T("p_u1", [64, 32, 16]); u2 = T("p_u2", [64, 32, 16])
            bc16 = lambda a: a.unsqueeze(2).broadcast_to([64, 32, 16])
            self.tt("dve", u1[:], br[:], bc16(fr[:]), ALU.mult, r=["p_br", "p_fr"], w=["p_u1"])
            self.tt("dve", u2[:], bi[:], bc16(fi[:]), ALU.mult, r=["p_bi", "p_fi"], w=["p_u2"])
            self.tt("dve", Bbr[:], u1[:], u2[:], ALU.subtract, r=["p_u1", "p_u2"], w=["p_Bbr"])
            self.tt("dve", u1[:], bi[:], bc16(fr[:]), ALU.mult, r=["p_bi", "p_fr"], w=["p_u1"])
            self.tt("dve", u2[:], br[:], bc16(fi[:]), ALU.mult, r=["p_br", "p_fi"], w=["p_u2"])
            self.tt("dve", Bbi[:], u1[:], u2[:], ALU.add, r=["p_u1", "p_u2"], w=["p_Bbi"])
            TE = T("p_TE", [64, 16, 32]); TA = T("p_TA", [64, 16, 32])
            PWr = T("p_PWr", [64, 16, 32]); PWi = T("p_PWi", [64, 16, 32])
            tf3 = T("p_tf3", [64, 16, 32]); ti3 = T("p_ti3", [64, 16, 32], I32)
            bt = lambda a: a.unsqueeze(1).broadcast_to([64, 16, 32])
            bg = lambda a: a.unsqueeze(2).broadcast_to([64, 16, 32])
            self.tt("dve", TE[:], bt(lrdt[:]), bg(tv[:]), ALU.mult, r=["p_lrdt", "p_tv"], w=["p_TE"])
            self.actf(TE[:], TE[:], AF.Exp, r=["p_TE"], w=["p_TE"])
            self.tt("dve", TA[:], bt(thr[:]), bg(tv[:]), ALU.mult, r=["p_thr", "p_tv"], w=["p_TA"])
            self.sincos(TA[:], "p_TA", PWi[:], PWr[:], "p_PWi", "p_PWr", tf3[:], ti3[:])
            self.tt("dve", PWr[:], PWr[:], TE[:], ALU.mult, r=["p_PWr", "p_TE"], w=["p_PWr"])
            self.tt("dve", PWi[:], PWi[:], TE[:], ALU.mult, r=["p_PWi", "p_TE"], w=["p_PWi"])
            HsR = T("p_HsR", [64, 32, 8, 16]); HsI = T("p_HsI", [64, 32, 8, 16])
            v1 = T("p_v1", [64, 32, 9, 16]); v2 = T("p_v2", [64, 32, 9, 16])
            def pw_b(PW, j0, n):
                return PW[:, j0:j0 + n, :].rearrange("p t g -> p g t").unsqueeze(3).broadcast_to([64, 32, n, 16])
            def x_b(x, n):
                return x.unsqueeze(2).broadcast_to([64, 32, n, 16])
            self.tt("dve", v1[:, :, 0:8, :], pw_b(PWr, 0, 8), x_b(Bbr[:], 8), ALU.mult, r=["p_PWr", "p_Bbr"], w=["p_v1"])
            self.tt("dve", v2[:, :, 0:8, :], pw_b(PWi, 0, 8), x_b(Bbi[:], 8), ALU.mult, r=["p_PWi", "p_Bbi"], w=["p_v2"])
            self.tt("dve", HsR[:], v1[:, :, 0:8, :], v2[:, :, 0:8, :], ALU.subtract, r=["p_v1", "p_v2"], w=["p_HsR"])
            self.tt("dve", v1[:, :, 0:8, :], pw_b(PWr, 0, 8), x_b(Bbi[:], 8), ALU.mult, r=["p_PWr", "p_Bbi"], w=["p_v1"])
            self.tt("dve", v2[:, :, 0:8, :], pw_b(PWi, 0, 8), x_b(Bbr[:], 8), ALU.mult, r=["p_PWi", "p_Bbr"], w=["p_v2"])
            self.tt("dve", HsI[:], v1[:, :, 0:8, :], v2[:, :, 0:8, :], ALU.add, r=["p_v1", "p_v2"], w=["p_HsI"])
            LtR = T("p_LtR", [64, 32, 9, 16]); nLtI = T("p_nLtI", [64, 32, 9, 16])
            for (s0_, j0, n) in ((0, 0, 1), (1, 8, 8)):
                sl = slice(s0_, s0_ + n)
                self.tt("dve", v1[:, :, sl, :], pw_b(PWr, j0, n), x_b(cr[:], n), ALU.mult, r=["p_PWr", "p_cr"], w=["p_v1"])
                self.tt("dve", v2[:, :, sl, :], pw_b(PWi, j0, n), x_b(ci[:], n), ALU.mult, r=["p_PWi", "p_ci"], w=["p_v2"])
                self.tt("dve", LtR[:, :, sl, :], v1[:, :, sl, :], v2[:, :, sl, :], ALU.subtract, r=["p_v1", "p_v2"], w=["p_LtR"])
                self.tt("dve", v1[:, :, sl, :], pw_b(PWi, j0, n), x_b(cr[:], n), ALU.mult, r=["p_PWi", "p_cr"], w=["p_v1"])
                self.tt("dve", v2[:, :, sl, :], pw_b(PWr, j0, n), x_b(ci[:], n), ALU.mult, r=["p_PWr", "p_ci"], w=["p_v2"])
                self.tt("dve", v1[:, :, sl, :], v1[:, :, sl, :], v2[:, :, sl, :], ALU.add, r=["p_v1", "p_v2"], w=["p_v1"])
                self.ts("dve", nLtI[:, :, sl, :], v1[:, :, sl, :], -1.0, ALU.mult, r=["p_v1"], w=["p_nLtI"])
            self.cp("dve", WivR[:], LtR[:, :, 1:9, :].rearrange("p g t c -> p g (t c)"), r=["p_LtR"], w=["WivR"])
            self.cp("dve", WivI[:], nLtI[:, :, 1:9, :].rearrange("p g t c -> p g (t c)"), r=["p_nLtI"], w=["WivI"])
            for g4 in range(8):
                pb = g4 % 2
                pk = "ps%d" % pb
                for gl in range(4):
                    g = g4 * 4 + gl
                    o = self.ps[pb][:, gl * 128:(gl + 1) * 128]
                    c.op("pe", lambda e: e.matmul(out=o, lhsT=HsR[:, g, :, :].rearrange("p s c -> p (s c)"),
                                                  rhs=LtR[:, g, 0:8, :].rearrange("p t c -> p (t c)"), start=True, stop=False),
                         r=["p_HsR", "p_LtR"], w=[pk])
                    c.op("pe", lambda e: e.matmul(out=o, lhsT=HsI[:, g, :, :].rearrange("p s c -> p (s c)"),
                                                  rhs=nLtI[:, g, 0:8, :].rearrange("p t c -> p (t c)"), start=False, stop=True),
                         r=["p_HsI", "p_nLtI"], w=[pk])
                self.tt("dve", Toep[:, g4 * 4:(g4 + 1) * 4, :], self.ps[pb][:].rearrange("p (g n) -> p g n", g=4),
                        msk[:].unsqueeze(1).broadcast_to([128, 4, 128]), ALU.mult, r=[pk, "p_msk"], w=["Toep"])
            GsR = LtR[:, :, 0:8, :].rearrange("p g t c -> p g (t c)")
            GsI = nLtI[:, :, 0:8, :].rearrange("p g t c -> p g (t c)")
            w1 = v1[:, :, 0:8, :].rearrange("p g t c -> p g (t c)")
            w2 = v2[:, :, 0:8, :].rearrange("p g t c -> p g (t c)")
            p7r = PWr[:, 14, :].unsqueeze(2).broadcast_to([64, 32, 128])
            p7i = PWi[:, 14, :].unsqueeze(2).broadcast_to([64, 32, 128])
            hr = HsR[:].rearrange("p g s c -> p g (s c)"); hi = HsI[:].rearrange("p g s c -> p g (s c)")
            self.tt("dve", w1, hr, p7r, ALU.mult, r=["p_HsR", "p_PWr"], w=["p_v1"])
            self.tt("dve", w2, hi, p7i, ALU.mult, r=["p_HsI", "p_PWi"], w=["p_v2"])
            self.tt("dve", GsR, w1, w2, ALU.subtract, r=["p_v1", "p_v2"], w=["p_LtR"])
            self.tt("dve", w1, hi, p7r, ALU.mult, r=["p_HsI", "p_PWr"], w=["p_v1"])
            self.tt("dve", w2, hr, p7i, ALU.mult, r=["p_HsR", "p_PWi"], w=["p_v2"])
            self.tt("dve", GsI, w1, w2, ALU.add, r=["p_v1", "p_v2"], w=["p_nLtI"])
            for g4 in range(8):
                pb = 2 + g4 % 2
                pk = "ps%d" % pb
                pv = self.ps[pb][:].rearrange("p (g r n) -> p g r n", g=4, r=2)
                for gl in range(4):
                    g = g4 * 4 + gl
                    c.op("pe", lambda e: e.transpose(out=pv[:, gl, 0, :], in_=GsR[:, g, :], identity=idf[0:64, 0:64]),
                         r=["p_LtR", "p_idf"], w=[pk])
                    c.op("pe", lambda e: e.transpose(out=pv[:, gl, 1, :], in_=GsI[:, g, :], identity=idf[0:64, 0:64]),
                         r=["p_nLtI", "p_idf"], w=[pk])
                self.cp("act", Wii[:, g4 * 4:(g4 + 1) * 4, :, :], pv, r=[pk], w=["Wii"])
            self.cp("dve", rho8[:], TE[:, 15, :], r=["p_TE"], w=["p_rho8"])
            if "toep_x" in self.export:
                c.dma(self.toep_x, Toep[:], r=["Toep"], w=["toep_x"])
                c.dma(self.wii_x, Wii[:], r=["Wii"], w=["wii_x"])
                c.dma(self.wiv_x[:, 0], WivR[:], r=["WivR"], w=["wiv_x"])
                c.dma(self.wiv_x[:, 1], WivI[:], r=["WivI"], w=["wiv_x"])
        c.barrier()
        with self.scope() as sp:
            T = lambda name, shape, dt=F32: sp.enter_context(self.sbt(name, shape, dt))
            phr = T("p_phr", [64, 32]); ph_s = T("p_phs", [64, 32]); phr2 = T("p_phr2", [64, 32])
            self.ts("dve", phr[:], thr[:], 8.0, ALU.mult, r=["p_thr"], w=["p_phr"])
            self.sincos(phr[:], "p_phr", ph_s[:], None, "p_phs", None, tfs[:], tis[:], red_o=(phr2[:], "p_phr2"))
            rho = T("p_rho", [64, 8, 256])
            ang = T("p_ang", [64, 8, 256]); sk = T("p_sk", [64, 8, 256]); ck = T("p_ck", [64, 8, 256])
            tf4 = T("p_tf4", [64, 8, 256]); ti4 = T("p_ti4", [64, 8, 256], I32)
            for gb in range(4):
                gs = slice(gb * 8, (gb + 1) * 8)
                self.cp("dve", rho[:], rho8[:, gs].unsqueeze(2).broadcast_to([64, 8, 256]), r=["p_rho8"], w=["p_rho"])
                c.op("dve", lambda e: e.memset(rho[:, :, 0:1], 0.0), r=[], w=["p_rho"])
                c.dma(self.rhot[:, gs, :], rho[:], r=["p_rho"], w=["rhot"])
                self.tt("dve", ang[:], phr2[:, gs].unsqueeze(2).broadcast_to([64, 8, 256]),
                        kv[:].unsqueeze(1).broadcast_to([64, 8, 256]), ALU.mult, r=["p_phr2", "p_kv"], w=["p_ang"])
                self.sincos(ang[:], "p_ang", sk[:], ck[:], "p_sk", "p_ck", tf4[:], ti4[:])
                c.dma(self.rots[:, gs, :], sk[:], r=["p_sk"], w=["rots"])
                c.dma(self.rotc[:, gs, :], ck[:], r=["p_ck"], w=["rotc"])
        c.barrier()
        so.close()

    def phase_s5(self):
        nc, c = self.nc, self.c
        with self.scope() as st:
            T = lambda name, shape, dt=F32: st.enter_context(self.sbt(name, shape, dt))
            Toep = T("Toep", [128, 32, 128], BF16)
            Wii = T("Wii", [128, 32, 2, 64], BF16)
            WivR = T("WivR", [64, 32, 128], BF16)
            WivI = T("WivI", [64, 32, 128], BF16)
            self.s5_precompute(st, Toep, Wii, WivR, WivI)
            if self.s5_main_enabled:
                self.s5_main(st, Toep, Wii, WivR, WivI)
        c.barrier()

    def s5_main(self, st0, Toep, Wii, WivR, WivI):
        nc, c = self.nc, self.c
        GB = 4
        with self.scope() as st:
            T = lambda name, shape, dt=F32: st.enter_context(self.sbt(name, shape, dt))
            gluw = T("gluw", [128, 4, BR], BF16)
            gst = T("gluw_st", [128, 4, BR], F32)
            c.dma(gst[:], self.glu_w.rearrange("(c p) n -> p c n", p=128), w=["gluw_st"])
            self.cp("dve", gluw[:], gst[:], r=["gluw_st"], w=["gluw"])
            Drep = T("Drep", [128, BR]); Brep = T("Brep", [128, BR])
            c.dma(Drep[:], self.s5d_rep, w=["Drep"])
            c.dma(Brep[:], self.glub_rep, w=["Brep"])
            for q in range(self.nseq):
                tb = q * self.S
                with self.scope() as sq:
                    TQ = lambda name, shape, dt=F32: sq.enter_context(self.sbt(name, shape, dt))
                    uck = [TQ("uck%d" % i, [128, 8 * BR], BF16) for i in range(2)]
                    szck = [TQ("szck%d" % i, [128, 8 * BR], BF16) for i in range(2)]
                    yck = [TQ("yck%d" % i, [128, 8, BR], BF16) for i in range(2)]
                    for kh in range(2):
                        rows = slice(tb + kh * 1024, tb + (kh + 1) * 1024)
                        c.dma(uck[kh][:], self.u_tok[rows, :].rearrange("(k t) f -> k (t f)", t=8), r=["u_tok"], w=["uck%d" % kh])
                        c.dma(szck[kh][:], self.sz_tok[rows, :].rearrange("(k t) f -> k (t f)", t=8), r=["sz_tok"], w=["szck%d" % kh])
                    with self.scope() as ss:
                        TS = lambda name, shape, dt=F32: ss.enter_context(self.sbt(name, shape, dt))
                        Ug = TS("Ug", [128, 32, 256], BF16)
                        Vb = TS("Vb", [64, 2, GB, 256])
                        Wb = TS("Wb", [64, 2, GB, 256])
                        tA = TS("tA", [64, GB, 256]); tB = TS("tB", [64, GB, 256])
                        ck = TS("ck", [64, GB, 256]); sk = TS("sk", [64, GB, 256]); rh = TS("rh", [64, GB, 256])
                        Xs = TS("Xs", [64, 2, GB, 256], BF16)
                        Ysb = TS("Ysb", [128, 8, 256], BF16)
                        c.op("pool", lambda e: e.memset(Xs[:], 0.0), w=["Xs"])
                        ucg = TS("ucg", [128, 32, 128], BF16)
                        for kh in range(2):
                            self.cp("pool" if kh == 0 else "dve", ucg[:].rearrange("p g (s c) -> p g s c", s=8),
                                    uck[kh][:].rearrange("p (s g c) -> p g s c", s=8, g=32), r=["uck%d" % kh], w=["ucg"])
                            for g8 in range(4):
                                pb = g8 % 2
                                pk = "ps%d" % pb
                                ptv = self.ps[pb][:].bitcast(BF16).rearrange("p (g k) -> p g k", g=8)
                                for gl in range(8):
                                    g = g8 * 8 + gl
                                    c.op("pe", lambda e: e.transpose(out=ptv[:, gl, :], in_=ucg[:, g, :],
                                                                     identity=self.ident[:]), r=["ucg", "ident"], w=[pk])
                                self.cp("dve" if g8 % 2 == 0 else "act", Ug[:, g8 * 8:(g8 + 1) * 8, kh * 128:(kh + 1) * 128], ptv,
                                        r=[pk], w=["Ug"])
                        for b in range(32 // GB):
                            gs = slice(b * GB, (b + 1) * GB)
                            c.dma(ck[:], self.rotc[:, gs, :], r=["rotc"], w=["ck"])
                            c.dma(sk[:], self.rots[:, gs, :], r=["rots"], w=["sk"])
                            c.dma(rh[:], self.rhot[:, gs, :], r=["rhot"], w=["rh"])
                            for gl in range(GB):
                                g = b * GB + gl
                                pb = 2 + gl % 2
                                pk = "ps%d" % pb
                                c.op("pe", lambda e: e.matmul(out=self.ps[pb][0:64, 0:256], lhsT=Wii[:, g, 0, :], rhs=Ug[:, g, :],
                                                              start=True, stop=True), r=["Wii", "Ug"], w=[pk])
                                c.op("pe", lambda e: e.matmul(out=self.ps[pb][0:64, 256:512], lhsT=Wii[:, g, 1, :], rhs=Ug[:, g, :],
                                                              start=True, stop=True), r=["Wii", "Ug"], w=[pk])
                                self.cp("act", Vb[:, :, gl, :], self.ps[pb][0:64, :].rearrange("p (r k) -> p r k", r=2), r=[pk], w=["Vb"])
                            self.tt("dve", tA[:], ck[:], Vb[:, 0], ALU.mult, r=["ck", "Vb"], w=["tA"])
                            self.tt("pool", tB[:], sk[:], Vb[:, 1], ALU.mult, r=["sk", "Vb"], w=["tB"])
                            self.tt("dve", Wb[:, 0], tA[:], tB[:], ALU.add, r=["tA", "tB"], w=["Wb0"])
                            self.tt("pool", tA[:], ck[:], Vb[:, 1], ALU.mult, r=["ck", "Vb"], w=["tA"])
                            self.tt("dve", tB[:], sk[:], Vb[:, 0], ALU.mult, r=["sk", "Vb"], w=["tB"])
                            self.tt("pool", Wb[:, 1], tA[:], tB[:], ALU.subtract, r=["tA", "tB"], w=["Wb1"])
                            fl = lambda a: a.rearrange("p g k -> p (g k)")
                            c.op("dve", lambda e: e.tensor_tensor_scan(out=fl(Vb[:, 0]), data0=fl(rh[:]), data1=fl(Wb[:, 0]), initial=0.0,
                                                                       op0=ALU.mult, op1=ALU.add), r=["rh", "Wb0", "Vb"], w=["Vb"])
                            c.op("dve", lambda e: e.tensor_tensor_scan(out=fl(Vb[:, 1]), data0=fl(rh[:]), data1=fl(Wb[:, 1]), initial=0.0,
                                                                       op0=ALU.mult, op1=ALU.add), r=["rh", "Wb1", "Vb"], w=["Vb"])
                            K1 = 255
                            self.tt("dve", tA[:, :, 0:K1], ck[:, :, 0:K1], Vb[:, 0, :, 0:K1], ALU.mult, r=["ck", "Vb"], w=["tA"])
                            self.tt("pool", tB[:, :, 0:K1], sk[:, :, 0:K1], Vb[:, 1, :, 0:K1], ALU.mult, r=["sk", "Vb"], w=["tB"])
                            self.tt("dve", Xs[:, 0, :, 1:256], tA[:, :, 0:K1], tB[:, :, 0:K1], ALU.subtract, r=["tA", "tB"], w=["Xs"])
                            self.tt("pool", tA[:, :, 0:K1], ck[:, :, 0:K1], Vb[:, 1, :, 0:K1], ALU.mult, r=["ck", "Vb"], w=["tA"])
                            self.tt("dve", tB[:, :, 0:K1], sk[:, :, 0:K1], Vb[:, 0, :, 0:K1], ALU.mult, r=["sk", "Vb"], w=["tB"])
                            self.tt("pool", Xs[:, 1, :, 1:256], tA[:, :, 0:K1], tB[:, :, 0:K1], ALU.add, r=["tA", "tB"], w=["Xs"])
                            for gl in range(GB):
                                g = b * GB + gl
                                pb = 4 + gl // 2 % 2
                                pk = "ps%d" % pb
                                o = self.ps[pb][:, (gl % 2) * 256:(gl % 2 + 1) * 256]
                                c.op("pe", lambda e: e.matmul(out=o, lhsT=Toep[:, g, :], rhs=Ug[:, g, :], start=True, stop=False),
                                     r=["Toep", "Ug"], w=[pk])
                                c.op("pe", lambda e: e.matmul(out=o, lhsT=WivR[:, g, :], rhs=Xs[:, 0, gl, :], start=False, stop=False),
                                     r=["WivR", "Xs"], w=[pk])
                                c.op("pe", lambda e: e.matmul(out=o, lhsT=WivI[:, g, :], rhs=Xs[:, 1, gl, :], start=False, stop=True),
                                     r=["WivI", "Xs"], w=[pk])
                                if gl % 2 == 1:
                                    g8l = (b * GB + gl - 1) % 8
                                    self.cp("act", Ysb[:, g8l:g8l + 2, :], self.ps[pb][:].rearrange("p (g k) -> p g k", g=2), r=[pk], w=["Ysb"])
                            if (b * GB + GB) % 8 == 0:
                                g8 = (b * GB) // 8
                                for kh in range(2):
                                    pb = 6 + kh
                                    pk = "ps%d" % pb
                                    ptv = self.ps[pb][:].bitcast(BF16).rearrange("p (g n) -> p g n", g=8)
                                    for gl in range(8):
                                        c.op("pe", lambda e: e.transpose(out=ptv[:, gl, :], in_=Ysb[:, gl, kh * 128:(kh + 1) * 128],
                                                                         identity=self.ident[:]), r=["Ysb", "ident"], w=[pk])
                                    self.cp("dve", yck[kh][:, :, g8 * 128:(g8 + 1) * 128].rearrange("p t (g c) -> p t g c", g=8),
                                            ptv.rearrange("p g (t c) -> p t g c", t=8), r=[pk], w=["yck%d" % kh])
                    with self.scope() as se:
                        TE_ = lambda name, shape, dt=F32: se.enter_context(self.sbt(name, shape, dt))
                        t1 = TE_("e_t1", [128, 8, BR]); t2 = TE_("e_t2", [128, 8, BR])
                        gck = TE_("gck", [128, 8, BR], BF16)
                        ack = TE_("ack", [128, 8, BR], BF16)
                        gT = [TE_("gT%d" % i, [128, 4, 128], BF16) for i in range(2)]
                        e1 = [TE_("e1_%d" % i, [128, BR]) for i in range(2)]
                        for kh in range(2):
                            uv = uck[kh][:].rearrange("p (s f) -> p s f", s=8)
                            zv = szck[kh][:].rearrange("p (s f) -> p s f", s=8)
                            self.tt("dve", t1[:], uv, Drep[:].unsqueeze(1).broadcast_to([128, 8, BR]), ALU.mult, r=["uck%d" % kh, "Drep"], w=["e_t1"])
                            self.tt("pool", t1[:], t1[:], yck[kh][:], ALU.add, r=["e_t1", "yck%d" % kh], w=["e_t1"])
                            self.actf(t2[:], t1[:], AF.Square, r=["e_t1"], w=["e_t2"])
                            self.ts("dve", t2[:], t2[:], 0.044715 * 0.7978845608, ALU.mult, r=["e_t2"], w=["e_t2"], s2=0.7978845608, op1=ALU.add)
                            self.tt("pool", t2[:], t2[:], t1[:], ALU.mult, r=["e_t2", "e_t1"], w=["e_t2"])
                            self.actf(t2[:], t2[:], AF.Tanh, r=["e_t2"], w=["e_t2"])
                            self.ts("dve", t2[:], t2[:], 1.0, ALU.add, r=["e_t2"], w=["e_t2"], s2=0.5, op1=ALU.mult)
                            self.tt("dve", gck[:], t2[:], t1[:], ALU.mult, r=["e_t2", "e_t1"], w=["gck"])
                            for tau in range(8):
                                i2 = tau % 2
                                pk = "ps%d" % i2
                                ptv = self.ps[i2][:].bitcast(BF16).rearrange("p (a b) -> p a b", a=8)
                                for fc in range(4):
                                    c.op("pe", lambda e: e.transpose(out=ptv[:, fc, :], in_=gck[:, tau, fc * 128:(fc + 1) * 128],
                                                                     identity=self.ident[:]), r=["gck", "ident"], w=[pk])
                                self.cp("act", gT[i2][:], ptv[:, 0:4, :], r=[pk], w=["gT%d" % i2])
                                pm = 2 + i2
                                for fc in range(4):
                                    c.op("pe", lambda e: e.matmul(out=self.ps[pm][:], lhsT=gT[i2][:, fc, :], rhs=gluw[:, fc, :],
                                                                  start=(fc == 0), stop=(fc == 3)), r=["gT%d" % i2, "gluw"], w=["ps%d" % pm])
                                ek = "e1_%d" % i2
                                self.tt("dve", e1[i2][:], self.ps[pm][:], Brep[:], ALU.add, r=["ps%d" % pm, "Brep"], w=[ek])
                                self.actf(e1[i2][:], e1[i2][:], AF.Tanh, r=[ek], w=[ek], scale=0.5)
                                self.ts("dve", e1[i2][:], e1[i2][:], 0.5, ALU.mult, r=[ek], w=[ek], s2=0.5, op1=ALU.add)
                                self.tt("pool", e1[i2][:], e1[i2][:], gck[:, tau, :], ALU.mult, r=[ek, "gck"], w=[ek])
                                self.tt("dve", ack[:, tau, :], e1[i2][:], zv[:, tau, :], ALU.mult, r=[ek, "szck%d" % kh], w=["ack"])
                            rows = slice(tb + kh * 1024, tb + (kh + 1) * 1024)
                            c.dma(self.a_tok[rows, :].rearrange("(k t) f -> k (t f)", t=8), ack[:].rearrange("p t f -> p (t f)"),
                                  r=["ack"], w=["a_tok"])

    def phase_ml(self):
        nc, c = self.nc, self.c
        S_ = self.S
        NB = S_ // 128
        SC = 128 ** -0.5
        with self.scope() as st:
            T = lambda name, shape, dt=F32: st.enter_context(self.sbt(name, shape, dt))
            cw = T("m_cw", [128, 4, 4]); cb = T("m_cb", [128, 4])
            mn = T("m_mn", [128, 4]); msk = T("m_msk", [128, 4])
            gbi = T("m_gbi", [4, 1]); gbf = T("m_gbf", [4, 1]); ngbf = T("m_ngbf", [4, 1])
            maskS = T("m_maskS", [128, 128]); idf = T("m_idf", [128, 128])
            ones4 = T("m_ones4", [4, 128])
            wst = T("m_wst", [128, 3, 4, 128]); wqkv = T("m_wqkv", [128, 3, 4, 128], BF16)
            gst = T("m_gst", [128, 12, 8]); gw = T("m_gw", [128, 12, 8], BF16)
            for (t, d, key) in ((cw, self.ml_cw, "m_cw"), (cb, self.ml_cb, "m_cb"), (mn, self.ml_mn, "m_mn"),
                                (msk, self.ml_msk, "m_msk"), (gbi, self.ml_gbi, "m_gbi"), (gbf, self.ml_gbf, "m_gbf"),
                                (maskS, self.ml_maskS, "m_maskS"), (idf, self.identf_d, "m_idf"),
                                (gst, self.ml_gw, "m_gst")):
                c.dma(t[:], d, w=[key])
            for i, d in enumerate((self.ml_wq, self.ml_wk, self.ml_wv)):
                c.dma(wst[:, i], d, w=["m_wst"])
            self.cp("dve", wqkv[:], wst[:], r=["m_wst"], w=["m_wqkv"])
            self.cp("dve", gw[:], gst[:], r=["m_gst"], w=["m_gw"])
            self.ts("dve", ngbf[:], gbf[:], -1.0, ALU.mult, r=["m_gbf"], w=["m_ngbf"])
            c.op("dve", lambda e: e.memset(ones4[:], 1.0), w=["m_ones4"])
            xmT_v = self.xmT.rearrange("(c p) t -> p c t", p=128)
            mzT_v = self.mzT.rearrange("(c p) t -> p c t", p=128)
            bT_v = self.bT.rearrange("(c p) t -> p c t", p=128)
            for q in range(self.nseq):
                tb = q * S_
                with self.scope() as sq:
                    TQ = lambda name, shape, dt=F32: sq.enter_context(self.sbt(name, shape, dt))
                    xcT = TQ("xcT", [128, 4, S_], BF16)
                    qT = TQ("qT", [128, 4, S_], BF16)
                    kT = TQ("kT", [128, 4, S_], BF16)
                    Ktok = TQ("Ktok", [128, NB, 4, 128], BF16)
                    Vtok = TQ("Vtok", [128, NB, 4, 129], BF16)
                    acol = TQ("acol", [128, NB, 4]); bcol = TQ("bcol", [128, NB, 4])
                    Rrep = TQ("Rrep", [128, NB + 1, 4])
                    Wt = TQ("Wt", [128, NB, 4]); Wp = TQ("Wp", [128, NB, 4])
                    Thr = TQ("Thr", [128, NB, 4]); Dec = TQ("Dec", [128, NB, 4])
                    c.op("pool", lambda e: e.memset(Vtok[:, :, :, 128:129], 1.0), w=["Vtok"])
                    with self.scope() as sa:
                        TA_ = lambda name, shape, dt=F32: sa.enter_context(self.sbt(name, shape, dt))
                        xm = TA_("xm", [128, 4, S_], BF16)
                        vT = TA_("vT", [128, 4, S_], BF16)
                        acc = TA_("acc", [128, S_])
                        g1f = TA_("g1", [32, S_]); g2f = TA_("g2", [32, S_]); g3f = TA_("g3", [32, S_]); onesr = TA_("onesr", [32, S_])
                        g1 = g1f[0:4, :]; g2 = g2f[0:4, :]; g3 = g3f[0:4, :]
                        c.op("pool", lambda e: e.memset(g1f[:], 0.0), w=["g1"])
                        c.op("pool", lambda e: e.memset(g2f[:], 0.0), w=["g2"])
                        rsel = TA_("rsel", [4, NB, 4])
                        c.dma(xm[:], xmT_v[:, :, tb:tb + S_], r=["xmT"], w=["xm"])
                        c.op("pool", lambda e: e.memset(onesr[:], 1.0), w=["onesr"])
                        for fc in range(4):
                            self.ts("dve", acc[:], xm[:, fc, :], cw[:, fc, 3:4], ALU.mult, r=["xm", "m_cw", "m_cb"], w=["acc"],
                                    s2=cb[:, fc:fc + 1], op1=ALU.add)
                            for sh in (1, 2, 3):
                                self.stt("dve", acc[:, sh:], xm[:, fc, 0:S_ - sh], cw[:, fc, 3 - sh:4 - sh], acc[:, sh:],
                                         ALU.mult, ALU.add, r=["xm", "m_cw", "acc"], w=["acc"])
                            self.actf(xcT[:, fc, :], acc[:], AF.Silu, r=["acc"], w=["xcT"])
                        for h in range(4):
                            for tl in range(S_ // 512):
                                ts_ = slice(tl * 512, (tl + 1) * 512)
                                for (i, src, skey, dst, dkey) in ((0, xcT, "xcT", qT, "qT"), (1, xcT, "xcT", kT, "kT"), (2, xm, "xm", vT, "vT")):
                                    pb = (h * 12 + tl * 3 + i) % 4
                                    pk = "ps%d" % pb
                                    c.op("pe", lambda e: e.matmul(out=self.ps[pb][:], lhsT=wqkv[:, i, h, :], rhs=src[:, h, ts_],
                                                                  start=True, stop=True), r=["m_wqkv", skey], w=[pk])
                                    self.cp("act" if i != 1 else "dve", dst[:, h, ts_], self.ps[pb][:], r=[pk], w=[dkey])
                        for blk in range(NB):
                            bs = slice(blk * 128, (blk + 1) * 128)
                            for (i, src, skey, dst, dkey, pb) in ((1, xcT, "xcT", Ktok, "Ktok", 4), (2, xm, "xm", Vtok, "Vtok", 5)):
                                pb = pb + 2 * (blk % 2)
                                pk = "ps%d" % pb
                                for h in range(4):
                                    c.op("pe", lambda e: e.matmul(out=self.ps[pb][:, h * 128:(h + 1) * 128], lhsT=src[:, h, bs],
                                                                  rhs=wqkv[:, i, h, :], start=True, stop=True), r=["m_wqkv", skey], w=[pk])
                                self.cp("act" if i == 1 else "dve", dst[:, blk, :, 0:128], self.ps[pb][:].rearrange("p (h e) -> p h e", h=4),
                                        r=[pk], w=[dkey])
                        for tl in range(S_ // 512):
                            ts_ = slice(tl * 512, (tl + 1) * 512)
                            for half in range(2):
                                pb = half
                                pk = "ps%d" % pb
                                for ch in range(12):
                                    src = (qT, kT, vT)[ch // 4]
                                    skey = ("qT", "kT", "vT")[ch // 4]
                                    c.op("pe", lambda e: e.matmul(out=self.ps[pb][0:4, :], lhsT=gw[:, ch, half * 4:half * 4 + 4],
                                                                  rhs=src[:, ch % 4, ts_], start=(ch == 0), stop=(ch == 11)),
                                         r=["m_gw", skey], w=[pk])
                                if half == 0:
                                    self.ts("dve", g1[:, ts_], self.ps[pb][0:4, :], gbi[:, 0:1], ALU.add, r=[pk, "m_gbi"], w=["g1"])
                                else:
                                    self.actf(g2[:, ts_], self.ps[pb][0:4, :], AF.Exp, r=[pk, "m_ngbf"], w=["g2"], scale=-1.0, bias=ngbf[:, 0:1])
                        self.actf(g2[:], g2[:], AF.Ln, r=["g2"], w=["g2"], bias=1.0)
                        c.op("dve", lambda e: e.tensor_tensor_scan(out=g3f[:], data0=onesr[:], data1=g2f[:], initial=0.0, op0=ALU.mult, op1=ALU.add),
                             r=["onesr", "g2"], w=["g3"])
                        self.tt("dve", g1[:], g1[:], g3[:], ALU.add, r=["g1", "g3"], w=["g1"])
                        c.op("dve", lambda e: e.tensor_tensor_scan(out=g2f[:], data0=onesr[:], data1=g1f[:], initial=0.0, op0=ALU.mult, op1=ALU.max),
                             r=["onesr", "g1", "g2"], w=["g2"])
                        pa = self.ps[2][:, 0:NB * 4].rearrange("p (b h) -> p b h", h=4)
                        pbn = self.ps[3][:, 0:NB * 4].rearrange("p (b h) -> p b h", h=4)
                        for blk in range(NB):
                            bs = slice(blk * 128, (blk + 1) * 128)
                            c.op("pe", lambda e: e.transpose(out=pa[:, blk, :], in_=g1[:, bs], identity=idf[0:4, 0:4]), r=["g1", "m_idf"], w=["ps2"])
                            c.op("pe", lambda e: e.transpose(out=pbn[:, blk, :], in_=g3[:, bs], identity=idf[0:4, 0:4]), r=["g3", "m_idf"], w=["ps3"])
                        self.cp("dve", acol[:], pa, r=["ps2"], w=["acol"])
                        self.cp("dve", bcol[:], pbn, r=["ps3"], w=["bcol"])
                        self.tt("dve", rsel[:], g2[:, 127::128].unsqueeze(2).broadcast_to([4, NB, 4]),
                                idf[0:4, 0:4].unsqueeze(1).broadcast_to([4, NB, 4]), ALU.mult, r=["g2", "m_idf"], w=["rsel"])
                        c.op("pe", lambda e: e.matmul(out=self.ps[0][:, 0:NB * 4], lhsT=ones4[:], rhs=rsel[:].rearrange("p b h -> p (b h)"),
                                                      start=True, stop=True), r=["m_ones4", "rsel"], w=["ps0"])
                        c.op("dve", lambda e: e.memset(Rrep[:, 0, :], 0.0), w=["Rrep"])
                        self.cp("dve", Rrep[:, 1:NB + 1, :], self.ps[0][:, 0:NB * 4].rearrange("p (b h) -> p b h", h=4), r=["ps0"], w=["Rrep"])
                    self.tt("dve", Wt[:], acol[:], Rrep[:, 0:NB, :], ALU.subtract, r=["acol", "Rrep"], w=["Wt"])
                    self.actf(Wt[:], Wt[:], AF.Exp, r=["Wt"], w=["Wt"])
                    self.tt("dve", Wp[:], acol[:], Rrep[:, 1:NB + 1, :], ALU.subtract, r=["acol", "Rrep"], w=["Wp"])
                    self.actf(Wp[:], Wp[:], AF.Exp, r=["Wp"], w=["Wp"])
                    self.ts("dve", Wp[:], Wp[:], SC, ALU.mult, r=["Wp"], w=["Wp"])
                    self.tt("dve", Thr[:], bcol[:], Rrep[:, 0:NB, :], ALU.subtract, r=["bcol", "Rrep"], w=["Thr"])
                    self.actf(Thr[:], Thr[:], AF.Exp, r=["Thr"], w=["Thr"])
                    self.tt("dve", Dec[:], Rrep[:, 0:NB, :], Rrep[:, 1:NB + 1, :], ALU.subtract, r=["Rrep"], w=["Dec"])
                    self.actf(Dec[:], Dec[:], AF.Exp, r=["Dec"], w=["Dec"])
                    with self.scope() as sm:
                        TM = lambda name, shape, dt=F32: sm.enter_context(self.sbt(name, shape, dt))
                        C32 = [TM("C32_%d" % h, [128, 129]) for h in range(4)]
                        Cb = [TM("Cb_%d" % h, [128, 129], BF16) for h in range(4)]
                        PT = [TM("PT%d" % i, [128, 128], BF16) for i in range(2)]
                        Vp = [TM("Vp%d" % i, [128, 129], BF16) for i in range(2)]
                        Vpp = [TM("Vpp%d" % i, [128, 129], BF16) for i in range(2)]
                        den = [TM("den%d" % i, [128, 1]) for i in range(2)]
                        hraw = [TM("hraw%d" % i, [128, 4, 128]) for i in range(2)]
                        bst = TM("bst", [128, 4, 6]); mv = TM("mv", [128, 4, 2]); rs = TM("rs", [128, 4])
                        hn = TM("hn", [128, 4, 128], BF16)
                        e1 = TM("m_e1", [128, 4, 128]); e2 = TM("m_e2", [128, 4, 128])
                        mz = [TM("mz%d" % i, [128, 4, 512], BF16) for i in range(2)]
                        bo = [TM("bo%d" % i, [128, 4, 512], BF16) for i in range(2)]
                        for I in range(NB):
                            bs = slice(I * 128, (I + 1) * 128)
                            i4 = (I // 4) % 2
                            if I % 4 == 0:
                                c.dma(mz[i4][:], mzT_v[:, :, tb + I * 128:tb + I * 128 + 512], r=["mzT"], w=["mz%d" % i4])
                            hk = "hraw%d" % (I % 2)
                            for h in range(4):
                                j = (I * 4 + h) % 2
                                pS = self.ps[j]; pO = self.ps[2 + j]; pC = self.ps[4 + j]
                                kS, kO, kC = "ps%d" % j, "ps%d" % (2 + j), "ps%d" % (4 + j)
                                c.op("pe", lambda e: e.matmul(out=pS[:, 0:128], lhsT=kT[:, h, bs], rhs=qT[:, h, bs], start=True, stop=True),
                                     r=["kT", "qT"], w=[kS])
                                self.tt("dve", PT[j][:], pS[:, 0:128], maskS[:], ALU.mult, r=[kS, "m_maskS"], w=["PT%d" % j])
                                c.op("act", lambda e: e.activation(out=Vp[j][:], in_=Vtok[:, I, h, :], func=AF.Copy, scale=Wt[:, I, h:h + 1]),
                                     r=["Vtok", "Wt"], w=["Vp%d" % j])
                                c.op("act", lambda e: e.activation(out=Vpp[j][:], in_=Vtok[:, I, h, :], func=AF.Copy, scale=Wp[:, I, h:h + 1]),
                                     r=["Vtok", "Wp"], w=["Vpp%d" % j])
                                c.op("pe", lambda e: e.matmul(out=pO[:, 0:129], lhsT=PT[j][:], rhs=Vp[j][:], start=True, stop=(I == 0)),
                                     r=["PT%d" % j, "Vp%d" % j], w=[kO])
                                if I > 0:
                                    c.op("pe", lambda e: e.matmul(out=pO[:, 0:129], lhsT=qT[:, h, bs], rhs=Cb[h][:], start=False, stop=True),
                                         r=["qT", "Cb_%d" % h], w=[kO])
                                if I < NB - 1:
                                    c.op("pe", lambda e: e.matmul(out=pC[:, 0:129], lhsT=Ktok[:, I, h, :], rhs=Vpp[j][:], start=True, stop=True),
                                         r=["Ktok", "Vpp%d" % j], w=[kC])
                                    if I == 0:
                                        self.cp("dve", C32[h][:], pC[:, 0:129], r=[kC], w=["C32_%d" % h])
                                    else:
                                        self.stt("dve", C32[h][:], C32[h][:], Dec[:, I, h:h + 1], pC[:, 0:129], ALU.mult, ALU.add,
                                                 r=["C32_%d" % h, "Dec", kC], w=["C32_%d" % h])
                                    self.cp("act", Cb[h][:], C32[h][:], r=["C32_%d" % h], w=["Cb_%d" % h])
                                dk = "den%d" % j
                                self.actf(den[j][:], pO[:, 128:129], AF.Abs, r=[kO], w=[dk])
                                self.tt("dve", den[j][:], den[j][:], Thr[:, I, h:h + 1], ALU.max, r=[dk, "Thr"], w=[dk])
                                c.op("dve", lambda e: e.reciprocal(out=den[j][:], in_=den[j][:]), r=[dk], w=[dk])
                                c.op("act", lambda e: e.activation(out=hraw[I % 2][:, h, :], in_=pO[:, 0:128], func=AF.Copy, scale=den[j][:, 0:1]),
                                     r=[kO, dk], w=[hk])
                                c.op("dve", lambda e: e.bn_stats(out=bst[:, h, :], in_=hraw[I % 2][:, h, :]), r=[hk], w=["bst"])
                                c.op("dve", lambda e: e.bn_aggr(out=mv[:, h, :], in_=bst[:, h, :]), r=["bst"], w=["mv"])
                            self.actf(rs[:], mv[:, :, 1], AF.Ln, r=["mv"], w=["rs"], bias=HEAD_EPS)
                            self.actf(rs[:], rs[:], AF.Exp, r=["rs"], w=["rs"], scale=-0.5)
                            self.tt("dve", e1[:], hraw[I % 2][:], mv[:, :, 0:1].broadcast_to([128, 4, 128]), ALU.subtract, r=[hk, "mv"], w=["m_e1"])
                            self.tt("dve", hn[:], e1[:], rs[:].unsqueeze(2).broadcast_to([128, 4, 128]), ALU.mult, r=["m_e1", "rs"], w=["hn"])
                            ptv = self.ps[6 + I % 2][:].bitcast(BF16).rearrange("p (a b) -> p a b", a=8)
                            pk = "ps%d" % (6 + I % 2)
                            for h in range(4):
                                c.op("pe", lambda e: e.transpose(out=ptv[:, h, :], in_=hn[:, h, :], identity=self.ident[:]), r=["hn", "ident"], w=[pk])
                            self.tt("dve", e1[:], ptv[:, 0:4, :], mn[:].unsqueeze(2).broadcast_to([128, 4, 128]), ALU.mult, r=[pk, "m_mn", "m_e1"], w=["m_e1"])
                            self.tt("pool", e2[:], xcT[:, :, bs], msk[:].unsqueeze(2).broadcast_to([128, 4, 128]), ALU.mult, r=["xcT", "m_msk"], w=["m_e2"])
                            self.tt("dve", e1[:], e1[:], e2[:], ALU.add, r=["m_e1", "m_e2"], w=["m_e1"])
                            off = (I % 4) * 128
                            self.tt("pool", bo[i4][:, :, off:off + 128], e1[:], mz[i4][:, :, off:off + 128], ALU.mult,
                                    r=["m_e1", "mz%d" % i4], w=["bo%d" % i4])
                            if I % 4 == 3:
                                c.dma(bT_v[:, :, tb + (I - 3) * 128:tb + (I + 1) * 128], bo[i4][:], r=["bo%d" % i4], w=["bT"])
        c.barrier()

    def out_proj_norm_res(self, lay, wout_d, pg_rep_d, lhs_provider, res_rows, dst_rows, dst_key, res_key):
        nc, c = self.nc, self.c
        NT = self.NT
        with self.scope() as st:
            T = lambda name, shape, dt=F32: st.enter_context(self.sbt(name, shape, dt))
            wout = T("wout", [128, 8, D], BF16)
            wst = [T("wost%d" % i, [128, D]) for i in range(2)]
            for fc in range(8):
                c.dma(wst[fc % 2][:], wout_d[fc * 128:(fc + 1) * 128, :], w=["wost%d" % (fc % 2)])
                self.cp("dve" if fc % 2 else "act", wout[:, fc, :], wst[fc % 2][:], r=["wost%d" % (fc % 2)], w=["wout"])
            pg = T("pg", [128, D])
            c.dma(pg[:], pg_rep_d, w=["pg"])
            xr = [T("xr%d" % i, [128, D]) for i in range(3)]
            yo = [T("yo%d" % i, [128, D]) for i in range(2)]
            junk = T("ojunk", [128, BR])
            ss = T("oss", [128, 2]); rstd = T("orstd", [128, 1])
            prov = lhs_provider(st)
            nblk = NT // 128
            for blk in range(nblk):
                rows = slice(blk * 128, (blk + 1) * 128)
                xk = "xr%d" % (blk % 3)
                c.dma(xr[blk % 3][:], res_rows[rows, :], r=[res_key % blk], w=[xk])
                lhs = prov(blk)
                for half in range(2):
                    pb = 4 + half + 2 * (blk % 2)
                    pk = "ps%d" % pb
                    for fc in range(8):
                        ap, key = lhs[fc]
                        c.op("pe", lambda e: e.matmul(out=self.ps[pb][:], lhsT=ap, rhs=wout[:, fc, half * BR:(half + 1) * BR],
                                                      start=(fc == 0), stop=(fc == 7)), r=[key, "wout"], w=[pk])
                    self.actf(junk[:], self.ps[pb][:], AF.Square, r=[pk], w=["ojunk", "oss"], accum_out=ss[:, half:half + 1])
                self.tt("dve", rstd[:], ss[:, 0:1], ss[:, 1:2], ALU.add, r=["oss"], w=["orstd"])
                self.actf(rstd[:], rstd[:], AF.Ln, r=["orstd"], w=["orstd"], scale=1.0 / D, bias=NORM_EPS)
                self.actf(rstd[:], rstd[:], AF.Exp, r=["orstd"], w=["orstd"], scale=-0.5)
                yk = "yo%d" % (blk % 2)
                for half in range(2):
                    pb = 4 + half + 2 * (blk % 2)
                    hs = slice(half * BR, (half + 1) * BR)
                    self.tt("dve", yo[blk % 2][:, hs], self.ps[pb][:], pg[:, hs], ALU.mult, r=["ps%d" % pb, "pg"], w=[yk])
                self.stt("dve" , yo[blk % 2][:], yo[blk % 2][:], rstd[:, 0:1], xr[blk % 3][:], ALU.mult, ALU.add, r=[yk, "orstd", xk], w=[yk])
                c.dma(dst_rows[rows, :], yo[blk % 2][:], r=[yk], w=[dst_key % blk])

    def phase_l0_out(self):
        nc, c = self.nc, self.c
        bT_v = self.bT.rearrange("(c p) t -> p c t", p=128)

        def provider(st):
            T = lambda name, shape, dt=F32: st.enter_context(self.sbt(name, shape, dt))
            at = [T("at%d" % i, [128, BR], BF16) for i in range(2)]
            aT = [T("aT%d" % i, [128, 4, 128], BF16) for i in range(2)]
            bt = [T("bt%d" % i, [128, 4, 512], BF16) for i in range(2)]

            def prov(blk):
                i = blk % 2
                i4 = (blk // 4) % 2
                c.dma(at[i][:], self.a_tok[blk * 128:(blk + 1) * 128, :], r=["a_tok"], w=["at%d" % i])
                if blk % 4 == 0:
                    c.dma(bt[i4][:], bT_v[:, :, blk * 128:blk * 128 + 512], r=["bT"], w=["bt%d" % i4])
                pk = "ps%d" % i
                ptv = self.ps[i][:].bitcast(BF16).rearrange("p (a b) -> p a b", a=8)
                for fc in range(4):
                    c.op("pe", lambda e: e.transpose(out=ptv[:, fc, :], in_=at[i][:, fc * 128:(fc + 1) * 128], identity=self.ident[:]),
                         r=["at%d" % i, "ident"], w=[pk])
                self.cp("act", aT[i][:], ptv[:, 0:4, :], r=[pk], w=["aT%d" % i])
                off = (blk % 4) * 128
                return [(aT[i][:, fc, :], "aT%d" % i) for fc in range(4)] + \
                       [(bt[i4][:, h, off:off + 128], "bt%d" % i4) for h in range(4)]
            return prov
        self.out_proj_norm_res(0, self.w_out_ab, self.post0_rep, provider, self.x, self.out, "h1_%d", "x%.0d")

    def phase_l1_proj(self):
        nc, c = self.nc, self.c
        NT = self.NT
        S_ = self.S
        ngrp = NT // 512
        nblk = NT // 128
        with self.scope() as st:
            T = lambda name, shape, dt=F32: st.enter_context(self.sbt(name, shape, dt))
            win = T("win1", [128, 8, 8 * BR], BF16)
            g1 = T("g1n", [128, 8])
            stage = [T("w1st%d" % i, [128, 4 * BR]) for i in range(2)]
            c.dma(g1[:], self.pre1, w=["g1n"])
            for dc in range(8):
                for hf in range(2):
                    i = (dc * 2 + hf) % 2
                    sk = "w1st%d" % i
                    c.dma(stage[i][:], self.w_in_cd[dc * 128:(dc + 1) * 128, hf * 2048:(hf + 1) * 2048], w=[sk])
                    if hf == 0:
                        c.op("act", lambda e: e.activation(out=win[:, dc, 0:2048], in_=stage[i][:], func=AF.Copy, scale=g1[:, dc:dc + 1]),
                             r=[sk, "g1n"], w=["win1"])
                    else:
                        self.ts("dve", win[:, dc, 2048:4096], stage[i][:], g1[:, dc:dc + 1], ALU.mult, r=[sk, "g1n"], w=["win1"])
            cosT = T("cosT", [128, S_]); sinT = T("sinT", [128, S_]); Rm = T("Rm", [128, 128], BF16)
            c.dma(cosT[:], self.rope_cos, w=["cosT"])
            c.dma(sinT[:], self.rope_sin, w=["sinT"])
            c.dma(Rm[:], self.rope_rm, w=["Rm"])
            NXB = 6
            xt = [T("x1t%d" % i, [128, D]) for i in range(NXB)]
            junk = T("junk1", [128, D])
            ss = T("ss1", [128, 4]); rstd = T("rstd1", [128, 4])
            zb = [T("z1b%d" % i, [128, D], BF16) for i in range(2)]
            zT = [T("z1T%d" % i, [128, 8, 512], BF16) for i in range(2)]
            vo = [T("vo%d" % i, [128, BR], BF16) for i in range(4)]
            xb = [T("xb%d" % i, [128, 512], BF16) for i in range(2)]
            r1 = [T("r1_%d" % i, [128, 512]) for i in range(2)]
            r2 = [T("r2_%d" % i, [128, 512]) for i in range(2)]
            fo = [T("fo%d" % i, [128, 4, 512], BF16) for i in range(2)]

            def load_x(b):
                if b < nblk:
                    c.dma(xt[b % NXB][:], self.h1src[b * 128:(b + 1) * 128, :], r=["h1_%d" % b], w=["x1t%d" % (b % NXB)])
            for b in range(4):
                load_x(b)
            fm_groups = [(0, self.cqT, "cqT", "rope"), (512, self.ckT, "ckT", "rope"), (2048, self.dqT, "dqT", "rope"),
                         (2560, self.dkT, "dkT", "rope"), (1536, self.czT, "czT", "silu"), (3584, self.dzT, "dzT", "silu")]
            cnt = 0
            for g in range(ngrp):
                zk = "z1T%d" % (g % 2)
                pos0 = (g * 512) % S_
                for j in range(4):
                    b = g * 4 + j
                    xk = "x1t%d" % (b % NXB)
                    c.op("act", lambda e: e.activation(out=junk[:], in_=xt[b % NXB][:], func=AF.Square, accum_out=ss[:, j:j + 1]),
                         r=[xk], w=["junk1", "ss1"])
                self.actf(rstd[:], ss[:], AF.Ln, r=["ss1"], w=["rstd1"], scale=1.0 / D, bias=NORM_EPS)
                self.actf(rstd[:], rstd[:], AF.Exp, r=["rstd1"], w=["rstd1"], scale=-0.5)
                for j in range(4):
                    b = g * 4 + j
                    xk = "x1t%d" % (b % NXB)
                    zbk = "z1b%d" % (b % 2)
                    pk = "ps%d" % (b % 2)
                    self.ts("dve", zb[b % 2][:], xt[b % NXB][:], rstd[:, j:j + 1], ALU.mult, r=[xk, "rstd1"], w=[zbk])
                    load_x(b + 4)
                    ptv = self.ps[b % 2][:].bitcast(BF16).rearrange("p (a b) -> p a b", a=8)
                    for dc in range(8):
                        c.op("pe", lambda e: e.transpose(out=ptv[:, dc, :], in_=zb[b % 2][:, dc * 128:(dc + 1) * 128], identity=self.ident[:]),
                             r=[zbk, "ident"], w=[pk])
                    self.cp("dve", zT[g % 2][:, :, j * 128:(j + 1) * 128], ptv, r=[pk], w=[zk])
                    for vi, (col0, dst, dkey) in enumerate(((1024, self.cv_tok, "cv_tok"), (3072, self.dv_tok, "dv_tok"))):
                        pb = 2 + vi
                        vk = "vo%d" % ((b % 2) * 2 + vi)
                        for dc in range(8):
                            c.op("pe", lambda e: e.matmul(out=self.ps[pb][:], lhsT=zT[g % 2][:, dc, j * 128:(j + 1) * 128],
                                                          rhs=win[:, dc, col0:col0 + BR], start=(dc == 0), stop=(dc == 7)), r=[zk, "win1"], w=["ps%d" % pb])
                        self.cp("act", vo[(b % 2) * 2 + vi][:], self.ps[pb][:], r=["ps%d" % pb], w=[vk])
                        c.dma(dst[b * 128:(b + 1) * 128, :], vo[(b % 2) * 2 + vi][:], r=[vk], w=[dkey])
                for (col0, dst, dkey, kind) in fm_groups:
                    fk = "fo%d" % (cnt % 2)
                    fot = fo[cnt % 2]
                    cnt += 1
                    for fc in range(4):
                        pb = 4 + fc % 2
                        pk = "ps%d" % pb
                        for dc in range(8):
                            c.op("pe", lambda e: e.matmul(out=self.ps[pb][:], lhsT=win[:, dc, col0 + fc * 128:col0 + (fc + 1) * 128],
                                                          rhs=zT[g % 2][:, dc, :], start=(dc == 0), stop=(dc == 7)), r=[zk, "win1"], w=[pk])
                        if kind == "silu":
                            self.actf(fot[:, fc, :], self.ps[pb][:], AF.Silu, r=[pk], w=[fk])
                        else:
                            i2 = fc % 2
                            pr = 6 + i2
                            self.cp("act", xb[i2][:], self.ps[pb][:], r=[pk], w=["xb%d" % i2])
                            c.op("pe", lambda e: e.matmul(out=self.ps[pr][:], lhsT=Rm[:], rhs=xb[i2][:], start=True, stop=True),
                                 r=["Rm", "xb%d" % i2], w=["ps%d" % pr])
                            self.tt("dve", r1[i2][:], self.ps[pb][:], cosT[:, pos0:pos0 + 512], ALU.mult, r=[pk, "cosT"], w=["r1_%d" % i2])
                            self.tt("dve", r2[i2][:], self.ps[pr][:], sinT[:, pos0:pos0 + 512], ALU.mult, r=["ps%d" % pr, "sinT"], w=["r2_%d" % i2])
                            self.tt("pool", fot[:, fc, :], r1[i2][:], r2[i2][:], ALU.add, r=["r1_%d" % i2, "r2_%d" % i2], w=[fk])
                    c.dma(dst.rearrange("(c p) t -> p c t", p=128)[:, :, g * 512:(g + 1) * 512], fot[:], r=[fk], w=[dkey])

    def phase_attn(self, kind):
        nc, c = self.nc, self.c
        S_ = self.S
        NB = S_ // 128
        NQT = S_ // 512
        dil = (kind == "dil")
        qT_d, kT_d, v_d, o_d = (self.cqT, self.ckT, self.cv_tok, self.oc_tok) if dil else (self.dqT, self.dkT, self.dv_tok, self.od_tok)
        okey = "oc_tok" if dil else "od_tok"
        NH = 8 if dil else 4
        VW = 64 if dil else 128
        nmask = 9 if dil else 4
        lam_init = 0.8 - 0.6 * math.exp(-0.3 * 1)
        with self.scope() as st:
            T = lambda name, shape, dt=F32: st.enter_context(self.sbt(name, shape, dt))
            masks = T("amask", [128, nmask, 512], BF16)
            c.dma(masks[:], self.dil_masks if dil else self.diff_masks, w=["amask"])
            if not dil:
                lqk = T("lqk", [1, 4, 64]); pr = T("lpr", [1, 2, 64]); sm = T("lsm", [1, 2]); nl = T("nl", [1, 1])
                ones1 = T("ones1", [1, 128]); nlam = T("nlam", [128, 1]); gdn = T("gdn", [128, 128])
                c.dma(lqk[:], self.diff_lqk, w=["lqk"])
                c.dma(gdn[:], self.diffnorm_rep, w=["gdn"])
                c.op("dve", lambda e: e.memset(ones1[:], 1.0), w=["ones1"])
                self.tt("dve", pr[:, 0, :], lqk[:, 0, :], lqk[:, 1, :], ALU.mult, r=["lqk"], w=["lpr"])
                self.tt("dve", pr[:, 1, :], lqk[:, 2, :], lqk[:, 3, :], ALU.mult, r=["lqk", "lpr"], w=["lpr"])
                c.op("dve", lambda e: e.reduce_sum(out=sm[:], in_=pr[:], axis=AX.X), r=["lpr"], w=["lsm"])
                self.actf(sm[:], sm[:], AF.Exp, r=["lsm"], w=["lsm"])
                self.tt("dve", nl[:], sm[:, 1:2], sm[:, 0:1], ALU.subtract, r=["lsm"], w=["nl"])
                self.ts("dve", nl[:], nl[:], -lam_init, ALU.add, r=["nl"], w=["nl"])
                c.op("pe", lambda e: e.matmul(out=self.ps[0][:, 0:1], lhsT=ones1[:], rhs=nl[:], start=True, stop=True), r=["ones1", "nl"], w=["ps0"])
                self.cp("dve", nlam[:], self.ps[0][:, 0:1], r=["ps0"], w=["nlam"])
                self.ts("dve", gdn[:], gdn[:], 1.0 - lam_init, ALU.mult, r=["gdn"], w=["gdn"])
            qT_v = qT_d.rearrange("(c p) t -> p c t", p=128)
            kT_v = kT_d.rearrange("(c p) t -> p c t", p=128)
            for q in range(self.nseq):
                tb = q * S_
                with self.scope() as sq:
                    TQ = lambda name, shape, dt=F32: sq.enter_context(self.sbt(name, shape, dt))
                    qT = TQ("aqT", [128, 4, S_], BF16); kT = TQ("akT", [128, 4, S_], BF16)
                    V1 = TQ("aV1", [128, NB, NH, VW + 1], BF16)
                    osb = TQ("aosb", [128, NB, BR], BF16)
                    E = [TQ("aE%d" % i, [128, 512], BF16) for i in range(2)]
                    P = [TQ("aP%d" % i, [128, 512], BF16) for i in range(2)]
                    rd = [TQ("ard%d" % i, [128, 1]) for i in range(4)]
                    if not dil:
                        o01 = [TQ("ao%d" % i, [128, NB, 128]) for i in range(2)]
                        sqj = TQ("asq", [128, 128]); ssn = TQ("assn", [128, NB]); t3 = TQ("at3", [128, 128])
                    c.dma(qT[:], qT_v[:, :, tb:tb + S_], r=[("cqT" if dil else "dqT")], w=["aqT"])
                    c.dma(kT[:], kT_v[:, :, tb:tb + S_], r=[("ckT" if dil else "dkT")], w=["akT"])
                    c.op("pool", lambda e: e.memset(V1[:, :, :, VW:VW + 1], 1.0), w=["aV1"])
                    for blk in range(NB):
                        c.dma(V1[:, blk, :, 0:VW], v_d[tb + blk * 128:tb + (blk + 1) * 128, :].rearrange("p (h d) -> p h d", h=NH),
                              r=[("cv_tok" if dil else "dv_tok")], w=["aV1"])
                    tcount = 0
                    for h in range(NH):
                        for m in range(1 if dil else 2):
                            ch = h // 2 if dil else h
                            rb = 64 * (h % 2) if dil else 64 * m
                            for qt in range(NQT):
                                qs = slice(qt * 512, (qt + 1) * 512)
                                for kb in range(4 * qt + 4):
                                    d0 = 4 * qt - kb
                                    i2 = tcount % 2
                                    tcount += 1
                                    pS = self.ps[i2]
                                    kS = "ps%d" % i2
                                    c.op("pe", lambda e: e.matmul(out=pS[:], lhsT=kT[rb:rb + 64, ch, kb * 128:(kb + 1) * 128], rhs=qT[rb:rb + 64, ch, qs],
                                                                  start=True, stop=True), r=["akT", "aqT"], w=[kS])
                                    if dil:
                                        mi = 8 if d0 >= 5 else d0 + 3
                                    else:
                                        mi = d0 + 3 if d0 <= 0 else None
                                    if mi is None:
                                        self.actf(P[i2][:], pS[:], AF.Exp, r=[kS], w=["aP%d" % i2], scale=0.125)
                                    else:
                                        self.actf(E[i2][:], pS[:], AF.Exp, r=[kS], w=["aE%d" % i2], scale=0.125)
                                        self.tt("dve" if tcount % 3 else "pool", P[i2][:], E[i2][:], masks[:, mi, :], ALU.mult,
                                                r=["aE%d" % i2, "amask"], w=["aP%d" % i2])
                                    for j in range(4):
                                        Q = 4 * qt + j
                                        if kb > Q:
                                            continue
                                        c.op("pe", lambda e: e.matmul(out=self.ps[4 + j][:, 0:VW + 1], lhsT=P[i2][:, j * 128:(j + 1) * 128],
                                                                      rhs=V1[:, kb, h, :], start=(kb == 0), stop=(kb == Q)),
                                             r=["aP%d" % i2, "aV1"], w=["ps%d" % (4 + j)])
                                for j in range(4):
                                    Q = 4 * qt + j
                                    pO = self.ps[4 + j]
                                    kO = "ps%d" % (4 + j)
                                    c.op("dve", lambda e: e.reciprocal(out=rd[j][:], in_=pO[:, VW:VW + 1]), r=[kO], w=["ard%d" % j])
                                    if dil:
                                        c.op("act", lambda e: e.activation(out=osb[:, Q, h * 64:(h + 1) * 64], in_=pO[:, 0:VW], func=AF.Copy,
                                                                           scale=rd[j][:, 0:1]), r=[kO, "ard%d" % j], w=["aosb"])
                                    else:
                                        c.op("act", lambda e: e.activation(out=o01[m][:, Q, :], in_=pO[:, 0:VW], func=AF.Copy,
                                                                           scale=rd[j][:, 0:1]), r=[kO, "ard%d" % j], w=["ao%d" % m])
                        if not dil:
                            self.stt("dve", o01[0][:], o01[1][:], nlam[:, 0:1], o01[0][:], ALU.mult, ALU.add, r=["ao0", "ao1", "nlam"], w=["ao0"])
                            for Q in range(NB):
                                self.actf(sqj[:], o01[0][:, Q, :], AF.Square, r=["ao0"], w=["asq", "assn"], accum_out=ssn[:, Q:Q + 1])
                            self.actf(ssn[:], ssn[:], AF.Ln, r=["assn"], w=["assn"], scale=1.0 / 128, bias=HEAD_EPS)
                            self.actf(ssn[:], ssn[:], AF.Exp, r=["assn"], w=["assn"], scale=-0.5)
                            for Q in range(NB):
                                self.ts("dve", t3[:], o01[0][:, Q, :], ssn[:, Q:Q + 1], ALU.mult, r=["ao0", "assn"], w=["at3"])
                                self.tt("pool", osb[:, Q, h * 128:(h + 1) * 128], t3[:], gdn[:], ALU.mult, r=["at3", "gdn"], w=["aosb"])
                    c.dma(o_d[tb:tb + S_, :].rearrange("(b p) f -> p b f", p=128), osb[:], r=["aosb"], w=[okey])

    def phase_l1_out(self):
        nc, c = self.nc, self.c
        czT_v = self.czT.rearrange("(c p) t -> p c t", p=128)
        dzT_v = self.dzT.rearrange("(c p) t -> p c t", p=128)

        def provider(st):
            T = lambda name, shape, dt=F32: st.enter_context(self.sbt(name, shape, dt))
            ot = [T("ot%d" % i, [128, 2, BR], BF16) for i in range(2)]
            gT = [T("gT1_%d" % i, [128, 8, 128], BF16) for i in range(2)]
            gz = [T("gz%d" % i, [128, 8, 512], BF16) for i in range(2)]

            def prov(blk):
                i = blk % 2
                i4 = (blk // 4) % 2
                c.dma(ot[i][:, 0, :], self.oc_tok[blk * 128:(blk + 1) * 128, :], r=["oc_tok"], w=["ot%d" % i])
                c.dma(ot[i][:, 1, :], self.od_tok[blk * 128:(blk + 1) * 128, :], r=["od_tok"], w=["ot%d" % i])
                if blk % 4 == 0:
                    c.dma(gz[i4][:, 0:4, :], czT_v[:, :, blk * 128:blk * 128 + 512], r=["czT"], w=["gz%d" % i4])
                    c.dma(gz[i4][:, 4:8, :], dzT_v[:, :, blk * 128:blk * 128 + 512], r=["dzT"], w=["gz%d" % i4])
                pk = "ps%d" % i
                ptv = self.ps[i][:].bitcast(BF16).rearrange("p (a b) -> p a b", a=8)
                for fc in range(8):
                    c.op("pe", lambda e: e.transpose(out=ptv[:, fc, :], in_=ot[i][:, fc // 4, (fc % 4) * 128:(fc % 4 + 1) * 128],
                                                     identity=self.ident[:]), r=["ot%d" % i, "ident"], w=[pk])
                off = (blk % 4) * 128
                self.tt("dve", gT[i][:], ptv, gz[i4][:, :, off:off + 128], ALU.mult, r=[pk, "gz%d" % i4], w=["gT1_%d" % i])
                return [(gT[i][:, fc, :], "gT1_%d" % i) for fc in range(8)]
            return prov
        self.out_proj_norm_res(1, self.w_out_cd, self.post1_rep, provider, self.h1src, self.out, "h1_%d", "h1_%d")


def core_inputs(inp, x_rows):
    m = {}
    m["x"] = np.ascontiguousarray(x_rows, dtype=np.float32)
    m["ident"] = np.eye(128, dtype=np.float32).astype(ml_dtypes.bfloat16)
    m["pre0"] = np.ascontiguousarray(inp["pre_norm"][0].reshape(8, 128).T)
    m["w_in_ab"] = inp["w_in_ab"][0]
    m["s5_lr"] = inp["s5_lambda_re"][0].T
    m["s5_li"] = inp["s5_lambda_im"][0].T
    m["s5_ldt"] = np.broadcast_to(inp["s5_log_dt"][0][None, :], (64, 32))
    m["s5_br"] = inp["s5_b_re"][0].transpose(1, 0, 2)
    m["s5_bi"] = inp["s5_b_im"][0].transpose(1, 0, 2)
    m["s5_cr"] = inp["s5_c_re"][0].transpose(2, 0, 1)
    m["s5_ci"] = inp["s5_c_im"][0].transpose(2, 0, 1)
    tv = np.array([0, -1, -2, -3, -4, -5, -6, -7, 1, 2, 3, 4, 5, 6, 7, 8], dtype=np.float32)
    m["tv"] = np.broadcast_to(tv[None, :], (64, 16))
    m["kv"] = np.broadcast_to(np.arange(256, dtype=np.float32)[None, :], (64, 256))
    sidx = np.arange(128) // 16
    m["toepmask"] = (sidx[None, :] >= sidx[:, None]).astype(np.float32)
    m["identf"] = np.eye(128, dtype=np.float32)
    m["s5d_rep"] = np.broadcast_to(inp["s5_d"][0][None, :], (128, 512))
    m["glub_rep"] = np.broadcast_to(inp["s5_glu_b"][0][None, :], (128, 512))
    m["glu_w"] = inp["s5_glu_w"][0]
    m["ml_cw"] = inp["ml_conv_w"][0].reshape(4, 4, 128).transpose(2, 1, 0)
    m["ml_cb"] = inp["ml_conv_b"][0].reshape(4, 128).T
    m["ml_mn"] = inp["ml_norm"][0].reshape(4, 128).T
    m["ml_msk"] = inp["ml_skip"][0].reshape(4, 128).T
    m["ml_gbi"] = inp["ml_gate_b"][0][0:4].reshape(4, 1)
    m["ml_gbf"] = inp["ml_gate_b"][0][4:8].reshape(4, 1)
    si = np.arange(128)
    m["ml_maskS"] = ((si[:, None] <= si[None, :]) * (128 ** -0.5)).astype(np.float32)
    m["ml_gw"] = inp["ml_gate_w"][0].reshape(12, 128, 8).transpose(1, 0, 2)
    m["ml_wq"] = inp["ml_wq"][0].transpose(1, 0, 2)
    m["ml_wk"] = inp["ml_wk"][0].transpose(1, 0, 2)
    m["ml_wv"] = inp["ml_wv"][0].transpose(1, 0, 2)
    m["w_out_ab"] = inp["w_out_ab"][0]
    m["pre1"] = np.ascontiguousarray(inp["pre_norm"][1].reshape(8, 128).T)
    m["w_in_cd"] = inp["w_in_cd"][0]
    pos = np.arange(S, dtype=np.float32)
    inv = (10000.0 ** (-np.arange(0, 64, 2, dtype=np.float32) / 64)).astype(np.float32)
    ang = pos[None, :] * inv[np.arange(128) % 32][:, None]
    m["rope_cos"] = np.cos(ang).astype(np.float32)
    m["rope_sin"] = np.sin(ang).astype(np.float32)
    rm = np.zeros((128, 128), dtype=np.float32)
    for mm in range(128):
        if mm % 64 < 32:
            rm[mm + 32, mm] = -1.0
        else:
            rm[mm - 32, mm] = 1.0
    m["rope_rm"] = rm.astype(ml_dtypes.bfloat16)
    kk = np.arange(128)[:, None]
    qq = np.arange(512)[None, :]
    dm = np.zeros((128, 9, 512), dtype=np.float32)
    fm = np.zeros((128, 4, 512), dtype=np.float32)
    for mi in range(9):
        d0 = mi - 3 if mi < 8 else 5
        dl = 128 * d0 + qq - kk
        mult = ((dl >= 0) & (dl <= 128)).astype(np.float32) + ((dl >= 0) & (dl % 4 == 0) & (dl <= 512)) + ((dl >= 0) & (dl % 16 == 0) & (dl <= 2048))
        dm[:, mi, :] = mult
        if mi < 4:
            fm[:, mi, :] = (dl >= 0)
    m["dil_masks"] = dm.astype(ml_dtypes.bfloat16)
    m["diff_masks"] = fm.astype(ml_dtypes.bfloat16)
    m["diff_lqk"] = np.stack([inp["diff_lq1"][0], inp["diff_lk1"][0], inp["diff_lq2"][0], inp["diff_lk2"][0]])[None]
    m["diffnorm_rep"] = np.broadcast_to(inp["diff_norm"][0][None, :], (128, 128))
    m["w_out_cd"] = inp["w_out_cd"][0]
    m["post1_rep"] = np.broadcast_to(inp["post_norm"][1][None, :], (128, 1024))
    m["post0_rep"] = np.broadcast_to(inp["post_norm"][0][None, :], (128, 1024))
    return m


_CACHE = {}


def kernel(**inputs):
    inp = {k_: np.asarray(v) for k_, v in inputs.items()}
    x = inp["x"]
    B = x.shape[0]
    nseq = B // NCORES
    if "prog" not in _CACHE:
        kb = K(nseq=nseq)
        kb.build()
        _CACHE["prog"] = kb
    kb = _CACHE["prog"]
    in_maps = []
    for ci in range(NCORES):
        m = core_inputs(inp, x[ci * nseq:(ci + 1) * nseq].reshape(-1, D))
        in_maps.append({n: np.ascontiguousarray(m[n]) for n in kb.inputs})
    res = run_bass_kernel_spmd(kb.nc, in_maps, core_ids=list(range(NCORES)))
    out = np.stack([np.asarray(res.results[ci]["out"]).reshape(nseq, S, D) for ci in range(NCORES)], axis=0)
    return out.reshape(B, S, D).astype(np.float32)
```

```python
import contextlib
import math
import numpy as np
import ml_dtypes
import concourse.bass as bass
import concourse.mybir as mybir
from concourse.bass_utils import run_bass_kernel_spmd

F32 = mybir.dt.float32
BF16 = mybir.dt.bfloat16
I32 = mybir.dt.int32
AF = mybir.ActivationFunctionType
ALU = mybir.AluOpType
AX = mybir.AxisListType

D = 1024
S = 2048
BR = 512
NCORES = 8
SAME_ENGINE_SYNC = True
NORM_EPS = 1e-6
HEAD_EPS = 1e-5


class Ctx:
    def __init__(self, nc, stack, n_dma_sems=48, same_engine_sync=SAME_ENGINE_SYNC):
        self.nc = nc
        self.eng = {"pe": nc.tensor, "act": nc.scalar, "dve": nc.vector,
                    "pool": nc.gpsimd, "sp": nc.sync}
        self.sem = {}
        self.cnt = {}
        for k in ("pe", "act", "dve", "pool"):
            self.sem[k] = stack.enter_context(nc.semaphore("s_" + k))
            self.cnt[k] = 0
        self.dma_sems = []
        for i in range(n_dma_sems):
            k = "dma%d" % i
            self.sem[k] = stack.enter_context(nc.semaphore("s_" + k))
            self.cnt[k] = 0
            self.dma_sems.append(k)
        self.dma_rr = 0
        self.waited = {k: {} for k in self.eng}
        self.last_w = {}
        self.readers = {}
        self.same_engine_sync = same_engine_sync
        self.n_instr = 0
        self.n_wait = 0

    def _deps(self, r, w):
        deps = []
        for x in r:
            if x in self.last_w:
                deps.append(self.last_w[x])
            if x.startswith("ps"):
                deps.extend(self.readers.get(x, ()))
        for x in w:
            if x in self.last_w:
                deps.append(self.last_w[x])
            deps.extend(self.readers.get(x, ()))
        return deps

    def _wait(self, e, deps):
        need = {}
        for (k, v) in deps:
            if k == e and (e == "pe" or not self.same_engine_sync):
                continue
            if need.get(k, 0) < v:
                need[k] = v
        for k, v in need.items():
            if self.waited[e].get(k, 0) >= v:
                continue
            self.eng[e].wait_ge(self.sem[k], v)
            self.waited[e][k] = v
            self.n_wait += 1

    def _commit(self, tok, r, w):
        for x in w:
            self.last_w[x] = tok
            self.readers[x] = []
        for x in r:
            if x in w:
                continue
            self.readers.setdefault(x, []).append(tok)

    def op(self, e, fn, r=(), w=()):
        self._wait(e, self._deps(r, w))
        ins = fn(self.eng[e])
        self.cnt[e] += 1
        ins.then_inc(self.sem[e], 1)
        self._commit((e, self.cnt[e]), r, w)
        self.n_instr += 1
        return ins

    def dma(self, out, in_, r=(), w=(), q="sp", **kw):
        k = self.dma_sems[self.dma_rr]
        self.dma_rr = (self.dma_rr + 1) % len(self.dma_sems)
        deps = self._deps(r, w)
        if self.cnt[k] > 0:
            deps.append((k, self.cnt[k]))
        self._wait(q, deps)
        ins = self.eng[q].dma_start(out=out, in_=in_, **kw)
        self.cnt[k] += 16
        ins.then_inc(self.sem[k], 16)
        self._commit((k, self.cnt[k]), r, w)
        self.n_instr += 1
        return ins

    def barrier(self):
        deps = [(k, v) for k, v in self.cnt.items() if v > 0]
        for e in self.eng:
            self._wait(e, deps)

    def finish(self, res):
        deps = [self.last_w[x] for x in res if x in self.last_w]
        self._wait("sp", deps)


class K:
    def __init__(self, nseq=2, export=(), phases=None, seqlen=S):
        self.nseq = nseq
        self.S = seqlen
        self.NT = nseq * seqlen
        self.export = set(export)
        self.phases = phases
        self.nc = bass.Bass("TRN2", target_bir_lowering=False)
        self.inputs = {}
        self.outputs = {}
        self.s5_main_enabled = True
        self._uid = 0

    def sbt(self, name, shape, dt):
        self._uid += 1
        return self.nc.sbuf_tensor("%s_u%d" % (name, self._uid), list(shape), dt)

    def din(self, name, shape, dt=F32):
        ap = self.nc.dram_tensor(name, list(shape), dt, kind="ExternalInput").ap()
        self.inputs[name] = ap
        return ap

    def dscr(self, name, shape, dt):
        kind = "ExternalOutput" if name in self.export else "Internal"
        ap = self.nc.dram_tensor(name, list(shape), dt, kind=kind).ap()
        if kind == "ExternalOutput":
            self.outputs[name] = ap
        return ap

    @contextlib.contextmanager
    def scope(self):
        with contextlib.ExitStack() as st:
            yield st
            self.c.barrier()

    def build(self):
        nc = self.nc
        NT = self.NT
        with contextlib.ExitStack() as st:
            self.c = Ctx(nc, st)
            self.ps = [st.enter_context(nc.psum_tensor("ps%d" % i, [128, 512], F32)) for i in range(8)]
            self.x = self.din("x", [NT, D])
            self.ident_d = self.din("ident", [128, 128], BF16)
            self.pre0 = self.din("pre0", [128, 8])
            self.w_in_ab = self.din("w_in_ab", [D, 4 * BR])
            for nm in ("s5_lr", "s5_li", "s5_ldt"):
                setattr(self, nm, self.din(nm, [64, 32]))
            for nm in ("s5_br", "s5_bi", "s5_cr", "s5_ci"):
                setattr(self, nm, self.din(nm, [64, 32, 16]))
            self.tv_d = self.din("tv", [64, 16])
            self.kv_d = self.din("kv", [64, 256])
            self.toepmask_d = self.din("toepmask", [128, 128])
            self.identf_d = self.din("identf", [128, 128])
            self.s5d_rep = self.din("s5d_rep", [128, BR])
            self.glub_rep = self.din("glub_rep", [128, BR])
            self.glu_w = self.din("glu_w", [BR, BR])
            self.ml_cw = self.din("ml_cw", [128, 4, 4]); self.ml_cb = self.din("ml_cb", [128, 4])
            self.ml_mn = self.din("ml_mn", [128, 4]); self.ml_msk = self.din("ml_msk", [128, 4])
            self.ml_gbi = self.din("ml_gbi", [4, 1]); self.ml_gbf = self.din("ml_gbf", [4, 1])
            self.ml_maskS = self.din("ml_maskS", [128, 128])
            self.ml_gw = self.din("ml_gw", [128, 12, 8])
            self.ml_wq = self.din("ml_wq", [128, 4, 128]); self.ml_wk = self.din("ml_wk", [128, 4, 128]); self.ml_wv = self.din("ml_wv", [128, 4, 128])
            self.bT = self.dscr("bT", [BR, NT], BF16)
            self.w_out_ab = self.din("w_out_ab", [D, D]); self.post0_rep = self.din("post0_rep", [128, D])
            self.out = self.nc.dram_tensor("out", [NT, D], F32, kind="ExternalOutput").ap()
            self.outputs["out"] = self.out
            self.h1src = self.out
            if self.phases is not None and "l0o" not in self.phases:
                self.h1src = self.din("h1_in", [NT, D])
            self.pre1 = self.din("pre1", [128, 8]); self.w_in_cd = self.din("w_in_cd", [D, 8 * BR])
            self.rope_cos = self.din("rope_cos", [128, self.S]); self.rope_sin = self.din("rope_sin", [128, self.S])
            self.rope_rm = self.din("rope_rm", [128, 128], BF16)
            for nm in ("cqT", "ckT", "dqT", "dkT", "czT", "dzT"):
                setattr(self, nm, self.dscr(nm, [BR, NT], BF16))
            for nm in ("cv_tok", "dv_tok", "oc_tok", "od_tok"):
                setattr(self, nm, self.dscr(nm, [NT, BR], BF16))
            self.dil_masks = self.din("dil_masks", [128, 9, 512], BF16); self.diff_masks = self.din("diff_masks", [128, 4, 512], BF16)
            self.diff_lqk = self.din("diff_lqk", [1, 4, 64]); self.diffnorm_rep = self.din("diffnorm_rep", [128, 128])
            self.w_out_cd = self.din("w_out_cd", [D, D]); self.post1_rep = self.din("post1_rep", [128, D])
            self.rotc = self.dscr("rotc", [64, 32, 256], F32)
            self.rots = self.dscr("rots", [64, 32, 256], F32)
            self.rhot = self.dscr("rhot", [64, 32, 256], F32)
            self.toep_x = self.dscr("toep_x", [128, 32, 128], BF16)
            self.wii_x = self.dscr("wii_x", [128, 32, 2, 64], BF16)
            self.wiv_x = self.dscr("wiv_x", [64, 2, 32, 128], BF16)
            self.a_tok = self.dscr("a_tok", [NT, BR], BF16)
            self.u_tok = self.dscr("u_tok", [NT, BR], BF16)
            self.sz_tok = self.dscr("sz_tok", [NT, BR], BF16)
            self.xmT = self.dscr("xmT", [BR, NT], BF16)
            self.mzT = self.dscr("mzT", [BR, NT], BF16)
            self.ident = st.enter_context(nc.sbuf_tensor("identb", [128, 128], BF16))
            self.c.dma(self.ident[:], self.ident_d, w=["ident"])
            ph = self.phases
            fin = []
            if ph is None or "l0p" in ph:
                self.phase_l0_proj()
                fin += ["u_tok", "sz_tok", "xmT", "mzT"]
            if ph is None or "s5" in ph:
                self.phase_s5()
                fin += ["a_tok", "rotc", "rots", "rhot", "toep_x", "wii_x", "wiv_x"]
            if ph is None or "ml" in ph:
                self.phase_ml()
                fin += ["bT"]
            if ph is None or "l0o" in ph:
                self.phase_l0_out()
                fin += ["h1_%d" % b for b in range(NT // 128)]
            if ph is None or "l1p" in ph:
                self.phase_l1_proj()
                fin += ["cqT", "ckT", "dqT", "dkT", "czT", "dzT", "cv_tok", "dv_tok"]
            if ph is None or "adil" in ph:
                self.phase_attn("dil")
                fin += ["oc_tok"]
            if ph is None or "adiff" in ph:
                self.phase_attn("diff")
                fin += ["od_tok"]
            if ph is None or "l1o" in ph:
                self.phase_l1_out()
                fin += ["h1_%d" % b for b in range(NT // 128)]
            self.c.finish(fin)
            self.c.barrier()
        return nc

    def rmsnorm_T(self, st, xsrc_rows, nblk, zT, zkey, tagp, ps_tr):
        raise NotImplementedError

    def phase_l0_proj(self):
        nc, c = self.nc, self.c
        NT = self.NT
        ngrp = NT // 512
        with self.scope() as st:
            T = lambda name, shape, dt: st.enter_context(self.sbt(name, shape, dt))
            win = T("win0", [128, 8, 4 * BR], BF16)
            g0 = T("g0", [128, 8], F32)
            stage = [T("wst%d" % i, [128, 4 * BR], F32) for i in range(2)]
            c.dma(g0[:], self.pre0, w=["g0"])
            for dc in range(8):
                sk = "wst%d" % (dc % 2)
                c.dma(stage[dc % 2][:], self.w_in_ab[dc * 128:(dc + 1) * 128, :], w=[sk])
                c.op("act", lambda e: e.activation(out=win[:, dc, :], in_=stage[dc % 2][:], func=AF.Copy,
                                                   scale=g0[:, dc:dc + 1]), r=[sk, "g0"], w=["win0"])
            NXB = 6
            xt = [T("xt%d" % i, [128, D], F32) for i in range(NXB)]
            junk = T("junk", [128, D], F32)
            ss2 = [T("ss%d" % i, [128, 4], F32) for i in range(2)]
            rstd2 = [T("rstd%d" % i, [128, 4], F32) for i in range(2)]
            zb = [T("zb%d" % i, [128, D], BF16) for i in range(2)]
            zT = [T("zT%d" % i, [128, 8, 512], BF16) for i in range(2)]
            uo = [T("uo%d" % i, [128, BR], BF16) for i in range(2)]
            so = [T("so%d" % i, [128, BR], BF16) for i in range(2)]
            xmo = [T("xmo%d" % i, [128, 4, 512], BF16) for i in range(2)]
            mzo = [T("mzo%d" % i, [128, 4, 512], BF16) for i in range(2)]
            nblk = NT // 128

            def load_x(b):
                if b < nblk:
                    c.dma(xt[b % NXB][:], self.x[b * 128:(b + 1) * 128, :], w=["xt%d" % (b % NXB)])
            for b in range(4):
                load_x(b)
            for g in range(ngrp):
                zk = "zT%d" % (g % 2)
                ss = ss2[g % 2]; rstd = rstd2[g % 2]
                ssk = "ss%d" % (g % 2); rsk = "rstd%d" % (g % 2)
                for j in range(4):
                    b = g * 4 + j
                    xk = "xt%d" % (b % NXB)
                    c.op("act", lambda e: e.activation(out=junk[:], in_=xt[b % NXB][:], func=AF.Square,
                                                       accum_out=ss[:, j:j + 1]), r=[xk], w=["junk", ssk])
                c.op("act", lambda e: e.activation(out=rstd[:], in_=ss[:], func=AF.Ln, scale=1.0 / D, bias=NORM_EPS),
                     r=[ssk], w=[rsk])
                c.op("act", lambda e: e.activation(out=rstd[:], in_=rstd[:], func=AF.Exp, scale=-0.5),
                     r=[rsk], w=[rsk])
                for j in range(4):
                    b = g * 4 + j
                    xk = "xt%d" % (b % NXB)
                    zbk = "zb%d" % (b % 2)
                    pst = self.ps[b % 2]
                    pk = "ps%d" % (b % 2)
                    c.op("dve", lambda e: e.tensor_scalar(out=zb[b % 2][:], in0=xt[b % NXB][:], scalar1=rstd[:, j:j + 1],
                                                          scalar2=None, op0=ALU.mult), r=[xk, rsk], w=[zbk])
                    load_x(b + 4)
                    ptv = pst[:].bitcast(BF16).rearrange("p (a b) -> p a b", a=8)
                    for dc in range(8):
                        c.op("pe", lambda e: e.transpose(out=ptv[:, dc, :], in_=zb[b % 2][:, dc * 128:(dc + 1) * 128],
                                                         identity=self.ident[:]), r=[zbk, "ident"], w=[pk])
                    c.op("dve", lambda e: e.tensor_copy(out=zT[g % 2][:, :, j * 128:(j + 1) * 128], in_=ptv),
                         r=[pk], w=[zk])
                    for dc in range(8):
                        c.op("pe", lambda e: e.matmul(out=self.ps[2][:], lhsT=zT[g % 2][:, dc, j * 128:(j + 1) * 128],
                                                      rhs=win[:, dc, 0:BR], start=(dc == 0), stop=(dc == 7)),
                             r=[zk, "win0"], w=["ps2"])
                    c.op("act", lambda e: e.copy(out=uo[b % 2][:], in_=self.ps[2][:]), r=["ps2"], w=["uo%d" % (b % 2)])
                    c.dma(self.u_tok[b * 128:(b + 1) * 128, :], uo[b % 2][:], r=["uo%d" % (b % 2)], w=["u_tok"])
                    for dc in range(8):
                        c.op("pe", lambda e: e.matmul(out=self.ps[3][:], lhsT=zT[g % 2][:, dc, j * 128:(j + 1) * 128],
                                                      rhs=win[:, dc, BR:2 * BR], start=(dc == 0), stop=(dc == 7)),
                             r=[zk, "win0"], w=["ps3"])
                    c.op("act", lambda e: e.activation(out=so[b % 2][:], in_=self.ps[3][:], func=AF.Silu),
                         r=["ps3"], w=["so%d" % (b % 2)])
                    c.dma(self.sz_tok[b * 128:(b + 1) * 128, :], so[b % 2][:], r=["so%d" % (b % 2)], w=["sz_tok"])
                for fc in range(8):
                    pb = 4 + fc % 4
                    for dc in range(8):
                        c.op("pe", lambda e: e.matmul(out=self.ps[pb][:], lhsT=win[:, dc, 2 * BR + fc * 128:2 * BR + (fc + 1) * 128],
                                                      rhs=zT[g % 2][:, dc, :], start=(dc == 0), stop=(dc == 7)),
                             r=[zk, "win0"], w=["ps%d" % pb])
                    if fc < 4:
                        c.op("dve", lambda e: e.tensor_copy(out=xmo[g % 2][:, fc, :], in_=self.ps[pb][:]),
                             r=["ps%d" % pb], w=["xmo%d" % (g % 2)])
                    else:
                        c.op("act", lambda e: e.activation(out=mzo[g % 2][:, fc - 4, :], in_=self.ps[pb][:], func=AF.Silu),
                             r=["ps%d" % pb], w=["mzo%d" % (g % 2)])
                c.dma(self.xmT.rearrange("(c p) t -> p c t", p=128)[:, :, g * 512:(g + 1) * 512], xmo[g % 2][:],
                      r=["xmo%d" % (g % 2)], w=["xmT"])
                c.dma(self.mzT.rearrange("(c p) t -> p c t", p=128)[:, :, g * 512:(g + 1) * 512], mzo[g % 2][:],
                      r=["mzo%d" % (g % 2)], w=["mzT"])
        c.barrier()

    def tt(self, e, out, a, b, op, r, w):
        return self.c.op(e, lambda en: en.tensor_tensor(out=out, in0=a, in1=b, op=op), r=r, w=w)

    def ts(self, e, out, a, s1, op0, r, w, s2=None, op1=None):
        if op1 is None:
            return self.c.op(e, lambda en: en.tensor_scalar(out=out, in0=a, scalar1=s1, scalar2=None, op0=op0), r=r, w=w)
        return self.c.op(e, lambda en: en.tensor_scalar(out=out, in0=a, scalar1=s1, scalar2=s2, op0=op0, op1=op1), r=r, w=w)

    def stt(self, e, out, a, s, b, op0, op1, r, w):
        return self.c.op(e, lambda en: en.scalar_tensor_tensor(out=out, in0=a, scalar=s, in1=b, op0=op0, op1=op1), r=r, w=w)

    def actf(self, out, in_, func, r, w, **kw):
        return self.c.op("act", lambda en: en.activation(out=out, in_=in_, func=func, **kw), r=r, w=w)

    def cp(self, e, out, in_, r, w):
        if e == "act":
            return self.c.op("act", lambda en: en.copy(out=out, in_=in_), r=r, w=w)
        return self.c.op(e, lambda en: en.tensor_copy(out=out, in_=in_), r=r, w=w)

    def sincos(self, ang, akey, sin_o, cos_o, skey, ckey, tf, ti, red_o=None):
        C1 = 6.28125
        C2 = 2 * math.pi - C1
        for (off, out, okey) in ((0.0, sin_o, skey), (math.pi / 2, cos_o, ckey)):
            if out is None:
                continue
            self.ts("dve", tf, ang, 1.0 / (2 * math.pi), ALU.mult, r=[akey], w=["sc_tf"], s2=off / (2 * math.pi), op1=ALU.add)
            self.cp("dve", ti, tf, r=["sc_tf"], w=["sc_ti"])
            self.cp("dve", tf, ti, r=["sc_ti"], w=["sc_tf"])
            self.stt("dve", out, tf, -C1, ang, ALU.mult, ALU.add, r=["sc_tf", akey], w=[okey])
            self.stt("dve", out, tf, -C2, out, ALU.mult, ALU.add, r=["sc_tf", okey], w=[okey])
            if off != 0.0:
                self.ts("dve", out, out, off, ALU.add, r=[okey], w=[okey])
            self.ts("dve", out, out, math.pi, ALU.min, r=[okey], w=[okey], s2=-math.pi, op1=ALU.max)
            if red_o is not None and off == 0.0:
                self.cp("dve", red_o[0], out, r=[okey], w=[red_o[1]])
            self.actf(out, out, AF.Sin, r=[okey], w=[okey])

    def s5_precompute(self, st, Toep, Wii, WivR, WivI):
        nc, c = self.nc, self.c
        so = contextlib.ExitStack()
        TO = lambda name, shape, dt=F32: so.enter_context(self.sbt(name, shape, dt))
        thr = TO("p_thr", [64, 32]); rho8 = TO("p_rho8", [64, 32]); kv = TO("p_kv", [64, 256])
        tfs = TO("p_tfs", [64, 32]); tis = TO("p_tis", [64, 32], I32)
        with self.scope() as sp:
            T = lambda name, shape, dt=F32: sp.enter_context(self.sbt(name, shape, dt))
            lr = T("p_lr", [64, 32]); li = T("p_li", [64, 32]); ldt = T("p_ldt", [64, 32])
            br = T("p_br", [64, 32, 16]); bi = T("p_bi", [64, 32, 16])
            cr = T("p_cr", [64, 32, 16]); ci = T("p_ci", [64, 32, 16])
            tv = T("p_tv", [64, 16])
            msk = T("p_msk", [128, 128]); idf = T("p_idf", [128, 128])
            for (t, d, key) in ((lr, self.s5_lr, "p_lr"), (li, self.s5_li, "p_li"), (ldt, self.s5_ldt, "p_ldt"),
                                (br, self.s5_br, "p_br"), (bi, self.s5_bi, "p_bi"), (cr, self.s5_cr, "p_cr"),
                                (ci, self.s5_ci, "p_ci"), (tv, self.tv_d, "p_tv"), (kv, self.kv_d, "p_kv"),
                                (msk, self.toepmask_d, "p_msk"), (idf, self.identf_d, "p_idf")):
                c.dma(t[:], d, w=[key])
            dt = T("p_dt", [64, 32]); lrdt = T("p_lrdt", [64, 32]); th = T("p_th", [64, 32])
            s0 = T("p_s0", [64, 32]); c0 = T("p_c0", [64, 32]); mag = T("p_mag", [64, 32])
            self.actf(dt[:], ldt[:], AF.Exp, r=["p_ldt"], w=["p_dt"])
            self.tt("dve", lrdt[:], lr[:], dt[:], ALU.mult, r=["p_lr", "p_dt"], w=["p_lrdt"])
            self.tt("dve", th[:], li[:], dt[:], ALU.mult, r=["p_li", "p_dt"], w=["p_th"])
            self.sincos(th[:], "p_th", s0[:], c0[:], "p_s0", "p_c0", tfs[:], tis[:], red_o=(thr[:], "p_thr"))
            self.actf(mag[:], lrdt[:], AF.Exp, r=["p_lrdt"], w=["p_mag"])
            abr = T("p_abr", [64, 32]); abi = T("p_abi", [64, 32]); am1 = T("p_am1", [64, 32])
            self.tt("dve", abr[:], mag[:], c0[:], ALU.mult, r=["p_mag", "p_c0"], w=["p_abr"])
            self.tt("dve", abi[:], mag[:], s0[:], ALU.mult, r=["p_mag", "p_s0"], w=["p_abi"])
            self.ts("dve", am1[:], abr[:], -1.0, ALU.add, r=["p_abr"], w=["p_am1"])
            den = T("p_den", [64, 32]); t1 = T("p_t1", [64, 32]); t2 = T("p_t2", [64, 32])
            fr = T("p_fr", [64, 32]); fi = T("p_fi", [64, 32])
            self.tt("dve", den[:], lr[:], lr[:], ALU.mult, r=["p_lr"], w=["p_den"])
            self.tt("dve", t1[:], li[:], li[:], ALU.mult, r=["p_li"], w=["p_t1"])
            self.tt("dve", den[:], den[:], t1[:], ALU.add, r=["p_den", "p_t1"], w=["p_den"])
            c.op("dve", lambda e: e.reciprocal(out=den[:], in_=den[:]), r=["p_den"], w=["p_den"])
            self.tt("dve", t1[:], am1[:], lr[:], ALU.mult, r=["p_am1", "p_lr"], w=["p_t1"])
            self.tt("dve", t2[:], abi[:], li[:], ALU.mult, r=["p_abi", "p_li"], w=["p_t2"])
            self.tt("dve", t1[:], t1[:], t2[:], ALU.add, r=["p_t1", "p_t2"], w=["p_t1"])
            self.tt("dve", fr[:], t1[:], den[:], ALU.mult, r=["p_t1", "p_den"], w=["p_fr"])
            self.tt("dve", t1[:], abi[:], lr[:], ALU.mult, r=["p_abi", "p_lr"], w=["p_t1"])
            self.tt("dve", t2[:], am1[:], li[:], ALU.mult, r=["p_am1", "p_li"], w=["p_t2"])
            self.tt("dve", t1[:], t1[:], t2[:], ALU.subtract, r=["p_t1", "p_t2"], w=["p_t1"])
            self.tt("dve", fi[:], t1[:], den[:], ALU.mult, r=["p_t1", "p_den"], w=["p_fi"])
            Bbr = T("p_Bbr", [64, 32, 16]); Bbi = T("p_Bbi", [64, 32, 16])
            u1 = T("p_u1", [64, 32, 16]); u2 = T("p_u2", [64, 32, 16])
            bc16 = lambda a: a.unsqueeze(2).broadcast_to([64, 32, 16])
            self.tt("dve", u1[:], br[:], bc16(fr[:]), ALU.mult, r=["p_br", "p_fr"], w=["p_u1"])
            self.tt("dve", u2[:], bi[:], bc16(fi[:]), ALU.mult, r=["p_bi", "p_fi"], w=["p_u2"])
            self.tt("dve", Bbr[:], u1[:], u2[:], ALU.subtract, r=["p_u1", "p_u2"], w=["p_Bbr"])
            self.tt("dve", u1[:], bi[:], bc16(fr[:]), ALU.mult, r=["p_bi", "p_fr"], w=["p_u1"])
            self.tt("dve", u2[:], br[:], bc16(fi[:]), ALU.mult, r=["p_br", "p_fi"], w=["p_u2"])
            self.tt("dve", Bbi[:], u1[:], u2[:], ALU.add, r=["p_u1", "p_u2"], w=["p_Bbi"])
            TE = T("p_TE", [64, 16, 32]); TA = T("p_TA", [64, 16, 32])
            PWr = T("p_PWr", [64, 16, 32]); PWi = T("p_PWi", [64, 16, 32])
            tf3 = T("p_tf3", [64, 16, 32]); ti3 = T("p_ti3", [64, 16, 32], I32)
            bt = lambda a: a.unsqueeze(1).broadcast_to([64, 16, 32])
            bg = lambda a: a.unsqueeze(2).broadcast_to([64, 16, 32])
            self.tt("dve", TE[:], bt(lrdt[:]), bg(tv[:]), ALU.mult, r=["p_lrdt", "p_tv"], w=["p_TE"])
            self.actf(TE[:], TE[:], AF.Exp, r=["p_TE"], w=["p_TE"])
            self.tt("dve", TA[:], bt(thr[:]), bg(tv[:]), ALU.mult, r=["p_thr", "p_tv"], w=["p_TA"])
            self.sincos(TA[:], "p_TA", PWi[:], PWr[:], "p_PWi", "p_PWr", tf3[:], ti3[:])
            self.tt("dve", PWr[:], PWr[:], TE[:], ALU.mult, r=["p_PWr", "p_TE"], w=["p_PWr"])
            self.tt("dve", PWi[:], PWi[:], TE[:], ALU.mult, r=["p_PWi", "p_TE"], w=["p_PWi"])
            HsR = T("p_HsR", [64, 32, 8, 16]); HsI = T("p_HsI", [64, 32, 8, 16])
            v1 = T("p_v1", [64, 32, 9, 16]); v2 = T("p_v2", [64, 32, 9, 16])
            def pw_b(PW, j0, n):
                return PW[:, j0:j0 + n, :].rearrange("p t g -> p g t").unsqueeze(3).broadcast_to([64, 32, n, 16])
            def x_b(x, n):
                return x.unsqueeze(2).broadcast_to([64, 32, n, 16])
            self.tt("dve", v1[:, :, 0:8, :], pw_b(PWr, 0, 8), x_b(Bbr[:], 8), ALU.mult, r=["p_PWr", "p_Bbr"], w=["p_v1"])
            self.tt("dve", v2[:, :, 0:8, :], pw_b(PWi, 0, 8), x_b(Bbi[:], 8), ALU.mult, r=["p_PWi", "p_Bbi"], w=["p_v2"])
            self.tt("dve", HsR[:], v1[:, :, 0:8, :], v2[:, :, 0:8, :], ALU.subtract, r=["p_v1", "p_v2"], w=["p_HsR"])
            self.tt("dve", v1[:, :, 0:8, :], pw_b(PWr, 0, 8), x_b(Bbi[:], 8), ALU.mult, r=["p_PWr", "p_Bbi"], w=["p_v1"])
            self.tt("dve", v2[:, :, 0:8, :], pw_b(PWi, 0, 8), x_b(Bbr[:], 8), ALU.mult, r=["p_PWi", "p_Bbr"], w=["p_v2"])
            self.tt("dve", HsI[:], v1[:, :, 0:8, :], v2[:, :, 0:8, :], ALU.add, r=["p_v1", "p_v2"], w=["p_HsI"])
            LtR = T("p_LtR", [64, 32, 9, 16]); nLtI = T("p_nLtI", [64, 32, 9, 16])
            for (s0_, j0, n) in ((0, 0, 1), (1, 8, 8)):
                sl = slice(s0_, s0_ + n)
                self.tt("dve", v1[:, :, sl, :], pw_b(PWr, j0, n), x_b(cr[:], n), ALU.mult, r=["p_PWr", "p_cr"], w=["p_v1"])
                self.tt("dve", v2[:, :, sl, :], pw_b(PWi, j0, n), x_b(ci[:], n), ALU.mult, r=["p_PWi", "p_ci"], w=["p_v2"])
                self.tt("dve", LtR[:, :, sl, :], v1[:, :, sl, :], v2[:, :, sl, :], ALU.subtract, r=["p_v1", "p_v2"], w=["p_LtR"])
                self.tt("dve", v1[:, :, sl, :], pw_b(PWi, j0, n), x_b(cr[:], n), ALU.mult, r=["p_PWi", "p_cr"], w=["p_v1"])
                self.tt("dve", v2[:, :, sl, :], pw_b(PWr, j0, n), x_b(ci[:], n), ALU.mult, r=["p_PWr", "p_ci"], w=["p_v2"])
                self.tt("dve", v1[:, :, sl, :], v1[:, :, sl, :], v2[:, :, sl, :], ALU.add, r=["p_v1", "p_v2"], w=["p_v1"])
                self.ts("dve", nLtI[:, :, sl, :], v1[:, :, sl, :], -1.0, ALU.mult, r=["p_v1"], w=["p_nLtI"])
            self.cp("dve", WivR[:], LtR[:, :, 1:9, :].rearrange("p g t c -> p g (t c)"), r=["p_LtR"], w=["WivR"])
            self.cp("dve", WivI[:], nLtI[:, :, 1:9, :].rearrange("p g t c -> p g (t c)"), r=["p_nLtI"], w=["WivI"])
            for g4 in range(8):
                pb = g4 % 2
                pk = "ps%d" % pb
                for gl in range(4):
                    g = g4 * 4 + gl
                    o = self.ps[pb][:, gl * 128:(gl + 1) * 128]
                    c.op("pe", lambda e: e.matmul(out=o, lhsT=HsR[:, g, :, :].rearrange("p s c -> p (s c)"),
                                                  rhs=LtR[:, g, 0:8, :].rearrange("p t c -> p (t c)"), start=True, stop=False),
                         r=["p_HsR", "p_LtR"], w=[pk])
                    c.op("pe", lambda e: e.matmul(out=o, lhsT=HsI[:, g, :, :].rearrange("p s c -> p (s c)"),
                                                  rhs=nLtI[:, g, 0:8, :].rearrange("p t c -> p (t c)"), start=False, stop=True),
                         r=["p_HsI", "p_nLtI"], w=[pk])
                self.tt("dve", Toep[:, g4 * 4:(g4 + 1) * 4, :], self.ps[pb][:].rearrange("p (g n) -> p g n", g=4),
                        msk[:].unsqueeze(1).broadcast_to([128, 4, 128]), ALU.mult, r=[pk, "p_msk"], w=["Toep"])
            GsR = LtR[:, :, 0:8, :].rearrange("p g t c -> p g (t c)")
            GsI = nLtI[:, :, 0:8, :].rearrange("p g t c -> p g (t c)")
            w1 = v1[:, :, 0:8, :].rearrange("p g t c -> p g (t c)")
            w2 = v2[:, :, 0:8, :].rearrange("p g t c -> p g (t c)")
            p7r = PWr[:, 14, :].unsqueeze(2).broadcast_to([64, 32, 128])
            p7i = PWi[:, 14, :].unsqueeze(2).broadcast_to([64, 32, 128])
            hr = HsR[:].rearrange("p g s c -> p g (s c)"); hi = HsI[:].rearrange("p g s c -> p g (s c)")
            self.tt("dve", w1, hr, p7r, ALU.mult, r=["p_HsR", "p_PWr"], w=["p_v1"])
            self.tt("dve", w2, hi, p7i, ALU.mult, r=["p_HsI", "p_PWi"], w=["p_v2"])
            self.tt("dve", GsR, w1, w2, ALU.subtract, r=["p_v1", "p_v2"], w=["p_LtR"])
            self.tt("dve", w1, hi, p7r, ALU.mult, r=["p_HsI", "p_PWr"], w=["p_v1"])
            self.tt("dve", w2, hr, p7i, ALU.mult, r=["p_HsR", "p_PWi"], w=["p_v2"])
            self.tt("dve", GsI, w1, w2, ALU.add, r=["p_v1", "p_v2"], w=["p_nLtI"])
            for g4 in range(8):
                pb = 2 + g4 % 2
                pk = "ps%d" % pb
                pv = self.ps[pb][:].rearrange("p (g r n) -> p g r n", g=4, r=2)
                for gl in range(4):
                    g = g4 * 4 + gl
                    c.op("pe", lambda e: e.transpose(out=pv[:, gl, 0, :], in_=GsR[:, g, :], identity=idf[0:64, 0:64]),
                         r=["p_LtR", "p_idf"], w=[pk])
                    c.op("pe", lambda e: e.transpose(out=pv[:, gl, 1, :], in_=GsI[:, g, :], identity=idf[0:64, 0:64]),
                         r=["p_nLtI", "p_idf"], w=[pk])
                self.cp("act", Wii[:, g4 * 4:(g4 + 1) * 4, :, :], pv, r=[pk], w=["Wii"])
            self.cp("dve", rho8[:], TE[:, 15, :], r=["p_TE"], w=["p_rho8"])
            if "toep_x" in self.export:
                c.dma(self.toep_x, Toep[:], r=["Toep"], w=["toep_x"])
                c.dma(self.wii_x, Wii[:], r=["Wii"], w=["wii_x"])
                c.dma(self.wiv_x[:, 0], WivR[:], r=["WivR"], w=["wiv_x"])
                c.dma(self.wiv_x[:, 1], WivI[:], r=["WivI"], w=["wiv_x"])
        c.barrier()
        with self.scope() as sp:
            T = lambda name, shape, dt=F32: sp.enter_context(self.sbt(name, shape, dt))
            phr = T("p_phr", [64, 32]); ph_s = T("p_phs", [64, 32]); phr2 = T("p_phr2", [64, 32])
            self.ts("dve", phr[:], thr[:], 8.0, ALU.mult, r=["p_thr"], w=["p_phr"])
            self.sincos(phr[:], "p_phr", ph_s[:], None, "p_phs", None, tfs[:], tis[:], red_o=(phr2[:], "p_phr2"))
            rho = T("p_rho", [64, 8, 256])
            ang = T("p_ang", [64, 8, 256]); sk = T("p_sk", [64, 8, 256]); ck = T("p_ck", [64, 8, 256])
            tf4 = T("p_tf4", [64, 8, 256]); ti4 = T("p_ti4", [64, 8, 256], I32)
            for gb in range(4):
                gs = slice(gb * 8, (gb + 1) * 8)
                self.cp("dve", rho[:], rho8[:, gs].unsqueeze(2).broadcast_to([64, 8, 256]), r=["p_rho8"], w=["p_rho"])
                c.op("dve", lambda e: e.memset(rho[:, :, 0:1], 0.0), r=[], w=["p_rho"])
                c.dma(self.rhot[:, gs, :], rho[:], r=["p_rho"], w=["rhot"])
                self.tt("dve", ang[:], phr2[:, gs].unsqueeze(2).broadcast_to([64, 8, 256]),
                        kv[:].unsqueeze(1).broadcast_to([64, 8, 256]), ALU.mult, r=["p_phr2", "p_kv"], w=["p_ang"])
                self.sincos(ang[:], "p_ang", sk[:], ck[:], "p_sk", "p_ck", tf4[:], ti4[:])
                c.dma(self.rots[:, gs, :], sk[:], r=["p_sk"], w=["rots"])
                c.dma(self.rotc[:, gs, :], ck[:], r=["p_ck"], w=["rotc"])
        c.barrier()
        so.close()

    def phase_s5(self):
        nc, c = self.nc, self.c
        with self.scope() as st:
            T = lambda name, shape, dt=F32: st.enter_context(self.sbt(name, shape, dt))
            Toep = T("Toep", [128, 32, 128], BF16)
            Wii = T("Wii", [128, 32, 2, 64], BF16)
            WivR = T("WivR", [64, 32, 128], BF16)
            WivI = T("WivI", [64, 32, 128], BF16)
            self.s5_precompute(st, Toep, Wii, WivR, WivI)
            if self.s5_main_enabled:
                self.s5_main(st, Toep, Wii, WivR, WivI)
        c.barrier()

    def s5_main(self, st0, Toep, Wii, WivR, WivI):
        nc, c = self.nc, self.c
        GB = 4
        with self.scope() as st:
            T = lambda name, shape, dt=F32: st.enter_context(self.sbt(name, shape, dt))
            gluw = T("gluw", [128, 4, BR], BF16)
            gst = T("gluw_st", [128, 4, BR], F32)
            c.dma(gst[:], self.glu_w.rearrange("(c p) n -> p c n", p=128), w=["gluw_st"])
            self.cp("dve", gluw[:], gst[:], r=["gluw_st"], w=["gluw"])
            Drep = T("Drep", [128, BR]); Brep = T("Brep", [128, BR])
            c.dma(Drep[:], self.s5d_rep, w=["Drep"])
            c.dma(Brep[:], self.glub_rep, w=["Brep"])
            for q in range(self.nseq):
                tb = q * self.S
                with self.scope() as sq:
                    TQ = lambda name, shape, dt=F32: sq.enter_context(self.sbt(name, shape, dt))
                    uck = [TQ("uck%d" % i, [128, 8 * BR], BF16) for i in range(2)]
                    szck = [TQ("szck%d" % i, [128, 8 * BR], BF16) for i in range(2)]
                    yck = [TQ("yck%d" % i, [128, 8, BR], BF16) for i in range(2)]
                    for kh in range(2):
                        rows = slice(tb + kh * 1024, tb + (kh + 1) * 1024)
                        c.dma(uck[kh][:], self.u_tok[rows, :].rearrange("(k t) f -> k (t f)", t=8), r=["u_tok"], w=["uck%d" % kh])
                        c.dma(szck[kh][:], self.sz_tok[rows, :].rearrange("(k t) f -> k (t f)", t=8), r=["sz_tok"], w=["szck%d" % kh])
                    with self.scope() as ss:
                        TS = lambda name, shape, dt=F32: ss.enter_context(self.sbt(name, shape, dt))
                        Ug = TS("Ug", [128, 32, 256], BF16)
                        Vb2 = [TS("Vb%d" % i, [64, 2, GB, 256]) for i in range(2)]
                        Wb2 = [TS("Wb%d" % i, [64, 2, GB, 256]) for i in range(2)]
                        tA2 = [TS("tA%d" % i, [64, GB, 256]) for i in range(2)]; tB2 = [TS("tB%d" % i, [64, GB, 256]) for i in range(2)]
                        ck2 = [TS("ck%d" % i, [64, GB, 256]) for i in range(2)]; sk2 = [TS("sk%d" % i, [64, GB, 256]) for i in range(2)]
                        rh2 = [TS("rh%d" % i, [64, GB, 256]) for i in range(2)]
                        Xs2 = [TS("Xs%d" % i, [64, 2, GB, 256], BF16) for i in range(2)]
                        Ysb = TS("Ysb", [128, 8, 256], BF16)
                        for i in range(2):
                            c.op("pool", lambda e: e.memset(Xs2[i][:], 0.0), w=["Xs%d" % i])

                        def load_tabs(b):
                            if b < 32 // GB:
                                gs_ = slice(b * GB, (b + 1) * GB)
                                c.dma(ck2[b % 2][:], self.rotc[:, gs_, :], r=["rotc"], w=["ck%d" % (b % 2)])
                                c.dma(sk2[b % 2][:], self.rots[:, gs_, :], r=["rots"], w=["sk%d" % (b % 2)])
                                c.dma(rh2[b % 2][:], self.rhot[:, gs_, :], r=["rhot"], w=["rh%d" % (b % 2)])
                        load_tabs(0)
                        ucg = TS("ucg", [128, 32, 128], BF16)
                        for kh in range(2):
                            self.cp("pool" if kh == 0 else "dve", ucg[:].rearrange("p g (s c) -> p g s c", s=8),
                                    uck[kh][:].rearrange("p (s g c) -> p g s c", s=8, g=32), r=["uck%d" % kh], w=["ucg"])
                            for g8 in range(4):
                                pb = g8 % 2
                                pk = "ps%d" % pb
                                ptv = self.ps[pb][:].bitcast(BF16).rearrange("p (g k) -> p g k", g=8)
                                for gl in range(8):
                                    g = g8 * 8 + gl
                                    c.op("pe", lambda e: e.transpose(out=ptv[:, gl, :], in_=ucg[:, g, :],
                                                                     identity=self.ident[:]), r=["ucg", "ident"], w=[pk])
                                self.cp("dve" if g8 % 2 == 0 else "act", Ug[:, g8 * 8:(g8 + 1) * 8, kh * 128:(kh + 1) * 128], ptv,
                                        r=[pk], w=["Ug"])
                        for b in range(32 // GB):
                            gs = slice(b * GB, (b + 1) * GB)
                            bp = b % 2
                            Vb, Wb, tA, tB, ck, sk, rh, Xs = Vb2[bp], Wb2[bp], tA2[bp], tB2[bp], ck2[bp], sk2[bp], rh2[bp], Xs2[bp]
                            kVb, kW0, kW1, ktA, ktB, kck, ksk, krh, kXs = ("Vb%d" % bp, "Wb0_%d" % bp, "Wb1_%d" % bp, "tA%d" % bp, "tB%d" % bp,
                                                                           "ck%d" % bp, "sk%d" % bp, "rh%d" % bp, "Xs%d" % bp)
                            load_tabs(b + 1)
                            for gl in range(GB):
                                g = b * GB + gl
                                pb = 2 + gl % 2
                                pk = "ps%d" % pb
                                c.op("pe", lambda e: e.matmul(out=self.ps[pb][0:64, 0:256], lhsT=Wii[:, g, 0, :], rhs=Ug[:, g, :],
                                                              start=True, stop=True), r=["Wii", "Ug"], w=[pk])
                                c.op("pe", lambda e: e.matmul(out=self.ps[pb][0:64, 256:512], lhsT=Wii[:, g, 1, :], rhs=Ug[:, g, :],
                                                              start=True, stop=True), r=["Wii", "Ug"], w=[pk])
                                self.cp("act", Vb[:, :, gl, :], self.ps[pb][0:64, :].rearrange("p (r k) -> p r k", r=2), r=[pk], w=[kVb])
                            self.tt("dve", tA[:], ck[:], Vb[:, 0], ALU.mult, r=[kck, kVb], w=[ktA])
                            self.tt("pool", tB[:], sk[:], Vb[:, 1], ALU.mult, r=[ksk, kVb], w=[ktB])
                            self.tt("dve", Wb[:, 0], tA[:], tB[:], ALU.add, r=[ktA, ktB], w=[kW0])
                            self.tt("pool", tA[:], ck[:], Vb[:, 1], ALU.mult, r=[kck, kVb], w=[ktA])
                            self.tt("dve", tB[:], sk[:], Vb[:, 0], ALU.mult, r=[ksk, kVb], w=[ktB])
                            self.tt("pool", Wb[:, 1], tA[:], tB[:], ALU.subtract, r=[ktA, ktB], w=[kW1])
                            fl = lambda a: a.rearrange("p g k -> p (g k)")
                            c.op("dve", lambda e: e.tensor_tensor_scan(out=fl(Vb[:, 0]), data0=fl(rh[:]), data1=fl(Wb[:, 0]), initial=0.0,
                                                                       op0=ALU.mult, op1=ALU.add), r=[krh, kW0, kVb], w=[kVb])
                            c.op("dve", lambda e: e.tensor_tensor_scan(out=fl(Vb[:, 1]), data0=fl(rh[:]), data1=fl(Wb[:, 1]), initial=0.0,
                                                                       op0=ALU.mult, op1=ALU.add), r=[krh, kW1, kVb], w=[kVb])
                            K1 = 255
                            self.tt("dve", tA[:, :, 0:K1], ck[:, :, 0:K1], Vb[:, 0, :, 0:K1], ALU.mult, r=[kck, kVb], w=[ktA])
                            self.tt("pool", tB[:, :, 0:K1], sk[:, :, 0:K1], Vb[:, 1, :, 0:K1], ALU.mult, r=[ksk, kVb], w=[ktB])
                            self.tt("dve", Xs[:, 0, :, 1:256], tA[:, :, 0:K1], tB[:, :, 0:K1], ALU.subtract, r=[ktA, ktB], w=[kXs])
                            self.tt("pool", tA[:, :, 0:K1], ck[:, :, 0:K1], Vb[:, 1, :, 0:K1], ALU.mult, r=[kck, kVb], w=[ktA])
                            self.tt("dve", tB[:, :, 0:K1], sk[:, :, 0:K1], Vb[:, 0, :, 0:K1], ALU.mult, r=[ksk, kVb], w=[ktB])
                            self.tt("pool", Xs[:, 1, :, 1:256], tA[:, :, 0:K1], tB[:, :, 0:K1], ALU.add, r=[ktA, ktB], w=[kXs])
                            for gl in range(GB):
                                g = b * GB + gl
                                pb = 4 + gl // 2 % 2
                                pk = "ps%d" % pb
                                o = self.ps[pb][:, (gl % 2) * 256:(gl % 2 + 1) * 256]
                                c.op("pe", lambda e: e.matmul(out=o, lhsT=Toep[:, g, :], rhs=Ug[:, g, :], start=True, stop=False),
                                     r=["Toep", "Ug"], w=[pk])
                                c.op("pe", lambda e: e.matmul(out=o, lhsT=WivR[:, g, :], rhs=Xs[:, 0, gl, :], start=False, stop=False),
                                     r=["WivR", kXs], w=[pk])
                                c.op("pe", lambda e: e.matmul(out=o, lhsT=WivI[:, g, :], rhs=Xs[:, 1, gl, :], start=False, stop=True),
                                     r=["WivI", kXs], w=[pk])
                                if gl % 2 == 1:
                                    g8l = (b * GB + gl - 1) % 8
                                    self.cp("act", Ysb[:, g8l:g8l + 2, :], self.ps[pb][:].rearrange("p (g k) -> p g k", g=2), r=[pk], w=["Ysb"])
                            if (b * GB + GB) % 8 == 0:
                                g8 = (b * GB) // 8
                                for kh in range(2):
                                    pb = 6 + kh
                                    pk = "ps%d" % pb
                                    ptv = self.ps[pb][:].bitcast(BF16).rearrange("p (g n) -> p g n", g=8)
                                    for gl in range(8):
                                        c.op("pe", lambda e: e.transpose(out=ptv[:, gl, :], in_=Ysb[:, gl, kh * 128:(kh + 1) * 128],
                                                                         identity=self.ident[:]), r=["Ysb", "ident"], w=[pk])
                                    self.cp("dve", yck[kh][:, :, g8 * 128:(g8 + 1) * 128].rearrange("p t (g c) -> p t g c", g=8),
                                            ptv.rearrange("p g (t c) -> p t g c", t=8), r=[pk], w=["yck%d" % kh])
                    with self.scope() as se:
                        TE_ = lambda name, shape, dt=F32: se.enter_context(self.sbt(name, shape, dt))
                        t1 = TE_("e_t1", [128, 8, BR]); t2 = TE_("e_t2", [128, 8, BR])
                        gck = TE_("gck", [128, 8, BR], BF16)
                        ack = TE_("ack", [128, 8, BR], BF16)
                        gT = [TE_("gT%d" % i, [128, 4, 128], BF16) for i in range(2)]
                        e1 = [TE_("e1_%d" % i, [128, BR]) for i in range(2)]
                        for kh in range(2):
                            uv = uck[kh][:].rearrange("p (s f) -> p s f", s=8)
                            zv = szck[kh][:].rearrange("p (s f) -> p s f", s=8)
                            self.tt("dve", t1[:], uv, Drep[:].unsqueeze(1).broadcast_to([128, 8, BR]), ALU.mult, r=["uck%d" % kh, "Drep"], w=["e_t1"])
                            self.tt("pool", t1[:], t1[:], yck[kh][:], ALU.add, r=["e_t1", "yck%d" % kh], w=["e_t1"])
                            self.actf(t2[:], t1[:], AF.Square, r=["e_t1"], w=["e_t2"])
                            self.ts("dve", t2[:], t2[:], 0.044715 * 0.7978845608, ALU.mult, r=["e_t2"], w=["e_t2"], s2=0.7978845608, op1=ALU.add)
                            self.tt("pool", t2[:], t2[:], t1[:], ALU.mult, r=["e_t2", "e_t1"], w=["e_t2"])
                            self.actf(t2[:], t2[:], AF.Tanh, r=["e_t2"], w=["e_t2"])
                            self.ts("dve", t2[:], t2[:], 1.0, ALU.add, r=["e_t2"], w=["e_t2"], s2=0.5, op1=ALU.mult)
                            self.tt("dve", gck[:], t2[:], t1[:], ALU.mult, r=["e_t2", "e_t1"], w=["gck"])
                            for tau in range(8):
                                i2 = tau % 2
                                pk = "ps%d" % i2
                                ptv = self.ps[i2][:].bitcast(BF16).rearrange("p (a b) -> p a b", a=8)
                                for fc in range(4):
                                    c.op("pe", lambda e: e.transpose(out=ptv[:, fc, :], in_=gck[:, tau, fc * 128:(fc + 1) * 128],
                                                                     identity=self.ident[:]), r=["gck", "ident"], w=[pk])
                                self.cp("act", gT[i2][:], ptv[:, 0:4, :], r=[pk], w=["gT%d" % i2])
                                pm = 2 + i2
                                for fc in range(4):
                                    c.op("pe", lambda e: e.matmul(out=self.ps[pm][:], lhsT=gT[i2][:, fc, :], rhs=gluw[:, fc, :],
                                                                  start=(fc == 0), stop=(fc == 3)), r=["gT%d" % i2, "gluw"], w=["ps%d" % pm])
                                ek = "e1_%d" % i2
                                self.tt("dve", e1[i2][:], self.ps[pm][:], Brep[:], ALU.add, r=["ps%d" % pm, "Brep"], w=[ek])
                                self.actf(e1[i2][:], e1[i2][:], AF.Tanh, r=[ek], w=[ek], scale=0.5)
                                self.ts("dve", e1[i2][:], e1[i2][:], 0.5, ALU.mult, r=[ek], w=[ek], s2=0.5, op1=ALU.add)
                                self.tt("pool", e1[i2][:], e1[i2][:], gck[:, tau, :], ALU.mult, r=[ek, "gck"], w=[ek])
                                self.tt("dve", ack[:, tau, :], e1[i2][:], zv[:, tau, :], ALU.mult, r=[ek, "szck%d" % kh], w=["ack"])
                            rows = slice(tb + kh * 1024, tb + (kh + 1) * 1024)
                            c.dma(self.a_tok[rows, :].rearrange("(k t) f -> k (t f)", t=8), ack[:].rearrange("p t f -> p (t f)"),
                                  r=["ack"], w=["a_tok"])

    def phase_ml(self):
        nc, c = self.nc, self.c
        S_ = self.S
        NB = S_ // 128
        SC = 128 ** -0.5
        with self.scope() as st:
            T = lambda name, shape, dt=F32: st.enter_context(self.sbt(name, shape, dt))
            cw = T("m_cw", [128, 4, 4]); cb = T("m_cb", [128, 4])
            mn = T("m_mn", [128, 4]); msk = T("m_msk", [128, 4])
            gbi = T("m_gbi", [4, 1]); gbf = T("m_gbf", [4, 1]); ngbf = T("m_ngbf", [4, 1])
            maskS = T("m_maskS", [128, 128]); idf = T("m_idf", [128, 128])
            ones4 = T("m_ones4", [4, 128])
            wst = T("m_wst", [128, 3, 4, 128]); wqkv = T("m_wqkv", [128, 3, 4, 128], BF16)
            gst = T("m_gst", [128, 12, 8]); gw = T("m_gw", [128, 12, 8], BF16)
            for (t, d, key) in ((cw, self.ml_cw, "m_cw"), (cb, self.ml_cb, "m_cb"), (mn, self.ml_mn, "m_mn"),
                                (msk, self.ml_msk, "m_msk"), (gbi, self.ml_gbi, "m_gbi"), (gbf, self.ml_gbf, "m_gbf"),
                                (maskS, self.ml_maskS, "m_maskS"), (idf, self.identf_d, "m_idf"),
                                (gst, self.ml_gw, "m_gst")):
                c.dma(t[:], d, w=[key])
            for i, d in enumerate((self.ml_wq, self.ml_wk, self.ml_wv)):
                c.dma(wst[:, i], d, w=["m_wst"])
            self.cp("dve", wqkv[:], wst[:], r=["m_wst"], w=["m_wqkv"])
            self.cp("dve", gw[:], gst[:], r=["m_gst"], w=["m_gw"])
            self.ts("dve", ngbf[:], gbf[:], -1.0, ALU.mult, r=["m_gbf"], w=["m_ngbf"])
            c.op("dve", lambda e: e.memset(ones4[:], 1.0), w=["m_ones4"])
            xmT_v = self.xmT.rearrange("(c p) t -> p c t", p=128)
            mzT_v = self.mzT.rearrange("(c p) t -> p c t", p=128)
            bT_v = self.bT.rearrange("(c p) t -> p c t", p=128)
            for q in range(self.nseq):
                tb = q * S_
                with self.scope() as sq:
                    TQ = lambda name, shape, dt=F32: sq.enter_context(self.sbt(name, shape, dt))
                    xcT = TQ("xcT", [128, 4, S_], BF16)
                    qT = TQ("qT", [128, 4, S_], BF16)
                    kT = TQ("kT", [128, 4, S_], BF16)
                    Ktok = TQ("Ktok", [128, NB, 4, 128], BF16)
                    Vtok = TQ("Vtok", [128, NB, 4, 129], BF16)
                    acol = TQ("acol", [128, NB, 4]); bcol = TQ("bcol", [128, NB, 4])
                    Rrep = TQ("Rrep", [128, NB + 1, 4])
                    Wt = TQ("Wt", [128, NB, 4]); Wp = TQ("Wp", [128, NB, 4])
                    Thr = TQ("Thr", [128, NB, 4]); Dec = TQ("Dec", [128, NB, 4])
                    c.op("pool", lambda e: e.memset(Vtok[:, :, :, 128:129], 1.0), w=["Vtok"])
                    with self.scope() as sa:
                        TA_ = lambda name, shape, dt=F32: sa.enter_context(self.sbt(name, shape, dt))
                        xm = TA_("xm", [128, 4, S_], BF16)
                        vT = TA_("vT", [128, 4, S_], BF16)
                        acc = TA_("acc", [128, S_])
                        g1f = TA_("g1", [32, S_]); g2f = TA_("g2", [32, S_]); g3f = TA_("g3", [32, S_]); onesr = TA_("onesr", [32, S_])
                        g1 = g1f[0:4, :]; g2 = g2f[0:4, :]; g3 = g3f[0:4, :]
                        c.op("pool", lambda e: e.memset(g1f[:], 0.0), w=["g1"])
                        c.op("pool", lambda e: e.memset(g2f[:], 0.0), w=["g2"])
                        rsel = TA_("rsel", [4, NB, 4])
                        c.dma(xm[:], xmT_v[:, :, tb:tb + S_], r=["xmT"], w=["xm"])
                        c.op("pool", lambda e: e.memset(onesr[:], 1.0), w=["onesr"])
                        for fc in range(4):
                            self.ts("dve", acc[:], xm[:, fc, :], cw[:, fc, 3:4], ALU.mult, r=["xm", "m_cw", "m_cb"], w=["acc"],
                                    s2=cb[:, fc:fc + 1], op1=ALU.add)
                            for sh in (1, 2, 3):
                                self.stt("dve", acc[:, sh:], xm[:, fc, 0:S_ - sh], cw[:, fc, 3 - sh:4 - sh], acc[:, sh:],
                                         ALU.mult, ALU.add, r=["xm", "m_cw", "acc"], w=["acc"])
                            self.actf(xcT[:, fc, :], acc[:], AF.Silu, r=["acc"], w=["xcT"])
                        for h in range(4):
                            for tl in range(S_ // 512):
                                ts_ = slice(tl * 512, (tl + 1) * 512)
                                for (i, src, skey, dst, dkey) in ((0, xcT, "xcT", qT, "qT"), (1, xcT, "xcT", kT, "kT"), (2, xm, "xm", vT, "vT")):
                                    pb = (h * 12 + tl * 3 + i) % 4
                                    pk = "ps%d" % pb
                                    c.op("pe", lambda e: e.matmul(out=self.ps[pb][:], lhsT=wqkv[:, i, h, :], rhs=src[:, h, ts_],
                                                                  start=True, stop=True), r=["m_wqkv", skey], w=[pk])
                                    self.cp("act" if i != 1 else "dve", dst[:, h, ts_], self.ps[pb][:], r=[pk], w=[dkey])
                        for blk in range(NB):
                            bs = slice(blk * 128, (blk + 1) * 128)
                            for (i, src, skey, dst, dkey, pb) in ((1, xcT, "xcT", Ktok, "Ktok", 4), (2, xm, "xm", Vtok, "Vtok", 5)):
                                pb = pb + 2 * (blk % 2)
                                pk = "ps%d" % pb
                                for h in range(4):
                                    c.op("pe", lambda e: e.matmul(out=self.ps[pb][:, h * 128:(h + 1) * 128], lhsT=src[:, h, bs],
                                                                  rhs=wqkv[:, i, h, :], start=True, stop=True), r=["m_wqkv", skey], w=[pk])
                                self.cp("act" if i == 1 else "dve", dst[:, blk, :, 0:128], self.ps[pb][:].rearrange("p (h e) -> p h e", h=4),
                                        r=[pk], w=[dkey])
                        for tl in range(S_ // 512):
                            ts_ = slice(tl * 512, (tl + 1) * 512)
                            for half in range(2):
                                pb = half
                                pk = "ps%d" % pb
                                for ch in range(12):
                                    src = (qT, kT, vT)[ch // 4]
                                    skey = ("qT", "kT", "vT")[ch // 4]
                                    c.op("pe", lambda e: e.matmul(out=self.ps[pb][0:4, :], lhsT=gw[:, ch, half * 4:half * 4 + 4],
                                                                  rhs=src[:, ch % 4, ts_], start=(ch == 0), stop=(ch == 11)),
                                         r=["m_gw", skey], w=[pk])
                                if half == 0:
                                    self.ts("dve", g1[:, ts_], self.ps[pb][0:4, :], gbi[:, 0:1], ALU.add, r=[pk, "m_gbi"], w=["g1"])
                                else:
                                    self.actf(g2[:, ts_], self.ps[pb][0:4, :], AF.Exp, r=[pk, "m_ngbf"], w=["g2"], scale=-1.0, bias=ngbf[:, 0:1])
                        self.actf(g2[:], g2[:], AF.Ln, r=["g2"], w=["g2"], bias=1.0)
                        c.op("dve", lambda e: e.tensor_tensor_scan(out=g3f[:], data0=onesr[:], data1=g2f[:], initial=0.0, op0=ALU.mult, op1=ALU.add),
                             r=["onesr", "g2"], w=["g3"])
                        self.tt("dve", g1[:], g1[:], g3[:], ALU.add, r=["g1", "g3"], w=["g1"])
                        c.op("dve", lambda e: e.tensor_tensor_scan(out=g2f[:], data0=onesr[:], data1=g1f[:], initial=0.0, op0=ALU.mult, op1=ALU.max),
                             r=["onesr", "g1", "g2"], w=["g2"])
                        pa = self.ps[2][:, 0:NB * 4].rearrange("p (b h) -> p b h", h=4)
                        pbn = self.ps[3][:, 0:NB * 4].rearrange("p (b h) -> p b h", h=4)
                        for blk in range(NB):
                            bs = slice(blk * 128, (blk + 1) * 128)
                            c.op("pe", lambda e: e.transpose(out=pa[:, blk, :], in_=g1[:, bs], identity=idf[0:4, 0:4]), r=["g1", "m_idf"], w=["ps2"])
                            c.op("pe", lambda e: e.transpose(out=pbn[:, blk, :], in_=g3[:, bs], identity=idf[0:4, 0:4]), r=["g3", "m_idf"], w=["ps3"])
                        self.cp("dve", acol[:], pa, r=["ps2"], w=["acol"])
                        self.cp("dve", bcol[:], pbn, r=["ps3"], w=["bcol"])
                        self.tt("dve", rsel[:], g2[:, 127::128].unsqueeze(2).broadcast_to([4, NB, 4]),
                                idf[0:4, 0:4].unsqueeze(1).broadcast_to([4, NB, 4]), ALU.mult, r=["g2", "m_idf"], w=["rsel"])
                        c.op("pe", lambda e: e.matmul(out=self.ps[0][:, 0:NB * 4], lhsT=ones4[:], rhs=rsel[:].rearrange("p b h -> p (b h)"),
                                                      start=True, stop=True), r=["m_ones4", "rsel"], w=["ps0"])
                        c.op("dve", lambda e: e.memset(Rrep[:, 0, :], 0.0), w=["Rrep"])
                        self.cp("dve", Rrep[:, 1:NB + 1, :], self.ps[0][:, 0:NB * 4].rearrange("p (b h) -> p b h", h=4), r=["ps0"], w=["Rrep"])
                    self.tt("dve", Wt[:], acol[:], Rrep[:, 0:NB, :], ALU.subtract, r=["acol", "Rrep"], w=["Wt"])
                    self.actf(Wt[:], Wt[:], AF.Exp, r=["Wt"], w=["Wt"])
                    self.tt("dve", Wp[:], acol[:], Rrep[:, 1:NB + 1, :], ALU.subtract, r=["acol", "Rrep"], w=["Wp"])
                    self.actf(Wp[:], Wp[:], AF.Exp, r=["Wp"], w=["Wp"])
                    self.ts("dve", Wp[:], Wp[:], SC, ALU.mult, r=["Wp"], w=["Wp"])
                    self.tt("dve", Thr[:], bcol[:], Rrep[:, 0:NB, :], ALU.subtract, r=["bcol", "Rrep"], w=["Thr"])
                    self.actf(Thr[:], Thr[:], AF.Exp, r=["Thr"], w=["Thr"])
                    self.tt("dve", Dec[:], Rrep[:, 0:NB, :], Rrep[:, 1:NB + 1, :], ALU.subtract, r=["Rrep"], w=["Dec"])
                    self.actf(Dec[:], Dec[:], AF.Exp, r=["Dec"], w=["Dec"])
                    with self.scope() as sm:
                        TM = lambda name, shape, dt=F32: sm.enter_context(self.sbt(name, shape, dt))
                        C32 = [TM("C32_%d" % h, [128, 129]) for h in range(4)]
                        Cb = [TM("Cb_%d" % h, [128, 129], BF16) for h in range(4)]
                        PT = [TM("PT%d" % i, [128, 128], BF16) for i in range(2)]
                        Vp = [TM("Vp%d" % i, [128, 129], BF16) for i in range(2)]
                        Vpp = [TM("Vpp%d" % i, [128, 129], BF16) for i in range(2)]
                        den = [TM("den%d" % i, [128, 1]) for i in range(2)]
                        hraw = [TM("hraw%d" % i, [128, 4, 128]) for i in range(2)]
                        bst = TM("bst", [128, 4, 6]); mv = TM("mv", [128, 4, 2]); rs = TM("rs", [128, 4])
                        hn = TM("hn", [128, 4, 128], BF16)
                        e1 = TM("m_e1", [128, 4, 128]); e2 = TM("m_e2", [128, 4, 128])
                        mz = [TM("mz%d" % i, [128, 4, 512], BF16) for i in range(2)]
                        bo = [TM("bo%d" % i, [128, 4, 512], BF16) for i in range(2)]
                        def emit_front(n):
                            I_, h_ = divmod(n, 4)
                            bs_ = slice(I_ * 128, (I_ + 1) * 128)
                            j_ = n % 2
                            c.op("pe", lambda e: e.matmul(out=self.ps[j_][:, 0:128], lhsT=kT[:, h_, bs_], rhs=qT[:, h_, bs_], start=True, stop=True),
                                 r=["kT", "qT"], w=["ps%d" % j_])
                            self.tt("dve", PT[j_][:], self.ps[j_][:, 0:128], maskS[:], ALU.mult, r=["ps%d" % j_, "m_maskS"], w=["PT%d" % j_])
                            c.op("act", lambda e: e.activation(out=Vp[j_][:], in_=Vtok[:, I_, h_, :], func=AF.Copy, scale=Wt[:, I_, h_:h_ + 1]),
                                 r=["Vtok", "Wt"], w=["Vp%d" % j_])
                            c.op("act", lambda e: e.activation(out=Vpp[j_][:], in_=Vtok[:, I_, h_, :], func=AF.Copy, scale=Wp[:, I_, h_:h_ + 1]),
                                 r=["Vtok", "Wp"], w=["Vpp%d" % j_])
                        emit_front(0)
                        for I in range(NB):
                            bs = slice(I * 128, (I + 1) * 128)
                            i4 = (I // 4) % 2
                            if I % 4 == 0:
                                c.dma(mz[i4][:], mzT_v[:, :, tb + I * 128:tb + I * 128 + 512], r=["mzT"], w=["mz%d" % i4])
                            hk = "hraw%d" % (I % 2)
                            for h in range(4):
                                n = I * 4 + h
                                j = n % 2
                                pO = self.ps[2 + j]; pC = self.ps[4 + j]
                                kO, kC = "ps%d" % (2 + j), "ps%d" % (4 + j)
                                c.op("pe", lambda e: e.matmul(out=pO[:, 0:129], lhsT=PT[j][:], rhs=Vp[j][:], start=True, stop=(I == 0)),
                                     r=["PT%d" % j, "Vp%d" % j], w=[kO])
                                if I > 0:
                                    c.op("pe", lambda e: e.matmul(out=pO[:, 0:129], lhsT=qT[:, h, bs], rhs=Cb[h][:], start=False, stop=True),
                                         r=["qT", "Cb_%d" % h], w=[kO])
                                if I < NB - 1:
                                    c.op("pe", lambda e: e.matmul(out=pC[:, 0:129], lhsT=Ktok[:, I, h, :], rhs=Vpp[j][:], start=True, stop=True),
                                         r=["Ktok", "Vpp%d" % j], w=[kC])
                                if n + 1 < NB * 4:
                                    emit_front(n + 1)
                                if I < NB - 1:
                                    if I == 0:
                                        self.cp("dve", C32[h][:], pC[:, 0:129], r=[kC], w=["C32_%d" % h])
                                    else:
                                        self.stt("dve", C32[h][:], C32[h][:], Dec[:, I, h:h + 1], pC[:, 0:129], ALU.mult, ALU.add,
                                                 r=["C32_%d" % h, "Dec", kC], w=["C32_%d" % h])
                                    self.cp("act", Cb[h][:], C32[h][:], r=["C32_%d" % h], w=["Cb_%d" % h])
                                dk = "den%d" % j
                                self.actf(den[j][:], pO[:, 128:129], AF.Abs, r=[kO], w=[dk])
                                self.tt("dve", den[j][:], den[j][:], Thr[:, I, h:h + 1], ALU.max, r=[dk, "Thr"], w=[dk])
                                c.op("dve", lambda e: e.reciprocal(out=den[j][:], in_=den[j][:]), r=[dk], w=[dk])
                                c.op("act", lambda e: e.activation(out=hraw[I % 2][:, h, :], in_=pO[:, 0:128], func=AF.Copy, scale=den[j][:, 0:1]),
                                     r=[kO, dk], w=[hk])
                                c.op("dve", lambda e: e.bn_stats(out=bst[:, h, :], in_=hraw[I % 2][:, h, :]), r=[hk], w=["bst"])
                                c.op("dve", lambda e: e.bn_aggr(out=mv[:, h, :], in_=bst[:, h, :]), r=["bst"], w=["mv"])
                            self.actf(rs[:], mv[:, :, 1], AF.Ln, r=["mv"], w=["rs"], bias=HEAD_EPS)
                            self.actf(rs[:], rs[:], AF.Exp, r=["rs"], w=["rs"], scale=-0.5)
                            self.tt("dve", e1[:], hraw[I % 2][:], mv[:, :, 0:1].broadcast_to([128, 4, 128]), ALU.subtract, r=[hk, "mv"], w=["m_e1"])
                            self.tt("dve", hn[:], e1[:], rs[:].unsqueeze(2).broadcast_to([128, 4, 128]), ALU.mult, r=["m_e1", "rs"], w=["hn"])
                            ptv = self.ps[6 + I % 2][:].bitcast(BF16).rearrange("p (a b) -> p a b", a=8)
                            pk = "ps%d" % (6 + I % 2)
                            for h in range(4):
                                c.op("pe", lambda e: e.transpose(out=ptv[:, h, :], in_=hn[:, h, :], identity=self.ident[:]), r=["hn", "ident"], w=[pk])
                            self.tt("dve", e1[:], ptv[:, 0:4, :], mn[:].unsqueeze(2).broadcast_to([128, 4, 128]), ALU.mult, r=[pk, "m_mn", "m_e1"], w=["m_e1"])
                            self.tt("pool", e2[:], xcT[:, :, bs], msk[:].unsqueeze(2).broadcast_to([128, 4, 128]), ALU.mult, r=["xcT", "m_msk"], w=["m_e2"])
                            self.tt("dve", e1[:], e1[:], e2[:], ALU.add, r=["m_e1", "m_e2"], w=["m_e1"])
                            off = (I % 4) * 128
                            self.tt("pool", bo[i4][:, :, off:off + 128], e1[:], mz[i4][:, :, off:off + 128], ALU.mult,
                                    r=["m_e1", "mz%d" % i4], w=["bo%d" % i4])
                            if I % 4 == 3:
                                c.dma(bT_v[:, :, tb + (I - 3) * 128:tb + (I + 1) * 128], bo[i4][:], r=["bo%d" % i4], w=["bT"])
        c.barrier()

    def out_proj_norm_res(self, lay, wout_d, pg_rep_d, lhs_provider, res_rows, dst_rows, dst_key, res_key):
        nc, c = self.nc, self.c
        NT = self.NT
        with self.scope() as st:
            T = lambda name, shape, dt=F32: st.enter_context(self.sbt(name, shape, dt))
            wout = T("wout", [128, 8, D], BF16)
            wst = [T("wost%d" % i, [128, D]) for i in range(2)]
            for fc in range(8):
                c.dma(wst[fc % 2][:], wout_d[fc * 128:(fc + 1) * 128, :], w=["wost%d" % (fc % 2)])
                self.cp("dve" if fc % 2 else "act", wout[:, fc, :], wst[fc % 2][:], r=["wost%d" % (fc % 2)], w=["wout"])
            pg = T("pg", [128, D])
            c.dma(pg[:], pg_rep_d, w=["pg"])
            NXR = 4
            xr = [T("xr%d" % i, [128, D]) for i in range(NXR)]
            yo = [T("yo%d" % i, [128, D]) for i in range(2)]
            junk = T("ojunk", [128, BR])
            ss2 = [T("oss%d" % i, [128, 2]) for i in range(2)]; rstd2 = [T("orstd%d" % i, [128, 1]) for i in range(2)]
            pref, prov = lhs_provider(st)
            nblk = NT // 128

            def prefetch(b):
                if b < nblk:
                    c.dma(xr[b % NXR][:], res_rows[b * 128:(b + 1) * 128, :], r=[res_key % b], w=["xr%d" % (b % NXR)])
                    pref(b)
            prefetch(0)
            prefetch(1)
            for blk in range(nblk):
                rows = slice(blk * 128, (blk + 1) * 128)
                xk = "xr%d" % (blk % NXR)
                prefetch(blk + 2)
                lhs = prov(blk)
                ss = ss2[blk % 2]; rstd = rstd2[blk % 2]
                ssk = "oss%d" % (blk % 2); rsk = "orstd%d" % (blk % 2)
                for half in range(2):
                    pb = 4 + half + 2 * (blk % 2)
                    pk = "ps%d" % pb
                    for fc in range(8):
                        ap, key = lhs[fc]
                        c.op("pe", lambda e: e.matmul(out=self.ps[pb][:], lhsT=ap, rhs=wout[:, fc, half * BR:(half + 1) * BR],
                                                      start=(fc == 0), stop=(fc == 7)), r=[key, "wout"], w=[pk])
                    self.actf(junk[:], self.ps[pb][:], AF.Square, r=[pk], w=["ojunk", ssk], accum_out=ss[:, half:half + 1])
                self.tt("dve", rstd[:], ss[:, 0:1], ss[:, 1:2], ALU.add, r=[ssk], w=[rsk])
                self.actf(rstd[:], rstd[:], AF.Ln, r=[rsk], w=[rsk], scale=1.0 / D, bias=NORM_EPS)
                self.actf(rstd[:], rstd[:], AF.Exp, r=[rsk], w=[rsk], scale=-0.5)
                yk = "yo%d" % (blk % 2)
                for half in range(2):
                    pb = 4 + half + 2 * (blk % 2)
                    hs = slice(half * BR, (half + 1) * BR)
                    self.tt("dve", yo[blk % 2][:, hs], self.ps[pb][:], pg[:, hs], ALU.mult, r=["ps%d" % pb, "pg"], w=[yk])
                self.stt("dve" , yo[blk % 2][:], yo[blk % 2][:], rstd[:, 0:1], xr[blk % NXR][:], ALU.mult, ALU.add, r=[yk, rsk, xk], w=[yk])
                c.dma(dst_rows[rows, :], yo[blk % 2][:], r=[yk], w=[dst_key % blk])

    def phase_l0_out(self):
        nc, c = self.nc, self.c
        bT_v = self.bT.rearrange("(c p) t -> p c t", p=128)

        def provider(st):
            T = lambda name, shape, dt=F32: st.enter_context(self.sbt(name, shape, dt))
            at = [T("at%d" % i, [128, BR], BF16) for i in range(4)]
            aT = [T("aT%d" % i, [128, 4, 128], BF16) for i in range(2)]
            bt = [T("bt%d" % i, [128, 4, 512], BF16) for i in range(2)]

            def pref(blk):
                i4 = (blk // 4) % 2
                c.dma(at[blk % 4][:], self.a_tok[blk * 128:(blk + 1) * 128, :], r=["a_tok"], w=["at%d" % (blk % 4)])
                if blk % 4 == 0:
                    c.dma(bt[i4][:], bT_v[:, :, blk * 128:blk * 128 + 512], r=["bT"], w=["bt%d" % i4])

            def prov(blk):
                i = blk % 2
                i4 = (blk // 4) % 2
                pk = "ps%d" % i
                ptv = self.ps[i][:].bitcast(BF16).rearrange("p (a b) -> p a b", a=8)
                for fc in range(4):
                    c.op("pe", lambda e: e.transpose(out=ptv[:, fc, :], in_=at[blk % 4][:, fc * 128:(fc + 1) * 128], identity=self.ident[:]),
                         r=["at%d" % (blk % 4), "ident"], w=[pk])
                self.cp("act", aT[i][:], ptv[:, 0:4, :], r=[pk], w=["aT%d" % i])
                off = (blk % 4) * 128
                return [(aT[i][:, fc, :], "aT%d" % i) for fc in range(4)] + \
                       [(bt[i4][:, h, off:off + 128], "bt%d" % i4) for h in range(4)]
            return pref, prov
        self.out_proj_norm_res(0, self.w_out_ab, self.post0_rep, provider, self.x, self.out, "h1_%d", "x%.0d")

    def phase_l1_proj(self):
        nc, c = self.nc, self.c
        NT = self.NT
        S_ = self.S
        ngrp = NT // 512
        nblk = NT // 128
        with self.scope() as st:
            T = lambda name, shape, dt=F32: st.enter_context(self.sbt(name, shape, dt))
            win = T("win1", [128, 8, 8 * BR], BF16)
            g1 = T("g1n", [128, 8])
            stage = [T("w1st%d" % i, [128, 4 * BR]) for i in range(2)]
            c.dma(g1[:], self.pre1, w=["g1n"])
            for dc in range(8):
                for hf in range(2):
                    i = (dc * 2 + hf) % 2
                    sk = "w1st%d" % i
                    c.dma(stage[i][:], self.w_in_cd[dc * 128:(dc + 1) * 128, hf * 2048:(hf + 1) * 2048], w=[sk])
                    if hf == 0:
                        c.op("act", lambda e: e.activation(out=win[:, dc, 0:2048], in_=stage[i][:], func=AF.Copy, scale=g1[:, dc:dc + 1]),
                             r=[sk, "g1n"], w=["win1"])
                    else:
                        self.ts("dve", win[:, dc, 2048:4096], stage[i][:], g1[:, dc:dc + 1], ALU.mult, r=[sk, "g1n"], w=["win1"])
            cosT = T("cosT", [128, S_]); sinT = T("sinT", [128, S_]); Rm = T("Rm", [128, 128], BF16)
            c.dma(cosT[:], self.rope_cos, w=["cosT"])
            c.dma(sinT[:], self.rope_sin, w=["sinT"])
            c.dma(Rm[:], self.rope_rm, w=["Rm"])
            NXB = 6
            xt = [T("x1t%d" % i, [128, D]) for i in range(NXB)]
            junk = T("junk1", [128, D])
            ss2 = [T("ss1_%d" % i, [128, 4]) for i in range(2)]; rstd2 = [T("rstd1_%d" % i, [128, 4]) for i in range(2)]
            zb = [T("z1b%d" % i, [128, D], BF16) for i in range(2)]
            zT = [T("z1T%d" % i, [128, 8, 512], BF16) for i in range(2)]
            vo = [T("vo%d" % i, [128, BR], BF16) for i in range(4)]
            xb = [T("xb%d" % i, [128, 512], BF16) for i in range(2)]
            r1 = [T("r1_%d" % i, [128, 512]) for i in range(2)]
            r2 = [T("r2_%d" % i, [128, 512]) for i in range(2)]
            fo = [T("fo%d" % i, [128, 4, 512], BF16) for i in range(2)]

            def load_x(b):
                if b < nblk:
                    c.dma(xt[b % NXB][:], self.h1src[b * 128:(b + 1) * 128, :], r=["h1_%d" % b], w=["x1t%d" % (b % NXB)])
            for b in range(4):
                load_x(b)
            fm_groups = [(0, self.cqT, "cqT", "rope"), (512, self.ckT, "ckT", "rope"), (2048, self.dqT, "dqT", "rope"),
                         (2560, self.dkT, "dkT", "rope"), (1536, self.czT, "czT", "silu"), (3584, self.dzT, "dzT", "silu")]
            cnt = 0
            for g in range(ngrp):
                zk = "z1T%d" % (g % 2)
                ss = ss2[g % 2]; rstd = rstd2[g % 2]
                ssk = "ss1_%d" % (g % 2); rsk = "rstd1_%d" % (g % 2)
                pos0 = (g * 512) % S_
                for j in range(4):
                    b = g * 4 + j
                    xk = "x1t%d" % (b % NXB)
                    c.op("act", lambda e: e.activation(out=junk[:], in_=xt[b % NXB][:], func=AF.Square, accum_out=ss[:, j:j + 1]),
                         r=[xk], w=["junk1", ssk])
                self.actf(rstd[:], ss[:], AF.Ln, r=[ssk], w=[rsk], scale=1.0 / D, bias=NORM_EPS)
                self.actf(rstd[:], rstd[:], AF.Exp, r=[rsk], w=[rsk], scale=-0.5)
                for j in range(4):
                    b = g * 4 + j
                    xk = "x1t%d" % (b % NXB)
                    zbk = "z1b%d" % (b % 2)
                    pk = "ps%d" % (b % 2)
                    self.ts("dve", zb[b % 2][:], xt[b % NXB][:], rstd[:, j:j + 1], ALU.mult, r=[xk, rsk], w=[zbk])
                    load_x(b + 4)
                    ptv = self.ps[b % 2][:].bitcast(BF16).rearrange("p (a b) -> p a b", a=8)
                    for dc in range(8):
                        c.op("pe", lambda e: e.transpose(out=ptv[:, dc, :], in_=zb[b % 2][:, dc * 128:(dc + 1) * 128], identity=self.ident[:]),
                             r=[zbk, "ident"], w=[pk])
                    self.cp("dve", zT[g % 2][:, :, j * 128:(j + 1) * 128], ptv, r=[pk], w=[zk])
                    for vi, (col0, dst, dkey) in enumerate(((1024, self.cv_tok, "cv_tok"), (3072, self.dv_tok, "dv_tok"))):
                        pb = 2 + vi
                        vk = "vo%d" % ((b % 2) * 2 + vi)
                        for dc in range(8):
                            c.op("pe", lambda e: e.matmul(out=self.ps[pb][:], lhsT=zT[g % 2][:, dc, j * 128:(j + 1) * 128],
                                                          rhs=win[:, dc, col0:col0 + BR], start=(dc == 0), stop=(dc == 7)), r=[zk, "win1"], w=["ps%d" % pb])
                        self.cp("act", vo[(b % 2) * 2 + vi][:], self.ps[pb][:], r=["ps%d" % pb], w=[vk])
                        c.dma(dst[b * 128:(b + 1) * 128, :], vo[(b % 2) * 2 + vi][:], r=[vk], w=[dkey])
                pend = []

                def flush_pend():
                    while pend:
                        (fot_, fc_, fk_, i2_, pb_, dst_, dkey_, lastfc) = pend.pop(0)
                        pr = 6 + i2_
                        c.op("pe", lambda e: e.matmul(out=self.ps[pr][:], lhsT=Rm[:], rhs=xb[i2_][:], start=True, stop=True),
                             r=["Rm", "xb%d" % i2_], w=["ps%d" % pr])
                        self.tt("dve", r1[i2_][:], self.ps[pb_][:], cosT[:, pos0:pos0 + 512], ALU.mult, r=["ps%d" % pb_, "cosT"], w=["r1_%d" % i2_])
                        self.tt("dve", r2[i2_][:], self.ps[pr][:], sinT[:, pos0:pos0 + 512], ALU.mult, r=["ps%d" % pr, "sinT"], w=["r2_%d" % i2_])
                        self.tt("pool", fot_[:, fc_, :], r1[i2_][:], r2[i2_][:], ALU.add, r=["r1_%d" % i2_, "r2_%d" % i2_], w=[fk_])
                        if lastfc:
                            c.dma(dst_.rearrange("(c p) t -> p c t", p=128)[:, :, g * 512:(g + 1) * 512], fot_[:], r=[fk_], w=[dkey_])
                for (col0, dst, dkey, kind) in fm_groups:
                    fk = "fo%d" % (cnt % 2)
                    fot = fo[cnt % 2]
                    cnt += 1
                    for fc in range(4):
                        pb = 4 + fc % 2
                        pk = "ps%d" % pb
                        for dc in range(8):
                            c.op("pe", lambda e: e.matmul(out=self.ps[pb][:], lhsT=win[:, dc, col0 + fc * 128:col0 + (fc + 1) * 128],
                                                          rhs=zT[g % 2][:, dc, :], start=(dc == 0), stop=(dc == 7)), r=[zk, "win1"], w=[pk])
                        flush_pend()
                        if kind == "silu":
                            self.actf(fot[:, fc, :], self.ps[pb][:], AF.Silu, r=[pk], w=[fk])
                            if fc == 3:
                                c.dma(dst.rearrange("(c p) t -> p c t", p=128)[:, :, g * 512:(g + 1) * 512], fot[:], r=[fk], w=[dkey])
                        else:
                            i2 = fc % 2
                            self.cp("act", xb[i2][:], self.ps[pb][:], r=[pk], w=["xb%d" % i2])
                            pend.append((fot, fc, fk, i2, pb, dst, dkey, fc == 3))
                flush_pend()

    def phase_attn(self, kind):
        nc, c = self.nc, self.c
        S_ = self.S
        NB = S_ // 128
        NQT = S_ // 512
        dil = (kind == "dil")
        qT_d, kT_d, v_d, o_d = (self.cqT, self.ckT, self.cv_tok, self.oc_tok) if dil else (self.dqT, self.dkT, self.dv_tok, self.od_tok)
        okey = "oc_tok" if dil else "od_tok"
        NH = 8 if dil else 4
        VW = 64 if dil else 128
        nmask = 9 if dil else 4
        lam_init = 0.8 - 0.6 * math.exp(-0.3 * 1)
        with self.scope() as st:
            T = lambda name, shape, dt=F32: st.enter_context(self.sbt(name, shape, dt))
            masks = T("amask", [128, nmask, 512], BF16)
            c.dma(masks[:], self.dil_masks if dil else self.diff_masks, w=["amask"])
            if not dil:
                lqk = T("lqk", [1, 4, 64]); pr = T("lpr", [1, 2, 64]); sm = T("lsm", [1, 2]); nl = T("nl", [1, 1])
                ones1 = T("ones1", [1, 128]); nlam = T("nlam", [128, 1]); gdn = T("gdn", [128, 128])
                c.dma(lqk[:], self.diff_lqk, w=["lqk"])
                c.dma(gdn[:], self.diffnorm_rep, w=["gdn"])
                c.op("dve", lambda e: e.memset(ones1[:], 1.0), w=["ones1"])
                self.tt("dve", pr[:, 0, :], lqk[:, 0, :], lqk[:, 1, :], ALU.mult, r=["lqk"], w=["lpr"])
                self.tt("dve", pr[:, 1, :], lqk[:, 2, :], lqk[:, 3, :], ALU.mult, r=["lqk", "lpr"], w=["lpr"])
                c.op("dve", lambda e: e.reduce_sum(out=sm[:], in_=pr[:], axis=AX.X), r=["lpr"], w=["lsm"])
                self.actf(sm[:], sm[:], AF.Exp, r=["lsm"], w=["lsm"])
                self.tt("dve", nl[:], sm[:, 1:2], sm[:, 0:1], ALU.subtract, r=["lsm"], w=["nl"])
                self.ts("dve", nl[:], nl[:], -lam_init, ALU.add, r=["nl"], w=["nl"])
                c.op("pe", lambda e: e.matmul(out=self.ps[0][:, 0:1], lhsT=ones1[:], rhs=nl[:], start=True, stop=True), r=["ones1", "nl"], w=["ps0"])
                self.cp("dve", nlam[:], self.ps[0][:, 0:1], r=["ps0"], w=["nlam"])
                self.ts("dve", gdn[:], gdn[:], 1.0 - lam_init, ALU.mult, r=["gdn"], w=["gdn"])
            qT_v = qT_d.rearrange("(c p) t -> p c t", p=128)
            kT_v = kT_d.rearrange("(c p) t -> p c t", p=128)
            for q in range(self.nseq):
                tb = q * S_
                with self.scope() as sq:
                    TQ = lambda name, shape, dt=F32: sq.enter_context(self.sbt(name, shape, dt))
                    qT = TQ("aqT", [128, 4, S_], BF16); kT = TQ("akT", [128, 4, S_], BF16)
                    V1 = TQ("aV1", [128, NB, NH, VW + 1], BF16)
                    osb = TQ("aosb", [128, NB, BR], BF16)
                    E = [TQ("aE%d" % i, [128, 512], BF16) for i in range(2)]
                    P = [TQ("aP%d" % i, [128, 512], BF16) for i in range(2)]
                    rd = [TQ("ard%d" % i, [128, 1]) for i in range(4)]
                    if not dil:
                        o01 = [TQ("ao%d" % i, [128, NB, 128]) for i in range(2)]
                        sqj = TQ("asq", [128, 128]); ssn = TQ("assn", [128, NB]); t3 = TQ("at3", [128, 128])
                    c.dma(qT[:], qT_v[:, :, tb:tb + S_], r=[("cqT" if dil else "dqT")], w=["aqT"])
                    c.dma(kT[:], kT_v[:, :, tb:tb + S_], r=[("ckT" if dil else "dkT")], w=["akT"])
                    c.op("pool", lambda e: e.memset(V1[:, :, :, VW:VW + 1], 1.0), w=["aV1"])
                    for blk in range(NB):
                        c.dma(V1[:, blk, :, 0:VW], v_d[tb + blk * 128:tb + (blk + 1) * 128, :].rearrange("p (h d) -> p h d", h=NH),
                              r=[("cv_tok" if dil else "dv_tok")], w=["aV1"])
                    tiles = []
                    for h in range(NH):
                        for m in range(1 if dil else 2):
                            for qt in range(NQT):
                                nkb = 4 * qt + 4
                                for kb in range(nkb):
                                    tiles.append((h, m, qt, kb, kb == nkb - 1))
                    LA = 2
                    NSB = 4

                    def emit_S(i):
                        h, m, qt, kb, _ = tiles[i]
                        ch = h // 2 if dil else h
                        rb = 64 * (h % 2) if dil else 64 * m
                        sb = i % NSB
                        c.op("pe", lambda e: e.matmul(out=self.ps[sb][:], lhsT=kT[rb:rb + 64, ch, kb * 128:(kb + 1) * 128],
                                                      rhs=qT[rb:rb + 64, ch, qt * 512:(qt + 1) * 512], start=True, stop=True),
                             r=["akT", "aqT"], w=["ps%d" % sb])
                    for i in range(min(LA, len(tiles))):
                        emit_S(i)
                    for i, (h, m, qt, kb, last) in enumerate(tiles):
                        if i + LA < len(tiles):
                            emit_S(i + LA)
                        sb = i % NSB
                        i2 = i % 2
                        pS = self.ps[sb]
                        kS = "ps%d" % sb
                        d0 = 4 * qt - kb
                        if dil:
                            mi = 8 if d0 >= 5 else d0 + 3
                        else:
                            mi = d0 + 3 if d0 <= 0 else None
                        if mi is None:
                            self.actf(P[i2][:], pS[:], AF.Exp, r=[kS], w=["aP%d" % i2], scale=0.125)
                        else:
                            self.actf(E[i2][:], pS[:], AF.Exp, r=[kS], w=["aE%d" % i2], scale=0.125)
                            self.tt("dve" if i % 3 else "pool", P[i2][:], E[i2][:], masks[:, mi, :], ALU.mult,
                                    r=["aE%d" % i2, "amask"], w=["aP%d" % i2])
                        for j in range(4):
                            Q = 4 * qt + j
                            if kb > Q:
                                continue
                            c.op("pe", lambda e: e.matmul(out=self.ps[4 + j][:, 0:VW + 1], lhsT=P[i2][:, j * 128:(j + 1) * 128],
                                                          rhs=V1[:, kb, h, :], start=(kb == 0), stop=(kb == Q)),
                                 r=["aP%d" % i2, "aV1"], w=["ps%d" % (4 + j)])
                        if last:
                            for j in range(4):
                                Q = 4 * qt + j
                                pO = self.ps[4 + j]
                                kO = "ps%d" % (4 + j)
                                c.op("dve", lambda e: e.reciprocal(out=rd[j][:], in_=pO[:, VW:VW + 1]), r=[kO], w=["ard%d" % j])
                                if dil:
                                    c.op("act", lambda e: e.activation(out=osb[:, Q, h * 64:(h + 1) * 64], in_=pO[:, 0:VW], func=AF.Copy,
                                                                       scale=rd[j][:, 0:1]), r=[kO, "ard%d" % j], w=["aosb"])
                                else:
                                    c.op("act", lambda e: e.activation(out=o01[m][:, Q, :], in_=pO[:, 0:VW], func=AF.Copy,
                                                                       scale=rd[j][:, 0:1]), r=[kO, "ard%d" % j], w=["ao%d" % m])
                            if (not dil) and m == 1 and qt == NQT - 1:
                                self.stt("dve", o01[0][:], o01[1][:], nlam[:, 0:1], o01[0][:], ALU.mult, ALU.add, r=["ao0", "ao1", "nlam"], w=["ao0"])
                                for Q in range(NB):
                                    self.actf(sqj[:], o01[0][:, Q, :], AF.Square, r=["ao0"], w=["asq", "assn"], accum_out=ssn[:, Q:Q + 1])
                                self.actf(ssn[:], ssn[:], AF.Ln, r=["assn"], w=["assn"], scale=1.0 / 128, bias=HEAD_EPS)
                                self.actf(ssn[:], ssn[:], AF.Exp, r=["assn"], w=["assn"], scale=-0.5)
                                for Q in range(NB):
                                    self.ts("dve", t3[:], o01[0][:, Q, :], ssn[:, Q:Q + 1], ALU.mult, r=["ao0", "assn"], w=["at3"])
                                    self.tt("pool", osb[:, Q, h * 128:(h + 1) * 128], t3[:], gdn[:], ALU.mult, r=["at3", "gdn"], w=["aosb"])
                    c.dma(o_d[tb:tb + S_, :].rearrange("(b p) f -> p b f", p=128), osb[:], r=["aosb"], w=[okey])

    def phase_l1_out(self):
        nc, c = self.nc, self.c
        czT_v = self.czT.rearrange("(c p) t -> p c t", p=128)
        dzT_v = self.dzT.rearrange("(c p) t -> p c t", p=128)

        def provider(st):
            T = lambda name, shape, dt=F32: st.enter_context(self.sbt(name, shape, dt))
            ot = [T("ot%d" % i, [128, 2, BR], BF16) for i in range(4)]
            gT = [T("gT1_%d" % i, [128, 8, 128], BF16) for i in range(2)]
            gz = [T("gz%d" % i, [128, 8, 512], BF16) for i in range(2)]

            def pref(blk):
                i = blk % 4
                i4 = (blk // 4) % 2
                c.dma(ot[i][:, 0, :], self.oc_tok[blk * 128:(blk + 1) * 128, :], r=["oc_tok"], w=["ot%d" % i])
                c.dma(ot[i][:, 1, :], self.od_tok[blk * 128:(blk + 1) * 128, :], r=["od_tok"], w=["ot%d" % i])
                if blk % 4 == 0:
                    c.dma(gz[i4][:, 0:4, :], czT_v[:, :, blk * 128:blk * 128 + 512], r=["czT"], w=["gz%d" % i4])
                    c.dma(gz[i4][:, 4:8, :], dzT_v[:, :, blk * 128:blk * 128 + 512], r=["dzT"], w=["gz%d" % i4])

            def prov(blk):
                i = blk % 2
                i4 = (blk // 4) % 2
                pk = "ps%d" % i
                ptv = self.ps[i][:].bitcast(BF16).rearrange("p (a b) -> p a b", a=8)
                for fc in range(8):
                    c.op("pe", lambda e: e.transpose(out=ptv[:, fc, :], in_=ot[blk % 4][:, fc // 4, (fc % 4) * 128:(fc % 4 + 1) * 128],
                                                     identity=self.ident[:]), r=["ot%d" % (blk % 4), "ident"], w=[pk])
                off = (blk % 4) * 128
                self.tt("dve", gT[i][:], ptv, gz[i4][:, :, off:off + 128], ALU.mult, r=[pk, "gz%d" % i4], w=["gT1_%d" % i])
                return [(gT[i][:, fc, :], "gT1_%d" % i) for fc in range(8)]
            return pref, prov
        self.out_proj_norm_res(1, self.w_out_cd, self.post1_rep, provider, self.h1src, self.out, "h1_%d", "h1_%d")


def core_inputs(inp, x_rows):
    m = {}
    m["x"] = np.ascontiguousarray(x_rows, dtype=np.float32)
    m["ident"] = np.eye(128, dtype=np.float32).astype(ml_dtypes.bfloat16)
    m["pre0"] = np.ascontiguousarray(inp["pre_norm"][0].reshape(8, 128).T)
    m["w_in_ab"] = inp["w_in_ab"][0]
    m["s5_lr"] = inp["s5_lambda_re"][0].T
    m["s5_li"] = inp["s5_lambda_im"][0].T
    m["s5_ldt"] = np.broadcast_to(inp["s5_log_dt"][0][None, :], (64, 32))
    m["s5_br"] = inp["s5_b_re"][0].transpose(1, 0, 2)
    m["s5_bi"] = inp["s5_b_im"][0].transpose(1, 0, 2)
    m["s5_cr"] = inp["s5_c_re"][0].transpose(2, 0, 1)
    m["s5_ci"] = inp["s5_c_im"][0].transpose(2, 0, 1)
    tv = np.array([0, -1, -2, -3, -4, -5, -6, -7, 1, 2, 3, 4, 5, 6, 7, 8], dtype=np.float32)
    m["tv"] = np.broadcast_to(tv[None, :], (64, 16))
    m["kv"] = np.broadcast_to(np.arange(256, dtype=np.float32)[None, :], (64, 256))
    sidx = np.arange(128) // 16
    m["toepmask"] = (sidx[None, :] >= sidx[:, None]).astype(np.float32)
    m["identf"] = np.eye(128, dtype=np.float32)
    m["s5d_rep"] = np.broadcast_to(inp["s5_d"][0][None, :], (128, 512))
    m["glub_rep"] = np.broadcast_to(inp["s5_glu_b"][0][None, :], (128, 512))
    m["glu_w"] = inp["s5_glu_w"][0]
    m["ml_cw"] = inp["ml_conv_w"][0].reshape(4, 4, 128).transpose(2, 1, 0)
    m["ml_cb"] = inp["ml_conv_b"][0].reshape(4, 128).T
    m["ml_mn"] = inp["ml_norm"][0].reshape(4, 128).T
    m["ml_msk"] = inp["ml_skip"][0].reshape(4, 128).T
    m["ml_gbi"] = inp["ml_gate_b"][0][0:4].reshape(4, 1)
    m["ml_gbf"] = inp["ml_gate_b"][0][4:8].reshape(4, 1)
    si = np.arange(128)
    m["ml_maskS"] = ((si[:, None] <= si[None, :]) * (128 ** -0.5)).astype(np.float32)
    m["ml_gw"] = inp["ml_gate_w"][0].reshape(12, 128, 8).transpose(1, 0, 2)
    m["ml_wq"] = inp["ml_wq"][0].transpose(1, 0, 2)
    m["ml_wk"] = inp["ml_wk"][0].transpose(1, 0, 2)
    m["ml_wv"] = inp["ml_wv"][0].transpose(1, 0, 2)
    m["w_out_ab"] = inp["w_out_ab"][0]
    m["pre1"] = np.ascontiguousarray(inp["pre_norm"][1].reshape(8, 128).T)
    m["w_in_cd"] = inp["w_in_cd"][0]
    pos = np.arange(S, dtype=np.float32)
    inv = (10000.0 ** (-np.arange(0, 64, 2, dtype=np.float32) / 64)).astype(np.float32)
    ang = pos[None, :] * inv[np.arange(128) % 32][:, None]
    m["rope_cos"] = np.cos(ang).astype(np.float32)
    m["rope_sin"] = np.sin(ang).astype(np.float32)
    rm = np.zeros((128, 128), dtype=np.float32)
    for mm in range(128):
        if mm % 64 < 32:
            rm[mm + 32, mm] = -1.0
        else:
            rm[mm - 32, mm] = 1.0
    m["rope_rm"] = rm.astype(ml_dtypes.bfloat16)
    kk = np.arange(128)[:, None]
    qq = np.arange(512)[None, :]
    dm = np.zeros((128, 9, 512), dtype=np.float32)
    fm = np.zeros((128, 4, 512), dtype=np.float32)
    for mi in range(9):
        d0 = mi - 3 if mi < 8 else 5
        dl = 128 * d0 + qq - kk
        mult = ((dl >= 0) & (dl <= 128)).astype(np.float32) + ((dl >= 0) & (dl % 4 == 0) & (dl <= 512)) + ((dl >= 0) & (dl % 16 == 0) & (dl <= 2048))
        dm[:, mi, :] = mult
        if mi < 4:
            fm[:, mi, :] = (dl >= 0)
    m["dil_masks"] = dm.astype(ml_dtypes.bfloat16)
    m["diff_masks"] = fm.astype(ml_dtypes.bfloat16)
    m["diff_lqk"] = np.stack([inp["diff_lq1"][0], inp["diff_lk1"][0], inp["diff_lq2"][0], inp["diff_lk2"][0]])[None]
    m["diffnorm_rep"] = np.broadcast_to(inp["diff_norm"][0][None, :], (128, 128))
    m["w_out_cd"] = inp["w_out_cd"][0]
    m["post1_rep"] = np.broadcast_to(inp["post_norm"][1][None, :], (128, 1024))
    m["post0_rep"] = np.broadcast_to(inp["post_norm"][0][None, :], (128, 1024))
    return m


_CACHE = {}


def kernel(**inputs):
    inp = {k_: np.asarray(v) for k_, v in inputs.items()}
    x = inp["x"]
    B = x.shape[0]
    nseq = B // NCORES
    if "prog" not in _CACHE:
        kb = K(nseq=nseq)
        kb.build()
        _CACHE["prog"] = kb
    kb = _CACHE["prog"]
    in_maps = []
    for ci in range(NCORES):
        m = core_inputs(inp, x[ci * nseq:(ci + 1) * nseq].reshape(-1, D))
        in_maps.append({n: np.ascontiguousarray(m[n]) for n in kb.inputs})
    res = run_bass_kernel_spmd(kb.nc, in_maps, core_ids=list(range(NCORES)))
    out = np.stack([np.asarray(res.results[ci]["out"]).reshape(nseq, S, D) for ci in range(NCORES)], axis=0)
    return out.reshape(B, S, D).astype(np.float32)
```

```python
import contextlib
import math
import numpy as np
import ml_dtypes
import concourse.bass as bass
import concourse.mybir as mybir
from concourse.bass_utils import run_bass_kernel_spmd

F32 = mybir.dt.float32
BF16 = mybir.dt.bfloat16
I32 = mybir.dt.int32
AF = mybir.ActivationFunctionType
ALU = mybir.AluOpType
AX = mybir.AxisListType

D = 1024
S = 2048
BR = 512
NCORES = 8
SAME_ENGINE_SYNC = True
NORM_EPS = 1e-6
HEAD_EPS = 1e-5


class Ctx:
    def __init__(self, nc, stack, n_dma_sems=48, same_engine_sync=SAME_ENGINE_SYNC):
        self.nc = nc
        self.eng = {"pe": nc.tensor, "act": nc.scalar, "dve": nc.vector,
                    "pool": nc.gpsimd, "sp": nc.sync}
        self.sem = {}
        self.cnt = {}
        for k in ("pe", "act", "dve", "pool"):
            self.sem[k] = stack.enter_context(nc.semaphore("s_" + k))
            self.cnt[k] = 0
        self.dma_sems = []
        for i in range(n_dma_sems):
            k = "dma%d" % i
            self.sem[k] = stack.enter_context(nc.semaphore("s_" + k))
            self.cnt[k] = 0
            self.dma_sems.append(k)
        self.dma_rr = 0
        self.waited = {k: {} for k in self.eng}
        self.last_w = {}
        self.readers = {}
        self.same_engine_sync = same_engine_sync
        self.n_instr = 0
        self.n_wait = 0

    def _deps(self, r, w):
        deps = []
        for x in r:
            if x in self.last_w:
                deps.append(self.last_w[x])
            if x.startswith("ps"):
                deps.extend(self.readers.get(x, ()))
        for x in w:
            if x in self.last_w:
                deps.append(self.last_w[x])
            deps.extend(self.readers.get(x, ()))
        return deps

    def _wait(self, e, deps):
        need = {}
        for (k, v) in deps:
            if k == e and (e == "pe" or not self.same_engine_sync):
                continue
            if need.get(k, 0) < v:
                need[k] = v
        for k, v in need.items():
            if self.waited[e].get(k, 0) >= v:
                continue
            self.eng[e].wait_ge(self.sem[k], v)
            self.waited[e][k] = v
            self.n_wait += 1

    def _commit(self, tok, r, w):
        for x in w:
            self.last_w[x] = tok
            self.readers[x] = []
        for x in r:
            if x in w:
                continue
            self.readers.setdefault(x, []).append(tok)

    def op(self, e, fn, r=(), w=()):
        self._wait(e, self._deps(r, w))
        ins = fn(self.eng[e])
        self.cnt[e] += 1
        ins.then_inc(self.sem[e], 1)
        self._commit((e, self.cnt[e]), r, w)
        self.n_instr += 1
        return ins

    def dma(self, out, in_, r=(), w=(), q="sp", **kw):
        k = self.dma_sems[self.dma_rr]
        self.dma_rr = (self.dma_rr + 1) % len(self.dma_sems)
        deps = self._deps(r, w)
        if self.cnt[k] > 0:
            deps.append((k, self.cnt[k]))
        self._wait(q, deps)
        ins = self.eng[q].dma_start(out=out, in_=in_, **kw)
        self.cnt[k] += 16
        ins.then_inc(self.sem[k], 16)
        self._commit((k, self.cnt[k]), r, w)
        self.n_instr += 1
        return ins

    def barrier(self):
        deps = [(k, v) for k, v in self.cnt.items() if v > 0]
        for e in self.eng:
            self._wait(e, deps)

    def finish(self, res):
        deps = [self.last_w[x] for x in res if x in self.last_w]
        self._wait("sp", deps)


class K:
    def __init__(self, nseq=2, export=(), phases=None, seqlen=S):
        self.nseq = nseq
        self.S = seqlen
        self.NT = nseq * seqlen
        self.export = set(export)
        self.phases = phases
        self.nc = bass.Bass("TRN2", target_bir_lowering=False)
        self.inputs = {}
        self.outputs = {}
        self.s5_main_enabled = True
        self._uid = 0

    def sbt(self, name, shape, dt):
        self._uid += 1
        return self.nc.sbuf_tensor("%s_u%d" % (name, self._uid), list(shape), dt)

    def din(self, name, shape, dt=F32):
        ap = self.nc.dram_tensor(name, list(shape), dt, kind="ExternalInput").ap()
        self.inputs[name] = ap
        return ap

    def dscr(self, name, shape, dt):
        kind = "ExternalOutput" if name in self.export else "Internal"
        ap = self.nc.dram_tensor(name, list(shape), dt, kind=kind).ap()
        if kind == "ExternalOutput":
            self.outputs[name] = ap
        return ap

    @contextlib.contextmanager
    def scope(self):
        with contextlib.ExitStack() as st:
            yield st
            self.c.barrier()

    def build(self):
        nc = self.nc
        NT = self.NT
        with contextlib.ExitStack() as st:
            self.c = Ctx(nc, st)
            self.ps = [st.enter_context(nc.psum_tensor("ps%d" % i, [128, 512], F32)) for i in range(8)]
            self.x = self.din("x", [NT, D])
            self.ident_d = self.din("ident", [128, 128], BF16)
            self.pre0 = self.din("pre0", [128, 8])
            self.w_in_ab = self.din("w_in_ab", [D, 4 * BR])
            for nm in ("s5_lr", "s5_li", "s5_ldt"):
                setattr(self, nm, self.din(nm, [64, 32]))
            for nm in ("s5_br", "s5_bi", "s5_cr", "s5_ci"):
                setattr(self, nm, self.din(nm, [64, 32, 16]))
            self.tv_d = self.din("tv", [64, 16])
            self.kv_d = self.din("kv", [64, 256])
            self.toepmask_d = self.din("toepmask", [128, 128])
            self.identf_d = self.din("identf", [128, 128])
            self.s5d_rep = self.din("s5d_rep", [128, BR])
            self.glub_rep = self.din("glub_rep", [128, BR])
            self.glu_w = self.din("glu_w", [BR, BR])
            self.ml_cw = self.din("ml_cw", [128, 4, 4]); self.ml_cb = self.din("ml_cb", [128, 4])
            self.ml_mn = self.din("ml_mn", [128, 4]); self.ml_msk = self.din("ml_msk", [128, 4])
            self.ml_gbi = self.din("ml_gbi", [4, 1]); self.ml_gbf = self.din("ml_gbf", [4, 1])
            self.ml_maskS = self.din("ml_maskS", [128, 128])
            self.ml_gw = self.din("ml_gw", [128, 12, 8])
            self.ml_wq = self.din("ml_wq", [128, 4, 128]); self.ml_wk = self.din("ml_wk", [128, 4, 128]); self.ml_wv = self.din("ml_wv", [128, 4, 128])
            self.bT = self.dscr("bT", [BR, NT], BF16)
            self.w_out_ab = self.din("w_out_ab", [D, D]); self.post0_rep = self.din("post0_rep", [128, D])
            self.out = self.nc.dram_tensor("out", [NT, D], F32, kind="ExternalOutput").ap()
            self.outputs["out"] = self.out
            self.h1src = self.out
            if self.phases is not None and "l0o" not in self.phases:
                self.h1src = self.din("h1_in", [NT, D])
            self.pre1 = self.din("pre1", [128, 8]); self.w_in_cd = self.din("w_in_cd", [D, 8 * BR])
            self.rope_cos = self.din("rope_cos", [128, self.S]); self.rope_sin = self.din("rope_sin", [128, self.S])
            self.rope_rm = self.din("rope_rm", [128, 128], BF16)
            for nm in ("cqT", "ckT", "dqT", "dkT", "czT", "dzT"):
                setattr(self, nm, self.dscr(nm, [BR, NT], BF16))
            for nm in ("cv_tok", "dv_tok", "oc_tok", "od_tok"):
                setattr(self, nm, self.dscr(nm, [NT, BR], BF16))
            self.dil_masks = self.din("dil_masks", [128, 9, 512], BF16); self.diff_masks = self.din("diff_masks", [128, 4, 512], BF16)
            self.diff_lqk = self.din("diff_lqk", [1, 4, 64]); self.diffnorm_rep = self.din("diffnorm_rep", [128, 128])
            self.w_out_cd = self.din("w_out_cd", [D, D]); self.post1_rep = self.din("post1_rep", [128, D])
            self.rotc = self.dscr("rotc", [64, 32, 256], F32)
            self.rots = self.dscr("rots", [64, 32, 256], F32)
            self.rhot = self.dscr("rhot", [64, 32, 256], F32)
            self.toep_x = self.dscr("toep_x", [128, 32, 128], BF16)
            self.wii_x = self.dscr("wii_x", [128, 32, 2, 64], BF16)
            self.wiv_x = self.dscr("wiv_x", [64, 2, 32, 128], BF16)
            self.a_tok = self.dscr("a_tok", [NT, BR], BF16)
            self.u_tok = self.dscr("u_tok", [NT, BR], BF16)
            self.sz_tok = self.dscr("sz_tok", [NT, BR], BF16)
            self.xmT = self.dscr("xmT", [BR, NT], BF16)
            self.mzT = self.dscr("mzT", [BR, NT], BF16)
            self.ident = st.enter_context(nc.sbuf_tensor("identb", [128, 128], BF16))
            self.c.dma(self.ident[:], self.ident_d, w=["ident"])
            ph = self.phases
            fin = []
            if ph is None or "l0p" in ph:
                self.phase_l0_proj()
                fin += ["u_tok", "sz_tok", "xmT", "mzT"]
            if ph is None or "s5" in ph:
                self.phase_s5()
                fin += ["a_tok", "rotc", "rots", "rhot", "toep_x", "wii_x", "wiv_x"]
            if ph is None or "ml" in ph:
                self.phase_ml()
                fin += ["bT"]
            if ph is None or "l0o" in ph:
                self.phase_l0_out()
                fin += ["h1_%d" % b for b in range(NT // 128)]
            if ph is None or "l1p" in ph:
                self.phase_l1_proj()
                fin += ["cqT", "ckT", "dqT", "dkT", "czT", "dzT", "cv_tok", "dv_tok"]
            if ph is None or "adil" in ph:
                self.phase_attn("dil")
                fin += ["oc_tok"]
            if ph is None or "adiff" in ph:
                self.phase_attn("diff")
                fin += ["od_tok"]
            if ph is None or "l1o" in ph:
                self.phase_l1_out()
                fin += ["h1_%d" % b for b in range(NT // 128)]
            self.c.finish(fin)
            self.c.barrier()
        return nc

    def rmsnorm_T(self, st, xsrc_rows, nblk, zT, zkey, tagp, ps_tr):
        raise NotImplementedError

    def phase_l0_proj(self):
        nc, c = self.nc, self.c
        NT = self.NT
        ngrp = NT // 512
        with self.scope() as st:
            T = lambda name, shape, dt: st.enter_context(self.sbt(name, shape, dt))
            win = T("win0", [128, 8, 4 * BR], BF16)
            g0 = T("g0", [128, 8], F32)
            stage = [T("wst%d" % i, [128, 4 * BR], F32) for i in range(2)]
            c.dma(g0[:], self.pre0, w=["g0"])
            for dc in range(8):
                sk = "wst%d" % (dc % 2)
                c.dma(stage[dc % 2][:], self.w_in_ab[dc * 128:(dc + 1) * 128, :], w=[sk])
                c.op("act", lambda e: e.activation(out=win[:, dc, :], in_=stage[dc % 2][:], func=AF.Copy,
                                                   scale=g0[:, dc:dc + 1]), r=[sk, "g0"], w=["win0"])
            NXB = 6
            xt = [T("xt%d" % i, [128, D], F32) for i in range(NXB)]
            junk = T("junk", [128, D], F32)
            ss2 = [T("ss%d" % i, [128, 4], F32) for i in range(2)]
            rstd2 = [T("rstd%d" % i, [128, 4], F32) for i in range(2)]
            zb = [T("zb%d" % i, [128, D], BF16) for i in range(2)]
            zT = [T("zT%d" % i, [128, 8, 512], BF16) for i in range(2)]
            uo = [T("uo%d" % i, [128, BR], BF16) for i in range(2)]
            so = [T("so%d" % i, [128, BR], BF16) for i in range(2)]
            xmo = [T("xmo%d" % i, [128, 4, 512], BF16) for i in range(2)]
            mzo = [T("mzo%d" % i, [128, 4, 512], BF16) for i in range(2)]
            nblk = NT // 128

            def load_x(b):
                if b < nblk:
                    c.dma(xt[b % NXB][:], self.x[b * 128:(b + 1) * 128, :], w=["xt%d" % (b % NXB)])
            for b in range(4):
                load_x(b)
            for g in range(ngrp):
                zk = "zT%d" % (g % 2)
                ss = ss2[g % 2]; rstd = rstd2[g % 2]
                ssk = "ss%d" % (g % 2); rsk = "rstd%d" % (g % 2)
                for j in range(4):
                    b = g * 4 + j
                    xk = "xt%d" % (b % NXB)
                    c.op("act", lambda e: e.activation(out=junk[:], in_=xt[b % NXB][:], func=AF.Square,
                                                       accum_out=ss[:, j:j + 1]), r=[xk], w=["junk", ssk])
                c.op("act", lambda e: e.activation(out=rstd[:], in_=ss[:], func=AF.Ln, scale=1.0 / D, bias=NORM_EPS),
                     r=[ssk], w=[rsk])
                c.op("act", lambda e: e.activation(out=rstd[:], in_=rstd[:], func=AF.Exp, scale=-0.5),
                     r=[rsk], w=[rsk])
                for j in range(4):
                    b = g * 4 + j
                    xk = "xt%d" % (b % NXB)
                    zbk = "zb%d" % (b % 2)
                    pst = self.ps[b % 2]
                    pk = "ps%d" % (b % 2)
                    c.op("dve", lambda e: e.tensor_scalar(out=zb[b % 2][:], in0=xt[b % NXB][:], scalar1=rstd[:, j:j + 1],
                                                          scalar2=None, op0=ALU.mult), r=[xk, rsk], w=[zbk])
                    load_x(b + 4)
                    ptv = pst[:].bitcast(BF16).rearrange("p (a b) -> p a b", a=8)
                    for dc in range(8):
                        c.op("pe", lambda e: e.transpose(out=ptv[:, dc, :], in_=zb[b % 2][:, dc * 128:(dc + 1) * 128],
                                                         identity=self.ident[:]), r=[zbk, "ident"], w=[pk])
                    c.op("dve", lambda e: e.tensor_copy(out=zT[g % 2][:, :, j * 128:(j + 1) * 128], in_=ptv),
                         r=[pk], w=[zk])
                    for dc in range(8):
                        c.op("pe", lambda e: e.matmul(out=self.ps[2][:], lhsT=zT[g % 2][:, dc, j * 128:(j + 1) * 128],
                                                      rhs=win[:, dc, 0:BR], start=(dc == 0), stop=(dc == 7)),
                             r=[zk, "win0"], w=["ps2"])
                    c.op("act", lambda e: e.copy(out=uo[b % 2][:], in_=self.ps[2][:]), r=["ps2"], w=["uo%d" % (b % 2)])
                    c.dma(self.u_tok[b * 128:(b + 1) * 128, :], uo[b % 2][:], r=["uo%d" % (b % 2)], w=["u_tok"])
                    for dc in range(8):
                        c.op("pe", lambda e: e.matmul(out=self.ps[3][:], lhsT=zT[g % 2][:, dc, j * 128:(j + 1) * 128],
                                                      rhs=win[:, dc, BR:2 * BR], start=(dc == 0), stop=(dc == 7)),
                             r=[zk, "win0"], w=["ps3"])
                    c.op("act", lambda e: e.activation(out=so[b % 2][:], in_=self.ps[3][:], func=AF.Silu),
                         r=["ps3"], w=["so%d" % (b % 2)])
                    c.dma(self.sz_tok[b * 128:(b + 1) * 128, :], so[b % 2][:], r=["so%d" % (b % 2)], w=["sz_tok"])
                for fc in range(8):
                    pb = 4 + fc % 4
                    for dc in range(8):
                        c.op("pe", lambda e: e.matmul(out=self.ps[pb][:], lhsT=win[:, dc, 2 * BR + fc * 128:2 * BR + (fc + 1) * 128],
                                                      rhs=zT[g % 2][:, dc, :], start=(dc == 0), stop=(dc == 7)),
                             r=[zk, "win0"], w=["ps%d" % pb])
                    if fc < 4:
                        c.op("dve", lambda e: e.tensor_copy(out=xmo[g % 2][:, fc, :], in_=self.ps[pb][:]),
                             r=["ps%d" % pb], w=["xmo%d" % (g % 2)])
                    else:
                        c.op("act", lambda e: e.activation(out=mzo[g % 2][:, fc - 4, :], in_=self.ps[pb][:], func=AF.Silu),
                             r=["ps%d" % pb], w=["mzo%d" % (g % 2)])
                c.dma(self.xmT.rearrange("(c p) t -> p c t", p=128)[:, :, g * 512:(g + 1) * 512], xmo[g % 2][:],
                      r=["xmo%d" % (g % 2)], w=["xmT"])
                c.dma(self.mzT.rearrange("(c p) t -> p c t", p=128)[:, :, g * 512:(g + 1) * 512], mzo[g % 2][:],
                      r=["mzo%d" % (g % 2)], w=["mzT"])
        c.barrier()

    def tt(self, e, out, a, b, op, r, w):
        return self.c.op(e, lambda en: en.tensor_tensor(out=out, in0=a, in1=b, op=op), r=r, w=w)

    def ts(self, e, out, a, s1, op0, r, w, s2=None, op1=None):
        if op1 is None:
            return self.c.op(e, lambda en: en.tensor_scalar(out=out, in0=a, scalar1=s1, scalar2=None, op0=op0), r=r, w=w)
        return self.c.op(e, lambda en: en.tensor_scalar(out=out, in0=a, scalar1=s1, scalar2=s2, op0=op0, op1=op1), r=r, w=w)

    def stt(self, e, out, a, s, b, op0, op1, r, w):
        return self.c.op(e, lambda en: en.scalar_tensor_tensor(out=out, in0=a, scalar=s, in1=b, op0=op0, op1=op1), r=r, w=w)

    def actf(self, out, in_, func, r, w, **kw):
        return self.c.op("act", lambda en: en.activation(out=out, in_=in_, func=func, **kw), r=r, w=w)

    def cp(self, e, out, in_, r, w):
        if e == "act":
            return self.c.op("act", lambda en: en.copy(out=out, in_=in_), r=r, w=w)
        return self.c.op(e, lambda en: en.tensor_copy(out=out, in_=in_), r=r, w=w)

    def sincos(self, ang, akey, sin_o, cos_o, skey, ckey, tf, ti, red_o=None):
        C1 = 6.28125
        C2 = 2 * math.pi - C1
        for (off, out, okey) in ((0.0, sin_o, skey), (math.pi / 2, cos_o, ckey)):
            if out is None:
                continue
            self.ts("dve", tf, ang, 1.0 / (2 * math.pi), ALU.mult, r=[akey], w=["sc_tf"], s2=off / (2 * math.pi), op1=ALU.add)
            self.cp("dve", ti, tf, r=["sc_tf"], w=["sc_ti"])
            self.cp("dve", tf, ti, r=["sc_ti"], w=["sc_tf"])
            self.stt("dve", out, tf, -C1, ang, ALU.mult, ALU.add, r=["sc_tf", akey], w=[okey])
            self.stt("dve", out, tf, -C2, out, ALU.mult, ALU.add, r=["sc_tf", okey], w=[okey])
            if off != 0.0:
                self.ts("dve", out, out, off, ALU.add, r=[okey], w=[okey])
            self.ts("dve", out, out, math.pi, ALU.min, r=[okey], w=[okey], s2=-math.pi, op1=ALU.max)
            if red_o is not None and off == 0.0:
                self.cp("dve", red_o[0], out, r=[okey], w=[red_o[1]])
            self.actf(out, out, AF.Sin, r=[okey], w=[okey])

    def s5_precompute(self, st, Toep, Wii, WivR, WivI):
        nc, c = self.nc, self.c
        so = contextlib.ExitStack()
        TO = lambda name, shape, dt=F32: so.enter_context(self.sbt(name, shape, dt))
        thr = TO("p_thr", [64, 32]); rho8 = TO("p_rho8", [64, 32]); kv = TO("p_kv", [64, 256])
        tfs = TO("p_tfs", [64, 32]); tis = TO("p_tis", [64, 32], I32)
        with self.scope() as sp:
            T = lambda name, shape, dt=F32: sp.enter_context(self.sbt(name, shape, dt))
            lr = T("p_lr", [64, 32]); li = T("p_li", [64, 32]); ldt = T("p_ldt", [64, 32])
            br = T("p_br", [64, 32, 16]); bi = T("p_bi", [64, 32, 16])
            cr = T("p_cr", [64, 32, 16]); ci = T("p_ci", [64, 32, 16])
            tv = T("p_tv", [64, 16])
            msk = T("p_msk", [128, 128]); idf = T("p_idf", [128, 128])
            for (t, d, key) in ((lr, self.s5_lr, "p_lr"), (li, self.s5_li, "p_li"), (ldt, self.s5_ldt, "p_ldt"),
                                (br, self.s5_br, "p_br"), (bi, self.s5_bi, "p_bi"), (cr, self.s5_cr, "p_cr"),
                                (ci, self.s5_ci, "p_ci"), (tv, self.tv_d, "p_tv"), (kv, self.kv_d, "p_kv"),
                                (msk, self.toepmask_d, "p_msk"), (idf, self.identf_d, "p_idf")):
                c.dma(t[:], d, w=[key])
            dt = T("p_dt", [64, 32]); lrdt = T("p_lrdt", [64, 32]); th = T("p_th", [64, 32])
            s0 = T("p_s0", [64, 32]); c0 = T("p_c0", [64, 32]); mag = T("p_mag", [64, 32])
            self.actf(dt[:], ldt[:], AF.Exp, r=["p_ldt"], w=["p_dt"])
            self.tt("dve", lrdt[:], lr[:], dt[:], ALU.mult, r=["p_lr", "p_dt"], w=["p_lrdt"])
            self.tt("dve", th[:], li[:], dt[:], ALU.mult, r=["p_li", "p_dt"], w=["p_th"])
            self.sincos(th[:], "p_th", s0[:], c0[:], "p_s0", "p_c0", tfs[:], tis[:], red_o=(thr[:], "p_thr"))
            self.actf(mag[:], lrdt[:], AF.Exp, r=["p_lrdt"], w=["p_mag"])
            abr = T("p_abr", [64, 32]); abi = T("p_abi", [64, 32]); am1 = T("p_am1", [64, 32])
            self.tt("dve", abr[:], mag[:], c0[:], ALU.mult, r=["p_mag", "p_c0"], w=["p_abr"])
            self.tt("dve", abi[:], mag[:], s0[:], ALU.mult, r=["p_mag", "p_s0"], w=["p_abi"])
            self.ts("dve", am1[:], abr[:], -1.0, ALU.add, r=["p_abr"], w=["p_am1"])
            den = T("p_den", [64, 32]); t1 = T("p_t1", [64, 32]); t2 = T("p_t2", [64, 32])
            fr = T("p_fr", [64, 32]); fi = T("p_fi", [64, 32])
            self.tt("dve", den[:], lr[:], lr[:], ALU.mult, r=["p_lr"], w=["p_den"])
            self.tt("dve", t1[:], li[:], li[:], ALU.mult, r=["p_li"], w=["p_t1"])
            self.tt("dve", den[:], den[:], t1[:], ALU.add, r=["p_den", "p_t1"], w=["p_den"])
            c.op("dve", lambda e: e.reciprocal(out=den[:], in_=den[:]), r=["p_den"], w=["p_den"])
            self.tt("dve", t1[:], am1[:], lr[:], ALU.mult, r=["p_am1", "p_lr"], w=["p_t1"])
            self.tt("dve", t2[:], abi[:], li[:], ALU.mult, r=["p_abi", "p_li"], w=["p_t2"])
            self.tt("dve", t1[:], t1[:], t2[:], ALU.add, r=["p_t1", "p_t2"], w=["p_t1"])
            self.tt("dve", fr[:], t1[:], den[:], ALU.mult, r=["p_t1", "p_den"], w=["p_fr"])
            self.tt("dve", t1[:], abi[:], lr[:], ALU.mult, r=["p_abi", "p_lr"], w=["p_t1"])
            self.tt("dve", t2[:], am1[:], li[:], ALU.mult, r=["p_am1", "p_li"], w=["p_t2"])
            self.tt("dve", t1[:], t1[:], t2[:], ALU.subtract, r=["p_t1", "p_t2"], w=["p_t1"])
            self.tt("dve", fi[:], t1[:], den[:], ALU.mult, r=["p_t1", "p_den"], w=["p_fi"])
            Bbr = T("p_Bbr", [64, 32, 16]); Bbi = T("p_Bbi", [64, 32, 16])
            u1 = T("p_u1", [64, 32, 16]); u2 = T("p_u2", [64, 32, 16])
            bc16 = lambda a: a.unsqueeze(2).broadcast_to([64, 32, 16])
            self.tt("dve", u1[:], br[:], bc16(fr[:]), ALU.mult, r=["p_br", "p_fr"], w=["p_u1"])
            self.tt("dve", u2[:], bi[:], bc16(fi[:]), ALU.mult, r=["p_bi", "p_fi"], w=["p_u2"])
            self.tt("dve", Bbr[:], u1[:], u2[:], ALU.subtract, r=["p_u1", "p_u2"], w=["p_Bbr"])
            self.tt("dve", u1[:], bi[:], bc16(fr[:]), ALU.mult, r=["p_bi", "p_fr"], w=["p_u1"])
            self.tt("dve", u2[:], br[:], bc16(fi[:]), ALU.mult, r=["p_br", "p_fi"], w=["p_u2"])
            self.tt("dve", Bbi[:], u1[:], u2[:], ALU.add, r=["p_u1", "p_u2"], w=["p_Bbi"])
            TE = T("p_TE", [64, 16, 32]); TA = T("p_TA", [64, 16, 32])
            PWr = T("p_PWr", [64, 16, 32]); PWi = T("p_PWi", [64, 16, 32])
            tf3 = T("p_tf3", [64, 16, 32]); ti3 = T("p_ti3", [64, 16, 32], I32)
            bt = lambda a: a.unsqueeze(1).broadcast_to([64, 16, 32])
            bg = lambda a: a.unsqueeze(2).broadcast_to([64, 16, 32])
            self.tt("dve", TE[:], bt(lrdt[:]), bg(tv[:]), ALU.mult, r=["p_lrdt", "p_tv"], w=["p_TE"])
            self.actf(TE[:], TE[:], AF.Exp, r=["p_TE"], w=["p_TE"])
            self.tt("dve", TA[:], bt(thr[:]), bg(tv[:]), ALU.mult, r=["p_thr", "p_tv"], w=["p_TA"])
            self.sincos(TA[:], "p_TA", PWi[:], PWr[:], "p_PWi", "p_PWr", tf3[:], ti3[:])
            self.tt("dve", PWr[:], PWr[:], TE[:], ALU.mult, r=["p_PWr", "p_TE"], w=["p_PWr"])
            self.tt("dve", PWi[:], PWi[:], TE[:], ALU.mult, r=["p_PWi", "p_TE"], w=["p_PWi"])
            HsR = T("p_HsR", [64, 32, 8, 16]); HsI = T("p_HsI", [64, 32, 8, 16])
            v1 = T("p_v1", [64, 32, 9, 16]); v2 = T("p_v2", [64, 32, 9, 16])
            def pw_b(PW, j0, n):
                return PW[:, j0:j0 + n, :].rearrange("p t g -> p g t").unsqueeze(3).broadcast_to([64, 32, n, 16])
            def x_b(x, n):
                return x.unsqueeze(2).broadcast_to([64, 32, n, 16])
            self.tt("dve", v1[:, :, 0:8, :], pw_b(PWr, 0, 8), x_b(Bbr[:], 8), ALU.mult, r=["p_PWr", "p_Bbr"], w=["p_v1"])
            self.tt("dve", v2[:, :, 0:8, :], pw_b(PWi, 0, 8), x_b(Bbi[:], 8), ALU.mult, r=["p_PWi", "p_Bbi"], w=["p_v2"])
            self.tt("dve", HsR[:], v1[:, :, 0:8, :], v2[:, :, 0:8, :], ALU.subtract, r=["p_v1", "p_v2"], w=["p_HsR"])
            self.tt("dve", v1[:, :, 0:8, :], pw_b(PWr, 0, 8), x_b(Bbi[:], 8), ALU.mult, r=["p_PWr", "p_Bbi"], w=["p_v1"])
            self.tt("dve", v2[:, :, 0:8, :], pw_b(PWi, 0, 8), x_b(Bbr[:], 8), ALU.mult, r=["p_PWi", "p_Bbr"], w=["p_v2"])
            self.tt("dve", HsI[:], v1[:, :, 0:8, :], v2[:, :, 0:8, :], ALU.add, r=["p_v1", "p_v2"], w=["p_HsI"])
            LtR = T("p_LtR", [64, 32, 9, 16]); nLtI = T("p_nLtI", [64, 32, 9, 16])
            for (s0_, j0, n) in ((0, 0, 1), (1, 8, 8)):
                sl = slice(s0_, s0_ + n)
                self.tt("dve", v1[:, :, sl, :], pw_b(PWr, j0, n), x_b(cr[:], n), ALU.mult, r=["p_PWr", "p_cr"], w=["p_v1"])
                self.tt("dve", v2[:, :, sl, :], pw_b(PWi, j0, n), x_b(ci[:], n), ALU.mult, r=["p_PWi", "p_ci"], w=["p_v2"])
                self.tt("dve", LtR[:, :, sl, :], v1[:, :, sl, :], v2[:, :, sl, :], ALU.subtract, r=["p_v1", "p_v2"], w=["p_LtR"])
                self.tt("dve", v1[:, :, sl, :], pw_b(PWi, j0, n), x_b(cr[:], n), ALU.mult, r=["p_PWi", "p_cr"], w=["p_v1"])
                self.tt("dve", v2[:, :, sl, :], pw_b(PWr, j0, n), x_b(ci[:], n), ALU.mult, r=["p_PWr", "p_ci"], w=["p_v2"])
                self.tt("dve", v1[:, :, sl, :], v1[:, :, sl, :], v2[:, :, sl, :], ALU.add, r=["p_v1", "p_v2"], w=["p_v1"])
                self.ts("dve", nLtI[:, :, sl, :], v1[:, :, sl, :], -1.0, ALU.mult, r=["p_v1"], w=["p_nLtI"])
            self.cp("dve", WivR[:], LtR[:, :, 1:9, :].rearrange("p g t c -> p g (t c)"), r=["p_LtR"], w=["WivR"])
            self.cp("dve", WivI[:], nLtI[:, :, 1:9, :].rearrange("p g t c -> p g (t c)"), r=["p_nLtI"], w=["WivI"])
            for g4 in range(8):
                pb = g4 % 2
                pk = "ps%d" % pb
                for gl in range(4):
                    g = g4 * 4 + gl
                    o = self.ps[pb][:, gl * 128:(gl + 1) * 128]
                    c.op("pe", lambda e: e.matmul(out=o, lhsT=HsR[:, g, :, :].rearrange("p s c -> p (s c)"),
                                                  rhs=LtR[:, g, 0:8, :].rearrange("p t c -> p (t c)"), start=True, stop=False),
                         r=["p_HsR", "p_LtR"], w=[pk])
                    c.op("pe", lambda e: e.matmul(out=o, lhsT=HsI[:, g, :, :].rearrange("p s c -> p (s c)"),
                                                  rhs=nLtI[:, g, 0:8, :].rearrange("p t c -> p (t c)"), start=False, stop=True),
                         r=["p_HsI", "p_nLtI"], w=[pk])
                self.tt("dve", Toep[:, g4 * 4:(g4 + 1) * 4, :], self.ps[pb][:].rearrange("p (g n) -> p g n", g=4),
                        msk[:].unsqueeze(1).broadcast_to([128, 4, 128]), ALU.mult, r=[pk, "p_msk"], w=["Toep"])
            GsR = LtR[:, :, 0:8, :].rearrange("p g t c -> p g (t c)")
            GsI = nLtI[:, :, 0:8, :].rearrange("p g t c -> p g (t c)")
            w1 = v1[:, :, 0:8, :].rearrange("p g t c -> p g (t c)")
            w2 = v2[:, :, 0:8, :].rearrange("p g t c -> p g (t c)")
            p7r = PWr[:, 14, :].unsqueeze(2).broadcast_to([64, 32, 128])
            p7i = PWi[:, 14, :].unsqueeze(2).broadcast_to([64, 32, 128])
            hr = HsR[:].rearrange("p g s c -> p g (s c)"); hi = HsI[:].rearrange("p g s c -> p g (s c)")
            self.tt("dve", w1, hr, p7r, ALU.mult, r=["p_HsR", "p_PWr"], w=["p_v1"])
            self.tt("dve", w2, hi, p7i, ALU.mult, r=["p_HsI", "p_PWi"], w=["p_v2"])
            self.tt("dve", GsR, w1, w2, ALU.subtract, r=["p_v1", "p_v2"], w=["p_LtR"])
            self.tt("dve", w1, hi, p7r, ALU.mult, r=["p_HsI", "p_PWr"], w=["p_v1"])
            self.tt("dve", w2, hr, p7i, ALU.mult, r=["p_HsR", "p_PWi"], w=["p_v2"])
            self.tt("dve", GsI, w1, w2, ALU.add, r=["p_v1", "p_v2"], w=["p_nLtI"])
            for g4 in range(8):
                pb = 2 + g4 % 2
                pk = "ps%d" % pb
                pv = self.ps[pb][:].rearrange("p (g r n) -> p g r n", g=4, r=2)
                for gl in range(4):
                    g = g4 * 4 + gl
                    c.op("pe", lambda e: e.transpose(out=pv[:, gl, 0, :], in_=GsR[:, g, :], identity=idf[0:64, 0:64]),
                         r=["p_LtR", "p_idf"], w=[pk])
                    c.op("pe", lambda e: e.transpose(out=pv[:, gl, 1, :], in_=GsI[:, g, :], identity=idf[0:64, 0:64]),
                         r=["p_nLtI", "p_idf"], w=[pk])
                self.cp("act", Wii[:, g4 * 4:(g4 + 1) * 4, :, :], pv, r=[pk], w=["Wii"])
            self.cp("dve", rho8[:], TE[:, 15, :], r=["p_TE"], w=["p_rho8"])
            if "toep_x" in self.export:
                c.dma(self.toep_x, Toep[:], r=["Toep"], w=["toep_x"])
                c.dma(self.wii_x, Wii[:], r=["Wii"], w=["wii_x"])
                c.dma(self.wiv_x[:, 0], WivR[:], r=["WivR"], w=["wiv_x"])
                c.dma(self.wiv_x[:, 1], WivI[:], r=["WivI"], w=["wiv_x"])
        c.barrier()
        with self.scope() as sp:
            T = lambda name, shape, dt=F32: sp.enter_context(self.sbt(name, shape, dt))
            phr = T("p_phr", [64, 32]); ph_s = T("p_phs", [64, 32]); phr2 = T("p_phr2", [64, 32])
            self.ts("dve", phr[:], thr[:], 8.0, ALU.mult, r=["p_thr"], w=["p_phr"])
            self.sincos(phr[:], "p_phr", ph_s[:], None, "p_phs", None, tfs[:], tis[:], red_o=(phr2[:], "p_phr2"))
            rho = T("p_rho", [64, 8, 256])
            ang = T("p_ang", [64, 8, 256]); sk = T("p_sk", [64, 8, 256]); ck = T("p_ck", [64, 8, 256])
            tf4 = T("p_tf4", [64, 8, 256]); ti4 = T("p_ti4", [64, 8, 256], I32)
            for gb in range(4):
                gs = slice(gb * 8, (gb + 1) * 8)
                self.cp("dve", rho[:], rho8[:, gs].unsqueeze(2).broadcast_to([64, 8, 256]), r=["p_rho8"], w=["p_rho"])
                c.op("dve", lambda e: e.memset(rho[:, :, 0:1], 0.0), r=[], w=["p_rho"])
                c.dma(self.rhot[:, gs, :], rho[:], r=["p_rho"], w=["rhot"])
                self.tt("dve", ang[:], phr2[:, gs].unsqueeze(2).broadcast_to([64, 8, 256]),
                        kv[:].unsqueeze(1).broadcast_to([64, 8, 256]), ALU.mult, r=["p_phr2", "p_kv"], w=["p_ang"])
                self.sincos(ang[:], "p_ang", sk[:], ck[:], "p_sk", "p_ck", tf4[:], ti4[:])
                c.dma(self.rots[:, gs, :], sk[:], r=["p_sk"], w=["rots"])
                c.dma(self.rotc[:, gs, :], ck[:], r=["p_ck"], w=["rotc"])
        c.barrier()
        so.close()

    def phase_s5(self):
        nc, c = self.nc, self.c
        with self.scope() as st:
            T = lambda name, shape, dt=F32: st.enter_context(self.sbt(name, shape, dt))
            Toep = T("Toep", [128, 32, 128], BF16)
            Wii = T("Wii", [128, 32, 2, 64], BF16)
            WivR = T("WivR", [64, 32, 128], BF16)
            WivI = T("WivI", [64, 32, 128], BF16)
            self.s5_precompute(st, Toep, Wii, WivR, WivI)
            if self.s5_main_enabled:
                self.s5_main(st, Toep, Wii, WivR, WivI)
        c.barrier()

    def s5_main(self, st0, Toep, Wii, WivR, WivI):
        nc, c = self.nc, self.c
        GB = 4
        with self.scope() as st:
            T = lambda name, shape, dt=F32: st.enter_context(self.sbt(name, shape, dt))
            gluw = T("gluw", [128, 4, BR], BF16)
            gst = T("gluw_st", [128, 4, BR], F32)
            c.dma(gst[:], self.glu_w.rearrange("(c p) n -> p c n", p=128), w=["gluw_st"])
            self.cp("dve", gluw[:], gst[:], r=["gluw_st"], w=["gluw"])
            Drep = T("Drep", [128, BR]); Brep = T("Brep", [128, BR])
            c.dma(Drep[:], self.s5d_rep, w=["Drep"])
            c.dma(Brep[:], self.glub_rep, w=["Brep"])
            for q in range(self.nseq):
                tb = q * self.S
                with self.scope() as sq:
                    TQ = lambda name, shape, dt=F32: sq.enter_context(self.sbt(name, shape, dt))
                    uck = [TQ("uck%d" % i, [128, 8 * BR], BF16) for i in range(2)]
                    szck = [TQ("szck%d" % i, [128, 8 * BR], BF16) for i in range(2)]
                    yck = [TQ("yck%d" % i, [128, 8, BR], BF16) for i in range(2)]
                    for kh in range(2):
                        rows = slice(tb + kh * 1024, tb + (kh + 1) * 1024)
                        c.dma(uck[kh][:], self.u_tok[rows, :].rearrange("(k t) f -> k (t f)", t=8), r=["u_tok"], w=["uck%d" % kh])
                        c.dma(szck[kh][:], self.sz_tok[rows, :].rearrange("(k t) f -> k (t f)", t=8), r=["sz_tok"], w=["szck%d" % kh])
                    with self.scope() as ss:
                        TS = lambda name, shape, dt=F32: ss.enter_context(self.sbt(name, shape, dt))
                        Ug = TS("Ug", [128, 32, 256], BF16)
                        Vb2 = [TS("Vb%d" % i, [64, 2, GB, 256]) for i in range(2)]
                        Wb2 = [TS("Wb%d" % i, [64, 2, GB, 256]) for i in range(2)]
                        tA2 = [TS("tA%d" % i, [64, GB, 256]) for i in range(2)]; tB2 = [TS("tB%d" % i, [64, GB, 256]) for i in range(2)]
                        ck2 = [TS("ck%d" % i, [64, GB, 256]) for i in range(2)]; sk2 = [TS("sk%d" % i, [64, GB, 256]) for i in range(2)]
                        rh2 = [TS("rh%d" % i, [64, GB, 256]) for i in range(2)]
                        Xs2 = [TS("Xs%d" % i, [64, 2, GB, 256], BF16) for i in range(2)]
                        Ysb = TS("Ysb", [128, 8, 256], BF16)
                        for i in range(2):
                            c.op("pool", lambda e: e.memset(Xs2[i][:], 0.0), w=["Xs%d" % i])

                        def load_tabs(b):
                            if b < 32 // GB:
                                gs_ = slice(b * GB, (b + 1) * GB)
                                c.dma(ck2[b % 2][:], self.rotc[:, gs_, :], r=["rotc"], w=["ck%d" % (b % 2)])
                                c.dma(sk2[b % 2][:], self.rots[:, gs_, :], r=["rots"], w=["sk%d" % (b % 2)])
                                c.dma(rh2[b % 2][:], self.rhot[:, gs_, :], r=["rhot"], w=["rh%d" % (b % 2)])
                        load_tabs(0)
                        ucg = TS("ucg", [128, 32, 128], BF16)
                        for kh in range(2):
                            self.cp("pool" if kh == 0 else "dve", ucg[:].rearrange("p g (s c) -> p g s c", s=8),
                                    uck[kh][:].rearrange("p (s g c) -> p g s c", s=8, g=32), r=["uck%d" % kh], w=["ucg"])
                            for g8 in range(4):
                                pb = g8 % 2
                                pk = "ps%d" % pb
                                ptv = self.ps[pb][:].bitcast(BF16).rearrange("p (g k) -> p g k", g=8)
                                for gl in range(8):
                                    g = g8 * 8 + gl
                                    c.op("pe", lambda e: e.transpose(out=ptv[:, gl, :], in_=ucg[:, g, :],
                                                                     identity=self.ident[:]), r=["ucg", "ident"], w=[pk])
                                self.cp("dve" if g8 % 2 == 0 else "act", Ug[:, g8 * 8:(g8 + 1) * 8, kh * 128:(kh + 1) * 128], ptv,
                                        r=[pk], w=["Ug"])
                        for b in range(32 // GB):
                            gs = slice(b * GB, (b + 1) * GB)
                            bp = b % 2
                            Vb, Wb, tA, tB, ck, sk, rh, Xs = Vb2[bp], Wb2[bp], tA2[bp], tB2[bp], ck2[bp], sk2[bp], rh2[bp], Xs2[bp]
                            kVb, kW0, kW1, ktA, ktB, kck, ksk, krh, kXs = ("Vb%d" % bp, "Wb0_%d" % bp, "Wb1_%d" % bp, "tA%d" % bp, "tB%d" % bp,
                                                                           "ck%d" % bp, "sk%d" % bp, "rh%d" % bp, "Xs%d" % bp)
                            load_tabs(b + 1)
                            for gl in range(GB):
                                g = b * GB + gl
                                pb = 2 + gl % 2
                                pk = "ps%d" % pb
                                c.op("pe", lambda e: e.matmul(out=self.ps[pb][0:64, 0:256], lhsT=Wii[:, g, 0, :], rhs=Ug[:, g, :],
                                                              start=True, stop=True), r=["Wii", "Ug"], w=[pk])
                                c.op("pe", lambda e: e.matmul(out=self.ps[pb][0:64, 256:512], lhsT=Wii[:, g, 1, :], rhs=Ug[:, g, :],
                                                              start=True, stop=True), r=["Wii", "Ug"], w=[pk])
                                self.cp("act", Vb[:, :, gl, :], self.ps[pb][0:64, :].rearrange("p (r k) -> p r k", r=2), r=[pk], w=[kVb])
                            self.tt("dve", tA[:], ck[:], Vb[:, 0], ALU.mult, r=[kck, kVb], w=[ktA])
                            self.tt("pool", tB[:], sk[:], Vb[:, 1], ALU.mult, r=[ksk, kVb], w=[ktB])
                            self.tt("dve", Wb[:, 0], tA[:], tB[:], ALU.add, r=[ktA, ktB], w=[kW0])
                            self.tt("pool", tA[:], ck[:], Vb[:, 1], ALU.mult, r=[kck, kVb], w=[ktA])
                            self.tt("dve", tB[:], sk[:], Vb[:, 0], ALU.mult, r=[ksk, kVb], w=[ktB])
                            self.tt("pool", Wb[:, 1], tA[:], tB[:], ALU.subtract, r=[ktA, ktB], w=[kW1])
                            fl = lambda a: a.rearrange("p g k -> p (g k)")
                            c.op("dve", lambda e: e.tensor_tensor_scan(out=fl(Vb[:, 0]), data0=fl(rh[:]), data1=fl(Wb[:, 0]), initial=0.0,
                                                                       op0=ALU.mult, op1=ALU.add), r=[krh, kW0, kVb], w=[kVb])
                            c.op("dve", lambda e: e.tensor_tensor_scan(out=fl(Vb[:, 1]), data0=fl(rh[:]), data1=fl(Wb[:, 1]), initial=0.0,
                                                                       op0=ALU.mult, op1=ALU.add), r=[krh, kW1, kVb], w=[kVb])
                            K1 = 255
                            self.tt("dve", tA[:, :, 0:K1], ck[:, :, 0:K1], Vb[:, 0, :, 0:K1], ALU.mult, r=[kck, kVb], w=[ktA])
                            self.tt("pool", tB[:, :, 0:K1], sk[:, :, 0:K1], Vb[:, 1, :, 0:K1], ALU.mult, r=[ksk, kVb], w=[ktB])
                            self.tt("dve", Xs[:, 0, :, 1:256], tA[:, :, 0:K1], tB[:, :, 0:K1], ALU.subtract, r=[ktA, ktB], w=[kXs])
                            self.tt("pool", tA[:, :, 0:K1], ck[:, :, 0:K1], Vb[:, 1, :, 0:K1], ALU.mult, r=[kck, kVb], w=[ktA])
                            self.tt("dve", tB[:, :, 0:K1], sk[:, :, 0:K1], Vb[:, 0, :, 0:K1], ALU.mult, r=[ksk, kVb], w=[ktB])
                            self.tt("pool", Xs[:, 1, :, 1:256], tA[:, :, 0:K1], tB[:, :, 0:K1], ALU.add, r=[ktA, ktB], w=[kXs])
                            for gl in range(GB):
                                g = b * GB + gl
                                pb = 4 + gl // 2 % 2
                                pk = "ps%d" % pb
                                o = self.ps[pb][:, (gl % 2) * 256:(gl % 2 + 1) * 256]
                                c.op("pe", lambda e: e.matmul(out=o, lhsT=Toep[:, g, :], rhs=Ug[:, g, :], start=True, stop=False),
                                     r=["Toep", "Ug"], w=[pk])
                                c.op("pe", lambda e: e.matmul(out=o, lhsT=WivR[:, g, :], rhs=Xs[:, 0, gl, :], start=False, stop=False),
                                     r=["WivR", kXs], w=[pk])
                                c.op("pe", lambda e: e.matmul(out=o, lhsT=WivI[:, g, :], rhs=Xs[:, 1, gl, :], start=False, stop=True),
                                     r=["WivI", kXs], w=[pk])
                                if gl % 2 == 1:
                                    g8l = (b * GB + gl - 1) % 8
                                    self.cp("act", Ysb[:, g8l:g8l + 2, :], self.ps[pb][:].rearrange("p (g k) -> p g k", g=2), r=[pk], w=["Ysb"])
                            if (b * GB + GB) % 8 == 0:
                                g8 = (b * GB) // 8
                                for kh in range(2):
                                    pb = 6 + kh
                                    pk = "ps%d" % pb
                                    ptv = self.ps[pb][:].bitcast(BF16).rearrange("p (g n) -> p g n", g=8)
                                    for gl in range(8):
                                        c.op("pe", lambda e: e.transpose(out=ptv[:, gl, :], in_=Ysb[:, gl, kh * 128:(kh + 1) * 128],
                                                                         identity=self.ident[:]), r=["Ysb", "ident"], w=[pk])
                                    self.cp("dve", yck[kh][:, :, g8 * 128:(g8 + 1) * 128].rearrange("p t (g c) -> p t g c", g=8),
                                            ptv.rearrange("p g (t c) -> p t g c", t=8), r=[pk], w=["yck%d" % kh])
                    with self.scope() as se:
                        TE_ = lambda name, shape, dt=F32: se.enter_context(self.sbt(name, shape, dt))
                        t1 = TE_("e_t1", [128, 8, BR]); t2 = TE_("e_t2", [128, 8, BR])
                        gck = TE_("gck", [128, 8, BR], BF16)
                        ack = TE_("ack", [128, 8, BR], BF16)
                        gT = [TE_("gT%d" % i, [128, 4, 128], BF16) for i in range(2)]
                        e1 = [TE_("e1_%d" % i, [128, BR]) for i in range(2)]
                        for kh in range(2):
                            uv = uck[kh][:].rearrange("p (s f) -> p s f", s=8)
                            zv = szck[kh][:].rearrange("p (s f) -> p s f", s=8)
                            self.tt("dve", t1[:], uv, Drep[:].unsqueeze(1).broadcast_to([128, 8, BR]), ALU.mult, r=["uck%d" % kh, "Drep"], w=["e_t1"])
                            self.tt("pool", t1[:], t1[:], yck[kh][:], ALU.add, r=["e_t1", "yck%d" % kh], w=["e_t1"])
                            self.actf(t2[:], t1[:], AF.Square, r=["e_t1"], w=["e_t2"])
                            self.ts("dve", t2[:], t2[:], 0.044715 * 0.7978845608, ALU.mult, r=["e_t2"], w=["e_t2"], s2=0.7978845608, op1=ALU.add)
                            self.tt("pool", t2[:], t2[:], t1[:], ALU.mult, r=["e_t2", "e_t1"], w=["e_t2"])
                            self.actf(t2[:], t2[:], AF.Tanh, r=["e_t2"], w=["e_t2"])
                            self.ts("dve", t2[:], t2[:], 1.0, ALU.add, r=["e_t2"], w=["e_t2"], s2=0.5, op1=ALU.mult)
                            self.tt("dve", gck[:], t2[:], t1[:], ALU.mult, r=["e_t2", "e_t1"], w=["gck"])
                            def glu_front(tau):
                                i2 = tau % 2
                                pk = "ps%d" % i2
                                ptv = self.ps[i2][:].bitcast(BF16).rearrange("p (a b) -> p a b", a=8)
                                for fc in range(4):
                                    c.op("pe", lambda e: e.transpose(out=ptv[:, fc, :], in_=gck[:, tau, fc * 128:(fc + 1) * 128],
                                                                     identity=self.ident[:]), r=["gck", "ident"], w=[pk])
                                self.cp("act", gT[i2][:], ptv[:, 0:4, :], r=[pk], w=["gT%d" % i2])
                            glu_front(0)
                            for tau in range(8):
                                i2 = tau % 2
                                pm = 2 + i2
                                for fc in range(4):
                                    c.op("pe", lambda e: e.matmul(out=self.ps[pm][:], lhsT=gT[i2][:, fc, :], rhs=gluw[:, fc, :],
                                                                  start=(fc == 0), stop=(fc == 3)), r=["gT%d" % i2, "gluw"], w=["ps%d" % pm])
                                if tau + 1 < 8:
                                    glu_front(tau + 1)
                                ek = "e1_%d" % i2
                                self.tt("dve", e1[i2][:], self.ps[pm][:], Brep[:], ALU.add, r=["ps%d" % pm, "Brep"], w=[ek])
                                self.actf(e1[i2][:], e1[i2][:], AF.Tanh, r=[ek], w=[ek], scale=0.5)
                                self.ts("dve", e1[i2][:], e1[i2][:], 0.5, ALU.mult, r=[ek], w=[ek], s2=0.5, op1=ALU.add)
                                self.tt("pool", e1[i2][:], e1[i2][:], gck[:, tau, :], ALU.mult, r=[ek, "gck"], w=[ek])
                                self.tt("dve", ack[:, tau, :], e1[i2][:], zv[:, tau, :], ALU.mult, r=[ek, "szck%d" % kh], w=["ack"])
                            rows = slice(tb + kh * 1024, tb + (kh + 1) * 1024)
                            c.dma(self.a_tok[rows, :].rearrange("(k t) f -> k (t f)", t=8), ack[:].rearrange("p t f -> p (t f)"),
                                  r=["ack"], w=["a_tok"])

    def phase_ml(self):
        nc, c = self.nc, self.c
        S_ = self.S
        NB = S_ // 128
        SC = 128 ** -0.5
        with self.scope() as st:
            T = lambda name, shape, dt=F32: st.enter_context(self.sbt(name, shape, dt))
            cw = T("m_cw", [128, 4, 4]); cb = T("m_cb", [128, 4])
            mn = T("m_mn", [128, 4]); msk = T("m_msk", [128, 4])
            gbi = T("m_gbi", [4, 1]); gbf = T("m_gbf", [4, 1]); ngbf = T("m_ngbf", [4, 1])
            maskS = T("m_maskS", [128, 128]); idf = T("m_idf", [128, 128])
            ones4 = T("m_ones4", [4, 128])
            wst = T("m_wst", [128, 3, 4, 128]); wqkv = T("m_wqkv", [128, 3, 4, 128], BF16)
            gst = T("m_gst", [128, 12, 8]); gw = T("m_gw", [128, 12, 8], BF16)
            for (t, d, key) in ((cw, self.ml_cw, "m_cw"), (cb, self.ml_cb, "m_cb"), (mn, self.ml_mn, "m_mn"),
                                (msk, self.ml_msk, "m_msk"), (gbi, self.ml_gbi, "m_gbi"), (gbf, self.ml_gbf, "m_gbf"),
                                (maskS, self.ml_maskS, "m_maskS"), (idf, self.identf_d, "m_idf"),
                                (gst, self.ml_gw, "m_gst")):
                c.dma(t[:], d, w=[key])
            for i, d in enumerate((self.ml_wq, self.ml_wk, self.ml_wv)):
                c.dma(wst[:, i], d, w=["m_wst"])
            self.cp("dve", wqkv[:], wst[:], r=["m_wst"], w=["m_wqkv"])
            self.cp("dve", gw[:], gst[:], r=["m_gst"], w=["m_gw"])
            self.ts("dve", ngbf[:], gbf[:], -1.0, ALU.mult, r=["m_gbf"], w=["m_ngbf"])
            c.op("dve", lambda e: e.memset(ones4[:], 1.0), w=["m_ones4"])
            xmT_v = self.xmT.rearrange("(c p) t -> p c t", p=128)
            mzT_v = self.mzT.rearrange("(c p) t -> p c t", p=128)
            bT_v = self.bT.rearrange("(c p) t -> p c t", p=128)
            for q in range(self.nseq):
                tb = q * S_
                with self.scope() as sq:
                    TQ = lambda name, shape, dt=F32: sq.enter_context(self.sbt(name, shape, dt))
                    xcT = TQ("xcT", [128, 4, S_], BF16)
                    qT = TQ("qT", [128, 4, S_], BF16)
                    kT = TQ("kT", [128, 4, S_], BF16)
                    Ktok = TQ("Ktok", [128, NB, 4, 128], BF16)
                    Vtok = TQ("Vtok", [128, NB, 4, 129], BF16)
                    acol = TQ("acol", [128, NB, 4]); bcol = TQ("bcol", [128, NB, 4])
                    Rrep = TQ("Rrep", [128, NB + 1, 4])
                    Wt = TQ("Wt", [128, NB, 4]); Wp = TQ("Wp", [128, NB, 4])
                    Thr = TQ("Thr", [128, NB, 4]); Dec = TQ("Dec", [128, NB, 4])
                    c.op("pool", lambda e: e.memset(Vtok[:, :, :, 128:129], 1.0), w=["Vtok"])
                    with self.scope() as sa:
                        TA_ = lambda name, shape, dt=F32: sa.enter_context(self.sbt(name, shape, dt))
                        xm = TA_("xm", [128, 4, S_], BF16)
                        vT = TA_("vT", [128, 4, S_], BF16)
                        acc = TA_("acc", [128, S_])
                        g1f = TA_("g1", [32, S_]); g2f = TA_("g2", [32, S_]); g3f = TA_("g3", [32, S_]); onesr = TA_("onesr", [32, S_])
                        g1 = g1f[0:4, :]; g2 = g2f[0:4, :]; g3 = g3f[0:4, :]
                        c.op("pool", lambda e: e.memset(g1f[:], 0.0), w=["g1"])
                        c.op("pool", lambda e: e.memset(g2f[:], 0.0), w=["g2"])
                        rsel = TA_("rsel", [4, NB, 4])
                        c.dma(xm[:], xmT_v[:, :, tb:tb + S_], r=["xmT"], w=["xm"])
                        c.op("pool", lambda e: e.memset(onesr[:], 1.0), w=["onesr"])
                        for fc in range(4):
                            self.ts("dve", acc[:], xm[:, fc, :], cw[:, fc, 3:4], ALU.mult, r=["xm", "m_cw", "m_cb"], w=["acc"],
                                    s2=cb[:, fc:fc + 1], op1=ALU.add)
                            for sh in (1, 2, 3):
                                self.stt("dve", acc[:, sh:], xm[:, fc, 0:S_ - sh], cw[:, fc, 3 - sh:4 - sh], acc[:, sh:],
                                         ALU.mult, ALU.add, r=["xm", "m_cw", "acc"], w=["acc"])
                            self.actf(xcT[:, fc, :], acc[:], AF.Silu, r=["acc"], w=["xcT"])
                        for h in range(4):
                            for tl in range(S_ // 512):
                                ts_ = slice(tl * 512, (tl + 1) * 512)
                                for (i, src, skey, dst, dkey) in ((0, xcT, "xcT", qT, "qT"), (1, xcT, "xcT", kT, "kT"), (2, xm, "xm", vT, "vT")):
                                    pb = (h * 12 + tl * 3 + i) % 4
                                    pk = "ps%d" % pb
                                    c.op("pe", lambda e: e.matmul(out=self.ps[pb][:], lhsT=wqkv[:, i, h, :], rhs=src[:, h, ts_],
                                                                  start=True, stop=True), r=["m_wqkv", skey], w=[pk])
                                    self.cp("act" if i != 1 else "dve", dst[:, h, ts_], self.ps[pb][:], r=[pk], w=[dkey])
                        for blk in range(NB):
                            bs = slice(blk * 128, (blk + 1) * 128)
                            for (i, src, skey, dst, dkey, pb) in ((1, xcT, "xcT", Ktok, "Ktok", 4), (2, xm, "xm", Vtok, "Vtok", 5)):
                                pb = pb + 2 * (blk % 2)
                                pk = "ps%d" % pb
                                for h in range(4):
                                    c.op("pe", lambda e: e.matmul(out=self.ps[pb][:, h * 128:(h + 1) * 128], lhsT=src[:, h, bs],
                                                                  rhs=wqkv[:, i, h, :], start=True, stop=True), r=["m_wqkv", skey], w=[pk])
                                self.cp("act" if i == 1 else "dve", dst[:, blk, :, 0:128], self.ps[pb][:].rearrange("p (h e) -> p h e", h=4),
                                        r=[pk], w=[dkey])
                        for tl in range(S_ // 512):
                            ts_ = slice(tl * 512, (tl + 1) * 512)
                            for half in range(2):
                                pb = half
                                pk = "ps%d" % pb
                                for ch in range(12):
                                    src = (qT, kT, vT)[ch // 4]
                                    skey = ("qT", "kT", "vT")[ch // 4]
                                    c.op("pe", lambda e: e.matmul(out=self.ps[pb][0:4, :], lhsT=gw[:, ch, half * 4:half * 4 + 4],
                                                                  rhs=src[:, ch % 4, ts_], start=(ch == 0), stop=(ch == 11)),
                                         r=["m_gw", skey], w=[pk])
                                if half == 0:
                                    self.ts("dve", g1[:, ts_], self.ps[pb][0:4, :], gbi[:, 0:1], ALU.add, r=[pk, "m_gbi"], w=["g1"])
                                else:
                                    self.actf(g2[:, ts_], self.ps[pb][0:4, :], AF.Exp, r=[pk, "m_ngbf"], w=["g2"], scale=-1.0, bias=ngbf[:, 0:1])
                        self.actf(g2[:], g2[:], AF.Ln, r=["g2"], w=["g2"], bias=1.0)
                        c.op("dve", lambda e: e.tensor_tensor_scan(out=g3f[:], data0=onesr[:], data1=g2f[:], initial=0.0, op0=ALU.mult, op1=ALU.add),
                             r=["onesr", "g2"], w=["g3"])
                        self.tt("dve", g1[:], g1[:], g3[:], ALU.add, r=["g1", "g3"], w=["g1"])
                        c.op("dve", lambda e: e.tensor_tensor_scan(out=g2f[:], data0=onesr[:], data1=g1f[:], initial=0.0, op0=ALU.mult, op1=ALU.max),
                             r=["onesr", "g1", "g2"], w=["g2"])
                        pa = self.ps[2][:, 0:NB * 4].rearrange("p (b h) -> p b h", h=4)
                        pbn = self.ps[3][:, 0:NB * 4].rearrange("p (b h) -> p b h", h=4)
                        for blk in range(NB):
                            bs = slice(blk * 128, (blk + 1) * 128)
                            c.op("pe", lambda e: e.transpose(out=pa[:, blk, :], in_=g1[:, bs], identity=idf[0:4, 0:4]), r=["g1", "m_idf"], w=["ps2"])
                            c.op("pe", lambda e: e.transpose(out=pbn[:, blk, :], in_=g3[:, bs], identity=idf[0:4, 0:4]), r=["g3", "m_idf"], w=["ps3"])
                        self.cp("dve", acol[:], pa, r=["ps2"], w=["acol"])
                        self.cp("dve", bcol[:], pbn, r=["ps3"], w=["bcol"])
                        self.tt("dve", rsel[:], g2[:, 127::128].unsqueeze(2).broadcast_to([4, NB, 4]),
                                idf[0:4, 0:4].unsqueeze(1).broadcast_to([4, NB, 4]), ALU.mult, r=["g2", "m_idf"], w=["rsel"])
                        c.op("pe", lambda e: e.matmul(out=self.ps[0][:, 0:NB * 4], lhsT=ones4[:], rhs=rsel[:].rearrange("p b h -> p (b h)"),
                                                      start=True, stop=True), r=["m_ones4", "rsel"], w=["ps0"])
                        c.op("dve", lambda e: e.memset(Rrep[:, 0, :], 0.0), w=["Rrep"])
                        self.cp("dve", Rrep[:, 1:NB + 1, :], self.ps[0][:, 0:NB * 4].rearrange("p (b h) -> p b h", h=4), r=["ps0"], w=["Rrep"])
                    self.tt("dve", Wt[:], acol[:], Rrep[:, 0:NB, :], ALU.subtract, r=["acol", "Rrep"], w=["Wt"])
                    self.actf(Wt[:], Wt[:], AF.Exp, r=["Wt"], w=["Wt"])
                    self.tt("dve", Wp[:], acol[:], Rrep[:, 1:NB + 1, :], ALU.subtract, r=["acol", "Rrep"], w=["Wp"])
                    self.actf(Wp[:], Wp[:], AF.Exp, r=["Wp"], w=["Wp"])
                    self.ts("dve", Wp[:], Wp[:], SC, ALU.mult, r=["Wp"], w=["Wp"])
                    self.tt("dve", Thr[:], bcol[:], Rrep[:, 0:NB, :], ALU.subtract, r=["bcol", "Rrep"], w=["Thr"])
                    self.actf(Thr[:], Thr[:], AF.Exp, r=["Thr"], w=["Thr"])
                    self.tt("dve", Dec[:], Rrep[:, 0:NB, :], Rrep[:, 1:NB + 1, :], ALU.subtract, r=["Rrep"], w=["Dec"])
                    self.actf(Dec[:], Dec[:], AF.Exp, r=["Dec"], w=["Dec"])
                    with self.scope() as sm:
                        TM = lambda name, shape, dt=F32: sm.enter_context(self.sbt(name, shape, dt))
                        C32 = TM("C32", [128, 4, 129]); Cm = TM("Cm", [128, 4, 129])
                        Cb = TM("Cb", [128, 4, 129], BF16)
                        PT4 = [TM("PT4_%d" % i, [128, 4, 128], BF16) for i in range(2)]
                        Vp4 = [TM("Vp4_%d" % i, [128, 4, 129], BF16) for i in range(2)]
                        Vpp4 = [TM("Vpp4_%d" % i, [128, 4, 129], BF16) for i in range(2)]
                        den = TM("den4", [128, 4])
                        hraw = [TM("hraw%d" % i, [128, 4, 128]) for i in range(2)]
                        bst = TM("bst", [128, 4, 6]); mv = TM("mv", [128, 4, 2]); rs = TM("rs", [128, 4])
                        hn = TM("hn", [128, 4, 128], BF16)
                        e1 = TM("m_e1", [128, 4, 128]); e2 = TM("m_e2", [128, 4, 128])
                        mz = [TM("mz%d" % i, [128, 4, 512], BF16) for i in range(2)]
                        bo = [TM("bo%d" % i, [128, 4, 512], BF16) for i in range(2)]

                        def emit_front(I_):
                            bs_ = slice(I_ * 128, (I_ + 1) * 128)
                            j_ = I_ % 2
                            for h_ in range(4):
                                c.op("pe", lambda e: e.matmul(out=self.ps[j_][:, h_ * 128:(h_ + 1) * 128], lhsT=kT[:, h_, bs_], rhs=qT[:, h_, bs_],
                                                              start=True, stop=True), r=["kT", "qT"], w=["ps%d" % j_])
                            self.tt("dve", PT4[j_][:], self.ps[j_][:].rearrange("p (h t) -> p h t", h=4),
                                    maskS[:].unsqueeze(1).broadcast_to([128, 4, 128]), ALU.mult, r=["ps%d" % j_, "m_maskS"], w=["PT4_%d" % j_])
                            self.tt("pool", Vp4[j_][:], Vtok[:, I_, :, :], Wt[:, I_, :].unsqueeze(2).broadcast_to([128, 4, 129]), ALU.mult,
                                    r=["Vtok", "Wt"], w=["Vp4_%d" % j_])
                            self.tt("pool", Vpp4[j_][:], Vtok[:, I_, :, :], Wp[:, I_, :].unsqueeze(2).broadcast_to([128, 4, 129]), ALU.mult,
                                    r=["Vtok", "Wp"], w=["Vpp4_%d" % j_])
                        emit_front(0)
                        for I in range(NB):
                            bs = slice(I * 128, (I + 1) * 128)
                            i4 = (I // 4) % 2
                            j = I % 2
                            if I % 4 == 0:
                                c.dma(mz[i4][:], mzT_v[:, :, tb + I * 128:tb + I * 128 + 512], r=["mzT"], w=["mz%d" % i4])
                            hk = "hraw%d" % j
                            for h in range(4):
                                pO = self.ps[2 + h // 2][:, (h % 2) * 129:(h % 2 + 1) * 129]
                                kO = "ps%d" % (2 + h // 2)
                                c.op("pe", lambda e: e.matmul(out=pO, lhsT=PT4[j][:, h, :], rhs=Vp4[j][:, h, :], start=True, stop=(I == 0)),
                                     r=["PT4_%d" % j, "Vp4_%d" % j], w=[kO])
                                if I > 0:
                                    c.op("pe", lambda e: e.matmul(out=pO, lhsT=qT[:, h, bs], rhs=Cb[:, h, :], start=False, stop=True),
                                         r=["qT", "Cb"], w=[kO])
                            if I < NB - 1:
                                for h in range(4):
                                    pC = self.ps[4 + h // 2][:, (h % 2) * 129:(h % 2 + 1) * 129]
                                    c.op("pe", lambda e: e.matmul(out=pC, lhsT=Ktok[:, I, h, :], rhs=Vpp4[j][:, h, :], start=True, stop=True),
                                         r=["Ktok", "Vpp4_%d" % j], w=["ps%d" % (4 + h // 2)])
                            if I + 1 < NB:
                                emit_front(I + 1)
                            if I < NB - 1:
                                if I == 0:
                                    for hb in range(2):
                                        self.cp("dve", C32[:, 2 * hb:2 * hb + 2, :], self.ps[4 + hb][:, 0:258].rearrange("p (h e) -> p h e", h=2),
                                                r=["ps%d" % (4 + hb)], w=["C32"])
                                else:
                                    self.tt("pool", Cm[:], C32[:], Dec[:, I, :].unsqueeze(2).broadcast_to([128, 4, 129]), ALU.mult, r=["C32", "Dec"], w=["Cm"])
                                    for hb in range(2):
                                        self.tt("dve", C32[:, 2 * hb:2 * hb + 2, :], self.ps[4 + hb][:, 0:258].rearrange("p (h e) -> p h e", h=2),
                                                Cm[:, 2 * hb:2 * hb + 2, :], ALU.add, r=["ps%d" % (4 + hb), "Cm"], w=["C32"])
                                self.cp("act", Cb[:], C32[:], r=["C32"], w=["Cb"])
                            for hb in range(2):
                                self.actf(den[:, 2 * hb:2 * hb + 2], self.ps[2 + hb][:, 0:258].rearrange("p (h e) -> p h e", h=2)[:, :, 128],
                                          AF.Abs, r=["ps%d" % (2 + hb)], w=["den4"])
                            self.tt("dve", den[:], den[:], Thr[:, I, :], ALU.max, r=["den4", "Thr"], w=["den4"])
                            c.op("dve", lambda e: e.reciprocal(out=den[:], in_=den[:]), r=["den4"], w=["den4"])
                            for hb in range(2):
                                self.tt("dve", hraw[j][:, 2 * hb:2 * hb + 2, :], self.ps[2 + hb][:, 0:258].rearrange("p (h e) -> p h e", h=2)[:, :, 0:128],
                                        den[:, 2 * hb:2 * hb + 2].unsqueeze(2).broadcast_to([128, 2, 128]), ALU.mult, r=["ps%d" % (2 + hb), "den4"], w=[hk])
                            for h in range(4):
                                c.op("dve", lambda e: e.bn_stats(out=bst[:, h, :], in_=hraw[j][:, h, :]), r=[hk], w=["bst"])
                                c.op("dve", lambda e: e.bn_aggr(out=mv[:, h, :], in_=bst[:, h, :]), r=["bst"], w=["mv"])
                            self.actf(rs[:], mv[:, :, 1], AF.Ln, r=["mv"], w=["rs"], bias=HEAD_EPS)
                            self.actf(rs[:], rs[:], AF.Exp, r=["rs"], w=["rs"], scale=-0.5)
                            self.tt("pool", e1[:], hraw[j][:], mv[:, :, 0:1].broadcast_to([128, 4, 128]), ALU.subtract, r=[hk, "mv"], w=["m_e1"])
                            self.tt("dve", hn[:], e1[:], rs[:].unsqueeze(2).broadcast_to([128, 4, 128]), ALU.mult, r=["m_e1", "rs"], w=["hn"])
                            ptv = self.ps[6 + I % 2][:].bitcast(BF16).rearrange("p (a b) -> p a b", a=8)
                            pk = "ps%d" % (6 + I % 2)
                            for h in range(4):
                                c.op("pe", lambda e: e.transpose(out=ptv[:, h, :], in_=hn[:, h, :], identity=self.ident[:]), r=["hn", "ident"], w=[pk])
                            self.tt("pool", e2[:], xcT[:, :, bs], msk[:].unsqueeze(2).broadcast_to([128, 4, 128]), ALU.mult, r=["xcT", "m_msk"], w=["m_e2"])
                            self.tt("dve", e1[:], ptv[:, 0:4, :], mn[:].unsqueeze(2).broadcast_to([128, 4, 128]), ALU.mult, r=[pk, "m_mn", "m_e1"], w=["m_e1"])
                            self.tt("dve", e1[:], e1[:], e2[:], ALU.add, r=["m_e1", "m_e2"], w=["m_e1"])
                            off = (I % 4) * 128
                            self.tt("pool", bo[i4][:, :, off:off + 128], e1[:], mz[i4][:, :, off:off + 128], ALU.mult,
                                    r=["m_e1", "mz%d" % i4], w=["bo%d" % i4])
                            if I % 4 == 3:
                                c.dma(bT_v[:, :, tb + (I - 3) * 128:tb + (I + 1) * 128], bo[i4][:], r=["bo%d" % i4], w=["bT"])
        c.barrier()

    def out_proj_norm_res(self, lay, wout_d, pg_rep_d, lhs_provider, res_rows, dst_rows, dst_key, res_key):
        nc, c = self.nc, self.c
        NT = self.NT
        with self.scope() as st:
            T = lambda name, shape, dt=F32: st.enter_context(self.sbt(name, shape, dt))
            wout = T("wout", [128, 8, D], BF16)
            wst = [T("wost%d" % i, [128, D]) for i in range(2)]
            for fc in range(8):
                c.dma(wst[fc % 2][:], wout_d[fc * 128:(fc + 1) * 128, :], w=["wost%d" % (fc % 2)])
                self.cp("dve" if fc % 2 else "act", wout[:, fc, :], wst[fc % 2][:], r=["wost%d" % (fc % 2)], w=["wout"])
            pg = T("pg", [128, D])
            c.dma(pg[:], pg_rep_d, w=["pg"])
            NXR = 4
            xr = [T("xr%d" % i, [128, D]) for i in range(NXR)]
            yo = [T("yo%d" % i, [128, D]) for i in range(2)]
            junk = T("ojunk", [128, BR])
            ss2 = [T("oss%d" % i, [128, 2]) for i in range(2)]; rstd2 = [T("orstd%d" % i, [128, 1]) for i in range(2)]
            pref, prov = lhs_provider(st)
            nblk = NT // 128

            def prefetch(b):
                if b < nblk:
                    c.dma(xr[b % NXR][:], res_rows[b * 128:(b + 1) * 128, :], r=[res_key % b], w=["xr%d" % (b % NXR)])
                    pref(b)
            prefetch(0)
            prefetch(1)
            lhs_next = prov(0)
            for blk in range(nblk):
                rows = slice(blk * 128, (blk + 1) * 128)
                xk = "xr%d" % (blk % NXR)
                prefetch(blk + 2)
                lhs = lhs_next
                ss = ss2[blk % 2]; rstd = rstd2[blk % 2]
                ssk = "oss%d" % (blk % 2); rsk = "orstd%d" % (blk % 2)
                for half in range(2):
                    pb = 4 + half + 2 * (blk % 2)
                    pk = "ps%d" % pb
                    for fc in range(8):
                        ap, key = lhs[fc]
                        c.op("pe", lambda e: e.matmul(out=self.ps[pb][:], lhsT=ap, rhs=wout[:, fc, half * BR:(half + 1) * BR],
                                                      start=(fc == 0), stop=(fc == 7)), r=[key, "wout"], w=[pk])
                if blk + 1 < nblk:
                    lhs_next = prov(blk + 1)
                for half in range(2):
                    pb = 4 + half + 2 * (blk % 2)
                    pk = "ps%d" % pb
                    self.actf(junk[:], self.ps[pb][:], AF.Square, r=[pk], w=["ojunk", ssk], accum_out=ss[:, half:half + 1])
                self.tt("dve", rstd[:], ss[:, 0:1], ss[:, 1:2], ALU.add, r=[ssk], w=[rsk])
                self.actf(rstd[:], rstd[:], AF.Ln, r=[rsk], w=[rsk], scale=1.0 / D, bias=NORM_EPS)
                self.actf(rstd[:], rstd[:], AF.Exp, r=[rsk], w=[rsk], scale=-0.5)
                yk = "yo%d" % (blk % 2)
                for half in range(2):
                    pb = 4 + half + 2 * (blk % 2)
                    hs = slice(half * BR, (half + 1) * BR)
                    self.tt("dve", yo[blk % 2][:, hs], self.ps[pb][:], pg[:, hs], ALU.mult, r=["ps%d" % pb, "pg"], w=[yk])
                self.stt("dve", yo[blk % 2][:], yo[blk % 2][:], rstd[:, 0:1], xr[blk % NXR][:], ALU.mult, ALU.add, r=[yk, rsk, xk], w=[yk])
                c.dma(dst_rows[rows, :], yo[blk % 2][:], r=[yk], w=[dst_key % blk])

    def phase_l0_out(self):
        nc, c = self.nc, self.c
        bT_v = self.bT.rearrange("(c p) t -> p c t", p=128)

        def provider(st):
            T = lambda name, shape, dt=F32: st.enter_context(self.sbt(name, shape, dt))
            at = [T("at%d" % i, [128, BR], BF16) for i in range(4)]
            aT = [T("aT%d" % i, [128, 4, 128], BF16) for i in range(2)]
            bt = [T("bt%d" % i, [128, 4, 512], BF16) for i in range(2)]

            def pref(blk):
                i4 = (blk // 4) % 2
                c.dma(at[blk % 4][:], self.a_tok[blk * 128:(blk + 1) * 128, :], r=["a_tok"], w=["at%d" % (blk % 4)])
                if blk % 4 == 0:
                    c.dma(bt[i4][:], bT_v[:, :, blk * 128:blk * 128 + 512], r=["bT"], w=["bt%d" % i4])

            def prov(blk):
                i = blk % 2
                i4 = (blk // 4) % 2
                pk = "ps%d" % i
                ptv = self.ps[i][:].bitcast(BF16).rearrange("p (a b) -> p a b", a=8)
                for fc in range(4):
                    c.op("pe", lambda e: e.transpose(out=ptv[:, fc, :], in_=at[blk % 4][:, fc * 128:(fc + 1) * 128], identity=self.ident[:]),
                         r=["at%d" % (blk % 4), "ident"], w=[pk])
                self.cp("act", aT[i][:], ptv[:, 0:4, :], r=[pk], w=["aT%d" % i])
                off = (blk % 4) * 128
                return [(aT[i][:, fc, :], "aT%d" % i) for fc in range(4)] + \
                       [(bt[i4][:, h, off:off + 128], "bt%d" % i4) for h in range(4)]
            return pref, prov
        self.out_proj_norm_res(0, self.w_out_ab, self.post0_rep, provider, self.x, self.out, "h1_%d", "x%.0d")

    def phase_l1_proj(self):
        nc, c = self.nc, self.c
        NT = self.NT
        S_ = self.S
        ngrp = NT // 512
        nblk = NT // 128
        with self.scope() as st:
            T = lambda name, shape, dt=F32: st.enter_context(self.sbt(name, shape, dt))
            win = T("win1", [128, 8, 8 * BR], BF16)
            g1 = T("g1n", [128, 8])
            stage = [T("w1st%d" % i, [128, 4 * BR]) for i in range(2)]
            c.dma(g1[:], self.pre1, w=["g1n"])
            for dc in range(8):
                for hf in range(2):
                    i = (dc * 2 + hf) % 2
                    sk = "w1st%d" % i
                    c.dma(stage[i][:], self.w_in_cd[dc * 128:(dc + 1) * 128, hf * 2048:(hf + 1) * 2048], w=[sk])
                    if hf == 0:
                        c.op("act", lambda e: e.activation(out=win[:, dc, 0:2048], in_=stage[i][:], func=AF.Copy, scale=g1[:, dc:dc + 1]),
                             r=[sk, "g1n"], w=["win1"])
                    else:
                        self.ts("dve", win[:, dc, 2048:4096], stage[i][:], g1[:, dc:dc + 1], ALU.mult, r=[sk, "g1n"], w=["win1"])
            cosT = T("cosT", [128, S_]); sinT = T("sinT", [128, S_]); Rm = T("Rm", [128, 128], BF16)
            c.dma(cosT[:], self.rope_cos, w=["cosT"])
            c.dma(sinT[:], self.rope_sin, w=["sinT"])
            c.dma(Rm[:], self.rope_rm, w=["Rm"])
            NXB = 6
            xt = [T("x1t%d" % i, [128, D]) for i in range(NXB)]
            junk = T("junk1", [128, D])
            ss2 = [T("ss1_%d" % i, [128, 4]) for i in range(2)]; rstd2 = [T("rstd1_%d" % i, [128, 4]) for i in range(2)]
            zb = [T("z1b%d" % i, [128, D], BF16) for i in range(2)]
            zT = [T("z1T%d" % i, [128, 8, 512], BF16) for i in range(2)]
            vo = [T("vo%d" % i, [128, BR], BF16) for i in range(4)]
            xb = [T("xb%d" % i, [128, 512], BF16) for i in range(2)]
            r1 = [T("r1_%d" % i, [128, 512]) for i in range(2)]
            r2 = [T("r2_%d" % i, [128, 512]) for i in range(2)]
            fo = [T("fo%d" % i, [128, 4, 512], BF16) for i in range(2)]

            def load_x(b):
                if b < nblk:
                    c.dma(xt[b % NXB][:], self.h1src[b * 128:(b + 1) * 128, :], r=["h1_%d" % b], w=["x1t%d" % (b % NXB)])
            for b in range(4):
                load_x(b)
            fm_groups = [(0, self.cqT, "cqT", "rope"), (512, self.ckT, "ckT", "rope"), (2048, self.dqT, "dqT", "rope"),
                         (2560, self.dkT, "dkT", "rope"), (1536, self.czT, "czT", "silu"), (3584, self.dzT, "dzT", "silu")]
            cnt = 0
            for g in range(ngrp):
                zk = "z1T%d" % (g % 2)
                ss = ss2[g % 2]; rstd = rstd2[g % 2]
                ssk = "ss1_%d" % (g % 2); rsk = "rstd1_%d" % (g % 2)
                pos0 = (g * 512) % S_
                for j in range(4):
                    b = g * 4 + j
                    xk = "x1t%d" % (b % NXB)
                    c.op("act", lambda e: e.activation(out=junk[:], in_=xt[b % NXB][:], func=AF.Square, accum_out=ss[:, j:j + 1]),
                         r=[xk], w=["junk1", ssk])
                self.actf(rstd[:], ss[:], AF.Ln, r=[ssk], w=[rsk], scale=1.0 / D, bias=NORM_EPS)
                self.actf(rstd[:], rstd[:], AF.Exp, r=[rsk], w=[rsk], scale=-0.5)
                for j in range(4):
                    b = g * 4 + j
                    xk = "x1t%d" % (b % NXB)
                    zbk = "z1b%d" % (b % 2)
                    pk = "ps%d" % (b % 2)
                    self.ts("dve", zb[b % 2][:], xt[b % NXB][:], rstd[:, j:j + 1], ALU.mult, r=[xk, rsk], w=[zbk])
                    load_x(b + 4)
                    ptv = self.ps[b % 2][:].bitcast(BF16).rearrange("p (a b) -> p a b", a=8)
                    for dc in range(8):
                        c.op("pe", lambda e: e.transpose(out=ptv[:, dc, :], in_=zb[b % 2][:, dc * 128:(dc + 1) * 128], identity=self.ident[:]),
                             r=[zbk, "ident"], w=[pk])
                    self.cp("dve", zT[g % 2][:, :, j * 128:(j + 1) * 128], ptv, r=[pk], w=[zk])
                    for vi, (col0, dst, dkey) in enumerate(((1024, self.cv_tok, "cv_tok"), (3072, self.dv_tok, "dv_tok"))):
                        pb = 2 + vi
                        vk = "vo%d" % ((b % 2) * 2 + vi)
                        for dc in range(8):
                            c.op("pe", lambda e: e.matmul(out=self.ps[pb][:], lhsT=zT[g % 2][:, dc, j * 128:(j + 1) * 128],
                                                          rhs=win[:, dc, col0:col0 + BR], start=(dc == 0), stop=(dc == 7)), r=[zk, "win1"], w=["ps%d" % pb])
                        self.cp("act", vo[(b % 2) * 2 + vi][:], self.ps[pb][:], r=["ps%d" % pb], w=[vk])
                        c.dma(dst[b * 128:(b + 1) * 128, :], vo[(b % 2) * 2 + vi][:], r=[vk], w=[dkey])
                pend = []

                def flush_pend():
                    while pend:
                        (fot_, fc_, fk_, i2_, pb_, dst_, dkey_, lastfc) = pend.pop(0)
                        pr = 6 + i2_
                        c.op("pe", lambda e: e.matmul(out=self.ps[pr][:], lhsT=Rm[:], rhs=xb[i2_][:], start=True, stop=True),
                             r=["Rm", "xb%d" % i2_], w=["ps%d" % pr])
                        self.tt("dve", r1[i2_][:], self.ps[pb_][:], cosT[:, pos0:pos0 + 512], ALU.mult, r=["ps%d" % pb_, "cosT"], w=["r1_%d" % i2_])
                        self.tt("dve", r2[i2_][:], self.ps[pr][:], sinT[:, pos0:pos0 + 512], ALU.mult, r=["ps%d" % pr, "sinT"], w=["r2_%d" % i2_])
                        self.tt("pool", fot_[:, fc_, :], r1[i2_][:], r2[i2_][:], ALU.add, r=["r1_%d" % i2_, "r2_%d" % i2_], w=[fk_])
                        if lastfc:
                            c.dma(dst_.rearrange("(c p) t -> p c t", p=128)[:, :, g * 512:(g + 1) * 512], fot_[:], r=[fk_], w=[dkey_])
                for (col0, dst, dkey, kind) in fm_groups:
                    fk = "fo%d" % (cnt % 2)
                    fot = fo[cnt % 2]
                    cnt += 1
                    for fc in range(4):
                        pb = 4 + fc % 2
                        pk = "ps%d" % pb
                        for dc in range(8):
                            c.op("pe", lambda e: e.matmul(out=self.ps[pb][:], lhsT=win[:, dc, col0 + fc * 128:col0 + (fc + 1) * 128],
                                                          rhs=zT[g % 2][:, dc, :], start=(dc == 0), stop=(dc == 7)), r=[zk, "win1"], w=[pk])
                        flush_pend()
                        if kind == "silu":
                            self.actf(fot[:, fc, :], self.ps[pb][:], AF.Silu, r=[pk], w=[fk])
                            if fc == 3:
                                c.dma(dst.rearrange("(c p) t -> p c t", p=128)[:, :, g * 512:(g + 1) * 512], fot[:], r=[fk], w=[dkey])
                        else:
                            i2 = fc % 2
                            self.cp("act", xb[i2][:], self.ps[pb][:], r=[pk], w=["xb%d" % i2])
                            pend.append((fot, fc, fk, i2, pb, dst, dkey, fc == 3))
                flush_pend()

    def phase_attn(self, kind):
        nc, c = self.nc, self.c
        S_ = self.S
        NB = S_ // 128
        NQT = S_ // 512
        dil = (kind == "dil")
        qT_d, kT_d, v_d, o_d = (self.cqT, self.ckT, self.cv_tok, self.oc_tok) if dil else (self.dqT, self.dkT, self.dv_tok, self.od_tok)
        okey = "oc_tok" if dil else "od_tok"
        NH = 8 if dil else 4
        VW = 64 if dil else 128
        nmask = 9 if dil else 4
        lam_init = 0.8 - 0.6 * math.exp(-0.3 * 1)
        with self.scope() as st:
            T = lambda name, shape, dt=F32: st.enter_context(self.sbt(name, shape, dt))
            masks = T("amask", [128, nmask, 512], BF16)
            c.dma(masks[:], self.dil_masks if dil else self.diff_masks, w=["amask"])
            if not dil:
                lqk = T("lqk", [1, 4, 64]); pr = T("lpr", [1, 2, 64]); sm = T("lsm", [1, 2]); nl = T("nl", [1, 1])
                ones1 = T("ones1", [1, 128]); nlam = T("nlam", [128, 1]); gdn = T("gdn", [128, 128])
                c.dma(lqk[:], self.diff_lqk, w=["lqk"])
                c.dma(gdn[:], self.diffnorm_rep, w=["gdn"])
                c.op("dve", lambda e: e.memset(ones1[:], 1.0), w=["ones1"])
                self.tt("dve", pr[:, 0, :], lqk[:, 0, :], lqk[:, 1, :], ALU.mult, r=["lqk"], w=["lpr"])
                self.tt("dve", pr[:, 1, :], lqk[:, 2, :], lqk[:, 3, :], ALU.mult, r=["lqk", "lpr"], w=["lpr"])
                c.op("dve", lambda e: e.reduce_sum(out=sm[:], in_=pr[:], axis=AX.X), r=["lpr"], w=["lsm"])
                self.actf(sm[:], sm[:], AF.Exp, r=["lsm"], w=["lsm"])
                self.tt("dve", nl[:], sm[:, 1:2], sm[:, 0:1], ALU.subtract, r=["lsm"], w=["nl"])
                self.ts("dve", nl[:], nl[:], -lam_init, ALU.add, r=["nl"], w=["nl"])
                c.op("pe", lambda e: e.matmul(out=self.ps[0][:, 0:1], lhsT=ones1[:], rhs=nl[:], start=True, stop=True), r=["ones1", "nl"], w=["ps0"])
                self.cp("dve", nlam[:], self.ps[0][:, 0:1], r=["ps0"], w=["nlam"])
                self.ts("dve", gdn[:], gdn[:], 1.0 - lam_init, ALU.mult, r=["gdn"], w=["gdn"])
            qT_v = qT_d.rearrange("(c p) t -> p c t", p=128)
            kT_v = kT_d.rearrange("(c p) t -> p c t", p=128)
            for q in range(self.nseq):
                tb = q * S_
                with self.scope() as sq:
                    TQ = lambda name, shape, dt=F32: sq.enter_context(self.sbt(name, shape, dt))
                    qT = TQ("aqT", [128, 4, S_], BF16); kT = TQ("akT", [128, 4, S_], BF16)
                    V1 = TQ("aV1", [128, NB, NH, VW + 1], BF16)
                    osb = TQ("aosb", [128, NB, BR], BF16)
                    NEP = 3
                    E = [TQ("aE%d" % i, [128, 512], BF16) for i in range(NEP)]
                    P = [TQ("aP%d" % i, [128, 512], BF16) for i in range(NEP)]
                    rd = [TQ("ard%d" % i, [128, 1]) for i in range(4)]
                    if not dil:
                        o01 = [TQ("ao%d" % i, [128, NB, 128]) for i in range(2)]
                        sqj = TQ("asq", [128, 128]); ssn = TQ("assn", [128, NB]); t3 = TQ("at3", [128, 128])
                    c.dma(qT[:], qT_v[:, :, tb:tb + S_], r=[("cqT" if dil else "dqT")], w=["aqT"])
                    c.dma(kT[:], kT_v[:, :, tb:tb + S_], r=[("ckT" if dil else "dkT")], w=["akT"])
                    c.op("pool", lambda e: e.memset(V1[:, :, :, VW:VW + 1], 1.0), w=["aV1"])
                    for blk in range(NB):
                        c.dma(V1[:, blk, :, 0:VW], v_d[tb + blk * 128:tb + (blk + 1) * 128, :].rearrange("p (h d) -> p h d", h=NH),
                              r=[("cv_tok" if dil else "dv_tok")], w=["aV1"])
                    tiles = []
                    for h in range(NH):
                        for m in range(1 if dil else 2):
                            for qt in range(NQT):
                                nkb = 4 * qt + 4
                                for kb in range(nkb):
                                    tiles.append((h, m, qt, kb, kb == nkb - 1))
                    LA = 3
                    NSB = 4

                    def emit_S(i):
                        h, m, qt, kb, _ = tiles[i]
                        ch = h // 2 if dil else h
                        rb = 64 * (h % 2) if dil else 64 * m
                        sb = i % NSB
                        c.op("pe", lambda e: e.matmul(out=self.ps[sb][:], lhsT=kT[rb:rb + 64, ch, kb * 128:(kb + 1) * 128],
                                                      rhs=qT[rb:rb + 64, ch, qt * 512:(qt + 1) * 512], start=True, stop=True),
                             r=["akT", "aqT"], w=["ps%d" % sb])
                    for i in range(min(LA, len(tiles))):
                        emit_S(i)
                    for i, (h, m, qt, kb, last) in enumerate(tiles):
                        if i + LA < len(tiles):
                            emit_S(i + LA)
                        sb = i % NSB
                        i2 = i % NEP
                        pS = self.ps[sb]
                        kS = "ps%d" % sb
                        d0 = 4 * qt - kb
                        if dil:
                            mi = 8 if d0 >= 5 else d0 + 3
                        else:
                            mi = d0 + 3 if d0 <= 0 else None
                        if mi is None:
                            self.actf(P[i2][:], pS[:], AF.Exp, r=[kS], w=["aP%d" % i2], scale=0.125)
                        else:
                            self.actf(E[i2][:], pS[:], AF.Exp, r=[kS], w=["aE%d" % i2], scale=0.125)
                            self.tt("dve", P[i2][:], E[i2][:], masks[:, mi, :], ALU.mult,
                                    r=["aE%d" % i2, "amask"], w=["aP%d" % i2])
                        for j in range(4):
                            Q = 4 * qt + j
                            if kb > Q:
                                continue
                            c.op("pe", lambda e: e.matmul(out=self.ps[4 + j][:, 0:VW + 1], lhsT=P[i2][:, j * 128:(j + 1) * 128],
                                                          rhs=V1[:, kb, h, :], start=(kb == 0), stop=(kb == Q)),
                                 r=["aP%d" % i2, "aV1"], w=["ps%d" % (4 + j)])
                        if last:
                            for j in range(4):
                                Q = 4 * qt + j
                                pO = self.ps[4 + j]
                                kO = "ps%d" % (4 + j)
                                c.op("dve", lambda e: e.reciprocal(out=rd[j][:], in_=pO[:, VW:VW + 1]), r=[kO], w=["ard%d" % j])
                                if dil:
                                    c.op("act", lambda e: e.activation(out=osb[:, Q, h * 64:(h + 1) * 64], in_=pO[:, 0:VW], func=AF.Copy,
                                                                       scale=rd[j][:, 0:1]), r=[kO, "ard%d" % j], w=["aosb"])
                                else:
                                    c.op("act", lambda e: e.activation(out=o01[m][:, Q, :], in_=pO[:, 0:VW], func=AF.Copy,
                                                                       scale=rd[j][:, 0:1]), r=[kO, "ard%d" % j], w=["ao%d" % m])
                            if (not dil) and m == 1 and qt == NQT - 1:
                                self.stt("dve", o01[0][:], o01[1][:], nlam[:, 0:1], o01[0][:], ALU.mult, ALU.add, r=["ao0", "ao1", "nlam"], w=["ao0"])
                                for Q in range(NB):
                                    self.actf(sqj[:], o01[0][:, Q, :], AF.Square, r=["ao0"], w=["asq", "assn"], accum_out=ssn[:, Q:Q + 1])
                                self.actf(ssn[:], ssn[:], AF.Ln, r=["assn"], w=["assn"], scale=1.0 / 128, bias=HEAD_EPS)
                                self.actf(ssn[:], ssn[:], AF.Exp, r=["assn"], w=["assn"], scale=-0.5)
                                for Q in range(NB):
                                    self.ts("dve", t3[:], o01[0][:, Q, :], ssn[:, Q:Q + 1], ALU.mult, r=["ao0", "assn"], w=["at3"])
                                    self.tt("pool", osb[:, Q, h * 128:(h + 1) * 128], t3[:], gdn[:], ALU.mult, r=["at3", "gdn"], w=["aosb"])
                    c.dma(o_d[tb:tb + S_, :].rearrange("(b p) f -> p b f", p=128), osb[:], r=["aosb"], w=[okey])

    def phase_l1_out(self):
        nc, c = self.nc, self.c
        czT_v = self.czT.rearrange("(c p) t -> p c t", p=128)
        dzT_v = self.dzT.rearrange("(c p) t -> p c t", p=128)

        def provider(st):
            T = lambda name, shape, dt=F32: st.enter_context(self.sbt(name, shape, dt))
            ot = [T("ot%d" % i, [128, 2, BR], BF16) for i in range(4)]
            gT = [T("gT1_%d" % i, [128, 8, 128], BF16) for i in range(2)]
            gz = [T("gz%d" % i, [128, 8, 512], BF16) for i in range(2)]

            def pref(blk):
                i = blk % 4
                i4 = (blk // 4) % 2
                c.dma(ot[i][:, 0, :], self.oc_tok[blk * 128:(blk + 1) * 128, :], r=["oc_tok"], w=["ot%d" % i])
                c.dma(ot[i][:, 1, :], self.od_tok[blk * 128:(blk + 1) * 128, :], r=["od_tok"], w=["ot%d" % i])
                if blk % 4 == 0:
                    c.dma(gz[i4][:, 0:4, :], czT_v[:, :, blk * 128:blk * 128 + 512], r=["czT"], w=["gz%d" % i4])
                    c.dma(gz[i4][:, 4:8, :], dzT_v[:, :, blk * 128:blk * 128 + 512], r=["dzT"], w=["gz%d" % i4])

            def prov(blk):
                i = blk % 2
                i4 = (blk // 4) % 2
                pk = "ps%d" % i
                ptv = self.ps[i][:].bitcast(BF16).rearrange("p (a b) -> p a b", a=8)
                for fc in range(8):
                    c.op("pe", lambda e: e.transpose(out=ptv[:, fc, :], in_=ot[blk % 4][:, fc // 4, (fc % 4) * 128:(fc % 4 + 1) * 128],
                                                     identity=self.ident[:]), r=["ot%d" % (blk % 4), "ident"], w=[pk])
                off = (blk % 4) * 128
                self.tt("dve", gT[i][:], ptv, gz[i4][:, :, off:off + 128], ALU.mult, r=[pk, "gz%d" % i4], w=["gT1_%d" % i])
                return [(gT[i][:, fc, :], "gT1_%d" % i) for fc in range(8)]
            return pref, prov
        self.out_proj_norm_res(1, self.w_out_cd, self.post1_rep, provider, self.h1src, self.out, "h1_%d", "h1_%d")


def core_inputs(inp, x_rows):
    m = {}
    m["x"] = np.ascontiguousarray(x_rows, dtype=np.float32)
    m["ident"] = np.eye(128, dtype=np.float32).astype(ml_dtypes.bfloat16)
    m["pre0"] = np.ascontiguousarray(inp["pre_norm"][0].reshape(8, 128).T)
    m["w_in_ab"] = inp["w_in_ab"][0]
    m["s5_lr"] = inp["s5_lambda_re"][0].T
    m["s5_li"] = inp["s5_lambda_im"][0].T
    m["s5_ldt"] = np.broadcast_to(inp["s5_log_dt"][0][None, :], (64, 32))
    m["s5_br"] = inp["s5_b_re"][0].transpose(1, 0, 2)
    m["s5_bi"] = inp["s5_b_im"][0].transpose(1, 0, 2)
    m["s5_cr"] = inp["s5_c_re"][0].transpose(2, 0, 1)
    m["s5_ci"] = inp["s5_c_im"][0].transpose(2, 0, 1)
    tv = np.array([0, -1, -2, -3, -4, -5, -6, -7, 1, 2, 3, 4, 5, 6, 7, 8], dtype=np.float32)
    m["tv"] = np.broadcast_to(tv[None, :], (64, 16))
    m["kv"] = np.broadcast_to(np.arange(256, dtype=np.float32)[None, :], (64, 256))
    sidx = np.arange(128) // 16
    m["toepmask"] = (sidx[None, :] >= sidx[:, None]).astype(np.float32)
    m["identf"] = np.eye(128, dtype=np.float32)
    m["s5d_rep"] = np.broadcast_to(inp["s5_d"][0][None, :], (128, 512))
    m["glub_rep"] = np.broadcast_to(inp["s5_glu_b"][0][None, :], (128, 512))
    m["glu_w"] = inp["s5_glu_w"][0]
    m["ml_cw"] = inp["ml_conv_w"][0].reshape(4, 4, 128).transpose(2, 1, 0)
    m["ml_cb"] = inp["ml_conv_b"][0].reshape(4, 128).T
    m["ml_mn"] = inp["ml_norm"][0].reshape(4, 128).T
    m["ml_msk"] = inp["ml_skip"][0].reshape(4, 128).T
    m["ml_gbi"] = inp["ml_gate_b"][0][0:4].reshape(4, 1)
    m["ml_gbf"] = inp["ml_gate_b"][0][4:8].reshape(4, 1)
    si = np.arange(128)
    m["ml_maskS"] = ((si[:, None] <= si[None, :]) * (128 ** -0.5)).astype(np.float32)
    m["ml_gw"] = inp["ml_gate_w"][0].reshape(12, 128, 8).transpose(1, 0, 2)
    m["ml_wq"] = inp["ml_wq"][0].transpose(1, 0, 2)
    m["ml_wk"] = inp["ml_wk"][0].transpose(1, 0, 2)
    m["ml_wv"] = inp["ml_wv"][0].transpose(1, 0, 2)
    m["w_out_ab"] = inp["w_out_ab"][0]
    m["pre1"] = np.ascontiguousarray(inp["pre_norm"][1].reshape(8, 128).T)
    m["w_in_cd"] = inp["w_in_cd"][0]
    pos = np.arange(S, dtype=np.float32)
    inv = (10000.0 ** (-np.arange(0, 64, 2, dtype=np.float32) / 64)).astype(np.float32)
    ang = pos[None, :] * inv[np.arange(128) % 32][:, None]
    m["rope_cos"] = np.cos(ang).astype(np.float32)
    m["rope_sin"] = np.sin(ang).astype(np.float32)
    rm = np.zeros((128, 128), dtype=np.float32)
    for mm in range(128):
        if mm % 64 < 32:
            rm[mm + 32, mm] = -1.0
        else:
            rm[mm - 32, mm] = 1.0
    m["rope_rm"] = rm.astype(ml_dtypes.bfloat16)
    kk = np.arange(128)[:, None]
    qq = np.arange(512)[None, :]
    dm = np.zeros((128, 9, 512), dtype=np.float32)
    fm = np.zeros((128, 4, 512), dtype=np.float32)
    for mi in range(9):
        d0 = mi - 3 if mi < 8 else 5
        dl = 128 * d0 + qq - kk
        mult = ((dl >= 0) & (dl <= 128)).astype(np.float32) + ((dl >= 0) & (dl % 4 == 0) & (dl <= 512)) + ((dl >= 0) & (dl % 16 == 0) & (dl <= 2048))
        dm[:, mi, :] = mult
        if mi < 4:
            fm[:, mi, :] = (dl >= 0)
    m["dil_masks"] = dm.astype(ml_dtypes.bfloat16)
    m["diff_masks"] = fm.astype(ml_dtypes.bfloat16)
    m["diff_lqk"] = np.stack([inp["diff_lq1"][0], inp["diff_lk1"][0], inp["diff_lq2"][0], inp["diff_lk2"][0]])[None]
    m["diffnorm_rep"] = np.broadcast_to(inp["diff_norm"][0][None, :], (128, 128))
    m["w_out_cd"] = inp["w_out_cd"][0]
    m["post1_rep"] = np.broadcast_to(inp["post_norm"][1][None, :], (128, 1024))
    m["post0_rep"] = np.broadcast_to(inp["post_norm"][0][None, :], (128, 1024))
    return m


_CACHE = {}


def kernel(**inputs):
    inp = {k_: np.asarray(v) for k_, v in inputs.items()}
    x = inp["x"]
    B = x.shape[0]
    nseq = B // NCORES
    if "prog" not in _CACHE:
        kb = K(nseq=nseq)
        kb.build()
        _CACHE["prog"] = kb
    kb = _CACHE["prog"]
    in_maps = []
    for ci in range(NCORES):
        m = core_inputs(inp, x[ci * nseq:(ci + 1) * nseq].reshape(-1, D))
        in_maps.append({n: np.ascontiguousarray(m[n]) for n in kb.inputs})
    res = run_bass_kernel_spmd(kb.nc, in_maps, core_ids=list(range(NCORES)))
    out = np.stack([np.asarray(res.results[ci]["out"]).reshape(nseq, S, D) for ci in range(NCORES)], axis=0)
    return out.reshape(B, S, D).astype(np.float32)
```

```python
import contextlib
import math
import numpy as np
import ml_dtypes
import concourse.bass as bass
import concourse.mybir as mybir
from concourse.bass_utils import run_bass_kernel_spmd

F32 = mybir.dt.float32
BF16 = mybir.dt.bfloat16
I32 = mybir.dt.int32
AF = mybir.ActivationFunctionType
ALU = mybir.AluOpType
AX = mybir.AxisListType

D = 1024
S = 2048
BR = 512
NCORES = 8
SAME_ENGINE_SYNC = True
NORM_EPS = 1e-6
HEAD_EPS = 1e-5


class Ctx:
    def __init__(self, nc, stack, n_dma_sems=48, same_engine_sync=SAME_ENGINE_SYNC):
        self.nc = nc
        self.eng = {"pe": nc.tensor, "act": nc.scalar, "dve": nc.vector,
                    "pool": nc.gpsimd, "sp": nc.sync}
        self.sem = {}
        self.cnt = {}
        for k in ("pe", "act", "dve", "pool"):
            self.sem[k] = stack.enter_context(nc.semaphore("s_" + k))
            self.cnt[k] = 0
        self.dma_sems = []
        for i in range(n_dma_sems):
            k = "dma%d" % i
            self.sem[k] = stack.enter_context(nc.semaphore("s_" + k))
            self.cnt[k] = 0
            self.dma_sems.append(k)
        self.dma_rr = 0
        self.waited = {k: {} for k in self.eng}
        self.last_w = {}
        self.readers = {}
        self.same_engine_sync = same_engine_sync
        self.n_instr = 0
        self.n_wait = 0

    def _deps(self, r, w):
        deps = []
        for x in r:
            if x in self.last_w:
                deps.append(self.last_w[x])
            if x.startswith("ps"):
                deps.extend(self.readers.get(x, ()))
        for x in w:
            if x in self.last_w:
                deps.append(self.last_w[x])
            deps.extend(self.readers.get(x, ()))
        return deps

    def _wait(self, e, deps):
        need = {}
        for (k, v) in deps:
            if k == e and (e == "pe" or not self.same_engine_sync):
                continue
            if need.get(k, 0) < v:
                need[k] = v
        for k, v in need.items():
            if self.waited[e].get(k, 0) >= v:
                continue
            self.eng[e].wait_ge(self.sem[k], v)
            self.waited[e][k] = v
            self.n_wait += 1

    def _commit(self, tok, r, w):
        for x in w:
            self.last_w[x] = tok
            self.readers[x] = []
        for x in r:
            if x in w:
                continue
            self.readers.setdefault(x, []).append(tok)

    def op(self, e, fn, r=(), w=()):
        self._wait(e, self._deps(r, w))
        ins = fn(self.eng[e])
        self.cnt[e] += 1
        ins.then_inc(self.sem[e], 1)
        self._commit((e, self.cnt[e]), r, w)
        self.n_instr += 1
        return ins

    def dma(self, out, in_, r=(), w=(), q="sp", **kw):
        k = self.dma_sems[self.dma_rr]
        self.dma_rr = (self.dma_rr + 1) % len(self.dma_sems)
        deps = self._deps(r, w)
        if self.cnt[k] > 0:
            deps.append((k, self.cnt[k]))
        self._wait(q, deps)
        ins = self.eng[q].dma_start(out=out, in_=in_, **kw)
        self.cnt[k] += 16
        ins.then_inc(self.sem[k], 16)
        self._commit((k, self.cnt[k]), r, w)
        self.n_instr += 1
        return ins

    def barrier(self):
        deps = [(k, v) for k, v in self.cnt.items() if v > 0]
        for e in self.eng:
            self._wait(e, deps)

    def finish(self, res):
        deps = [self.last_w[x] for x in res if x in self.last_w]
        self._wait("sp", deps)


class K:
    def __init__(self, nseq=2, export=(), phases=None, seqlen=S):
        self.nseq = nseq
        self.S = seqlen
        self.NT = nseq * seqlen
        self.export = set(export)
        self.phases = phases
        self.nc = bass.Bass("TRN2", target_bir_lowering=False)
        self.inputs = {}
        self.outputs = {}
        self.s5_main_enabled = True
        self._uid = 0

    def sbt(self, name, shape, dt):
        self._uid += 1
        return self.nc.sbuf_tensor("%s_u%d" % (name, self._uid), list(shape), dt)

    def din(self, name, shape, dt=F32):
        ap = self.nc.dram_tensor(name, list(shape), dt, kind="ExternalInput").ap()
        self.inputs[name] = ap
        return ap

    def dscr(self, name, shape, dt):
        kind = "ExternalOutput" if name in self.export else "Internal"
        ap = self.nc.dram_tensor(name, list(shape), dt, kind=kind).ap()
        if kind == "ExternalOutput":
            self.outputs[name] = ap
        return ap

    @contextlib.contextmanager
    def scope(self):
        with contextlib.ExitStack() as st:
            yield st
            self.c.barrier()

    def build(self):
        nc = self.nc
        NT = self.NT
        with contextlib.ExitStack() as st:
            self.c = Ctx(nc, st)
            self.ps = [st.enter_context(nc.psum_tensor("ps%d" % i, [128, 512], F32)) for i in range(8)]
            self.x = self.din("x", [NT, D])
            self.ident_d = self.din("ident", [128, 128], BF16)
            self.pre0 = self.din("pre0", [128, 8])
            self.w_in_ab = self.din("w_in_ab", [D, 4 * BR])
            for nm in ("s5_lr", "s5_li", "s5_ldt"):
                setattr(self, nm, self.din(nm, [64, 32]))
            for nm in ("s5_br", "s5_bi", "s5_cr", "s5_ci"):
                setattr(self, nm, self.din(nm, [64, 32, 16]))
            self.tv_d = self.din("tv", [64, 16])
            self.kv_d = self.din("kv", [64, 256])
            self.toepmask_d = self.din("toepmask", [128, 128])
            self.identf_d = self.din("identf", [128, 128])
            self.s5d_rep = self.din("s5d_rep", [128, BR])
            self.glub_rep = self.din("glub_rep", [128, BR])
            self.glu_w = self.din("glu_w", [BR, BR])
            self.ml_cw = self.din("ml_cw", [128, 4, 4]); self.ml_cb = self.din("ml_cb", [128, 4])
            self.ml_mn = self.din("ml_mn", [128, 4]); self.ml_msk = self.din("ml_msk", [128, 4])
            self.ml_gbi = self.din("ml_gbi", [4, 1]); self.ml_gbf = self.din("ml_gbf", [4, 1])
            self.ml_maskS = self.din("ml_maskS", [128, 128])
            self.ml_gw = self.din("ml_gw", [128, 12, 8])
            self.ml_wq = self.din("ml_wq", [128, 4, 128]); self.ml_wk = self.din("ml_wk", [128, 4, 128]); self.ml_wv = self.din("ml_wv", [128, 4, 128])
            self.bT = self.dscr("bT", [BR, NT], BF16)
            self.w_out_ab = self.din("w_out_ab", [D, D]); self.post0_rep = self.din("post0_rep", [128, D])
            self.out = self.nc.dram_tensor("out", [NT, D], F32, kind="ExternalOutput").ap()
            self.outputs["out"] = self.out
            self.h1src = self.out
            if self.phases is not None and "l0o" not in self.phases:
                self.h1src = self.din("h1_in", [NT, D])
            self.pre1 = self.din("pre1", [128, 8]); self.w_in_cd = self.din("w_in_cd", [D, 8 * BR])
            self.rope_cos = self.din("rope_cos", [128, self.S]); self.rope_sin = self.din("rope_sin", [128, self.S])
            self.rope_rm = self.din("rope_rm", [128, 128], BF16)
            for nm in ("cqT", "ckT", "dqT", "dkT", "czT", "dzT"):
                setattr(self, nm, self.dscr(nm, [BR, NT], BF16))
            for nm in ("cv_tok", "dv_tok", "oc_tok", "od_tok"):
                setattr(self, nm, self.dscr(nm, [NT, BR], BF16))
            self.dil_masks = self.din("dil_masks", [128, 9, 512], BF16); self.diff_masks = self.din("diff_masks", [128, 4, 512], BF16)
            self.diff_lqk = self.din("diff_lqk", [1, 4, 64]); self.diffnorm_rep = self.din("diffnorm_rep", [128, 128])
            self.w_out_cd = self.din("w_out_cd", [D, D]); self.post1_rep = self.din("post1_rep", [128, D])
            self.rotc = self.dscr("rotc", [64, 32, 256], F32)
            self.rots = self.dscr("rots", [64, 32, 256], F32)
            self.rhot = self.dscr("rhot", [64, 32, 256], F32)
            self.toep_x = self.dscr("toep_x", [128, 32, 128], BF16)
            self.wii_x = self.dscr("wii_x", [128, 32, 2, 64], BF16)
            self.wiv_x = self.dscr("wiv_x", [64, 2, 32, 128], BF16)
            self.a_tok = self.dscr("a_tok", [NT, BR], BF16)
            self.u_tok = self.dscr("u_tok", [NT, BR], BF16)
            self.sz_tok = self.dscr("sz_tok", [NT, BR], BF16)
            self.xmT = self.dscr("xmT", [BR, NT], BF16)
            self.mzT = self.dscr("mzT", [BR, NT], BF16)
            self.ident = st.enter_context(nc.sbuf_tensor("identb", [128, 128], BF16))
            self.c.dma(self.ident[:], self.ident_d, w=["ident"])
            ph = self.phases
            fin = []
            if ph is None or "l0p" in ph:
                self.phase_l0_proj()
                fin += ["u_tok", "sz_tok", "xmT", "mzT"]
            if ph is None or "s5" in ph:
                self.phase_s5()
                fin += ["a_tok", "rotc", "rots", "rhot", "toep_x", "wii_x", "wiv_x"]
            if ph is None or "ml" in ph:
                self.phase_ml()
                fin += ["bT"]
            if ph is None or "l0o" in ph:
                self.phase_l0_out()
                fin += ["h1_%d" % b for b in range(NT // 128)]
            if ph is None or "l1p" in ph:
                self.phase_l1_proj()
                fin += ["cqT", "ckT", "dqT", "dkT", "czT", "dzT", "cv_tok", "dv_tok"]
            if ph is None or "adil" in ph:
                self.phase_attn("dil")
                fin += ["oc_tok"]
            if ph is None or "adiff" in ph:
                self.phase_attn("diff")
                fin += ["od_tok"]
            if ph is None or "l1o" in ph:
                self.phase_l1_out()
                fin += ["h1_%d" % b for b in range(NT // 128)]
            self.c.finish(fin)
            self.c.barrier()
        return nc

    def rmsnorm_T(self, st, xsrc_rows, nblk, zT, zkey, tagp, ps_tr):
        raise NotImplementedError

    def phase_l0_proj(self):
        nc, c = self.nc, self.c
        NT = self.NT
        ngrp = NT // 512
        with self.scope() as st:
            T = lambda name, shape, dt: st.enter_context(self.sbt(name, shape, dt))
            win = T("win0", [128, 8, 4 * BR], BF16)
            g0 = T("g0", [128, 8], F32)
            stage = [T("wst%d" % i, [128, 4 * BR], F32) for i in range(2)]
            c.dma(g0[:], self.pre0, w=["g0"])
            for dc in range(8):
                sk = "wst%d" % (dc % 2)
                c.dma(stage[dc % 2][:], self.w_in_ab[dc * 128:(dc + 1) * 128, :], w=[sk])
                c.op("act", lambda e: e.activation(out=win[:, dc, :], in_=stage[dc % 2][:], func=AF.Copy,
                                                   scale=g0[:, dc:dc + 1]), r=[sk, "g0"], w=["win0"])
            NXB = 6
            xt = [T("xt%d" % i, [128, D], F32) for i in range(NXB)]
            junk = T("junk", [128, D], F32)
            ss2 = [T("ss%d" % i, [128, 4], F32) for i in range(2)]
            rstd2 = [T("rstd%d" % i, [128, 4], F32) for i in range(2)]
            zb = [T("zb%d" % i, [128, D], BF16) for i in range(2)]
            zT = [T("zT%d" % i, [128, 8, 512], BF16) for i in range(2)]
            uo = [T("uo%d" % i, [128, BR], BF16) for i in range(2)]
            so = [T("so%d" % i, [128, BR], BF16) for i in range(2)]
            xmo = [T("xmo%d" % i, [128, 4, 512], BF16) for i in range(2)]
            mzo = [T("mzo%d" % i, [128, 4, 512], BF16) for i in range(2)]
            nblk = NT // 128

            def load_x(b):
                if b < nblk:
                    c.dma(xt[b % NXB][:], self.x[b * 128:(b + 1) * 128, :], w=["xt%d" % (b % NXB)])
            for b in range(4):
                load_x(b)
            for g in range(ngrp):
                zk = "zT%d" % (g % 2)
                ss = ss2[g % 2]; rstd = rstd2[g % 2]
                ssk = "ss%d" % (g % 2); rsk = "rstd%d" % (g % 2)
                for j in range(4):
                    b = g * 4 + j
                    xk = "xt%d" % (b % NXB)
                    c.op("act", lambda e: e.activation(out=junk[:], in_=xt[b % NXB][:], func=AF.Square,
                                                       accum_out=ss[:, j:j + 1]), r=[xk], w=["junk", ssk])
                c.op("act", lambda e: e.activation(out=rstd[:], in_=ss[:], func=AF.Ln, scale=1.0 / D, bias=NORM_EPS),
                     r=[ssk], w=[rsk])
                c.op("act", lambda e: e.activation(out=rstd[:], in_=rstd[:], func=AF.Exp, scale=-0.5),
                     r=[rsk], w=[rsk])
                for j in range(4):
                    b = g * 4 + j
                    xk = "xt%d" % (b % NXB)
                    zbk = "zb%d" % (b % 2)
                    pst = self.ps[b % 2]
                    pk = "ps%d" % (b % 2)
                    c.op("dve", lambda e: e.tensor_scalar(out=zb[b % 2][:], in0=xt[b % NXB][:], scalar1=rstd[:, j:j + 1],
                                                          scalar2=None, op0=ALU.mult), r=[xk, rsk], w=[zbk])
                    load_x(b + 4)
                    ptv = pst[:].bitcast(BF16).rearrange("p (a b) -> p a b", a=8)
                    for dc in range(8):
                        c.op("pe", lambda e: e.transpose(out=ptv[:, dc, :], in_=zb[b % 2][:, dc * 128:(dc + 1) * 128],
                                                         identity=self.ident[:]), r=[zbk, "ident"], w=[pk])
                    c.op("dve", lambda e: e.tensor_copy(out=zT[g % 2][:, :, j * 128:(j + 1) * 128], in_=ptv),
                         r=[pk], w=[zk])
                    for dc in range(8):
                        c.op("pe", lambda e: e.matmul(out=self.ps[2][:], lhsT=zT[g % 2][:, dc, j * 128:(j + 1) * 128],
                                                      rhs=win[:, dc, 0:BR], start=(dc == 0), stop=(dc == 7)),
                             r=[zk, "win0"], w=["ps2"])
                    c.op("act", lambda e: e.copy(out=uo[b % 2][:], in_=self.ps[2][:]), r=["ps2"], w=["uo%d" % (b % 2)])
                    c.dma(self.u_tok[b * 128:(b + 1) * 128, :], uo[b % 2][:], r=["uo%d" % (b % 2)], w=["u_tok"])
                    for dc in range(8):
                        c.op("pe", lambda e: e.matmul(out=self.ps[3][:], lhsT=zT[g % 2][:, dc, j * 128:(j + 1) * 128],
                                                      rhs=win[:, dc, BR:2 * BR], start=(dc == 0), stop=(dc == 7)),
                             r=[zk, "win0"], w=["ps3"])
                    c.op("act", lambda e: e.activation(out=so[b % 2][:], in_=self.ps[3][:], func=AF.Silu),
                         r=["ps3"], w=["so%d" % (b % 2)])
                    c.dma(self.sz_tok[b * 128:(b + 1) * 128, :], so[b % 2][:], r=["so%d" % (b % 2)], w=["sz_tok"])
                for fc in range(8):
                    pb = 4 + fc % 4
                    for dc in range(8):
                        c.op("pe", lambda e: e.matmul(out=self.ps[pb][:], lhsT=win[:, dc, 2 * BR + fc * 128:2 * BR + (fc + 1) * 128],
                                                      rhs=zT[g % 2][:, dc, :], start=(dc == 0), stop=(dc == 7)),
                             r=[zk, "win0"], w=["ps%d" % pb])
                    if fc < 4:
                        c.op("dve", lambda e: e.tensor_copy(out=xmo[g % 2][:, fc, :], in_=self.ps[pb][:]),
                             r=["ps%d" % pb], w=["xmo%d" % (g % 2)])
                    else:
                        c.op("act", lambda e: e.activation(out=mzo[g % 2][:, fc - 4, :], in_=self.ps[pb][:], func=AF.Silu),
                             r=["ps%d" % pb], w=["mzo%d" % (g % 2)])
                c.dma(self.xmT.rearrange("(c p) t -> p c t", p=128)[:, :, g * 512:(g + 1) * 512], xmo[g % 2][:],
                      r=["xmo%d" % (g % 2)], w=["xmT"])
                c.dma(self.mzT.rearrange("(c p) t -> p c t", p=128)[:, :, g * 512:(g + 1) * 512], mzo[g % 2][:],
                      r=["mzo%d" % (g % 2)], w=["mzT"])
        c.barrier()

    def tt(self, e, out, a, b, op, r, w):
        return self.c.op(e, lambda en: en.tensor_tensor(out=out, in0=a, in1=b, op=op), r=r, w=w)

    def ts(self, e, out, a, s1, op0, r, w, s2=None, op1=None):
        if op1 is None:
            return self.c.op(e, lambda en: en.tensor_scalar(out=out, in0=a, scalar1=s1, scalar2=None, op0=op0), r=r, w=w)
        return self.c.op(e, lambda en: en.tensor_scalar(out=out, in0=a, scalar1=s1, scalar2=s2, op0=op0, op1=op1), r=r, w=w)

    def stt(self, e, out, a, s, b, op0, op1, r, w):
        return self.c.op(e, lambda en: en.scalar_tensor_tensor(out=out, in0=a, scalar=s, in1=b, op0=op0, op1=op1), r=r, w=w)

    def actf(self, out, in_, func, r, w, **kw):
        return self.c.op("act", lambda en: en.activation(out=out, in_=in_, func=func, **kw), r=r, w=w)

    def cp(self, e, out, in_, r, w):
        if e == "act":
            return self.c.op("act", lambda en: en.copy(out=out, in_=in_), r=r, w=w)
        return self.c.op(e, lambda en: en.tensor_copy(out=out, in_=in_), r=r, w=w)

    def sincos(self, ang, akey, sin_o, cos_o, skey, ckey, tf, ti, red_o=None):
        C1 = 6.28125
        C2 = 2 * math.pi - C1
        for (off, out, okey) in ((0.0, sin_o, skey), (math.pi / 2, cos_o, ckey)):
            if out is None:
                continue
            self.ts("dve", tf, ang, 1.0 / (2 * math.pi), ALU.mult, r=[akey], w=["sc_tf"], s2=off / (2 * math.pi), op1=ALU.add)
            self.cp("dve", ti, tf, r=["sc_tf"], w=["sc_ti"])
            self.cp("dve", tf, ti, r=["sc_ti"], w=["sc_tf"])
            self.stt("dve", out, tf, -C1, ang, ALU.mult, ALU.add, r=["sc_tf", akey], w=[okey])
            self.stt("dve", out, tf, -C2, out, ALU.mult, ALU.add, r=["sc_tf", okey], w=[okey])
            if off != 0.0:
                self.ts("dve", out, out, off, ALU.add, r=[okey], w=[okey])
            self.ts("dve", out, out, math.pi, ALU.min, r=[okey], w=[okey], s2=-math.pi, op1=ALU.max)
            if red_o is not None and off == 0.0:
                self.cp("dve", red_o[0], out, r=[okey], w=[red_o[1]])
            self.actf(out, out, AF.Sin, r=[okey], w=[okey])

    def s5_precompute(self, st, Toep, Wii, WivR, WivI):
        nc, c = self.nc, self.c
        so = contextlib.ExitStack()
        TO = lambda name, shape, dt=F32: so.enter_context(self.sbt(name, shape, dt))
        thr = TO("p_thr", [64, 32]); rho8 = TO("p_rho8", [64, 32]); kv = TO("p_kv", [64, 256])
        tfs = TO("p_tfs", [64, 32]); tis = TO("p_tis", [64, 32], I32)
        with self.scope() as sp:
            T = lambda name, shape, dt=F32: sp.enter_context(self.sbt(name, shape, dt))
            lr = T("p_lr", [64, 32]); li = T("p_li", [64, 32]); ldt = T("p_ldt", [64, 32])
            br = T("p_br", [64, 32, 16]); bi = T("p_bi", [64, 32, 16])
            cr = T("p_cr", [64, 32, 16]); ci = T("p_ci", [64, 32, 16])
            tv = T("p_tv", [64, 16])
            msk = T("p_msk", [128, 128]); idf = T("p_idf", [128, 128])
            for (t, d, key) in ((lr, self.s5_lr, "p_lr"), (li, self.s5_li, "p_li"), (ldt, self.s5_ldt, "p_ldt"),
                                (br, self.s5_br, "p_br"), (bi, self.s5_bi, "p_bi"), (cr, self.s5_cr, "p_cr"),
                                (ci, self.s5_ci, "p_ci"), (tv, self.tv_d, "p_tv"), (kv, self.kv_d, "p_kv"),
                                (msk, self.toepmask_d, "p_msk"), (idf, self.identf_d, "p_idf")):
                c.dma(t[:], d, w=[key])
            dt = T("p_dt", [64, 32]); lrdt = T("p_lrdt", [64, 32]); th = T("p_th", [64, 32])
            s0 = T("p_s0", [64, 32]); c0 = T("p_c0", [64, 32]); mag = T("p_mag", [64, 32])
            self.actf(dt[:], ldt[:], AF.Exp, r=["p_ldt"], w=["p_dt"])
            self.tt("dve", lrdt[:], lr[:], dt[:], ALU.mult, r=["p_lr", "p_dt"], w=["p_lrdt"])
            self.tt("dve", th[:], li[:], dt[:], ALU.mult, r=["p_li", "p_dt"], w=["p_th"])
            self.sincos(th[:], "p_th", s0[:], c0[:], "p_s0", "p_c0", tfs[:], tis[:], red_o=(thr[:], "p_thr"))
            self.actf(mag[:], lrdt[:], AF.Exp, r=["p_lrdt"], w=["p_mag"])
            abr = T("p_abr", [64, 32]); abi = T("p_abi", [64, 32]); am1 = T("p_am1", [64, 32])
            self.tt("dve", abr[:], mag[:], c0[:], ALU.mult, r=["p_mag", "p_c0"], w=["p_abr"])
            self.tt("dve", abi[:], mag[:], s0[:], ALU.mult, r=["p_mag", "p_s0"], w=["p_abi"])
            self.ts("dve", am1[:], abr[:], -1.0, ALU.add, r=["p_abr"], w=["p_am1"])
            den = T("p_den", [64, 32]); t1 = T("p_t1", [64, 32]); t2 = T("p_t2", [64, 32])
            fr = T("p_fr", [64, 32]); fi = T("p_fi", [64, 32])
            self.tt("dve", den[:], lr[:], lr[:], ALU.mult, r=["p_lr"], w=["p_den"])
            self.tt("dve", t1[:], li[:], li[:], ALU.mult, r=["p_li"], w=["p_t1"])
            self.tt("dve", den[:], den[:], t1[:], ALU.add, r=["p_den", "p_t1"], w=["p_den"])
            c.op("dve", lambda e: e.reciprocal(out=den[:], in_=den[:]), r=["p_den"], w=["p_den"])
            self.tt("dve", t1[:], am1[:], lr[:], ALU.mult, r=["p_am1", "p_lr"], w=["p_t1"])
            self.tt("dve", t2[:], abi[:], li[:], ALU.mult, r=["p_abi", "p_li"], w=["p_t2"])
            self.tt("dve", t1[:], t1[:], t2[:], ALU.add, r=["p_t1", "p_t2"], w=["p_t1"])
            self.tt("dve", fr[:], t1[:], den[:], ALU.mult, r=["p_t1", "p_den"], w=["p_fr"])
            self.tt("dve", t1[:], abi[:], lr[:], ALU.mult, r=["p_abi", "p_lr"], w=["p_t1"])
            self.tt("dve", t2[:], am1[:], li[:], ALU.mult, r=["p_am1", "p_li"], w=["p_t2"])
            self.tt("dve", t1[:], t1[:], t2[:], ALU.subtract, r=["p_t1", "p_t2"], w=["p_t1"])
            self.tt("dve", fi[:], t1[:], den[:], ALU.mult, r=["p_t1", "p_den"], w=["p_fi"])
            Bbr = T("p_Bbr", [64, 32, 16]); Bbi = T("p_Bbi", [64, 32, 16])
            u1 = T("p_u1", [64, 32, 16]); u2 = T("p_u2", [64, 32, 16])
            bc16 = lambda a: a.unsqueeze(2).broadcast_to([64, 32, 16])
            self.tt("dve", u1[:], br[:], bc16(fr[:]), ALU.mult, r=["p_br", "p_fr"], w=["p_u1"])
            self.tt("dve", u2[:], bi[:], bc16(fi[:]), ALU.mult, r=["p_bi", "p_fi"], w=["p_u2"])
            self.tt("dve", Bbr[:], u1[:], u2[:], ALU.subtract, r=["p_u1", "p_u2"], w=["p_Bbr"])
            self.tt("dve", u1[:], bi[:], bc16(fr[:]), ALU.mult, r=["p_bi", "p_fr"], w=["p_u1"])
            self.tt("dve", u2[:], br[:], bc16(fi[:]), ALU.mult, r=["p_br", "p_fi"], w=["p_u2"])
            self.tt("dve", Bbi[:], u1[:], u2[:], ALU.add, r=["p_u1", "p_u2"], w=["p_Bbi"])
            TE = T("p_TE", [64, 16, 32]); TA = T("p_TA", [64, 16, 32])
            PWr = T("p_PWr", [64, 16, 32]); PWi = T("p_PWi", [64, 16, 32])
            tf3 = T("p_tf3", [64, 16, 32]); ti3 = T("p_ti3", [64, 16, 32], I32)
            bt = lambda a: a.unsqueeze(1).broadcast_to([64, 16, 32])
            bg = lambda a: a.unsqueeze(2).broadcast_to([64, 16, 32])
            self.tt("dve", TE[:], bt(lrdt[:]), bg(tv[:]), ALU.mult, r=["p_lrdt", "p_tv"], w=["p_TE"])
            self.actf(TE[:], TE[:], AF.Exp, r=["p_TE"], w=["p_TE"])
            self.tt("dve", TA[:], bt(thr[:]), bg(tv[:]), ALU.mult, r=["p_thr", "p_tv"], w=["p_TA"])
            self.sincos(TA[:], "p_TA", PWi[:], PWr[:], "p_PWi", "p_PWr", tf3[:], ti3[:])
            self.tt("dve", PWr[:], PWr[:], TE[:], ALU.mult, r=["p_PWr", "p_TE"], w=["p_PWr"])
            self.tt("dve", PWi[:], PWi[:], TE[:], ALU.mult, r=["p_PWi", "p_TE"], w=["p_PWi"])
            HsR = T("p_HsR", [64, 32, 8, 16]); HsI = T("p_HsI", [64, 32, 8, 16])
            v1 = T("p_v1", [64, 32, 9, 16]); v2 = T("p_v2", [64, 32, 9, 16])
            def pw_b(PW, j0, n):
                return PW[:, j0:j0 + n, :].rearrange("p t g -> p g t").unsqueeze(3).broadcast_to([64, 32, n, 16])
            def x_b(x, n):
                return x.unsqueeze(2).broadcast_to([64, 32, n, 16])
            self.tt("dve", v1[:, :, 0:8, :], pw_b(PWr, 0, 8), x_b(Bbr[:], 8), ALU.mult, r=["p_PWr", "p_Bbr"], w=["p_v1"])
            self.tt("dve", v2[:, :, 0:8, :], pw_b(PWi, 0, 8), x_b(Bbi[:], 8), ALU.mult, r=["p_PWi", "p_Bbi"], w=["p_v2"])
            self.tt("dve", HsR[:], v1[:, :, 0:8, :], v2[:, :, 0:8, :], ALU.subtract, r=["p_v1", "p_v2"], w=["p_HsR"])
            self.tt("dve", v1[:, :, 0:8, :], pw_b(PWr, 0, 8), x_b(Bbi[:], 8), ALU.mult, r=["p_PWr", "p_Bbi"], w=["p_v1"])
            self.tt("dve", v2[:, :, 0:8, :], pw_b(PWi, 0, 8), x_b(Bbr[:], 8), ALU.mult, r=["p_PWi", "p_Bbr"], w=["p_v2"])
            self.tt("dve", HsI[:], v1[:, :, 0:8, :], v2[:, :, 0:8, :], ALU.add, r=["p_v1", "p_v2"], w=["p_HsI"])
            LtR = T("p_LtR", [64, 32, 9, 16]); nLtI = T("p_nLtI", [64, 32, 9, 16])
            for (s0_, j0, n) in ((0, 0, 1), (1, 8, 8)):
                sl = slice(s0_, s0_ + n)
                self.tt("dve", v1[:, :, sl, :], pw_b(PWr, j0, n), x_b(cr[:], n), ALU.mult, r=["p_PWr", "p_cr"], w=["p_v1"])
                self.tt("dve", v2[:, :, sl, :], pw_b(PWi, j0, n), x_b(ci[:], n), ALU.mult, r=["p_PWi", "p_ci"], w=["p_v2"])
                self.tt("dve", LtR[:, :, sl, :], v1[:, :, sl, :], v2[:, :, sl, :], ALU.subtract, r=["p_v1", "p_v2"], w=["p_LtR"])
                self.tt("dve", v1[:, :, sl, :], pw_b(PWi, j0, n), x_b(cr[:], n), ALU.mult, r=["p_PWi", "p_cr"], w=["p_v1"])
                self.tt("dve", v2[:, :, sl, :], pw_b(PWr, j0, n), x_b(ci[:], n), ALU.mult, r=["p_PWr", "p_ci"], w=["p_v2"])
                self.tt("dve", v1[:, :, sl, :], v1[:, :, sl, :], v2[:, :, sl, :], ALU.add, r=["p_v1", "p_v2"], w=["p_v1"])
                self.ts("dve", nLtI[:, :, sl, :], v1[:, :, sl, :], -1.0, ALU.mult, r=["p_v1"], w=["p_nLtI"])
            self.cp("dve", WivR[:], LtR[:, :, 1:9, :].rearrange("p g t c -> p g (t c)"), r=["p_LtR"], w=["WivR"])
            self.cp("dve", WivI[:], nLtI[:, :, 1:9, :].rearrange("p g t c -> p g (t c)"), r=["p_nLtI"], w=["WivI"])
            for g4 in range(8):
                pb = g4 % 2
                pk = "ps%d" % pb
                for gl in range(4):
                    g = g4 * 4 + gl
                    o = self.ps[pb][:, gl * 128:(gl + 1) * 128]
                    c.op("pe", lambda e: e.matmul(out=o, lhsT=HsR[:, g, :, :].rearrange("p s c -> p (s c)"),
                                                  rhs=LtR[:, g, 0:8, :].rearrange("p t c -> p (t c)"), start=True, stop=False),
                         r=["p_HsR", "p_LtR"], w=[pk])
                    c.op("pe", lambda e: e.matmul(out=o, lhsT=HsI[:, g, :, :].rearrange("p s c -> p (s c)"),
                                                  rhs=nLtI[:, g, 0:8, :].rearrange("p t c -> p (t c)"), start=False, stop=True),
                         r=["p_HsI", "p_nLtI"], w=[pk])
                self.tt("dve", Toep[:, g4 * 4:(g4 + 1) * 4, :], self.ps[pb][:].rearrange("p (g n) -> p g n", g=4),
                        msk[:].unsqueeze(1).broadcast_to([128, 4, 128]), ALU.mult, r=[pk, "p_msk"], w=["Toep"])
            GsR = LtR[:, :, 0:8, :].rearrange("p g t c -> p g (t c)")
            GsI = nLtI[:, :, 0:8, :].rearrange("p g t c -> p g (t c)")
            w1 = v1[:, :, 0:8, :].rearrange("p g t c -> p g (t c)")
            w2 = v2[:, :, 0:8, :].rearrange("p g t c -> p g (t c)")
            p7r = PWr[:, 14, :].unsqueeze(2).broadcast_to([64, 32, 128])
            p7i = PWi[:, 14, :].unsqueeze(2).broadcast_to([64, 32, 128])
            hr = HsR[:].rearrange("p g s c -> p g (s c)"); hi = HsI[:].rearrange("p g s c -> p g (s c)")
            self.tt("dve", w1, hr, p7r, ALU.mult, r=["p_HsR", "p_PWr"], w=["p_v1"])
            self.tt("dve", w2, hi, p7i, ALU.mult, r=["p_HsI", "p_PWi"], w=["p_v2"])
            self.tt("dve", GsR, w1, w2, ALU.subtract, r=["p_v1", "p_v2"], w=["p_LtR"])
            self.tt("dve", w1, hi, p7r, ALU.mult, r=["p_HsI", "p_PWr"], w=["p_v1"])
            self.tt("dve", w2, hr, p7i, ALU.mult, r=["p_HsR", "p_PWi"], w=["p_v2"])
            self.tt("dve", GsI, w1, w2, ALU.add, r=["p_v1", "p_v2"], w=["p_nLtI"])
            for g4 in range(8):
                pb = 2 + g4 % 2
                pk = "ps%d" % pb
                pv = self.ps[pb][:].rearrange("p (g r n) -> p g r n", g=4, r=2)
                for gl in range(4):
                    g = g4 * 4 + gl
                    c.op("pe", lambda e: e.transpose(out=pv[:, gl, 0, :], in_=GsR[:, g, :], identity=idf[0:64, 0:64]),
                         r=["p_LtR", "p_idf"], w=[pk])
                    c.op("pe", lambda e: e.transpose(out=pv[:, gl, 1, :], in_=GsI[:, g, :], identity=idf[0:64, 0:64]),
                         r=["p_nLtI", "p_idf"], w=[pk])
                self.cp("act", Wii[:, g4 * 4:(g4 + 1) * 4, :, :], pv, r=[pk], w=["Wii"])
            self.cp("dve", rho8[:], TE[:, 15, :], r=["p_TE"], w=["p_rho8"])
            if "toep_x" in self.export:
                c.dma(self.toep_x, Toep[:], r=["Toep"], w=["toep_x"])
                c.dma(self.wii_x, Wii[:], r=["Wii"], w=["wii_x"])
                c.dma(self.wiv_x[:, 0], WivR[:], r=["WivR"], w=["wiv_x"])
                c.dma(self.wiv_x[:, 1], WivI[:], r=["WivI"], w=["wiv_x"])
        c.barrier()
        with self.scope() as sp:
            T = lambda name, shape, dt=F32: sp.enter_context(self.sbt(name, shape, dt))
            phr = T("p_phr", [64, 32]); ph_s = T("p_phs", [64, 32]); phr2 = T("p_phr2", [64, 32])
            self.ts("dve", phr[:], thr[:], 8.0, ALU.mult, r=["p_thr"], w=["p_phr"])
            self.sincos(phr[:], "p_phr", ph_s[:], None, "p_phs", None, tfs[:], tis[:], red_o=(phr2[:], "p_phr2"))
            rho = T("p_rho", [64, 8, 256])
            ang = T("p_ang", [64, 8, 256]); sk = T("p_sk", [64, 8, 256]); ck = T("p_ck", [64, 8, 256])
            tf4 = T("p_tf4", [64, 8, 256]); ti4 = T("p_ti4", [64, 8, 256], I32)
            for gb in range(4):
                gs = slice(gb * 8, (gb + 1) * 8)
                self.cp("dve", rho[:], rho8[:, gs].unsqueeze(2).broadcast_to([64, 8, 256]), r=["p_rho8"], w=["p_rho"])
                c.op("dve", lambda e: e.memset(rho[:, :, 0:1], 0.0), r=[], w=["p_rho"])
                c.dma(self.rhot[:, gs, :], rho[:], r=["p_rho"], w=["rhot"])
                self.tt("dve", ang[:], phr2[:, gs].unsqueeze(2).broadcast_to([64, 8, 256]),
                        kv[:].unsqueeze(1).broadcast_to([64, 8, 256]), ALU.mult, r=["p_phr2", "p_kv"], w=["p_ang"])
                self.sincos(ang[:], "p_ang", sk[:], ck[:], "p_sk", "p_ck", tf4[:], ti4[:])
                c.dma(self.rots[:, gs, :], sk[:], r=["p_sk"], w=["rots"])
                c.dma(self.rotc[:, gs, :], ck[:], r=["p_ck"], w=["rotc"])
        c.barrier()
        so.close()

    def phase_s5(self):
        nc, c = self.nc, self.c
        with self.scope() as st:
            T = lambda name, shape, dt=F32: st.enter_context(self.sbt(name, shape, dt))
            Toep = T("Toep", [128, 32, 128], BF16)
            Wii = T("Wii", [128, 32, 2, 64], BF16)
            WivR = T("WivR", [64, 32, 128], BF16)
            WivI = T("WivI", [64, 32, 128], BF16)
            self.s5_precompute(st, Toep, Wii, WivR, WivI)
            if self.s5_main_enabled:
                self.s5_main(st, Toep, Wii, WivR, WivI)
        c.barrier()

    def s5_main(self, st0, Toep, Wii, WivR, WivI):
        nc, c = self.nc, self.c
        GB = 4
        with self.scope() as st:
            T = lambda name, shape, dt=F32: st.enter_context(self.sbt(name, shape, dt))
            gluw = T("gluw", [128, 4, BR], BF16)
            gst = T("gluw_st", [128, 4, BR], F32)
            c.dma(gst[:], self.glu_w.rearrange("(c p) n -> p c n", p=128), w=["gluw_st"])
            self.cp("dve", gluw[:], gst[:], r=["gluw_st"], w=["gluw"])
            Drep = T("Drep", [128, BR]); Brep = T("Brep", [128, BR])
            c.dma(Drep[:], self.s5d_rep, w=["Drep"])
            c.dma(Brep[:], self.glub_rep, w=["Brep"])
            for q in range(self.nseq):
                tb = q * self.S
                with self.scope() as sq:
                    TQ = lambda name, shape, dt=F32: sq.enter_context(self.sbt(name, shape, dt))
                    uck = [TQ("uck%d" % i, [128, 8 * BR], BF16) for i in range(2)]
                    szck = [TQ("szck%d" % i, [128, 8 * BR], BF16) for i in range(2)]
                    yck = [TQ("yck%d" % i, [128, 8, BR], BF16) for i in range(2)]
                    for kh in range(2):
                        rows = slice(tb + kh * 1024, tb + (kh + 1) * 1024)
                        c.dma(uck[kh][:], self.u_tok[rows, :].rearrange("(k t) f -> k (t f)", t=8), r=["u_tok"], w=["uck%d" % kh])
                        c.dma(szck[kh][:], self.sz_tok[rows, :].rearrange("(k t) f -> k (t f)", t=8), r=["sz_tok"], w=["szck%d" % kh])
                    with self.scope() as ss:
                        TS = lambda name, shape, dt=F32: ss.enter_context(self.sbt(name, shape, dt))
                        Ug = TS("Ug", [128, 32, 256], BF16)
                        Vb2 = [TS("Vb%d" % i, [64, 2, GB, 256]) for i in range(2)]
                        Wb2 = [TS("Wb%d" % i, [64, 2, GB, 256]) for i in range(2)]
                        tA2 = [TS("tA%d" % i, [64, GB, 256]) for i in range(2)]; tB2 = [TS("tB%d" % i, [64, GB, 256]) for i in range(2)]
                        ck2 = [TS("ck%d" % i, [64, GB, 256]) for i in range(2)]; sk2 = [TS("sk%d" % i, [64, GB, 256]) for i in range(2)]
                        rh2 = [TS("rh%d" % i, [64, GB, 256]) for i in range(2)]
                        Xs2 = [TS("Xs%d" % i, [64, 2, GB, 256], BF16) for i in range(2)]
                        Ysb = TS("Ysb", [128, 8, 256], BF16)
                        for i in range(2):
                            c.op("pool", lambda e: e.memset(Xs2[i][:], 0.0), w=["Xs%d" % i])

                        def load_tabs(b):
                            if b < 32 // GB:
                                gs_ = slice(b * GB, (b + 1) * GB)
                                c.dma(ck2[b % 2][:], self.rotc[:, gs_, :], r=["rotc"], w=["ck%d" % (b % 2)])
                                c.dma(sk2[b % 2][:], self.rots[:, gs_, :], r=["rots"], w=["sk%d" % (b % 2)])
                                c.dma(rh2[b % 2][:], self.rhot[:, gs_, :], r=["rhot"], w=["rh%d" % (b % 2)])
                        load_tabs(0)
                        ucg = TS("ucg", [128, 32, 128], BF16)
                        for kh in range(2):
                            self.cp("pool" if kh == 0 else "dve", ucg[:].rearrange("p g (s c) -> p g s c", s=8),
                                    uck[kh][:].rearrange("p (s g c) -> p g s c", s=8, g=32), r=["uck%d" % kh], w=["ucg"])
                            for g8 in range(4):
                                pb = g8 % 2
                                pk = "ps%d" % pb
                                ptv = self.ps[pb][:].bitcast(BF16).rearrange("p (g k) -> p g k", g=8)
                                for gl in range(8):
                                    g = g8 * 8 + gl
                                    c.op("pe", lambda e: e.transpose(out=ptv[:, gl, :], in_=ucg[:, g, :],
                                                                     identity=self.ident[:]), r=["ucg", "ident"], w=[pk])
                                self.cp("dve" if g8 % 2 == 0 else "act", Ug[:, g8 * 8:(g8 + 1) * 8, kh * 128:(kh + 1) * 128], ptv,
                                        r=[pk], w=["Ug"])
                        for b in range(32 // GB):
                            gs = slice(b * GB, (b + 1) * GB)
                            bp = b % 2
                            Vb, Wb, tA, tB, ck, sk, rh, Xs = Vb2[bp], Wb2[bp], tA2[bp], tB2[bp], ck2[bp], sk2[bp], rh2[bp], Xs2[bp]
                            kVb, kW0, kW1, ktA, ktB, kck, ksk, krh, kXs = ("Vb%d" % bp, "Wb0_%d" % bp, "Wb1_%d" % bp, "tA%d" % bp, "tB%d" % bp,
                                                                           "ck%d" % bp, "sk%d" % bp, "rh%d" % bp, "Xs%d" % bp)
                            load_tabs(b + 1)
                            for gl in range(GB):
                                g = b * GB + gl
                                pb = 2 + gl % 2
                                pk = "ps%d" % pb
                                c.op("pe", lambda e: e.matmul(out=self.ps[pb][0:64, 0:256], lhsT=Wii[:, g, 0, :], rhs=Ug[:, g, :],
                                                              start=True, stop=True), r=["Wii", "Ug"], w=[pk])
                                c.op("pe", lambda e: e.matmul(out=self.ps[pb][0:64, 256:512], lhsT=Wii[:, g, 1, :], rhs=Ug[:, g, :],
                                                              start=True, stop=True), r=["Wii", "Ug"], w=[pk])
                                self.cp("act", Vb[:, :, gl, :], self.ps[pb][0:64, :].rearrange("p (r k) -> p r k", r=2), r=[pk], w=[kVb])
                            self.tt("dve", tA[:], ck[:], Vb[:, 0], ALU.mult, r=[kck, kVb], w=[ktA])
                            self.tt("pool", tB[:], sk[:], Vb[:, 1], ALU.mult, r=[ksk, kVb], w=[ktB])
                            self.tt("dve", Wb[:, 0], tA[:], tB[:], ALU.add, r=[ktA, ktB], w=[kW0])
                            self.tt("pool", tA[:], ck[:], Vb[:, 1], ALU.mult, r=[kck, kVb], w=[ktA])
                            self.tt("dve", tB[:], sk[:], Vb[:, 0], ALU.mult, r=[ksk, kVb], w=[ktB])
                            self.tt("pool", Wb[:, 1], tA[:], tB[:], ALU.subtract, r=[ktA, ktB], w=[kW1])
                            fl = lambda a: a.rearrange("p g k -> p (g k)")
                            c.op("dve", lambda e: e.tensor_tensor_scan(out=fl(Vb[:, 0]), data0=fl(rh[:]), data1=fl(Wb[:, 0]), initial=0.0,
                                                                       op0=ALU.mult, op1=ALU.add), r=[krh, kW0, kVb], w=[kVb])
                            c.op("dve", lambda e: e.tensor_tensor_scan(out=fl(Vb[:, 1]), data0=fl(rh[:]), data1=fl(Wb[:, 1]), initial=0.0,
                                                                       op0=ALU.mult, op1=ALU.add), r=[krh, kW1, kVb], w=[kVb])
                            K1 = 255
                            self.tt("dve", tA[:, :, 0:K1], ck[:, :, 0:K1], Vb[:, 0, :, 0:K1], ALU.mult, r=[kck, kVb], w=[ktA])
                            self.tt("pool", tB[:, :, 0:K1], sk[:, :, 0:K1], Vb[:, 1, :, 0:K1], ALU.mult, r=[ksk, kVb], w=[ktB])
                            self.tt("dve", Xs[:, 0, :, 1:256], tA[:, :, 0:K1], tB[:, :, 0:K1], ALU.subtract, r=[ktA, ktB], w=[kXs])
                            self.tt("pool", tA[:, :, 0:K1], ck[:, :, 0:K1], Vb[:, 1, :, 0:K1], ALU.mult, r=[kck, kVb], w=[ktA])
                            self.tt("dve", tB[:, :, 0:K1], sk[:, :, 0:K1], Vb[:, 0, :, 0:K1], ALU.mult, r=[ksk, kVb], w=[ktB])
                            self.tt("pool", Xs[:, 1, :, 1:256], tA[:, :, 0:K1], tB[:, :, 0:K1], ALU.add, r=[ktA, ktB], w=[kXs])
                            for gl in range(GB):
                                g = b * GB + gl
                                pb = 4 + gl // 2 % 2
                                pk = "ps%d" % pb
                                o = self.ps[pb][:, (gl % 2) * 256:(gl % 2 + 1) * 256]
                                c.op("pe", lambda e: e.matmul(out=o, lhsT=Toep[:, g, :], rhs=Ug[:, g, :], start=True, stop=False),
                                     r=["Toep", "Ug"], w=[pk])
                                c.op("pe", lambda e: e.matmul(out=o, lhsT=WivR[:, g, :], rhs=Xs[:, 0, gl, :], start=False, stop=False),
                                     r=["WivR", kXs], w=[pk])
                                c.op("pe", lambda e: e.matmul(out=o, lhsT=WivI[:, g, :], rhs=Xs[:, 1, gl, :], start=False, stop=True),
                                     r=["WivI", kXs], w=[pk])
                                if gl % 2 == 1:
                                    g8l = (b * GB + gl - 1) % 8
                                    self.cp("act", Ysb[:, g8l:g8l + 2, :], self.ps[pb][:].rearrange("p (g k) -> p g k", g=2), r=[pk], w=["Ysb"])
                            if (b * GB + GB) % 8 == 0:
                                g8 = (b * GB) // 8
                                for kh in range(2):
                                    pb = 6 + kh
                                    pk = "ps%d" % pb
                                    ptv = self.ps[pb][:].bitcast(BF16).rearrange("p (g n) -> p g n", g=8)
                                    for gl in range(8):
                                        c.op("pe", lambda e: e.transpose(out=ptv[:, gl, :], in_=Ysb[:, gl, kh * 128:(kh + 1) * 128],
                                                                         identity=self.ident[:]), r=["Ysb", "ident"], w=[pk])
                                    self.cp("dve", yck[kh][:, :, g8 * 128:(g8 + 1) * 128].rearrange("p t (g c) -> p t g c", g=8),
                                            ptv.rearrange("p g (t c) -> p t g c", t=8), r=[pk], w=["yck%d" % kh])
                    with self.scope() as se:
                        TE_ = lambda name, shape, dt=F32: se.enter_context(self.sbt(name, shape, dt))
                        t1 = TE_("e_t1", [128, 8, BR]); t2 = TE_("e_t2", [128, 8, BR])
                        gck = TE_("gck", [128, 8, BR], BF16)
                        ack = TE_("ack", [128, 8, BR], BF16)
                        gT = [TE_("gT%d" % i, [128, 4, 128], BF16) for i in range(2)]
                        e1 = [TE_("e1_%d" % i, [128, BR]) for i in range(2)]
                        for kh in range(2):
                            uv = uck[kh][:].rearrange("p (s f) -> p s f", s=8)
                            zv = szck[kh][:].rearrange("p (s f) -> p s f", s=8)
                            self.tt("dve", t1[:], uv, Drep[:].unsqueeze(1).broadcast_to([128, 8, BR]), ALU.mult, r=["uck%d" % kh, "Drep"], w=["e_t1"])
                            self.tt("pool", t1[:], t1[:], yck[kh][:], ALU.add, r=["e_t1", "yck%d" % kh], w=["e_t1"])
                            self.actf(t2[:], t1[:], AF.Square, r=["e_t1"], w=["e_t2"])
                            self.ts("dve", t2[:], t2[:], 0.044715 * 0.7978845608, ALU.mult, r=["e_t2"], w=["e_t2"], s2=0.7978845608, op1=ALU.add)
                            self.tt("pool", t2[:], t2[:], t1[:], ALU.mult, r=["e_t2", "e_t1"], w=["e_t2"])
                            self.actf(t2[:], t2[:], AF.Tanh, r=["e_t2"], w=["e_t2"])
                            self.ts("dve", t2[:], t2[:], 1.0, ALU.add, r=["e_t2"], w=["e_t2"], s2=0.5, op1=ALU.mult)
                            self.tt("dve", gck[:], t2[:], t1[:], ALU.mult, r=["e_t2", "e_t1"], w=["gck"])
                            def glu_front(tau):
                                i2 = tau % 2
                                pk = "ps%d" % i2
                                ptv = self.ps[i2][:].bitcast(BF16).rearrange("p (a b) -> p a b", a=8)
                                for fc in range(4):
                                    c.op("pe", lambda e: e.transpose(out=ptv[:, fc, :], in_=gck[:, tau, fc * 128:(fc + 1) * 128],
                                                                     identity=self.ident[:]), r=["gck", "ident"], w=[pk])
                                self.cp("act", gT[i2][:], ptv[:, 0:4, :], r=[pk], w=["gT%d" % i2])
                            glu_front(0)
                            for tau in range(8):
                                i2 = tau % 2
                                pm = 2 + i2
                                for fc in range(4):
                                    c.op("pe", lambda e: e.matmul(out=self.ps[pm][:], lhsT=gT[i2][:, fc, :], rhs=gluw[:, fc, :],
                                                                  start=(fc == 0), stop=(fc == 3)), r=["gT%d" % i2, "gluw"], w=["ps%d" % pm])
                                if tau + 1 < 8:
                                    glu_front(tau + 1)
                                ek = "e1_%d" % i2
                                self.tt("dve", e1[i2][:], self.ps[pm][:], Brep[:], ALU.add, r=["ps%d" % pm, "Brep"], w=[ek])
                                self.actf(e1[i2][:], e1[i2][:], AF.Tanh, r=[ek], w=[ek], scale=0.5)
                                self.ts("dve", e1[i2][:], e1[i2][:], 0.5, ALU.mult, r=[ek], w=[ek], s2=0.5, op1=ALU.add)
                                self.tt("pool", e1[i2][:], e1[i2][:], gck[:, tau, :], ALU.mult, r=[ek, "gck"], w=[ek])
                                self.tt("dve", ack[:, tau, :], e1[i2][:], zv[:, tau, :], ALU.mult, r=[ek, "szck%d" % kh], w=["ack"])
                            rows = slice(tb + kh * 1024, tb + (kh + 1) * 1024)
                            c.dma(self.a_tok[rows, :].rearrange("(k t) f -> k (t f)", t=8), ack[:].rearrange("p t f -> p (t f)"),
                                  r=["ack"], w=["a_tok"])

    def phase_ml(self):
        nc, c = self.nc, self.c
        S_ = self.S
        NB = S_ // 128
        SC = 128 ** -0.5
        with self.scope() as st:
            T = lambda name, shape, dt=F32: st.enter_context(self.sbt(name, shape, dt))
            cw = T("m_cw", [128, 4, 4]); cb = T("m_cb", [128, 4])
            mn = T("m_mn", [128, 4]); msk = T("m_msk", [128, 4])
            gbi = T("m_gbi", [4, 1]); gbf = T("m_gbf", [4, 1]); ngbf = T("m_ngbf", [4, 1])
            maskS = T("m_maskS", [128, 128]); idf = T("m_idf", [128, 128])
            ones4 = T("m_ones4", [4, 128])
            wst = T("m_wst", [128, 3, 4, 128]); wqkv = T("m_wqkv", [128, 3, 4, 128], BF16)
            gst = T("m_gst", [128, 12, 8]); gw = T("m_gw", [128, 12, 8], BF16)
            for (t, d, key) in ((cw, self.ml_cw, "m_cw"), (cb, self.ml_cb, "m_cb"), (mn, self.ml_mn, "m_mn"),
                                (msk, self.ml_msk, "m_msk"), (gbi, self.ml_gbi, "m_gbi"), (gbf, self.ml_gbf, "m_gbf"),
                                (maskS, self.ml_maskS, "m_maskS"), (idf, self.identf_d, "m_idf"),
                                (gst, self.ml_gw, "m_gst")):
                c.dma(t[:], d, w=[key])
            for i, d in enumerate((self.ml_wq, self.ml_wk, self.ml_wv)):
                c.dma(wst[:, i], d, w=["m_wst"])
            self.cp("dve", wqkv[:], wst[:], r=["m_wst"], w=["m_wqkv"])
            self.cp("dve", gw[:], gst[:], r=["m_gst"], w=["m_gw"])
            self.ts("dve", ngbf[:], gbf[:], -1.0, ALU.mult, r=["m_gbf"], w=["m_ngbf"])
            c.op("dve", lambda e: e.memset(ones4[:], 1.0), w=["m_ones4"])
            xmT_v = self.xmT.rearrange("(c p) t -> p c t", p=128)
            mzT_v = self.mzT.rearrange("(c p) t -> p c t", p=128)
            bT_v = self.bT.rearrange("(c p) t -> p c t", p=128)
            for q in range(self.nseq):
                tb = q * S_
                with self.scope() as sq:
                    TQ = lambda name, shape, dt=F32: sq.enter_context(self.sbt(name, shape, dt))
                    xcT = TQ("xcT", [128, 4, S_], BF16)
                    qT = TQ("qT", [128, 4, S_], BF16)
                    kT = TQ("kT", [128, 4, S_], BF16)
                    Ktok = TQ("Ktok", [128, NB, 4, 128], BF16)
                    Vtok = TQ("Vtok", [128, NB, 4, 129], BF16)
                    acol = TQ("acol", [128, NB, 4]); bcol = TQ("bcol", [128, NB, 4])
                    Rrep = TQ("Rrep", [128, NB + 1, 4])
                    Wt = TQ("Wt", [128, NB, 4]); Wp = TQ("Wp", [128, NB, 4])
                    Thr = TQ("Thr", [128, NB, 4]); Dec = TQ("Dec", [128, NB, 4])
                    c.op("pool", lambda e: e.memset(Vtok[:, :, :, 128:129], 1.0), w=["Vtok"])
                    with self.scope() as sa:
                        TA_ = lambda name, shape, dt=F32: sa.enter_context(self.sbt(name, shape, dt))
                        xm = TA_("xm", [128, 4, S_], BF16)
                        vT = TA_("vT", [128, 4, S_], BF16)
                        acc = TA_("acc", [128, S_])
                        g1f = TA_("g1", [32, S_]); g2f = TA_("g2", [32, S_]); g3f = TA_("g3", [32, S_]); onesr = TA_("onesr", [32, S_])
                        g1 = g1f[0:4, :]; g2 = g2f[0:4, :]; g3 = g3f[0:4, :]
                        c.op("pool", lambda e: e.memset(g1f[:], 0.0), w=["g1"])
                        c.op("pool", lambda e: e.memset(g2f[:], 0.0), w=["g2"])
                        rsel = TA_("rsel", [4, NB, 4])
                        c.dma(xm[:], xmT_v[:, :, tb:tb + S_], r=["xmT"], w=["xm"])
                        c.op("pool", lambda e: e.memset(onesr[:], 1.0), w=["onesr"])
                        for fc in range(4):
                            self.ts("dve", acc[:], xm[:, fc, :], cw[:, fc, 3:4], ALU.mult, r=["xm", "m_cw", "m_cb"], w=["acc"],
                                    s2=cb[:, fc:fc + 1], op1=ALU.add)
                            for sh in (1, 2, 3):
                                self.stt("dve", acc[:, sh:], xm[:, fc, 0:S_ - sh], cw[:, fc, 3 - sh:4 - sh], acc[:, sh:],
                                         ALU.mult, ALU.add, r=["xm", "m_cw", "acc"], w=["acc"])
                            self.actf(xcT[:, fc, :], acc[:], AF.Silu, r=["acc"], w=["xcT"])
                        for h in range(4):
                            for tl in range(S_ // 512):
                                ts_ = slice(tl * 512, (tl + 1) * 512)
                                for (i, src, skey, dst, dkey) in ((0, xcT, "xcT", qT, "qT"), (1, xcT, "xcT", kT, "kT"), (2, xm, "xm", vT, "vT")):
                                    pb = (h * 12 + tl * 3 + i) % 4
                                    pk = "ps%d" % pb
                                    c.op("pe", lambda e: e.matmul(out=self.ps[pb][:], lhsT=wqkv[:, i, h, :], rhs=src[:, h, ts_],
                                                                  start=True, stop=True), r=["m_wqkv", skey], w=[pk])
                                    self.cp("act" if i != 1 else "dve", dst[:, h, ts_], self.ps[pb][:], r=[pk], w=[dkey])
                        for blk in range(NB):
                            bs = slice(blk * 128, (blk + 1) * 128)
                            for (i, src, skey, dst, dkey, pb) in ((1, xcT, "xcT", Ktok, "Ktok", 4), (2, xm, "xm", Vtok, "Vtok", 5)):
                                pb = pb + 2 * (blk % 2)
                                pk = "ps%d" % pb
                                for h in range(4):
                                    c.op("pe", lambda e: e.matmul(out=self.ps[pb][:, h * 128:(h + 1) * 128], lhsT=src[:, h, bs],
                                                                  rhs=wqkv[:, i, h, :], start=True, stop=True), r=["m_wqkv", skey], w=[pk])
                                self.cp("act" if i == 1 else "dve", dst[:, blk, :, 0:128], self.ps[pb][:].rearrange("p (h e) -> p h e", h=4),
                                        r=[pk], w=[dkey])
                        for tl in range(S_ // 512):
                            ts_ = slice(tl * 512, (tl + 1) * 512)
                            for half in range(2):
                                pb = half
                                pk = "ps%d" % pb
                                for ch in range(12):
                                    src = (qT, kT, vT)[ch // 4]
                                    skey = ("qT", "kT", "vT")[ch // 4]
                                    c.op("pe", lambda e: e.matmul(out=self.ps[pb][0:4, :], lhsT=gw[:, ch, half * 4:half * 4 + 4],
                                                                  rhs=src[:, ch % 4, ts_], start=(ch == 0), stop=(ch == 11)),
                                         r=["m_gw", skey], w=[pk])
                                if half == 0:
                                    self.ts("dve", g1[:, ts_], self.ps[pb][0:4, :], gbi[:, 0:1], ALU.add, r=[pk, "m_gbi"], w=["g1"])
                                else:
                                    self.actf(g2[:, ts_], self.ps[pb][0:4, :], AF.Exp, r=[pk, "m_ngbf"], w=["g2"], scale=-1.0, bias=ngbf[:, 0:1])
                        self.actf(g2[:], g2[:], AF.Ln, r=["g2"], w=["g2"], bias=1.0)
                        c.op("dve", lambda e: e.tensor_tensor_scan(out=g3f[:], data0=onesr[:], data1=g2f[:], initial=0.0, op0=ALU.mult, op1=ALU.add),
                             r=["onesr", "g2"], w=["g3"])
                        self.tt("dve", g1[:], g1[:], g3[:], ALU.add, r=["g1", "g3"], w=["g1"])
                        c.op("dve", lambda e: e.tensor_tensor_scan(out=g2f[:], data0=onesr[:], data1=g1f[:], initial=0.0, op0=ALU.mult, op1=ALU.max),
                             r=["onesr", "g1", "g2"], w=["g2"])
                        pa = self.ps[2][:, 0:NB * 4].rearrange("p (b h) -> p b h", h=4)
                        pbn = self.ps[3][:, 0:NB * 4].rearrange("p (b h) -> p b h", h=4)
                        for blk in range(NB):
                            bs = slice(blk * 128, (blk + 1) * 128)
                            c.op("pe", lambda e: e.transpose(out=pa[:, blk, :], in_=g1[:, bs], identity=idf[0:4, 0:4]), r=["g1", "m_idf"], w=["ps2"])
                            c.op("pe", lambda e: e.transpose(out=pbn[:, blk, :], in_=g3[:, bs], identity=idf[0:4, 0:4]), r=["g3", "m_idf"], w=["ps3"])
                        self.cp("dve", acol[:], pa, r=["ps2"], w=["acol"])
                        self.cp("dve", bcol[:], pbn, r=["ps3"], w=["bcol"])
                        self.tt("dve", rsel[:], g2[:, 127::128].unsqueeze(2).broadcast_to([4, NB, 4]),
                                idf[0:4, 0:4].unsqueeze(1).broadcast_to([4, NB, 4]), ALU.mult, r=["g2", "m_idf"], w=["rsel"])
                        c.op("pe", lambda e: e.matmul(out=self.ps[0][:, 0:NB * 4], lhsT=ones4[:], rhs=rsel[:].rearrange("p b h -> p (b h)"),
                                                      start=True, stop=True), r=["m_ones4", "rsel"], w=["ps0"])
                        c.op("dve", lambda e: e.memset(Rrep[:, 0, :], 0.0), w=["Rrep"])
                        self.cp("dve", Rrep[:, 1:NB + 1, :], self.ps[0][:, 0:NB * 4].rearrange("p (b h) -> p b h", h=4), r=["ps0"], w=["Rrep"])
                    self.tt("dve", Wt[:], acol[:], Rrep[:, 0:NB, :], ALU.subtract, r=["acol", "Rrep"], w=["Wt"])
                    self.actf(Wt[:], Wt[:], AF.Exp, r=["Wt"], w=["Wt"])
                    self.tt("dve", Wp[:], acol[:], Rrep[:, 1:NB + 1, :], ALU.subtract, r=["acol", "Rrep"], w=["Wp"])
                    self.actf(Wp[:], Wp[:], AF.Exp, r=["Wp"], w=["Wp"])
                    self.ts("dve", Wp[:], Wp[:], SC, ALU.mult, r=["Wp"], w=["Wp"])
                    self.tt("dve", Thr[:], bcol[:], Rrep[:, 0:NB, :], ALU.subtract, r=["bcol", "Rrep"], w=["Thr"])
                    self.actf(Thr[:], Thr[:], AF.Exp, r=["Thr"], w=["Thr"])
                    self.tt("dve", Dec[:], Rrep[:, 0:NB, :], Rrep[:, 1:NB + 1, :], ALU.subtract, r=["Rrep"], w=["Dec"])
                    self.actf(Dec[:], Dec[:], AF.Exp, r=["Dec"], w=["Dec"])
                    with self.scope() as sm:
                        TM = lambda name, shape, dt=F32: sm.enter_context(self.sbt(name, shape, dt))
                        C32 = TM("C32", [128, 4, 129]); Cm = TM("Cm", [128, 4, 129])
                        Cb = TM("Cb", [128, 4, 129], BF16)
                        PT4 = [TM("PT4_%d" % i, [128, 4, 128], BF16) for i in range(2)]
                        Vp4 = [TM("Vp4_%d" % i, [128, 4, 129], BF16) for i in range(2)]
                        Vpp4 = [TM("Vpp4_%d" % i, [128, 4, 129], BF16) for i in range(2)]
                        den = TM("den4", [128, 4])
                        hraw = [TM("hraw%d" % i, [128, 4, 128]) for i in range(2)]
                        bst = TM("bst", [128, 4, 6]); mv = TM("mv", [128, 4, 2]); rs = TM("rs", [128, 4])
                        hn = TM("hn", [128, 4, 128], BF16)
                        e1 = TM("m_e1", [128, 4, 128]); e2 = TM("m_e2", [128, 4, 128])
                        mz = [TM("mz%d" % i, [128, 4, 512], BF16) for i in range(2)]
                        bo = [TM("bo%d" % i, [128, 4, 512], BF16) for i in range(2)]

                        def emit_front(I_):
                            bs_ = slice(I_ * 128, (I_ + 1) * 128)
                            j_ = I_ % 2
                            for h_ in range(4):
                                c.op("pe", lambda e: e.matmul(out=self.ps[j_][:, h_ * 128:(h_ + 1) * 128], lhsT=kT[:, h_, bs_], rhs=qT[:, h_, bs_],
                                                              start=True, stop=True), r=["kT", "qT"], w=["ps%d" % j_])
                            self.tt("dve", PT4[j_][:], self.ps[j_][:].rearrange("p (h t) -> p h t", h=4),
                                    maskS[:].unsqueeze(1).broadcast_to([128, 4, 128]), ALU.mult, r=["ps%d" % j_, "m_maskS"], w=["PT4_%d" % j_])
                            self.tt("pool", Vp4[j_][:], Vtok[:, I_, :, :], Wt[:, I_, :].unsqueeze(2).broadcast_to([128, 4, 129]), ALU.mult,
                                    r=["Vtok", "Wt"], w=["Vp4_%d" % j_])
                            self.tt("pool", Vpp4[j_][:], Vtok[:, I_, :, :], Wp[:, I_, :].unsqueeze(2).broadcast_to([128, 4, 129]), ALU.mult,
                                    r=["Vtok", "Wp"], w=["Vpp4_%d" % j_])
                        emit_front(0)
                        for I in range(NB):
                            bs = slice(I * 128, (I + 1) * 128)
                            i4 = (I // 4) % 2
                            j = I % 2
                            if I % 4 == 0:
                                c.dma(mz[i4][:], mzT_v[:, :, tb + I * 128:tb + I * 128 + 512], r=["mzT"], w=["mz%d" % i4])
                            hk = "hraw%d" % j
                            for h in range(4):
                                pO = self.ps[2 + h // 2][:, (h % 2) * 129:(h % 2 + 1) * 129]
                                kO = "ps%d" % (2 + h // 2)
                                c.op("pe", lambda e: e.matmul(out=pO, lhsT=PT4[j][:, h, :], rhs=Vp4[j][:, h, :], start=True, stop=(I == 0)),
                                     r=["PT4_%d" % j, "Vp4_%d" % j], w=[kO])
                                if I > 0:
                                    c.op("pe", lambda e: e.matmul(out=pO, lhsT=qT[:, h, bs], rhs=Cb[:, h, :], start=False, stop=True),
                                         r=["qT", "Cb"], w=[kO])
                            if I < NB - 1:
                                for h in range(4):
                                    pC = self.ps[4 + h // 2][:, (h % 2) * 129:(h % 2 + 1) * 129]
                                    c.op("pe", lambda e: e.matmul(out=pC, lhsT=Ktok[:, I, h, :], rhs=Vpp4[j][:, h, :], start=True, stop=True),
                                         r=["Ktok", "Vpp4_%d" % j], w=["ps%d" % (4 + h // 2)])
                            if I + 1 < NB:
                                emit_front(I + 1)
                            if I < NB - 1:
                                if I == 0:
                                    for hb in range(2):
                                        self.cp("dve", C32[:, 2 * hb:2 * hb + 2, :], self.ps[4 + hb][:, 0:258].rearrange("p (h e) -> p h e", h=2),
                                                r=["ps%d" % (4 + hb)], w=["C32"])
                                else:
                                    self.tt("pool", Cm[:], C32[:], Dec[:, I, :].unsqueeze(2).broadcast_to([128, 4, 129]), ALU.mult, r=["C32", "Dec"], w=["Cm"])
                                    for hb in range(2):
                                        self.tt("dve", C32[:, 2 * hb:2 * hb + 2, :], self.ps[4 + hb][:, 0:258].rearrange("p (h e) -> p h e", h=2),
                                                Cm[:, 2 * hb:2 * hb + 2, :], ALU.add, r=["ps%d" % (4 + hb), "Cm"], w=["C32"])
                                self.cp("act", Cb[:], C32[:], r=["C32"], w=["Cb"])
                            for hb in range(2):
                                self.actf(den[:, 2 * hb:2 * hb + 2], self.ps[2 + hb][:, 0:258].rearrange("p (h e) -> p h e", h=2)[:, :, 128],
                                          AF.Abs, r=["ps%d" % (2 + hb)], w=["den4"])
                            self.tt("dve", den[:], den[:], Thr[:, I, :], ALU.max, r=["den4", "Thr"], w=["den4"])
                            c.op("dve", lambda e: e.reciprocal(out=den[:], in_=den[:]), r=["den4"], w=["den4"])
                            for hb in range(2):
                                self.tt("dve", hraw[j][:, 2 * hb:2 * hb + 2, :], self.ps[2 + hb][:, 0:258].rearrange("p (h e) -> p h e", h=2)[:, :, 0:128],
                                        den[:, 2 * hb:2 * hb + 2].unsqueeze(2).broadcast_to([128, 2, 128]), ALU.mult, r=["ps%d" % (2 + hb), "den4"], w=[hk])
                            for h in range(4):
                                c.op("dve", lambda e: e.bn_stats(out=bst[:, h, :], in_=hraw[j][:, h, :]), r=[hk], w=["bst"])
                                c.op("dve", lambda e: e.bn_aggr(out=mv[:, h, :], in_=bst[:, h, :]), r=["bst"], w=["mv"])
                            self.actf(rs[:], mv[:, :, 1], AF.Ln, r=["mv"], w=["rs"], bias=HEAD_EPS)
                            self.actf(rs[:], rs[:], AF.Exp, r=["rs"], w=["rs"], scale=-0.5)
                            self.tt("pool", e1[:], hraw[j][:], mv[:, :, 0:1].broadcast_to([128, 4, 128]), ALU.subtract, r=[hk, "mv"], w=["m_e1"])
                            self.tt("dve", hn[:], e1[:], rs[:].unsqueeze(2).broadcast_to([128, 4, 128]), ALU.mult, r=["m_e1", "rs"], w=["hn"])
                            ptv = self.ps[6 + I % 2][:].bitcast(BF16).rearrange("p (a b) -> p a b", a=8)
                            pk = "ps%d" % (6 + I % 2)
                            for h in range(4):
                                c.op("pe", lambda e: e.transpose(out=ptv[:, h, :], in_=hn[:, h, :], identity=self.ident[:]), r=["hn", "ident"], w=[pk])
                            self.tt("pool", e2[:], xcT[:, :, bs], msk[:].unsqueeze(2).broadcast_to([128, 4, 128]), ALU.mult, r=["xcT", "m_msk"], w=["m_e2"])
                            self.tt("dve", e1[:], ptv[:, 0:4, :], mn[:].unsqueeze(2).broadcast_to([128, 4, 128]), ALU.mult, r=[pk, "m_mn", "m_e1"], w=["m_e1"])
                            self.tt("dve", e1[:], e1[:], e2[:], ALU.add, r=["m_e1", "m_e2"], w=["m_e1"])
                            off = (I % 4) * 128
                            self.tt("pool", bo[i4][:, :, off:off + 128], e1[:], mz[i4][:, :, off:off + 128], ALU.mult,
                                    r=["m_e1", "mz%d" % i4], w=["bo%d" % i4])
                            if I % 4 == 3:
                                c.dma(bT_v[:, :, tb + (I - 3) * 128:tb + (I + 1) * 128], bo[i4][:], r=["bo%d" % i4], w=["bT"])
        c.barrier()

    def out_proj_norm_res(self, lay, wout_d, pg_rep_d, lhs_provider, res_rows, dst_rows, dst_key, res_key):
        nc, c = self.nc, self.c
        NT = self.NT
        with self.scope() as st:
            T = lambda name, shape, dt=F32: st.enter_context(self.sbt(name, shape, dt))
            wout = T("wout", [128, 8, D], BF16)
            wst = [T("wost%d" % i, [128, D]) for i in range(2)]
            for fc in range(8):
                c.dma(wst[fc % 2][:], wout_d[fc * 128:(fc + 1) * 128, :], w=["wost%d" % (fc % 2)])
                self.cp("dve" if fc % 2 else "act", wout[:, fc, :], wst[fc % 2][:], r=["wost%d" % (fc % 2)], w=["wout"])
            pg = T("pg", [128, D])
            c.dma(pg[:], pg_rep_d, w=["pg"])
            NXR = 4
            xr = [T("xr%d" % i, [128, D]) for i in range(NXR)]
            yo = [T("yo%d" % i, [128, D]) for i in range(2)]
            junk = T("ojunk", [128, BR])
            ss2 = [T("oss%d" % i, [128, 2]) for i in range(2)]; rstd2 = [T("orstd%d" % i, [128, 1]) for i in range(2)]
            pref, prov = lhs_provider(st)
            nblk = NT // 128

            def prefetch(b):
                if b < nblk:
                    c.dma(xr[b % NXR][:], res_rows[b * 128:(b + 1) * 128, :], r=[res_key % b], w=["xr%d" % (b % NXR)])
                    pref(b)
            prefetch(0)
            prefetch(1)
            lhs_next = prov(0)
            for blk in range(nblk):
                rows = slice(blk * 128, (blk + 1) * 128)
                xk = "xr%d" % (blk % NXR)
                prefetch(blk + 2)
                lhs = lhs_next
                ss = ss2[blk % 2]; rstd = rstd2[blk % 2]
                ssk = "oss%d" % (blk % 2); rsk = "orstd%d" % (blk % 2)
                for half in range(2):
                    pb = 4 + half + 2 * (blk % 2)
                    pk = "ps%d" % pb
                    for fc in range(8):
                        ap, key = lhs[fc]
                        c.op("pe", lambda e: e.matmul(out=self.ps[pb][:], lhsT=ap, rhs=wout[:, fc, half * BR:(half + 1) * BR],
                                                      start=(fc == 0), stop=(fc == 7)), r=[key, "wout"], w=[pk])
                if blk + 1 < nblk:
                    lhs_next = prov(blk + 1)
                for half in range(2):
                    pb = 4 + half + 2 * (blk % 2)
                    pk = "ps%d" % pb
                    self.actf(junk[:], self.ps[pb][:], AF.Square, r=[pk], w=["ojunk", ssk], accum_out=ss[:, half:half + 1])
                self.tt("dve", rstd[:], ss[:, 0:1], ss[:, 1:2], ALU.add, r=[ssk], w=[rsk])
                self.actf(rstd[:], rstd[:], AF.Ln, r=[rsk], w=[rsk], scale=1.0 / D, bias=NORM_EPS)
                self.actf(rstd[:], rstd[:], AF.Exp, r=[rsk], w=[rsk], scale=-0.5)
                yk = "yo%d" % (blk % 2)
                for half in range(2):
                    pb = 4 + half + 2 * (blk % 2)
                    hs = slice(half * BR, (half + 1) * BR)
                    self.tt("dve", yo[blk % 2][:, hs], self.ps[pb][:], pg[:, hs], ALU.mult, r=["ps%d" % pb, "pg"], w=[yk])
                self.stt("dve", yo[blk % 2][:], yo[blk % 2][:], rstd[:, 0:1], xr[blk % NXR][:], ALU.mult, ALU.add, r=[yk, rsk, xk], w=[yk])
                c.dma(dst_rows[rows, :], yo[blk % 2][:], r=[yk], w=[dst_key % blk])

    def phase_l0_out(self):
        nc, c = self.nc, self.c
        bT_v = self.bT.rearrange("(c p) t -> p c t", p=128)

        def provider(st):
            T = lambda name, shape, dt=F32: st.enter_context(self.sbt(name, shape, dt))
            at = [T("at%d" % i, [128, BR], BF16) for i in range(4)]
            aT = [T("aT%d" % i, [128, 4, 128], BF16) for i in range(2)]
            bt = [T("bt%d" % i, [128, 4, 512], BF16) for i in range(2)]

            def pref(blk):
                i4 = (blk // 4) % 2
                c.dma(at[blk % 4][:], self.a_tok[blk * 128:(blk + 1) * 128, :], r=["a_tok"], w=["at%d" % (blk % 4)])
                if blk % 4 == 0:
                    c.dma(bt[i4][:], bT_v[:, :, blk * 128:blk * 128 + 512], r=["bT"], w=["bt%d" % i4])

            def prov(blk):
                i = blk % 2
                i4 = (blk // 4) % 2
                pk = "ps%d" % i
                ptv = self.ps[i][:].bitcast(BF16).rearrange("p (a b) -> p a b", a=8)
                for fc in range(4):
                    c.op("pe", lambda e: e.transpose(out=ptv[:, fc, :], in_=at[blk % 4][:, fc * 128:(fc + 1) * 128], identity=self.ident[:]),
                         r=["at%d" % (blk % 4), "ident"], w=[pk])
                self.cp("act", aT[i][:], ptv[:, 0:4, :], r=[pk], w=["aT%d" % i])
                off = (blk % 4) * 128
                return [(aT[i][:, fc, :], "aT%d" % i) for fc in range(4)] + \
                       [(bt[i4][:, h, off:off + 128], "bt%d" % i4) for h in range(4)]
            return pref, prov
        self.out_proj_norm_res(0, self.w_out_ab, self.post0_rep, provider, self.x, self.out, "h1_%d", "x%.0d")

    def phase_l1_proj(self):
        nc, c = self.nc, self.c
        NT = self.NT
        S_ = self.S
        ngrp = NT // 512
        nblk = NT // 128
        with self.scope() as st:
            T = lambda name, shape, dt=F32: st.enter_context(self.sbt(name, shape, dt))
            win = T("win1", [128, 8, 8 * BR], BF16)
            g1 = T("g1n", [128, 8])
            stage = [T("w1st%d" % i, [128, 4 * BR]) for i in range(2)]
            c.dma(g1[:], self.pre1, w=["g1n"])
            for dc in range(8):
                for hf in range(2):
                    i = (dc * 2 + hf) % 2
                    sk = "w1st%d" % i
                    c.dma(stage[i][:], self.w_in_cd[dc * 128:(dc + 1) * 128, hf * 2048:(hf + 1) * 2048], w=[sk])
                    if hf == 0:
                        c.op("act", lambda e: e.activation(out=win[:, dc, 0:2048], in_=stage[i][:], func=AF.Copy, scale=g1[:, dc:dc + 1]),
                             r=[sk, "g1n"], w=["win1"])
                    else:
                        self.ts("dve", win[:, dc, 2048:4096], stage[i][:], g1[:, dc:dc + 1], ALU.mult, r=[sk, "g1n"], w=["win1"])
            cosT = T("cosT", [128, S_]); sinT = T("sinT", [128, S_]); Rm = T("Rm", [128, 128], BF16)
            c.dma(cosT[:], self.rope_cos, w=["cosT"])
            c.dma(sinT[:], self.rope_sin, w=["sinT"])
            c.dma(Rm[:], self.rope_rm, w=["Rm"])
            NXB = 6
            xt = [T("x1t%d" % i, [128, D]) for i in range(NXB)]
            junk = T("junk1", [128, D])
            ss2 = [T("ss1_%d" % i, [128, 4]) for i in range(2)]; rstd2 = [T("rstd1_%d" % i, [128, 4]) for i in range(2)]
            zb = [T("z1b%d" % i, [128, D], BF16) for i in range(2)]
            zT = [T("z1T%d" % i, [128, 8, 512], BF16) for i in range(2)]
            vo = [T("vo%d" % i, [128, BR], BF16) for i in range(4)]
            xb = [T("xb%d" % i, [128, 512], BF16) for i in range(2)]
            r1 = [T("r1_%d" % i, [128, 512]) for i in range(2)]
            r2 = [T("r2_%d" % i, [128, 512]) for i in range(2)]
            fo = [T("fo%d" % i, [128, 4, 512], BF16) for i in range(2)]

            def load_x(b):
                if b < nblk:
                    c.dma(xt[b % NXB][:], self.h1src[b * 128:(b + 1) * 128, :], r=["h1_%d" % b], w=["x1t%d" % (b % NXB)])
            for b in range(4):
                load_x(b)
            fm_groups = [(0, self.cqT, "cqT", "rope"), (512, self.ckT, "ckT", "rope"), (2048, self.dqT, "dqT", "rope"),
                         (2560, self.dkT, "dkT", "rope"), (1536, self.czT, "czT", "silu"), (3584, self.dzT, "dzT", "silu")]
            cnt = 0
            for g in range(ngrp):
                zk = "z1T%d" % (g % 2)
                ss = ss2[g % 2]; rstd = rstd2[g % 2]
                ssk = "ss1_%d" % (g % 2); rsk = "rstd1_%d" % (g % 2)
                pos0 = (g * 512) % S_
                for j in range(4):
                    b = g * 4 + j
                    xk = "x1t%d" % (b % NXB)
                    c.op("act", lambda e: e.activation(out=junk[:], in_=xt[b % NXB][:], func=AF.Square, accum_out=ss[:, j:j + 1]),
                         r=[xk], w=["junk1", ssk])
                self.actf(rstd[:], ss[:], AF.Ln, r=[ssk], w=[rsk], scale=1.0 / D, bias=NORM_EPS)
                self.actf(rstd[:], rstd[:], AF.Exp, r=[rsk], w=[rsk], scale=-0.5)
                for j in range(4):
                    b = g * 4 + j
                    xk = "x1t%d" % (b % NXB)
                    zbk = "z1b%d" % (b % 2)
                    pk = "ps%d" % (b % 2)
                    self.ts("dve", zb[b % 2][:], xt[b % NXB][:], rstd[:, j:j + 1], ALU.mult, r=[xk, rsk], w=[zbk])
                    load_x(b + 4)
                    ptv = self.ps[b % 2][:].bitcast(BF16).rearrange("p (a b) -> p a b", a=8)
                    for dc in range(8):
                        c.op("pe", lambda e: e.transpose(out=ptv[:, dc, :], in_=zb[b % 2][:, dc * 128:(dc + 1) * 128], identity=self.ident[:]),
                             r=[zbk, "ident"], w=[pk])
                    self.cp("dve", zT[g % 2][:, :, j * 128:(j + 1) * 128], ptv, r=[pk], w=[zk])
                    for vi, (col0, dst, dkey) in enumerate(((1024, self.cv_tok, "cv_tok"), (3072, self.dv_tok, "dv_tok"))):
                        pb = 2 + vi
                        vk = "vo%d" % ((b % 2) * 2 + vi)
                        for dc in range(8):
                            c.op("pe", lambda e: e.matmul(out=self.ps[pb][:], lhsT=zT[g % 2][:, dc, j * 128:(j + 1) * 128],
                                                          rhs=win[:, dc, col0:col0 + BR], start=(dc == 0), stop=(dc == 7)), r=[zk, "win1"], w=["ps%d" % pb])
                        self.cp("act", vo[(b % 2) * 2 + vi][:], self.ps[pb][:], r=["ps%d" % pb], w=[vk])
                        c.dma(dst[b * 128:(b + 1) * 128, :], vo[(b % 2) * 2 + vi][:], r=[vk], w=[dkey])
                pend = []

                def flush_pend():
                    while pend:
                        (fot_, fc_, fk_, i2_, pb_, dst_, dkey_, lastfc) = pend.pop(0)
                        pr = 6 + i2_
                        c.op("pe", lambda e: e.matmul(out=self.ps[pr][:], lhsT=Rm[:], rhs=xb[i2_][:], start=True, stop=True),
                             r=["Rm", "xb%d" % i2_], w=["ps%d" % pr])
                        self.tt("dve", r1[i2_][:], self.ps[pb_][:], cosT[:, pos0:pos0 + 512], ALU.mult, r=["ps%d" % pb_, "cosT"], w=["r1_%d" % i2_])
                        self.tt("dve", r2[i2_][:], self.ps[pr][:], sinT[:, pos0:pos0 + 512], ALU.mult, r=["ps%d" % pr, "sinT"], w=["r2_%d" % i2_])
                        self.tt("pool", fot_[:, fc_, :], r1[i2_][:], r2[i2_][:], ALU.add, r=["r1_%d" % i2_, "r2_%d" % i2_], w=[fk_])
                        if lastfc:
                            c.dma(dst_.rearrange("(c p) t -> p c t", p=128)[:, :, g * 512:(g + 1) * 512], fot_[:], r=[fk_], w=[dkey_])
                for (col0, dst, dkey, kind) in fm_groups:
                    fk = "fo%d" % (cnt % 2)
                    fot = fo[cnt % 2]
                    cnt += 1
                    for fc in range(4):
                        pb = 4 + fc % 2
                        pk = "ps%d" % pb
                        for dc in range(8):
                            c.op("pe", lambda e: e.matmul(out=self.ps[pb][:], lhsT=win[:, dc, col0 + fc * 128:col0 + (fc + 1) * 128],
                                                          rhs=zT[g % 2][:, dc, :], start=(dc == 0), stop=(dc == 7)), r=[zk, "win1"], w=[pk])
                        flush_pend()
                        if kind == "silu":
                            self.actf(fot[:, fc, :], self.ps[pb][:], AF.Silu, r=[pk], w=[fk])
                            if fc == 3:
                                c.dma(dst.rearrange("(c p) t -> p c t", p=128)[:, :, g * 512:(g + 1) * 512], fot[:], r=[fk], w=[dkey])
                        else:
                            i2 = fc % 2
                            self.cp("act", xb[i2][:], self.ps[pb][:], r=[pk], w=["xb%d" % i2])
                            pend.append((fot, fc, fk, i2, pb, dst, dkey, fc == 3))
                flush_pend()

    def phase_attn(self, kind):
        nc, c = self.nc, self.c
        S_ = self.S
        NB = S_ // 128
        NQT = S_ // 512
        dil = (kind == "dil")
        qT_d, kT_d, v_d, o_d = (self.cqT, self.ckT, self.cv_tok, self.oc_tok) if dil else (self.dqT, self.dkT, self.dv_tok, self.od_tok)
        okey = "oc_tok" if dil else "od_tok"
        NH = 8 if dil else 4
        VW = 64 if dil else 128
        nmask = 9 if dil else 4
        lam_init = 0.8 - 0.6 * math.exp(-0.3 * 1)
        with self.scope() as st:
            T = lambda name, shape, dt=F32: st.enter_context(self.sbt(name, shape, dt))
            masks = T("amask", [128, nmask, 512], BF16)
            c.dma(masks[:], self.dil_masks if dil else self.diff_masks, w=["amask"])
            if not dil:
                lqk = T("lqk", [1, 4, 64]); pr = T("lpr", [1, 2, 64]); sm = T("lsm", [1, 2]); nl = T("nl", [1, 1])
                ones1 = T("ones1", [1, 128]); nlam = T("nlam", [128, 1]); gdn = T("gdn", [128, 128])
                c.dma(lqk[:], self.diff_lqk, w=["lqk"])
                c.dma(gdn[:], self.diffnorm_rep, w=["gdn"])
                c.op("dve", lambda e: e.memset(ones1[:], 1.0), w=["ones1"])
                self.tt("dve", pr[:, 0, :], lqk[:, 0, :], lqk[:, 1, :], ALU.mult, r=["lqk"], w=["lpr"])
                self.tt("dve", pr[:, 1, :], lqk[:, 2, :], lqk[:, 3, :], ALU.mult, r=["lqk", "lpr"], w=["lpr"])
                c.op("dve", lambda e: e.reduce_sum(out=sm[:], in_=pr[:], axis=AX.X), r=["lpr"], w=["lsm"])
                self.actf(sm[:], sm[:], AF.Exp, r=["lsm"], w=["lsm"])
                self.tt("dve", nl[:], sm[:, 1:2], sm[:, 0:1], ALU.subtract, r=["lsm"], w=["nl"])
                self.ts("dve", nl[:], nl[:], -lam_init, ALU.add, r=["nl"], w=["nl"])
                c.op("pe", lambda e: e.matmul(out=self.ps[0][:, 0:1], lhsT=ones1[:], rhs=nl[:], start=True, stop=True), r=["ones1", "nl"], w=["ps0"])
                self.cp("dve", nlam[:], self.ps[0][:, 0:1], r=["ps0"], w=["nlam"])
                self.ts("dve", gdn[:], gdn[:], 1.0 - lam_init, ALU.mult, r=["gdn"], w=["gdn"])
            qT_v = qT_d.rearrange("(c p) t -> p c t", p=128)
            kT_v = kT_d.rearrange("(c p) t -> p c t", p=128)
            for q in range(self.nseq):
                tb = q * S_
                with self.scope() as sq:
                    TQ = lambda name, shape, dt=F32: sq.enter_context(self.sbt(name, shape, dt))
                    qz = [TQ("aqz%d" % i, [128, 4, S_], BF16) for i in range(2)]
                    kT = TQ("akT", [128, 4, S_], BF16)
                    V1 = TQ("aV1", [128, NB, NH, VW + 1], BF16)
                    osb = TQ("aosb", [128, NB, BR], BF16)
                    NEP = 3
                    E = [TQ("aE%d" % i, [128, 512], BF16) for i in range(NEP)]
                    P = [TQ("aP%d" % i, [128, 512], BF16) for i in range(NEP)]
                    rd = [TQ("ard%d" % i, [128, 1]) for i in range(4)]
                    if not dil:
                        o01 = [TQ("ao%d" % i, [128, NB, 128]) for i in range(2)]
                        sqj = TQ("asq", [128, 128]); ssn = TQ("assn", [128, NB]); t3 = TQ("at3", [128, 128])
                    c.op("pool", lambda e: e.memset(qz[0][64:128, :, :], 0.0), w=["aqT"])
                    c.op("pool", lambda e: e.memset(qz[1][0:64, :, :], 0.0), w=["aqT"])
                    c.dma(qz[0][0:64, :, :], qT_v[0:64, :, tb:tb + S_], r=[("cqT" if dil else "dqT")], w=["aqT"])
                    c.dma(qz[1][64:128, :, :], qT_v[64:128, :, tb:tb + S_], r=[("cqT" if dil else "dqT")], w=["aqT"])
                    c.dma(kT[:], kT_v[:, :, tb:tb + S_], r=[("ckT" if dil else "dkT")], w=["akT"])
                    c.op("pool", lambda e: e.memset(V1[:, :, :, VW:VW + 1], 1.0), w=["aV1"])
                    for blk in range(NB):
                        c.dma(V1[:, blk, :, 0:VW], v_d[tb + blk * 128:tb + (blk + 1) * 128, :].rearrange("p (h d) -> p h d", h=NH),
                              r=[("cv_tok" if dil else "dv_tok")], w=["aV1"])
                    tiles = []
                    for h in range(NH):
                        for m in range(1 if dil else 2):
                            for qt in range(NQT):
                                nkb = 4 * qt + 4
                                for kb in range(nkb):
                                    tiles.append((h, m, qt, kb, kb == nkb - 1))
                    LA = 3
                    NSB = 4

                    def emit_S(i):
                        h, m, qt, kb, _ = tiles[i]
                        ch = h // 2 if dil else h
                        rb = 64 * (h % 2) if dil else 64 * m
                        sb = i % NSB
                        c0 = 128 * max(0, kb - 4 * qt)
                        c.op("pe", lambda e: e.matmul(out=self.ps[sb][:, c0:512], lhsT=kT[:, ch, kb * 128:(kb + 1) * 128],
                                                      rhs=qz[rb // 64][:, ch, qt * 512 + c0:(qt + 1) * 512], start=True, stop=True),
                             r=["akT", "aqT"], w=["ps%d" % sb])
                    for i in range(min(LA, len(tiles))):
                        emit_S(i)
                    for i, (h, m, qt, kb, last) in enumerate(tiles):
                        if i + LA < len(tiles):
                            emit_S(i + LA)
                        sb = i % NSB
                        i2 = i % NEP
                        pS = self.ps[sb]
                        kS = "ps%d" % sb
                        d0 = 4 * qt - kb
                        if dil:
                            mi = 8 if d0 >= 5 else d0 + 3
                        else:
                            mi = d0 + 3 if d0 <= 0 else None
                        c0 = 128 * max(0, kb - 4 * qt)
                        if mi is None:
                            self.actf(P[i2][:, c0:512], pS[:, c0:512], AF.Exp, r=[kS], w=["aP%d" % i2], scale=0.125)
                        else:
                            self.actf(E[i2][:, c0:512], pS[:, c0:512], AF.Exp, r=[kS], w=["aE%d" % i2], scale=0.125)
                            self.tt("dve", P[i2][:, c0:512], E[i2][:, c0:512], masks[:, mi, c0:512], ALU.mult,
                                    r=["aE%d" % i2, "amask"], w=["aP%d" % i2])
                        for j in range(4):
                            Q = 4 * qt + j
                            if kb > Q:
                                continue
                            c.op("pe", lambda e: e.matmul(out=self.ps[4 + j][:, 0:VW + 1], lhsT=P[i2][:, j * 128:(j + 1) * 128],
                                                          rhs=V1[:, kb, h, :], start=(kb == 0), stop=(kb == Q)),
                                 r=["aP%d" % i2, "aV1"], w=["ps%d" % (4 + j)])
                        if last:
                            for j in range(4):
                                Q = 4 * qt + j
                                pO = self.ps[4 + j]
                                kO = "ps%d" % (4 + j)
                                c.op("dve", lambda e: e.reciprocal(out=rd[j][:], in_=pO[:, VW:VW + 1]), r=[kO], w=["ard%d" % j])
                                if dil:
                                    c.op("act", lambda e: e.activation(out=osb[:, Q, h * 64:(h + 1) * 64], in_=pO[:, 0:VW], func=AF.Copy,
                                                                       scale=rd[j][:, 0:1]), r=[kO, "ard%d" % j], w=["aosb"])
                                else:
                                    c.op("act", lambda e: e.activation(out=o01[m][:, Q, :], in_=pO[:, 0:VW], func=AF.Copy,
                                                                       scale=rd[j][:, 0:1]), r=[kO, "ard%d" % j], w=["ao%d" % m])
                            if (not dil) and m == 1 and qt == NQT - 1:
                                self.stt("dve", o01[0][:], o01[1][:], nlam[:, 0:1], o01[0][:], ALU.mult, ALU.add, r=["ao0", "ao1", "nlam"], w=["ao0"])
                                for Q in range(NB):
                                    self.actf(sqj[:], o01[0][:, Q, :], AF.Square, r=["ao0"], w=["asq", "assn"], accum_out=ssn[:, Q:Q + 1])
                                self.actf(ssn[:], ssn[:], AF.Ln, r=["assn"], w=["assn"], scale=1.0 / 128, bias=HEAD_EPS)
                                self.actf(ssn[:], ssn[:], AF.Exp, r=["assn"], w=["assn"], scale=-0.5)
                                for Q in range(NB):
                                    self.ts("dve", t3[:], o01[0][:, Q, :], ssn[:, Q:Q + 1], ALU.mult, r=["ao0", "assn"], w=["at3"])
                                    self.tt("pool", osb[:, Q, h * 128:(h + 1) * 128], t3[:], gdn[:], ALU.mult, r=["at3", "gdn"], w=["aosb"])
                    c.dma(o_d[tb:tb + S_, :].rearrange("(b p) f -> p b f", p=128), osb[:], r=["aosb"], w=[okey])

    def phase_l1_out(self):
        nc, c = self.nc, self.c
        czT_v = self.czT.rearrange("(c p) t -> p c t", p=128)
        dzT_v = self.dzT.rearrange("(c p) t -> p c t", p=128)

        def provider(st):
            T = lambda name, shape, dt=F32: st.enter_context(self.sbt(name, shape, dt))
            ot = [T("ot%d" % i, [128, 2, BR], BF16) for i in range(4)]
            gT = [T("gT1_%d" % i, [128, 8, 128], BF16) for i in range(2)]
            gz = [T("gz%d" % i, [128, 8, 512], BF16) for i in range(2)]

            def pref(blk):
                i = blk % 4
                i4 = (blk // 4) % 2
                c.dma(ot[i][:, 0, :], self.oc_tok[blk * 128:(blk + 1) * 128, :], r=["oc_tok"], w=["ot%d" % i])
                c.dma(ot[i][:, 1, :], self.od_tok[blk * 128:(blk + 1) * 128, :], r=["od_tok"], w=["ot%d" % i])
                if blk % 4 == 0:
                    c.dma(gz[i4][:, 0:4, :], czT_v[:, :, blk * 128:blk * 128 + 512], r=["czT"], w=["gz%d" % i4])
                    c.dma(gz[i4][:, 4:8, :], dzT_v[:, :, blk * 128:blk * 128 + 512], r=["dzT"], w=["gz%d" % i4])

            def prov(blk):
                i = blk % 2
                i4 = (blk // 4) % 2
                pk = "ps%d" % i
                ptv = self.ps[i][:].bitcast(BF16).rearrange("p (a b) -> p a b", a=8)
                for fc in range(8):
                    c.op("pe", lambda e: e.transpose(out=ptv[:, fc, :], in_=ot[blk % 4][:, fc // 4, (fc % 4) * 128:(fc % 4 + 1) * 128],
                                                     identity=self.ident[:]), r=["ot%d" % (blk % 4), "ident"], w=[pk])
                off = (blk % 4) * 128
                self.tt("dve", gT[i][:], ptv, gz[i4][:, :, off:off + 128], ALU.mult, r=[pk, "gz%d" % i4], w=["gT1_%d" % i])
                return [(gT[i][:, fc, :], "gT1_%d" % i) for fc in range(8)]
            return pref, prov
        self.out_proj_norm_res(1, self.w_out_cd, self.post1_rep, provider, self.h1src, self.out, "h1_%d", "h1_%d")


def core_inputs(inp, x_rows):
    m = {}
    m["x"] = np.ascontiguousarray(x_rows, dtype=np.float32)
    m["ident"] = np.eye(128, dtype=np.float32).astype(ml_dtypes.bfloat16)
    m["pre0"] = np.ascontiguousarray(inp["pre_norm"][0].reshape(8, 128).T)
    m["w_in_ab"] = inp["w_in_ab"][0]
    m["s5_lr"] = inp["s5_lambda_re"][0].T
    m["s5_li"] = inp["s5_lambda_im"][0].T
    m["s5_ldt"] = np.broadcast_to(inp["s5_log_dt"][0][None, :], (64, 32))
    m["s5_br"] = inp["s5_b_re"][0].transpose(1, 0, 2)
    m["s5_bi"] = inp["s5_b_im"][0].transpose(1, 0, 2)
    m["s5_cr"] = inp["s5_c_re"][0].transpose(2, 0, 1)
    m["s5_ci"] = inp["s5_c_im"][0].transpose(2, 0, 1)
    tv = np.array([0, -1, -2, -3, -4, -5, -6, -7, 1, 2, 3, 4, 5, 6, 7, 8], dtype=np.float32)
    m["tv"] = np.broadcast_to(tv[None, :], (64, 16))
    m["kv"] = np.broadcast_to(np.arange(256, dtype=np.float32)[None, :], (64, 256))
    sidx = np.arange(128) // 16
    m["toepmask"] = (sidx[None, :] >= sidx[:, None]).astype(np.float32)
    m["identf"] = np.eye(128, dtype=np.float32)
    m["s5d_rep"] = np.broadcast_to(inp["s5_d"][0][None, :], (128, 512))
    m["glub_rep"] = np.broadcast_to(inp["s5_glu_b"][0][None, :], (128, 512))
    m["glu_w"] = inp["s5_glu_w"][0]
    m["ml_cw"] = inp["ml_conv_w"][0].reshape(4, 4, 128).transpose(2, 1, 0)
    m["ml_cb"] = inp["ml_conv_b"][0].reshape(4, 128).T
    m["ml_mn"] = inp["ml_norm"][0].reshape(4, 128).T
    m["ml_msk"] = inp["ml_skip"][0].reshape(4, 128).T
    m["ml_gbi"] = inp["ml_gate_b"][0][0:4].reshape(4, 1)
    m["ml_gbf"] = inp["ml_gate_b"][0][4:8].reshape(4, 1)
    si = np.arange(128)
    m["ml_maskS"] = ((si[:, None] <= si[None, :]) * (128 ** -0.5)).astype(np.float32)
    m["ml_gw"] = inp["ml_gate_w"][0].reshape(12, 128, 8).transpose(1, 0, 2)
    m["ml_wq"] = inp["ml_wq"][0].transpose(1, 0, 2)
    m["ml_wk"] = inp["ml_wk"][0].transpose(1, 0, 2)
    m["ml_wv"] = inp["ml_wv"][0].transpose(1, 0, 2)
    m["w_out_ab"] = inp["w_out_ab"][0]
    m["pre1"] = np.ascontiguousarray(inp["pre_norm"][1].reshape(8, 128).T)
    m["w_in_cd"] = inp["w_in_cd"][0]
    pos = np.arange(S, dtype=np.float32)
    inv = (10000.0 ** (-np.arange(0, 64, 2, dtype=np.float32) / 64)).astype(np.float32)
    ang = pos[None, :] * inv[np.arange(128) % 32][:, None]
    m["rope_cos"] = np.cos(ang).astype(np.float32)
    m["rope_sin"] = np.sin(ang).astype(np.float32)
    rm = np.zeros((128, 128), dtype=np.float32)
    for mm in range(128):
        if mm % 64 < 32:
            rm[mm + 32, mm] = -1.0
        else:
            rm[mm - 32, mm] = 1.0
    m["rope_rm"] = rm.astype(ml_dtypes.bfloat16)
    kk = np.arange(128)[:, None]
    qq = np.arange(512)[None, :]
    dm = np.zeros((128, 9, 512), dtype=np.float32)
    fm = np.zeros((128, 4, 512), dtype=np.float32)
    for mi in range(9):
        d0 = mi - 3 if mi < 8 else 5
        dl = 128 * d0 + qq - kk
        mult = ((dl >= 0) & (dl <= 128)).astype(np.float32) + ((dl >= 0) & (dl % 4 == 0) & (dl <= 512)) + ((dl >= 0) & (dl % 16 == 0) & (dl <= 2048))
        dm[:, mi, :] = mult
        if mi < 4:
            fm[:, mi, :] = (dl >= 0)
    m["dil_masks"] = dm.astype(ml_dtypes.bfloat16)
    m["diff_masks"] = fm.astype(ml_dtypes.bfloat16)
    m["diff_lqk"] = np.stack([inp["diff_lq1"][0], inp["diff_lk1"][0], inp["diff_lq2"][0], inp["diff_lk2"][0]])[None]
    m["diffnorm_rep"] = np.broadcast_to(inp["diff_norm"][0][None, :], (128, 128))
    m["w_out_cd"] = inp["w_out_cd"][0]
    m["post1_rep"] = np.broadcast_to(inp["post_norm"][1][None, :], (128, 1024))
    m["post0_rep"] = np.broadcast_to(inp["post_norm"][0][None, :], (128, 1024))
    return m


_CACHE = {}


def kernel(**inputs):
    inp = {k_: np.asarray(v) for k_, v in inputs.items()}
    x = inp["x"]
    B = x.shape[0]
    nseq = B // NCORES
    if "prog" not in _CACHE:
        kb = K(nseq=nseq)
        kb.build()
        _CACHE["prog"] = kb
    kb = _CACHE["prog"]
    in_maps = []
    for ci in range(NCORES):
        m = core_inputs(inp, x[ci * nseq:(ci + 1) * nseq].reshape(-1, D))
        in_maps.append({n: np.ascontiguousarray(m[n]) for n in kb.inputs})
    res = run_bass_kernel_spmd(kb.nc, in_maps, core_ids=list(range(NCORES)))
    out = np.stack([np.asarray(res.results[ci]["out"]).reshape(nseq, S, D) for ci in range(NCORES)], axis=0)
    return out.reshape(B, S, D).astype(np.float32)
```

```python
import contextlib
import math
import numpy as np
import ml_dtypes
import concourse.bass as bass
import concourse.mybir as mybir
from concourse.bass_utils import run_bass_kernel_spmd

F32 = mybir.dt.float32
BF16 = mybir.dt.bfloat16
I32 = mybir.dt.int32
AF = mybir.ActivationFunctionType
ALU = mybir.AluOpType
AX = mybir.AxisListType

D = 1024
S = 2048
BR = 512
NCORES = 8
SAME_ENGINE_SYNC = True
NORM_EPS = 1e-6
HEAD_EPS = 1e-5


class Ctx:
    def __init__(self, nc, stack, n_dma_sems=48, same_engine_sync=SAME_ENGINE_SYNC):
        self.nc = nc
        self.eng = {"pe": nc.tensor, "act": nc.scalar, "dve": nc.vector,
                    "pool": nc.gpsimd, "sp": nc.sync}
        self.sem = {}
        self.cnt = {}
        for k in ("pe", "act", "dve", "pool"):
            self.sem[k] = stack.enter_context(nc.semaphore("s_" + k))
            self.cnt[k] = 0
        self.dma_sems = []
        for i in range(n_dma_sems):
            k = "dma%d" % i
            self.sem[k] = stack.enter_context(nc.semaphore("s_" + k))
            self.cnt[k] = 0
            self.dma_sems.append(k)
        self.dma_rr = 0
        self.waited = {k: {} for k in self.eng}
        self.last_w = {}
        self.readers = {}
        self.same_engine_sync = same_engine_sync
        self.n_instr = 0
        self.n_wait = 0

    def _deps(self, r, w):
        deps = []
        for x in r:
            if x in self.last_w:
                deps.append(self.last_w[x])
            if x.startswith("ps"):
                deps.extend(self.readers.get(x, ()))
        for x in w:
            if x in self.last_w:
                deps.append(self.last_w[x])
            deps.extend(self.readers.get(x, ()))
        return deps

    def _wait(self, e, deps):
        need = {}
        for (k, v) in deps:
            if k == e and (e == "pe" or not self.same_engine_sync):
                continue
            if need.get(k, 0) < v:
                need[k] = v
        for k, v in need.items():
            if self.waited[e].get(k, 0) >= v:
                continue
            self.eng[e].wait_ge(self.sem[k], v)
            self.waited[e][k] = v
            self.n_wait += 1

    def _commit(self, tok, r, w):
        for x in w:
            self.last_w[x] = tok
            self.readers[x] = []
        for x in r:
            if x in w:
                continue
            self.readers.setdefault(x, []).append(tok)

    def op(self, e, fn, r=(), w=()):
        self._wait(e, self._deps(r, w))
        ins = fn(self.eng[e])
        self.cnt[e] += 1
        ins.then_inc(self.sem[e], 1)
        self._commit((e, self.cnt[e]), r, w)
        self.n_instr += 1
        return ins

    def dma(self, out, in_, r=(), w=(), q="sp", **kw):
        k = self.dma_sems[self.dma_rr]
        self.dma_rr = (self.dma_rr + 1) % len(self.dma_sems)
        deps = self._deps(r, w)
        if self.cnt[k] > 0:
            deps.append((k, self.cnt[k]))
        self._wait(q, deps)
        ins = self.eng[q].dma_start(out=out, in_=in_, **kw)
        self.cnt[k] += 16
        ins.then_inc(self.sem[k], 16)
        self._commit((k, self.cnt[k]), r, w)
        self.n_instr += 1
        return ins

    def barrier(self):
        deps = [(k, v) for k, v in self.cnt.items() if v > 0]
        for e in self.eng:
            self._wait(e, deps)

    def finish(self, res):
        deps = [self.last_w[x] for x in res if x in self.last_w]
        self._wait("sp", deps)


class K:
    def __init__(self, nseq=2, export=(), phases=None, seqlen=S):
        self.nseq = nseq
        self.S = seqlen
        self.NT = nseq * seqlen
        self.export = set(export)
        self.phases = phases
        self.nc = bass.Bass("TRN2", target_bir_lowering=False)
        self.inputs = {}
        self.outputs = {}
        self.s5_main_enabled = True
        self._uid = 0

    def sbt(self, name, shape, dt):
        self._uid += 1
        return self.nc.sbuf_tensor("%s_u%d" % (name, self._uid), list(shape), dt)

    def din(self, name, shape, dt=F32):
        ap = self.nc.dram_tensor(name, list(shape), dt, kind="ExternalInput").ap()
        self.inputs[name] = ap
        return ap

    def dscr(self, name, shape, dt):
        kind = "ExternalOutput" if name in self.export else "Internal"
        ap = self.nc.dram_tensor(name, list(shape), dt, kind=kind).ap()
        if kind == "ExternalOutput":
            self.outputs[name] = ap
        return ap

    @contextlib.contextmanager
    def scope(self):
        with contextlib.ExitStack() as st:
            yield st
            self.c.barrier()

    def build(self):
        nc = self.nc
        NT = self.NT
        with contextlib.ExitStack() as st:
            self.c = Ctx(nc, st)
            self.ps = [st.enter_context(nc.psum_tensor("ps%d" % i, [128, 512], F32)) for i in range(8)]
            self.x = self.din("x", [NT, D])
            self.ident_d = self.din("ident", [128, 128], BF16)
            self.pre0 = self.din("pre0", [128, 8])
            self.w_in_ab = self.din("w_in_ab", [D, 4 * BR])
            for nm in ("s5_lr", "s5_li", "s5_ldt"):
                setattr(self, nm, self.din(nm, [64, 32]))
            for nm in ("s5_br", "s5_bi", "s5_cr", "s5_ci"):
                setattr(self, nm, self.din(nm, [64, 32, 16]))
            self.tv_d = self.din("tv", [64, 16])
            self.kv_d = self.din("kv", [64, 256])
            self.toepmask_d = self.din("toepmask", [128, 128])
            self.identf_d = self.din("identf", [128, 128])
            self.s5d_rep = self.din("s5d_rep", [128, BR])
            self.glub_rep = self.din("glub_rep", [128, BR])
            self.glu_w = self.din("glu_w", [BR, BR])
            self.ml_cw = self.din("ml_cw", [128, 4, 4]); self.ml_cb = self.din("ml_cb", [128, 4])
            self.ml_mn = self.din("ml_mn", [128, 4]); self.ml_msk = self.din("ml_msk", [128, 4])
            self.ml_gbi = self.din("ml_gbi", [4, 1]); self.ml_gbf = self.din("ml_gbf", [4, 1])
            self.ml_maskS = self.din("ml_maskS", [128, 128])
            self.ml_gw = self.din("ml_gw", [128, 12, 8])
            self.ml_wq = self.din("ml_wq", [128, 4, 128]); self.ml_wk = self.din("ml_wk", [128, 4, 128]); self.ml_wv = self.din("ml_wv", [128, 4, 128])
            self.bT = self.dscr("bT", [BR, NT], BF16)
            self.w_out_ab = self.din("w_out_ab", [D, D]); self.post0_rep = self.din("post0_rep", [128, D])
            self.out = self.nc.dram_tensor("out", [NT, D], F32, kind="ExternalOutput").ap()
            self.outputs["out"] = self.out
            self.h1src = self.out
            if self.phases is not None and "l0o" not in self.phases:
                self.h1src = self.din("h1_in", [NT, D])
            self.pre1 = self.din("pre1", [128, 8]); self.w_in_cd = self.din("w_in_cd", [D, 8 * BR])
            self.rope_cos = self.din("rope_cos", [128, self.S]); self.rope_sin = self.din("rope_sin", [128, self.S])
            self.rope_rm = self.din("rope_rm", [128, 128], BF16)
            for nm in ("cqT", "ckT", "dqT", "dkT", "czT", "dzT"):
                setattr(self, nm, self.dscr(nm, [BR, NT], BF16))
            for nm in ("cv_tok", "dv_tok", "oc_tok", "od_tok"):
                setattr(self, nm, self.dscr(nm, [NT, BR], BF16))
            self.dil_masks = self.din("dil_masks", [128, 9, 512], BF16); self.diff_masks = self.din("diff_masks", [128, 4, 512], BF16)
            self.diff_lqk = self.din("diff_lqk", [1, 4, 64]); self.diffnorm_rep = self.din("diffnorm_rep", [128, 128])
            self.w_out_cd = self.din("w_out_cd", [D, D]); self.post1_rep = self.din("post1_rep", [128, D])
            self.rotc = self.dscr("rotc", [64, 32, 256], F32)
            self.rots = self.dscr("rots", [64, 32, 256], F32)
            self.rhot = self.dscr("rhot", [64, 32, 256], F32)
            self.toep_x = self.dscr("toep_x", [128, 32, 128], BF16)
            self.wii_x = self.dscr("wii_x", [128, 32, 2, 64], BF16)
            self.wiv_x = self.dscr("wiv_x", [64, 2, 32, 128], BF16)
            self.a_tok = self.dscr("a_tok", [NT, BR], BF16)
            self.u_tok = self.dscr("u_tok", [NT, BR], BF16)
            self.sz_tok = self.dscr("sz_tok", [NT, BR], BF16)
            self.xmT = self.dscr("xmT", [BR, NT], BF16)
            self.mzT = self.dscr("mzT", [BR, NT], BF16)
            self.ident = st.enter_context(nc.sbuf_tensor("identb", [128, 128], BF16))
            self.c.dma(self.ident[:], self.ident_d, w=["ident"])
            ph = self.phases
            fin = []
            if ph is None or "l0p" in ph:
                self.phase_l0_proj()
                fin += ["u_tok", "sz_tok", "xmT", "mzT"]
            if ph is None or "s5" in ph:
                self.phase_s5()
                fin += ["a_tok", "rotc", "rots", "rhot", "toep_x", "wii_x", "wiv_x"]
            if ph is None or "ml" in ph:
                self.phase_ml()
                fin += ["bT"]
            if ph is None or "l0o" in ph:
                self.phase_l0_out()
                fin += ["h1_%d" % b for b in range(NT // 128)]
            if ph is None or "l1p" in ph:
                self.phase_l1_proj()
                fin += ["cqT", "ckT", "dqT", "dkT", "czT", "dzT", "cv_tok", "dv_tok"]
            if ph is None or "adil" in ph:
                self.phase_attn("dil")
                fin += ["oc_tok"]
            if ph is None or "adiff" in ph:
                self.phase_attn("diff")
                fin += ["od_tok"]
            if ph is None or "l1o" in ph:
                self.phase_l1_out()
                fin += ["h1_%d" % b for b in range(NT // 128)]
            self.c.finish(fin)
            self.c.barrier()
        return nc

    def rmsnorm_T(self, st, xsrc_rows, nblk, zT, zkey, tagp, ps_tr):
        raise NotImplementedError

    def phase_l0_proj(self):
        nc, c = self.nc, self.c
        NT = self.NT
        ngrp = NT // 512
        with self.scope() as st:
            T = lambda name, shape, dt: st.enter_context(self.sbt(name, shape, dt))
            win = T("win0", [128, 8, 4 * BR], BF16)
            g0 = T("g0", [128, 8], F32)
            stage = [T("wst%d" % i, [128, 4 * BR], F32) for i in range(2)]
            c.dma(g0[:], self.pre0, w=["g0"])
            for dc in range(8):
                sk = "wst%d" % (dc % 2)
                c.dma(stage[dc % 2][:], self.w_in_ab[dc * 128:(dc + 1) * 128, :], w=[sk])
                c.op("act", lambda e: e.activation(out=win[:, dc, :], in_=stage[dc % 2][:], func=AF.Copy,
                                                   scale=g0[:, dc:dc + 1]), r=[sk, "g0"], w=["win0"])
            NXB = 6
            xt = [T("xt%d" % i, [128, D], F32) for i in range(NXB)]
            junk = T("junk", [128, D], F32)
            ss2 = [T("ss%d" % i, [128, 4], F32) for i in range(2)]
            rstd2 = [T("rstd%d" % i, [128, 4], F32) for i in range(2)]
            zb = [T("zb%d" % i, [128, D], BF16) for i in range(2)]
            zT = [T("zT%d" % i, [128, 8, 512], BF16) for i in range(2)]
            uo = [T("uo%d" % i, [128, BR], BF16) for i in range(2)]
            so = [T("so%d" % i, [128, BR], BF16) for i in range(2)]
            xmo = [T("xmo%d" % i, [128, 4, 512], BF16) for i in range(2)]
            mzo = [T("mzo%d" % i, [128, 4, 512], BF16) for i in range(2)]
            nblk = NT // 128

            def load_x(b):
                if b < nblk:
                    c.dma(xt[b % NXB][:], self.x[b * 128:(b + 1) * 128, :], w=["xt%d" % (b % NXB)])
            for b in range(4):
                load_x(b)
            for g in range(ngrp):
                zk = "zT%d" % (g % 2)
                ss = ss2[g % 2]; rstd = rstd2[g % 2]
                ssk = "ss%d" % (g % 2); rsk = "rstd%d" % (g % 2)
                for j in range(4):
                    b = g * 4 + j
                    xk = "xt%d" % (b % NXB)
                    c.op("act", lambda e: e.activation(out=junk[:], in_=xt[b % NXB][:], func=AF.Square,
                                                       accum_out=ss[:, j:j + 1]), r=[xk], w=["junk", ssk])
                c.op("act", lambda e: e.activation(out=rstd[:], in_=ss[:], func=AF.Ln, scale=1.0 / D, bias=NORM_EPS),
                     r=[ssk], w=[rsk])
                c.op("act", lambda e: e.activation(out=rstd[:], in_=rstd[:], func=AF.Exp, scale=-0.5),
                     r=[rsk], w=[rsk])
                for j in range(4):
                    b = g * 4 + j
                    xk = "xt%d" % (b % NXB)
                    zbk = "zb%d" % (b % 2)
                    pst = self.ps[b % 2]
                    pk = "ps%d" % (b % 2)
                    c.op("dve", lambda e: e.tensor_scalar(out=zb[b % 2][:], in0=xt[b % NXB][:], scalar1=rstd[:, j:j + 1],
                                                          scalar2=None, op0=ALU.mult), r=[xk, rsk], w=[zbk])
                    load_x(b + 4)
                    ptv = pst[:].bitcast(BF16).rearrange("p (a b) -> p a b", a=8)
                    for dc in range(8):
                        c.op("pe", lambda e: e.transpose(out=ptv[:, dc, :], in_=zb[b % 2][:, dc * 128:(dc + 1) * 128],
                                                         identity=self.ident[:]), r=[zbk, "ident"], w=[pk])
                    c.op("dve", lambda e: e.tensor_copy(out=zT[g % 2][:, :, j * 128:(j + 1) * 128], in_=ptv),
                         r=[pk], w=[zk])
                    for dc in range(8):
                        c.op("pe", lambda e: e.matmul(out=self.ps[2][:], lhsT=zT[g % 2][:, dc, j * 128:(j + 1) * 128],
                                                      rhs=win[:, dc, 0:BR], start=(dc == 0), stop=(dc == 7)),
                             r=[zk, "win0"], w=["ps2"])
                    c.op("act", lambda e: e.copy(out=uo[b % 2][:], in_=self.ps[2][:]), r=["ps2"], w=["uo%d" % (b % 2)])
                    c.dma(self.u_tok[b * 128:(b + 1) * 128, :], uo[b % 2][:], r=["uo%d" % (b % 2)], w=["u_tok"])
                    for dc in range(8):
                        c.op("pe", lambda e: e.matmul(out=self.ps[3][:], lhsT=zT[g % 2][:, dc, j * 128:(j + 1) * 128],
                                                      rhs=win[:, dc, BR:2 * BR], start=(dc == 0), stop=(dc == 7)),
                             r=[zk, "win0"], w=["ps3"])
                    c.op("act", lambda e: e.activation(out=so[b % 2][:], in_=self.ps[3][:], func=AF.Silu),
                         r=["ps3"], w=["so%d" % (b % 2)])
                    c.dma(self.sz_tok[b * 128:(b + 1) * 128, :], so[b % 2][:], r=["so%d" % (b % 2)], w=["sz_tok"])
                for fc in range(8):
                    pb = 4 + fc % 4
                    for dc in range(8):
                        c.op("pe", lambda e: e.matmul(out=self.ps[pb][:], lhsT=win[:, dc, 2 * BR + fc * 128:2 * BR + (fc + 1) * 128],
                                                      rhs=zT[g % 2][:, dc, :], start=(dc == 0), stop=(dc == 7)),
                             r=[zk, "win0"], w=["ps%d" % pb])
                    if fc < 4:
                        c.op("dve", lambda e: e.tensor_copy(out=xmo[g % 2][:, fc, :], in_=self.ps[pb][:]),
                             r=["ps%d" % pb], w=["xmo%d" % (g % 2)])
                    else:
                        c.op("act", lambda e: e.activation(out=mzo[g % 2][:, fc - 4, :], in_=self.ps[pb][:], func=AF.Silu),
                             r=["ps%d" % pb], w=["mzo%d" % (g % 2)])
                c.dma(self.xmT.rearrange("(c p) t -> p c t", p=128)[:, :, g * 512:(g + 1) * 512], xmo[g % 2][:],
                      r=["xmo%d" % (g % 2)], w=["xmT"])
                c.dma(self.mzT.rearrange("(c p) t -> p c t", p=128)[:, :, g * 512:(g + 1) * 512], mzo[g % 2][:],
                      r=["mzo%d" % (g % 2)], w=["mzT"])
        c.barrier()

    def tt(self, e, out, a, b, op, r, w):
        return self.c.op(e, lambda en: en.tensor_tensor(out=out, in0=a, in1=b, op=op), r=r, w=w)

    def ts(self, e, out, a, s1, op0, r, w, s2=None, op1=None):
        if op1 is None:
            return self.c.op(e, lambda en: en.tensor_scalar(out=out, in0=a, scalar1=s1, scalar2=None, op0=op0), r=r, w=w)
        return self.c.op(e, lambda en: en.tensor_scalar(out=out, in0=a, scalar1=s1, scalar2=s2, op0=op0, op1=op1), r=r, w=w)

    def stt(self, e, out, a, s, b, op0, op1, r, w):
        return self.c.op(e, lambda en: en.scalar_tensor_tensor(out=out, in0=a, scalar=s, in1=b, op0=op0, op1=op1), r=r, w=w)

    def actf(self, out, in_, func, r, w, **kw):
        return self.c.op("act", lambda en: en.activation(out=out, in_=in_, func=func, **kw), r=r, w=w)

    def cp(self, e, out, in_, r, w):
        if e == "act":
            return self.c.op("act", lambda en: en.copy(out=out, in_=in_), r=r, w=w)
        return self.c.op(e, lambda en: en.tensor_copy(out=out, in_=in_), r=r, w=w)

    def sincos(self, ang, akey, sin_o, cos_o, skey, ckey, tf, ti, red_o=None):
        C1 = 6.28125
        C2 = 2 * math.pi - C1
        for (off, out, okey) in ((0.0, sin_o, skey), (math.pi / 2, cos_o, ckey)):
            if out is None:
                continue
            self.ts("dve", tf, ang, 1.0 / (2 * math.pi), ALU.mult, r=[akey], w=["sc_tf"], s2=off / (2 * math.pi), op1=ALU.add)
            self.cp("dve", ti, tf, r=["sc_tf"], w=["sc_ti"])
            self.cp("dve", tf, ti, r=["sc_ti"], w=["sc_tf"])
            self.stt("dve", out, tf, -C1, ang, ALU.mult, ALU.add, r=["sc_tf", akey], w=[okey])
            self.stt("dve", out, tf, -C2, out, ALU.mult, ALU.add, r=["sc_tf", okey], w=[okey])
            if off != 0.0:
                self.ts("dve", out, out, off, ALU.add, r=[okey], w=[okey])
            self.ts("dve", out, out, math.pi, ALU.min, r=[okey], w=[okey], s2=-math.pi, op1=ALU.max)
            if red_o is not None and off == 0.0:
                self.cp("dve", red_o[0], out, r=[okey], w=[red_o[1]])
            self.actf(out, out, AF.Sin, r=[okey], w=[okey])

    def s5_precompute(self, st, Toep, Wii, WivR, WivI):
        nc, c = self.nc, self.c
        so = contextlib.ExitStack()
        TO = lambda name, shape, dt=F32: so.enter_context(self.sbt(name, shape, dt))
        thr = TO("p_thr", [64, 32]); rho8 = TO("p_rho8", [64, 32]); kv = TO("p_kv", [64, 256])
        tfs = TO("p_tfs", [64, 32]); tis = TO("p_tis", [64, 32], I32)
        with self.scope() as sp:
            T = lambda name, shape, dt=F32: sp.enter_context(self.sbt(name, shape, dt))
            lr = T("p_lr", [64, 32]); li = T("p_li", [64, 32]); ldt = T("p_ldt", [64, 32])
            br = T("p_br", [64, 32, 16]); bi = T("p_bi", [64, 32, 16])
            cr = T("p_cr", [64, 32, 16]); ci = T("p_ci", [64, 32, 16])
            tv = T("p_tv", [64, 16])
            msk = T("p_msk", [128, 128]); idf = T("p_idf", [128, 128])
            for (t, d, key) in ((lr, self.s5_lr, "p_lr"), (li, self.s5_li, "p_li"), (ldt, self.s5_ldt, "p_ldt"),
                                (br, self.s5_br, "p_br"), (bi, self.s5_bi, "p_bi"), (cr, self.s5_cr, "p_cr"),
                                (ci, self.s5_ci, "p_ci"), (tv, self.tv_d, "p_tv"), (kv, self.kv_d, "p_kv"),
                                (msk, self.toepmask_d, "p_msk"), (idf, self.identf_d, "p_idf")):
                c.dma(t[:], d, w=[key])
            dt = T("p_dt", [64, 32]); lrdt = T("p_lrdt", [64, 32]); th = T("p_th", [64, 32])
            s0 = T("p_s0", [64, 32]); c0 = T("p_c0", [64, 32]); mag = T("p_mag", [64, 32])
            self.actf(dt[:], ldt[:], AF.Exp, r=["p_ldt"], w=["p_dt"])
            self.tt("dve", lrdt[:], lr[:], dt[:], ALU.mult, r=["p_lr", "p_dt"], w=["p_lrdt"])
            self.tt("dve", th[:], li[:], dt[:], ALU.mult, r=["p_li", "p_dt"], w=["p_th"])
            self.sincos(th[:], "p_th", s0[:], c0[:], "p_s0", "p_c0", tfs[:], tis[:], red_o=(thr[:], "p_thr"))
            self.actf(mag[:], lrdt[:], AF.Exp, r=["p_lrdt"], w=["p_mag"])
            abr = T("p_abr", [64, 32]); abi = T("p_abi", [64, 32]); am1 = T("p_am1", [64, 32])
            self.tt("dve", abr[:], mag[:], c0[:], ALU.mult, r=["p_mag", "p_c0"], w=["p_abr"])
            self.tt("dve", abi[:], mag[:], s0[:], ALU.mult, r=["p_mag", "p_s0"], w=["p_abi"])
            self.ts("dve", am1[:], abr[:], -1.0, ALU.add, r=["p_abr"], w=["p_am1"])
            den = T("p_den", [64, 32]); t1 = T("p_t1", [64, 32]); t2 = T("p_t2", [64, 32])
            fr = T("p_fr", [64, 32]); fi = T("p_fi", [64, 32])
            self.tt("dve", den[:], lr[:], lr[:], ALU.mult, r=["p_lr"], w=["p_den"])
            self.tt("dve", t1[:], li[:], li[:], ALU.mult, r=["p_li"], w=["p_t1"])
            self.tt("dve", den[:], den[:], t1[:], ALU.add, r=["p_den", "p_t1"], w=["p_den"])
            c.op("dve", lambda e: e.reciprocal(out=den[:], in_=den[:]), r=["p_den"], w=["p_den"])
            self.tt("dve", t1[:], am1[:], lr[:], ALU.mult, r=["p_am1", "p_lr"], w=["p_t1"])
            self.tt("dve", t2[:], abi[:], li[:], ALU.mult, r=["p_abi", "p_li"], w=["p_t2"])
            self.tt("dve", t1[:], t1[:], t2[:], ALU.add, r=["p_t1", "p_t2"], w=["p_t1"])
            self.tt("dve", fr[:], t1[:], den[:], ALU.mult, r=["p_t1", "p_den"], w=["p_fr"])
            self.tt("dve", t1[:], abi[:], lr[:], ALU.mult, r=["p_abi", "p_lr"], w=["p_t1"])
            self.tt("dve", t2[:], am1[:], li[:], ALU.mult, r=["p_am1", "p_li"], w=["p_t2"])
            self.tt("dve", t1[:], t1[:], t2[:], ALU.subtract, r=["p_t1", "p_t2"], w=["p_t1"])
            self.tt("dve", fi[:], t1[:], den[:], ALU.mult, r=["p_t1", "p_den"], w=["p_fi"])
            Bbr = T("p_Bbr", [64, 32, 16]); Bbi = T("p_Bbi", [64, 32, 16])
            u1 = T("p_u1", [64, 32, 16]); u2 = T("p_u2", [64, 32, 16])
            bc16 = lambda a: a.unsqueeze(2).broadcast_to([64, 32, 16])
            self.tt("dve", u1[:], br[:], bc16(fr[:]), ALU.mult, r=["p_br", "p_fr"], w=["p_u1"])
            self.tt("dve", u2[:], bi[:], bc16(fi[:]), ALU.mult, r=["p_bi", "p_fi"], w=["p_u2"])
            self.tt("dve", Bbr[:], u1[:], u2[:], ALU.subtract, r=["p_u1", "p_u2"], w=["p_Bbr"])
            self.tt("dve", u1[:], bi[:], bc16(fr[:]), ALU.mult, r=["p_bi", "p_fr"], w=["p_u1"])
            self.tt("dve", u2[:], br[:], bc16(fi[:]), ALU.mult, r=["p_br", "p_fi"], w=["p_u2"])
            self.tt("dve", Bbi[:], u1[:], u2[:], ALU.add, r=["p_u1", "p_u2"], w=["p_Bbi"])
            TE = T("p_TE", [64, 16, 32]); TA = T("p_TA", [64, 16, 32])
            PWr = T("p_PWr", [64, 16, 32]); PWi = T("p_PWi", [64, 16, 32])
            tf3 = T("p_tf3", [64, 16, 32]); ti3 = T("p_ti3", [64, 16, 32], I32)
            bt = lambda a: a.unsqueeze(1).broadcast_to([64, 16, 32])
            bg = lambda a: a.unsqueeze(2).broadcast_to([64, 16, 32])
            self.tt("dve", TE[:], bt(lrdt[:]), bg(tv[:]), ALU.mult, r=["p_lrdt", "p_tv"], w=["p_TE"])
            self.actf(TE[:], TE[:], AF.Exp, r=["p_TE"], w=["p_TE"])
            self.tt("dve", TA[:], bt(thr[:]), bg(tv[:]), ALU.mult, r=["p_thr", "p_tv"], w=["p_TA"])
            self.sincos(TA[:], "p_TA", PWi[:], PWr[:], "p_PWi", "p_PWr", tf3[:], ti3[:])
            self.tt("dve", PWr[:], PWr[:], TE[:], ALU.mult, r=["p_PWr", "p_TE"], w=["p_PWr"])
            self.tt("dve", PWi[:], PWi[:], TE[:], ALU.mult, r=["p_PWi", "p_TE"], w=["p_PWi"])
            HsR = T("p_HsR", [64, 32, 8, 16]); HsI = T("p_HsI", [64, 32, 8, 16])
            v1 = T("p_v1", [64, 32, 9, 16]); v2 = T("p_v2", [64, 32, 9, 16])
            def pw_b(PW, j0, n):
                return PW[:, j0:j0 + n, :].rearrange("p t g -> p g t").unsqueeze(3).broadcast_to([64, 32, n, 16])
            def x_b(x, n):
                return x.unsqueeze(2).broadcast_to([64, 32, n, 16])
            self.tt("dve", v1[:, :, 0:8, :], pw_b(PWr, 0, 8), x_b(Bbr[:], 8), ALU.mult, r=["p_PWr", "p_Bbr"], w=["p_v1"])
            self.tt("dve", v2[:, :, 0:8, :], pw_b(PWi, 0, 8), x_b(Bbi[:], 8), ALU.mult, r=["p_PWi", "p_Bbi"], w=["p_v2"])
            self.tt("dve", HsR[:], v1[:, :, 0:8, :], v2[:, :, 0:8, :], ALU.subtract, r=["p_v1", "p_v2"], w=["p_HsR"])
            self.tt("dve", v1[:, :, 0:8, :], pw_b(PWr, 0, 8), x_b(Bbi[:], 8), ALU.mult, r=["p_PWr", "p_Bbi"], w=["p_v1"])
            self.tt("dve", v2[:, :, 0:8, :], pw_b(PWi, 0, 8), x_b(Bbr[:], 8), ALU.mult, r=["p_PWi", "p_Bbr"], w=["p_v2"])
            self.tt("dve", HsI[:], v1[:, :, 0:8, :], v2[:, :, 0:8, :], ALU.add, r=["p_v1", "p_v2"], w=["p_HsI"])
            LtR = T("p_LtR", [64, 32, 9, 16]); nLtI = T("p_nLtI", [64, 32, 9, 16])
            for (s0_, j0, n) in ((0, 0, 1), (1, 8, 8)):
                sl = slice(s0_, s0_ + n)
                self.tt("dve", v1[:, :, sl, :], pw_b(PWr, j0, n), x_b(cr[:], n), ALU.mult, r=["p_PWr", "p_cr"], w=["p_v1"])
                self.tt("dve", v2[:, :, sl, :], pw_b(PWi, j0, n), x_b(ci[:], n), ALU.mult, r=["p_PWi", "p_ci"], w=["p_v2"])
                self.tt("dve", LtR[:, :, sl, :], v1[:, :, sl, :], v2[:, :, sl, :], ALU.subtract, r=["p_v1", "p_v2"], w=["p_LtR"])
                self.tt("dve", v1[:, :, sl, :], pw_b(PWi, j0, n), x_b(cr[:], n), ALU.mult, r=["p_PWi", "p_cr"], w=["p_v1"])
                self.tt("dve", v2[:, :, sl, :], pw_b(PWr, j0, n), x_b(ci[:], n), ALU.mult, r=["p_PWr", "p_ci"], w=["p_v2"])
                self.tt("dve", v1[:, :, sl, :], v1[:, :, sl, :], v2[:, :, sl, :], ALU.add, r=["p_v1", "p_v2"], w=["p_v1"])
                self.ts("dve", nLtI[:, :, sl, :], v1[:, :, sl, :], -1.0, ALU.mult, r=["p_v1"], w=["p_nLtI"])
            self.cp("dve", WivR[:], LtR[:, :, 1:9, :].rearrange("p g t c -> p g (t c)"), r=["p_LtR"], w=["WivR"])
            self.cp("dve", WivI[:], nLtI[:, :, 1:9, :].rearrange("p g t c -> p g (t c)"), r=["p_nLtI"], w=["WivI"])
            for g4 in range(8):
                pb = g4 % 2
                pk = "ps%d" % pb
                for gl in range(4):
                    g = g4 * 4 + gl
                    o = self.ps[pb][:, gl * 128:(gl + 1) * 128]
                    c.op("pe", lambda e: e.matmul(out=o, lhsT=HsR[:, g, :, :].rearrange("p s c -> p (s c)"),
                                                  rhs=LtR[:, g, 0:8, :].rearrange("p t c -> p (t c)"), start=True, stop=False),
                         r=["p_HsR", "p_LtR"], w=[pk])
                    c.op("pe", lambda e: e.matmul(out=o, lhsT=HsI[:, g, :, :].rearrange("p s c -> p (s c)"),
                                                  rhs=nLtI[:, g, 0:8, :].rearrange("p t c -> p (t c)"), start=False, stop=True),
                         r=["p_HsI", "p_nLtI"], w=[pk])
                self.tt("dve", Toep[:, g4 * 4:(g4 + 1) * 4, :], self.ps[pb][:].rearrange("p (g n) -> p g n", g=4),
                        msk[:].unsqueeze(1).broadcast_to([128, 4, 128]), ALU.mult, r=[pk, "p_msk"], w=["Toep"])
            GsR = LtR[:, :, 0:8, :].rearrange("p g t c -> p g (t c)")
            GsI = nLtI[:, :, 0:8, :].rearrange("p g t c -> p g (t c)")
            w1 = v1[:, :, 0:8, :].rearrange("p g t c -> p g (t c)")
            w2 = v2[:, :, 0:8, :].rearrange("p g t c -> p g (t c)")
            p7r = PWr[:, 14, :].unsqueeze(2).broadcast_to([64, 32, 128])
            p7i = PWi[:, 14, :].unsqueeze(2).broadcast_to([64, 32, 128])
            hr = HsR[:].rearrange("p g s c -> p g (s c)"); hi = HsI[:].rearrange("p g s c -> p g (s c)")
            self.tt("dve", w1, hr, p7r, ALU.mult, r=["p_HsR", "p_PWr"], w=["p_v1"])
            self.tt("dve", w2, hi, p7i, ALU.mult, r=["p_HsI", "p_PWi"], w=["p_v2"])
            self.tt("dve", GsR, w1, w2, ALU.subtract, r=["p_v1", "p_v2"], w=["p_LtR"])
            self.tt("dve", w1, hi, p7r, ALU.mult, r=["p_HsI", "p_PWr"], w=["p_v1"])
            self.tt("dve", w2, hr, p7i, ALU.mult, r=["p_HsR", "p_PWi"], w=["p_v2"])
            self.tt("dve", GsI, w1, w2, ALU.add, r=["p_v1", "p_v2"], w=["p_nLtI"])
            for g4 in range(8):
                pb = 2 + g4 % 2
                pk = "ps%d" % pb
                pv = self.ps[pb][:].rearrange("p (g r n) -> p g r n", g=4, r=2)
                for gl in range(4):
                    g = g4 * 4 + gl
                    c.op("pe", lambda e: e.transpose(out=pv[:, gl, 0, :], in_=GsR[:, g, :], identity=idf[0:64, 0:64]),
                         r=["p_LtR", "p_idf"], w=[pk])
                    c.op("pe", lambda e: e.transpose(out=pv[:, gl, 1, :], in_=GsI[:, g, :], identity=idf[0:64, 0:64]),
                         r=["p_nLtI", "p_idf"], w=[pk])
                self.cp("act", Wii[:, g4 * 4:(g4 + 1) * 4, :, :], pv, r=[pk], w=["Wii"])
            self.cp("dve", rho8[:], TE[:, 15, :], r=["p_TE"], w=["p_rho8"])
            if "toep_x" in self.export:
                c.dma(self.toep_x, Toep[:], r=["Toep"], w=["toep_x"])
                c.dma(self.wii_x, Wii[:], r=["Wii"], w=["wii_x"])
                c.dma(self.wiv_x[:, 0], WivR[:], r=["WivR"], w=["wiv_x"])
                c.dma(self.wiv_x[:, 1], WivI[:], r=["WivI"], w=["wiv_x"])
        c.barrier()
        with self.scope() as sp:
            T = lambda name, shape, dt=F32: sp.enter_context(self.sbt(name, shape, dt))
            phr = T("p_phr", [64, 32]); ph_s = T("p_phs", [64, 32]); phr2 = T("p_phr2", [64, 32])
            self.ts("dve", phr[:], thr[:], 8.0, ALU.mult, r=["p_thr"], w=["p_phr"])
            self.sincos(phr[:], "p_phr", ph_s[:], None, "p_phs", None, tfs[:], tis[:], red_o=(phr2[:], "p_phr2"))
            rho = T("p_rho", [64, 8, 256])
            ang = T("p_ang", [64, 8, 256]); sk = T("p_sk", [64, 8, 256]); ck = T("p_ck", [64, 8, 256])
            tf4 = T("p_tf4", [64, 8, 256]); ti4 = T("p_ti4", [64, 8, 256], I32)
            for gb in range(4):
                gs = slice(gb * 8, (gb + 1) * 8)
                self.cp("dve", rho[:], rho8[:, gs].unsqueeze(2).broadcast_to([64, 8, 256]), r=["p_rho8"], w=["p_rho"])
                c.op("dve", lambda e: e.memset(rho[:, :, 0:1], 0.0), r=[], w=["p_rho"])
                c.dma(self.rhot[:, gs, :], rho[:], r=["p_rho"], w=["rhot"])
                self.tt("dve", ang[:], phr2[:, gs].unsqueeze(2).broadcast_to([64, 8, 256]),
                        kv[:].unsqueeze(1).broadcast_to([64, 8, 256]), ALU.mult, r=["p_phr2", "p_kv"], w=["p_ang"])
                self.sincos(ang[:], "p_ang", sk[:], ck[:], "p_sk", "p_ck", tf4[:], ti4[:])
                c.dma(self.rots[:, gs, :], sk[:], r=["p_sk"], w=["rots"])
                c.dma(self.rotc[:, gs, :], ck[:], r=["p_ck"], w=["rotc"])
        c.barrier()
        so.close()

    def phase_s5(self):
        nc, c = self.nc, self.c
        with self.scope() as st:
            T = lambda name, shape, dt=F32: st.enter_context(self.sbt(name, shape, dt))
            Toep = T("Toep", [128, 32, 128], BF16)
            Wii = T("Wii", [128, 32, 2, 64], BF16)
            WivR = T("WivR", [64, 32, 128], BF16)
            WivI = T("WivI", [64, 32, 128], BF16)
            self.s5_precompute(st, Toep, Wii, WivR, WivI)
            if self.s5_main_enabled:
                self.s5_main(st, Toep, Wii, WivR, WivI)
        c.barrier()

    def s5_main(self, st0, Toep, Wii, WivR, WivI):
        nc, c = self.nc, self.c
        GB = 4
        with self.scope() as st:
            T = lambda name, shape, dt=F32: st.enter_context(self.sbt(name, shape, dt))
            gluw = T("gluw", [128, 4, BR], BF16)
            with self.scope() as sg:
                gst = sg.enter_context(self.sbt("gluw_st", [128, 4, BR], F32))
                c.dma(gst[:], self.glu_w.rearrange("(c p) n -> p c n", p=128), w=["gluw_st"])
                self.cp("dve", gluw[:], gst[:], r=["gluw_st"], w=["gluw"])
            Drep = T("Drep", [128, BR]); Brep = T("Brep", [128, BR])
            c.dma(Drep[:], self.s5d_rep, w=["Drep"])
            c.dma(Brep[:], self.glub_rep, w=["Brep"])
            for q in range(self.nseq):
                tb = q * self.S
                with self.scope() as sq:
                    TQ = lambda name, shape, dt=F32: sq.enter_context(self.sbt(name, shape, dt))
                    uck = [TQ("uck%d" % i, [128, 8 * BR], BF16) for i in range(2)]
                    szck = [TQ("szck%d" % i, [128, 8 * BR], BF16) for i in range(2)]
                    yck = [TQ("yck%d" % i, [128, 8, BR], BF16) for i in range(2)]
                    for kh in range(2):
                        rows = slice(tb + kh * 1024, tb + (kh + 1) * 1024)
                        c.dma(uck[kh][:], self.u_tok[rows, :].rearrange("(k t) f -> k (t f)", t=8), r=["u_tok"], w=["uck%d" % kh])
                        c.dma(szck[kh][:], self.sz_tok[rows, :].rearrange("(k t) f -> k (t f)", t=8), r=["sz_tok"], w=["szck%d" % kh])
                    with self.scope() as ss:
                        TS = lambda name, shape, dt=F32: ss.enter_context(self.sbt(name, shape, dt))
                        Ug = TS("Ug", [128, 32, 256], BF16)
                        Vb2 = [TS("Vb%d" % i, [64, 2, GB, 256]) for i in range(2)]
                        Wb2 = [TS("Wb%d" % i, [64, 2, GB, 256]) for i in range(2)]
                        tA2 = [TS("tA%d" % i, [64, GB, 256]) for i in range(2)]; tB2 = [TS("tB%d" % i, [64, GB, 256]) for i in range(2)]
                        tC2 = [TS("tC0", [64, GB, 256])] * 2; tD2 = [TS("tD0", [64, GB, 256])] * 2
                        ck2 = [TS("ck%d" % i, [64, GB, 256]) for i in range(2)]; sk2 = [TS("sk%d" % i, [64, GB, 256]) for i in range(2)]
                        rh2 = [TS("rh%d" % i, [64, GB, 256]) for i in range(2)]
                        Xs2 = [TS("Xs%d" % i, [64, 2, GB, 256], BF16) for i in range(2)]
                        Ysb = TS("Ysb", [128, 8, 256], BF16)
                        for i in range(2):
                            c.op("pool", lambda e: e.memset(Xs2[i][:], 0.0), w=["Xs%d" % i])

                        def load_tabs(b):
                            if b < 32 // GB:
                                gs_ = slice(b * GB, (b + 1) * GB)
                                c.dma(ck2[b % 2][:], self.rotc[:, gs_, :], r=["rotc"], w=["ck%d" % (b % 2)])
                                c.dma(sk2[b % 2][:], self.rots[:, gs_, :], r=["rots"], w=["sk%d" % (b % 2)])
                                c.dma(rh2[b % 2][:], self.rhot[:, gs_, :], r=["rhot"], w=["rh%d" % (b % 2)])
                        load_tabs(0)
                        ucg = TS("ucg", [128, 32, 128], BF16)
                        for kh in range(2):
                            self.cp("pool" if kh == 0 else "dve", ucg[:].rearrange("p g (s c) -> p g s c", s=8),
                                    uck[kh][:].rearrange("p (s g c) -> p g s c", s=8, g=32), r=["uck%d" % kh], w=["ucg"])
                            for g8 in range(4):
                                pb = g8 % 2
                                pk = "ps%d" % pb
                                ptv = self.ps[pb][:].bitcast(BF16).rearrange("p (g k) -> p g k", g=8)
                                for gl in range(8):
                                    g = g8 * 8 + gl
                                    c.op("pe", lambda e: e.transpose(out=ptv[:, gl, :], in_=ucg[:, g, :],
                                                                     identity=self.ident[:]), r=["ucg", "ident"], w=[pk])
                                self.cp("dve" if g8 % 2 == 0 else "act", Ug[:, g8 * 8:(g8 + 1) * 8, kh * 128:(kh + 1) * 128], ptv,
                                        r=[pk], w=["Ug"])
                        for b in range(32 // GB):
                            gs = slice(b * GB, (b + 1) * GB)
                            bp = b % 2
                            Vb, Wb, tA, tB, ck, sk, rh, Xs = Vb2[bp], Wb2[bp], tA2[bp], tB2[bp], ck2[bp], sk2[bp], rh2[bp], Xs2[bp]
                            tC, tD = tC2[bp], tD2[bp]
                            ktC, ktD = "tC0", "tD0"
                            kVb, kW0, kW1, ktA, ktB, kck, ksk, krh, kXs = ("Vb%d" % bp, "Wb0_%d" % bp, "Wb1_%d" % bp, "tA%d" % bp, "tB%d" % bp,
                                                                           "ck%d" % bp, "sk%d" % bp, "rh%d" % bp, "Xs%d" % bp)
                            load_tabs(b + 1)
                            for gl in range(GB):
                                g = b * GB + gl
                                pb = 2 + gl % 2
                                pk = "ps%d" % pb
                                c.op("pe", lambda e: e.matmul(out=self.ps[pb][0:64, 0:256], lhsT=Wii[:, g, 0, :], rhs=Ug[:, g, :],
                                                              start=True, stop=True), r=["Wii", "Ug"], w=[pk])
                                c.op("pe", lambda e: e.matmul(out=self.ps[pb][0:64, 256:512], lhsT=Wii[:, g, 1, :], rhs=Ug[:, g, :],
                                                              start=True, stop=True), r=["Wii", "Ug"], w=[pk])
                                self.cp("act", Vb[:, :, gl, :], self.ps[pb][0:64, :].rearrange("p (r k) -> p r k", r=2), r=[pk], w=[kVb])
                            self.tt("pool", tB[:], sk[:], Vb[:, 1], ALU.mult, r=[ksk, kVb], w=[ktB])
                            self.tt("dve", tA[:], ck[:], Vb[:, 0], ALU.mult, r=[kck, kVb], w=[ktA])
                            self.tt("dve", tC[:], ck[:], Vb[:, 1], ALU.mult, r=[kck, kVb], w=[ktC])
                            self.tt("dve", tD[:], sk[:], Vb[:, 0], ALU.mult, r=[ksk, kVb], w=[ktD])
                            self.tt("dve", Wb[:, 1], tC[:], tD[:], ALU.subtract, r=[ktC, ktD], w=[kW1])
                            self.tt("dve", Wb[:, 0], tA[:], tB[:], ALU.add, r=[ktA, ktB], w=[kW0])
                            fl = lambda a: a.rearrange("p g k -> p (g k)")
                            c.op("dve", lambda e: e.tensor_tensor_scan(out=fl(Vb[:, 1]), data0=fl(rh[:]), data1=fl(Wb[:, 1]), initial=0.0,
                                                                       op0=ALU.mult, op1=ALU.add), r=[krh, kW1, kVb], w=[kVb])
                            c.op("dve", lambda e: e.tensor_tensor_scan(out=fl(Vb[:, 0]), data0=fl(rh[:]), data1=fl(Wb[:, 0]), initial=0.0,
                                                                       op0=ALU.mult, op1=ALU.add), r=[krh, kW0, kVb], w=[kVb])
                            K1 = 255
                            self.tt("pool", tB[:, :, 0:K1], sk[:, :, 0:K1], Vb[:, 1, :, 0:K1], ALU.mult, r=[ksk, kVb], w=[ktB])
                            self.tt("dve", tA[:, :, 0:K1], ck[:, :, 0:K1], Vb[:, 0, :, 0:K1], ALU.mult, r=[kck, kVb], w=[ktA])
                            self.tt("dve", tC[:, :, 0:K1], ck[:, :, 0:K1], Vb[:, 1, :, 0:K1], ALU.mult, r=[kck, kVb], w=[ktC])
                            self.tt("dve", tD[:, :, 0:K1], sk[:, :, 0:K1], Vb[:, 0, :, 0:K1], ALU.mult, r=[ksk, kVb], w=[ktD])
                            self.tt("dve", Xs[:, 1, :, 1:256], tC[:, :, 0:K1], tD[:, :, 0:K1], ALU.add, r=[ktC, ktD], w=[kXs])
                            self.tt("dve", Xs[:, 0, :, 1:256], tA[:, :, 0:K1], tB[:, :, 0:K1], ALU.subtract, r=[ktA, ktB], w=[kXs])
                            for gl in range(GB):
                                g = b * GB + gl
                                pb = 4 + gl // 2 % 2
                                pk = "ps%d" % pb
                                o = self.ps[pb][:, (gl % 2) * 256:(gl % 2 + 1) * 256]
                                c.op("pe", lambda e: e.matmul(out=o, lhsT=Toep[:, g, :], rhs=Ug[:, g, :], start=True, stop=False),
                                     r=["Toep", "Ug"], w=[pk])
                                c.op("pe", lambda e: e.matmul(out=o, lhsT=WivR[:, g, :], rhs=Xs[:, 0, gl, :], start=False, stop=False),
                                     r=["WivR", kXs], w=[pk])
                                c.op("pe", lambda e: e.matmul(out=o, lhsT=WivI[:, g, :], rhs=Xs[:, 1, gl, :], start=False, stop=True),
                                     r=["WivI", kXs], w=[pk])
                                if gl % 2 == 1:
                                    g8l = (b * GB + gl - 1) % 8
                                    self.cp("act", Ysb[:, g8l:g8l + 2, :], self.ps[pb][:].rearrange("p (g k) -> p g k", g=2), r=[pk], w=["Ysb"])
                            if (b * GB + GB) % 8 == 0:
                                g8 = (b * GB) // 8
                                for kh in range(2):
                                    pb = 6 + kh
                                    pk = "ps%d" % pb
                                    ptv = self.ps[pb][:].bitcast(BF16).rearrange("p (g n) -> p g n", g=8)
                                    for gl in range(8):
                                        c.op("pe", lambda e: e.transpose(out=ptv[:, gl, :], in_=Ysb[:, gl, kh * 128:(kh + 1) * 128],
                                                                         identity=self.ident[:]), r=["Ysb", "ident"], w=[pk])
                                    self.cp("dve", yck[kh][:, :, g8 * 128:(g8 + 1) * 128].rearrange("p t (g c) -> p t g c", g=8),
                                            ptv.rearrange("p g (t c) -> p t g c", t=8), r=[pk], w=["yck%d" % kh])
                    with self.scope() as se:
                        TE_ = lambda name, shape, dt=F32: se.enter_context(self.sbt(name, shape, dt))
                        t1 = TE_("e_t1", [128, 8, BR]); t2 = TE_("e_t2", [128, 8, BR])
                        gck = TE_("gck", [128, 8, BR], BF16)
                        ack = TE_("ack", [128, 8, BR], BF16)
                        gT = [TE_("gT%d" % i, [128, 4, 128], BF16) for i in range(2)]
                        e1 = [TE_("e1_%d" % i, [128, BR]) for i in range(2)]
                        for kh in range(2):
                            uv = uck[kh][:].rearrange("p (s f) -> p s f", s=8)
                            zv = szck[kh][:].rearrange("p (s f) -> p s f", s=8)
                            self.tt("dve", t1[:], uv, Drep[:].unsqueeze(1).broadcast_to([128, 8, BR]), ALU.mult, r=["uck%d" % kh, "Drep"], w=["e_t1"])
                            self.tt("dve", t1[:], t1[:], yck[kh][:], ALU.add, r=["e_t1", "yck%d" % kh], w=["e_t1"])
                            self.actf(t2[:], t1[:], AF.Square, r=["e_t1"], w=["e_t2"])
                            self.actf(t2[:], t2[:], AF.Copy, r=["e_t2"], w=["e_t2"], scale=0.044715 * 0.7978845608, bias=0.7978845608)
                            self.tt("dve", t2[:], t2[:], t1[:], ALU.mult, r=["e_t2", "e_t1"], w=["e_t2"])
                            self.actf(t2[:], t2[:], AF.Tanh, r=["e_t2"], w=["e_t2"])
                            self.actf(t2[:], t2[:], AF.Copy, r=["e_t2"], w=["e_t2"], scale=0.5, bias=0.5)
                            self.tt("dve", gck[:], t2[:], t1[:], ALU.mult, r=["e_t2", "e_t1"], w=["gck"])
                            def glu_front(tau):
                                i2 = tau % 2
                                pk = "ps%d" % i2
                                ptv = self.ps[i2][:].bitcast(BF16).rearrange("p (a b) -> p a b", a=8)
                                for fc in range(4):
                                    c.op("pe", lambda e: e.transpose(out=ptv[:, fc, :], in_=gck[:, tau, fc * 128:(fc + 1) * 128],
                                                                     identity=self.ident[:]), r=["gck", "ident"], w=[pk])
                                self.cp("act", gT[i2][:], ptv[:, 0:4, :], r=[pk], w=["gT%d" % i2])
                            glu_front(0)
                            for tau in range(8):
                                i2 = tau % 2
                                pm = 2 + i2
                                for fc in range(4):
                                    c.op("pe", lambda e: e.matmul(out=self.ps[pm][:], lhsT=gT[i2][:, fc, :], rhs=gluw[:, fc, :],
                                                                  start=(fc == 0), stop=(fc == 3)), r=["gT%d" % i2, "gluw"], w=["ps%d" % pm])
                                if tau + 1 < 8:
                                    glu_front(tau + 1)
                                ek = "e1_%d" % i2
                                self.tt("dve", e1[i2][:], self.ps[pm][:], Brep[:], ALU.add, r=["ps%d" % pm, "Brep"], w=[ek])
                                self.actf(e1[i2][:], e1[i2][:], AF.Tanh, r=[ek], w=[ek], scale=0.5)
                                self.ts("dve", e1[i2][:], e1[i2][:], 0.5, ALU.mult, r=[ek], w=[ek], s2=0.5, op1=ALU.add)
                                self.tt("pool", e1[i2][:], e1[i2][:], gck[:, tau, :], ALU.mult, r=[ek, "gck"], w=[ek])
                                self.tt("dve", ack[:, tau, :], e1[i2][:], zv[:, tau, :], ALU.mult, r=[ek, "szck%d" % kh], w=["ack"])
                            rows = slice(tb + kh * 1024, tb + (kh + 1) * 1024)
                            c.dma(self.a_tok[rows, :].rearrange("(k t) f -> k (t f)", t=8), ack[:].rearrange("p t f -> p (t f)"),
                                  r=["ack"], w=["a_tok"])

    def phase_ml(self):
        nc, c = self.nc, self.c
        S_ = self.S
        NB = S_ // 128
        SC = 128 ** -0.5
        with self.scope() as st:
            T = lambda name, shape, dt=F32: st.enter_context(self.sbt(name, shape, dt))
            cw = T("m_cw", [128, 4, 4]); cb = T("m_cb", [128, 4])
            mn = T("m_mn", [128, 4]); msk = T("m_msk", [128, 4])
            gbi = T("m_gbi", [4, 1]); gbf = T("m_gbf", [4, 1]); ngbf = T("m_ngbf", [4, 1])
            maskS = T("m_maskS", [128, 128]); idf = T("m_idf", [128, 128])
            ones4 = T("m_ones4", [4, 128])
            wst = T("m_wst", [128, 3, 4, 128]); wqkv = T("m_wqkv", [128, 3, 4, 128], BF16)
            gst = T("m_gst", [128, 12, 8]); gw = T("m_gw", [128, 12, 8], BF16)
            for (t, d, key) in ((cw, self.ml_cw, "m_cw"), (cb, self.ml_cb, "m_cb"), (mn, self.ml_mn, "m_mn"),
                                (msk, self.ml_msk, "m_msk"), (gbi, self.ml_gbi, "m_gbi"), (gbf, self.ml_gbf, "m_gbf"),
                                (maskS, self.ml_maskS, "m_maskS"), (idf, self.identf_d, "m_idf"),
                                (gst, self.ml_gw, "m_gst")):
                c.dma(t[:], d, w=[key])
            for i, d in enumerate((self.ml_wq, self.ml_wk, self.ml_wv)):
                c.dma(wst[:, i], d, w=["m_wst"])
            self.cp("dve", wqkv[:], wst[:], r=["m_wst"], w=["m_wqkv"])
            self.cp("dve", gw[:], gst[:], r=["m_gst"], w=["m_gw"])
            self.ts("dve", ngbf[:], gbf[:], -1.0, ALU.mult, r=["m_gbf"], w=["m_ngbf"])
            c.op("dve", lambda e: e.memset(ones4[:], 1.0), w=["m_ones4"])
            xmT_v = self.xmT.rearrange("(c p) t -> p c t", p=128)
            mzT_v = self.mzT.rearrange("(c p) t -> p c t", p=128)
            bT_v = self.bT.rearrange("(c p) t -> p c t", p=128)
            for q in range(self.nseq):
                tb = q * S_
                with self.scope() as sq:
                    TQ = lambda name, shape, dt=F32: sq.enter_context(self.sbt(name, shape, dt))
                    xcT = TQ("xcT", [128, 4, S_], BF16)
                    qT = TQ("qT", [128, 4, S_], BF16)
                    kT = TQ("kT", [128, 4, S_], BF16)
                    Ktok = TQ("Ktok", [128, NB, 4, 128], BF16)
                    Vtok = TQ("Vtok", [128, NB, 4, 129], BF16)
                    acol = TQ("acol", [128, NB, 4]); bcol = TQ("bcol", [128, NB, 4])
                    Rrep = TQ("Rrep", [128, NB + 1, 4])
                    Wt = TQ("Wt", [128, NB, 4]); Wp = TQ("Wp", [128, NB, 4])
                    Thr = TQ("Thr", [128, NB, 4]); Dec = TQ("Dec", [128, NB, 4])
                    c.op("pool", lambda e: e.memset(Vtok[:, :, :, 128:129], 1.0), w=["Vtok"])
                    with self.scope() as sa:
                        TA_ = lambda name, shape, dt=F32: sa.enter_context(self.sbt(name, shape, dt))
                        xm = TA_("xm", [128, 4, S_], BF16)
                        vT = TA_("vT", [128, 4, S_], BF16)
                        acc = TA_("acc", [128, S_])
                        g1f = TA_("g1", [32, S_]); g2f = TA_("g2", [32, S_]); g3f = TA_("g3", [32, S_]); onesr = TA_("onesr", [32, S_])
                        g1 = g1f[0:4, :]; g2 = g2f[0:4, :]; g3 = g3f[0:4, :]
                        c.op("pool", lambda e: e.memset(g1f[:], 0.0), w=["g1"])
                        c.op("pool", lambda e: e.memset(g2f[:], 0.0), w=["g2"])
                        rsel = TA_("rsel", [4, NB, 4])
                        c.dma(xm[:], xmT_v[:, :, tb:tb + S_], r=["xmT"], w=["xm"])
                        c.op("pool", lambda e: e.memset(onesr[:], 1.0), w=["onesr"])
                        for fc in range(4):
                            self.ts("dve", acc[:], xm[:, fc, :], cw[:, fc, 3:4], ALU.mult, r=["xm", "m_cw", "m_cb"], w=["acc"],
                                    s2=cb[:, fc:fc + 1], op1=ALU.add)
                            for sh in (1, 2, 3):
                                self.stt("dve", acc[:, sh:], xm[:, fc, 0:S_ - sh], cw[:, fc, 3 - sh:4 - sh], acc[:, sh:],
                                         ALU.mult, ALU.add, r=["xm", "m_cw", "acc"], w=["acc"])
                            self.actf(xcT[:, fc, :], acc[:], AF.Silu, r=["acc"], w=["xcT"])
                        for h in range(4):
                            for tl in range(S_ // 512):
                                ts_ = slice(tl * 512, (tl + 1) * 512)
                                for (i, src, skey, dst, dkey) in ((0, xcT, "xcT", qT, "qT"), (1, xcT, "xcT", kT, "kT"), (2, xm, "xm", vT, "vT")):
                                    pb = (h * 12 + tl * 3 + i) % 4
                                    pk = "ps%d" % pb
                                    c.op("pe", lambda e: e.matmul(out=self.ps[pb][:], lhsT=wqkv[:, i, h, :], rhs=src[:, h, ts_],
                                                                  start=True, stop=True), r=["m_wqkv", skey], w=[pk])
                                    self.cp("act" if i != 1 else "dve", dst[:, h, ts_], self.ps[pb][:], r=[pk], w=[dkey])
                        for blk in range(NB):
                            bs = slice(blk * 128, (blk + 1) * 128)
                            for (i, src, skey, dst, dkey, pb) in ((1, xcT, "xcT", Ktok, "Ktok", 4), (2, xm, "xm", Vtok, "Vtok", 5)):
                                pb = pb + 2 * (blk % 2)
                                pk = "ps%d" % pb
                                for h in range(4):
                                    c.op("pe", lambda e: e.matmul(out=self.ps[pb][:, h * 128:(h + 1) * 128], lhsT=src[:, h, bs],
                                                                  rhs=wqkv[:, i, h, :], start=True, stop=True), r=["m_wqkv", skey], w=[pk])
                                self.cp("act" if i == 1 else "dve", dst[:, blk, :, 0:128], self.ps[pb][:].rearrange("p (h e) -> p h e", h=4),
                                        r=[pk], w=[dkey])
                        for tl in range(S_ // 512):
                            ts_ = slice(tl * 512, (tl + 1) * 512)
                            for half in range(2):
                                pb = half
                                pk = "ps%d" % pb
                                for ch in range(12):
                                    src = (qT, kT, vT)[ch // 4]
                                    skey = ("qT", "kT", "vT")[ch // 4]
                                    c.op("pe", lambda e: e.matmul(out=self.ps[pb][0:4, :], lhsT=gw[:, ch, half * 4:half * 4 + 4],
                                                                  rhs=src[:, ch % 4, ts_], start=(ch == 0), stop=(ch == 11)),
                                         r=["m_gw", skey], w=[pk])
                                if half == 0:
                                    self.ts("dve", g1[:, ts_], self.ps[pb][0:4, :], gbi[:, 0:1], ALU.add, r=[pk, "m_gbi"], w=["g1"])
                                else:
                                    self.actf(g2[:, ts_], self.ps[pb][0:4, :], AF.Exp, r=[pk, "m_ngbf"], w=["g2"], scale=-1.0, bias=ngbf[:, 0:1])
                        self.actf(g2[:], g2[:], AF.Ln, r=["g2"], w=["g2"], bias=1.0)
                        c.op("dve", lambda e: e.tensor_tensor_scan(out=g3f[:], data0=onesr[:], data1=g2f[:], initial=0.0, op0=ALU.mult, op1=ALU.add),
                             r=["onesr", "g2"], w=["g3"])
                        self.tt("dve", g1[:], g1[:], g3[:], ALU.add, r=["g1", "g3"], w=["g1"])
                        c.op("dve", lambda e: e.tensor_tensor_scan(out=g2f[:], data0=onesr[:], data1=g1f[:], initial=0.0, op0=ALU.mult, op1=ALU.max),
                             r=["onesr", "g1", "g2"], w=["g2"])
                        pa = self.ps[2][:, 0:NB * 4].rearrange("p (b h) -> p b h", h=4)
                        pbn = self.ps[3][:, 0:NB * 4].rearrange("p (b h) -> p b h", h=4)
                        for blk in range(NB):
                            bs = slice(blk * 128, (blk + 1) * 128)
                            c.op("pe", lambda e: e.transpose(out=pa[:, blk, :], in_=g1[:, bs], identity=idf[0:4, 0:4]), r=["g1", "m_idf"], w=["ps2"])
                            c.op("pe", lambda e: e.transpose(out=pbn[:, blk, :], in_=g3[:, bs], identity=idf[0:4, 0:4]), r=["g3", "m_idf"], w=["ps3"])
                        self.cp("dve", acol[:], pa, r=["ps2"], w=["acol"])
                        self.cp("dve", bcol[:], pbn, r=["ps3"], w=["bcol"])
                        self.tt("dve", rsel[:], g2[:, 127::128].unsqueeze(2).broadcast_to([4, NB, 4]),
                                idf[0:4, 0:4].unsqueeze(1).broadcast_to([4, NB, 4]), ALU.mult, r=["g2", "m_idf"], w=["rsel"])
                        c.op("pe", lambda e: e.matmul(out=self.ps[0][:, 0:NB * 4], lhsT=ones4[:], rhs=rsel[:].rearrange("p b h -> p (b h)"),
                                                      start=True, stop=True), r=["m_ones4", "rsel"], w=["ps0"])
                        c.op("dve", lambda e: e.memset(Rrep[:, 0, :], 0.0), w=["Rrep"])
                        self.cp("dve", Rrep[:, 1:NB + 1, :], self.ps[0][:, 0:NB * 4].rearrange("p (b h) -> p b h", h=4), r=["ps0"], w=["Rrep"])
                    self.tt("dve", Wt[:], acol[:], Rrep[:, 0:NB, :], ALU.subtract, r=["acol", "Rrep"], w=["Wt"])
                    self.actf(Wt[:], Wt[:], AF.Exp, r=["Wt"], w=["Wt"])
                    self.tt("dve", Wp[:], acol[:], Rrep[:, 1:NB + 1, :], ALU.subtract, r=["acol", "Rrep"], w=["Wp"])
                    self.actf(Wp[:], Wp[:], AF.Exp, r=["Wp"], w=["Wp"])
                    self.ts("dve", Wp[:], Wp[:], SC, ALU.mult, r=["Wp"], w=["Wp"])
                    self.tt("dve", Thr[:], bcol[:], Rrep[:, 0:NB, :], ALU.subtract, r=["bcol", "Rrep"], w=["Thr"])
                    self.actf(Thr[:], Thr[:], AF.Exp, r=["Thr"], w=["Thr"])
                    self.tt("dve", Dec[:], Rrep[:, 0:NB, :], Rrep[:, 1:NB + 1, :], ALU.subtract, r=["Rrep"], w=["Dec"])
                    self.actf(Dec[:], Dec[:], AF.Exp, r=["Dec"], w=["Dec"])
                    with self.scope() as sm:
                        TM = lambda name, shape, dt=F32: sm.enter_context(self.sbt(name, shape, dt))
                        C32 = TM("C32", [128, 4, 129]); Cm = TM("Cm", [128, 4, 129])
                        Cb = TM("Cb", [128, 4, 129], BF16)
                        PT4 = [TM("PT4_%d" % i, [128, 4, 128], BF16) for i in range(2)]
                        Vp4 = [TM("Vp4_%d" % i, [128, 4, 129], BF16) for i in range(2)]
                        Vpp4 = [TM("Vpp4_%d" % i, [128, 4, 129], BF16) for i in range(2)]
                        den = TM("den4", [128, 4])
                        hraw = [TM("hraw%d" % i, [128, 4, 128]) for i in range(2)]
                        bst = TM("bst", [128, 4, 6]); mv = TM("mv", [128, 4, 2]); rs = TM("rs", [128, 4])
                        hn = TM("hn", [128, 4, 128], BF16)
                        e1 = TM("m_e1", [128, 4, 128]); e2 = TM("m_e2", [128, 4, 128])
                        mz = [TM("mz%d" % i, [128, 4, 512], BF16) for i in range(2)]
                        bo = [TM("bo%d" % i, [128, 4, 512], BF16) for i in range(2)]

                        def emit_front(I_):
                            bs_ = slice(I_ * 128, (I_ + 1) * 128)
                            j_ = I_ % 2
                            for h_ in range(4):
                                c.op("pe", lambda e: e.matmul(out=self.ps[j_][:, h_ * 128:(h_ + 1) * 128], lhsT=kT[:, h_, bs_], rhs=qT[:, h_, bs_],
                                                              start=True, stop=True), r=["kT", "qT"], w=["ps%d" % j_])
                            self.tt("dve", PT4[j_][:], self.ps[j_][:].rearrange("p (h t) -> p h t", h=4),
                                    maskS[:].unsqueeze(1).broadcast_to([128, 4, 128]), ALU.mult, r=["ps%d" % j_, "m_maskS"], w=["PT4_%d" % j_])
                            for h_ in range(4):
                                c.op("act", lambda e: e.activation(out=Vp4[j_][:, h_, :], in_=Vtok[:, I_, h_, :], func=AF.Copy, scale=Wt[:, I_, h_:h_ + 1]),
                                     r=["Vtok", "Wt"], w=["Vp4_%d" % j_])
                                c.op("act", lambda e: e.activation(out=Vpp4[j_][:, h_, :], in_=Vtok[:, I_, h_, :], func=AF.Copy, scale=Wp[:, I_, h_:h_ + 1]),
                                     r=["Vtok", "Wp"], w=["Vpp4_%d" % j_])
                        emit_front(0)
                        for I in range(NB):
                            bs = slice(I * 128, (I + 1) * 128)
                            i4 = (I // 4) % 2
                            j = I % 2
                            if I % 4 == 0:
                                c.dma(mz[i4][:], mzT_v[:, :, tb + I * 128:tb + I * 128 + 512], r=["mzT"], w=["mz%d" % i4])
                            hk = "hraw%d" % j
                            for h in range(4):
                                pO = self.ps[2 + h // 2][:, (h % 2) * 129:(h % 2 + 1) * 129]
                                kO = "ps%d" % (2 + h // 2)
                                c.op("pe", lambda e: e.matmul(out=pO, lhsT=PT4[j][:, h, :], rhs=Vp4[j][:, h, :], start=True, stop=(I == 0)),
                                     r=["PT4_%d" % j, "Vp4_%d" % j], w=[kO])
                                if I > 0:
                                    c.op("pe", lambda e: e.matmul(out=pO, lhsT=qT[:, h, bs], rhs=Cb[:, h, :], start=False, stop=True),
                                         r=["qT", "Cb"], w=[kO])
                            if I < NB - 1:
                                for h in range(4):
                                    pC = self.ps[4 + h // 2][:, (h % 2) * 129:(h % 2 + 1) * 129]
                                    c.op("pe", lambda e: e.matmul(out=pC, lhsT=Ktok[:, I, h, :], rhs=Vpp4[j][:, h, :], start=True, stop=True),
                                         r=["Ktok", "Vpp4_%d" % j], w=["ps%d" % (4 + h // 2)])
                            if I + 1 < NB:
                                emit_front(I + 1)
                            if I < NB - 1:
                                if I == 0:
                                    for hb in range(2):
                                        self.cp("dve", C32[:, 2 * hb:2 * hb + 2, :], self.ps[4 + hb][:, 0:258].rearrange("p (h e) -> p h e", h=2),
                                                r=["ps%d" % (4 + hb)], w=["C32"])
                                else:
                                    self.tt("pool", Cm[:], C32[:], Dec[:, I, :].unsqueeze(2).broadcast_to([128, 4, 129]), ALU.mult, r=["C32", "Dec"], w=["Cm"])
                                    for hb in range(2):
                                        self.tt("dve", C32[:, 2 * hb:2 * hb + 2, :], self.ps[4 + hb][:, 0:258].rearrange("p (h e) -> p h e", h=2),
                                                Cm[:, 2 * hb:2 * hb + 2, :], ALU.add, r=["ps%d" % (4 + hb), "Cm"], w=["C32"])
                                self.cp("act", Cb[:], C32[:], r=["C32"], w=["Cb"])
                            for hb in range(2):
                                self.actf(den[:, 2 * hb:2 * hb + 2], self.ps[2 + hb][:, 0:258].rearrange("p (h e) -> p h e", h=2)[:, :, 128],
                                          AF.Abs, r=["ps%d" % (2 + hb)], w=["den4"])
                            self.tt("dve", den[:], den[:], Thr[:, I, :], ALU.max, r=["den4", "Thr"], w=["den4"])
                            c.op("dve", lambda e: e.reciprocal(out=den[:], in_=den[:]), r=["den4"], w=["den4"])
                            for hb in range(2):
                                self.tt("dve", hraw[j][:, 2 * hb:2 * hb + 2, :], self.ps[2 + hb][:, 0:258].rearrange("p (h e) -> p h e", h=2)[:, :, 0:128],
                                        den[:, 2 * hb:2 * hb + 2].unsqueeze(2).broadcast_to([128, 2, 128]), ALU.mult, r=["ps%d" % (2 + hb), "den4"], w=[hk])
                            for h in range(4):
                                c.op("dve", lambda e: e.bn_stats(out=bst[:, h, :], in_=hraw[j][:, h, :]), r=[hk], w=["bst"])
                                c.op("dve", lambda e: e.bn_aggr(out=mv[:, h, :], in_=bst[:, h, :]), r=["bst"], w=["mv"])
                            self.actf(rs[:], mv[:, :, 1], AF.Ln, r=["mv"], w=["rs"], bias=HEAD_EPS)
                            self.actf(rs[:], rs[:], AF.Exp, r=["rs"], w=["rs"], scale=-0.5)
                            self.tt("pool", e1[:], hraw[j][:], mv[:, :, 0:1].broadcast_to([128, 4, 128]), ALU.subtract, r=[hk, "mv"], w=["m_e1"])
                            self.tt("dve", hn[:], e1[:], rs[:].unsqueeze(2).broadcast_to([128, 4, 128]), ALU.mult, r=["m_e1", "rs"], w=["hn"])
                            ptv = self.ps[6 + I % 2][:].bitcast(BF16).rearrange("p (a b) -> p a b", a=8)
                            pk = "ps%d" % (6 + I % 2)
                            for h in range(4):
                                c.op("pe", lambda e: e.transpose(out=ptv[:, h, :], in_=hn[:, h, :], identity=self.ident[:]), r=["hn", "ident"], w=[pk])
                            self.tt("pool", e2[:], xcT[:, :, bs], msk[:].unsqueeze(2).broadcast_to([128, 4, 128]), ALU.mult, r=["xcT", "m_msk"], w=["m_e2"])
                            self.tt("dve", e1[:], ptv[:, 0:4, :], mn[:].unsqueeze(2).broadcast_to([128, 4, 128]), ALU.mult, r=[pk, "m_mn", "m_e1"], w=["m_e1"])
                            self.tt("dve", e1[:], e1[:], e2[:], ALU.add, r=["m_e1", "m_e2"], w=["m_e1"])
                            off = (I % 4) * 128
                            self.tt("pool", bo[i4][:, :, off:off + 128], e1[:], mz[i4][:, :, off:off + 128], ALU.mult,
                                    r=["m_e1", "mz%d" % i4], w=["bo%d" % i4])
                            if I % 4 == 3:
                                c.dma(bT_v[:, :, tb + (I - 3) * 128:tb + (I + 1) * 128], bo[i4][:], r=["bo%d" % i4], w=["bT"])
        c.barrier()

    def out_proj_norm_res(self, lay, wout_d, pg_rep_d, lhs_provider, res_rows, dst_rows, dst_key, res_key):
        nc, c = self.nc, self.c
        NT = self.NT
        with self.scope() as st:
            T = lambda name, shape, dt=F32: st.enter_context(self.sbt(name, shape, dt))
            wout = T("wout", [128, 8, D], BF16)
            wst = [T("wost%d" % i, [128, D]) for i in range(2)]
            for fc in range(8):
                c.dma(wst[fc % 2][:], wout_d[fc * 128:(fc + 1) * 128, :], w=["wost%d" % (fc % 2)])
                self.cp("dve" if fc % 2 else "act", wout[:, fc, :], wst[fc % 2][:], r=["wost%d" % (fc % 2)], w=["wout"])
            pg = T("pg", [128, D])
            c.dma(pg[:], pg_rep_d, w=["pg"])
            NXR = 4
            xr = [T("xr%d" % i, [128, D]) for i in range(NXR)]
            yo = [T("yo%d" % i, [128, D]) for i in range(2)]
            junk = T("ojunk", [128, BR])
            ss2 = [T("oss%d" % i, [128, 2]) for i in range(2)]; rstd2 = [T("orstd%d" % i, [128, 1]) for i in range(2)]
            pref, prov = lhs_provider(st)
            nblk = NT // 128

            def prefetch(b):
                if b < nblk:
                    c.dma(xr[b % NXR][:], res_rows[b * 128:(b + 1) * 128, :], r=[res_key % b], w=["xr%d" % (b % NXR)])
                    pref(b)
            prefetch(0)
            prefetch(1)
            lhs_next = prov(0)
            for blk in range(nblk):
                rows = slice(blk * 128, (blk + 1) * 128)
                xk = "xr%d" % (blk % NXR)
                prefetch(blk + 2)
                lhs = lhs_next
                ss = ss2[blk % 2]; rstd = rstd2[blk % 2]
                ssk = "oss%d" % (blk % 2); rsk = "orstd%d" % (blk % 2)
                for half in range(2):
                    pb = 4 + half + 2 * (blk % 2)
                    pk = "ps%d" % pb
                    for fc in range(8):
                        ap, key = lhs[fc]
                        c.op("pe", lambda e: e.matmul(out=self.ps[pb][:], lhsT=ap, rhs=wout[:, fc, half * BR:(half + 1) * BR],
                                                      start=(fc == 0), stop=(fc == 7)), r=[key, "wout"], w=[pk])
                if blk + 1 < nblk:
                    lhs_next = prov(blk + 1)
                for half in range(2):
                    pb = 4 + half + 2 * (blk % 2)
                    pk = "ps%d" % pb
                    self.actf(junk[:], self.ps[pb][:], AF.Square, r=[pk], w=["ojunk", ssk], accum_out=ss[:, half:half + 1])
                self.tt("dve", rstd[:], ss[:, 0:1], ss[:, 1:2], ALU.add, r=[ssk], w=[rsk])
                self.actf(rstd[:], rstd[:], AF.Ln, r=[rsk], w=[rsk], scale=1.0 / D, bias=NORM_EPS)
                self.actf(rstd[:], rstd[:], AF.Exp, r=[rsk], w=[rsk], scale=-0.5)
                yk = "yo%d" % (blk % 2)
                for half in range(2):
                    pb = 4 + half + 2 * (blk % 2)
                    hs = slice(half * BR, (half + 1) * BR)
                    self.tt("dve", yo[blk % 2][:, hs], self.ps[pb][:], pg[:, hs], ALU.mult, r=["ps%d" % pb, "pg"], w=[yk])
                self.stt("dve", yo[blk % 2][:], yo[blk % 2][:], rstd[:, 0:1], xr[blk % NXR][:], ALU.mult, ALU.add, r=[yk, rsk, xk], w=[yk])
                c.dma(dst_rows[rows, :], yo[blk % 2][:], r=[yk], w=[dst_key % blk])

    def phase_l0_out(self):
        nc, c = self.nc, self.c
        bT_v = self.bT.rearrange("(c p) t -> p c t", p=128)

        def provider(st):
            T = lambda name, shape, dt=F32: st.enter_context(self.sbt(name, shape, dt))
            at = [T("at%d" % i, [128, BR], BF16) for i in range(4)]
            aT = [T("aT%d" % i, [128, 4, 128], BF16) for i in range(2)]
            bt = [T("bt%d" % i, [128, 4, 512], BF16) for i in range(2)]

            def pref(blk):
                i4 = (blk // 4) % 2
                c.dma(at[blk % 4][:], self.a_tok[blk * 128:(blk + 1) * 128, :], r=["a_tok"], w=["at%d" % (blk % 4)])
                if blk % 4 == 0:
                    c.dma(bt[i4][:], bT_v[:, :, blk * 128:blk * 128 + 512], r=["bT"], w=["bt%d" % i4])

            def prov(blk):
                i = blk % 2
                i4 = (blk // 4) % 2
                pk = "ps%d" % i
                ptv = self.ps[i][:].bitcast(BF16).rearrange("p (a b) -> p a b", a=8)
                for fc in range(4):
                    c.op("pe", lambda e: e.transpose(out=ptv[:, fc, :], in_=at[blk % 4][:, fc * 128:(fc + 1) * 128], identity=self.ident[:]),
                         r=["at%d" % (blk % 4), "ident"], w=[pk])
                self.cp("act", aT[i][:], ptv[:, 0:4, :], r=[pk], w=["aT%d" % i])
                off = (blk % 4) * 128
                return [(aT[i][:, fc, :], "aT%d" % i) for fc in range(4)] + \
                       [(bt[i4][:, h, off:off + 128], "bt%d" % i4) for h in range(4)]
            return pref, prov
        self.out_proj_norm_res(0, self.w_out_ab, self.post0_rep, provider, self.x, self.out, "h1_%d", "x%.0d")

    def phase_l1_proj(self):
        nc, c = self.nc, self.c
        NT = self.NT
        S_ = self.S
        ngrp = NT // 512
        nblk = NT // 128
        with self.scope() as st:
            T = lambda name, shape, dt=F32: st.enter_context(self.sbt(name, shape, dt))
            win = T("win1", [128, 8, 8 * BR], BF16)
            g1 = T("g1n", [128, 8])
            stage = [T("w1st%d" % i, [128, 4 * BR]) for i in range(2)]
            c.dma(g1[:], self.pre1, w=["g1n"])
            for dc in range(8):
                for hf in range(2):
                    i = (dc * 2 + hf) % 2
                    sk = "w1st%d" % i
                    c.dma(stage[i][:], self.w_in_cd[dc * 128:(dc + 1) * 128, hf * 2048:(hf + 1) * 2048], w=[sk])
                    if hf == 0:
                        c.op("act", lambda e: e.activation(out=win[:, dc, 0:2048], in_=stage[i][:], func=AF.Copy, scale=g1[:, dc:dc + 1]),
                             r=[sk, "g1n"], w=["win1"])
                    else:
                        self.ts("dve", win[:, dc, 2048:4096], stage[i][:], g1[:, dc:dc + 1], ALU.mult, r=[sk, "g1n"], w=["win1"])
            cosT = T("cosT", [128, S_]); sinT = T("sinT", [128, S_]); Rm = T("Rm", [128, 128], BF16)
            c.dma(cosT[:], self.rope_cos, w=["cosT"])
            c.dma(sinT[:], self.rope_sin, w=["sinT"])
            c.dma(Rm[:], self.rope_rm, w=["Rm"])
            NXB = 6
            xt = [T("x1t%d" % i, [128, D]) for i in range(NXB)]
            junk = T("junk1", [128, D])
            ss2 = [T("ss1_%d" % i, [128, 4]) for i in range(2)]; rstd2 = [T("rstd1_%d" % i, [128, 4]) for i in range(2)]
            zb = [T("z1b%d" % i, [128, D], BF16) for i in range(2)]
            zT = [T("z1T%d" % i, [128, 8, 512], BF16) for i in range(2)]
            vo = [T("vo%d" % i, [128, BR], BF16) for i in range(4)]
            xb = [T("xb%d" % i, [128, 512], BF16) for i in range(2)]
            r1 = [T("r1_%d" % i, [128, 512]) for i in range(2)]
            r2 = [T("r2_%d" % i, [128, 512]) for i in range(2)]
            fo = [T("fo%d" % i, [128, 4, 512], BF16) for i in range(2)]

            def load_x(b):
                if b < nblk:
                    c.dma(xt[b % NXB][:], self.h1src[b * 128:(b + 1) * 128, :], r=["h1_%d" % b], w=["x1t%d" % (b % NXB)])
            for b in range(4):
                load_x(b)
            fm_groups = [(0, self.cqT, "cqT", "rope"), (512, self.ckT, "ckT", "rope"), (2048, self.dqT, "dqT", "rope"),
                         (2560, self.dkT, "dkT", "rope"), (1536, self.czT, "czT", "silu"), (3584, self.dzT, "dzT", "silu")]
            cnt = 0
            for g in range(ngrp):
                zk = "z1T%d" % (g % 2)
                ss = ss2[g % 2]; rstd = rstd2[g % 2]
                ssk = "ss1_%d" % (g % 2); rsk = "rstd1_%d" % (g % 2)
                pos0 = (g * 512) % S_
                for j in range(4):
                    b = g * 4 + j
                    xk = "x1t%d" % (b % NXB)
                    c.op("act", lambda e: e.activation(out=junk[:], in_=xt[b % NXB][:], func=AF.Square, accum_out=ss[:, j:j + 1]),
                         r=[xk], w=["junk1", ssk])
                self.actf(rstd[:], ss[:], AF.Ln, r=[ssk], w=[rsk], scale=1.0 / D, bias=NORM_EPS)
                self.actf(rstd[:], rstd[:], AF.Exp, r=[rsk], w=[rsk], scale=-0.5)
                for j in range(4):
                    b = g * 4 + j
                    xk = "x1t%d" % (b % NXB)
                    zbk = "z1b%d" % (b % 2)
                    pk = "ps%d" % (b % 2)
                    self.ts("dve", zb[b % 2][:], xt[b % NXB][:], rstd[:, j:j + 1], ALU.mult, r=[xk, rsk], w=[zbk])
                    load_x(b + 4)
                    ptv = self.ps[b % 2][:].bitcast(BF16).rearrange("p (a b) -> p a b", a=8)
                    for dc in range(8):
                        c.op("pe", lambda e: e.transpose(out=ptv[:, dc, :], in_=zb[b % 2][:, dc * 128:(dc + 1) * 128], identity=self.ident[:]),
                             r=[zbk, "ident"], w=[pk])
                    self.cp("dve", zT[g % 2][:, :, j * 128:(j + 1) * 128], ptv, r=[pk], w=[zk])
                    for vi, (col0, dst, dkey) in enumerate(((1024, self.cv_tok, "cv_tok"), (3072, self.dv_tok, "dv_tok"))):
                        pb = 2 + vi
                        vk = "vo%d" % ((b % 2) * 2 + vi)
                        for dc in range(8):
                            c.op("pe", lambda e: e.matmul(out=self.ps[pb][:], lhsT=zT[g % 2][:, dc, j * 128:(j + 1) * 128],
                                                          rhs=win[:, dc, col0:col0 + BR], start=(dc == 0), stop=(dc == 7)), r=[zk, "win1"], w=["ps%d" % pb])
                        self.cp("act", vo[(b % 2) * 2 + vi][:], self.ps[pb][:], r=["ps%d" % pb], w=[vk])
                        c.dma(dst[b * 128:(b + 1) * 128, :], vo[(b % 2) * 2 + vi][:], r=[vk], w=[dkey])
                pend = []

                def flush_pend():
                    while pend:
                        (fot_, fc_, fk_, i2_, pb_, dst_, dkey_, lastfc) = pend.pop(0)
                        pr = 6 + i2_
                        c.op("pe", lambda e: e.matmul(out=self.ps[pr][:], lhsT=Rm[:], rhs=xb[i2_][:], start=True, stop=True),
                             r=["Rm", "xb%d" % i2_], w=["ps%d" % pr])
                        self.tt("dve", r1[i2_][:], self.ps[pb_][:], cosT[:, pos0:pos0 + 512], ALU.mult, r=["ps%d" % pb_, "cosT"], w=["r1_%d" % i2_])
                        self.tt("dve", r2[i2_][:], self.ps[pr][:], sinT[:, pos0:pos0 + 512], ALU.mult, r=["ps%d" % pr, "sinT"], w=["r2_%d" % i2_])
                        self.tt("pool", fot_[:, fc_, :], r1[i2_][:], r2[i2_][:], ALU.add, r=["r1_%d" % i2_, "r2_%d" % i2_], w=[fk_])
                        if lastfc:
                            c.dma(dst_.rearrange("(c p) t -> p c t", p=128)[:, :, g * 512:(g + 1) * 512], fot_[:], r=[fk_], w=[dkey_])
                for (col0, dst, dkey, kind) in fm_groups:
                    fk = "fo%d" % (cnt % 2)
                    fot = fo[cnt % 2]
                    cnt += 1
                    for fc in range(4):
                        pb = 4 + fc % 2
                        pk = "ps%d" % pb
                        for dc in range(8):
                            c.op("pe", lambda e: e.matmul(out=self.ps[pb][:], lhsT=win[:, dc, col0 + fc * 128:col0 + (fc + 1) * 128],
                                                          rhs=zT[g % 2][:, dc, :], start=(dc == 0), stop=(dc == 7)), r=[zk, "win1"], w=[pk])
                        flush_pend()
                        if kind == "silu":
                            self.actf(fot[:, fc, :], self.ps[pb][:], AF.Silu, r=[pk], w=[fk])
                            if fc == 3:
                                c.dma(dst.rearrange("(c p) t -> p c t", p=128)[:, :, g * 512:(g + 1) * 512], fot[:], r=[fk], w=[dkey])
                        else:
                            i2 = fc % 2
                            self.cp("act", xb[i2][:], self.ps[pb][:], r=[pk], w=["xb%d" % i2])
                            pend.append((fot, fc, fk, i2, pb, dst, dkey, fc == 3))
                flush_pend()

    def phase_attn(self, kind):
        nc, c = self.nc, self.c
        S_ = self.S
        NB = S_ // 128
        NQT = S_ // 512
        dil = (kind == "dil")
        qT_d, kT_d, v_d, o_d = (self.cqT, self.ckT, self.cv_tok, self.oc_tok) if dil else (self.dqT, self.dkT, self.dv_tok, self.od_tok)
        okey = "oc_tok" if dil else "od_tok"
        NH = 8 if dil else 4
        VW = 64 if dil else 128
        nmask = 9 if dil else 4
        lam_init = 0.8 - 0.6 * math.exp(-0.3 * 1)
        with self.scope() as st:
            T = lambda name, shape, dt=F32: st.enter_context(self.sbt(name, shape, dt))
            masks = T("amask", [128, nmask, 512], BF16)
            c.dma(masks[:], self.dil_masks if dil else self.diff_masks, w=["amask"])
            if not dil:
                lqk = T("lqk", [1, 4, 64]); pr = T("lpr", [1, 2, 64]); sm = T("lsm", [1, 2]); nl = T("nl", [1, 1])
                ones1 = T("ones1", [1, 128]); nlam = T("nlam", [128, 1]); gdn = T("gdn", [128, 128])
                c.dma(lqk[:], self.diff_lqk, w=["lqk"])
                c.dma(gdn[:], self.diffnorm_rep, w=["gdn"])
                c.op("dve", lambda e: e.memset(ones1[:], 1.0), w=["ones1"])
                self.tt("dve", pr[:, 0, :], lqk[:, 0, :], lqk[:, 1, :], ALU.mult, r=["lqk"], w=["lpr"])
                self.tt("dve", pr[:, 1, :], lqk[:, 2, :], lqk[:, 3, :], ALU.mult, r=["lqk", "lpr"], w=["lpr"])
                c.op("dve", lambda e: e.reduce_sum(out=sm[:], in_=pr[:], axis=AX.X), r=["lpr"], w=["lsm"])
                self.actf(sm[:], sm[:], AF.Exp, r=["lsm"], w=["lsm"])
                self.tt("dve", nl[:], sm[:, 1:2], sm[:, 0:1], ALU.subtract, r=["lsm"], w=["nl"])
                self.ts("dve", nl[:], nl[:], -lam_init, ALU.add, r=["nl"], w=["nl"])
                c.op("pe", lambda e: e.matmul(out=self.ps[0][:, 0:1], lhsT=ones1[:], rhs=nl[:], start=True, stop=True), r=["ones1", "nl"], w=["ps0"])
                self.cp("dve", nlam[:], self.ps[0][:, 0:1], r=["ps0"], w=["nlam"])
                self.ts("dve", gdn[:], gdn[:], 1.0 - lam_init, ALU.mult, r=["gdn"], w=["gdn"])
            qT_v = qT_d.rearrange("(c p) t -> p c t", p=128)
            kT_v = kT_d.rearrange("(c p) t -> p c t", p=128)
            for q in range(self.nseq):
                tb = q * S_
                with self.scope() as sq:
                    TQ = lambda name, shape, dt=F32: sq.enter_context(self.sbt(name, shape, dt))
                    qz = [TQ("aqz%d" % i, [128, 4, S_], BF16) for i in range(2)]
                    kT = TQ("akT", [128, 4, S_], BF16)
                    V1 = TQ("aV1", [128, NB, NH, VW + 1], BF16)
                    osb = TQ("aosb", [128, NB, BR], BF16)
                    NEP = 3
                    E = [TQ("aE%d" % i, [128, 512], BF16) for i in range(NEP)]
                    P = [TQ("aP%d" % i, [128, 512], BF16) for i in range(NEP)]
                    rd = [TQ("ard%d" % i, [128, 1]) for i in range(4)]
                    if not dil:
                        o01 = [TQ("ao%d" % i, [128, NB, 128]) for i in range(2)]
                        sqj = TQ("asq", [128, 128]); ssn = TQ("assn", [128, NB]); t3 = TQ("at3", [128, 128])
                    c.op("pool", lambda e: e.memset(qz[0][64:128, :, :], 0.0), w=["aqT"])
                    c.op("pool", lambda e: e.memset(qz[1][0:64, :, :], 0.0), w=["aqT"])
                    c.dma(qz[0][0:64, :, :], qT_v[0:64, :, tb:tb + S_], r=[("cqT" if dil else "dqT")], w=["aqT"])
                    c.dma(qz[1][64:128, :, :], qT_v[64:128, :, tb:tb + S_], r=[("cqT" if dil else "dqT")], w=["aqT"])
                    c.dma(kT[:], kT_v[:, :, tb:tb + S_], r=[("ckT" if dil else "dkT")], w=["akT"])
                    c.op("pool", lambda e: e.memset(V1[:, :, :, VW:VW + 1], 1.0), w=["aV1"])
                    for blk in range(NB):
                        c.dma(V1[:, blk, :, 0:VW], v_d[tb + blk * 128:tb + (blk + 1) * 128, :].rearrange("p (h d) -> p h d", h=NH),
                              r=[("cv_tok" if dil else "dv_tok")], w=["aV1"])
                    tiles = []
                    for h in range(NH):
                        for m in range(1 if dil else 2):
                            for qt in range(NQT):
                                nkb = 4 * qt + 4
                                for kb in range(nkb):
                                    tiles.append((h, m, qt, kb, kb == nkb - 1))
                    LA = 3
                    NSB = 4

                    def emit_S(i):
                        h, m, qt, kb, _ = tiles[i]
                        ch = h // 2 if dil else h
                        rb = 64 * (h % 2) if dil else 64 * m
                        sb = i % NSB
                        c0 = 128 * max(0, kb - 4 * qt)
                        c.op("pe", lambda e: e.matmul(out=self.ps[sb][:, c0:512], lhsT=kT[:, ch, kb * 128:(kb + 1) * 128],
                                                      rhs=qz[rb // 64][:, ch, qt * 512 + c0:(qt + 1) * 512], start=True, stop=True),
                             r=["akT", "aqT"], w=["ps%d" % sb])
                    for i in range(min(LA, len(tiles))):
                        emit_S(i)
                    for i, (h, m, qt, kb, last) in enumerate(tiles):
                        if i + LA < len(tiles):
                            emit_S(i + LA)
                        sb = i % NSB
                        i2 = i % NEP
                        pS = self.ps[sb]
                        kS = "ps%d" % sb
                        d0 = 4 * qt - kb
                        if dil:
                            mi = 8 if d0 >= 5 else d0 + 3
                        else:
                            mi = d0 + 3 if d0 <= 0 else None
                        c0 = 128 * max(0, kb - 4 * qt)
                        if mi is None:
                            self.actf(P[i2][:, c0:512], pS[:, c0:512], AF.Exp, r=[kS], w=["aP%d" % i2], scale=0.125)
                        else:
                            self.actf(E[i2][:, c0:512], pS[:, c0:512], AF.Exp, r=[kS], w=["aE%d" % i2], scale=0.125)
                            self.tt("dve", P[i2][:, c0:512], E[i2][:, c0:512], masks[:, mi, c0:512], ALU.mult,
                                    r=["aE%d" % i2, "amask"], w=["aP%d" % i2])
                        for j in range(4):
                            Q = 4 * qt + j
                            if kb > Q:
                                continue
                            c.op("pe", lambda e: e.matmul(out=self.ps[4 + j][:, 0:VW + 1], lhsT=P[i2][:, j * 128:(j + 1) * 128],
                                                          rhs=V1[:, kb, h, :], start=(kb == 0), stop=(kb == Q)),
                                 r=["aP%d" % i2, "aV1"], w=["ps%d" % (4 + j)])
                        if last:
                            for j in range(4):
                                Q = 4 * qt + j
                                pO = self.ps[4 + j]
                                kO = "ps%d" % (4 + j)
                                c.op("dve", lambda e: e.reciprocal(out=rd[j][:], in_=pO[:, VW:VW + 1]), r=[kO], w=["ard%d" % j])
                                if dil:
                                    self.ts("dve", osb[:, Q, h * 64:(h + 1) * 64], pO[:, 0:VW], rd[j][:, 0:1], ALU.mult,
                                            r=[kO, "ard%d" % j], w=["aosb"])
                                else:
                                    self.ts("dve", o01[m][:, Q, :], pO[:, 0:VW], rd[j][:, 0:1], ALU.mult,
                                            r=[kO, "ard%d" % j], w=["ao%d" % m])
                            if (not dil) and m == 1 and qt == NQT - 1:
                                self.stt("dve", o01[0][:], o01[1][:], nlam[:, 0:1], o01[0][:], ALU.mult, ALU.add, r=["ao0", "ao1", "nlam"], w=["ao0"])
                                for Q in range(NB):
                                    self.actf(sqj[:], o01[0][:, Q, :], AF.Square, r=["ao0"], w=["asq", "assn"], accum_out=ssn[:, Q:Q + 1])
                                self.actf(ssn[:], ssn[:], AF.Ln, r=["assn"], w=["assn"], scale=1.0 / 128, bias=HEAD_EPS)
                                self.actf(ssn[:], ssn[:], AF.Exp, r=["assn"], w=["assn"], scale=-0.5)
                                for Q in range(NB):
                                    self.ts("dve", t3[:], o01[0][:, Q, :], ssn[:, Q:Q + 1], ALU.mult, r=["ao0", "assn"], w=["at3"])
                                    self.tt("pool", osb[:, Q, h * 128:(h + 1) * 128], t3[:], gdn[:], ALU.mult, r=["at3", "gdn"], w=["aosb"])
                    c.dma(o_d[tb:tb + S_, :].rearrange("(b p) f -> p b f", p=128), osb[:], r=["aosb"], w=[okey])

    def phase_l1_out(self):
        nc, c = self.nc, self.c
        czT_v = self.czT.rearrange("(c p) t -> p c t", p=128)
        dzT_v = self.dzT.rearrange("(c p) t -> p c t", p=128)

        def provider(st):
            T = lambda name, shape, dt=F32: st.enter_context(self.sbt(name, shape, dt))
            ot = [T("ot%d" % i, [128, 2, BR], BF16) for i in range(4)]
            gT = [T("gT1_%d" % i, [128, 8, 128], BF16) for i in range(2)]
            gz = [T("gz%d" % i, [128, 8, 512], BF16) for i in range(2)]

            def pref(blk):
                i = blk % 4
                i4 = (blk // 4) % 2
                c.dma(ot[i][:, 0, :], self.oc_tok[blk * 128:(blk + 1) * 128, :], r=["oc_tok"], w=["ot%d" % i])
                c.dma(ot[i][:, 1, :], self.od_tok[blk * 128:(blk + 1) * 128, :], r=["od_tok"], w=["ot%d" % i])
                if blk % 4 == 0:
                    c.dma(gz[i4][:, 0:4, :], czT_v[:, :, blk * 128:blk * 128 + 512], r=["czT"], w=["gz%d" % i4])
                    c.dma(gz[i4][:, 4:8, :], dzT_v[:, :, blk * 128:blk * 128 + 512], r=["dzT"], w=["gz%d" % i4])

            def prov(blk):
                i = blk % 2
                i4 = (blk // 4) % 2
                pk = "ps%d" % i
                ptv = self.ps[i][:].bitcast(BF16).rearrange("p (a b) -> p a b", a=8)
                for fc in range(8):
                    c.op("pe", lambda e: e.transpose(out=ptv[:, fc, :], in_=ot[blk % 4][:, fc // 4, (fc % 4) * 128:(fc % 4 + 1) * 128],
                                                     identity=self.ident[:]), r=["ot%d" % (blk % 4), "ident"], w=[pk])
                off = (blk % 4) * 128
                self.tt("dve", gT[i][:], ptv, gz[i4][:, :, off:off + 128], ALU.mult, r=[pk, "gz%d" % i4], w=["gT1_%d" % i])
                return [(gT[i][:, fc, :], "gT1_%d" % i) for fc in range(8)]
            return pref, prov
        self.out_proj_norm_res(1, self.w_out_cd, self.post1_rep, provider, self.h1src, self.out, "h1_%d", "h1_%d")


def core_inputs(inp, x_rows):
    m = {}
    m["x"] = np.ascontiguousarray(x_rows, dtype=np.float32)
    m["ident"] = np.eye(128, dtype=np.float32).astype(ml_dtypes.bfloat16)
    m["pre0"] = np.ascontiguousarray(inp["pre_norm"][0].reshape(8, 128).T)
    m["w_in_ab"] = inp["w_in_ab"][0]
    m["s5_lr"] = inp["s5_lambda_re"][0].T
    m["s5_li"] = inp["s5_lambda_im"][0].T
    m["s5_ldt"] = np.broadcast_to(inp["s5_log_dt"][0][None, :], (64, 32))
    m["s5_br"] = inp["s5_b_re"][0].transpose(1, 0, 2)
    m["s5_bi"] = inp["s5_b_im"][0].transpose(1, 0, 2)
    m["s5_cr"] = inp["s5_c_re"][0].transpose(2, 0, 1)
    m["s5_ci"] = inp["s5_c_im"][0].transpose(2, 0, 1)
    tv = np.array([0, -1, -2, -3, -4, -5, -6, -7, 1, 2, 3, 4, 5, 6, 7, 8], dtype=np.float32)
    m["tv"] = np.broadcast_to(tv[None, :], (64, 16))
    m["kv"] = np.broadcast_to(np.arange(256, dtype=np.float32)[None, :], (64, 256))
    sidx = np.arange(128) // 16
    m["toepmask"] = (sidx[None, :] >= sidx[:, None]).astype(np.float32)
    m["identf"] = np.eye(128, dtype=np.float32)
    m["s5d_rep"] = np.broadcast_to(inp["s5_d"][0][None, :], (128, 512))
    m["glub_rep"] = np.broadcast_to(inp["s5_glu_b"][0][None, :], (128, 512))
    m["glu_w"] = inp["s5_glu_w"][0]
    m["ml_cw"] = inp["ml_conv_w"][0].reshape(4, 4, 128).transpose(2, 1, 0)
    m["ml_cb"] = inp["ml_conv_b"][0].reshape(4, 128).T
    m["ml_mn"] = inp["ml_norm"][0].reshape(4, 128).T
    m["ml_msk"] = inp["ml_skip"][0].reshape(4, 128).T
    m["ml_gbi"] = inp["ml_gate_b"][0][0:4].reshape(4, 1)
    m["ml_gbf"] = inp["ml_gate_b"][0][4:8].reshape(4, 1)
    si = np.arange(128)
    m["ml_maskS"] = ((si[:, None] <= si[None, :]) * (128 ** -0.5)).astype(np.float32)
    m["ml_gw"] = inp["ml_gate_w"][0].reshape(12, 128, 8).transpose(1, 0, 2)
    m["ml_wq"] = inp["ml_wq"][0].transpose(1, 0, 2)
    m["ml_wk"] = inp["ml_wk"][0].transpose(1, 0, 2)
    m["ml_wv"] = inp["ml_wv"][0].transpose(1, 0, 2)
    m["w_out_ab"] = inp["w_out_ab"][0]
    m["pre1"] = np.ascontiguousarray(inp["pre_norm"][1].reshape(8, 128).T)
    m["w_in_cd"] = inp["w_in_cd"][0]
    pos = np.arange(S, dtype=np.float32)
    inv = (10000.0 ** (-np.arange(0, 64, 2, dtype=np.float32) / 64)).astype(np.float32)
    ang = pos[None, :] * inv[np.arange(128) % 32][:, None]
    m["rope_cos"] = np.cos(ang).astype(np.float32)
    m["rope_sin"] = np.sin(ang).astype(np.float32)
    rm = np.zeros((128, 128), dtype=np.float32)
    for mm in range(128):
        if mm % 64 < 32:
            rm[mm + 32, mm] = -1.0
        else:
            rm[mm - 32, mm] = 1.0
    m["rope_rm"] = rm.astype(ml_dtypes.bfloat16)
    kk = np.arange(128)[:, None]
    qq = np.arange(512)[None, :]
    dm = np.zeros((128, 9, 512), dtype=np.float32)
    fm = np.zeros((128, 4, 512), dtype=np.float32)
    for mi in range(9):
        d0 = mi - 3 if mi < 8 else 5
        dl = 128 * d0 + qq - kk
        mult = ((dl >= 0) & (dl <= 128)).astype(np.float32) + ((dl >= 0) & (dl % 4 == 0) & (dl <= 512)) + ((dl >= 0) & (dl % 16 == 0) & (dl <= 2048))
        dm[:, mi, :] = mult
        if mi < 4:
            fm[:, mi, :] = (dl >= 0)
    m["dil_masks"] = dm.astype(ml_dtypes.bfloat16)
    m["diff_masks"] = fm.astype(ml_dtypes.bfloat16)
    m["diff_lqk"] = np.stack([inp["diff_lq1"][0], inp["diff_lk1"][0], inp["diff_lq2"][0], inp["diff_lk2"][0]])[None]
    m["diffnorm_rep"] = np.broadcast_to(inp["diff_norm"][0][None, :], (128, 128))
    m["w_out_cd"] = inp["w_out_cd"][0]
    m["post1_rep"] = np.broadcast_to(inp["post_norm"][1][None, :], (128, 1024))
    m["post0_rep"] = np.broadcast_to(inp["post_norm"][0][None, :], (128, 1024))
    return m


_CACHE = {}


def kernel(**inputs):
    inp = {k_: np.asarray(v) for k_, v in inputs.items()}
    x = inp["x"]
    B = x.shape[0]
    nseq = B // NCORES
    if "prog" not in _CACHE:
        kb = K(nseq=nseq)
        kb.build()
        _CACHE["prog"] = kb
    kb = _CACHE["prog"]
    in_maps = []
    for ci in range(NCORES):
        m = core_inputs(inp, x[ci * nseq:(ci + 1) * nseq].reshape(-1, D))
        in_maps.append({n: np.ascontiguousarray(m[n]) for n in kb.inputs})
    res = run_bass_kernel_spmd(kb.nc, in_maps, core_ids=list(range(NCORES)))
    out = np.stack([np.asarray(res.results[ci]["out"]).reshape(nseq, S, D) for ci in range(NCORES)], axis=0)
    return out.reshape(B, S, D).astype(np.float32)
```

```python
import contextlib
import math
import numpy as np
import ml_dtypes
import concourse.bass as bass
import concourse.mybir as mybir
from concourse.bass_utils import run_bass_kernel_spmd

F32 = mybir.dt.float32
BF16 = mybir.dt.bfloat16
I32 = mybir.dt.int32
AF = mybir.ActivationFunctionType
ALU = mybir.AluOpType
AX = mybir.AxisListType

D = 1024
S = 2048
BR = 512
NCORES = 8
SAME_ENGINE_SYNC = True
NORM_EPS = 1e-6
HEAD_EPS = 1e-5


class Ctx:
    def __init__(self, nc, stack, n_dma_sems=48, same_engine_sync=SAME_ENGINE_SYNC):
        self.nc = nc
        self.eng = {"pe": nc.tensor, "act": nc.scalar, "dve": nc.vector,
                    "pool": nc.gpsimd, "sp": nc.sync}
        self.sem = {}
        self.cnt = {}
        for k in ("pe", "act", "dve", "pool"):
            self.sem[k] = stack.enter_context(nc.semaphore("s_" + k))
            self.cnt[k] = 0
        self.dma_sems = []
        for i in range(n_dma_sems):
            k = "dma%d" % i
            self.sem[k] = stack.enter_context(nc.semaphore("s_" + k))
            self.cnt[k] = 0
            self.dma_sems.append(k)
        self.dma_rr = 0
        self.waited = {k: {} for k in self.eng}
        self.last_w = {}
        self.readers = {}
        self.same_engine_sync = same_engine_sync
        self.n_instr = 0
        self.n_wait = 0

    def _deps(self, r, w):
        deps = []
        for x in r:
            if x in self.last_w:
                deps.append(self.last_w[x])
            if x.startswith("ps"):
                deps.extend(self.readers.get(x, ()))
        for x in w:
            if x in self.last_w:
                deps.append(self.last_w[x])
            deps.extend(self.readers.get(x, ()))
        return deps

    def _wait(self, e, deps):
        need = {}
        for (k, v) in deps:
            if k == e and (e == "pe" or not self.same_engine_sync):
                continue
            if need.get(k, 0) < v:
                need[k] = v
        for k, v in need.items():
            if self.waited[e].get(k, 0) >= v:
                continue
            self.eng[e].wait_ge(self.sem[k], v)
            self.waited[e][k] = v
            self.n_wait += 1

    def _commit(self, tok, r, w):
        for x in w:
            self.last_w[x] = tok
            self.readers[x] = []
        for x in r:
            if x in w:
                continue
            self.readers.setdefault(x, []).append(tok)

    def op(self, e, fn, r=(), w=()):
        self._wait(e, self._deps(r, w))
        ins = fn(self.eng[e])
        self.cnt[e] += 1
        ins.then_inc(self.sem[e], 1)
        self._commit((e, self.cnt[e]), r, w)
        self.n_instr += 1
        return ins

    def dma(self, out, in_, r=(), w=(), q="sp", **kw):
        k = self.dma_sems[self.dma_rr]
        self.dma_rr = (self.dma_rr + 1) % len(self.dma_sems)
        deps = self._deps(r, w)
        if self.cnt[k] > 0:
            deps.append((k, self.cnt[k]))
        self._wait(q, deps)
        ins = self.eng[q].dma_start(out=out, in_=in_, **kw)
        self.cnt[k] += 16
        ins.then_inc(self.sem[k], 16)
        self._commit((k, self.cnt[k]), r, w)
        self.n_instr += 1
        return ins

    def barrier(self):
        deps = [(k, v) for k, v in self.cnt.items() if v > 0]
        for e in self.eng:
            self._wait(e, deps)

    def finish(self, res):
        deps = [self.last_w[x] for x in res if x in self.last_w]
        self._wait("sp", deps)


class K:
    def __init__(self, nseq=2, export=(), phases=None, seqlen=S):
        self.nseq = nseq
        self.S = seqlen
        self.NT = nseq * seqlen
        self.export = set(export)
        self.phases = phases
        self.nc = bass.Bass("TRN2", target_bir_lowering=False)
        self.inputs = {}
        self.outputs = {}
        self.s5_main_enabled = True
        self._uid = 0

    def sbt(self, name, shape, dt):
        self._uid += 1
        return self.nc.sbuf_tensor("%s_u%d" % (name, self._uid), list(shape), dt)

    def din(self, name, shape, dt=F32):
        ap = self.nc.dram_tensor(name, list(shape), dt, kind="ExternalInput").ap()
        self.inputs[name] = ap
        return ap

    def dscr(self, name, shape, dt):
        kind = "ExternalOutput" if name in self.export else "Internal"
        ap = self.nc.dram_tensor(name, list(shape), dt, kind=kind).ap()
        if kind == "ExternalOutput":
            self.outputs[name] = ap
        return ap

    @contextlib.contextmanager
    def scope(self):
        with contextlib.ExitStack() as st:
            yield st
            self.c.barrier()

    def build(self):
        nc = self.nc
        NT = self.NT
        with contextlib.ExitStack() as st:
            self.c = Ctx(nc, st)
            self.ps = [st.enter_context(nc.psum_tensor("ps%d" % i, [128, 512], F32)) for i in range(8)]
            self.x = self.din("x", [NT, D])
            self.ident_d = self.din("ident", [128, 128], BF16)
            self.pre0 = self.din("pre0", [128, 8])
            self.w_in_ab = self.din("w_in_ab", [D, 4 * BR])
            for nm in ("s5_lr", "s5_li", "s5_ldt"):
                setattr(self, nm, self.din(nm, [64, 32]))
            for nm in ("s5_br", "s5_bi", "s5_cr", "s5_ci"):
                setattr(self, nm, self.din(nm, [64, 32, 16]))
            self.tv_d = self.din("tv", [64, 16])
            self.kv_d = self.din("kv", [64, 256])
            self.toepmask_d = self.din("toepmask", [128, 128])
            self.identf_d = self.din("identf", [128, 128])
            self.s5d_rep = self.din("s5d_rep", [128, BR])
            self.glub_rep = self.din("glub_rep", [128, BR])
            self.glu_w = self.din("glu_w", [BR, BR])
            self.ml_cw = self.din("ml_cw", [128, 4, 4]); self.ml_cb = self.din("ml_cb", [128, 4])
            self.ml_mn = self.din("ml_mn", [128, 4]); self.ml_msk = self.din("ml_msk", [128, 4])
            self.ml_gbi = self.din("ml_gbi", [4, 1]); self.ml_gbf = self.din("ml_gbf", [4, 1])
            self.ml_maskS = self.din("ml_maskS", [128, 128])
            self.ml_gw = self.din("ml_gw", [128, 12, 8])
            self.ml_wq = self.din("ml_wq", [128, 4, 128]); self.ml_wk = self.din("ml_wk", [128, 4, 128]); self.ml_wv = self.din("ml_wv", [128, 4, 128])
            self.bT = self.dscr("bT", [BR, NT], BF16)
            self.w_out_ab = self.din("w_out_ab", [D, D]); self.post0_rep = self.din("post0_rep", [128, D])
            self.out = self.nc.dram_tensor("out", [NT, D], F32, kind="ExternalOutput").ap()
            self.outputs["out"] = self.out
            self.h1src = self.out
            if self.phases is not None and "l0o" not in self.phases:
                self.h1src = self.din("h1_in", [NT, D])
            self.pre1 = self.din("pre1", [128, 8]); self.w_in_cd = self.din("w_in_cd", [D, 8 * BR])
            self.rope_cos = self.din("rope_cos", [128, self.S]); self.rope_sin = self.din("rope_sin", [128, self.S])
            self.rope_rm = self.din("rope_rm", [128, 128], BF16)
            for nm in ("cqT", "ckT", "dqT", "dkT", "czT", "dzT"):
                setattr(self, nm, self.dscr(nm, [BR, NT], BF16))
            for nm in ("cv_tok", "dv_tok", "oc_tok", "od_tok"):
                setattr(self, nm, self.dscr(nm, [NT, BR], BF16))
            self.dil_masks = self.din("dil_masks", [128, 9, 512], BF16); self.diff_masks = self.din("diff_masks", [128, 4, 512], BF16)
            self.diff_lqk = self.din("diff_lqk", [1, 4, 64]); self.diffnorm_rep = self.din("diffnorm_rep", [128, 128])
            self.w_out_cd = self.din("w_out_cd", [D, D]); self.post1_rep = self.din("post1_rep", [128, D])
            self.rotc = self.dscr("rotc", [64, 32, 256], F32)
            self.rots = self.dscr("rots", [64, 32, 256], F32)
            self.rhot = self.dscr("rhot", [64, 32, 256], F32)
            self.toep_x = self.dscr("toep_x", [128, 32, 128], BF16)
            self.wii_x = self.dscr("wii_x", [128, 32, 2, 64], BF16)
            self.wiv_x = self.dscr("wiv_x", [64, 2, 32, 128], BF16)
            self.a_tok = self.dscr("a_tok", [NT, BR], BF16)
            self.u_tok = self.dscr("u_tok", [NT, BR], BF16)
            self.sz_tok = self.dscr("sz_tok", [NT, BR], BF16)
            self.xmT = self.dscr("xmT", [BR, NT], BF16)
            self.mzT = self.dscr("mzT", [BR, NT], BF16)
            self.ident = st.enter_context(nc.sbuf_tensor("identb", [128, 128], BF16))
            self.c.dma(self.ident[:], self.ident_d, w=["ident"])
            ph = self.phases
            fin = []
            if ph is None or "l0p" in ph:
                self.phase_l0_proj()
                fin += ["u_tok", "sz_tok", "xmT", "mzT"]
            if ph is None or "s5" in ph:
                self.phase_s5()
                fin += ["a_tok", "rotc", "rots", "rhot", "toep_x", "wii_x", "wiv_x"]
            if ph is None or "ml" in ph:
                self.phase_ml()
                fin += ["bT"]
            if ph is None or "l0o" in ph:
                self.phase_l0_out()
                fin += ["h1_%d" % b for b in range(NT // 128)]
            if ph is None or "l1p" in ph:
                self.phase_l1_proj()
                fin += ["cqT", "ckT", "dqT", "dkT", "czT", "dzT", "cv_tok", "dv_tok"]
            if ph is None or "adil" in ph:
                self.phase_attn("dil")
                fin += ["oc_tok"]
            if ph is None or "adiff" in ph:
                self.phase_attn("diff")
                fin += ["od_tok"]
            if ph is None or "l1o" in ph:
                self.phase_l1_out()
                fin += ["h1_%d" % b for b in range(NT // 128)]
            self.c.finish(fin)
            self.c.barrier()
        return nc

    def rmsnorm_T(self, st, xsrc_rows, nblk, zT, zkey, tagp, ps_tr):
        raise NotImplementedError

    def phase_l0_proj(self):
        nc, c = self.nc, self.c
        NT = self.NT
        ngrp = NT // 512
        with self.scope() as st:
            T = lambda name, shape, dt: st.enter_context(self.sbt(name, shape, dt))
            win = T("win0", [128, 8, 4 * BR], BF16)
            g0 = T("g0", [128, 8], F32)
            stage = [T("wst%d" % i, [128, 4 * BR], F32) for i in range(2)]
            c.dma(g0[:], self.pre0, w=["g0"])
            for dc in range(8):
                sk = "wst%d" % (dc % 2)
                c.dma(stage[dc % 2][:], self.w_in_ab[dc * 128:(dc + 1) * 128, :], w=[sk])
                c.op("act", lambda e: e.activation(out=win[:, dc, :], in_=stage[dc % 2][:], func=AF.Copy,
                                                   scale=g0[:, dc:dc + 1]), r=[sk, "g0"], w=["win0"])
            NXB = 6
            xt = [T("xt%d" % i, [128, D], F32) for i in range(NXB)]
            junk = T("junk", [128, D], F32)
            ss2 = [T("ss%d" % i, [128, 4], F32) for i in range(2)]
            rstd2 = [T("rstd%d" % i, [128, 4], F32) for i in range(2)]
            zb = [T("zb%d" % i, [128, D], BF16) for i in range(2)]
            zT = [T("zT%d" % i, [128, 8, 512], BF16) for i in range(2)]
            uo = [T("uo%d" % i, [128, BR], BF16) for i in range(2)]
            so = [T("so%d" % i, [128, BR], BF16) for i in range(2)]
            xmo = [T("xmo%d" % i, [128, 4, 512], BF16) for i in range(2)]
            mzo = [T("mzo%d" % i, [128, 4, 512], BF16) for i in range(2)]
            nblk = NT // 128

            def load_x(b):
                if b < nblk:
                    c.dma(xt[b % NXB][:], self.x[b * 128:(b + 1) * 128, :], w=["xt%d" % (b % NXB)])
            for b in range(4):
                load_x(b)
            for g in range(ngrp):
                zk = "zT%d" % (g % 2)
                ss = ss2[g % 2]; rstd = rstd2[g % 2]
                ssk = "ss%d" % (g % 2); rsk = "rstd%d" % (g % 2)
                for j in range(4):
                    b = g * 4 + j
                    xk = "xt%d" % (b % NXB)
                    c.op("act", lambda e: e.activation(out=junk[:], in_=xt[b % NXB][:], func=AF.Square,
                                                       accum_out=ss[:, j:j + 1]), r=[xk], w=["junk", ssk])
                c.op("act", lambda e: e.activation(out=rstd[:], in_=ss[:], func=AF.Ln, scale=1.0 / D, bias=NORM_EPS),
                     r=[ssk], w=[rsk])
                c.op("act", lambda e: e.activation(out=rstd[:], in_=rstd[:], func=AF.Exp, scale=-0.5),
                     r=[rsk], w=[rsk])
                for j in range(4):
                    b = g * 4 + j
                    xk = "xt%d" % (b % NXB)
                    zbk = "zb%d" % (b % 2)
                    pst = self.ps[b % 2]
                    pk = "ps%d" % (b % 2)
                    c.op("dve", lambda e: e.tensor_scalar(out=zb[b % 2][:], in0=xt[b % NXB][:], scalar1=rstd[:, j:j + 1],
                                                          scalar2=None, op0=ALU.mult), r=[xk, rsk], w=[zbk])
                    load_x(b + 4)
                    ptv = pst[:].bitcast(BF16).rearrange("p (a b) -> p a b", a=8)
                    for dc in range(8):
                        c.op("pe", lambda e: e.transpose(out=ptv[:, dc, :], in_=zb[b % 2][:, dc * 128:(dc + 1) * 128],
                                                         identity=self.ident[:]), r=[zbk, "ident"], w=[pk])
                    c.op("dve", lambda e: e.tensor_copy(out=zT[g % 2][:, :, j * 128:(j + 1) * 128], in_=ptv),
                         r=[pk], w=[zk])
                    for dc in range(8):
                        c.op("pe", lambda e: e.matmul(out=self.ps[2][:], lhsT=zT[g % 2][:, dc, j * 128:(j + 1) * 128],
                                                      rhs=win[:, dc, 0:BR], start=(dc == 0), stop=(dc == 7)),
                             r=[zk, "win0"], w=["ps2"])
                    c.op("act", lambda e: e.copy(out=uo[b % 2][:], in_=self.ps[2][:]), r=["ps2"], w=["uo%d" % (b % 2)])
                    c.dma(self.u_tok[b * 128:(b + 1) * 128, :], uo[b % 2][:], r=["uo%d" % (b % 2)], w=["u_tok"])
                    for dc in range(8):
                        c.op("pe", lambda e: e.matmul(out=self.ps[3][:], lhsT=zT[g % 2][:, dc, j * 128:(j + 1) * 128],
                                                      rhs=win[:, dc, BR:2 * BR], start=(dc == 0), stop=(dc == 7)),
                             r=[zk, "win0"], w=["ps3"])
                    c.op("act", lambda e: e.activation(out=so[b % 2][:], in_=self.ps[3][:], func=AF.Silu),
                         r=["ps3"], w=["so%d" % (b % 2)])
                    c.dma(self.sz_tok[b * 128:(b + 1) * 128, :], so[b % 2][:], r=["so%d" % (b % 2)], w=["sz_tok"])
                for fc in range(8):
                    pb = 4 + fc % 4
                    for dc in range(8):
                        c.op("pe", lambda e: e.matmul(out=self.ps[pb][:], lhsT=win[:, dc, 2 * BR + fc * 128:2 * BR + (fc + 1) * 128],
                                                      rhs=zT[g % 2][:, dc, :], start=(dc == 0), stop=(dc == 7)),
                             r=[zk, "win0"], w=["ps%d" % pb])
                    if fc < 4:
                        c.op("dve", lambda e: e.tensor_copy(out=xmo[g % 2][:, fc, :], in_=self.ps[pb][:]),
                             r=["ps%d" % pb], w=["xmo%d" % (g % 2)])
                    else:
                        c.op("act", lambda e: e.activation(out=mzo[g % 2][:, fc - 4, :], in_=self.ps[pb][:], func=AF.Silu),
                             r=["ps%d" % pb], w=["mzo%d" % (g % 2)])
                c.dma(self.xmT.rearrange("(c p) t -> p c t", p=128)[:, :, g * 512:(g + 1) * 512], xmo[g % 2][:],
                      r=["xmo%d" % (g % 2)], w=["xmT"])
                c.dma(self.mzT.rearrange("(c p) t -> p c t", p=128)[:, :, g * 512:(g + 1) * 512], mzo[g % 2][:],
                      r=["mzo%d" % (g % 2)], w=["mzT"])
        c.barrier()

    def tt(self, e, out, a, b, op, r, w):
        return self.c.op(e, lambda en: en.tensor_tensor(out=out, in0=a, in1=b, op=op), r=r, w=w)

    def ts(self, e, out, a, s1, op0, r, w, s2=None, op1=None):
        if op1 is None:
            return self.c.op(e, lambda en: en.tensor_scalar(out=out, in0=a, scalar1=s1, scalar2=None, op0=op0), r=r, w=w)
        return self.c.op(e, lambda en: en.tensor_scalar(out=out, in0=a, scalar1=s1, scalar2=s2, op0=op0, op1=op1), r=r, w=w)

    def stt(self, e, out, a, s, b, op0, op1, r, w):
        return self.c.op(e, lambda en: en.scalar_tensor_tensor(out=out, in0=a, scalar=s, in1=b, op0=op0, op1=op1), r=r, w=w)

    def actf(self, out, in_, func, r, w, **kw):
        return self.c.op("act", lambda en: en.activation(out=out, in_=in_, func=func, **kw), r=r, w=w)

    def cp(self, e, out, in_, r, w):
        if e == "act":
            return self.c.op("act", lambda en: en.copy(out=out, in_=in_), r=r, w=w)
        return self.c.op(e, lambda en: en.tensor_copy(out=out, in_=in_), r=r, w=w)

    def sincos(self, ang, akey, sin_o, cos_o, skey, ckey, tf, ti, red_o=None):
        C1 = 6.28125
        C2 = 2 * math.pi - C1
        for (off, out, okey) in ((0.0, sin_o, skey), (math.pi / 2, cos_o, ckey)):
            if out is None:
                continue
            self.ts("dve", tf, ang, 1.0 / (2 * math.pi), ALU.mult, r=[akey], w=["sc_tf"], s2=off / (2 * math.pi), op1=ALU.add)
            self.cp("dve", ti, tf, r=["sc_tf"], w=["sc_ti"])
            self.cp("dve", tf, ti, r=["sc_ti"], w=["sc_tf"])
            self.stt("dve", out, tf, -C1, ang, ALU.mult, ALU.add, r=["sc_tf", akey], w=[okey])
            self.stt("dve", out, tf, -C2, out, ALU.mult, ALU.add, r=["sc_tf", okey], w=[okey])
            if off != 0.0:
                self.ts("dve", out, out, off, ALU.add, r=[okey], w=[okey])
            self.ts("dve", out, out, math.pi, ALU.min, r=[okey], w=[okey], s2=-math.pi, op1=ALU.max)
            if red_o is not None and off == 0.0:
                self.cp("dve", red_o[0], out, r=[okey], w=[red_o[1]])
            self.actf(out, out, AF.Sin, r=[okey], w=[okey])

    def s5_precompute(self, st, Toep, Wii, WivR, WivI):
        nc, c = self.nc, self.c
        so = contextlib.ExitStack()
        TO = lambda name, shape, dt=F32: so.enter_context(self.sbt(name, shape, dt))
        thr = TO("p_thr", [64, 32]); rho8 = TO("p_rho8", [64, 32]); kv = TO("p_kv", [64, 256])
        tfs = TO("p_tfs", [64, 32]); tis = TO("p_tis", [64, 32], I32)
        with self.scope() as sp:
            T = lambda name, shape, dt=F32: sp.enter_context(self.sbt(name, shape, dt))
            lr = T("p_lr", [64, 32]); li = T("p_li", [64, 32]); ldt = T("p_ldt", [64, 32])
            br = T("p_br", [64, 32, 16]); bi = T("p_bi", [64, 32, 16])
            cr = T("p_cr", [64, 32, 16]); ci = T("p_ci", [64, 32, 16])
            tv = T("p_tv", [64, 16])
            msk = T("p_msk", [128, 128]); idf = T("p_idf", [128, 128])
            for (t, d, key) in ((lr, self.s5_lr, "p_lr"), (li, self.s5_li, "p_li"), (ldt, self.s5_ldt, "p_ldt"),
                                (br, self.s5_br, "p_br"), (bi, self.s5_bi, "p_bi"), (cr, self.s5_cr, "p_cr"),
                                (ci, self.s5_ci, "p_ci"), (tv, self.tv_d, "p_tv"), (kv, self.kv_d, "p_kv"),
                                (msk, self.toepmask_d, "p_msk"), (idf, self.identf_d, "p_idf")):
                c.dma(t[:], d, w=[key])
            dt = T("p_dt", [64, 32]); lrdt = T("p_lrdt", [64, 32]); th = T("p_th", [64, 32])
            s0 = T("p_s0", [64, 32]); c0 = T("p_c0", [64, 32]); mag = T("p_mag", [64, 32])
            self.actf(dt[:], ldt[:], AF.Exp, r=["p_ldt"], w=["p_dt"])
            self.tt("dve", lrdt[:], lr[:], dt[:], ALU.mult, r=["p_lr", "p_dt"], w=["p_lrdt"])
            self.tt("dve", th[:], li[:], dt[:], ALU.mult, r=["p_li", "p_dt"], w=["p_th"])
            self.sincos(th[:], "p_th", s0[:], c0[:], "p_s0", "p_c0", tfs[:], tis[:], red_o=(thr[:], "p_thr"))
            self.actf(mag[:], lrdt[:], AF.Exp, r=["p_lrdt"], w=["p_mag"])
            abr = T("p_abr", [64, 32]); abi = T("p_abi", [64, 32]); am1 = T("p_am1", [64, 32])
            self.tt("dve", abr[:], mag[:], c0[:], ALU.mult, r=["p_mag", "p_c0"], w=["p_abr"])
            self.tt("dve", abi[:], mag[:], s0[:], ALU.mult, r=["p_mag", "p_s0"], w=["p_abi"])
            self.ts("dve", am1[:], abr[:], -1.0, ALU.add, r=["p_abr"], w=["p_am1"])
            den = T("p_den", [64, 32]); t1 = T("p_t1", [64, 32]); t2 = T("p_t2", [64, 32])
            fr = T("p_fr", [64, 32]); fi = T("p_fi", [64, 32])
            self.tt("dve", den[:], lr[:], lr[:], ALU.mult, r=["p_lr"], w=["p_den"])
            self.tt("dve", t1[:], li[:], li[:], ALU.mult, r=["p_li"], w=["p_t1"])
            self.tt("dve", den[:], den[:], t1[:], ALU.add, r=["p_den", "p_t1"], w=["p_den"])
            c.op("dve", lambda e: e.reciprocal(out=den[:], in_=den[:]), r=["p_den"], w=["p_den"])
            self.tt("dve", t1[:], am1[:], lr[:], ALU.mult, r=["p_am1", "p_lr"], w=["p_t1"])
            self.tt("dve", t2[:], abi[:], li[:], ALU.mult, r=["p_abi", "p_li"], w=["p_t2"])
            self.tt("dve", t1[:], t1[:], t2[:], ALU.add, r=["p_t1", "p_t2"], w=["p_t1"])
            self.tt("dve", fr[:], t1[:], den[:], ALU.mult, r=["p_t1", "p_den"], w=["p_fr"])
            self.tt("dve", t1[:], abi[:], lr[:], ALU.mult, r=["p_abi", "p_lr"], w=["p_t1"])
            self.tt("dve", t2[:], am1[:], li[:], ALU.mult, r=["p_am1", "p_li"], w=["p_t2"])
            self.tt("dve", t1[:], t1[:], t2[:], ALU.subtract, r=["p_t1", "p_t2"], w=["p_t1"])
            self.tt("dve", fi[:], t1[:], den[:], ALU.mult, r=["p_t1", "p_den"], w=["p_fi"])
            Bbr = T("p_Bbr", [64, 32, 16]); Bbi = T("p_Bbi", [64, 32, 16])
            u1 = T("p_u1", [64, 32, 16]); u2 = T("p_u2", [64, 32, 16])
            bc16 = lambda a: a.unsqueeze(2).broadcast_to([64, 32, 16])
            self.tt("dve", u1[:], br[:], bc16(fr[:]), ALU.mult, r=["p_br", "p_fr"], w=["p_u1"])
            self.tt("dve", u2[:], bi[:], bc16(fi[:]), ALU.mult, r=["p_bi", "p_fi"], w=["p_u2"])
            self.tt("dve", Bbr[:], u1[:], u2[:], ALU.subtract, r=["p_u1", "p_u2"], w=["p_Bbr"])
            self.tt("dve", u1[:], bi[:], bc16(fr[:]), ALU.mult, r=["p_bi", "p_fr"], w=["p_u1"])
            self.tt("dve", u2[:], br[:], bc16(fi[:]), ALU.mult, r=["p_br", "p_fi"], w=["p_u2"])
            self.tt("dve", Bbi[:], u1[:], u2[:], ALU.add, r=["p_u1", "p_u2"], w=["p_Bbi"])
            TE = T("p_TE", [64, 16, 32]); TA = T("p_TA", [64, 16, 32])
            PWr = T("p_PWr", [64, 16, 32]); PWi = T("p_PWi", [64, 16, 32])
            tf3 = T("p_tf3", [64, 16, 32]); ti3 = T("p_ti3", [64, 16, 32], I32)
            bt = lambda a: a.unsqueeze(1).broadcast_to([64, 16, 32])
            bg = lambda a: a.unsqueeze(2).broadcast_to([64, 16, 32])
            self.tt("dve", TE[:], bt(lrdt[:]), bg(tv[:]), ALU.mult, r=["p_lrdt", "p_tv"], w=["p_TE"])
            self.actf(TE[:], TE[:], AF.Exp, r=["p_TE"], w=["p_TE"])
            self.tt("dve", TA[:], bt(thr[:]), bg(tv[:]), ALU.mult, r=["p_thr", "p_tv"], w=["p_TA"])
            self.sincos(TA[:], "p_TA", PWi[:], PWr[:], "p_PWi", "p_PWr", tf3[:], ti3[:])
            self.tt("dve", PWr[:], PWr[:], TE[:], ALU.mult, r=["p_PWr", "p_TE"], w=["p_PWr"])
            self.tt("dve", PWi[:], PWi[:], TE[:], ALU.mult, r=["p_PWi", "p_TE"], w=["p_PWi"])
            HsR = T("p_HsR", [64, 32, 8, 16]); HsI = T("p_HsI", [64, 32, 8, 16])
            v1 = T("p_v1", [64, 32, 9, 16]); v2 = T("p_v2", [64, 32, 9, 16])
            def pw_b(PW, j0, n):
                return PW[:, j0:j0 + n, :].rearrange("p t g -> p g t").unsqueeze(3).broadcast_to([64, 32, n, 16])
            def x_b(x, n):
                return x.unsqueeze(2).broadcast_to([64, 32, n, 16])
            self.tt("dve", v1[:, :, 0:8, :], pw_b(PWr, 0, 8), x_b(Bbr[:], 8), ALU.mult, r=["p_PWr", "p_Bbr"], w=["p_v1"])
            self.tt("dve", v2[:, :, 0:8, :], pw_b(PWi, 0, 8), x_b(Bbi[:], 8), ALU.mult, r=["p_PWi", "p_Bbi"], w=["p_v2"])
            self.tt("dve", HsR[:], v1[:, :, 0:8, :], v2[:, :, 0:8, :], ALU.subtract, r=["p_v1", "p_v2"], w=["p_HsR"])
            self.tt("dve", v1[:, :, 0:8, :], pw_b(PWr, 0, 8), x_b(Bbi[:], 8), ALU.mult, r=["p_PWr", "p_Bbi"], w=["p_v1"])
            self.tt("dve", v2[:, :, 0:8, :], pw_b(PWi, 0, 8), x_b(Bbr[:], 8), ALU.mult, r=["p_PWi", "p_Bbr"], w=["p_v2"])
            self.tt("dve", HsI[:], v1[:, :, 0:8, :], v2[:, :, 0:8, :], ALU.add, r=["p_v1", "p_v2"], w=["p_HsI"])
            LtR = T("p_LtR", [64, 32, 9, 16]); nLtI = T("p_nLtI", [64, 32, 9, 16])
            for (s0_, j0, n) in ((0, 0, 1), (1, 8, 8)):
                sl = slice(s0_, s0_ + n)
                self.tt("dve", v1[:, :, sl, :], pw_b(PWr, j0, n), x_b(cr[:], n), ALU.mult, r=["p_PWr", "p_cr"], w=["p_v1"])
                self.tt("dve", v2[:, :, sl, :], pw_b(PWi, j0, n), x_b(ci[:], n), ALU.mult, r=["p_PWi", "p_ci"], w=["p_v2"])
                self.tt("dve", LtR[:, :, sl, :], v1[:, :, sl, :], v2[:, :, sl, :], ALU.subtract, r=["p_v1", "p_v2"], w=["p_LtR"])
                self.tt("dve", v1[:, :, sl, :], pw_b(PWi, j0, n), x_b(cr[:], n), ALU.mult, r=["p_PWi", "p_cr"], w=["p_v1"])
                self.tt("dve", v2[:, :, sl, :], pw_b(PWr, j0, n), x_b(ci[:], n), ALU.mult, r=["p_PWr", "p_ci"], w=["p_v2"])
                self.tt("dve", v1[:, :, sl, :], v1[:, :, sl, :], v2[:, :, sl, :], ALU.add, r=["p_v1", "p_v2"], w=["p_v1"])
                self.ts("dve", nLtI[:, :, sl, :], v1[:, :, sl, :], -1.0, ALU.mult, r=["p_v1"], w=["p_nLtI"])
            self.cp("dve", WivR[:], LtR[:, :, 1:9, :].rearrange("p g t c -> p g (t c)"), r=["p_LtR"], w=["WivR"])
            self.cp("dve", WivI[:], nLtI[:, :, 1:9, :].rearrange("p g t c -> p g (t c)"), r=["p_nLtI"], w=["WivI"])
            for g4 in range(8):
                pb = g4 % 2
                pk = "ps%d" % pb
                for gl in range(4):
                    g = g4 * 4 + gl
                    o = self.ps[pb][:, gl * 128:(gl + 1) * 128]
                    c.op("pe", lambda e: e.matmul(out=o, lhsT=HsR[:, g, :, :].rearrange("p s c -> p (s c)"),
                                                  rhs=LtR[:, g, 0:8, :].rearrange("p t c -> p (t c)"), start=True, stop=False),
                         r=["p_HsR", "p_LtR"], w=[pk])
                    c.op("pe", lambda e: e.matmul(out=o, lhsT=HsI[:, g, :, :].rearrange("p s c -> p (s c)"),
                                                  rhs=nLtI[:, g, 0:8, :].rearrange("p t c -> p (t c)"), start=False, stop=True),
                         r=["p_HsI", "p_nLtI"], w=[pk])
                self.tt("dve", Toep[:, g4 * 4:(g4 + 1) * 4, :], self.ps[pb][:].rearrange("p (g n) -> p g n", g=4),
                        msk[:].unsqueeze(1).broadcast_to([128, 4, 128]), ALU.mult, r=[pk, "p_msk"], w=["Toep"])
            GsR = LtR[:, :, 0:8, :].rearrange("p g t c -> p g (t c)")
            GsI = nLtI[:, :, 0:8, :].rearrange("p g t c -> p g (t c)")
            w1 = v1[:, :, 0:8, :].rearrange("p g t c -> p g (t c)")
            w2 = v2[:, :, 0:8, :].rearrange("p g t c -> p g (t c)")
            p7r = PWr[:, 14, :].unsqueeze(2).broadcast_to([64, 32, 128])
            p7i = PWi[:, 14, :].unsqueeze(2).broadcast_to([64, 32, 128])
            hr = HsR[:].rearrange("p g s c -> p g (s c)"); hi = HsI[:].rearrange("p g s c -> p g (s c)")
            self.tt("dve", w1, hr, p7r, ALU.mult, r=["p_HsR", "p_PWr"], w=["p_v1"])
            self.tt("dve", w2, hi, p7i, ALU.mult, r=["p_HsI", "p_PWi"], w=["p_v2"])
            self.tt("dve", GsR, w1, w2, ALU.subtract, r=["p_v1", "p_v2"], w=["p_LtR"])
            self.tt("dve", w1, hi, p7r, ALU.mult, r=["p_HsI", "p_PWr"], w=["p_v1"])
            self.tt("dve", w2, hr, p7i, ALU.mult, r=["p_HsR", "p_PWi"], w=["p_v2"])
            self.tt("dve", GsI, w1, w2, ALU.add, r=["p_v1", "p_v2"], w=["p_nLtI"])
            for g4 in range(8):
                pb = 2 + g4 % 2
                pk = "ps%d" % pb
                pv = self.ps[pb][:].rearrange("p (g r n) -> p g r n", g=4, r=2)
                for gl in range(4):
                    g = g4 * 4 + gl
                    c.op("pe", lambda e: e.transpose(out=pv[:, gl, 0, :], in_=GsR[:, g, :], identity=idf[0:64, 0:64]),
                         r=["p_LtR", "p_idf"], w=[pk])
                    c.op("pe", lambda e: e.transpose(out=pv[:, gl, 1, :], in_=GsI[:, g, :], identity=idf[0:64, 0:64]),
                         r=["p_nLtI", "p_idf"], w=[pk])
                self.cp("act", Wii[:, g4 * 4:(g4 + 1) * 4, :, :], pv, r=[pk], w=["Wii"])
            self.cp("dve", rho8[:], TE[:, 15, :], r=["p_TE"], w=["p_rho8"])
            if "toep_x" in self.export:
                c.dma(self.toep_x, Toep[:], r=["Toep"], w=["toep_x"])
                c.dma(self.wii_x, Wii[:], r=["Wii"], w=["wii_x"])
                c.dma(self.wiv_x[:, 0], WivR[:], r=["WivR"], w=["wiv_x"])
                c.dma(self.wiv_x[:, 1], WivI[:], r=["WivI"], w=["wiv_x"])
        c.barrier()
        with self.scope() as sp:
            T = lambda name, shape, dt=F32: sp.enter_context(self.sbt(name, shape, dt))
            phr = T("p_phr", [64, 32]); ph_s = T("p_phs", [64, 32]); phr2 = T("p_phr2", [64, 32])
            self.ts("dve", phr[:], thr[:], 8.0, ALU.mult, r=["p_thr"], w=["p_phr"])
            self.sincos(phr[:], "p_phr", ph_s[:], None, "p_phs", None, tfs[:], tis[:], red_o=(phr2[:], "p_phr2"))
            rho = T("p_rho", [64, 8, 256])
            ang = T("p_ang", [64, 8, 256]); sk = T("p_sk", [64, 8, 256]); ck = T("p_ck", [64, 8, 256])
            tf4 = T("p_tf4", [64, 8, 256]); ti4 = T("p_ti4", [64, 8, 256], I32)
            for gb in range(4):
                gs = slice(gb * 8, (gb + 1) * 8)
                self.cp("dve", rho[:], rho8[:, gs].unsqueeze(2).broadcast_to([64, 8, 256]), r=["p_rho8"], w=["p_rho"])
                c.op("dve", lambda e: e.memset(rho[:, :, 0:1], 0.0), r=[], w=["p_rho"])
                c.dma(self.rhot[:, gs, :], rho[:], r=["p_rho"], w=["rhot"])
                self.tt("dve", ang[:], phr2[:, gs].unsqueeze(2).broadcast_to([64, 8, 256]),
                        kv[:].unsqueeze(1).broadcast_to([64, 8, 256]), ALU.mult, r=["p_phr2", "p_kv"], w=["p_ang"])
                self.sincos(ang[:], "p_ang", sk[:], ck[:], "p_sk", "p_ck", tf4[:], ti4[:])
                c.dma(self.rots[:, gs, :], sk[:], r=["p_sk"], w=["rots"])
                c.dma(self.rotc[:, gs, :], ck[:], r=["p_ck"], w=["rotc"])
        c.barrier()
        so.close()

    def phase_s5(self):
        nc, c = self.nc, self.c
        with self.scope() as st:
            T = lambda name, shape, dt=F32: st.enter_context(self.sbt(name, shape, dt))
            Toep = T("Toep", [128, 32, 128], BF16)
            Wii = T("Wii", [128, 32, 2, 64], BF16)
            WivR = T("WivR", [64, 32, 128], BF16)
            WivI = T("WivI", [64, 32, 128], BF16)
            self.s5_precompute(st, Toep, Wii, WivR, WivI)
            if self.s5_main_enabled:
                self.s5_main(st, Toep, Wii, WivR, WivI)
        c.barrier()

    def s5_main(self, st0, Toep, Wii, WivR, WivI):
        nc, c = self.nc, self.c
        GB = 4
        with self.scope() as st:
            T = lambda name, shape, dt=F32: st.enter_context(self.sbt(name, shape, dt))
            gluw = T("gluw", [128, 4, BR], BF16)
            with self.scope() as sg:
                gst = sg.enter_context(self.sbt("gluw_st", [128, 4, BR], F32))
                c.dma(gst[:], self.glu_w.rearrange("(c p) n -> p c n", p=128), w=["gluw_st"])
                self.cp("dve", gluw[:], gst[:], r=["gluw_st"], w=["gluw"])
            Drep = T("Drep", [128, BR]); Brep = T("Brep", [128, BR])
            c.dma(Drep[:], self.s5d_rep, w=["Drep"])
            c.dma(Brep[:], self.glub_rep, w=["Brep"])
            for q in range(self.nseq):
                tb = q * self.S
                with self.scope() as sq:
                    TQ = lambda name, shape, dt=F32: sq.enter_context(self.sbt(name, shape, dt))
                    uck = [TQ("uck%d" % i, [128, 8 * BR], BF16) for i in range(2)]
                    szck = [TQ("szck%d" % i, [128, 8 * BR], BF16) for i in range(2)]
                    yck = [TQ("yck%d" % i, [128, 8, BR], BF16) for i in range(2)]
                    for kh in range(2):
                        rows = slice(tb + kh * 1024, tb + (kh + 1) * 1024)
                        c.dma(uck[kh][:], self.u_tok[rows, :].rearrange("(k t) f -> k (t f)", t=8), r=["u_tok"], w=["uck%d" % kh])
                        c.dma(szck[kh][:], self.sz_tok[rows, :].rearrange("(k t) f -> k (t f)", t=8), r=["sz_tok"], w=["szck%d" % kh])
                    with self.scope() as ss:
                        TS = lambda name, shape, dt=F32: ss.enter_context(self.sbt(name, shape, dt))
                        Ug = TS("Ug", [128, 32, 256], BF16)
                        Vb2 = [TS("Vb%d" % i, [64, 2, GB, 256]) for i in range(2)]
                        Wb2 = [TS("Wb%d" % i, [64, 2, GB, 256]) for i in range(2)]
                        tA2 = [TS("tA%d" % i, [64, GB, 256]) for i in range(2)]; tB2 = [TS("tB%d" % i, [64, GB, 256]) for i in range(2)]
                        tC2 = [TS("tC0", [64, GB, 256])] * 2; tD2 = [TS("tD0", [64, GB, 256])] * 2
                        ck2 = [TS("ck%d" % i, [64, GB, 256]) for i in range(2)]; sk2 = [TS("sk%d" % i, [64, GB, 256]) for i in range(2)]
                        rh2 = [TS("rh%d" % i, [64, GB, 256]) for i in range(2)]
                        Xs2 = [TS("Xs%d" % i, [64, 2, GB, 256], BF16) for i in range(2)]
                        Ysb = TS("Ysb", [128, 8, 256], BF16)
                        for i in range(2):
                            c.op("pool", lambda e: e.memset(Xs2[i][:], 0.0), w=["Xs%d" % i])

                        def load_tabs(b):
                            if b < 32 // GB:
                                gs_ = slice(b * GB, (b + 1) * GB)
                                c.dma(ck2[b % 2][:], self.rotc[:, gs_, :], r=["rotc"], w=["ck%d" % (b % 2)])
                                c.dma(sk2[b % 2][:], self.rots[:, gs_, :], r=["rots"], w=["sk%d" % (b % 2)])
                                c.dma(rh2[b % 2][:], self.rhot[:, gs_, :], r=["rhot"], w=["rh%d" % (b % 2)])
                        load_tabs(0)
                        ucg = TS("ucg", [128, 32, 128], BF16)
                        for kh in range(2):
                            self.cp("pool" if kh == 0 else "dve", ucg[:].rearrange("p g (s c) -> p g s c", s=8),
                                    uck[kh][:].rearrange("p (s g c) -> p g s c", s=8, g=32), r=["uck%d" % kh], w=["ucg"])
                            for g8 in range(4):
                                pb = g8 % 2
                                pk = "ps%d" % pb
                                ptv = self.ps[pb][:].bitcast(BF16).rearrange("p (g k) -> p g k", g=8)
                                for gl in range(8):
                                    g = g8 * 8 + gl
                                    c.op("pe", lambda e: e.transpose(out=ptv[:, gl, :], in_=ucg[:, g, :],
                                                                     identity=self.ident[:]), r=["ucg", "ident"], w=[pk])
                                self.cp("dve" if g8 % 2 == 0 else "act", Ug[:, g8 * 8:(g8 + 1) * 8, kh * 128:(kh + 1) * 128], ptv,
                                        r=[pk], w=["Ug"])
                        for b in range(32 // GB):
                            gs = slice(b * GB, (b + 1) * GB)
                            bp = b % 2
                            Vb, Wb, tA, tB, ck, sk, rh, Xs = Vb2[bp], Wb2[bp], tA2[bp], tB2[bp], ck2[bp], sk2[bp], rh2[bp], Xs2[bp]
                            tC, tD = tC2[bp], tD2[bp]
                            ktC, ktD = "tC0", "tD0"
                            kVb, kW0, kW1, ktA, ktB, kck, ksk, krh, kXs = ("Vb%d" % bp, "Wb0_%d" % bp, "Wb1_%d" % bp, "tA%d" % bp, "tB%d" % bp,
                                                                           "ck%d" % bp, "sk%d" % bp, "rh%d" % bp, "Xs%d" % bp)
                            load_tabs(b + 1)
                            for gl in range(GB):
                                g = b * GB + gl
                                pb = 2 + gl % 2
                                pk = "ps%d" % pb
                                c.op("pe", lambda e: e.matmul(out=self.ps[pb][0:64, 0:256], lhsT=Wii[:, g, 0, :], rhs=Ug[:, g, :],
                                                              start=True, stop=True), r=["Wii", "Ug"], w=[pk])
                                c.op("pe", lambda e: e.matmul(out=self.ps[pb][0:64, 256:512], lhsT=Wii[:, g, 1, :], rhs=Ug[:, g, :],
                                                              start=True, stop=True), r=["Wii", "Ug"], w=[pk])
                                self.cp("act", Vb[:, :, gl, :], self.ps[pb][0:64, :].rearrange("p (r k) -> p r k", r=2), r=[pk], w=[kVb])
                            self.tt("pool", tB[:], sk[:], Vb[:, 1], ALU.mult, r=[ksk, kVb], w=[ktB])
                            self.tt("dve", tA[:], ck[:], Vb[:, 0], ALU.mult, r=[kck, kVb], w=[ktA])
                            self.tt("dve", tC[:], ck[:], Vb[:, 1], ALU.mult, r=[kck, kVb], w=[ktC])
                            self.tt("dve", tD[:], sk[:], Vb[:, 0], ALU.mult, r=[ksk, kVb], w=[ktD])
                            self.tt("dve", Wb[:, 1], tC[:], tD[:], ALU.subtract, r=[ktC, ktD], w=[kW1])
                            self.tt("dve", Wb[:, 0], tA[:], tB[:], ALU.add, r=[ktA, ktB], w=[kW0])
                            fl = lambda a: a.rearrange("p g k -> p (g k)")
                            c.op("dve", lambda e: e.tensor_tensor_scan(out=fl(Vb[:, 1]), data0=fl(rh[:]), data1=fl(Wb[:, 1]), initial=0.0,
                                                                       op0=ALU.mult, op1=ALU.add), r=[krh, kW1, kVb], w=[kVb])
                            c.op("dve", lambda e: e.tensor_tensor_scan(out=fl(Vb[:, 0]), data0=fl(rh[:]), data1=fl(Wb[:, 0]), initial=0.0,
                                                                       op0=ALU.mult, op1=ALU.add), r=[krh, kW0, kVb], w=[kVb])
                            K1 = 255
                            self.tt("pool", tB[:, :, 0:K1], sk[:, :, 0:K1], Vb[:, 1, :, 0:K1], ALU.mult, r=[ksk, kVb], w=[ktB])
                            self.tt("dve", tA[:, :, 0:K1], ck[:, :, 0:K1], Vb[:, 0, :, 0:K1], ALU.mult, r=[kck, kVb], w=[ktA])
                            self.tt("dve", tC[:, :, 0:K1], ck[:, :, 0:K1], Vb[:, 1, :, 0:K1], ALU.mult, r=[kck, kVb], w=[ktC])
                            self.tt("dve", tD[:, :, 0:K1], sk[:, :, 0:K1], Vb[:, 0, :, 0:K1], ALU.mult, r=[ksk, kVb], w=[ktD])
                            self.tt("dve", Xs[:, 1, :, 1:256], tC[:, :, 0:K1], tD[:, :, 0:K1], ALU.add, r=[ktC, ktD], w=[kXs])
                            self.tt("dve", Xs[:, 0, :, 1:256], tA[:, :, 0:K1], tB[:, :, 0:K1], ALU.subtract, r=[ktA, ktB], w=[kXs])
                            for gl in range(GB):
                                g = b * GB + gl
                                pb = 4 + gl // 2 % 2
                                pk = "ps%d" % pb
                                o = self.ps[pb][:, (gl % 2) * 256:(gl % 2 + 1) * 256]
                                c.op("pe", lambda e: e.matmul(out=o, lhsT=Toep[:, g, :], rhs=Ug[:, g, :], start=True, stop=False),
                                     r=["Toep", "Ug"], w=[pk])
                                c.op("pe", lambda e: e.matmul(out=o, lhsT=WivR[:, g, :], rhs=Xs[:, 0, gl, :], start=False, stop=False),
                                     r=["WivR", kXs], w=[pk])
                                c.op("pe", lambda e: e.matmul(out=o, lhsT=WivI[:, g, :], rhs=Xs[:, 1, gl, :], start=False, stop=True),
                                     r=["WivI", kXs], w=[pk])
                                if gl % 2 == 1:
                                    g8l = (b * GB + gl - 1) % 8
                                    self.cp("act", Ysb[:, g8l:g8l + 2, :], self.ps[pb][:].rearrange("p (g k) -> p g k", g=2), r=[pk], w=["Ysb"])
                            if (b * GB + GB) % 8 == 0:
                                g8 = (b * GB) // 8
                                for kh in range(2):
                                    pb = 6 + kh
                                    pk = "ps%d" % pb
                                    ptv = self.ps[pb][:].bitcast(BF16).rearrange("p (g n) -> p g n", g=8)
                                    for gl in range(8):
                                        c.op("pe", lambda e: e.transpose(out=ptv[:, gl, :], in_=Ysb[:, gl, kh * 128:(kh + 1) * 128],
                                                                         identity=self.ident[:]), r=["Ysb", "ident"], w=[pk])
                                    self.cp("dve", yck[kh][:, :, g8 * 128:(g8 + 1) * 128].rearrange("p t (g c) -> p t g c", g=8),
                                            ptv.rearrange("p g (t c) -> p t g c", t=8), r=[pk], w=["yck%d" % kh])
                    with self.scope() as se:
                        TE_ = lambda name, shape, dt=F32: se.enter_context(self.sbt(name, shape, dt))
                        t1 = TE_("e_t1", [128, 8, BR]); t2 = TE_("e_t2", [128, 8, BR])
                        gck = TE_("gck", [128, 8, BR], BF16)
                        ack = TE_("ack", [128, 8, BR], BF16)
                        gT = [TE_("gT%d" % i, [128, 4, 128], BF16) for i in range(2)]
                        e1 = [TE_("e1_%d" % i, [128, BR]) for i in range(2)]
                        for kh in range(2):
                            uv = uck[kh][:].rearrange("p (s f) -> p s f", s=8)
                            zv = szck[kh][:].rearrange("p (s f) -> p s f", s=8)
                            self.tt("dve", t1[:], uv, Drep[:].unsqueeze(1).broadcast_to([128, 8, BR]), ALU.mult, r=["uck%d" % kh, "Drep"], w=["e_t1"])
                            self.tt("dve", t1[:], t1[:], yck[kh][:], ALU.add, r=["e_t1", "yck%d" % kh], w=["e_t1"])
                            self.actf(t2[:], t1[:], AF.Square, r=["e_t1"], w=["e_t2"])
                            self.actf(t2[:], t2[:], AF.Copy, r=["e_t2"], w=["e_t2"], scale=0.044715 * 0.7978845608, bias=0.7978845608)
                            self.tt("dve", t2[:], t2[:], t1[:], ALU.mult, r=["e_t2", "e_t1"], w=["e_t2"])
                            self.actf(t2[:], t2[:], AF.Tanh, r=["e_t2"], w=["e_t2"])
                            self.actf(t2[:], t2[:], AF.Copy, r=["e_t2"], w=["e_t2"], scale=0.5, bias=0.5)
                            self.tt("dve", gck[:], t2[:], t1[:], ALU.mult, r=["e_t2", "e_t1"], w=["gck"])
                            def glu_front(tau):
                                i2 = tau % 2
                                pk = "ps%d" % i2
                                ptv = self.ps[i2][:].bitcast(BF16).rearrange("p (a b) -> p a b", a=8)
                                for fc in range(4):
                                    c.op("pe", lambda e: e.transpose(out=ptv[:, fc, :], in_=gck[:, tau, fc * 128:(fc + 1) * 128],
                                                                     identity=self.ident[:]), r=["gck", "ident"], w=[pk])
                                self.cp("act", gT[i2][:], ptv[:, 0:4, :], r=[pk], w=["gT%d" % i2])
                            glu_front(0)
                            for tau in range(8):
                                i2 = tau % 2
                                pm = 2 + i2
                                for fc in range(4):
                                    c.op("pe", lambda e: e.matmul(out=self.ps[pm][:], lhsT=gT[i2][:, fc, :], rhs=gluw[:, fc, :],
                                                                  start=(fc == 0), stop=(fc == 3)), r=["gT%d" % i2, "gluw"], w=["ps%d" % pm])
                                if tau + 1 < 8:
                                    glu_front(tau + 1)
                                ek = "e1_%d" % i2
                                self.tt("dve", e1[i2][:], self.ps[pm][:], Brep[:], ALU.add, r=["ps%d" % pm, "Brep"], w=[ek])
                                self.actf(e1[i2][:], e1[i2][:], AF.Tanh, r=[ek], w=[ek], scale=0.5)
                                self.ts("dve", e1[i2][:], e1[i2][:], 0.5, ALU.mult, r=[ek], w=[ek], s2=0.5, op1=ALU.add)
                                self.tt("pool", e1[i2][:], e1[i2][:], gck[:, tau, :], ALU.mult, r=[ek, "gck"], w=[ek])
                                self.tt("dve", ack[:, tau, :], e1[i2][:], zv[:, tau, :], ALU.mult, r=[ek, "szck%d" % kh], w=["ack"])
                            rows = slice(tb + kh * 1024, tb + (kh + 1) * 1024)
                            c.dma(self.a_tok[rows, :].rearrange("(k t) f -> k (t f)", t=8), ack[:].rearrange("p t f -> p (t f)"),
                                  r=["ack"], w=["a_tok"])

    def phase_ml(self):
        nc, c = self.nc, self.c
        S_ = self.S
        NB = S_ // 128
        SC = 128 ** -0.5
        with self.scope() as st:
            T = lambda name, shape, dt=F32: st.enter_context(self.sbt(name, shape, dt))
            cw = T("m_cw", [128, 4, 4]); cb = T("m_cb", [128, 4])
            mn = T("m_mn", [128, 4]); msk = T("m_msk", [128, 4])
            gbi = T("m_gbi", [4, 1]); gbf = T("m_gbf", [4, 1]); ngbf = T("m_ngbf", [4, 1])
            maskS = T("m_maskS", [128, 128]); idf = T("m_idf", [128, 128])
            ones4 = T("m_ones4", [4, 128])
            wst = T("m_wst", [128, 3, 4, 128]); wqkv = T("m_wqkv", [128, 3, 4, 128], BF16)
            gst = T("m_gst", [128, 12, 8]); gw = T("m_gw", [128, 12, 8], BF16)
            for (t, d, key) in ((cw, self.ml_cw, "m_cw"), (cb, self.ml_cb, "m_cb"), (mn, self.ml_mn, "m_mn"),
                                (msk, self.ml_msk, "m_msk"), (gbi, self.ml_gbi, "m_gbi"), (gbf, self.ml_gbf, "m_gbf"),
                                (maskS, self.ml_maskS, "m_maskS"), (idf, self.identf_d, "m_idf"),
                                (gst, self.ml_gw, "m_gst")):
                c.dma(t[:], d, w=[key])
            for i, d in enumerate((self.ml_wq, self.ml_wk, self.ml_wv)):
                c.dma(wst[:, i], d, w=["m_wst"])
            self.cp("dve", wqkv[:], wst[:], r=["m_wst"], w=["m_wqkv"])
            self.cp("dve", gw[:], gst[:], r=["m_gst"], w=["m_gw"])
            self.ts("dve", ngbf[:], gbf[:], -1.0, ALU.mult, r=["m_gbf"], w=["m_ngbf"])
            c.op("dve", lambda e: e.memset(ones4[:], 1.0), w=["m_ones4"])
            xmT_v = self.xmT.rearrange("(c p) t -> p c t", p=128)
            mzT_v = self.mzT.rearrange("(c p) t -> p c t", p=128)
            bT_v = self.bT.rearrange("(c p) t -> p c t", p=128)
            for q in range(self.nseq):
                tb = q * S_
                with self.scope() as sq:
                    TQ = lambda name, shape, dt=F32: sq.enter_context(self.sbt(name, shape, dt))
                    xcT = TQ("xcT", [128, 4, S_], BF16)
                    qT = TQ("qT", [128, 4, S_], BF16)
                    kT = TQ("kT", [128, 4, S_], BF16)
                    Ktok = TQ("Ktok", [128, NB, 4, 128], BF16)
                    Vtok = TQ("Vtok", [128, NB, 4, 129], BF16)
                    acol = TQ("acol", [128, NB, 4]); bcol = TQ("bcol", [128, NB, 4])
                    Rrep = TQ("Rrep", [128, NB + 1, 4])
                    Wt = TQ("Wt", [128, NB, 4]); Wp = TQ("Wp", [128, NB, 4])
                    Thr = TQ("Thr", [128, NB, 4]); Dec = TQ("Dec", [128, NB, 4])
                    c.op("pool", lambda e: e.memset(Vtok[:, :, :, 128:129], 1.0), w=["Vtok"])
                    with self.scope() as sa:
                        TA_ = lambda name, shape, dt=F32: sa.enter_context(self.sbt(name, shape, dt))
                        xm = TA_("xm", [128, 4, S_], BF16)
                        vT = TA_("vT", [128, 4, S_], BF16)
                        acc = TA_("acc", [128, S_])
                        g1f = TA_("g1", [32, S_]); g2f = TA_("g2", [32, S_]); g3f = TA_("g3", [32, S_]); onesr = TA_("onesr", [32, S_])
                        g1 = g1f[0:4, :]; g2 = g2f[0:4, :]; g3 = g3f[0:4, :]
                        c.op("pool", lambda e: e.memset(g1f[:], 0.0), w=["g1"])
                        c.op("pool", lambda e: e.memset(g2f[:], 0.0), w=["g2"])
                        rsel = TA_("rsel", [4, NB, 4])
                        c.dma(xm[:], xmT_v[:, :, tb:tb + S_], r=["xmT"], w=["xm"])
                        c.op("pool", lambda e: e.memset(onesr[:], 1.0), w=["onesr"])
                        for fc in range(4):
                            self.ts("dve", acc[:], xm[:, fc, :], cw[:, fc, 3:4], ALU.mult, r=["xm", "m_cw", "m_cb"], w=["acc"],
                                    s2=cb[:, fc:fc + 1], op1=ALU.add)
                            for sh in (1, 2, 3):
                                self.stt("dve", acc[:, sh:], xm[:, fc, 0:S_ - sh], cw[:, fc, 3 - sh:4 - sh], acc[:, sh:],
                                         ALU.mult, ALU.add, r=["xm", "m_cw", "acc"], w=["acc"])
                            self.actf(xcT[:, fc, :], acc[:], AF.Silu, r=["acc"], w=["xcT"])
                        for h in range(4):
                            for tl in range(S_ // 512):
                                ts_ = slice(tl * 512, (tl + 1) * 512)
                                for (i, src, skey, dst, dkey) in ((0, xcT, "xcT", qT, "qT"), (1, xcT, "xcT", kT, "kT"), (2, xm, "xm", vT, "vT")):
                                    pb = (h * 12 + tl * 3 + i) % 4
                                    pk = "ps%d" % pb
                                    c.op("pe", lambda e: e.matmul(out=self.ps[pb][:], lhsT=wqkv[:, i, h, :], rhs=src[:, h, ts_],
                                                                  start=True, stop=True), r=["m_wqkv", skey], w=[pk])
                                    self.cp("act" if i != 1 else "dve", dst[:, h, ts_], self.ps[pb][:], r=[pk], w=[dkey])
                        for blk in range(NB):
                            bs = slice(blk * 128, (blk + 1) * 128)
                            for (i, src, skey, dst, dkey, pb) in ((1, xcT, "xcT", Ktok, "Ktok", 4), (2, xm, "xm", Vtok, "Vtok", 5)):
                                pb = pb + 2 * (blk % 2)
                                pk = "ps%d" % pb
                                for h in range(4):
                                    c.op("pe", lambda e: e.matmul(out=self.ps[pb][:, h * 128:(h + 1) * 128], lhsT=src[:, h, bs],
                                                                  rhs=wqkv[:, i, h, :], start=True, stop=True), r=["m_wqkv", skey], w=[pk])
                                self.cp("act" if i == 1 else "dve", dst[:, blk, :, 0:128], self.ps[pb][:].rearrange("p (h e) -> p h e", h=4),
                                        r=[pk], w=[dkey])
                        for tl in range(S_ // 512):
                            ts_ = slice(tl * 512, (tl + 1) * 512)
                            for half in range(2):
                                pb = half
                                pk = "ps%d" % pb
                                for ch in range(12):
                                    src = (qT, kT, vT)[ch // 4]
                                    skey = ("qT", "kT", "vT")[ch // 4]
                                    c.op("pe", lambda e: e.matmul(out=self.ps[pb][0:4, :], lhsT=gw[:, ch, half * 4:half * 4 + 4],
                                                                  rhs=src[:, ch % 4, ts_], start=(ch == 0), stop=(ch == 11)),
                                         r=["m_gw", skey], w=[pk])
                                if half == 0:
                                    self.ts("dve", g1[:, ts_], self.ps[pb][0:4, :], gbi[:, 0:1], ALU.add, r=[pk, "m_gbi"], w=["g1"])
                                else:
                                    self.actf(g2[:, ts_], self.ps[pb][0:4, :], AF.Exp, r=[pk, "m_ngbf"], w=["g2"], scale=-1.0, bias=ngbf[:, 0:1])
                        self.actf(g2[:], g2[:], AF.Ln, r=["g2"], w=["g2"], bias=1.0)
                        c.op("dve", lambda e: e.tensor_tensor_scan(out=g3f[:], data0=onesr[:], data1=g2f[:], initial=0.0, op0=ALU.mult, op1=ALU.add),
                             r=["onesr", "g2"], w=["g3"])
                        self.tt("dve", g1[:], g1[:], g3[:], ALU.add, r=["g1", "g3"], w=["g1"])
                        c.op("dve", lambda e: e.tensor_tensor_scan(out=g2f[:], data0=onesr[:], data1=g1f[:], initial=0.0, op0=ALU.mult, op1=ALU.max),
                             r=["onesr", "g1", "g2"], w=["g2"])
                        pa = self.ps[2][:, 0:NB * 4].rearrange("p (b h) -> p b h", h=4)
                        pbn = self.ps[3][:, 0:NB * 4].rearrange("p (b h) -> p b h", h=4)
                        for blk in range(NB):
                            bs = slice(blk * 128, (blk + 1) * 128)
                            c.op("pe", lambda e: e.transpose(out=pa[:, blk, :], in_=g1[:, bs], identity=idf[0:4, 0:4]), r=["g1", "m_idf"], w=["ps2"])
                            c.op("pe", lambda e: e.transpose(out=pbn[:, blk, :], in_=g3[:, bs], identity=idf[0:4, 0:4]), r=["g3", "m_idf"], w=["ps3"])
                        self.cp("dve", acol[:], pa, r=["ps2"], w=["acol"])
                        self.cp("dve", bcol[:], pbn, r=["ps3"], w=["bcol"])
                        self.tt("dve", rsel[:], g2[:, 127::128].unsqueeze(2).broadcast_to([4, NB, 4]),
                                idf[0:4, 0:4].unsqueeze(1).broadcast_to([4, NB, 4]), ALU.mult, r=["g2", "m_idf"], w=["rsel"])
                        c.op("pe", lambda e: e.matmul(out=self.ps[0][:, 0:NB * 4], lhsT=ones4[:], rhs=rsel[:].rearrange("p b h -> p (b h)"),
                                                      start=True, stop=True), r=["m_ones4", "rsel"], w=["ps0"])
                        c.op("dve", lambda e: e.memset(Rrep[:, 0, :], 0.0), w=["Rrep"])
                        self.cp("dve", Rrep[:, 1:NB + 1, :], self.ps[0][:, 0:NB * 4].rearrange("p (b h) -> p b h", h=4), r=["ps0"], w=["Rrep"])
                    self.tt("dve", Wt[:], acol[:], Rrep[:, 0:NB, :], ALU.subtract, r=["acol", "Rrep"], w=["Wt"])
                    self.actf(Wt[:], Wt[:], AF.Exp, r=["Wt"], w=["Wt"])
                    self.tt("dve", Wp[:], acol[:], Rrep[:, 1:NB + 1, :], ALU.subtract, r=["acol", "Rrep"], w=["Wp"])
                    self.actf(Wp[:], Wp[:], AF.Exp, r=["Wp"], w=["Wp"])
                    self.ts("dve", Wp[:], Wp[:], SC, ALU.mult, r=["Wp"], w=["Wp"])
                    self.tt("dve", Thr[:], bcol[:], Rrep[:, 0:NB, :], ALU.subtract, r=["bcol", "Rrep"], w=["Thr"])
                    self.actf(Thr[:], Thr[:], AF.Exp, r=["Thr"], w=["Thr"])
                    self.tt("dve", Dec[:], Rrep[:, 0:NB, :], Rrep[:, 1:NB + 1, :], ALU.subtract, r=["Rrep"], w=["Dec"])
                    self.actf(Dec[:], Dec[:], AF.Exp, r=["Dec"], w=["Dec"])
                    with self.scope() as sm:
                        TM = lambda name, shape, dt=F32: sm.enter_context(self.sbt(name, shape, dt))
                        C32 = TM("C32", [128, 4, 129]); Cm = TM("Cm", [128, 4, 129])
                        Cb = TM("Cb", [128, 4, 129], BF16)
                        PT4 = [TM("PT4_%d" % i, [128, 4, 128], BF16) for i in range(2)]
                        Vp4 = [TM("Vp4_%d" % i, [128, 4, 129], BF16) for i in range(2)]
                        Vpp4 = [TM("Vpp4_%d" % i, [128, 4, 129], BF16) for i in range(2)]
                        den = TM("den4", [128, 4])
                        hraw = [TM("hraw%d" % i, [128, 4, 128]) for i in range(2)]
                        bst = TM("bst", [128, 4, 6]); mv = TM("mv", [128, 4, 2]); rs = TM("rs", [128, 4])
                        hn = TM("hn", [128, 4, 128], BF16)
                        e1 = TM("m_e1", [128, 4, 128]); e2 = TM("m_e2", [128, 4, 128])
                        mz = [TM("mz%d" % i, [128, 4, 512], BF16) for i in range(2)]
                        bo = [TM("bo%d" % i, [128, 4, 512], BF16) for i in range(2)]

                        def emit_front(I_):
                            bs_ = slice(I_ * 128, (I_ + 1) * 128)
                            j_ = I_ % 2
                            for h_ in range(4):
                                c.op("pe", lambda e: e.matmul(out=self.ps[j_][:, h_ * 128:(h_ + 1) * 128], lhsT=kT[:, h_, bs_], rhs=qT[:, h_, bs_],
                                                              start=True, stop=True), r=["kT", "qT"], w=["ps%d" % j_])
                            self.tt("dve", PT4[j_][:], self.ps[j_][:].rearrange("p (h t) -> p h t", h=4),
                                    maskS[:].unsqueeze(1).broadcast_to([128, 4, 128]), ALU.mult, r=["ps%d" % j_, "m_maskS"], w=["PT4_%d" % j_])
                            for h_ in range(4):
                                c.op("act", lambda e: e.activation(out=Vp4[j_][:, h_, :], in_=Vtok[:, I_, h_, :], func=AF.Copy, scale=Wt[:, I_, h_:h_ + 1]),
                                     r=["Vtok", "Wt"], w=["Vp4_%d" % j_])
                                c.op("act", lambda e: e.activation(out=Vpp4[j_][:, h_, :], in_=Vtok[:, I_, h_, :], func=AF.Copy, scale=Wp[:, I_, h_:h_ + 1]),
                                     r=["Vtok", "Wp"], w=["Vpp4_%d" % j_])
                        emit_front(0)
                        for I in range(NB):
                            bs = slice(I * 128, (I + 1) * 128)
                            i4 = (I // 4) % 2
                            j = I % 2
                            if I % 4 == 0:
                                c.dma(mz[i4][:], mzT_v[:, :, tb + I * 128:tb + I * 128 + 512], r=["mzT"], w=["mz%d" % i4])
                            hk = "hraw%d" % j
                            for h in range(4):
                                pO = self.ps[2 + h // 2][:, (h % 2) * 129:(h % 2 + 1) * 129]
                                kO = "ps%d" % (2 + h // 2)
                                c.op("pe", lambda e: e.matmul(out=pO, lhsT=PT4[j][:, h, :], rhs=Vp4[j][:, h, :], start=True, stop=(I == 0)),
                                     r=["PT4_%d" % j, "Vp4_%d" % j], w=[kO])
                                if I > 0:
                                    c.op("pe", lambda e: e.matmul(out=pO, lhsT=qT[:, h, bs], rhs=Cb[:, h, :], start=False, stop=True),
                                         r=["qT", "Cb"], w=[kO])
                            if I < NB - 1:
                                for h in range(4):
                                    pC = self.ps[4 + h // 2][:, (h % 2) * 129:(h % 2 + 1) * 129]
                                    c.op("pe", lambda e: e.matmul(out=pC, lhsT=Ktok[:, I, h, :], rhs=Vpp4[j][:, h, :], start=True, stop=True),
                                         r=["Ktok", "Vpp4_%d" % j], w=["ps%d" % (4 + h // 2)])
                            if I + 1 < NB:
                                emit_front(I + 1)
                            if I < NB - 1:
                                if I == 0:
                                    for hb in range(2):
                                        self.cp("dve", C32[:, 2 * hb:2 * hb + 2, :], self.ps[4 + hb][:, 0:258].rearrange("p (h e) -> p h e", h=2),
                                                r=["ps%d" % (4 + hb)], w=["C32"])
                                else:
                                    self.tt("pool", Cm[:], C32[:], Dec[:, I, :].unsqueeze(2).broadcast_to([128, 4, 129]), ALU.mult, r=["C32", "Dec"], w=["Cm"])
                                    for hb in range(2):
                                        self.tt("dve", C32[:, 2 * hb:2 * hb + 2, :], self.ps[4 + hb][:, 0:258].rearrange("p (h e) -> p h e", h=2),
                                                Cm[:, 2 * hb:2 * hb + 2, :], ALU.add, r=["ps%d" % (4 + hb), "Cm"], w=["C32"])
                                self.cp("act", Cb[:], C32[:], r=["C32"], w=["Cb"])
                            for hb in range(2):
                                self.actf(den[:, 2 * hb:2 * hb + 2], self.ps[2 + hb][:, 0:258].rearrange("p (h e) -> p h e", h=2)[:, :, 128],
                                          AF.Abs, r=["ps%d" % (2 + hb)], w=["den4"])
                            self.tt("dve", den[:], den[:], Thr[:, I, :], ALU.max, r=["den4", "Thr"], w=["den4"])
                            c.op("dve", lambda e: e.reciprocal(out=den[:], in_=den[:]), r=["den4"], w=["den4"])
                            for hb in range(2):
                                self.tt("dve", hraw[j][:, 2 * hb:2 * hb + 2, :], self.ps[2 + hb][:, 0:258].rearrange("p (h e) -> p h e", h=2)[:, :, 0:128],
                                        den[:, 2 * hb:2 * hb + 2].unsqueeze(2).broadcast_to([128, 2, 128]), ALU.mult, r=["ps%d" % (2 + hb), "den4"], w=[hk])
                            mvk = ["mv%d" % h for h in range(4)]
                            for h in range(4):
                                c.op("dve", lambda e: e.bn_stats(out=bst[:, h, :], in_=hraw[j][:, h, :]), r=[hk], w=["bst%d" % h])
                            for h in range(4):
                                c.op("dve", lambda e: e.bn_aggr(out=mv[:, h, :], in_=bst[:, h, :]), r=["bst%d" % h], w=["mv%d" % h])
                            self.actf(rs[:], mv[:, :, 1], AF.Ln, r=mvk, w=["rs"], bias=HEAD_EPS)
                            self.actf(rs[:], rs[:], AF.Exp, r=["rs"], w=["rs"], scale=-0.5)
                            self.tt("pool", e1[:], hraw[j][:], mv[:, :, 0:1].broadcast_to([128, 4, 128]), ALU.subtract, r=[hk] + mvk, w=["m_e1"])
                            self.tt("dve", hn[:], e1[:], rs[:].unsqueeze(2).broadcast_to([128, 4, 128]), ALU.mult, r=["m_e1", "rs"], w=["hn"])
                            ptv = self.ps[6 + I % 2][:].bitcast(BF16).rearrange("p (a b) -> p a b", a=8)
                            pk = "ps%d" % (6 + I % 2)
                            for h in range(4):
                                c.op("pe", lambda e: e.transpose(out=ptv[:, h, :], in_=hn[:, h, :], identity=self.ident[:]), r=["hn", "ident"], w=[pk])
                            self.tt("pool", e2[:], xcT[:, :, bs], msk[:].unsqueeze(2).broadcast_to([128, 4, 128]), ALU.mult, r=["xcT", "m_msk"], w=["m_e2"])
                            self.tt("dve", e1[:], ptv[:, 0:4, :], mn[:].unsqueeze(2).broadcast_to([128, 4, 128]), ALU.mult, r=[pk, "m_mn", "m_e1"], w=["m_e1"])
                            self.tt("dve", e1[:], e1[:], e2[:], ALU.add, r=["m_e1", "m_e2"], w=["m_e1"])
                            off = (I % 4) * 128
                            self.tt("pool", bo[i4][:, :, off:off + 128], e1[:], mz[i4][:, :, off:off + 128], ALU.mult,
                                    r=["m_e1", "mz%d" % i4], w=["bo%d" % i4])
                            if I % 4 == 3:
                                c.dma(bT_v[:, :, tb + (I - 3) * 128:tb + (I + 1) * 128], bo[i4][:], r=["bo%d" % i4], w=["bT"])
        c.barrier()

    def out_proj_norm_res(self, lay, wout_d, pg_rep_d, lhs_provider, res_rows, dst_rows, dst_key, res_key):
        nc, c = self.nc, self.c
        NT = self.NT
        with self.scope() as st:
            T = lambda name, shape, dt=F32: st.enter_context(self.sbt(name, shape, dt))
            wout = T("wout", [128, 8, D], BF16)
            wst = [T("wost%d" % i, [128, D]) for i in range(2)]
            for fc in range(8):
                c.dma(wst[fc % 2][:], wout_d[fc * 128:(fc + 1) * 128, :], w=["wost%d" % (fc % 2)])
                self.cp("dve" if fc % 2 else "act", wout[:, fc, :], wst[fc % 2][:], r=["wost%d" % (fc % 2)], w=["wout"])
            pg = T("pg", [128, D])
            c.dma(pg[:], pg_rep_d, w=["pg"])
            NXR = 4
            xr = [T("xr%d" % i, [128, D]) for i in range(NXR)]
            yo = [T("yo%d" % i, [128, D]) for i in range(2)]
            junk = T("ojunk", [128, BR])
            ss2 = [T("oss%d" % i, [128, 2]) for i in range(2)]; rstd2 = [T("orstd%d" % i, [128, 1]) for i in range(2)]
            pref, prov = lhs_provider(st)
            nblk = NT // 128

            def prefetch(b):
                if b < nblk:
                    c.dma(xr[b % NXR][:], res_rows[b * 128:(b + 1) * 128, :], r=[res_key % b], w=["xr%d" % (b % NXR)])
                    pref(b)
            prefetch(0)
            prefetch(1)
            lhs_next = prov(0)
            for blk in range(nblk):
                rows = slice(blk * 128, (blk + 1) * 128)
                xk = "xr%d" % (blk % NXR)
                prefetch(blk + 2)
                lhs = lhs_next
                ss = ss2[blk % 2]; rstd = rstd2[blk % 2]
                ssk = "oss%d" % (blk % 2); rsk = "orstd%d" % (blk % 2)
                for half in range(2):
                    pb = 4 + half + 2 * (blk % 2)
                    pk = "ps%d" % pb
                    for fc in range(8):
                        ap, key = lhs[fc]
                        c.op("pe", lambda e: e.matmul(out=self.ps[pb][:], lhsT=ap, rhs=wout[:, fc, half * BR:(half + 1) * BR],
                                                      start=(fc == 0), stop=(fc == 7)), r=[key, "wout"], w=[pk])
                if blk + 1 < nblk:
                    lhs_next = prov(blk + 1)
                for half in range(2):
                    pb = 4 + half + 2 * (blk % 2)
                    pk = "ps%d" % pb
                    self.actf(junk[:], self.ps[pb][:], AF.Square, r=[pk], w=["ojunk", ssk], accum_out=ss[:, half:half + 1])
                self.tt("dve", rstd[:], ss[:, 0:1], ss[:, 1:2], ALU.add, r=[ssk], w=[rsk])
                self.actf(rstd[:], rstd[:], AF.Ln, r=[rsk], w=[rsk], scale=1.0 / D, bias=NORM_EPS)
                self.actf(rstd[:], rstd[:], AF.Exp, r=[rsk], w=[rsk], scale=-0.5)
                yk = "yo%d" % (blk % 2)
                for half in range(2):
                    pb = 4 + half + 2 * (blk % 2)
                    hs = slice(half * BR, (half + 1) * BR)
                    self.tt("dve", yo[blk % 2][:, hs], self.ps[pb][:], pg[:, hs], ALU.mult, r=["ps%d" % pb, "pg"], w=[yk])
                self.stt("dve", yo[blk % 2][:], yo[blk % 2][:], rstd[:, 0:1], xr[blk % NXR][:], ALU.mult, ALU.add, r=[yk, rsk, xk], w=[yk])
                c.dma(dst_rows[rows, :], yo[blk % 2][:], r=[yk], w=[dst_key % blk])

    def phase_l0_out(self):
        nc, c = self.nc, self.c
        bT_v = self.bT.rearrange("(c p) t -> p c t", p=128)

        def provider(st):
            T = lambda name, shape, dt=F32: st.enter_context(self.sbt(name, shape, dt))
            at = [T("at%d" % i, [128, BR], BF16) for i in range(4)]
            aT = [T("aT%d" % i, [128, 4, 128], BF16) for i in range(2)]
            bt = [T("bt%d" % i, [128, 4, 512], BF16) for i in range(2)]

            def pref(blk):
                i4 = (blk // 4) % 2
                c.dma(at[blk % 4][:], self.a_tok[blk * 128:(blk + 1) * 128, :], r=["a_tok"], w=["at%d" % (blk % 4)])
                if blk % 4 == 0:
                    c.dma(bt[i4][:], bT_v[:, :, blk * 128:blk * 128 + 512], r=["bT"], w=["bt%d" % i4])

            def prov(blk):
                i = blk % 2
                i4 = (blk // 4) % 2
                pk = "ps%d" % i
                ptv = self.ps[i][:].bitcast(BF16).rearrange("p (a b) -> p a b", a=8)
                for fc in range(4):
                    c.op("pe", lambda e: e.transpose(out=ptv[:, fc, :], in_=at[blk % 4][:, fc * 128:(fc + 1) * 128], identity=self.ident[:]),
                         r=["at%d" % (blk % 4), "ident"], w=[pk])
                self.cp("act", aT[i][:], ptv[:, 0:4, :], r=[pk], w=["aT%d" % i])
                off = (blk % 4) * 128
                return [(aT[i][:, fc, :], "aT%d" % i) for fc in range(4)] + \
                       [(bt[i4][:, h, off:off + 128], "bt%d" % i4) for h in range(4)]
            return pref, prov
        self.out_proj_norm_res(0, self.w_out_ab, self.post0_rep, provider, self.x, self.out, "h1_%d", "x%.0d")

    def phase_l1_proj(self):
        nc, c = self.nc, self.c
        NT = self.NT
        S_ = self.S
        ngrp = NT // 512
        nblk = NT // 128
        with self.scope() as st:
            T = lambda name, shape, dt=F32: st.enter_context(self.sbt(name, shape, dt))
            win = T("win1", [128, 8, 8 * BR], BF16)
            g1 = T("g1n", [128, 8])
            stage = [T("w1st%d" % i, [128, 4 * BR]) for i in range(2)]
            c.dma(g1[:], self.pre1, w=["g1n"])
            for dc in range(8):
                for hf in range(2):
                    i = (dc * 2 + hf) % 2
                    sk = "w1st%d" % i
                    c.dma(stage[i][:], self.w_in_cd[dc * 128:(dc + 1) * 128, hf * 2048:(hf + 1) * 2048], w=[sk])
                    if hf == 0:
                        c.op("act", lambda e: e.activation(out=win[:, dc, 0:2048], in_=stage[i][:], func=AF.Copy, scale=g1[:, dc:dc + 1]),
                             r=[sk, "g1n"], w=["win1"])
                    else:
                        self.ts("dve", win[:, dc, 2048:4096], stage[i][:], g1[:, dc:dc + 1], ALU.mult, r=[sk, "g1n"], w=["win1"])
            cosT = T("cosT", [128, S_]); sinT = T("sinT", [128, S_]); Rm = T("Rm", [128, 128], BF16)
            c.dma(cosT[:], self.rope_cos, w=["cosT"])
            c.dma(sinT[:], self.rope_sin, w=["sinT"])
            c.dma(Rm[:], self.rope_rm, w=["Rm"])
            NXB = 6
            xt = [T("x1t%d" % i, [128, D]) for i in range(NXB)]
            junk = T("junk1", [128, D])
            ss2 = [T("ss1_%d" % i, [128, 4]) for i in range(2)]; rstd2 = [T("rstd1_%d" % i, [128, 4]) for i in range(2)]
            zb = [T("z1b%d" % i, [128, D], BF16) for i in range(2)]
            zT = [T("z1T%d" % i, [128, 8, 512], BF16) for i in range(2)]
            vo = [T("vo%d" % i, [128, BR], BF16) for i in range(4)]
            xb = [T("xb%d" % i, [128, 512], BF16) for i in range(2)]
            r1 = [T("r1_%d" % i, [128, 512]) for i in range(2)]
            r2 = [T("r2_%d" % i, [128, 512]) for i in range(2)]
            fo = [T("fo%d" % i, [128, 4, 512], BF16) for i in range(2)]

            def load_x(b):
                if b < nblk:
                    c.dma(xt[b % NXB][:], self.h1src[b * 128:(b + 1) * 128, :], r=["h1_%d" % b], w=["x1t%d" % (b % NXB)])
            for b in range(4):
                load_x(b)
            fm_groups = [(0, self.cqT, "cqT", "rope"), (512, self.ckT, "ckT", "rope"), (2048, self.dqT, "dqT", "rope"),
                         (2560, self.dkT, "dkT", "rope"), (1536, self.czT, "czT", "silu"), (3584, self.dzT, "dzT", "silu")]
            cnt = 0
            for g in range(ngrp):
                zk = "z1T%d" % (g % 2)
                ss = ss2[g % 2]; rstd = rstd2[g % 2]
                ssk = "ss1_%d" % (g % 2); rsk = "rstd1_%d" % (g % 2)
                pos0 = (g * 512) % S_
                for j in range(4):
                    b = g * 4 + j
                    xk = "x1t%d" % (b % NXB)
                    c.op("act", lambda e: e.activation(out=junk[:], in_=xt[b % NXB][:], func=AF.Square, accum_out=ss[:, j:j + 1]),
                         r=[xk], w=["junk1", ssk])
                self.actf(rstd[:], ss[:], AF.Ln, r=[ssk], w=[rsk], scale=1.0 / D, bias=NORM_EPS)
                self.actf(rstd[:], rstd[:], AF.Exp, r=[rsk], w=[rsk], scale=-0.5)
                for j in range(4):
                    b = g * 4 + j
                    xk = "x1t%d" % (b % NXB)
                    zbk = "z1b%d" % (b % 2)
                    pk = "ps%d" % (b % 2)
                    self.ts("dve", zb[b % 2][:], xt[b % NXB][:], rstd[:, j:j + 1], ALU.mult, r=[xk, rsk], w=[zbk])
                    load_x(b + 4)
                    ptv = self.ps[b % 2][:].bitcast(BF16).rearrange("p (a b) -> p a b", a=8)
                    for dc in range(8):
                        c.op("pe", lambda e: e.transpose(out=ptv[:, dc, :], in_=zb[b % 2][:, dc * 128:(dc + 1) * 128], identity=self.ident[:]),
                             r=[zbk, "ident"], w=[pk])
                    self.cp("dve", zT[g % 2][:, :, j * 128:(j + 1) * 128], ptv, r=[pk], w=[zk])
                    for vi, (col0, dst, dkey) in enumerate(((1024, self.cv_tok, "cv_tok"), (3072, self.dv_tok, "dv_tok"))):
                        pb = 2 + vi
                        vk = "vo%d" % ((b % 2) * 2 + vi)
                        for dc in range(8):
                            c.op("pe", lambda e: e.matmul(out=self.ps[pb][:], lhsT=zT[g % 2][:, dc, j * 128:(j + 1) * 128],
                                                          rhs=win[:, dc, col0:col0 + BR], start=(dc == 0), stop=(dc == 7)), r=[zk, "win1"], w=["ps%d" % pb])
                        self.cp("act", vo[(b % 2) * 2 + vi][:], self.ps[pb][:], r=["ps%d" % pb], w=[vk])
                        c.dma(dst[b * 128:(b + 1) * 128, :], vo[(b % 2) * 2 + vi][:], r=[vk], w=[dkey])
                pend = []

                def flush_pend():
                    while pend:
                        (fot_, fc_, fk_, i2_, pb_, dst_, dkey_, lastfc) = pend.pop(0)
                        pr = 6 + i2_
                        c.op("pe", lambda e: e.matmul(out=self.ps[pr][:], lhsT=Rm[:], rhs=xb[i2_][:], start=True, stop=True),
                             r=["Rm", "xb%d" % i2_], w=["ps%d" % pr])
                        self.tt("dve", r1[i2_][:], self.ps[pb_][:], cosT[:, pos0:pos0 + 512], ALU.mult, r=["ps%d" % pb_, "cosT"], w=["r1_%d" % i2_])
                        self.tt("dve", r2[i2_][:], self.ps[pr][:], sinT[:, pos0:pos0 + 512], ALU.mult, r=["ps%d" % pr, "sinT"], w=["r2_%d" % i2_])
                        self.tt("pool", fot_[:, fc_, :], r1[i2_][:], r2[i2_][:], ALU.add, r=["r1_%d" % i2_, "r2_%d" % i2_], w=[fk_])
                        if lastfc:
                            c.dma(dst_.rearrange("(c p) t -> p c t", p=128)[:, :, g * 512:(g + 1) * 512], fot_[:], r=[fk_], w=[dkey_])
                for (col0, dst, dkey, kind) in fm_groups:
                    fk = "fo%d" % (cnt % 2)
                    fot = fo[cnt % 2]
                    cnt += 1
                    for fc in range(4):
                        pb = 4 + fc % 2
                        pk = "ps%d" % pb
                        for dc in range(8):
                            c.op("pe", lambda e: e.matmul(out=self.ps[pb][:], lhsT=win[:, dc, col0 + fc * 128:col0 + (fc + 1) * 128],
                                                          rhs=zT[g % 2][:, dc, :], start=(dc == 0), stop=(dc == 7)), r=[zk, "win1"], w=[pk])
                        flush_pend()
                        if kind == "silu":
                            self.actf(fot[:, fc, :], self.ps[pb][:], AF.Silu, r=[pk], w=[fk])
                            if fc == 3:
                                c.dma(dst.rearrange("(c p) t -> p c t", p=128)[:, :, g * 512:(g + 1) * 512], fot[:], r=[fk], w=[dkey])
                        else:
                            i2 = fc % 2
                            self.cp("act", xb[i2][:], self.ps[pb][:], r=[pk], w=["xb%d" % i2])
                            pend.append((fot, fc, fk, i2, pb, dst, dkey, fc == 3))
                flush_pend()

    def phase_attn(self, kind):
        nc, c = self.nc, self.c
        S_ = self.S
        NB = S_ // 128
        NQT = S_ // 512
        dil = (kind == "dil")
        qT_d, kT_d, v_d, o_d = (self.cqT, self.ckT, self.cv_tok, self.oc_tok) if dil else (self.dqT, self.dkT, self.dv_tok, self.od_tok)
        okey = "oc_tok" if dil else "od_tok"
        NH = 8 if dil else 4
        VW = 64 if dil else 128
        nmask = 9 if dil else 4
        lam_init = 0.8 - 0.6 * math.exp(-0.3 * 1)
        with self.scope() as st:
            T = lambda name, shape, dt=F32: st.enter_context(self.sbt(name, shape, dt))
            masks = T("amask", [128, nmask, 512], BF16)
            c.dma(masks[:], self.dil_masks if dil else self.diff_masks, w=["amask"])
            if not dil:
                lqk = T("lqk", [1, 4, 64]); pr = T("lpr", [1, 2, 64]); sm = T("lsm", [1, 2]); nl = T("nl", [1, 1])
                ones1 = T("ones1", [1, 128]); nlam = T("nlam", [128, 1]); gdn = T("gdn", [128, 128])
                c.dma(lqk[:], self.diff_lqk, w=["lqk"])
                c.dma(gdn[:], self.diffnorm_rep, w=["gdn"])
                c.op("dve", lambda e: e.memset(ones1[:], 1.0), w=["ones1"])
                self.tt("dve", pr[:, 0, :], lqk[:, 0, :], lqk[:, 1, :], ALU.mult, r=["lqk"], w=["lpr"])
                self.tt("dve", pr[:, 1, :], lqk[:, 2, :], lqk[:, 3, :], ALU.mult, r=["lqk", "lpr"], w=["lpr"])
                c.op("dve", lambda e: e.reduce_sum(out=sm[:], in_=pr[:], axis=AX.X), r=["lpr"], w=["lsm"])
                self.actf(sm[:], sm[:], AF.Exp, r=["lsm"], w=["lsm"])
                self.tt("dve", nl[:], sm[:, 1:2], sm[:, 0:1], ALU.subtract, r=["lsm"], w=["nl"])
                self.ts("dve", nl[:], nl[:], -lam_init, ALU.add, r=["nl"], w=["nl"])
                c.op("pe", lambda e: e.matmul(out=self.ps[0][:, 0:1], lhsT=ones1[:], rhs=nl[:], start=True, stop=True), r=["ones1", "nl"], w=["ps0"])
                self.cp("dve", nlam[:], self.ps[0][:, 0:1], r=["ps0"], w=["nlam"])
                self.ts("dve", gdn[:], gdn[:], 1.0 - lam_init, ALU.mult, r=["gdn"], w=["gdn"])
            qT_v = qT_d.rearrange("(c p) t -> p c t", p=128)
            kT_v = kT_d.rearrange("(c p) t -> p c t", p=128)
            sets = []
            for q in range(self.nseq):
                tb = q * S_
                qz_ = [T("aqz%d_%d" % (q, i), [128, 4, S_], BF16) for i in range(2)]
                kT_ = T("akT_%d" % q, [128, 4, S_], BF16)
                V1_ = T("aV1_%d" % q, [128, NB, NH, VW + 1], BF16)
                kq, kk, kv = "aqT%d" % q, "akT%d" % q, "aV1%d" % q
                c.op("pool", lambda e: e.memset(qz_[0][64:128, :, :], 0.0), w=[kq])
                c.op("pool", lambda e: e.memset(qz_[1][0:64, :, :], 0.0), w=[kq])
                c.dma(qz_[0][0:64, :, :], qT_v[0:64, :, tb:tb + S_], r=[("cqT" if dil else "dqT")], w=[kq])
                c.dma(qz_[1][64:128, :, :], qT_v[64:128, :, tb:tb + S_], r=[("cqT" if dil else "dqT")], w=[kq])
                c.dma(kT_[:], kT_v[:, :, tb:tb + S_], r=[("ckT" if dil else "dkT")], w=[kk])
                c.op("pool", lambda e: e.memset(V1_[:, :, :, VW:VW + 1], 1.0), w=[kv])
                for blk in range(NB):
                    c.dma(V1_[:, blk, :, 0:VW], v_d[tb + blk * 128:tb + (blk + 1) * 128, :].rearrange("p (h d) -> p h d", h=NH),
                          r=[("cv_tok" if dil else "dv_tok")], w=[kv])
                sets.append((qz_, kT_, V1_, kq, kk, kv))
            for q in range(self.nseq):
                tb = q * S_
                with self.scope() as sq:
                    TQ = lambda name, shape, dt=F32: sq.enter_context(self.sbt(name, shape, dt))
                    qz, kT, V1, kq, kk, kv = sets[q]
                    osb = TQ("aosb", [128, NB, BR], BF16)
                    NEP = 3
                    E = [TQ("aE%d" % i, [128, 512], BF16) for i in range(NEP)]
                    P = [TQ("aP%d" % i, [128, 512], BF16) for i in range(NEP)]
                    rd = [TQ("ard%d" % i, [128, 1]) for i in range(4)]
                    if not dil:
                        o01 = [TQ("ao%d" % i, [128, NB, 128]) for i in range(2)]
                        sqj = TQ("asq", [128, 128]); ssn = TQ("assn", [128, NB]); t3 = TQ("at3", [128, 128])
                    tiles = []
                    for h in range(NH):
                        for m in range(1 if dil else 2):
                            for qt in range(NQT):
                                nkb = 4 * qt + 4
                                for kb in range(nkb):
                                    tiles.append((h, m, qt, kb, kb == nkb - 1))
                    LA = 3
                    NSB = 4

                    def emit_S(i):
                        h, m, qt, kb, _ = tiles[i]
                        ch = h // 2 if dil else h
                        rb = 64 * (h % 2) if dil else 64 * m
                        sb = i % NSB
                        c0 = 128 * max(0, kb - 4 * qt)
                        c.op("pe", lambda e: e.matmul(out=self.ps[sb][:, c0:512], lhsT=kT[:, ch, kb * 128:(kb + 1) * 128],
                                                      rhs=qz[rb // 64][:, ch, qt * 512 + c0:(qt + 1) * 512], start=True, stop=True),
                             r=[kk, kq], w=["ps%d" % sb])
                    for i in range(min(LA, len(tiles))):
                        emit_S(i)
                    for i, (h, m, qt, kb, last) in enumerate(tiles):
                        if i + LA < len(tiles):
                            emit_S(i + LA)
                        sb = i % NSB
                        i2 = i % NEP
                        pS = self.ps[sb]
                        kS = "ps%d" % sb
                        d0 = 4 * qt - kb
                        if dil:
                            mi = 8 if d0 >= 5 else d0 + 3
                        else:
                            mi = d0 + 3 if d0 <= 0 else None
                        c0 = 128 * max(0, kb - 4 * qt)
                        if mi is None:
                            self.actf(P[i2][:, c0:512], pS[:, c0:512], AF.Exp, r=[kS], w=["aP%d" % i2], scale=0.125)
                        else:
                            self.actf(E[i2][:, c0:512], pS[:, c0:512], AF.Exp, r=[kS], w=["aE%d" % i2], scale=0.125)
                            self.tt("dve", P[i2][:, c0:512], E[i2][:, c0:512], masks[:, mi, c0:512], ALU.mult,
                                    r=["aE%d" % i2, "amask"], w=["aP%d" % i2])
                        for j in range(4):
                            Q = 4 * qt + j
                            if kb > Q:
                                continue
                            c.op("pe", lambda e: e.matmul(out=self.ps[4 + j][:, 0:VW + 1], lhsT=P[i2][:, j * 128:(j + 1) * 128],
                                                          rhs=V1[:, kb, h, :], start=(kb == 0), stop=(kb == Q)),
                                 r=["aP%d" % i2, kv], w=["ps%d" % (4 + j)])
                        if last:
                            for j in range(4):
                                c.op("dve", lambda e: e.reciprocal(out=rd[j][:], in_=self.ps[4 + j][:, VW:VW + 1]), r=["ps%d" % (4 + j)], w=["ard%d" % j])
                            for j in range(4):
                                Q = 4 * qt + j
                                pO = self.ps[4 + j]
                                kO = "ps%d" % (4 + j)
                                if dil:
                                    self.ts("dve", osb[:, Q, h * 64:(h + 1) * 64], pO[:, 0:VW], rd[j][:, 0:1], ALU.mult,
                                            r=[kO, "ard%d" % j], w=["aosb%d" % j])
                                else:
                                    self.ts("dve", o01[m][:, Q, :], pO[:, 0:VW], rd[j][:, 0:1], ALU.mult,
                                            r=[kO, "ard%d" % j], w=["ao%d_%d" % (m, j)])
                            if (not dil) and m == 1 and qt == NQT - 1:
                                aok = ["ao%d_%d" % (mm_, jj_) for mm_ in range(2) for jj_ in range(4)]
                                self.stt("dve", o01[0][:], o01[1][:], nlam[:, 0:1], o01[0][:], ALU.mult, ALU.add, r=aok + ["nlam"], w=aok + ["ao0"])
                                for Q in range(NB):
                                    self.actf(sqj[:], o01[0][:, Q, :], AF.Square, r=aok, w=["asq", "assn"], accum_out=ssn[:, Q:Q + 1])
                                self.actf(ssn[:], ssn[:], AF.Ln, r=["assn"], w=["assn"], scale=1.0 / 128, bias=HEAD_EPS)
                                self.actf(ssn[:], ssn[:], AF.Exp, r=["assn"], w=["assn"], scale=-0.5)
                                for Q in range(NB):
                                    self.ts("dve", t3[:], o01[0][:, Q, :], ssn[:, Q:Q + 1], ALU.mult, r=aok + ["assn"], w=["at3"])
                                    self.tt("pool", osb[:, Q, h * 128:(h + 1) * 128], t3[:], gdn[:], ALU.mult, r=["at3", "gdn"], w=["aosb0"])
                    c.dma(o_d[tb:tb + S_, :].rearrange("(b p) f -> p b f", p=128), osb[:], r=["aosb%d" % jj_ for jj_ in range(4)], w=[okey])

    def phase_l1_out(self):
        nc, c = self.nc, self.c
        czT_v = self.czT.rearrange("(c p) t -> p c t", p=128)
        dzT_v = self.dzT.rearrange("(c p) t -> p c t", p=128)

        def provider(st):
            T = lambda name, shape, dt=F32: st.enter_context(self.sbt(name, shape, dt))
            ot = [T("ot%d" % i, [128, 2, BR], BF16) for i in range(4)]
            gT = [T("gT1_%d" % i, [128, 8, 128], BF16) for i in range(2)]
            gz = [T("gz%d" % i, [128, 8, 512], BF16) for i in range(2)]

            def pref(blk):
                i = blk % 4
                i4 = (blk // 4) % 2
                c.dma(ot[i][:, 0, :], self.oc_tok[blk * 128:(blk + 1) * 128, :], r=["oc_tok"], w=["ot%d" % i])
                c.dma(ot[i][:, 1, :], self.od_tok[blk * 128:(blk + 1) * 128, :], r=["od_tok"], w=["ot%d" % i])
                if blk % 4 == 0:
                    c.dma(gz[i4][:, 0:4, :], czT_v[:, :, blk * 128:blk * 128 + 512], r=["czT"], w=["gz%d" % i4])
                    c.dma(gz[i4][:, 4:8, :], dzT_v[:, :, blk * 128:blk * 128 + 512], r=["dzT"], w=["gz%d" % i4])

            def prov(blk):
                i = blk % 2
                i4 = (blk // 4) % 2
                pk = "ps%d" % i
                ptv = self.ps[i][:].bitcast(BF16).rearrange("p (a b) -> p a b", a=8)
                for fc in range(8):
                    c.op("pe", lambda e: e.transpose(out=ptv[:, fc, :], in_=ot[blk % 4][:, fc // 4, (fc % 4) * 128:(fc % 4 + 1) * 128],
                                                     identity=self.ident[:]), r=["ot%d" % (blk % 4), "ident"], w=[pk])
                off = (blk % 4) * 128
                self.tt("dve", gT[i][:], ptv, gz[i4][:, :, off:off + 128], ALU.mult, r=[pk, "gz%d" % i4], w=["gT1_%d" % i])
                return [(gT[i][:, fc, :], "gT1_%d" % i) for fc in range(8)]
            return pref, prov
        self.out_proj_norm_res(1, self.w_out_cd, self.post1_rep, provider, self.h1src, self.out, "h1_%d", "h1_%d")


def core_inputs(inp, x_rows):
    m = {}
    m["x"] = np.ascontiguousarray(x_rows, dtype=np.float32)
    m["ident"] = np.eye(128, dtype=np.float32).astype(ml_dtypes.bfloat16)
    m["pre0"] = np.ascontiguousarray(inp["pre_norm"][0].reshape(8, 128).T)
    m["w_in_ab"] = inp["w_in_ab"][0]
    m["s5_lr"] = inp["s5_lambda_re"][0].T
    m["s5_li"] = inp["s5_lambda_im"][0].T
    m["s5_ldt"] = np.broadcast_to(inp["s5_log_dt"][0][None, :], (64, 32))
    m["s5_br"] = inp["s5_b_re"][0].transpose(1, 0, 2)
    m["s5_bi"] = inp["s5_b_im"][0].transpose(1, 0, 2)
    m["s5_cr"] = inp["s5_c_re"][0].transpose(2, 0, 1)
    m["s5_ci"] = inp["s5_c_im"][0].transpose(2, 0, 1)
    tv = np.array([0, -1, -2, -3, -4, -5, -6, -7, 1, 2, 3, 4, 5, 6, 7, 8], dtype=np.float32)
    m["tv"] = np.broadcast_to(tv[None, :], (64, 16))
    m["kv"] = np.broadcast_to(np.arange(256, dtype=np.float32)[None, :], (64, 256))
    sidx = np.arange(128) // 16
    m["toepmask"] = (sidx[None, :] >= sidx[:, None]).astype(np.float32)
    m["identf"] = np.eye(128, dtype=np.float32)
    m["s5d_rep"] = np.broadcast_to(inp["s5_d"][0][None, :], (128, 512))
    m["glub_rep"] = np.broadcast_to(inp["s5_glu_b"][0][None, :], (128, 512))
    m["glu_w"] = inp["s5_glu_w"][0]
    m["ml_cw"] = inp["ml_conv_w"][0].reshape(4, 4, 128).transpose(2, 1, 0)
    m["ml_cb"] = inp["ml_conv_b"][0].reshape(4, 128).T
    m["ml_mn"] = inp["ml_norm"][0].reshape(4, 128).T
    m["ml_msk"] = inp["ml_skip"][0].reshape(4, 128).T
    m["ml_gbi"] = inp["ml_gate_b"][0][0:4].reshape(4, 1)
    m["ml_gbf"] = inp["ml_gate_b"][0][4:8].reshape(4, 1)
    si = np.arange(128)
    m["ml_maskS"] = ((si[:, None] <= si[None, :]) * (128 ** -0.5)).astype(np.float32)
    m["ml_gw"] = inp["ml_gate_w"][0].reshape(12, 128, 8).transpose(1, 0, 2)
    m["ml_wq"] = inp["ml_wq"][0].transpose(1, 0, 2)
    m["ml_wk"] = inp["ml_wk"][0].transpose(1, 0, 2)
    m["ml_wv"] = inp["ml_wv"][0].transpose(1, 0, 2)
    m["w_out_ab"] = inp["w_out_ab"][0]
    m["pre1"] = np.ascontiguousarray(inp["pre_norm"][1].reshape(8, 128).T)
    m["w_in_cd"] = inp["w_in_cd"][0]
    pos = np.arange(S, dtype=np.float32)
    inv = (10000.0 ** (-np.arange(0, 64, 2, dtype=np.float32) / 64)).astype(np.float32)
    ang = pos[None, :] * inv[np.arange(128) % 32][:, None]
    m["rope_cos"] = np.cos(ang).astype(np.float32)
    m["rope_sin"] = np.sin(ang).astype(np.float32)
    rm = np.zeros((128, 128), dtype=np.float32)
    for mm in range(128):
        if mm % 64 < 32:
            rm[mm + 32, mm] = -1.0
        else:
            rm[mm - 32, mm] = 1.0
    m["rope_rm"] = rm.astype(ml_dtypes.bfloat16)
    kk = np.arange(128)[:, None]
    qq = np.arange(512)[None, :]
    dm = np.zeros((128, 9, 512), dtype=np.float32)
    fm = np.zeros((128, 4, 512), dtype=np.float32)
    for mi in range(9):
        d0 = mi - 3 if mi < 8 else 5
        dl = 128 * d0 + qq - kk
        mult = ((dl >= 0) & (dl <= 128)).astype(np.float32) + ((dl >= 0) & (dl % 4 == 0) & (dl <= 512)) + ((dl >= 0) & (dl % 16 == 0) & (dl <= 2048))
        dm[:, mi, :] = mult
        if mi < 4:
            fm[:, mi, :] = (dl >= 0)
    m["dil_masks"] = dm.astype(ml_dtypes.bfloat16)
    m["diff_masks"] = fm.astype(ml_dtypes.bfloat16)
    m["diff_lqk"] = np.stack([inp["diff_lq1"][0], inp["diff_lk1"][0], inp["diff_lq2"][0], inp["diff_lk2"][0]])[None]
    m["diffnorm_rep"] = np.broadcast_to(inp["diff_norm"][0][None, :], (128, 128))
    m["w_out_cd"] = inp["w_out_cd"][0]
    m["post1_rep"] = np.broadcast_to(inp["post_norm"][1][None, :], (128, 1024))
    m["post0_rep"] = np.broadcast_to(inp["post_norm"][0][None, :], (128, 1024))
    return m


_CACHE = {}


def kernel(**inputs):
    inp = {k_: np.asarray(v) for k_, v in inputs.items()}
    x = inp["x"]
    B = x.shape[0]
    nseq = B // NCORES
    if "prog" not in _CACHE:
        kb = K(nseq=nseq)
        kb.build()
        _CACHE["prog"] = kb
    kb = _CACHE["prog"]
    in_maps = []
    for ci in range(NCORES):
        m = core_inputs(inp, x[ci * nseq:(ci + 1) * nseq].reshape(-1, D))
        in_maps.append({n: np.ascontiguousarray(m[n]) for n in kb.inputs})
    res = run_bass_kernel_spmd(kb.nc, in_maps, core_ids=list(range(NCORES)))
    out = np.stack([np.asarray(res.results[ci]["out"]).reshape(nseq, S, D) for ci in range(NCORES)], axis=0)
    return out.reshape(B, S, D).astype(np.float32)
```

```python
import contextlib
import math
import numpy as np
import ml_dtypes
import concourse.bass as bass
import concourse.mybir as mybir
from concourse.bass_utils import run_bass_kernel_spmd

F32 = mybir.dt.float32
BF16 = mybir.dt.bfloat16
I32 = mybir.dt.int32
AF = mybir.ActivationFunctionType
ALU = mybir.AluOpType
AX = mybir.AxisListType

D = 1024
S = 2048
BR = 512
NCORES = 8
SAME_ENGINE_SYNC = True
NORM_EPS = 1e-6
HEAD_EPS = 1e-5


class Ctx:
    def __init__(self, nc, stack, n_dma_sems=48, same_engine_sync=SAME_ENGINE_SYNC):
        self.nc = nc
        self.eng = {"pe": nc.tensor, "act": nc.scalar, "dve": nc.vector,
                    "pool": nc.gpsimd, "sp": nc.sync}
        self.sem = {}
        self.cnt = {}
        for k in ("pe", "act", "dve", "pool"):
            self.sem[k] = stack.enter_context(nc.semaphore("s_" + k))
            self.cnt[k] = 0
        self.dma_sems = []
        for i in range(n_dma_sems):
            k = "dma%d" % i
            self.sem[k] = stack.enter_context(nc.semaphore("s_" + k))
            self.cnt[k] = 0
            self.dma_sems.append(k)
        self.dma_rr = 0
        self.waited = {k: {} for k in self.eng}
        self.last_w = {}
        self.readers = {}
        self.same_engine_sync = same_engine_sync
        self.n_instr = 0
        self.n_wait = 0

    def _deps(self, r, w):
        deps = []
        for x in r:
            if x in self.last_w:
                deps.append(self.last_w[x])
            if x.startswith("ps"):
                deps.extend(self.readers.get(x, ()))
        for x in w:
            if x in self.last_w:
                deps.append(self.last_w[x])
            deps.extend(self.readers.get(x, ()))
        return deps

    def _wait(self, e, deps):
        need = {}
        for (k, v) in deps:
            if k == e and (e == "pe" or not self.same_engine_sync):
                continue
            if need.get(k, 0) < v:
                need[k] = v
        for k, v in need.items():
            if self.waited[e].get(k, 0) >= v:
                continue
            self.eng[e].wait_ge(self.sem[k], v)
            self.waited[e][k] = v
            self.n_wait += 1

    def _commit(self, tok, r, w):
        for x in w:
            self.last_w[x] = tok
            self.readers[x] = []
        for x in r:
            if x in w:
                continue
            self.readers.setdefault(x, []).append(tok)

    def op(self, e, fn, r=(), w=()):
        self._wait(e, self._deps(r, w))
        ins = fn(self.eng[e])
        self.cnt[e] += 1
        ins.then_inc(self.sem[e], 1)
        self._commit((e, self.cnt[e]), r, w)
        self.n_instr += 1
        return ins

    def dma(self, out, in_, r=(), w=(), q="sp", **kw):
        k = self.dma_sems[self.dma_rr]
        self.dma_rr = (self.dma_rr + 1) % len(self.dma_sems)
        deps = self._deps(r, w)
        if self.cnt[k] > 0:
            deps.append((k, self.cnt[k]))
        self._wait(q, deps)
        ins = self.eng[q].dma_start(out=out, in_=in_, **kw)
        self.cnt[k] += 16
        ins.then_inc(self.sem[k], 16)
        self._commit((k, self.cnt[k]), r, w)
        self.n_instr += 1
        return ins

    def barrier(self):
        deps = [(k, v) for k, v in self.cnt.items() if v > 0]
        for e in self.eng:
            self._wait(e, deps)

    def finish(self, res):
        deps = [self.last_w[x] for x in res if x in self.last_w]
        self._wait("sp", deps)


class K:
    def __init__(self, nseq=2, export=(), phases=None, seqlen=S):
        self.nseq = nseq
        self.S = seqlen
        self.NT = nseq * seqlen
        self.export = set(export)
        self.phases = phases
        self.nc = bass.Bass("TRN2", target_bir_lowering=False)
        self.inputs = {}
        self.outputs = {}
        self.s5_main_enabled = True
        self._uid = 0

    def sbt(self, name, shape, dt):
        self._uid += 1
        return self.nc.sbuf_tensor("%s_u%d" % (name, self._uid), list(shape), dt)

    def din(self, name, shape, dt=F32):
        ap = self.nc.dram_tensor(name, list(shape), dt, kind="ExternalInput").ap()
        self.inputs[name] = ap
        return ap

    def dscr(self, name, shape, dt):
        kind = "ExternalOutput" if name in self.export else "Internal"
        ap = self.nc.dram_tensor(name, list(shape), dt, kind=kind).ap()
        if kind == "ExternalOutput":
            self.outputs[name] = ap
        return ap

    @contextlib.contextmanager
    def scope(self):
        with contextlib.ExitStack() as st:
            yield st
            self.c.barrier()

    def build(self):
        nc = self.nc
        NT = self.NT
        with contextlib.ExitStack() as st:
            self.c = Ctx(nc, st)
            self.ps = [st.enter_context(nc.psum_tensor("ps%d" % i, [128, 512], F32)) for i in range(8)]
            self.x = self.din("x", [NT, D])
            self.ident_d = self.din("ident", [128, 128], BF16)
            self.pre0 = self.din("pre0", [128, 8])
            self.w_in_ab = self.din("w_in_ab", [D, 4 * BR])
            for nm in ("s5_lr", "s5_li", "s5_ldt"):
                setattr(self, nm, self.din(nm, [64, 32]))
            for nm in ("s5_br", "s5_bi", "s5_cr", "s5_ci"):
                setattr(self, nm, self.din(nm, [64, 32, 16]))
            self.tv_d = self.din("tv", [64, 16])
            self.kv_d = self.din("kv", [64, 256])
            self.toepmask_d = self.din("toepmask", [128, 128])
            self.identf_d = self.din("identf", [128, 128])
            self.s5d_rep = self.din("s5d_rep", [128, BR])
            self.glub_rep = self.din("glub_rep", [128, BR])
            self.glu_w = self.din("glu_w", [BR, BR])
            self.ml_cw = self.din("ml_cw", [128, 4, 4]); self.ml_cb = self.din("ml_cb", [128, 4])
            self.ml_mn = self.din("ml_mn", [128, 4]); self.ml_msk = self.din("ml_msk", [128, 4])
            self.ml_gbi = self.din("ml_gbi", [4, 1]); self.ml_gbf = self.din("ml_gbf", [4, 1])
            self.ml_maskS = self.din("ml_maskS", [128, 128])
            self.ml_gw = self.din("ml_gw", [128, 12, 8])
            self.ml_wq = self.din("ml_wq", [128, 4, 128]); self.ml_wk = self.din("ml_wk", [128, 4, 128]); self.ml_wv = self.din("ml_wv", [128, 4, 128])
            self.bT = self.dscr("bT", [BR, NT], BF16)
            self.w_out_ab = self.din("w_out_ab", [D, D]); self.post0_rep = self.din("post0_rep", [128, D])
            self.out = self.nc.dram_tensor("out", [NT, D], F32, kind="ExternalOutput").ap()
            self.outputs["out"] = self.out
            self.h1src = self.out
            if self.phases is not None and "l0o" not in self.phases:
                self.h1src = self.din("h1_in", [NT, D])
            self.pre1 = self.din("pre1", [128, 8]); self.w_in_cd = self.din("w_in_cd", [D, 8 * BR])
            self.rope_cos = self.din("rope_cos", [128, self.S]); self.rope_sin = self.din("rope_sin", [128, self.S])
            self.rope_rm = self.din("rope_rm", [128, 128], BF16)
            for nm in ("cqT", "ckT", "dqT", "dkT", "czT", "dzT"):
                setattr(self, nm, self.dscr(nm, [BR, NT], BF16))
            for nm in ("cv_tok", "dv_tok", "oc_tok", "od_tok"):
                setattr(self, nm, self.dscr(nm, [NT, BR], BF16))
            self.dil_masks = self.din("dil_masks", [128, 9, 512], BF16); self.diff_masks = self.din("diff_masks", [128, 4, 512], BF16)
            self.diff_lqk = self.din("diff_lqk", [1, 4, 64]); self.diffnorm_rep = self.din("diffnorm_rep", [128, 128])
            self.w_out_cd = self.din("w_out_cd", [D, D]); self.post1_rep = self.din("post1_rep", [128, D])
            self.rotc = self.dscr("rotc", [64, 32, 256], F32)
            self.rots = self.dscr("rots", [64, 32, 256], F32)
            self.rhot = self.dscr("rhot", [64, 32, 256], F32)
            self.toep_x = self.dscr("toep_x", [128, 32, 128], BF16)
            self.wii_x = self.dscr("wii_x", [128, 32, 2, 64], BF16)
            self.wiv_x = self.dscr("wiv_x", [64, 2, 32, 128], BF16)
            self.a_tok = self.dscr("a_tok", [NT, BR], BF16)
            self.u_tok = self.dscr("u_tok", [NT, BR], BF16)
            self.sz_tok = self.dscr("sz_tok", [NT, BR], BF16)
            self.xmT = self.dscr("xmT", [BR, NT], BF16)
            self.mzT = self.dscr("mzT", [BR, NT], BF16)
            self.ident = st.enter_context(nc.sbuf_tensor("identb", [128, 128], BF16))
            self.c.dma(self.ident[:], self.ident_d, w=["ident"])
            ph = self.phases
            fin = []
            self.tables_gen_factory = None
            if ph is None or "s5" in ph:
                TP = lambda name, shape, dt=F32: st.enter_context(self.sbt(name, shape, dt))
                self.s5w = (TP("Toep", [128, 32, 128], BF16), TP("Wii", [128, 32, 2, 64], BF16),
                            TP("WivR", [64, 32, 128], BF16), TP("WivI", [64, 32, 128], BF16))
                small = (TP("p_thr", [64, 32]), TP("p_rho8", [64, 32]), TP("p_kv", [64, 256]),
                         TP("p_tfs", [64, 32]), TP("p_tis", [64, 32], I32))
                self.s5_precompute(*self.s5w, small)
                self.tables_gen_factory = lambda T: self.s5_tables_gen(T, small)
                if not (ph is None or "l0p" in ph):
                    with self.scope() as sp_:
                        for _ in self.s5_tables_gen(lambda name, shape, dt=F32: sp_.enter_context(self.sbt(name, shape, dt)), small):
                            pass
            if ph is None or "l0p" in ph:
                self.phase_l0_proj()
                fin += ["u_tok", "sz_tok", "xmT", "mzT"]
            if ph is None or "s5" in ph:
                self.phase_s5()
                fin += ["a_tok", "rotc", "rots", "rhot", "toep_x", "wii_x", "wiv_x"]
            if ph is None or "ml" in ph:
                self.phase_ml()
                fin += ["bT"]
            if ph is None or "l0o" in ph:
                self.phase_l0_out()
                fin += ["h1_%d" % b for b in range(NT // 128)]
            if ph is None or "l1p" in ph:
                self.phase_l1_proj()
                fin += ["cqT", "ckT", "dqT", "dkT", "czT", "dzT", "cv_tok", "dv_tok"]
            if ph is None or "adil" in ph:
                self.phase_attn("dil")
                fin += ["oc_tok"]
            if ph is None or "adiff" in ph:
                self.phase_attn("diff")
                fin += ["od_tok"]
            if ph is None or "l1o" in ph:
                self.phase_l1_out()
                fin += ["h1_%d" % b for b in range(NT // 128)]
            self.c.finish(fin)
            self.c.barrier()
        return nc

    def rmsnorm_T(self, st, xsrc_rows, nblk, zT, zkey, tagp, ps_tr):
        raise NotImplementedError

    def phase_l0_proj(self):
        nc, c = self.nc, self.c
        NT = self.NT
        ngrp = NT // 512
        with self.scope() as st:
            T = lambda name, shape, dt: st.enter_context(self.sbt(name, shape, dt))
            win = T("win0", [128, 8, 4 * BR], BF16)
            g0 = T("g0", [128, 8], F32)
            stage = [T("wst%d" % i, [128, 4 * BR], F32) for i in range(2)]
            c.dma(g0[:], self.pre0, w=["g0"])
            for dc in range(8):
                sk = "wst%d" % (dc % 2)
                c.dma(stage[dc % 2][:], self.w_in_ab[dc * 128:(dc + 1) * 128, :], w=[sk])
                c.op("act", lambda e: e.activation(out=win[:, dc, :], in_=stage[dc % 2][:], func=AF.Copy,
                                                   scale=g0[:, dc:dc + 1]), r=[sk, "g0"], w=["win0"])
            gen = self.tables_gen_factory(lambda name, shape, dt=F32: st.enter_context(self.sbt(name, shape, dt))) if self.tables_gen_factory else None

            def pump(n):
                if gen is not None:
                    for _ in range(n):
                        if next(gen, "done") == "done":
                            break
            NXB = 6
            xt = [T("xt%d" % i, [128, D], F32) for i in range(NXB)]
            junk = T("junk", [128, D], F32)
            ss2 = [T("ss%d" % i, [128, 4], F32) for i in range(2)]
            rstd2 = [T("rstd%d" % i, [128, 4], F32) for i in range(2)]
            zb = [T("zb%d" % i, [128, D], BF16) for i in range(2)]
            zT = [T("zT%d" % i, [128, 8, 512], BF16) for i in range(2)]
            uo = [T("uo%d" % i, [128, BR], BF16) for i in range(2)]
            so = [T("so%d" % i, [128, BR], BF16) for i in range(2)]
            xmo = [T("xmo%d" % i, [128, 4, 512], BF16) for i in range(2)]
            mzo = [T("mzo%d" % i, [128, 4, 512], BF16) for i in range(2)]
            nblk = NT // 128

            def load_x(b):
                if b < nblk:
                    c.dma(xt[b % NXB][:], self.x[b * 128:(b + 1) * 128, :], w=["xt%d" % (b % NXB)])
            for b in range(4):
                load_x(b)
            for g in range(ngrp):
                zk = "zT%d" % (g % 2)
                ss = ss2[g % 2]; rstd = rstd2[g % 2]
                ssk = "ss%d" % (g % 2); rsk = "rstd%d" % (g % 2)
                for j in range(4):
                    b = g * 4 + j
                    xk = "xt%d" % (b % NXB)
                    c.op("act", lambda e: e.activation(out=junk[:], in_=xt[b % NXB][:], func=AF.Square,
                                                       accum_out=ss[:, j:j + 1]), r=[xk], w=["junk", ssk])
                c.op("act", lambda e: e.activation(out=rstd[:], in_=ss[:], func=AF.Ln, scale=1.0 / D, bias=NORM_EPS),
                     r=[ssk], w=[rsk])
                c.op("act", lambda e: e.activation(out=rstd[:], in_=rstd[:], func=AF.Exp, scale=-0.5),
                     r=[rsk], w=[rsk])
                for j in range(4):
                    b = g * 4 + j
                    xk = "xt%d" % (b % NXB)
                    zbk = "zb%d" % (b % 2)
                    pst = self.ps[b % 2]
                    pk = "ps%d" % (b % 2)
                    c.op("dve", lambda e: e.tensor_scalar(out=zb[b % 2][:], in0=xt[b % NXB][:], scalar1=rstd[:, j:j + 1],
                                                          scalar2=None, op0=ALU.mult), r=[xk, rsk], w=[zbk])
                    load_x(b + 4)
                    ptv = pst[:].bitcast(BF16).rearrange("p (a b) -> p a b", a=8)
                    for dc in range(8):
                        c.op("pe", lambda e: e.transpose(out=ptv[:, dc, :], in_=zb[b % 2][:, dc * 128:(dc + 1) * 128],
                                                         identity=self.ident[:]), r=[zbk, "ident"], w=[pk])
                    c.op("dve", lambda e: e.tensor_copy(out=zT[g % 2][:, :, j * 128:(j + 1) * 128], in_=ptv),
                         r=[pk], w=[zk])
                    for dc in range(8):
                        c.op("pe", lambda e: e.matmul(out=self.ps[2][:], lhsT=zT[g % 2][:, dc, j * 128:(j + 1) * 128],
                                                      rhs=win[:, dc, 0:BR], start=(dc == 0), stop=(dc == 7)),
                             r=[zk, "win0"], w=["ps2"])
                    c.op("act", lambda e: e.copy(out=uo[b % 2][:], in_=self.ps[2][:]), r=["ps2"], w=["uo%d" % (b % 2)])
                    c.dma(self.u_tok[b * 128:(b + 1) * 128, :], uo[b % 2][:], r=["uo%d" % (b % 2)], w=["u_tok"])
                    for dc in range(8):
                        c.op("pe", lambda e: e.matmul(out=self.ps[3][:], lhsT=zT[g % 2][:, dc, j * 128:(j + 1) * 128],
                                                      rhs=win[:, dc, BR:2 * BR], start=(dc == 0), stop=(dc == 7)),
                             r=[zk, "win0"], w=["ps3"])
                    c.op("act", lambda e: e.activation(out=so[b % 2][:], in_=self.ps[3][:], func=AF.Silu),
                         r=["ps3"], w=["so%d" % (b % 2)])
                    c.dma(self.sz_tok[b * 128:(b + 1) * 128, :], so[b % 2][:], r=["so%d" % (b % 2)], w=["sz_tok"])
                    pump(6)
                for fc in range(8):
                    pb = 4 + fc % 4
                    for dc in range(8):
                        c.op("pe", lambda e: e.matmul(out=self.ps[pb][:], lhsT=win[:, dc, 2 * BR + fc * 128:2 * BR + (fc + 1) * 128],
                                                      rhs=zT[g % 2][:, dc, :], start=(dc == 0), stop=(dc == 7)),
                             r=[zk, "win0"], w=["ps%d" % pb])
                    if fc < 4:
                        c.op("dve", lambda e: e.tensor_copy(out=xmo[g % 2][:, fc, :], in_=self.ps[pb][:]),
                             r=["ps%d" % pb], w=["xmo%d" % (g % 2)])
                    else:
                        c.op("act", lambda e: e.activation(out=mzo[g % 2][:, fc - 4, :], in_=self.ps[pb][:], func=AF.Silu),
                             r=["ps%d" % pb], w=["mzo%d" % (g % 2)])
                c.dma(self.xmT.rearrange("(c p) t -> p c t", p=128)[:, :, g * 512:(g + 1) * 512], xmo[g % 2][:],
                      r=["xmo%d" % (g % 2)], w=["xmT"])
                c.dma(self.mzT.rearrange("(c p) t -> p c t", p=128)[:, :, g * 512:(g + 1) * 512], mzo[g % 2][:],
                      r=["mzo%d" % (g % 2)], w=["mzT"])
            pump(10 ** 6)
        c.barrier()

    def tt(self, e, out, a, b, op, r, w):
        return self.c.op(e, lambda en: en.tensor_tensor(out=out, in0=a, in1=b, op=op), r=r, w=w)

    def ts(self, e, out, a, s1, op0, r, w, s2=None, op1=None):
        if op1 is None:
            return self.c.op(e, lambda en: en.tensor_scalar(out=out, in0=a, scalar1=s1, scalar2=None, op0=op0), r=r, w=w)
        return self.c.op(e, lambda en: en.tensor_scalar(out=out, in0=a, scalar1=s1, scalar2=s2, op0=op0, op1=op1), r=r, w=w)

    def stt(self, e, out, a, s, b, op0, op1, r, w):
        return self.c.op(e, lambda en: en.scalar_tensor_tensor(out=out, in0=a, scalar=s, in1=b, op0=op0, op1=op1), r=r, w=w)

    def actf(self, out, in_, func, r, w, **kw):
        return self.c.op("act", lambda en: en.activation(out=out, in_=in_, func=func, **kw), r=r, w=w)

    def cp(self, e, out, in_, r, w):
        if e == "act":
            return self.c.op("act", lambda en: en.copy(out=out, in_=in_), r=r, w=w)
        return self.c.op(e, lambda en: en.tensor_copy(out=out, in_=in_), r=r, w=w)

    def sincos(self, *a, **kw):
        for _ in self.sincos_g(*a, **kw):
            pass

    def sincos_g(self, ang, akey, sin_o, cos_o, skey, ckey, tf, ti, red_o=None):
        C1 = 6.28125
        C2 = 2 * math.pi - C1
        for (off, out, okey) in ((0.0, sin_o, skey), (math.pi / 2, cos_o, ckey)):
            if out is None:
                continue
            self.ts("dve", tf, ang, 1.0 / (2 * math.pi), ALU.mult, r=[akey], w=["sc_tf"], s2=off / (2 * math.pi), op1=ALU.add)
            yield
            self.cp("dve", ti, tf, r=["sc_tf"], w=["sc_ti"])
            yield
            self.cp("dve", tf, ti, r=["sc_ti"], w=["sc_tf"])
            yield
            self.stt("dve", out, tf, -C1, ang, ALU.mult, ALU.add, r=["sc_tf", akey], w=[okey])
            yield
            self.stt("dve", out, tf, -C2, out, ALU.mult, ALU.add, r=["sc_tf", okey], w=[okey])
            yield
            if off != 0.0:
                self.ts("dve", out, out, off, ALU.add, r=[okey], w=[okey])
                yield
            self.ts("dve", out, out, math.pi, ALU.min, r=[okey], w=[okey], s2=-math.pi, op1=ALU.max)
            yield
            if red_o is not None and off == 0.0:
                self.cp("dve", red_o[0], out, r=[okey], w=[red_o[1]])
                yield
            self.actf(out, out, AF.Sin, r=[okey], w=[okey])
            yield

    def s5_precompute(self, Toep, Wii, WivR, WivI, small):
        nc, c = self.nc, self.c
        thr, rho8, kv, tfs, tis = small
        with self.scope() as sp:
            T = lambda name, shape, dt=F32: sp.enter_context(self.sbt(name, shape, dt))
            lr = T("p_lr", [64, 32]); li = T("p_li", [64, 32]); ldt = T("p_ldt", [64, 32])
            br = T("p_br", [64, 32, 16]); bi = T("p_bi", [64, 32, 16])
            cr = T("p_cr", [64, 32, 16]); ci = T("p_ci", [64, 32, 16])
            tv = T("p_tv", [64, 16])
            msk = T("p_msk", [128, 128]); idf = T("p_idf", [128, 128])
            for (t, d, key) in ((lr, self.s5_lr, "p_lr"), (li, self.s5_li, "p_li"), (ldt, self.s5_ldt, "p_ldt"),
                                (br, self.s5_br, "p_br"), (bi, self.s5_bi, "p_bi"), (cr, self.s5_cr, "p_cr"),
                                (ci, self.s5_ci, "p_ci"), (tv, self.tv_d, "p_tv"), (kv, self.kv_d, "p_kv"),
                                (msk, self.toepmask_d, "p_msk"), (idf, self.identf_d, "p_idf")):
                c.dma(t[:], d, w=[key])
            dt = T("p_dt", [64, 32]); lrdt = T("p_lrdt", [64, 32]); th = T("p_th", [64, 32])
            s0 = T("p_s0", [64, 32]); c0 = T("p_c0", [64, 32]); mag = T("p_mag", [64, 32])
            self.actf(dt[:], ldt[:], AF.Exp, r=["p_ldt"], w=["p_dt"])
            self.tt("dve", lrdt[:], lr[:], dt[:], ALU.mult, r=["p_lr", "p_dt"], w=["p_lrdt"])
            self.tt("dve", th[:], li[:], dt[:], ALU.mult, r=["p_li", "p_dt"], w=["p_th"])
            self.sincos(th[:], "p_th", s0[:], c0[:], "p_s0", "p_c0", tfs[:], tis[:], red_o=(thr[:], "p_thr"))
            self.actf(mag[:], lrdt[:], AF.Exp, r=["p_lrdt"], w=["p_mag"])
            abr = T("p_abr", [64, 32]); abi = T("p_abi", [64, 32]); am1 = T("p_am1", [64, 32])
            self.tt("dve", abr[:], mag[:], c0[:], ALU.mult, r=["p_mag", "p_c0"], w=["p_abr"])
            self.tt("dve", abi[:], mag[:], s0[:], ALU.mult, r=["p_mag", "p_s0"], w=["p_abi"])
            self.ts("dve", am1[:], abr[:], -1.0, ALU.add, r=["p_abr"], w=["p_am1"])
            den = T("p_den", [64, 32]); t1 = T("p_t1", [64, 32]); t2 = T("p_t2", [64, 32])
            fr = T("p_fr", [64, 32]); fi = T("p_fi", [64, 32])
            self.tt("dve", den[:], lr[:], lr[:], ALU.mult, r=["p_lr"], w=["p_den"])
            self.tt("dve", t1[:], li[:], li[:], ALU.mult, r=["p_li"], w=["p_t1"])
            self.tt("dve", den[:], den[:], t1[:], ALU.add, r=["p_den", "p_t1"], w=["p_den"])
            c.op("dve", lambda e: e.reciprocal(out=den[:], in_=den[:]), r=["p_den"], w=["p_den"])
            self.tt("dve", t1[:], am1[:], lr[:], ALU.mult, r=["p_am1", "p_lr"], w=["p_t1"])
            self.tt("dve", t2[:], abi[:], li[:], ALU.mult, r=["p_abi", "p_li"], w=["p_t2"])
            self.tt("dve", t1[:], t1[:], t2[:], ALU.add, r=["p_t1", "p_t2"], w=["p_t1"])
            self.tt("dve", fr[:], t1[:], den[:], ALU.mult, r=["p_t1", "p_den"], w=["p_fr"])
            self.tt("dve", t1[:], abi[:], lr[:], ALU.mult, r=["p_abi", "p_lr"], w=["p_t1"])
            self.tt("dve", t2[:], am1[:], li[:], ALU.mult, r=["p_am1", "p_li"], w=["p_t2"])
            self.tt("dve", t1[:], t1[:], t2[:], ALU.subtract, r=["p_t1", "p_t2"], w=["p_t1"])
            self.tt("dve", fi[:], t1[:], den[:], ALU.mult, r=["p_t1", "p_den"], w=["p_fi"])
            Bbr = T("p_Bbr", [64, 32, 16]); Bbi = T("p_Bbi", [64, 32, 16])
            u1 = T("p_u1", [64, 32, 16]); u2 = T("p_u2", [64, 32, 16])
            bc16 = lambda a: a.unsqueeze(2).broadcast_to([64, 32, 16])
            self.tt("dve", u1[:], br[:], bc16(fr[:]), ALU.mult, r=["p_br", "p_fr"], w=["p_u1"])
            self.tt("dve", u2[:], bi[:], bc16(fi[:]), ALU.mult, r=["p_bi", "p_fi"], w=["p_u2"])
            self.tt("dve", Bbr[:], u1[:], u2[:], ALU.subtract, r=["p_u1", "p_u2"], w=["p_Bbr"])
            self.tt("dve", u1[:], bi[:], bc16(fr[:]), ALU.mult, r=["p_bi", "p_fr"], w=["p_u1"])
            self.tt("dve", u2[:], br[:], bc16(fi[:]), ALU.mult, r=["p_br", "p_fi"], w=["p_u2"])
            self.tt("dve", Bbi[:], u1[:], u2[:], ALU.add, r=["p_u1", "p_u2"], w=["p_Bbi"])
            TE = T("p_TE", [64, 16, 32]); TA = T("p_TA", [64, 16, 32])
            PWr = T("p_PWr", [64, 16, 32]); PWi = T("p_PWi", [64, 16, 32])
            tf3 = T("p_tf3", [64, 16, 32]); ti3 = T("p_ti3", [64, 16, 32], I32)
            bt = lambda a: a.unsqueeze(1).broadcast_to([64, 16, 32])
            bg = lambda a: a.unsqueeze(2).broadcast_to([64, 16, 32])
            self.tt("dve", TE[:], bt(lrdt[:]), bg(tv[:]), ALU.mult, r=["p_lrdt", "p_tv"], w=["p_TE"])
            self.actf(TE[:], TE[:], AF.Exp, r=["p_TE"], w=["p_TE"])
            self.tt("dve", TA[:], bt(thr[:]), bg(tv[:]), ALU.mult, r=["p_thr", "p_tv"], w=["p_TA"])
            self.sincos(TA[:], "p_TA", PWi[:], PWr[:], "p_PWi", "p_PWr", tf3[:], ti3[:])
            self.tt("dve", PWr[:], PWr[:], TE[:], ALU.mult, r=["p_PWr", "p_TE"], w=["p_PWr"])
            self.tt("dve", PWi[:], PWi[:], TE[:], ALU.mult, r=["p_PWi", "p_TE"], w=["p_PWi"])
            HsR = T("p_HsR", [64, 32, 8, 16]); HsI = T("p_HsI", [64, 32, 8, 16])
            v1 = T("p_v1", [64, 32, 9, 16]); v2 = T("p_v2", [64, 32, 9, 16])
            def pw_b(PW, j0, n):
                return PW[:, j0:j0 + n, :].rearrange("p t g -> p g t").unsqueeze(3).broadcast_to([64, 32, n, 16])
            def x_b(x, n):
                return x.unsqueeze(2).broadcast_to([64, 32, n, 16])
            self.tt("dve", v1[:, :, 0:8, :], pw_b(PWr, 0, 8), x_b(Bbr[:], 8), ALU.mult, r=["p_PWr", "p_Bbr"], w=["p_v1"])
            self.tt("dve", v2[:, :, 0:8, :], pw_b(PWi, 0, 8), x_b(Bbi[:], 8), ALU.mult, r=["p_PWi", "p_Bbi"], w=["p_v2"])
            self.tt("dve", HsR[:], v1[:, :, 0:8, :], v2[:, :, 0:8, :], ALU.subtract, r=["p_v1", "p_v2"], w=["p_HsR"])
            self.tt("dve", v1[:, :, 0:8, :], pw_b(PWr, 0, 8), x_b(Bbi[:], 8), ALU.mult, r=["p_PWr", "p_Bbi"], w=["p_v1"])
            self.tt("dve", v2[:, :, 0:8, :], pw_b(PWi, 0, 8), x_b(Bbr[:], 8), ALU.mult, r=["p_PWi", "p_Bbr"], w=["p_v2"])
            self.tt("dve", HsI[:], v1[:, :, 0:8, :], v2[:, :, 0:8, :], ALU.add, r=["p_v1", "p_v2"], w=["p_HsI"])
            LtR = T("p_LtR", [64, 32, 9, 16]); nLtI = T("p_nLtI", [64, 32, 9, 16])
            for (s0_, j0, n) in ((0, 0, 1), (1, 8, 8)):
                sl = slice(s0_, s0_ + n)
                self.tt("dve", v1[:, :, sl, :], pw_b(PWr, j0, n), x_b(cr[:], n), ALU.mult, r=["p_PWr", "p_cr"], w=["p_v1"])
                self.tt("dve", v2[:, :, sl, :], pw_b(PWi, j0, n), x_b(ci[:], n), ALU.mult, r=["p_PWi", "p_ci"], w=["p_v2"])
                self.tt("dve", LtR[:, :, sl, :], v1[:, :, sl, :], v2[:, :, sl, :], ALU.subtract, r=["p_v1", "p_v2"], w=["p_LtR"])
                self.tt("dve", v1[:, :, sl, :], pw_b(PWi, j0, n), x_b(cr[:], n), ALU.mult, r=["p_PWi", "p_cr"], w=["p_v1"])
                self.tt("dve", v2[:, :, sl, :], pw_b(PWr, j0, n), x_b(ci[:], n), ALU.mult, r=["p_PWr", "p_ci"], w=["p_v2"])
                self.tt("dve", v1[:, :, sl, :], v1[:, :, sl, :], v2[:, :, sl, :], ALU.add, r=["p_v1", "p_v2"], w=["p_v1"])
                self.ts("dve", nLtI[:, :, sl, :], v1[:, :, sl, :], -1.0, ALU.mult, r=["p_v1"], w=["p_nLtI"])
            self.cp("dve", WivR[:], LtR[:, :, 1:9, :].rearrange("p g t c -> p g (t c)"), r=["p_LtR"], w=["WivR"])
            self.cp("dve", WivI[:], nLtI[:, :, 1:9, :].rearrange("p g t c -> p g (t c)"), r=["p_nLtI"], w=["WivI"])
            for g4 in range(8):
                pb = g4 % 2
                pk = "ps%d" % pb
                for gl in range(4):
                    g = g4 * 4 + gl
                    o = self.ps[pb][:, gl * 128:(gl + 1) * 128]
                    c.op("pe", lambda e: e.matmul(out=o, lhsT=HsR[:, g, :, :].rearrange("p s c -> p (s c)"),
                                                  rhs=LtR[:, g, 0:8, :].rearrange("p t c -> p (t c)"), start=True, stop=False),
                         r=["p_HsR", "p_LtR"], w=[pk])
                    c.op("pe", lambda e: e.matmul(out=o, lhsT=HsI[:, g, :, :].rearrange("p s c -> p (s c)"),
                                                  rhs=nLtI[:, g, 0:8, :].rearrange("p t c -> p (t c)"), start=False, stop=True),
                         r=["p_HsI", "p_nLtI"], w=[pk])
                self.tt("dve", Toep[:, g4 * 4:(g4 + 1) * 4, :], self.ps[pb][:].rearrange("p (g n) -> p g n", g=4),
                        msk[:].unsqueeze(1).broadcast_to([128, 4, 128]), ALU.mult, r=[pk, "p_msk"], w=["Toep"])
            GsR = LtR[:, :, 0:8, :].rearrange("p g t c -> p g (t c)")
            GsI = nLtI[:, :, 0:8, :].rearrange("p g t c -> p g (t c)")
            w1 = v1[:, :, 0:8, :].rearrange("p g t c -> p g (t c)")
            w2 = v2[:, :, 0:8, :].rearrange("p g t c -> p g (t c)")
            p7r = PWr[:, 14, :].unsqueeze(2).broadcast_to([64, 32, 128])
            p7i = PWi[:, 14, :].unsqueeze(2).broadcast_to([64, 32, 128])
            hr = HsR[:].rearrange("p g s c -> p g (s c)"); hi = HsI[:].rearrange("p g s c -> p g (s c)")
            self.tt("dve", w1, hr, p7r, ALU.mult, r=["p_HsR", "p_PWr"], w=["p_v1"])
            self.tt("dve", w2, hi, p7i, ALU.mult, r=["p_HsI", "p_PWi"], w=["p_v2"])
            self.tt("dve", GsR, w1, w2, ALU.subtract, r=["p_v1", "p_v2"], w=["p_LtR"])
            self.tt("dve", w1, hi, p7r, ALU.mult, r=["p_HsI", "p_PWr"], w=["p_v1"])
            self.tt("dve", w2, hr, p7i, ALU.mult, r=["p_HsR", "p_PWi"], w=["p_v2"])
            self.tt("dve", GsI, w1, w2, ALU.add, r=["p_v1", "p_v2"], w=["p_nLtI"])
            for g4 in range(8):
                pb = 2 + g4 % 2
                pk = "ps%d" % pb
                pv = self.ps[pb][:].rearrange("p (g r n) -> p g r n", g=4, r=2)
                for gl in range(4):
                    g = g4 * 4 + gl
                    c.op("pe", lambda e: e.transpose(out=pv[:, gl, 0, :], in_=GsR[:, g, :], identity=idf[0:64, 0:64]),
                         r=["p_LtR", "p_idf"], w=[pk])
                    c.op("pe", lambda e: e.transpose(out=pv[:, gl, 1, :], in_=GsI[:, g, :], identity=idf[0:64, 0:64]),
                         r=["p_nLtI", "p_idf"], w=[pk])
                self.cp("act", Wii[:, g4 * 4:(g4 + 1) * 4, :, :], pv, r=[pk], w=["Wii"])
            self.cp("dve", rho8[:], TE[:, 15, :], r=["p_TE"], w=["p_rho8"])
            if "toep_x" in self.export:
                c.dma(self.toep_x, Toep[:], r=["Toep"], w=["toep_x"])
                c.dma(self.wii_x, Wii[:], r=["Wii"], w=["wii_x"])
                c.dma(self.wiv_x[:, 0], WivR[:], r=["WivR"], w=["wiv_x"])
                c.dma(self.wiv_x[:, 1], WivI[:], r=["WivI"], w=["wiv_x"])
        c.barrier()

    def s5_tables_gen(self, T, small):
        nc, c = self.nc, self.c
        thr, rho8, kv, tfs, tis = small
        NG = 4
        phr = T("p_phr", [64, 32]); ph_s = T("p_phs", [64, 32]); phr2 = T("p_phr2", [64, 32])
        rho = T("p_rho", [64, NG, 256])
        ang = T("p_ang", [64, NG, 256]); sk = T("p_sk", [64, NG, 256]); ck = T("p_ck", [64, NG, 256])
        tf4 = T("p_tf4", [64, NG, 256]); ti4 = T("p_ti4", [64, NG, 256], I32)
        self.ts("dve", phr[:], thr[:], 8.0, ALU.mult, r=["p_thr"], w=["p_phr"])
        yield
        yield from self.sincos_g(phr[:], "p_phr", ph_s[:], None, "p_phs", None, tfs[:], tis[:], red_o=(phr2[:], "p_phr2"))
        for gb in range(32 // NG):
            gs = slice(gb * NG, (gb + 1) * NG)
            self.cp("dve", rho[:], rho8[:, gs].unsqueeze(2).broadcast_to([64, NG, 256]), r=["p_rho8"], w=["p_rho"])
            yield
            c.op("dve", lambda e: e.memset(rho[:, :, 0:1], 0.0), r=[], w=["p_rho"])
            c.dma(self.rhot[:, gs, :], rho[:], r=["p_rho"], w=["rhot"])
            yield
            self.tt("dve", ang[:], phr2[:, gs].unsqueeze(2).broadcast_to([64, NG, 256]),
                    kv[:].unsqueeze(1).broadcast_to([64, NG, 256]), ALU.mult, r=["p_phr2", "p_kv"], w=["p_ang"])
            yield
            yield from self.sincos_g(ang[:], "p_ang", sk[:], ck[:], "p_sk", "p_ck", tf4[:], ti4[:])
            c.dma(self.rots[:, gs, :], sk[:], r=["p_sk"], w=["rots"])
            c.dma(self.rotc[:, gs, :], ck[:], r=["p_ck"], w=["rotc"])
            yield

    def phase_s5(self):
        Toep, Wii, WivR, WivI = self.s5w
        if self.s5_main_enabled:
            self.s5_main(None, Toep, Wii, WivR, WivI)
        self.c.barrier()

    def s5_main(self, st0, Toep, Wii, WivR, WivI):
        nc, c = self.nc, self.c
        GB = 4
        with self.scope() as st:
            T = lambda name, shape, dt=F32: st.enter_context(self.sbt(name, shape, dt))
            gluw = T("gluw", [128, 4, BR], BF16)
            with self.scope() as sg:
                gst = sg.enter_context(self.sbt("gluw_st", [128, 4, BR], F32))
                c.dma(gst[:], self.glu_w.rearrange("(c p) n -> p c n", p=128), w=["gluw_st"])
                self.cp("dve", gluw[:], gst[:], r=["gluw_st"], w=["gluw"])
            Drep = T("Drep", [128, BR]); Brep = T("Brep", [128, BR])
            c.dma(Drep[:], self.s5d_rep, w=["Drep"])
            c.dma(Brep[:], self.glub_rep, w=["Brep"])
            for q in range(self.nseq):
                tb = q * self.S
                with self.scope() as sq:
                    TQ = lambda name, shape, dt=F32: sq.enter_context(self.sbt(name, shape, dt))
                    uck = [TQ("uck%d" % i, [128, 8 * BR], BF16) for i in range(2)]
                    szck = [TQ("szck%d" % i, [128, 8 * BR], BF16) for i in range(2)]
                    yck = [TQ("yck%d" % i, [128, 8, BR], BF16) for i in range(2)]
                    for kh in range(2):
                        rows = slice(tb + kh * 1024, tb + (kh + 1) * 1024)
                        c.dma(uck[kh][:], self.u_tok[rows, :].rearrange("(k t) f -> k (t f)", t=8), r=["u_tok"], w=["uck%d" % kh])
                        c.dma(szck[kh][:], self.sz_tok[rows, :].rearrange("(k t) f -> k (t f)", t=8), r=["sz_tok"], w=["szck%d" % kh])
                    with self.scope() as ss:
                        TS = lambda name, shape, dt=F32: ss.enter_context(self.sbt(name, shape, dt))
                        Ug = TS("Ug", [128, 32, 256], BF16)
                        Vb2 = [TS("Vb%d" % i, [64, 2, GB, 256]) for i in range(2)]
                        Wb2 = [TS("Wb%d" % i, [64, 2, GB, 256]) for i in range(2)]
                        tA2 = [TS("tA%d" % i, [64, GB, 256]) for i in range(2)]; tB2 = [TS("tB%d" % i, [64, GB, 256]) for i in range(2)]
                        tC2 = [TS("tC0", [64, GB, 256])] * 2; tD2 = [TS("tD0", [64, GB, 256])] * 2
                        ck2 = [TS("ck%d" % i, [64, GB, 256]) for i in range(2)]; sk2 = [TS("sk%d" % i, [64, GB, 256]) for i in range(2)]
                        rh2 = [TS("rh%d" % i, [64, GB, 256]) for i in range(2)]
                        Xs2 = [TS("Xs%d" % i, [64, 2, GB, 256], BF16) for i in range(2)]
                        Ysb = TS("Ysb", [128, 8, 256], BF16)
                        for i in range(2):
                            c.op("pool", lambda e: e.memset(Xs2[i][:], 0.0), w=["Xs%d" % i])

                        def load_tabs(b):
                            if b < 32 // GB:
                                gs_ = slice(b * GB, (b + 1) * GB)
                                c.dma(ck2[b % 2][:], self.rotc[:, gs_, :], r=["rotc"], w=["ck%d" % (b % 2)])
                                c.dma(sk2[b % 2][:], self.rots[:, gs_, :], r=["rots"], w=["sk%d" % (b % 2)])
                                c.dma(rh2[b % 2][:], self.rhot[:, gs_, :], r=["rhot"], w=["rh%d" % (b % 2)])
                        load_tabs(0)
                        ucg = TS("ucg", [128, 32, 128], BF16)
                        for kh in range(2):
                            self.cp("pool" if kh == 0 else "dve", ucg[:].rearrange("p g (s c) -> p g s c", s=8),
                                    uck[kh][:].rearrange("p (s g c) -> p g s c", s=8, g=32), r=["uck%d" % kh], w=["ucg"])
                            for g8 in range(4):
                                pb = g8 % 2
                                pk = "ps%d" % pb
                                ptv = self.ps[pb][:].bitcast(BF16).rearrange("p (g k) -> p g k", g=8)
                                for gl in range(8):
                                    g = g8 * 8 + gl
                                    c.op("pe", lambda e: e.transpose(out=ptv[:, gl, :], in_=ucg[:, g, :],
                                                                     identity=self.ident[:]), r=["ucg", "ident"], w=[pk])
                                self.cp("dve" if g8 % 2 == 0 else "act", Ug[:, g8 * 8:(g8 + 1) * 8, kh * 128:(kh + 1) * 128], ptv,
                                        r=[pk], w=["Ug"])
                        for b in range(32 // GB):
                            gs = slice(b * GB, (b + 1) * GB)
                            bp = b % 2
                            Vb, Wb, tA, tB, ck, sk, rh, Xs = Vb2[bp], Wb2[bp], tA2[bp], tB2[bp], ck2[bp], sk2[bp], rh2[bp], Xs2[bp]
                            tC, tD = tC2[bp], tD2[bp]
                            ktC, ktD = "tC0", "tD0"
                            kVb, kW0, kW1, ktA, ktB, kck, ksk, krh, kXs = ("Vb%d" % bp, "Wb0_%d" % bp, "Wb1_%d" % bp, "tA%d" % bp, "tB%d" % bp,
                                                                           "ck%d" % bp, "sk%d" % bp, "rh%d" % bp, "Xs%d" % bp)
                            load_tabs(b + 1)
                            for gl in range(GB):
                                g = b * GB + gl
                                pb = 2 + gl % 2
                                pk = "ps%d" % pb
                                c.op("pe", lambda e: e.matmul(out=self.ps[pb][0:64, 0:256], lhsT=Wii[:, g, 0, :], rhs=Ug[:, g, :],
                                                              start=True, stop=True), r=["Wii", "Ug"], w=[pk])
                                c.op("pe", lambda e: e.matmul(out=self.ps[pb][0:64, 256:512], lhsT=Wii[:, g, 1, :], rhs=Ug[:, g, :],
                                                              start=True, stop=True), r=["Wii", "Ug"], w=[pk])
                                self.cp("act", Vb[:, :, gl, :], self.ps[pb][0:64, :].rearrange("p (r k) -> p r k", r=2), r=[pk], w=[kVb])
                            self.tt("pool", tB[:], sk[:], Vb[:, 1], ALU.mult, r=[ksk, kVb], w=[ktB])
                            self.tt("dve", tA[:], ck[:], Vb[:, 0], ALU.mult, r=[kck, kVb], w=[ktA])
                            self.tt("dve", tC[:], ck[:], Vb[:, 1], ALU.mult, r=[kck, kVb], w=[ktC])
                            self.tt("dve", tD[:], sk[:], Vb[:, 0], ALU.mult, r=[ksk, kVb], w=[ktD])
                            self.tt("dve", Wb[:, 1], tC[:], tD[:], ALU.subtract, r=[ktC, ktD], w=[kW1])
                            self.tt("dve", Wb[:, 0], tA[:], tB[:], ALU.add, r=[ktA, ktB], w=[kW0])
                            fl = lambda a: a.rearrange("p g k -> p (g k)")
                            c.op("dve", lambda e: e.tensor_tensor_scan(out=fl(Vb[:, 1]), data0=fl(rh[:]), data1=fl(Wb[:, 1]), initial=0.0,
                                                                       op0=ALU.mult, op1=ALU.add), r=[krh, kW1, kVb], w=[kVb])
                            c.op("dve", lambda e: e.tensor_tensor_scan(out=fl(Vb[:, 0]), data0=fl(rh[:]), data1=fl(Wb[:, 0]), initial=0.0,
                                                                       op0=ALU.mult, op1=ALU.add), r=[krh, kW0, kVb], w=[kVb])
                            K1 = 255
                            self.tt("pool", tB[:, :, 0:K1], sk[:, :, 0:K1], Vb[:, 1, :, 0:K1], ALU.mult, r=[ksk, kVb], w=[ktB])
                            self.tt("dve", tA[:, :, 0:K1], ck[:, :, 0:K1], Vb[:, 0, :, 0:K1], ALU.mult, r=[kck, kVb], w=[ktA])
                            self.tt("dve", tC[:, :, 0:K1], ck[:, :, 0:K1], Vb[:, 1, :, 0:K1], ALU.mult, r=[kck, kVb], w=[ktC])
                            self.tt("dve", tD[:, :, 0:K1], sk[:, :, 0:K1], Vb[:, 0, :, 0:K1], ALU.mult, r=[ksk, kVb], w=[ktD])
                            self.tt("dve", Xs[:, 1, :, 1:256], tC[:, :, 0:K1], tD[:, :, 0:K1], ALU.add, r=[ktC, ktD], w=[kXs])
                            self.tt("dve", Xs[:, 0, :, 1:256], tA[:, :, 0:K1], tB[:, :, 0:K1], ALU.subtract, r=[ktA, ktB], w=[kXs])
                            for gl in range(GB):
                                g = b * GB + gl
                                pb = 4 + gl // 2 % 2
                                pk = "ps%d" % pb
                                o = self.ps[pb][:, (gl % 2) * 256:(gl % 2 + 1) * 256]
                                c.op("pe", lambda e: e.matmul(out=o, lhsT=Toep[:, g, :], rhs=Ug[:, g, :], start=True, stop=False),
                                     r=["Toep", "Ug"], w=[pk])
                                c.op("pe", lambda e: e.matmul(out=o, lhsT=WivR[:, g, :], rhs=Xs[:, 0, gl, :], start=False, stop=False),
                                     r=["WivR", kXs], w=[pk])
                                c.op("pe", lambda e: e.matmul(out=o, lhsT=WivI[:, g, :], rhs=Xs[:, 1, gl, :], start=False, stop=True),
                                     r=["WivI", kXs], w=[pk])
                                if gl % 2 == 1:
                                    g8l = (b * GB + gl - 1) % 8
                                    self.cp("act", Ysb[:, g8l:g8l + 2, :], self.ps[pb][:].rearrange("p (g k) -> p g k", g=2), r=[pk], w=["Ysb"])
                            if (b * GB + GB) % 8 == 0:
                                g8 = (b * GB) // 8
                                for kh in range(2):
                                    pb = 6 + kh
                                    pk = "ps%d" % pb
                                    ptv = self.ps[pb][:].bitcast(BF16).rearrange("p (g n) -> p g n", g=8)
                                    for gl in range(8):
                                        c.op("pe", lambda e: e.transpose(out=ptv[:, gl, :], in_=Ysb[:, gl, kh * 128:(kh + 1) * 128],
                                                                         identity=self.ident[:]), r=["Ysb", "ident"], w=[pk])
                                    self.cp("dve", yck[kh][:, :, g8 * 128:(g8 + 1) * 128].rearrange("p t (g c) -> p t g c", g=8),
                                            ptv.rearrange("p g (t c) -> p t g c", t=8), r=[pk], w=["yck%d" % kh])
                    with self.scope() as se:
                        TE_ = lambda name, shape, dt=F32: se.enter_context(self.sbt(name, shape, dt))
                        t1 = TE_("e_t1", [128, 8, BR]); t2 = TE_("e_t2", [128, 8, BR])
                        gck = TE_("gck", [128, 8, BR], BF16)
                        ack = TE_("ack", [128, 8, BR], BF16)
                        gT = [TE_("gT%d" % i, [128, 4, 128], BF16) for i in range(2)]
                        e1 = [TE_("e1_%d" % i, [128, BR]) for i in range(2)]
                        for kh in range(2):
                            uv = uck[kh][:].rearrange("p (s f) -> p s f", s=8)
                            zv = szck[kh][:].rearrange("p (s f) -> p s f", s=8)
                            self.tt("dve", t1[:], uv, Drep[:].unsqueeze(1).broadcast_to([128, 8, BR]), ALU.mult, r=["uck%d" % kh, "Drep"], w=["e_t1"])
                            self.tt("dve", t1[:], t1[:], yck[kh][:], ALU.add, r=["e_t1", "yck%d" % kh], w=["e_t1"])
                            self.actf(t2[:], t1[:], AF.Square, r=["e_t1"], w=["e_t2"])
                            self.actf(t2[:], t2[:], AF.Copy, r=["e_t2"], w=["e_t2"], scale=0.044715 * 0.7978845608, bias=0.7978845608)
                            self.tt("dve", t2[:], t2[:], t1[:], ALU.mult, r=["e_t2", "e_t1"], w=["e_t2"])
                            self.actf(t2[:], t2[:], AF.Tanh, r=["e_t2"], w=["e_t2"])
                            self.actf(t2[:], t2[:], AF.Copy, r=["e_t2"], w=["e_t2"], scale=0.5, bias=0.5)
                            self.tt("dve", gck[:], t2[:], t1[:], ALU.mult, r=["e_t2", "e_t1"], w=["gck"])
                            def glu_front(tau):
                                i2 = tau % 2
                                pk = "ps%d" % i2
                                ptv = self.ps[i2][:].bitcast(BF16).rearrange("p (a b) -> p a b", a=8)
                                for fc in range(4):
                                    c.op("pe", lambda e: e.transpose(out=ptv[:, fc, :], in_=gck[:, tau, fc * 128:(fc + 1) * 128],
                                                                     identity=self.ident[:]), r=["gck", "ident"], w=[pk])
                                self.cp("act", gT[i2][:], ptv[:, 0:4, :], r=[pk], w=["gT%d" % i2])
                            glu_front(0)
                            for tau in range(8):
                                i2 = tau % 2
                                pm = 2 + i2
                                for fc in range(4):
                                    c.op("pe", lambda e: e.matmul(out=self.ps[pm][:], lhsT=gT[i2][:, fc, :], rhs=gluw[:, fc, :],
                                                                  start=(fc == 0), stop=(fc == 3)), r=["gT%d" % i2, "gluw"], w=["ps%d" % pm])
                                if tau + 1 < 8:
                                    glu_front(tau + 1)
                                ek = "e1_%d" % i2
                                self.tt("dve", e1[i2][:], self.ps[pm][:], Brep[:], ALU.add, r=["ps%d" % pm, "Brep"], w=[ek])
                                self.actf(e1[i2][:], e1[i2][:], AF.Tanh, r=[ek], w=[ek], scale=0.5)
                                self.ts("dve", e1[i2][:], e1[i2][:], 0.5, ALU.mult, r=[ek], w=[ek], s2=0.5, op1=ALU.add)
                                self.tt("pool", e1[i2][:], e1[i2][:], gck[:, tau, :], ALU.mult, r=[ek, "gck"], w=[ek])
                                self.tt("dve", ack[:, tau, :], e1[i2][:], zv[:, tau, :], ALU.mult, r=[ek, "szck%d" % kh], w=["ack"])
                            rows = slice(tb + kh * 1024, tb + (kh + 1) * 1024)
                            c.dma(self.a_tok[rows, :].rearrange("(k t) f -> k (t f)", t=8), ack[:].rearrange("p t f -> p (t f)"),
                                  r=["ack"], w=["a_tok"])

    def phase_ml(self):
        nc, c = self.nc, self.c
        S_ = self.S
        NB = S_ // 128
        SC = 128 ** -0.5
        with self.scope() as st:
            T = lambda name, shape, dt=F32: st.enter_context(self.sbt(name, shape, dt))
            cw = T("m_cw", [128, 4, 4]); cb = T("m_cb", [128, 4])
            mn = T("m_mn", [128, 4]); msk = T("m_msk", [128, 4])
            gbi = T("m_gbi", [4, 1]); gbf = T("m_gbf", [4, 1]); ngbf = T("m_ngbf", [4, 1])
            maskS = T("m_maskS", [128, 128]); idf = T("m_idf", [128, 128])
            ones4 = T("m_ones4", [4, 128])
            wst = T("m_wst", [128, 3, 4, 128]); wqkv = T("m_wqkv", [128, 3, 4, 128], BF16)
            gst = T("m_gst", [128, 12, 8]); gw = T("m_gw", [128, 12, 8], BF16)
            for (t, d, key) in ((cw, self.ml_cw, "m_cw"), (cb, self.ml_cb, "m_cb"), (mn, self.ml_mn, "m_mn"),
                                (msk, self.ml_msk, "m_msk"), (gbi, self.ml_gbi, "m_gbi"), (gbf, self.ml_gbf, "m_gbf"),
                                (maskS, self.ml_maskS, "m_maskS"), (idf, self.identf_d, "m_idf"),
                                (gst, self.ml_gw, "m_gst")):
                c.dma(t[:], d, w=[key])
            for i, d in enumerate((self.ml_wq, self.ml_wk, self.ml_wv)):
                c.dma(wst[:, i], d, w=["m_wst"])
            self.cp("dve", wqkv[:], wst[:], r=["m_wst"], w=["m_wqkv"])
            self.cp("dve", gw[:], gst[:], r=["m_gst"], w=["m_gw"])
            self.ts("dve", ngbf[:], gbf[:], -1.0, ALU.mult, r=["m_gbf"], w=["m_ngbf"])
            c.op("dve", lambda e: e.memset(ones4[:], 1.0), w=["m_ones4"])
            xmT_v = self.xmT.rearrange("(c p) t -> p c t", p=128)
            mzT_v = self.mzT.rearrange("(c p) t -> p c t", p=128)
            bT_v = self.bT.rearrange("(c p) t -> p c t", p=128)
            for q in range(self.nseq):
                tb = q * S_
                with self.scope() as sq:
                    TQ = lambda name, shape, dt=F32: sq.enter_context(self.sbt(name, shape, dt))
                    xcT = TQ("xcT", [128, 4, S_], BF16)
                    qT = TQ("qT", [128, 4, S_], BF16)
                    kT = TQ("kT", [128, 4, S_], BF16)
                    Ktok = TQ("Ktok", [128, NB, 4, 128], BF16)
                    Vtok = TQ("Vtok", [128, NB, 4, 129], BF16)
                    acol = TQ("acol", [128, NB, 4]); bcol = TQ("bcol", [128, NB, 4])
                    Rrep = TQ("Rrep", [128, NB + 1, 4])
                    Wt = TQ("Wt", [128, NB, 4]); Wp = TQ("Wp", [128, NB, 4])
                    Thr = TQ("Thr", [128, NB, 4]); Dec = TQ("Dec", [128, NB, 4])
                    c.op("pool", lambda e: e.memset(Vtok[:, :, :, 128:129], 1.0), w=["Vtok"])
                    with self.scope() as sa:
                        TA_ = lambda name, shape, dt=F32: sa.enter_context(self.sbt(name, shape, dt))
                        xm = TA_("xm", [128, 4, S_], BF16)
                        vT = TA_("vT", [128, 4, S_], BF16)
                        acc = TA_("acc", [128, S_])
                        g1f = TA_("g1", [32, S_]); g2f = TA_("g2", [32, S_]); g3f = TA_("g3", [32, S_]); onesr = TA_("onesr", [32, S_])
                        g1 = g1f[0:4, :]; g2 = g2f[0:4, :]; g3 = g3f[0:4, :]
                        c.op("pool", lambda e: e.memset(g1f[:], 0.0), w=["g1"])
                        c.op("pool", lambda e: e.memset(g2f[:], 0.0), w=["g2"])
                        rsel = TA_("rsel", [4, NB, 4])
                        c.dma(xm[:], xmT_v[:, :, tb:tb + S_], r=["xmT"], w=["xm"])
                        c.op("pool", lambda e: e.memset(onesr[:], 1.0), w=["onesr"])
                        for fc in range(4):
                            self.ts("dve", acc[:], xm[:, fc, :], cw[:, fc, 3:4], ALU.mult, r=["xm", "m_cw", "m_cb"], w=["acc"],
                                    s2=cb[:, fc:fc + 1], op1=ALU.add)
                            for sh in (1, 2, 3):
                                self.stt("dve", acc[:, sh:], xm[:, fc, 0:S_ - sh], cw[:, fc, 3 - sh:4 - sh], acc[:, sh:],
                                         ALU.mult, ALU.add, r=["xm", "m_cw", "acc"], w=["acc"])
                            self.actf(xcT[:, fc, :], acc[:], AF.Silu, r=["acc"], w=["xcT"])
                        for h in range(4):
                            for tl in range(S_ // 512):
                                ts_ = slice(tl * 512, (tl + 1) * 512)
                                for (i, src, skey, dst, dkey) in ((0, xcT, "xcT", qT, "qT"), (1, xcT, "xcT", kT, "kT"), (2, xm, "xm", vT, "vT")):
                                    pb = (h * 12 + tl * 3 + i) % 4
                                    pk = "ps%d" % pb
                                    c.op("pe", lambda e: e.matmul(out=self.ps[pb][:], lhsT=wqkv[:, i, h, :], rhs=src[:, h, ts_],
                                                                  start=True, stop=True), r=["m_wqkv", skey], w=[pk])
                                    self.cp("act" if i != 1 else "dve", dst[:, h, ts_], self.ps[pb][:], r=[pk], w=[dkey])
                        for blk in range(NB):
                            bs = slice(blk * 128, (blk + 1) * 128)
                            for (i, src, skey, dst, dkey, pb) in ((1, xcT, "xcT", Ktok, "Ktok", 4), (2, xm, "xm", Vtok, "Vtok", 5)):
                                pb = pb + 2 * (blk % 2)
                                pk = "ps%d" % pb
                                for h in range(4):
                                    c.op("pe", lambda e: e.matmul(out=self.ps[pb][:, h * 128:(h + 1) * 128], lhsT=src[:, h, bs],
                                                                  rhs=wqkv[:, i, h, :], start=True, stop=True), r=["m_wqkv", skey], w=[pk])
                                self.cp("act" if i == 1 else "dve", dst[:, blk, :, 0:128], self.ps[pb][:].rearrange("p (h e) -> p h e", h=4),
                                        r=[pk], w=[dkey])
                        for tl in range(S_ // 512):
                            ts_ = slice(tl * 512, (tl + 1) * 512)
                            for half in range(2):
                                pb = half
                                pk = "ps%d" % pb
                                for ch in range(12):
                                    src = (qT, kT, vT)[ch // 4]
                                    skey = ("qT", "kT", "vT")[ch // 4]
                                    c.op("pe", lambda e: e.matmul(out=self.ps[pb][0:4, :], lhsT=gw[:, ch, half * 4:half * 4 + 4],
                                                                  rhs=src[:, ch % 4, ts_], start=(ch == 0), stop=(ch == 11)),
                                         r=["m_gw", skey], w=[pk])
                                if half == 0:
                                    self.ts("dve", g1[:, ts_], self.ps[pb][0:4, :], gbi[:, 0:1], ALU.add, r=[pk, "m_gbi"], w=["g1"])
                                else:
                                    self.actf(g2[:, ts_], self.ps[pb][0:4, :], AF.Exp, r=[pk, "m_ngbf"], w=["g2"], scale=-1.0, bias=ngbf[:, 0:1])
                        self.actf(g2[:], g2[:], AF.Ln, r=["g2"], w=["g2"], bias=1.0)
                        c.op("dve", lambda e: e.tensor_tensor_scan(out=g3f[:], data0=onesr[:], data1=g2f[:], initial=0.0, op0=ALU.mult, op1=ALU.add),
                             r=["onesr", "g2"], w=["g3"])
                        self.tt("dve", g1[:], g1[:], g3[:], ALU.add, r=["g1", "g3"], w=["g1"])
                        c.op("dve", lambda e: e.tensor_tensor_scan(out=g2f[:], data0=onesr[:], data1=g1f[:], initial=0.0, op0=ALU.mult, op1=ALU.max),
                             r=["onesr", "g1", "g2"], w=["g2"])
                        pa = self.ps[2][:, 0:NB * 4].rearrange("p (b h) -> p b h", h=4)
                        pbn = self.ps[3][:, 0:NB * 4].rearrange("p (b h) -> p b h", h=4)
                        for blk in range(NB):
                            bs = slice(blk * 128, (blk + 1) * 128)
                            c.op("pe", lambda e: e.transpose(out=pa[:, blk, :], in_=g1[:, bs], identity=idf[0:4, 0:4]), r=["g1", "m_idf"], w=["ps2"])
                            c.op("pe", lambda e: e.transpose(out=pbn[:, blk, :], in_=g3[:, bs], identity=idf[0:4, 0:4]), r=["g3", "m_idf"], w=["ps3"])
                        self.cp("dve", acol[:], pa, r=["ps2"], w=["acol"])
                        self.cp("dve", bcol[:], pbn, r=["ps3"], w=["bcol"])
                        self.tt("dve", rsel[:], g2[:, 127::128].unsqueeze(2).broadcast_to([4, NB, 4]),
                                idf[0:4, 0:4].unsqueeze(1).broadcast_to([4, NB, 4]), ALU.mult, r=["g2", "m_idf"], w=["rsel"])
                        c.op("pe", lambda e: e.matmul(out=self.ps[0][:, 0:NB * 4], lhsT=ones4[:], rhs=rsel[:].rearrange("p b h -> p (b h)"),
                                                      start=True, stop=True), r=["m_ones4", "rsel"], w=["ps0"])
                        c.op("dve", lambda e: e.memset(Rrep[:, 0, :], 0.0), w=["Rrep"])
                        self.cp("dve", Rrep[:, 1:NB + 1, :], self.ps[0][:, 0:NB * 4].rearrange("p (b h) -> p b h", h=4), r=["ps0"], w=["Rrep"])
                    self.tt("dve", Wt[:], acol[:], Rrep[:, 0:NB, :], ALU.subtract, r=["acol", "Rrep"], w=["Wt"])
                    self.actf(Wt[:], Wt[:], AF.Exp, r=["Wt"], w=["Wt"])
                    self.tt("dve", Wp[:], acol[:], Rrep[:, 1:NB + 1, :], ALU.subtract, r=["acol", "Rrep"], w=["Wp"])
                    self.actf(Wp[:], Wp[:], AF.Exp, r=["Wp"], w=["Wp"])
                    self.ts("dve", Wp[:], Wp[:], SC, ALU.mult, r=["Wp"], w=["Wp"])
                    self.tt("dve", Thr[:], bcol[:], Rrep[:, 0:NB, :], ALU.subtract, r=["bcol", "Rrep"], w=["Thr"])
                    self.actf(Thr[:], Thr[:], AF.Exp, r=["Thr"], w=["Thr"])
                    self.tt("dve", Dec[:], Rrep[:, 0:NB, :], Rrep[:, 1:NB + 1, :], ALU.subtract, r=["Rrep"], w=["Dec"])
                    self.actf(Dec[:], Dec[:], AF.Exp, r=["Dec"], w=["Dec"])
                    with self.scope() as sm:
                        TM = lambda name, shape, dt=F32: sm.enter_context(self.sbt(name, shape, dt))
                        C32 = TM("C32", [128, 4, 129]); Cm = TM("Cm", [128, 4, 129])
                        Cb = TM("Cb", [128, 4, 129], BF16)
                        PT4 = [TM("PT4_%d" % i, [128, 4, 128], BF16) for i in range(2)]
                        Vp4 = [TM("Vp4_%d" % i, [128, 4, 129], BF16) for i in range(2)]
                        Vpp4 = [TM("Vpp4_%d" % i, [128, 4, 129], BF16) for i in range(2)]
                        den = TM("den4", [128, 4])
                        hraw = [TM("hraw%d" % i, [128, 4, 128]) for i in range(2)]
                        bst = TM("bst", [128, 4, 6]); mv = TM("mv", [128, 4, 2]); rs = TM("rs", [128, 4])
                        hn = TM("hn", [128, 4, 128], BF16)
                        e1 = TM("m_e1", [128, 4, 128]); e2 = TM("m_e2", [128, 4, 128])
                        mz = [TM("mz%d" % i, [128, 4, 512], BF16) for i in range(2)]
                        bo = [TM("bo%d" % i, [128, 4, 512], BF16) for i in range(2)]

                        def emit_front(I_):
                            bs_ = slice(I_ * 128, (I_ + 1) * 128)
                            j_ = I_ % 2
                            for h_ in range(4):
                                c.op("pe", lambda e: e.matmul(out=self.ps[j_][:, h_ * 128:(h_ + 1) * 128], lhsT=kT[:, h_, bs_], rhs=qT[:, h_, bs_],
                                                              start=True, stop=True), r=["kT", "qT"], w=["ps%d" % j_])
                            self.tt("dve", PT4[j_][:], self.ps[j_][:].rearrange("p (h t) -> p h t", h=4),
                                    maskS[:].unsqueeze(1).broadcast_to([128, 4, 128]), ALU.mult, r=["ps%d" % j_, "m_maskS"], w=["PT4_%d" % j_])
                            for h_ in range(4):
                                c.op("act", lambda e: e.activation(out=Vp4[j_][:, h_, :], in_=Vtok[:, I_, h_, :], func=AF.Copy, scale=Wt[:, I_, h_:h_ + 1]),
                                     r=["Vtok", "Wt"], w=["Vp4_%d" % j_])
                                c.op("act", lambda e: e.activation(out=Vpp4[j_][:, h_, :], in_=Vtok[:, I_, h_, :], func=AF.Copy, scale=Wp[:, I_, h_:h_ + 1]),
                                     r=["Vtok", "Wp"], w=["Vpp4_%d" % j_])
                        emit_front(0)
                        for I in range(NB):
                            bs = slice(I * 128, (I + 1) * 128)
                            i4 = (I // 4) % 2
                            j = I % 2
                            if I % 4 == 0:
                                c.dma(mz[i4][:], mzT_v[:, :, tb + I * 128:tb + I * 128 + 512], r=["mzT"], w=["mz%d" % i4])
                            hk = "hraw%d" % j
                            for h in range(4):
                                pO = self.ps[2 + h // 2][:, (h % 2) * 129:(h % 2 + 1) * 129]
                                kO = "ps%d" % (2 + h // 2)
                                c.op("pe", lambda e: e.matmul(out=pO, lhsT=PT4[j][:, h, :], rhs=Vp4[j][:, h, :], start=True, stop=(I == 0)),
                                     r=["PT4_%d" % j, "Vp4_%d" % j], w=[kO])
                                if I > 0:
                                    c.op("pe", lambda e: e.matmul(out=pO, lhsT=qT[:, h, bs], rhs=Cb[:, h, :], start=False, stop=True),
                                         r=["qT", "Cb"], w=[kO])
                            if I < NB - 1:
                                for h in range(4):
                                    pC = self.ps[4 + h // 2][:, (h % 2) * 129:(h % 2 + 1) * 129]
                                    c.op("pe", lambda e: e.matmul(out=pC, lhsT=Ktok[:, I, h, :], rhs=Vpp4[j][:, h, :], start=True, stop=True),
                                         r=["Ktok", "Vpp4_%d" % j], w=["ps%d" % (4 + h // 2)])
                            if I + 1 < NB:
                                emit_front(I + 1)
                            if I < NB - 1:
                                if I == 0:
                                    for hb in range(2):
                                        self.cp("dve", C32[:, 2 * hb:2 * hb + 2, :], self.ps[4 + hb][:, 0:258].rearrange("p (h e) -> p h e", h=2),
                                                r=["ps%d" % (4 + hb)], w=["C32"])
                                else:
                                    self.tt("pool", Cm[:], C32[:], Dec[:, I, :].unsqueeze(2).broadcast_to([128, 4, 129]), ALU.mult, r=["C32", "Dec"], w=["Cm"])
                                    for hb in range(2):
                                        self.tt("dve", C32[:, 2 * hb:2 * hb + 2, :], self.ps[4 + hb][:, 0:258].rearrange("p (h e) -> p h e", h=2),
                                                Cm[:, 2 * hb:2 * hb + 2, :], ALU.add, r=["ps%d" % (4 + hb), "Cm"], w=["C32"])
                                self.cp("act", Cb[:], C32[:], r=["C32"], w=["Cb"])
                            for hb in range(2):
                                self.actf(den[:, 2 * hb:2 * hb + 2], self.ps[2 + hb][:, 0:258].rearrange("p (h e) -> p h e", h=2)[:, :, 128],
                                          AF.Abs, r=["ps%d" % (2 + hb)], w=["den4"])
                            self.tt("dve", den[:], den[:], Thr[:, I, :], ALU.max, r=["den4", "Thr"], w=["den4"])
                            c.op("dve", lambda e: e.reciprocal(out=den[:], in_=den[:]), r=["den4"], w=["den4"])
                            for hb in range(2):
                                self.tt("dve", hraw[j][:, 2 * hb:2 * hb + 2, :], self.ps[2 + hb][:, 0:258].rearrange("p (h e) -> p h e", h=2)[:, :, 0:128],
                                        den[:, 2 * hb:2 * hb + 2].unsqueeze(2).broadcast_to([128, 2, 128]), ALU.mult, r=["ps%d" % (2 + hb), "den4"], w=[hk])
                            mvk = ["mv%d" % h for h in range(4)]
                            for h in range(4):
                                c.op("dve", lambda e: e.bn_stats(out=bst[:, h, :], in_=hraw[j][:, h, :]), r=[hk], w=["bst%d" % h])
                            for h in range(4):
                                c.op("dve", lambda e: e.bn_aggr(out=mv[:, h, :], in_=bst[:, h, :]), r=["bst%d" % h], w=["mv%d" % h])
                            self.actf(rs[:], mv[:, :, 1], AF.Ln, r=mvk, w=["rs"], bias=HEAD_EPS)
                            self.actf(rs[:], rs[:], AF.Exp, r=["rs"], w=["rs"], scale=-0.5)
                            self.tt("pool", e1[:], hraw[j][:], mv[:, :, 0:1].broadcast_to([128, 4, 128]), ALU.subtract, r=[hk] + mvk, w=["m_e1"])
                            self.tt("dve", hn[:], e1[:], rs[:].unsqueeze(2).broadcast_to([128, 4, 128]), ALU.mult, r=["m_e1", "rs"], w=["hn"])
                            ptv = self.ps[6 + I % 2][:].bitcast(BF16).rearrange("p (a b) -> p a b", a=8)
                            pk = "ps%d" % (6 + I % 2)
                            for h in range(4):
                                c.op("pe", lambda e: e.transpose(out=ptv[:, h, :], in_=hn[:, h, :], identity=self.ident[:]), r=["hn", "ident"], w=[pk])
                            self.tt("pool", e2[:], xcT[:, :, bs], msk[:].unsqueeze(2).broadcast_to([128, 4, 128]), ALU.mult, r=["xcT", "m_msk"], w=["m_e2"])
                            self.tt("dve", e1[:], ptv[:, 0:4, :], mn[:].unsqueeze(2).broadcast_to([128, 4, 128]), ALU.mult, r=[pk, "m_mn", "m_e1"], w=["m_e1"])
                            self.tt("dve", e1[:], e1[:], e2[:], ALU.add, r=["m_e1", "m_e2"], w=["m_e1"])
                            off = (I % 4) * 128
                            self.tt("pool", bo[i4][:, :, off:off + 128], e1[:], mz[i4][:, :, off:off + 128], ALU.mult,
                                    r=["m_e1", "mz%d" % i4], w=["bo%d" % i4])
                            if I % 4 == 3:
                                c.dma(bT_v[:, :, tb + (I - 3) * 128:tb + (I + 1) * 128], bo[i4][:], r=["bo%d" % i4], w=["bT"])
        c.barrier()

    def out_proj_norm_res(self, lay, wout_d, pg_rep_d, lhs_provider, res_rows, dst_rows, dst_key, res_key):
        nc, c = self.nc, self.c
        NT = self.NT
        with self.scope() as st:
            T = lambda name, shape, dt=F32: st.enter_context(self.sbt(name, shape, dt))
            wout = T("wout", [128, 8, D], BF16)
            wst = [T("wost%d" % i, [128, D]) for i in range(2)]
            for fc in range(8):
                c.dma(wst[fc % 2][:], wout_d[fc * 128:(fc + 1) * 128, :], w=["wost%d" % (fc % 2)])
                self.cp("dve" if fc % 2 else "act", wout[:, fc, :], wst[fc % 2][:], r=["wost%d" % (fc % 2)], w=["wout"])
            pg = T("pg", [128, D])
            c.dma(pg[:], pg_rep_d, w=["pg"])
            NXR = 4
            xr = [T("xr%d" % i, [128, D]) for i in range(NXR)]
            yo = [T("yo%d" % i, [128, D]) for i in range(2)]
            junk = T("ojunk", [128, BR])
            ss2 = [T("oss%d" % i, [128, 2]) for i in range(2)]; rstd2 = [T("orstd%d" % i, [128, 1]) for i in range(2)]
            pref, prov = lhs_provider(st)
            nblk = NT // 128

            def prefetch(b):
                if b < nblk:
                    c.dma(xr[b % NXR][:], res_rows[b * 128:(b + 1) * 128, :], r=[res_key % b], w=["xr%d" % (b % NXR)])
                    pref(b)
            prefetch(0)
            prefetch(1)
            lhs_next = prov(0)
            for blk in range(nblk):
                rows = slice(blk * 128, (blk + 1) * 128)
                xk = "xr%d" % (blk % NXR)
                prefetch(blk + 2)
                lhs = lhs_next
                ss = ss2[blk % 2]; rstd = rstd2[blk % 2]
                ssk = "oss%d" % (blk % 2); rsk = "orstd%d" % (blk % 2)
                for half in range(2):
                    pb = 4 + half + 2 * (blk % 2)
                    pk = "ps%d" % pb
                    for fc in range(8):
                        ap, key = lhs[fc]
                        c.op("pe", lambda e: e.matmul(out=self.ps[pb][:], lhsT=ap, rhs=wout[:, fc, half * BR:(half + 1) * BR],
                                                      start=(fc == 0), stop=(fc == 7)), r=[key, "wout"], w=[pk])
                if blk + 1 < nblk:
                    lhs_next = prov(blk + 1)
                for half in range(2):
                    pb = 4 + half + 2 * (blk % 2)
                    pk = "ps%d" % pb
                    self.actf(junk[:], self.ps[pb][:], AF.Square, r=[pk], w=["ojunk", ssk], accum_out=ss[:, half:half + 1])
                self.tt("dve", rstd[:], ss[:, 0:1], ss[:, 1:2], ALU.add, r=[ssk], w=[rsk])
                self.actf(rstd[:], rstd[:], AF.Ln, r=[rsk], w=[rsk], scale=1.0 / D, bias=NORM_EPS)
                self.actf(rstd[:], rstd[:], AF.Exp, r=[rsk], w=[rsk], scale=-0.5)
                yk = "yo%d" % (blk % 2)
                for half in range(2):
                    pb = 4 + half + 2 * (blk % 2)
                    hs = slice(half * BR, (half + 1) * BR)
                    self.tt("dve", yo[blk % 2][:, hs], self.ps[pb][:], pg[:, hs], ALU.mult, r=["ps%d" % pb, "pg"], w=[yk])
                self.stt("dve", yo[blk % 2][:], yo[blk % 2][:], rstd[:, 0:1], xr[blk % NXR][:], ALU.mult, ALU.add, r=[yk, rsk, xk], w=[yk])
                c.dma(dst_rows[rows, :], yo[blk % 2][:], r=[yk], w=[dst_key % blk])

    def phase_l0_out(self):
        nc, c = self.nc, self.c
        bT_v = self.bT.rearrange("(c p) t -> p c t", p=128)

        def provider(st):
            T = lambda name, shape, dt=F32: st.enter_context(self.sbt(name, shape, dt))
            at = [T("at%d" % i, [128, BR], BF16) for i in range(4)]
            aT = [T("aT%d" % i, [128, 4, 128], BF16) for i in range(2)]
            bt = [T("bt%d" % i, [128, 4, 512], BF16) for i in range(2)]

            def pref(blk):
                i4 = (blk // 4) % 2
                c.dma(at[blk % 4][:], self.a_tok[blk * 128:(blk + 1) * 128, :], r=["a_tok"], w=["at%d" % (blk % 4)])
                if blk % 4 == 0:
                    c.dma(bt[i4][:], bT_v[:, :, blk * 128:blk * 128 + 512], r=["bT"], w=["bt%d" % i4])

            def prov(blk):
                i = blk % 2
                i4 = (blk // 4) % 2
                pk = "ps%d" % i
                ptv = self.ps[i][:].bitcast(BF16).rearrange("p (a b) -> p a b", a=8)
                for fc in range(4):
                    c.op("pe", lambda e: e.transpose(out=ptv[:, fc, :], in_=at[blk % 4][:, fc * 128:(fc + 1) * 128], identity=self.ident[:]),
                         r=["at%d" % (blk % 4), "ident"], w=[pk])
                self.cp("act", aT[i][:], ptv[:, 0:4, :], r=[pk], w=["aT%d" % i])
                off = (blk % 4) * 128
                return [(aT[i][:, fc, :], "aT%d" % i) for fc in range(4)] + \
                       [(bt[i4][:, h, off:off + 128], "bt%d" % i4) for h in range(4)]
            return pref, prov
        self.out_proj_norm_res(0, self.w_out_ab, self.post0_rep, provider, self.x, self.out, "h1_%d", "x%.0d")

    def phase_l1_proj(self):
        nc, c = self.nc, self.c
        NT = self.NT
        S_ = self.S
        ngrp = NT // 512
        nblk = NT // 128
        with self.scope() as st:
            T = lambda name, shape, dt=F32: st.enter_context(self.sbt(name, shape, dt))
            win = T("win1", [128, 8, 8 * BR], BF16)
            g1 = T("g1n", [128, 8])
            stage = [T("w1st%d" % i, [128, 4 * BR]) for i in range(2)]
            c.dma(g1[:], self.pre1, w=["g1n"])
            for dc in range(8):
                for hf in range(2):
                    i = (dc * 2 + hf) % 2
                    sk = "w1st%d" % i
                    c.dma(stage[i][:], self.w_in_cd[dc * 128:(dc + 1) * 128, hf * 2048:(hf + 1) * 2048], w=[sk])
                    if hf == 0:
                        c.op("act", lambda e: e.activation(out=win[:, dc, 0:2048], in_=stage[i][:], func=AF.Copy, scale=g1[:, dc:dc + 1]),
                             r=[sk, "g1n"], w=["win1"])
                    else:
                        self.ts("dve", win[:, dc, 2048:4096], stage[i][:], g1[:, dc:dc + 1], ALU.mult, r=[sk, "g1n"], w=["win1"])
            cosT = T("cosT", [128, S_]); sinT = T("sinT", [128, S_]); Rm = T("Rm", [128, 128], BF16)
            c.dma(cosT[:], self.rope_cos, w=["cosT"])
            c.dma(sinT[:], self.rope_sin, w=["sinT"])
            c.dma(Rm[:], self.rope_rm, w=["Rm"])
            NXB = 6
            xt = [T("x1t%d" % i, [128, D]) for i in range(NXB)]
            junk = T("junk1", [128, D])
            ss2 = [T("ss1_%d" % i, [128, 4]) for i in range(2)]; rstd2 = [T("rstd1_%d" % i, [128, 4]) for i in range(2)]
            zb = [T("z1b%d" % i, [128, D], BF16) for i in range(2)]
            zT = [T("z1T%d" % i, [128, 8, 512], BF16) for i in range(2)]
            vo = [T("vo%d" % i, [128, BR], BF16) for i in range(4)]
            xb = [T("xb%d" % i, [128, 512], BF16) for i in range(2)]
            r1 = [T("r1_%d" % i, [128, 512]) for i in range(2)]
            r2 = [T("r2_%d" % i, [128, 512]) for i in range(2)]
            fo = [T("fo%d" % i, [128, 4, 512], BF16) for i in range(2)]

            def load_x(b):
                if b < nblk:
                    c.dma(xt[b % NXB][:], self.h1src[b * 128:(b + 1) * 128, :], r=["h1_%d" % b], w=["x1t%d" % (b % NXB)])
            for b in range(4):
                load_x(b)
            fm_groups = [(0, self.cqT, "cqT", "rope"), (512, self.ckT, "ckT", "rope"), (2048, self.dqT, "dqT", "rope"),
                         (2560, self.dkT, "dkT", "rope"), (1536, self.czT, "czT", "silu"), (3584, self.dzT, "dzT", "silu")]
            cnt = 0
            for g in range(ngrp):
                zk = "z1T%d" % (g % 2)
                ss = ss2[g % 2]; rstd = rstd2[g % 2]
                ssk = "ss1_%d" % (g % 2); rsk = "rstd1_%d" % (g % 2)
                pos0 = (g * 512) % S_
                for j in range(4):
                    b = g * 4 + j
                    xk = "x1t%d" % (b % NXB)
                    c.op("act", lambda e: e.activation(out=junk[:], in_=xt[b % NXB][:], func=AF.Square, accum_out=ss[:, j:j + 1]),
                         r=[xk], w=["junk1", ssk])
                self.actf(rstd[:], ss[:], AF.Ln, r=[ssk], w=[rsk], scale=1.0 / D, bias=NORM_EPS)
                self.actf(rstd[:], rstd[:], AF.Exp, r=[rsk], w=[rsk], scale=-0.5)
                for j in range(4):
                    b = g * 4 + j
                    xk = "x1t%d" % (b % NXB)
                    zbk = "z1b%d" % (b % 2)
                    pk = "ps%d" % (b % 2)
                    self.ts("dve", zb[b % 2][:], xt[b % NXB][:], rstd[:, j:j + 1], ALU.mult, r=[xk, rsk], w=[zbk])
                    load_x(b + 4)
                    ptv = self.ps[b % 2][:].bitcast(BF16).rearrange("p (a b) -> p a b", a=8)
                    for dc in range(8):
                        c.op("pe", lambda e: e.transpose(out=ptv[:, dc, :], in_=zb[b % 2][:, dc * 128:(dc + 1) * 128], identity=self.ident[:]),
                             r=[zbk, "ident"], w=[pk])
                    self.cp("dve", zT[g % 2][:, :, j * 128:(j + 1) * 128], ptv, r=[pk], w=[zk])
                    for vi, (col0, dst, dkey) in enumerate(((1024, self.cv_tok, "cv_tok"), (3072, self.dv_tok, "dv_tok"))):
                        pb = 2 + vi
                        vk = "vo%d" % ((b % 2) * 2 + vi)
                        for dc in range(8):
                            c.op("pe", lambda e: e.matmul(out=self.ps[pb][:], lhsT=zT[g % 2][:, dc, j * 128:(j + 1) * 128],
                                                          rhs=win[:, dc, col0:col0 + BR], start=(dc == 0), stop=(dc == 7)), r=[zk, "win1"], w=["ps%d" % pb])
                        self.cp("act", vo[(b % 2) * 2 + vi][:], self.ps[pb][:], r=["ps%d" % pb], w=[vk])
                        c.dma(dst[b * 128:(b + 1) * 128, :], vo[(b % 2) * 2 + vi][:], r=[vk], w=[dkey])
                pend = []

                def flush_pend():
                    while pend:
                        (fot_, fc_, fk_, i2_, pb_, dst_, dkey_, lastfc) = pend.pop(0)
                        pr = 6 + i2_
                        c.op("pe", lambda e: e.matmul(out=self.ps[pr][:], lhsT=Rm[:], rhs=xb[i2_][:], start=True, stop=True),
                             r=["Rm", "xb%d" % i2_], w=["ps%d" % pr])
                        self.tt("dve", r1[i2_][:], self.ps[pb_][:], cosT[:, pos0:pos0 + 512], ALU.mult, r=["ps%d" % pb_, "cosT"], w=["r1_%d" % i2_])
                        self.tt("dve", r2[i2_][:], self.ps[pr][:], sinT[:, pos0:pos0 + 512], ALU.mult, r=["ps%d" % pr, "sinT"], w=["r2_%d" % i2_])
                        self.tt("pool", fot_[:, fc_, :], r1[i2_][:], r2[i2_][:], ALU.add, r=["r1_%d" % i2_, "r2_%d" % i2_], w=[fk_])
                        if lastfc:
                            c.dma(dst_.rearrange("(c p) t -> p c t", p=128)[:, :, g * 512:(g + 1) * 512], fot_[:], r=[fk_], w=[dkey_])
                for (col0, dst, dkey, kind) in fm_groups:
                    fk = "fo%d" % (cnt % 2)
                    fot = fo[cnt % 2]
                    cnt += 1
                    for fc in range(4):
                        pb = 4 + fc % 2
                        pk = "ps%d" % pb
                        for dc in range(8):
                            c.op("pe", lambda e: e.matmul(out=self.ps[pb][:], lhsT=win[:, dc, col0 + fc * 128:col0 + (fc + 1) * 128],
                                                          rhs=zT[g % 2][:, dc, :], start=(dc == 0), stop=(dc == 7)), r=[zk, "win1"], w=[pk])
                        flush_pend()
                        if kind == "silu":
                            self.actf(fot[:, fc, :], self.ps[pb][:], AF.Silu, r=[pk], w=[fk])
                            if fc == 3:
                                c.dma(dst.rearrange("(c p) t -> p c t", p=128)[:, :, g * 512:(g + 1) * 512], fot[:], r=[fk], w=[dkey])
                        else:
                            i2 = fc % 2
                            self.cp("act", xb[i2][:], self.ps[pb][:], r=[pk], w=["xb%d" % i2])
                            pend.append((fot, fc, fk, i2, pb, dst, dkey, fc == 3))
                flush_pend()

    def phase_attn(self, kind):
        nc, c = self.nc, self.c
        S_ = self.S
        NB = S_ // 128
        NQT = S_ // 512
        dil = (kind == "dil")
        qT_d, kT_d, v_d, o_d = (self.cqT, self.ckT, self.cv_tok, self.oc_tok) if dil else (self.dqT, self.dkT, self.dv_tok, self.od_tok)
        okey = "oc_tok" if dil else "od_tok"
        NH = 8 if dil else 4
        VW = 64 if dil else 128
        nmask = 9 if dil else 4
        lam_init = 0.8 - 0.6 * math.exp(-0.3 * 1)
        with self.scope() as st:
            T = lambda name, shape, dt=F32: st.enter_context(self.sbt(name, shape, dt))
            masks = T("amask", [128, nmask, 512], BF16)
            c.dma(masks[:], self.dil_masks if dil else self.diff_masks, w=["amask"])
            if not dil:
                lqk = T("lqk", [1, 4, 64]); pr = T("lpr", [1, 2, 64]); sm = T("lsm", [1, 2]); nl = T("nl", [1, 1])
                ones1 = T("ones1", [1, 128]); nlam = T("nlam", [128, 1]); gdn = T("gdn", [128, 128])
                c.dma(lqk[:], self.diff_lqk, w=["lqk"])
                c.dma(gdn[:], self.diffnorm_rep, w=["gdn"])
                c.op("dve", lambda e: e.memset(ones1[:], 1.0), w=["ones1"])
                self.tt("dve", pr[:, 0, :], lqk[:, 0, :], lqk[:, 1, :], ALU.mult, r=["lqk"], w=["lpr"])
                self.tt("dve", pr[:, 1, :], lqk[:, 2, :], lqk[:, 3, :], ALU.mult, r=["lqk", "lpr"], w=["lpr"])
                c.op("dve", lambda e: e.reduce_sum(out=sm[:], in_=pr[:], axis=AX.X), r=["lpr"], w=["lsm"])
                self.actf(sm[:], sm[:], AF.Exp, r=["lsm"], w=["lsm"])
                self.tt("dve", nl[:], sm[:, 1:2], sm[:, 0:1], ALU.subtract, r=["lsm"], w=["nl"])
                self.ts("dve", nl[:], nl[:], -lam_init, ALU.add, r=["nl"], w=["nl"])
                c.op("pe", lambda e: e.matmul(out=self.ps[0][:, 0:1], lhsT=ones1[:], rhs=nl[:], start=True, stop=True), r=["ones1", "nl"], w=["ps0"])
                self.cp("dve", nlam[:], self.ps[0][:, 0:1], r=["ps0"], w=["nlam"])
                self.ts("dve", gdn[:], gdn[:], 1.0 - lam_init, ALU.mult, r=["gdn"], w=["gdn"])
            qT_v = qT_d.rearrange("(c p) t -> p c t", p=128)
            kT_v = kT_d.rearrange("(c p) t -> p c t", p=128)
            sets = []
            for q in range(self.nseq):
                tb = q * S_
                qz_ = [T("aqz%d_%d" % (q, i), [128, 4, S_], BF16) for i in range(2)]
                kT_ = T("akT_%d" % q, [128, 4, S_], BF16)
                V1_ = T("aV1_%d" % q, [128, NB, NH, VW + 1], BF16)
                kq, kk, kv = "aqT%d" % q, "akT%d" % q, "aV1%d" % q
                c.op("pool", lambda e: e.memset(qz_[0][64:128, :, :], 0.0), w=[kq])
                c.op("pool", lambda e: e.memset(qz_[1][0:64, :, :], 0.0), w=[kq])
                c.dma(qz_[0][0:64, :, :], qT_v[0:64, :, tb:tb + S_], r=[("cqT" if dil else "dqT")], w=[kq])
                c.dma(qz_[1][64:128, :, :], qT_v[64:128, :, tb:tb + S_], r=[("cqT" if dil else "dqT")], w=[kq])
                c.dma(kT_[:], kT_v[:, :, tb:tb + S_], r=[("ckT" if dil else "dkT")], w=[kk])
                c.op("pool", lambda e: e.memset(V1_[:, :, :, VW:VW + 1], 1.0), w=[kv])
                for blk in range(NB):
                    c.dma(V1_[:, blk, :, 0:VW], v_d[tb + blk * 128:tb + (blk + 1) * 128, :].rearrange("p (h d) -> p h d", h=NH),
                          r=[("cv_tok" if dil else "dv_tok")], w=[kv])
                sets.append((qz_, kT_, V1_, kq, kk, kv))
            for q in range(self.nseq):
                tb = q * S_
                with self.scope() as sq:
                    TQ = lambda name, shape, dt=F32: sq.enter_context(self.sbt(name, shape, dt))
                    qz, kT, V1, kq, kk, kv = sets[q]
                    osb = TQ("aosb", [128, NB, BR], BF16)
                    NEP = 3
                    E = [TQ("aE%d" % i, [128, 512], BF16) for i in range(NEP)]
                    P = [TQ("aP%d" % i, [128, 512], BF16) for i in range(NEP)]
                    rd = [TQ("ard%d" % i, [128, 1]) for i in range(4)]
                    if not dil:
                        o01 = [TQ("ao%d" % i, [128, NB, 128]) for i in range(2)]
                        sqj = TQ("asq", [128, 128]); ssn = TQ("assn", [128, NB]); t3 = TQ("at3", [128, 128])
                    tiles = []
                    for h in range(NH):
                        for m in range(1 if dil else 2):
                            for qt in range(NQT):
                                nkb = 4 * qt + 4
                                for kb in range(nkb):
                                    tiles.append((h, m, qt, kb, kb == nkb - 1))
                    LA = 3
                    NSB = 4

                    def emit_S(i):
                        h, m, qt, kb, _ = tiles[i]
                        ch = h // 2 if dil else h
                        rb = 64 * (h % 2) if dil else 64 * m
                        sb = i % NSB
                        c0 = 128 * max(0, kb - 4 * qt)
                        c.op("pe", lambda e: e.matmul(out=self.ps[sb][:, c0:512], lhsT=kT[:, ch, kb * 128:(kb + 1) * 128],
                                                      rhs=qz[rb // 64][:, ch, qt * 512 + c0:(qt + 1) * 512], start=True, stop=True),
                             r=[kk, kq], w=["ps%d" % sb])
                    for i in range(min(LA, len(tiles))):
                        emit_S(i)
                    for i, (h, m, qt, kb, last) in enumerate(tiles):
                        if i + LA < len(tiles):
                            emit_S(i + LA)
                        sb = i % NSB
                        i2 = i % NEP
                        pS = self.ps[sb]
                        kS = "ps%d" % sb
                        d0 = 4 * qt - kb
                        if dil:
                            mi = 8 if d0 >= 5 else d0 + 3
                        else:
                            mi = d0 + 3 if d0 <= 0 else None
                        c0 = 128 * max(0, kb - 4 * qt)
                        if mi is None:
                            self.actf(P[i2][:, c0:512], pS[:, c0:512], AF.Exp, r=[kS], w=["aP%d" % i2], scale=0.125)
                        else:
                            self.actf(E[i2][:, c0:512], pS[:, c0:512], AF.Exp, r=[kS], w=["aE%d" % i2], scale=0.125)
                            self.tt("dve", P[i2][:, c0:512], E[i2][:, c0:512], masks[:, mi, c0:512], ALU.mult,
                                    r=["aE%d" % i2, "amask"], w=["aP%d" % i2])
                        for j in range(4):
                            Q = 4 * qt + j
                            if kb > Q:
                                continue
                            c.op("pe", lambda e: e.matmul(out=self.ps[4 + j][:, 0:VW + 1], lhsT=P[i2][:, j * 128:(j + 1) * 128],
                                                          rhs=V1[:, kb, h, :], start=(kb == 0), stop=(kb == Q)),
                                 r=["aP%d" % i2, kv], w=["ps%d" % (4 + j)])
                        if last:
                            for j in range(4):
                                c.op("dve", lambda e: e.reciprocal(out=rd[j][:], in_=self.ps[4 + j][:, VW:VW + 1]), r=["ps%d" % (4 + j)], w=["ard%d" % j])
                            for j in range(4):
                                Q = 4 * qt + j
                                pO = self.ps[4 + j]
                                kO = "ps%d" % (4 + j)
                                if dil:
                                    self.ts("dve", osb[:, Q, h * 64:(h + 1) * 64], pO[:, 0:VW], rd[j][:, 0:1], ALU.mult,
                                            r=[kO, "ard%d" % j], w=["aosb%d" % j])
                                else:
                                    self.ts("dve", o01[m][:, Q, :], pO[:, 0:VW], rd[j][:, 0:1], ALU.mult,
                                            r=[kO, "ard%d" % j], w=["ao%d_%d" % (m, j)])
                            if (not dil) and m == 1 and qt == NQT - 1:
                                aok = ["ao%d_%d" % (mm_, jj_) for mm_ in range(2) for jj_ in range(4)]
                                self.stt("dve", o01[0][:], o01[1][:], nlam[:, 0:1], o01[0][:], ALU.mult, ALU.add, r=aok + ["nlam"], w=aok + ["ao0"])
                                for Q in range(NB):
                                    self.actf(sqj[:], o01[0][:, Q, :], AF.Square, r=aok, w=["asq", "assn"], accum_out=ssn[:, Q:Q + 1])
                                self.actf(ssn[:], ssn[:], AF.Ln, r=["assn"], w=["assn"], scale=1.0 / 128, bias=HEAD_EPS)
                                self.actf(ssn[:], ssn[:], AF.Exp, r=["assn"], w=["assn"], scale=-0.5)
                                for Q in range(NB):
                                    self.ts("dve", t3[:], o01[0][:, Q, :], ssn[:, Q:Q + 1], ALU.mult, r=aok + ["assn"], w=["at3"])
                                    self.tt("pool", osb[:, Q, h * 128:(h + 1) * 128], t3[:], gdn[:], ALU.mult, r=["at3", "gdn"], w=["aosb0"])
                    c.dma(o_d[tb:tb + S_, :].rearrange("(b p) f -> p b f", p=128), osb[:], r=["aosb%d" % jj_ for jj_ in range(4)], w=[okey])

    def phase_l1_out(self):
        nc, c = self.nc, self.c
        czT_v = self.czT.rearrange("(c p) t -> p c t", p=128)
        dzT_v = self.dzT.rearrange("(c p) t -> p c t", p=128)

        def provider(st):
            T = lambda name, shape, dt=F32: st.enter_context(self.sbt(name, shape, dt))
            ot = [T("ot%d" % i, [128, 2, BR], BF16) for i in range(4)]
            gT = [T("gT1_%d" % i, [128, 8, 128], BF16) for i in range(2)]
            gz = [T("gz%d" % i, [128, 8, 512], BF16) for i in range(2)]

            def pref(blk):
                i = blk % 4
                i4 = (blk // 4) % 2
                c.dma(ot[i][:, 0, :], self.oc_tok[blk * 128:(blk + 1) * 128, :], r=["oc_tok"], w=["ot%d" % i])
                c.dma(ot[i][:, 1, :], self.od_tok[blk * 128:(blk + 1) * 128, :], r=["od_tok"], w=["ot%d" % i])
                if blk % 4 == 0:
                    c.dma(gz[i4][:, 0:4, :], czT_v[:, :, blk * 128:blk * 128 + 512], r=["czT"], w=["gz%d" % i4])
                    c.dma(gz[i4][:, 4:8, :], dzT_v[:, :, blk * 128:blk * 128 + 512], r=["dzT"], w=["gz%d" % i4])

            def prov(blk):
                i = blk % 2
                i4 = (blk // 4) % 2
                pk = "ps%d" % i
                ptv = self.ps[i][:].bitcast(BF16).rearrange("p (a b) -> p a b", a=8)
                for fc in range(8):
                    c.op("pe", lambda e: e.transpose(out=ptv[:, fc, :], in_=ot[blk % 4][:, fc // 4, (fc % 4) * 128:(fc % 4 + 1) * 128],
                                                     identity=self.ident[:]), r=["ot%d" % (blk % 4), "ident"], w=[pk])
                off = (blk % 4) * 128
                self.tt("dve", gT[i][:], ptv, gz[i4][:, :, off:off + 128], ALU.mult, r=[pk, "gz%d" % i4], w=["gT1_%d" % i])
                return [(gT[i][:, fc, :], "gT1_%d" % i) for fc in range(8)]
            return pref, prov
        self.out_proj_norm_res(1, self.w_out_cd, self.post1_rep, provider, self.h1src, self.out, "h1_%d", "h1_%d")


def core_inputs(inp, x_rows):
    m = {}
    m["x"] = np.ascontiguousarray(x_rows, dtype=np.float32)
    m["ident"] = np.eye(128, dtype=np.float32).astype(ml_dtypes.bfloat16)
    m["pre0"] = np.ascontiguousarray(inp["pre_norm"][0].reshape(8, 128).T)
    m["w_in_ab"] = inp["w_in_ab"][0]
    m["s5_lr"] = inp["s5_lambda_re"][0].T
    m["s5_li"] = inp["s5_lambda_im"][0].T
    m["s5_ldt"] = np.broadcast_to(inp["s5_log_dt"][0][None, :], (64, 32))
    m["s5_br"] = inp["s5_b_re"][0].transpose(1, 0, 2)
    m["s5_bi"] = inp["s5_b_im"][0].transpose(1, 0, 2)
    m["s5_cr"] = inp["s5_c_re"][0].transpose(2, 0, 1)
    m["s5_ci"] = inp["s5_c_im"][0].transpose(2, 0, 1)
    tv = np.array([0, -1, -2, -3, -4, -5, -6, -7, 1, 2, 3, 4, 5, 6, 7, 8], dtype=np.float32)
    m["tv"] = np.broadcast_to(tv[None, :], (64, 16))
    m["kv"] = np.broadcast_to(np.arange(256, dtype=np.float32)[None, :], (64, 256))
    sidx = np.arange(128) // 16
    m["toepmask"] = (sidx[None, :] >= sidx[:, None]).astype(np.float32)
    m["identf"] = np.eye(128, dtype=np.float32)
    m["s5d_rep"] = np.broadcast_to(inp["s5_d"][0][None, :], (128, 512))
    m["glub_rep"] = np.broadcast_to(inp["s5_glu_b"][0][None, :], (128, 512))
    m["glu_w"] = inp["s5_glu_w"][0]
    m["ml_cw"] = inp["ml_conv_w"][0].reshape(4, 4, 128).transpose(2, 1, 0)
    m["ml_cb"] = inp["ml_conv_b"][0].reshape(4, 128).T
    m["ml_mn"] = inp["ml_norm"][0].reshape(4, 128).T
    m["ml_msk"] = inp["ml_skip"][0].reshape(4, 128).T
    m["ml_gbi"] = inp["ml_gate_b"][0][0:4].reshape(4, 1)
    m["ml_gbf"] = inp["ml_gate_b"][0][4:8].reshape(4, 1)
    si = np.arange(128)
    m["ml_maskS"] = ((si[:, None] <= si[None, :]) * (128 ** -0.5)).astype(np.float32)
    m["ml_gw"] = inp["ml_gate_w"][0].reshape(12, 128, 8).transpose(1, 0, 2)
    m["ml_wq"] = inp["ml_wq"][0].transpose(1, 0, 2)
    m["ml_wk"] = inp["ml_wk"][0].transpose(1, 0, 2)
    m["ml_wv"] = inp["ml_wv"][0].transpose(1, 0, 2)
    m["w_out_ab"] = inp["w_out_ab"][0]
    m["pre1"] = np.ascontiguousarray(inp["pre_norm"][1].reshape(8, 128).T)
    m["w_in_cd"] = inp["w_in_cd"][0]
    pos = np.arange(S, dtype=np.float32)
    inv = (10000.0 ** (-np.arange(0, 64, 2, dtype=np.float32) / 64)).astype(np.float32)
    ang = pos[None, :] * inv[np.arange(128) % 32][:, None]
    m["rope_cos"] = np.cos(ang).astype(np.float32)
    m["rope_sin"] = np.sin(ang).astype(np.float32)
    rm = np.zeros((128, 128), dtype=np.float32)
    for mm in range(128):
        if mm % 64 < 32:
            rm[mm + 32, mm] = -1.0
        else:
            rm[mm - 32, mm] = 1.0
    m["rope_rm"] = rm.astype(ml_dtypes.bfloat16)
    kk = np.arange(128)[:, None]
    qq = np.arange(512)[None, :]
    dm = np.zeros((128, 9, 512), dtype=np.float32)
    fm = np.zeros((128, 4, 512), dtype=np.float32)
    for mi in range(9):
        d0 = mi - 3 if mi < 8 else 5
        dl = 128 * d0 + qq - kk
        mult = ((dl >= 0) & (dl <= 128)).astype(np.float32) + ((dl >= 0) & (dl % 4 == 0) & (dl <= 512)) + ((dl >= 0) & (dl % 16 == 0) & (dl <= 2048))
        dm[:, mi, :] = mult
        if mi < 4:
            fm[:, mi, :] = (dl >= 0)
    m["dil_masks"] = dm.astype(ml_dtypes.bfloat16)
    m["diff_masks"] = fm.astype(ml_dtypes.bfloat16)
    m["diff_lqk"] = np.stack([inp["diff_lq1"][0], inp["diff_lk1"][0], inp["diff_lq2"][0], inp["diff_lk2"][0]])[None]
    m["diffnorm_rep"] = np.broadcast_to(inp["diff_norm"][0][None, :], (128, 128))
    m["w_out_cd"] = inp["w_out_cd"][0]
    m["post1_rep"] = np.broadcast_to(inp["post_norm"][1][None, :], (128, 1024))
    m["post0_rep"] = np.broadcast_to(inp["post_norm"][0][None, :], (128, 1024))
    return m


_CACHE = {}


def kernel(**inputs):
    inp = {k_: np.asarray(v) for k_, v in inputs.items()}
    x = inp["x"]
    B = x.shape[0]
    nseq = B // NCORES
    if "prog" not in _CACHE:
        kb = K(nseq=nseq)
        kb.build()
        _CACHE["prog"] = kb
    kb = _CACHE["prog"]
    in_maps = []
    for ci in range(NCORES):
        m = core_inputs(inp, x[ci * nseq:(ci + 1) * nseq].reshape(-1, D))
        in_maps.append({n: np.ascontiguousarray(m[n]) for n in kb.inputs})
    res = run_bass_kernel_spmd(kb.nc, in_maps, core_ids=list(range(NCORES)))
    out = np.stack([np.asarray(res.results[ci]["out"]).reshape(nseq, S, D) for ci in range(NCORES)], axis=0)
    return out.reshape(B, S, D).astype(np.float32)
```

```python
import contextlib
import math
import numpy as np
import ml_dtypes
import concourse.bass as bass
import concourse.mybir as mybir
from concourse.bass_utils import run_bass_kernel_spmd

F32 = mybir.dt.float32
BF16 = mybir.dt.bfloat16
I32 = mybir.dt.int32
AF = mybir.ActivationFunctionType
ALU = mybir.AluOpType
AX = mybir.AxisListType

D = 1024
S = 2048
BR = 512
NCORES = 8
SAME_ENGINE_SYNC = True
NORM_EPS = 1e-6
HEAD_EPS = 1e-5


class Ctx:
    def __init__(self, nc, stack, n_dma_sems=48, same_engine_sync=SAME_ENGINE_SYNC):
        self.nc = nc
        self.eng = {"pe": nc.tensor, "act": nc.scalar, "dve": nc.vector,
                    "pool": nc.gpsimd, "sp": nc.sync}
        self.sem = {}
        self.cnt = {}
        for k in ("pe", "act", "dve", "pool"):
            self.sem[k] = stack.enter_context(nc.semaphore("s_" + k))
            self.cnt[k] = 0
        self.dma_sems = []
        for i in range(n_dma_sems):
            k = "dma%d" % i
            self.sem[k] = stack.enter_context(nc.semaphore("s_" + k))
            self.cnt[k] = 0
            self.dma_sems.append(k)
        self.dma_rr = 0
        self.waited = {k: {} for k in self.eng}
        self.last_w = {}
        self.readers = {}
        self.same_engine_sync = same_engine_sync
        self.n_instr = 0
        self.n_wait = 0

    def _deps(self, r, w):
        deps = []
        for x in r:
            if x in self.last_w:
                deps.append(self.last_w[x])
            if x.startswith("ps"):
                deps.extend(self.readers.get(x, ()))
        for x in w:
            if x in self.last_w:
                deps.append(self.last_w[x])
            deps.extend(self.readers.get(x, ()))
        return deps

    def _wait(self, e, deps):
        need = {}
        for (k, v) in deps:
            if k == e and (e == "pe" or not self.same_engine_sync):
                continue
            if need.get(k, 0) < v:
                need[k] = v
        for k, v in need.items():
            if self.waited[e].get(k, 0) >= v:
                continue
            self.eng[e].wait_ge(self.sem[k], v)
            self.waited[e][k] = v
            self.n_wait += 1

    def _commit(self, tok, r, w):
        for x in w:
            self.last_w[x] = tok
            self.readers[x] = []
        for x in r:
            if x in w:
                continue
            self.readers.setdefault(x, []).append(tok)

    def op(self, e, fn, r=(), w=()):
        self._wait(e, self._deps(r, w))
        ins = fn(self.eng[e])
        self.cnt[e] += 1
        ins.then_inc(self.sem[e], 1)
        self._commit((e, self.cnt[e]), r, w)
        self.n_instr += 1
        return ins

    def dma(self, out, in_, r=(), w=(), q="sp", **kw):
        k = self.dma_sems[self.dma_rr]
        self.dma_rr = (self.dma_rr + 1) % len(self.dma_sems)
        deps = self._deps(r, w)
        if self.cnt[k] > 0:
            deps.append((k, self.cnt[k]))
        self._wait(q, deps)
        ins = self.eng[q].dma_start(out=out, in_=in_, **kw)
        self.cnt[k] += 16
        ins.then_inc(self.sem[k], 16)
        self._commit((k, self.cnt[k]), r, w)
        self.n_instr += 1
        return ins

    def barrier(self):
        deps = [(k, v) for k, v in self.cnt.items() if v > 0]
        for e in self.eng:
            self._wait(e, deps)

    def finish(self, res):
        deps = [self.last_w[x] for x in res if x in self.last_w]
        self._wait("sp", deps)


class K:
    def __init__(self, nseq=2, export=(), phases=None, seqlen=S):
        self.nseq = nseq
        self.S = seqlen
        self.NT = nseq * seqlen
        self.export = set(export)
        self.phases = phases
        self.nc = bass.Bass("TRN2", target_bir_lowering=False)
        self.inputs = {}
        self.outputs = {}
        self.s5_main_enabled = True
        self._uid = 0

    def sbt(self, name, shape, dt):
        self._uid += 1
        return self.nc.sbuf_tensor("%s_u%d" % (name, self._uid), list(shape), dt)

    def din(self, name, shape, dt=F32):
        ap = self.nc.dram_tensor(name, list(shape), dt, kind="ExternalInput").ap()
        self.inputs[name] = ap
        return ap

    def dscr(self, name, shape, dt):
        kind = "ExternalOutput" if name in self.export else "Internal"
        ap = self.nc.dram_tensor(name, list(shape), dt, kind=kind).ap()
        if kind == "ExternalOutput":
            self.outputs[name] = ap
        return ap

    @contextlib.contextmanager
    def scope(self):
        with contextlib.ExitStack() as st:
            yield st
            self.c.barrier()

    def build(self):
        nc = self.nc
        NT = self.NT
        with contextlib.ExitStack() as st:
            self.c = Ctx(nc, st)
            self.ps = [st.enter_context(nc.psum_tensor("ps%d" % i, [128, 512], F32)) for i in range(8)]
            self.x = self.din("x", [NT, D])
            self.ident_d = self.din("ident", [128, 128], BF16)
            self.pre0 = self.din("pre0", [128, 8])
            self.w_in_ab = self.din("w_in_ab", [D, 4 * BR])
            for nm in ("s5_lr", "s5_li", "s5_ldt"):
                setattr(self, nm, self.din(nm, [64, 32]))
            for nm in ("s5_br", "s5_bi", "s5_cr", "s5_ci"):
                setattr(self, nm, self.din(nm, [64, 32, 16]))
            self.tv_d = self.din("tv", [64, 16])
            self.kv_d = self.din("kv", [64, 256])
            self.toepmask_d = self.din("toepmask", [128, 128])
            self.identf_d = self.din("identf", [128, 128])
            self.s5d_rep = self.din("s5d_rep", [128, BR])
            self.glub_rep = self.din("glub_rep", [128, BR])
            self.glu_w = self.din("glu_w", [BR, BR])
            self.ml_cw = self.din("ml_cw", [128, 4, 4]); self.ml_cb = self.din("ml_cb", [128, 4])
            self.ml_mn = self.din("ml_mn", [128, 4]); self.ml_msk = self.din("ml_msk", [128, 4])
            self.ml_gbi = self.din("ml_gbi", [4, 1]); self.ml_gbf = self.din("ml_gbf", [4, 1])
            self.ml_maskS = self.din("ml_maskS", [128, 128])
            self.ml_gw = self.din("ml_gw", [128, 12, 8])
            self.ml_wq = self.din("ml_wq", [128, 4, 128]); self.ml_wk = self.din("ml_wk", [128, 4, 128]); self.ml_wv = self.din("ml_wv", [128, 4, 128])
            self.bT = self.dscr("bT", [BR, NT], BF16)
            self.w_out_ab = self.din("w_out_ab", [D, D]); self.post0_rep = self.din("post0_rep", [128, D])
            self.out = self.nc.dram_tensor("out", [NT, D], F32, kind="ExternalOutput").ap()
            self.outputs["out"] = self.out
            self.h1src = self.out
            if self.phases is not None and "l0o" not in self.phases:
                self.h1src = self.din("h1_in", [NT, D])
            self.pre1 = self.din("pre1", [128, 8]); self.w_in_cd = self.din("w_in_cd", [D, 8 * BR])
            self.rope_cos = self.din("rope_cos", [128, self.S]); self.rope_sin = self.din("rope_sin", [128, self.S])
            self.rope_rm = self.din("rope_rm", [128, 128], BF16)
            for nm in ("cqT", "ckT", "dqT", "dkT", "czT", "dzT"):
                setattr(self, nm, self.dscr(nm, [BR, NT], BF16))
            for nm in ("cv_tok", "dv_tok", "oc_tok", "od_tok"):
                setattr(self, nm, self.dscr(nm, [NT, BR], BF16))
            self.dil_masks = self.din("dil_masks", [128, 9, 512], BF16); self.diff_masks = self.din("diff_masks", [128, 4, 512], BF16)
            self.diff_lqk = self.din("diff_lqk", [1, 4, 64]); self.diffnorm_rep = self.din("diffnorm_rep", [128, 128])
            self.w_out_cd = self.din("w_out_cd", [D, D]); self.post1_rep = self.din("post1_rep", [128, D])
            self.rotc = self.dscr("rotc", [64, 32, 256], F32)
            self.rots = self.dscr("rots", [64, 32, 256], F32)
            self.rhot = self.dscr("rhot", [64, 32, 256], F32)
            self.toep_x = self.dscr("toep_x", [128, 32, 128], BF16)
            self.wii_x = self.dscr("wii_x", [128, 32, 2, 64], BF16)
            self.wiv_x = self.dscr("wiv_x", [64, 2, 32, 128], BF16)
            self.a_tok = self.dscr("a_tok", [NT, BR], BF16)
            self.u_tok = self.dscr("u_tok", [NT, BR], BF16)
            self.sz_tok = self.dscr("sz_tok", [NT, BR], BF16)
            self.xmT = self.dscr("xmT", [BR, NT], BF16)
            self.mzT = self.dscr("mzT", [BR, NT], BF16)
            self.ident = st.enter_context(nc.sbuf_tensor("identb", [128, 128], BF16))
            self.c.dma(self.ident[:], self.ident_d, w=["ident"])
            ph = self.phases
            fin = []
            self.tables_gen_factory = None
            if ph is None or "s5" in ph:
                TP = lambda name, shape, dt=F32: st.enter_context(self.sbt(name, shape, dt))
                self.s5w = (TP("Toep", [128, 32, 128], BF16), TP("Wii", [128, 32, 2, 64], BF16),
                            TP("WivR", [64, 32, 128], BF16), TP("WivI", [64, 32, 128], BF16))
                small = (TP("p_thr", [64, 32]), TP("p_rho8", [64, 32]), TP("p_kv", [64, 256]),
                         TP("p_tfs", [64, 32]), TP("p_tis", [64, 32], I32))
                self.s5_precompute(*self.s5w, small)
                self.tables_gen_factory = lambda T: self.s5_tables_gen(T, small)
                if not (ph is None or "l0p" in ph):
                    with self.scope() as sp_:
                        for _ in self.s5_tables_gen(lambda name, shape, dt=F32: sp_.enter_context(self.sbt(name, shape, dt)), small):
                            pass
            if ph is None or "l0p" in ph:
                self.phase_l0_proj()
                fin += ["u_tok", "sz_tok", "xmT", "mzT"]
            if ph is None or "s5" in ph:
                self.phase_s5()
                fin += ["a_tok", "rotc", "rots", "rhot", "toep_x", "wii_x", "wiv_x"]
            if ph is None or "ml" in ph:
                self.phase_ml()
                fin += ["bT"]
            if ph is None or "l0o" in ph:
                self.phase_l0_out()
                fin += ["h1_%d" % b for b in range(NT // 128)]
            if ph is None or "l1p" in ph:
                self.phase_l1_proj()
                fin += ["cqT", "ckT", "dqT", "dkT", "czT", "dzT", "cv_tok", "dv_tok"]
            if ph is None or "adil" in ph:
                self.phase_attn("dil")
                fin += ["oc_tok"]
            if ph is None or "adiff" in ph:
                self.phase_attn("diff")
                fin += ["od_tok"]
            if ph is None or "l1o" in ph:
                self.phase_l1_out()
                fin += ["h1_%d" % b for b in range(NT // 128)]
            self.c.finish(fin)
            self.c.barrier()
        return nc

    def rmsnorm_T(self, st, xsrc_rows, nblk, zT, zkey, tagp, ps_tr):
        raise NotImplementedError

    def phase_l0_proj(self):
        nc, c = self.nc, self.c
        NT = self.NT
        ngrp = NT // 512
        with self.scope() as st:
            T = lambda name, shape, dt: st.enter_context(self.sbt(name, shape, dt))
            win = T("win0", [128, 8, 4 * BR], BF16)
            g0 = T("g0", [128, 8], F32)
            stage = [T("wst%d" % i, [128, 4 * BR], F32) for i in range(2)]
            c.dma(g0[:], self.pre0, w=["g0"])
            for dc in range(8):
                sk = "wst%d" % (dc % 2)
                c.dma(stage[dc % 2][:], self.w_in_ab[dc * 128:(dc + 1) * 128, :], w=[sk])
                c.op("act", lambda e: e.activation(out=win[:, dc, :], in_=stage[dc % 2][:], func=AF.Copy,
                                                   scale=g0[:, dc:dc + 1]), r=[sk, "g0"], w=["win0"])
            gen = self.tables_gen_factory(lambda name, shape, dt=F32: st.enter_context(self.sbt(name, shape, dt))) if self.tables_gen_factory else None

            def pump(n):
                if gen is not None:
                    for _ in range(n):
                        if next(gen, "done") == "done":
                            break
            NXB = 6
            xt = [T("xt%d" % i, [128, D], F32) for i in range(NXB)]
            junk = T("junk", [128, D], F32)
            ss2 = [T("ss%d" % i, [128, 4], F32) for i in range(2)]
            rstd2 = [T("rstd%d" % i, [128, 4], F32) for i in range(2)]
            zb = [T("zb%d" % i, [128, D], BF16) for i in range(2)]
            zT = [T("zT%d" % i, [128, 8, 512], BF16) for i in range(2)]
            uo = [T("uo%d" % i, [128, BR], BF16) for i in range(2)]
            so = [T("so%d" % i, [128, BR], BF16) for i in range(2)]
            xmo = [T("xmo%d" % i, [128, 4, 512], BF16) for i in range(2)]
            mzo = [T("mzo%d" % i, [128, 4, 512], BF16) for i in range(2)]
            nblk = NT // 128

            def load_x(b):
                if b < nblk:
                    c.dma(xt[b % NXB][:], self.x[b * 128:(b + 1) * 128, :], w=["xt%d" % (b % NXB)])
            for b in range(4):
                load_x(b)
            for g in range(ngrp):
                zk = "zT%d" % (g % 2)
                ss = ss2[g % 2]; rstd = rstd2[g % 2]
                ssk = "ss%d" % (g % 2); rsk = "rstd%d" % (g % 2)
                for j in range(4):
                    b = g * 4 + j
                    xk = "xt%d" % (b % NXB)
                    c.op("act", lambda e: e.activation(out=junk[:], in_=xt[b % NXB][:], func=AF.Square,
                                                       accum_out=ss[:, j:j + 1]), r=[xk], w=["junk", ssk])
                c.op("act", lambda e: e.activation(out=rstd[:], in_=ss[:], func=AF.Ln, scale=1.0 / D, bias=NORM_EPS),
                     r=[ssk], w=[rsk])
                c.op("act", lambda e: e.activation(out=rstd[:], in_=rstd[:], func=AF.Exp, scale=-0.5),
                     r=[rsk], w=[rsk])
                for j in range(4):
                    b = g * 4 + j
                    xk = "xt%d" % (b % NXB)
                    zbk = "zb%d" % (b % 2)
                    pst = self.ps[b % 2]
                    pk = "ps%d" % (b % 2)
                    c.op("dve", lambda e: e.tensor_scalar(out=zb[b % 2][:], in0=xt[b % NXB][:], scalar1=rstd[:, j:j + 1],
                                                          scalar2=None, op0=ALU.mult), r=[xk, rsk], w=[zbk])
                    load_x(b + 4)
                    ptv = pst[:].bitcast(BF16).rearrange("p (a b) -> p a b", a=8)
                    for dc in range(8):
                        c.op("pe", lambda e: e.transpose(out=ptv[:, dc, :], in_=zb[b % 2][:, dc * 128:(dc + 1) * 128],
                                                         identity=self.ident[:]), r=[zbk, "ident"], w=[pk])
                    c.op("dve", lambda e: e.tensor_copy(out=zT[g % 2][:, :, j * 128:(j + 1) * 128], in_=ptv),
                         r=[pk], w=[zk])
                    for dc in range(8):
                        c.op("pe", lambda e: e.matmul(out=self.ps[2][:], lhsT=zT[g % 2][:, dc, j * 128:(j + 1) * 128],
                                                      rhs=win[:, dc, 0:BR], start=(dc == 0), stop=(dc == 7)),
                             r=[zk, "win0"], w=["ps2"])
                    c.op("act", lambda e: e.copy(out=uo[b % 2][:], in_=self.ps[2][:]), r=["ps2"], w=["uo%d" % (b % 2)])
                    c.dma(self.u_tok[b * 128:(b + 1) * 128, :], uo[b % 2][:], r=["uo%d" % (b % 2)], w=["u_tok"])
                    for dc in range(8):
                        c.op("pe", lambda e: e.matmul(out=self.ps[3][:], lhsT=zT[g % 2][:, dc, j * 128:(j + 1) * 128],
                                                      rhs=win[:, dc, BR:2 * BR], start=(dc == 0), stop=(dc == 7)),
                             r=[zk, "win0"], w=["ps3"])
                    c.op("act", lambda e: e.activation(out=so[b % 2][:], in_=self.ps[3][:], func=AF.Silu),
                         r=["ps3"], w=["so%d" % (b % 2)])
                    c.dma(self.sz_tok[b * 128:(b + 1) * 128, :], so[b % 2][:], r=["so%d" % (b % 2)], w=["sz_tok"])
                    pump(6)
                for fc in range(8):
                    pb = 4 + fc % 4
                    for dc in range(8):
                        c.op("pe", lambda e: e.matmul(out=self.ps[pb][:], lhsT=win[:, dc, 2 * BR + fc * 128:2 * BR + (fc + 1) * 128],
                                                      rhs=zT[g % 2][:, dc, :], start=(dc == 0), stop=(dc == 7)),
                             r=[zk, "win0"], w=["ps%d" % pb])
                    if fc < 4:
                        c.op("dve", lambda e: e.tensor_copy(out=xmo[g % 2][:, fc, :], in_=self.ps[pb][:]),
                             r=["ps%d" % pb], w=["xmo%d_%d" % (g % 2, fc)])
                    else:
                        c.op("act", lambda e: e.activation(out=mzo[g % 2][:, fc - 4, :], in_=self.ps[pb][:], func=AF.Silu),
                             r=["ps%d" % pb], w=["mzo%d_%d" % (g % 2, fc - 4)])
                c.dma(self.xmT.rearrange("(c p) t -> p c t", p=128)[:, :, g * 512:(g + 1) * 512], xmo[g % 2][:],
                      r=["xmo%d_%d" % (g % 2, f_) for f_ in range(4)], w=["xmT"])
                c.dma(self.mzT.rearrange("(c p) t -> p c t", p=128)[:, :, g * 512:(g + 1) * 512], mzo[g % 2][:],
                      r=["mzo%d_%d" % (g % 2, f_) for f_ in range(4)], w=["mzT"])
            pump(10 ** 6)
        c.barrier()

    def tt(self, e, out, a, b, op, r, w):
        return self.c.op(e, lambda en: en.tensor_tensor(out=out, in0=a, in1=b, op=op), r=r, w=w)

    def ts(self, e, out, a, s1, op0, r, w, s2=None, op1=None):
        if op1 is None:
            return self.c.op(e, lambda en: en.tensor_scalar(out=out, in0=a, scalar1=s1, scalar2=None, op0=op0), r=r, w=w)
        return self.c.op(e, lambda en: en.tensor_scalar(out=out, in0=a, scalar1=s1, scalar2=s2, op0=op0, op1=op1), r=r, w=w)

    def stt(self, e, out, a, s, b, op0, op1, r, w):
        return self.c.op(e, lambda en: en.scalar_tensor_tensor(out=out, in0=a, scalar=s, in1=b, op0=op0, op1=op1), r=r, w=w)

    def actf(self, out, in_, func, r, w, **kw):
        return self.c.op("act", lambda en: en.activation(out=out, in_=in_, func=func, **kw), r=r, w=w)

    def cp(self, e, out, in_, r, w):
        if e == "act":
            return self.c.op("act", lambda en: en.copy(out=out, in_=in_), r=r, w=w)
        return self.c.op(e, lambda en: en.tensor_copy(out=out, in_=in_), r=r, w=w)

    def sincos(self, *a, **kw):
        for _ in self.sincos_g(*a, **kw):
            pass

    def sincos_g(self, ang, akey, sin_o, cos_o, skey, ckey, tf, ti, red_o=None):
        C1 = 6.28125
        C2 = 2 * math.pi - C1
        for (off, out, okey) in ((0.0, sin_o, skey), (math.pi / 2, cos_o, ckey)):
            if out is None:
                continue
            self.ts("dve", tf, ang, 1.0 / (2 * math.pi), ALU.mult, r=[akey], w=["sc_tf"], s2=off / (2 * math.pi), op1=ALU.add)
            yield
            self.cp("dve", ti, tf, r=["sc_tf"], w=["sc_ti"])
            yield
            self.cp("dve", tf, ti, r=["sc_ti"], w=["sc_tf"])
            yield
            self.stt("dve", out, tf, -C1, ang, ALU.mult, ALU.add, r=["sc_tf", akey], w=[okey])
            yield
            self.stt("dve", out, tf, -C2, out, ALU.mult, ALU.add, r=["sc_tf", okey], w=[okey])
            yield
            if off != 0.0:
                self.ts("dve", out, out, off, ALU.add, r=[okey], w=[okey])
                yield
            self.ts("dve", out, out, math.pi, ALU.min, r=[okey], w=[okey], s2=-math.pi, op1=ALU.max)
            yield
            if red_o is not None and off == 0.0:
                self.cp("dve", red_o[0], out, r=[okey], w=[red_o[1]])
                yield
            self.actf(out, out, AF.Sin, r=[okey], w=[okey])
            yield

    def s5_precompute(self, Toep, Wii, WivR, WivI, small):
        nc, c = self.nc, self.c
        thr, rho8, kv, tfs, tis = small
        with self.scope() as sp:
            T = lambda name, shape, dt=F32: sp.enter_context(self.sbt(name, shape, dt))
            lr = T("p_lr", [64, 32]); li = T("p_li", [64, 32]); ldt = T("p_ldt", [64, 32])
            br = T("p_br", [64, 32, 16]); bi = T("p_bi", [64, 32, 16])
            cr = T("p_cr", [64, 32, 16]); ci = T("p_ci", [64, 32, 16])
            tv = T("p_tv", [64, 16])
            msk = T("p_msk", [128, 128]); idf = T("p_idf", [128, 128])
            for (t, d, key) in ((lr, self.s5_lr, "p_lr"), (li, self.s5_li, "p_li"), (ldt, self.s5_ldt, "p_ldt"),
                                (br, self.s5_br, "p_br"), (bi, self.s5_bi, "p_bi"), (cr, self.s5_cr, "p_cr"),
                                (ci, self.s5_ci, "p_ci"), (tv, self.tv_d, "p_tv"), (kv, self.kv_d, "p_kv"),
                                (msk, self.toepmask_d, "p_msk"), (idf, self.identf_d, "p_idf")):
                c.dma(t[:], d, w=[key])
            dt = T("p_dt", [64, 32]); lrdt = T("p_lrdt", [64, 32]); th = T("p_th", [64, 32])
            s0 = T("p_s0", [64, 32]); c0 = T("p_c0", [64, 32]); mag = T("p_mag", [64, 32])
            self.actf(dt[:], ldt[:], AF.Exp, r=["p_ldt"], w=["p_dt"])
            self.tt("dve", lrdt[:], lr[:], dt[:], ALU.mult, r=["p_lr", "p_dt"], w=["p_lrdt"])
            self.tt("dve", th[:], li[:], dt[:], ALU.mult, r=["p_li", "p_dt"], w=["p_th"])
            self.sincos(th[:], "p_th", s0[:], c0[:], "p_s0", "p_c0", tfs[:], tis[:], red_o=(thr[:], "p_thr"))
            self.actf(mag[:], lrdt[:], AF.Exp, r=["p_lrdt"], w=["p_mag"])
            abr = T("p_abr", [64, 32]); abi = T("p_abi", [64, 32]); am1 = T("p_am1", [64, 32])
            self.tt("dve", abr[:], mag[:], c0[:], ALU.mult, r=["p_mag", "p_c0"], w=["p_abr"])
            self.tt("dve", abi[:], mag[:], s0[:], ALU.mult, r=["p_mag", "p_s0"], w=["p_abi"])
            self.ts("dve", am1[:], abr[:], -1.0, ALU.add, r=["p_abr"], w=["p_am1"])
            den = T("p_den", [64, 32]); t1 = T("p_t1", [64, 32]); t2 = T("p_t2", [64, 32])
            fr = T("p_fr", [64, 32]); fi = T("p_fi", [64, 32])
            self.tt("dve", den[:], lr[:], lr[:], ALU.mult, r=["p_lr"], w=["p_den"])
            self.tt("dve", t1[:], li[:], li[:], ALU.mult, r=["p_li"], w=["p_t1"])
            self.tt("dve", den[:], den[:], t1[:], ALU.add, r=["p_den", "p_t1"], w=["p_den"])
            c.op("dve", lambda e: e.reciprocal(out=den[:], in_=den[:]), r=["p_den"], w=["p_den"])
            self.tt("dve", t1[:], am1[:], lr[:], ALU.mult, r=["p_am1", "p_lr"], w=["p_t1"])
            self.tt("dve", t2[:], abi[:], li[:], ALU.mult, r=["p_abi", "p_li"], w=["p_t2"])
            self.tt("dve", t1[:], t1[:], t2[:], ALU.add, r=["p_t1", "p_t2"], w=["p_t1"])
            self.tt("dve", fr[:], t1[:], den[:], ALU.mult, r=["p_t1", "p_den"], w=["p_fr"])
            self.tt("dve", t1[:], abi[:], lr[:], ALU.mult, r=["p_abi", "p_lr"], w=["p_t1"])
            self.tt("dve", t2[:], am1[:], li[:], ALU.mult, r=["p_am1", "p_li"], w=["p_t2"])
            self.tt("dve", t1[:], t1[:], t2[:], ALU.subtract, r=["p_t1", "p_t2"], w=["p_t1"])
            self.tt("dve", fi[:], t1[:], den[:], ALU.mult, r=["p_t1", "p_den"], w=["p_fi"])
            Bbr = T("p_Bbr", [64, 32, 16]); Bbi = T("p_Bbi", [64, 32, 16])
            u1 = T("p_u1", [64, 32, 16]); u2 = T("p_u2", [64, 32, 16])
            bc16 = lambda a: a.unsqueeze(2).broadcast_to([64, 32, 16])
            self.tt("dve", u1[:], br[:], bc16(fr[:]), ALU.mult, r=["p_br", "p_fr"], w=["p_u1"])
            self.tt("dve", u2[:], bi[:], bc16(fi[:]), ALU.mult, r=["p_bi", "p_fi"], w=["p_u2"])
            self.tt("dve", Bbr[:], u1[:], u2[:], ALU.subtract, r=["p_u1", "p_u2"], w=["p_Bbr"])
            self.tt("dve", u1[:], bi[:], bc16(fr[:]), ALU.mult, r=["p_bi", "p_fr"], w=["p_u1"])
            self.tt("dve", u2[:], br[:], bc16(fi[:]), ALU.mult, r=["p_br", "p_fi"], w=["p_u2"])
            self.tt("dve", Bbi[:], u1[:], u2[:], ALU.add, r=["p_u1", "p_u2"], w=["p_Bbi"])
            TE = T("p_TE", [64, 16, 32]); TA = T("p_TA", [64, 16, 32])
            PWr = T("p_PWr", [64, 16, 32]); PWi = T("p_PWi", [64, 16, 32])
            tf3 = T("p_tf3", [64, 16, 32]); ti3 = T("p_ti3", [64, 16, 32], I32)
            bt = lambda a: a.unsqueeze(1).broadcast_to([64, 16, 32])
            bg = lambda a: a.unsqueeze(2).broadcast_to([64, 16, 32])
            self.tt("dve", TE[:], bt(lrdt[:]), bg(tv[:]), ALU.mult, r=["p_lrdt", "p_tv"], w=["p_TE"])
            self.actf(TE[:], TE[:], AF.Exp, r=["p_TE"], w=["p_TE"])
            self.tt("dve", TA[:], bt(thr[:]), bg(tv[:]), ALU.mult, r=["p_thr", "p_tv"], w=["p_TA"])
            self.sincos(TA[:], "p_TA", PWi[:], PWr[:], "p_PWi", "p_PWr", tf3[:], ti3[:])
            self.tt("dve", PWr[:], PWr[:], TE[:], ALU.mult, r=["p_PWr", "p_TE"], w=["p_PWr"])
            self.tt("dve", PWi[:], PWi[:], TE[:], ALU.mult, r=["p_PWi", "p_TE"], w=["p_PWi"])
            HsR = T("p_HsR", [64, 32, 8, 16]); HsI = T("p_HsI", [64, 32, 8, 16])
            v1 = T("p_v1", [64, 32, 9, 16]); v2 = T("p_v2", [64, 32, 9, 16])
            def pw_b(PW, j0, n):
                return PW[:, j0:j0 + n, :].rearrange("p t g -> p g t").unsqueeze(3).broadcast_to([64, 32, n, 16])
            def x_b(x, n):
                return x.unsqueeze(2).broadcast_to([64, 32, n, 16])
            self.tt("dve", v1[:, :, 0:8, :], pw_b(PWr, 0, 8), x_b(Bbr[:], 8), ALU.mult, r=["p_PWr", "p_Bbr"], w=["p_v1"])
            self.tt("dve", v2[:, :, 0:8, :], pw_b(PWi, 0, 8), x_b(Bbi[:], 8), ALU.mult, r=["p_PWi", "p_Bbi"], w=["p_v2"])
            self.tt("dve", HsR[:], v1[:, :, 0:8, :], v2[:, :, 0:8, :], ALU.subtract, r=["p_v1", "p_v2"], w=["p_HsR"])
            self.tt("dve", v1[:, :, 0:8, :], pw_b(PWr, 0, 8), x_b(Bbi[:], 8), ALU.mult, r=["p_PWr", "p_Bbi"], w=["p_v1"])
            self.tt("dve", v2[:, :, 0:8, :], pw_b(PWi, 0, 8), x_b(Bbr[:], 8), ALU.mult, r=["p_PWi", "p_Bbr"], w=["p_v2"])
            self.tt("dve", HsI[:], v1[:, :, 0:8, :], v2[:, :, 0:8, :], ALU.add, r=["p_v1", "p_v2"], w=["p_HsI"])
            LtR = T("p_LtR", [64, 32, 9, 16]); nLtI = T("p_nLtI", [64, 32, 9, 16])
            for (s0_, j0, n) in ((0, 0, 1), (1, 8, 8)):
                sl = slice(s0_, s0_ + n)
                self.tt("dve", v1[:, :, sl, :], pw_b(PWr, j0, n), x_b(cr[:], n), ALU.mult, r=["p_PWr", "p_cr"], w=["p_v1"])
                self.tt("dve", v2[:, :, sl, :], pw_b(PWi, j0, n), x_b(ci[:], n), ALU.mult, r=["p_PWi", "p_ci"], w=["p_v2"])
                self.tt("dve", LtR[:, :, sl, :], v1[:, :, sl, :], v2[:, :, sl, :], ALU.subtract, r=["p_v1", "p_v2"], w=["p_LtR"])
                self.tt("dve", v1[:, :, sl, :], pw_b(PWi, j0, n), x_b(cr[:], n), ALU.mult, r=["p_PWi", "p_cr"], w=["p_v1"])
                self.tt("dve", v2[:, :, sl, :], pw_b(PWr, j0, n), x_b(ci[:], n), ALU.mult, r=["p_PWr", "p_ci"], w=["p_v2"])
                self.tt("dve", v1[:, :, sl, :], v1[:, :, sl, :], v2[:, :, sl, :], ALU.add, r=["p_v1", "p_v2"], w=["p_v1"])
                self.ts("dve", nLtI[:, :, sl, :], v1[:, :, sl, :], -1.0, ALU.mult, r=["p_v1"], w=["p_nLtI"])
            self.cp("dve", WivR[:], LtR[:, :, 1:9, :].rearrange("p g t c -> p g (t c)"), r=["p_LtR"], w=["WivR"])
            self.cp("dve", WivI[:], nLtI[:, :, 1:9, :].rearrange("p g t c -> p g (t c)"), r=["p_nLtI"], w=["WivI"])
            for g4 in range(8):
                pb = g4 % 2
                pk = "ps%d" % pb
                for gl in range(4):
                    g = g4 * 4 + gl
                    o = self.ps[pb][:, gl * 128:(gl + 1) * 128]
                    c.op("pe", lambda e: e.matmul(out=o, lhsT=HsR[:, g, :, :].rearrange("p s c -> p (s c)"),
                                                  rhs=LtR[:, g, 0:8, :].rearrange("p t c -> p (t c)"), start=True, stop=False),
                         r=["p_HsR", "p_LtR"], w=[pk])
                    c.op("pe", lambda e: e.matmul(out=o, lhsT=HsI[:, g, :, :].rearrange("p s c -> p (s c)"),
                                                  rhs=nLtI[:, g, 0:8, :].rearrange("p t c -> p (t c)"), start=False, stop=True),
                         r=["p_HsI", "p_nLtI"], w=[pk])
                self.tt("dve", Toep[:, g4 * 4:(g4 + 1) * 4, :], self.ps[pb][:].rearrange("p (g n) -> p g n", g=4),
                        msk[:].unsqueeze(1).broadcast_to([128, 4, 128]), ALU.mult, r=[pk, "p_msk"], w=["Toep"])
            GsR = LtR[:, :, 0:8, :].rearrange("p g t c -> p g (t c)")
            GsI = nLtI[:, :, 0:8, :].rearrange("p g t c -> p g (t c)")
            w1 = v1[:, :, 0:8, :].rearrange("p g t c -> p g (t c)")
            w2 = v2[:, :, 0:8, :].rearrange("p g t c -> p g (t c)")
            p7r = PWr[:, 14, :].unsqueeze(2).broadcast_to([64, 32, 128])
            p7i = PWi[:, 14, :].unsqueeze(2).broadcast_to([64, 32, 128])
            hr = HsR[:].rearrange("p g s c -> p g (s c)"); hi = HsI[:].rearrange("p g s c -> p g (s c)")
            self.tt("dve", w1, hr, p7r, ALU.mult, r=["p_HsR", "p_PWr"], w=["p_v1"])
            self.tt("dve", w2, hi, p7i, ALU.mult, r=["p_HsI", "p_PWi"], w=["p_v2"])
            self.tt("dve", GsR, w1, w2, ALU.subtract, r=["p_v1", "p_v2"], w=["p_LtR"])
            self.tt("dve", w1, hi, p7r, ALU.mult, r=["p_HsI", "p_PWr"], w=["p_v1"])
            self.tt("dve", w2, hr, p7i, ALU.mult, r=["p_HsR", "p_PWi"], w=["p_v2"])
            self.tt("dve", GsI, w1, w2, ALU.add, r=["p_v1", "p_v2"], w=["p_nLtI"])
            for g4 in range(8):
                pb = 2 + g4 % 2
                pk = "ps%d" % pb
                pv = self.ps[pb][:].rearrange("p (g r n) -> p g r n", g=4, r=2)
                for gl in range(4):
                    g = g4 * 4 + gl
                    c.op("pe", lambda e: e.transpose(out=pv[:, gl, 0, :], in_=GsR[:, g, :], identity=idf[0:64, 0:64]),
                         r=["p_LtR", "p_idf"], w=[pk])
                    c.op("pe", lambda e: e.transpose(out=pv[:, gl, 1, :], in_=GsI[:, g, :], identity=idf[0:64, 0:64]),
                         r=["p_nLtI", "p_idf"], w=[pk])
                self.cp("act", Wii[:, g4 * 4:(g4 + 1) * 4, :, :], pv, r=[pk], w=["Wii"])
            self.cp("dve", rho8[:], TE[:, 15, :], r=["p_TE"], w=["p_rho8"])
            if "toep_x" in self.export:
                c.dma(self.toep_x, Toep[:], r=["Toep"], w=["toep_x"])
                c.dma(self.wii_x, Wii[:], r=["Wii"], w=["wii_x"])
                c.dma(self.wiv_x[:, 0], WivR[:], r=["WivR"], w=["wiv_x"])
                c.dma(self.wiv_x[:, 1], WivI[:], r=["WivI"], w=["wiv_x"])
        c.barrier()

    def s5_tables_gen(self, T, small):
        nc, c = self.nc, self.c
        thr, rho8, kv, tfs, tis = small
        NG = 4
        phr = T("p_phr", [64, 32]); ph_s = T("p_phs", [64, 32]); phr2 = T("p_phr2", [64, 32])
        rho = T("p_rho", [64, NG, 256])
        ang = T("p_ang", [64, NG, 256]); sk = T("p_sk", [64, NG, 256]); ck = T("p_ck", [64, NG, 256])
        tf4 = T("p_tf4", [64, NG, 256]); ti4 = T("p_ti4", [64, NG, 256], I32)
        self.ts("dve", phr[:], thr[:], 8.0, ALU.mult, r=["p_thr"], w=["p_phr"])
        yield
        yield from self.sincos_g(phr[:], "p_phr", ph_s[:], None, "p_phs", None, tfs[:], tis[:], red_o=(phr2[:], "p_phr2"))
        for gb in range(32 // NG):
            gs = slice(gb * NG, (gb + 1) * NG)
            self.cp("dve", rho[:], rho8[:, gs].unsqueeze(2).broadcast_to([64, NG, 256]), r=["p_rho8"], w=["p_rho"])
            yield
            c.op("dve", lambda e: e.memset(rho[:, :, 0:1], 0.0), r=[], w=["p_rho"])
            c.dma(self.rhot[:, gs, :], rho[:], r=["p_rho"], w=["rhot"])
            yield
            self.tt("dve", ang[:], phr2[:, gs].unsqueeze(2).broadcast_to([64, NG, 256]),
                    kv[:].unsqueeze(1).broadcast_to([64, NG, 256]), ALU.mult, r=["p_phr2", "p_kv"], w=["p_ang"])
            yield
            yield from self.sincos_g(ang[:], "p_ang", sk[:], ck[:], "p_sk", "p_ck", tf4[:], ti4[:])
            c.dma(self.rots[:, gs, :], sk[:], r=["p_sk"], w=["rots"])
            c.dma(self.rotc[:, gs, :], ck[:], r=["p_ck"], w=["rotc"])
            yield

    def phase_s5(self):
        Toep, Wii, WivR, WivI = self.s5w
        if self.s5_main_enabled:
            self.s5_main(None, Toep, Wii, WivR, WivI)
        self.c.barrier()

    def s5_main(self, st0, Toep, Wii, WivR, WivI):
        nc, c = self.nc, self.c
        GB = 4
        with self.scope() as st:
            T = lambda name, shape, dt=F32: st.enter_context(self.sbt(name, shape, dt))
            gluw = T("gluw", [128, 4, BR], BF16)
            with self.scope() as sg:
                gst = sg.enter_context(self.sbt("gluw_st", [128, 4, BR], F32))
                c.dma(gst[:], self.glu_w.rearrange("(c p) n -> p c n", p=128), w=["gluw_st"])
                self.cp("dve", gluw[:], gst[:], r=["gluw_st"], w=["gluw"])
            Drep = T("Drep", [128, BR]); Brep = T("Brep", [128, BR])
            c.dma(Drep[:], self.s5d_rep, w=["Drep"])
            c.dma(Brep[:], self.glub_rep, w=["Brep"])
            for q in range(self.nseq):
                tb = q * self.S
                with self.scope() as sq:
                    TQ = lambda name, shape, dt=F32: sq.enter_context(self.sbt(name, shape, dt))
                    uck = [TQ("uck%d" % i, [128, 8 * BR], BF16) for i in range(2)]
                    szck = [TQ("szck%d" % i, [128, 8 * BR], BF16) for i in range(2)]
                    yck = [TQ("yck%d" % i, [128, 8, BR], BF16) for i in range(2)]
                    for kh in range(2):
                        rows = slice(tb + kh * 1024, tb + (kh + 1) * 1024)
                        c.dma(uck[kh][:], self.u_tok[rows, :].rearrange("(k t) f -> k (t f)", t=8), r=["u_tok"], w=["uck%d" % kh])
                        c.dma(szck[kh][:], self.sz_tok[rows, :].rearrange("(k t) f -> k (t f)", t=8), r=["sz_tok"], w=["szck%d" % kh])
                    with self.scope() as ss:
                        TS = lambda name, shape, dt=F32: ss.enter_context(self.sbt(name, shape, dt))
                        Ug = TS("Ug", [128, 32, 256], BF16)
                        Vb2 = [TS("Vb%d" % i, [64, 2, GB, 256]) for i in range(2)]
                        Wb2 = [TS("Wb%d" % i, [64, 2, GB, 256]) for i in range(2)]
                        tA2 = [TS("tA%d" % i, [64, GB, 256]) for i in range(2)]; tB2 = [TS("tB%d" % i, [64, GB, 256]) for i in range(2)]
                        tC2 = [TS("tC0", [64, GB, 256])] * 2; tD2 = [TS("tD0", [64, GB, 256])] * 2
                        ck2 = [TS("ck%d" % i, [64, GB, 256]) for i in range(2)]; sk2 = [TS("sk%d" % i, [64, GB, 256]) for i in range(2)]
                        rh2 = [TS("rh%d" % i, [64, GB, 256]) for i in range(2)]
                        Xs2 = [TS("Xs%d" % i, [64, 2, GB, 256], BF16) for i in range(2)]
                        Ysb = TS("Ysb", [128, 8, 256], BF16)
                        for i in range(2):
                            c.op("pool", lambda e: e.memset(Xs2[i][:], 0.0), w=["Xs%d" % i])

                        def load_tabs(b):
                            if b < 32 // GB:
                                gs_ = slice(b * GB, (b + 1) * GB)
                                c.dma(ck2[b % 2][:], self.rotc[:, gs_, :], r=["rotc"], w=["ck%d" % (b % 2)])
                                c.dma(sk2[b % 2][:], self.rots[:, gs_, :], r=["rots"], w=["sk%d" % (b % 2)])
                                c.dma(rh2[b % 2][:], self.rhot[:, gs_, :], r=["rhot"], w=["rh%d" % (b % 2)])
                        load_tabs(0)
                        ucg = TS("ucg", [128, 32, 128], BF16)
                        for kh in range(2):
                            self.cp("pool" if kh == 0 else "dve", ucg[:].rearrange("p g (s c) -> p g s c", s=8),
                                    uck[kh][:].rearrange("p (s g c) -> p g s c", s=8, g=32), r=["uck%d" % kh], w=["ucg"])
                            for g8 in range(4):
                                pb = g8 % 2
                                pk = "ps%d" % pb
                                ptv = self.ps[pb][:].bitcast(BF16).rearrange("p (g k) -> p g k", g=8)
                                for gl in range(8):
                                    g = g8 * 8 + gl
                                    c.op("pe", lambda e: e.transpose(out=ptv[:, gl, :], in_=ucg[:, g, :],
                                                                     identity=self.ident[:]), r=["ucg", "ident"], w=[pk])
                                self.cp("dve" if g8 % 2 == 0 else "act", Ug[:, g8 * 8:(g8 + 1) * 8, kh * 128:(kh + 1) * 128], ptv,
                                        r=[pk], w=["Ug"])
                        for b in range(32 // GB):
                            gs = slice(b * GB, (b + 1) * GB)
                            bp = b % 2
                            Vb, Wb, tA, tB, ck, sk, rh, Xs = Vb2[bp], Wb2[bp], tA2[bp], tB2[bp], ck2[bp], sk2[bp], rh2[bp], Xs2[bp]
                            tC, tD = tC2[bp], tD2[bp]
                            ktC, ktD = "tC0", "tD0"
                            kVb, kW0, kW1, ktA, ktB, kck, ksk, krh, kXs = ("Vb%d" % bp, "Wb0_%d" % bp, "Wb1_%d" % bp, "tA%d" % bp, "tB%d" % bp,
                                                                           "ck%d" % bp, "sk%d" % bp, "rh%d" % bp, "Xs%d" % bp)
                            load_tabs(b + 1)
                            for gl in range(GB):
                                g = b * GB + gl
                                pb = 2 + gl % 2
                                pk = "ps%d" % pb
                                c.op("pe", lambda e: e.matmul(out=self.ps[pb][0:64, 0:256], lhsT=Wii[:, g, 0, :], rhs=Ug[:, g, :],
                                                              start=True, stop=True), r=["Wii", "Ug"], w=[pk])
                                c.op("pe", lambda e: e.matmul(out=self.ps[pb][0:64, 256:512], lhsT=Wii[:, g, 1, :], rhs=Ug[:, g, :],
                                                              start=True, stop=True), r=["Wii", "Ug"], w=[pk])
                                self.cp("act", Vb[:, :, gl, :], self.ps[pb][0:64, :].rearrange("p (r k) -> p r k", r=2), r=[pk], w=[kVb + "_g%d" % gl])
                            self.tt("pool", tB[:], sk[:], Vb[:, 1], ALU.mult, r=[ksk, kVb] + [kVb + "_g%d" % g_ for g_ in range(GB)], w=[ktB])
                            self.tt("dve", tA[:], ck[:], Vb[:, 0], ALU.mult, r=[kck, kVb] + [kVb + "_g%d" % g_ for g_ in range(GB)], w=[ktA])
                            self.tt("dve", tC[:], ck[:], Vb[:, 1], ALU.mult, r=[kck, kVb] + [kVb + "_g%d" % g_ for g_ in range(GB)], w=[ktC])
                            self.tt("dve", tD[:], sk[:], Vb[:, 0], ALU.mult, r=[ksk, kVb] + [kVb + "_g%d" % g_ for g_ in range(GB)], w=[ktD])
                            self.tt("dve", Wb[:, 1], tC[:], tD[:], ALU.subtract, r=[ktC, ktD], w=[kW1])
                            self.tt("dve", Wb[:, 0], tA[:], tB[:], ALU.add, r=[ktA, ktB], w=[kW0])
                            fl = lambda a: a.rearrange("p g k -> p (g k)")
                            c.op("dve", lambda e: e.tensor_tensor_scan(out=fl(Vb[:, 1]), data0=fl(rh[:]), data1=fl(Wb[:, 1]), initial=0.0,
                                                                       op0=ALU.mult, op1=ALU.add), r=[krh, kW1, kVb], w=[kVb] + [kVb + "_g%d" % g_ for g_ in range(GB)])
                            c.op("dve", lambda e: e.tensor_tensor_scan(out=fl(Vb[:, 0]), data0=fl(rh[:]), data1=fl(Wb[:, 0]), initial=0.0,
                                                                       op0=ALU.mult, op1=ALU.add), r=[krh, kW0, kVb], w=[kVb] + [kVb + "_g%d" % g_ for g_ in range(GB)])
                            K1 = 255
                            self.tt("pool", tB[:, :, 0:K1], sk[:, :, 0:K1], Vb[:, 1, :, 0:K1], ALU.mult, r=[ksk, kVb], w=[ktB])
                            self.tt("dve", tA[:, :, 0:K1], ck[:, :, 0:K1], Vb[:, 0, :, 0:K1], ALU.mult, r=[kck, kVb], w=[ktA])
                            self.tt("dve", tC[:, :, 0:K1], ck[:, :, 0:K1], Vb[:, 1, :, 0:K1], ALU.mult, r=[kck, kVb], w=[ktC])
                            self.tt("dve", tD[:, :, 0:K1], sk[:, :, 0:K1], Vb[:, 0, :, 0:K1], ALU.mult, r=[ksk, kVb], w=[ktD])
                            self.tt("dve", Xs[:, 1, :, 1:256], tC[:, :, 0:K1], tD[:, :, 0:K1], ALU.add, r=[ktC, ktD], w=[kXs])
                            self.tt("dve", Xs[:, 0, :, 1:256], tA[:, :, 0:K1], tB[:, :, 0:K1], ALU.subtract, r=[ktA, ktB], w=[kXs])
                            for gl in range(GB):
                                g = b * GB + gl
                                pb = 4 + gl // 2 % 2
                                pk = "ps%d" % pb
                                o = self.ps[pb][:, (gl % 2) * 256:(gl % 2 + 1) * 256]
                                c.op("pe", lambda e: e.matmul(out=o, lhsT=Toep[:, g, :], rhs=Ug[:, g, :], start=True, stop=False),
                                     r=["Toep", "Ug"], w=[pk])
                                c.op("pe", lambda e: e.matmul(out=o, lhsT=WivR[:, g, :], rhs=Xs[:, 0, gl, :], start=False, stop=False),
                                     r=["WivR", kXs], w=[pk])
                                c.op("pe", lambda e: e.matmul(out=o, lhsT=WivI[:, g, :], rhs=Xs[:, 1, gl, :], start=False, stop=True),
                                     r=["WivI", kXs], w=[pk])
                                if gl % 2 == 1:
                                    g8l = (b * GB + gl - 1) % 8
                                    self.cp("act", Ysb[:, g8l:g8l + 2, :], self.ps[pb][:].rearrange("p (g k) -> p g k", g=2), r=[pk], w=["Ysb"])
                            if (b * GB + GB) % 8 == 0:
                                g8 = (b * GB) // 8
                                for kh in range(2):
                                    pb = 6 + kh
                                    pk = "ps%d" % pb
                                    ptv = self.ps[pb][:].bitcast(BF16).rearrange("p (g n) -> p g n", g=8)
                                    for gl in range(8):
                                        c.op("pe", lambda e: e.transpose(out=ptv[:, gl, :], in_=Ysb[:, gl, kh * 128:(kh + 1) * 128],
                                                                         identity=self.ident[:]), r=["Ysb", "ident"], w=[pk])
                                    self.cp("dve", yck[kh][:, :, g8 * 128:(g8 + 1) * 128].rearrange("p t (g c) -> p t g c", g=8),
                                            ptv.rearrange("p g (t c) -> p t g c", t=8), r=[pk], w=["yck%d" % kh])
                    with self.scope() as se:
                        TE_ = lambda name, shape, dt=F32: se.enter_context(self.sbt(name, shape, dt))
                        t1 = TE_("e_t1", [128, 8, BR]); t2 = TE_("e_t2", [128, 8, BR])
                        gck = TE_("gck", [128, 8, BR], BF16)
                        ack = TE_("ack", [128, 8, BR], BF16)
                        gT = [TE_("gT%d" % i, [128, 4, 128], BF16) for i in range(2)]
                        e1 = [TE_("e1_%d" % i, [128, BR]) for i in range(2)]
                        for kh in range(2):
                            uv = uck[kh][:].rearrange("p (s f) -> p s f", s=8)
                            zv = szck[kh][:].rearrange("p (s f) -> p s f", s=8)
                            self.tt("dve", t1[:], uv, Drep[:].unsqueeze(1).broadcast_to([128, 8, BR]), ALU.mult, r=["uck%d" % kh, "Drep"], w=["e_t1"])
                            self.tt("dve", t1[:], t1[:], yck[kh][:], ALU.add, r=["e_t1", "yck%d" % kh], w=["e_t1"])
                            self.actf(t2[:], t1[:], AF.Square, r=["e_t1"], w=["e_t2"])
                            self.actf(t2[:], t2[:], AF.Copy, r=["e_t2"], w=["e_t2"], scale=0.044715 * 0.7978845608, bias=0.7978845608)
                            self.tt("dve", t2[:], t2[:], t1[:], ALU.mult, r=["e_t2", "e_t1"], w=["e_t2"])
                            self.actf(t2[:], t2[:], AF.Tanh, r=["e_t2"], w=["e_t2"])
                            self.actf(t2[:], t2[:], AF.Copy, r=["e_t2"], w=["e_t2"], scale=0.5, bias=0.5)
                            self.tt("dve", gck[:], t2[:], t1[:], ALU.mult, r=["e_t2", "e_t1"], w=["gck"])
                            def glu_front(tau):
                                i2 = tau % 2
                                pk = "ps%d" % i2
                                ptv = self.ps[i2][:].bitcast(BF16).rearrange("p (a b) -> p a b", a=8)
                                for fc in range(4):
                                    c.op("pe", lambda e: e.transpose(out=ptv[:, fc, :], in_=gck[:, tau, fc * 128:(fc + 1) * 128],
                                                                     identity=self.ident[:]), r=["gck", "ident"], w=[pk])
                                self.cp("act", gT[i2][:], ptv[:, 0:4, :], r=[pk], w=["gT%d" % i2])
                            glu_front(0)
                            for tau in range(8):
                                i2 = tau % 2
                                pm = 2 + i2
                                for fc in range(4):
                                    c.op("pe", lambda e: e.matmul(out=self.ps[pm][:], lhsT=gT[i2][:, fc, :], rhs=gluw[:, fc, :],
                                                                  start=(fc == 0), stop=(fc == 3)), r=["gT%d" % i2, "gluw"], w=["ps%d" % pm])
                                if tau + 1 < 8:
                                    glu_front(tau + 1)
                                ek = "e1_%d" % i2
                                self.tt("dve", e1[i2][:], self.ps[pm][:], Brep[:], ALU.add, r=["ps%d" % pm, "Brep"], w=[ek])
                                self.actf(e1[i2][:], e1[i2][:], AF.Tanh, r=[ek], w=[ek], scale=0.5)
                                self.ts("dve", e1[i2][:], e1[i2][:], 0.5, ALU.mult, r=[ek], w=[ek], s2=0.5, op1=ALU.add)
                                self.tt("pool", e1[i2][:], e1[i2][:], gck[:, tau, :], ALU.mult, r=[ek, "gck"], w=[ek])
                                self.tt("dve", ack[:, tau, :], e1[i2][:], zv[:, tau, :], ALU.mult, r=[ek, "szck%d" % kh], w=["ack"])
                            rows = slice(tb + kh * 1024, tb + (kh + 1) * 1024)
                            c.dma(self.a_tok[rows, :].rearrange("(k t) f -> k (t f)", t=8), ack[:].rearrange("p t f -> p (t f)"),
                                  r=["ack"], w=["a_tok"])

    def phase_ml(self):
        nc, c = self.nc, self.c
        S_ = self.S
        NB = S_ // 128
        SC = 128 ** -0.5
        with self.scope() as st:
            T = lambda name, shape, dt=F32: st.enter_context(self.sbt(name, shape, dt))
            cw = T("m_cw", [128, 4, 4]); cb = T("m_cb", [128, 4])
            mn = T("m_mn", [128, 4]); msk = T("m_msk", [128, 4])
            gbi = T("m_gbi", [4, 1]); gbf = T("m_gbf", [4, 1]); ngbf = T("m_ngbf", [4, 1])
            maskS = T("m_maskS", [128, 128]); idf = T("m_idf", [128, 128])
            ones4 = T("m_ones4", [4, 128])
            wst = T("m_wst", [128, 3, 4, 128]); wqkv = T("m_wqkv", [128, 3, 4, 128], BF16)
            gst = T("m_gst", [128, 12, 8]); gw = T("m_gw", [128, 12, 8], BF16)
            for (t, d, key) in ((cw, self.ml_cw, "m_cw"), (cb, self.ml_cb, "m_cb"), (mn, self.ml_mn, "m_mn"),
                                (msk, self.ml_msk, "m_msk"), (gbi, self.ml_gbi, "m_gbi"), (gbf, self.ml_gbf, "m_gbf"),
                                (maskS, self.ml_maskS, "m_maskS"), (idf, self.identf_d, "m_idf"),
                                (gst, self.ml_gw, "m_gst")):
                c.dma(t[:], d, w=[key])
            for i, d in enumerate((self.ml_wq, self.ml_wk, self.ml_wv)):
                c.dma(wst[:, i], d, w=["m_wst"])
            self.cp("dve", wqkv[:], wst[:], r=["m_wst"], w=["m_wqkv"])
            self.cp("dve", gw[:], gst[:], r=["m_gst"], w=["m_gw"])
            self.ts("dve", ngbf[:], gbf[:], -1.0, ALU.mult, r=["m_gbf"], w=["m_ngbf"])
            c.op("dve", lambda e: e.memset(ones4[:], 1.0), w=["m_ones4"])
            xmT_v = self.xmT.rearrange("(c p) t -> p c t", p=128)
            mzT_v = self.mzT.rearrange("(c p) t -> p c t", p=128)
            bT_v = self.bT.rearrange("(c p) t -> p c t", p=128)
            for q in range(self.nseq):
                tb = q * S_
                with self.scope() as sq:
                    TQ = lambda name, shape, dt=F32: sq.enter_context(self.sbt(name, shape, dt))
                    xcT = TQ("xcT", [128, 4, S_], BF16)
                    qT = TQ("qT", [128, 4, S_], BF16)
                    kT = TQ("kT", [128, 4, S_], BF16)
                    Ktok = TQ("Ktok", [128, NB, 4, 128], BF16)
                    Vtok = TQ("Vtok", [128, NB, 4, 129], BF16)
                    acol = TQ("acol", [128, NB, 4]); bcol = TQ("bcol", [128, NB, 4])
                    Rrep = TQ("Rrep", [128, NB + 1, 4])
                    Wt = TQ("Wt", [128, NB, 4]); Wp = TQ("Wp", [128, NB, 4])
                    Thr = TQ("Thr", [128, NB, 4]); Dec = TQ("Dec", [128, NB, 4])
                    c.op("pool", lambda e: e.memset(Vtok[:, :, :, 128:129], 1.0), w=["Vtok"])
                    with self.scope() as sa:
                        TA_ = lambda name, shape, dt=F32: sa.enter_context(self.sbt(name, shape, dt))
                        xm = TA_("xm", [128, 4, S_], BF16)
                        vT = TA_("vT", [128, 4, S_], BF16)
                        acc = TA_("acc", [128, S_])
                        g1f = TA_("g1", [32, S_]); g2f = TA_("g2", [32, S_]); g3f = TA_("g3", [32, S_]); onesr = TA_("onesr", [32, S_])
                        g1 = g1f[0:4, :]; g2 = g2f[0:4, :]; g3 = g3f[0:4, :]
                        c.op("pool", lambda e: e.memset(g1f[:], 0.0), w=["g1"])
                        c.op("pool", lambda e: e.memset(g2f[:], 0.0), w=["g2"])
                        rsel = TA_("rsel", [4, NB, 4])
                        c.dma(xm[:], xmT_v[:, :, tb:tb + S_], r=["xmT"], w=["xm"])
                        c.op("pool", lambda e: e.memset(onesr[:], 1.0), w=["onesr"])
                        for fc in range(4):
                            self.ts("dve", acc[:], xm[:, fc, :], cw[:, fc, 3:4], ALU.mult, r=["xm", "m_cw", "m_cb"], w=["acc"],
                                    s2=cb[:, fc:fc + 1], op1=ALU.add)
                            for sh in (1, 2, 3):
                                self.stt("dve", acc[:, sh:], xm[:, fc, 0:S_ - sh], cw[:, fc, 3 - sh:4 - sh], acc[:, sh:],
                                         ALU.mult, ALU.add, r=["xm", "m_cw", "acc"], w=["acc"])
                            self.actf(xcT[:, fc, :], acc[:], AF.Silu, r=["acc"], w=["xcT"])
                        for h in range(4):
                            for tl in range(S_ // 512):
                                ts_ = slice(tl * 512, (tl + 1) * 512)
                                for (i, src, skey, dst, dkey) in ((0, xcT, "xcT", qT, "qT"), (1, xcT, "xcT", kT, "kT"), (2, xm, "xm", vT, "vT")):
                                    pb = (h * 12 + tl * 3 + i) % 4
                                    pk = "ps%d" % pb
                                    c.op("pe", lambda e: e.matmul(out=self.ps[pb][:], lhsT=wqkv[:, i, h, :], rhs=src[:, h, ts_],
                                                                  start=True, stop=True), r=["m_wqkv", skey], w=[pk])
                                    self.cp("act" if i != 1 else "dve", dst[:, h, ts_], self.ps[pb][:], r=[pk], w=[dkey])
                        for blk in range(NB):
                            bs = slice(blk * 128, (blk + 1) * 128)
                            for (i, src, skey, dst, dkey, pb) in ((1, xcT, "xcT", Ktok, "Ktok", 4), (2, xm, "xm", Vtok, "Vtok", 5)):
                                pb = pb + 2 * (blk % 2)
                                pk = "ps%d" % pb
                                for h in range(4):
                                    c.op("pe", lambda e: e.matmul(out=self.ps[pb][:, h * 128:(h + 1) * 128], lhsT=src[:, h, bs],
                                                                  rhs=wqkv[:, i, h, :], start=True, stop=True), r=["m_wqkv", skey], w=[pk])
                                self.cp("act" if i == 1 else "dve", dst[:, blk, :, 0:128], self.ps[pb][:].rearrange("p (h e) -> p h e", h=4),
                                        r=[pk], w=[dkey])
                        for tl in range(S_ // 512):
                            ts_ = slice(tl * 512, (tl + 1) * 512)
                            for half in range(2):
                                pb = half
                                pk = "ps%d" % pb
                                for ch in range(12):
                                    src = (qT, kT, vT)[ch // 4]
                                    skey = ("qT", "kT", "vT")[ch // 4]
                                    c.op("pe", lambda e: e.matmul(out=self.ps[pb][0:4, :], lhsT=gw[:, ch, half * 4:half * 4 + 4],
                                                                  rhs=src[:, ch % 4, ts_], start=(ch == 0), stop=(ch == 11)),
                                         r=["m_gw", skey], w=[pk])
                                if half == 0:
                                    self.ts("dve", g1[:, ts_], self.ps[pb][0:4, :], gbi[:, 0:1], ALU.add, r=[pk, "m_gbi"], w=["g1"])
                                else:
                                    self.actf(g2[:, ts_], self.ps[pb][0:4, :], AF.Exp, r=[pk, "m_ngbf"], w=["g2"], scale=-1.0, bias=ngbf[:, 0:1])
                        self.actf(g2[:], g2[:], AF.Ln, r=["g2"], w=["g2"], bias=1.0)
                        c.op("dve", lambda e: e.tensor_tensor_scan(out=g3f[:], data0=onesr[:], data1=g2f[:], initial=0.0, op0=ALU.mult, op1=ALU.add),
                             r=["onesr", "g2"], w=["g3"])
                        self.tt("dve", g1[:], g1[:], g3[:], ALU.add, r=["g1", "g3"], w=["g1"])
                        c.op("dve", lambda e: e.tensor_tensor_scan(out=g2f[:], data0=onesr[:], data1=g1f[:], initial=0.0, op0=ALU.mult, op1=ALU.max),
                             r=["onesr", "g1", "g2"], w=["g2"])
                        pa = self.ps[2][:, 0:NB * 4].rearrange("p (b h) -> p b h", h=4)
                        pbn = self.ps[3][:, 0:NB * 4].rearrange("p (b h) -> p b h", h=4)
                        for blk in range(NB):
                            bs = slice(blk * 128, (blk + 1) * 128)
                            c.op("pe", lambda e: e.transpose(out=pa[:, blk, :], in_=g1[:, bs], identity=idf[0:4, 0:4]), r=["g1", "m_idf"], w=["ps2"])
                            c.op("pe", lambda e: e.transpose(out=pbn[:, blk, :], in_=g3[:, bs], identity=idf[0:4, 0:4]), r=["g3", "m_idf"], w=["ps3"])
                        self.cp("dve", acol[:], pa, r=["ps2"], w=["acol"])
                        self.cp("dve", bcol[:], pbn, r=["ps3"], w=["bcol"])
                        self.tt("dve", rsel[:], g2[:, 127::128].unsqueeze(2).broadcast_to([4, NB, 4]),
                                idf[0:4, 0:4].unsqueeze(1).broadcast_to([4, NB, 4]), ALU.mult, r=["g2", "m_idf"], w=["rsel"])
                        c.op("pe", lambda e: e.matmul(out=self.ps[0][:, 0:NB * 4], lhsT=ones4[:], rhs=rsel[:].rearrange("p b h -> p (b h)"),
                                                      start=True, stop=True), r=["m_ones4", "rsel"], w=["ps0"])
                        c.op("dve", lambda e: e.memset(Rrep[:, 0, :], 0.0), w=["Rrep"])
                        self.cp("dve", Rrep[:, 1:NB + 1, :], self.ps[0][:, 0:NB * 4].rearrange("p (b h) -> p b h", h=4), r=["ps0"], w=["Rrep"])
                    self.tt("dve", Wt[:], acol[:], Rrep[:, 0:NB, :], ALU.subtract, r=["acol", "Rrep"], w=["Wt"])
                    self.actf(Wt[:], Wt[:], AF.Exp, r=["Wt"], w=["Wt"])
                    self.tt("dve", Wp[:], acol[:], Rrep[:, 1:NB + 1, :], ALU.subtract, r=["acol", "Rrep"], w=["Wp"])
                    self.actf(Wp[:], Wp[:], AF.Exp, r=["Wp"], w=["Wp"])
                    self.ts("dve", Wp[:], Wp[:], SC, ALU.mult, r=["Wp"], w=["Wp"])
                    self.tt("dve", Thr[:], bcol[:], Rrep[:, 0:NB, :], ALU.subtract, r=["bcol", "Rrep"], w=["Thr"])
                    self.actf(Thr[:], Thr[:], AF.Exp, r=["Thr"], w=["Thr"])
                    self.tt("dve", Dec[:], Rrep[:, 0:NB, :], Rrep[:, 1:NB + 1, :], ALU.subtract, r=["Rrep"], w=["Dec"])
                    self.actf(Dec[:], Dec[:], AF.Exp, r=["Dec"], w=["Dec"])
                    with self.scope() as sm:
                        TM = lambda name, shape, dt=F32: sm.enter_context(self.sbt(name, shape, dt))
                        C32 = TM("C32", [128, 4, 129]); Cm = TM("Cm", [128, 4, 129])
                        Cb = TM("Cb", [128, 4, 129], BF16)
                        PT4 = [TM("PT4_%d" % i, [128, 4, 128], BF16) for i in range(2)]
                        Vp4 = [TM("Vp4_%d" % i, [128, 4, 129], BF16) for i in range(2)]
                        Vpp4 = [TM("Vpp4_%d" % i, [128, 4, 129], BF16) for i in range(2)]
                        den = TM("den4", [128, 4])
                        hraw = [TM("hraw%d" % i, [128, 4, 128]) for i in range(2)]
                        bst = TM("bst", [128, 4, 6]); mv = TM("mv", [128, 4, 2]); rs = TM("rs", [128, 4])
                        hn = TM("hn", [128, 4, 128], BF16)
                        e1 = TM("m_e1", [128, 4, 128]); e2 = TM("m_e2", [128, 4, 128])
                        mz = [TM("mz%d" % i, [128, 4, 512], BF16) for i in range(2)]
                        bo = [TM("bo%d" % i, [128, 4, 512], BF16) for i in range(2)]

                        def emit_front(I_):
                            bs_ = slice(I_ * 128, (I_ + 1) * 128)
                            j_ = I_ % 2
                            for h_ in range(4):
                                c.op("pe", lambda e: e.matmul(out=self.ps[j_][:, h_ * 128:(h_ + 1) * 128], lhsT=kT[:, h_, bs_], rhs=qT[:, h_, bs_],
                                                              start=True, stop=True), r=["kT", "qT"], w=["ps%d" % j_])
                            self.tt("dve", PT4[j_][:], self.ps[j_][:].rearrange("p (h t) -> p h t", h=4),
                                    maskS[:].unsqueeze(1).broadcast_to([128, 4, 128]), ALU.mult, r=["ps%d" % j_, "m_maskS"], w=["PT4_%d" % j_])
                            for h_ in range(4):
                                c.op("act", lambda e: e.activation(out=Vp4[j_][:, h_, :], in_=Vtok[:, I_, h_, :], func=AF.Copy, scale=Wt[:, I_, h_:h_ + 1]),
                                     r=["Vtok", "Wt"], w=["Vp4_%d" % j_])
                                c.op("act", lambda e: e.activation(out=Vpp4[j_][:, h_, :], in_=Vtok[:, I_, h_, :], func=AF.Copy, scale=Wp[:, I_, h_:h_ + 1]),
                                     r=["Vtok", "Wp"], w=["Vpp4_%d" % j_])
                        emit_front(0)
                        for I in range(NB):
                            bs = slice(I * 128, (I + 1) * 128)
                            i4 = (I // 4) % 2
                            j = I % 2
                            if I % 4 == 0:
                                c.dma(mz[i4][:], mzT_v[:, :, tb + I * 128:tb + I * 128 + 512], r=["mzT"], w=["mz%d" % i4])
                            hk = "hraw%d" % j
                            for h in range(4):
                                pO = self.ps[2 + h // 2][:, (h % 2) * 129:(h % 2 + 1) * 129]
                                kO = "ps%d" % (2 + h // 2)
                                c.op("pe", lambda e: e.matmul(out=pO, lhsT=PT4[j][:, h, :], rhs=Vp4[j][:, h, :], start=True, stop=(I == 0)),
                                     r=["PT4_%d" % j, "Vp4_%d" % j], w=[kO])
                                if I > 0:
                                    c.op("pe", lambda e: e.matmul(out=pO, lhsT=qT[:, h, bs], rhs=Cb[:, h, :], start=False, stop=True),
                                         r=["qT", "Cb"], w=[kO])
                            if I < NB - 1:
                                for h in range(4):
                                    pC = self.ps[4 + h // 2][:, (h % 2) * 129:(h % 2 + 1) * 129]
                                    c.op("pe", lambda e: e.matmul(out=pC, lhsT=Ktok[:, I, h, :], rhs=Vpp4[j][:, h, :], start=True, stop=True),
                                         r=["Ktok", "Vpp4_%d" % j], w=["ps%d" % (4 + h // 2)])
                            if I + 1 < NB:
                                emit_front(I + 1)
                            if I < NB - 1:
                                if I == 0:
                                    for hb in range(2):
                                        self.cp("dve", C32[:, 2 * hb:2 * hb + 2, :], self.ps[4 + hb][:, 0:258].rearrange("p (h e) -> p h e", h=2),
                                                r=["ps%d" % (4 + hb)], w=["C32"])
                                else:
                                    self.tt("pool", Cm[:], C32[:], Dec[:, I, :].unsqueeze(2).broadcast_to([128, 4, 129]), ALU.mult, r=["C32", "Dec"], w=["Cm"])
                                    for hb in range(2):
                                        self.tt("dve", C32[:, 2 * hb:2 * hb + 2, :], self.ps[4 + hb][:, 0:258].rearrange("p (h e) -> p h e", h=2),
                                                Cm[:, 2 * hb:2 * hb + 2, :], ALU.add, r=["ps%d" % (4 + hb), "Cm"], w=["C32"])
                                self.cp("act", Cb[:], C32[:], r=["C32"], w=["Cb"])
                            for hb in range(2):
                                self.actf(den[:, 2 * hb:2 * hb + 2], self.ps[2 + hb][:, 0:258].rearrange("p (h e) -> p h e", h=2)[:, :, 128],
                                          AF.Abs, r=["ps%d" % (2 + hb)], w=["den4_%d" % hb])
                            self.tt("dve", den[:], den[:], Thr[:, I, :], ALU.max, r=["den4_0", "den4_1", "Thr"], w=["den4", "den4_0", "den4_1"])
                            c.op("dve", lambda e: e.reciprocal(out=den[:], in_=den[:]), r=["den4"], w=["den4"])
                            for hb in range(2):
                                self.tt("dve", hraw[j][:, 2 * hb:2 * hb + 2, :], self.ps[2 + hb][:, 0:258].rearrange("p (h e) -> p h e", h=2)[:, :, 0:128],
                                        den[:, 2 * hb:2 * hb + 2].unsqueeze(2).broadcast_to([128, 2, 128]), ALU.mult, r=["ps%d" % (2 + hb), "den4"], w=[hk + "_%d" % hb])
                            mvk = ["mv%d" % h for h in range(4)]
                            for h in range(4):
                                c.op("dve", lambda e: e.bn_stats(out=bst[:, h, :], in_=hraw[j][:, h, :]), r=[hk + "_%d" % (h // 2)], w=["bst%d" % h])
                            for h in range(4):
                                c.op("dve", lambda e: e.bn_aggr(out=mv[:, h, :], in_=bst[:, h, :]), r=["bst%d" % h], w=["mv%d" % h])
                            self.actf(rs[:], mv[:, :, 1], AF.Ln, r=mvk, w=["rs"], bias=HEAD_EPS)
                            self.actf(rs[:], rs[:], AF.Exp, r=["rs"], w=["rs"], scale=-0.5)
                            self.tt("pool", e1[:], hraw[j][:], mv[:, :, 0:1].broadcast_to([128, 4, 128]), ALU.subtract, r=[hk + "_0", hk + "_1"] + mvk, w=["m_e1"])
                            self.tt("dve", hn[:], e1[:], rs[:].unsqueeze(2).broadcast_to([128, 4, 128]), ALU.mult, r=["m_e1", "rs"], w=["hn"])
                            ptv = self.ps[6 + I % 2][:].bitcast(BF16).rearrange("p (a b) -> p a b", a=8)
                            pk = "ps%d" % (6 + I % 2)
                            for h in range(4):
                                c.op("pe", lambda e: e.transpose(out=ptv[:, h, :], in_=hn[:, h, :], identity=self.ident[:]), r=["hn", "ident"], w=[pk])
                            self.tt("pool", e2[:], xcT[:, :, bs], msk[:].unsqueeze(2).broadcast_to([128, 4, 128]), ALU.mult, r=["xcT", "m_msk"], w=["m_e2"])
                            self.tt("dve", e1[:], ptv[:, 0:4, :], mn[:].unsqueeze(2).broadcast_to([128, 4, 128]), ALU.mult, r=[pk, "m_mn", "m_e1"], w=["m_e1"])
                            self.tt("dve", e1[:], e1[:], e2[:], ALU.add, r=["m_e1", "m_e2"], w=["m_e1"])
                            off = (I % 4) * 128
                            self.tt("pool", bo[i4][:, :, off:off + 128], e1[:], mz[i4][:, :, off:off + 128], ALU.mult,
                                    r=["m_e1", "mz%d" % i4], w=["bo%d" % i4])
                            if I % 4 == 3:
                                c.dma(bT_v[:, :, tb + (I - 3) * 128:tb + (I + 1) * 128], bo[i4][:], r=["bo%d" % i4], w=["bT"])
        c.barrier()

    def out_proj_norm_res(self, lay, wout_d, pg_rep_d, lhs_provider, res_rows, dst_rows, dst_key, res_key):
        nc, c = self.nc, self.c
        NT = self.NT
        with self.scope() as st:
            T = lambda name, shape, dt=F32: st.enter_context(self.sbt(name, shape, dt))
            wout = T("wout", [128, 8, D], BF16)
            wst = [T("wost%d" % i, [128, D]) for i in range(2)]
            for fc in range(8):
                c.dma(wst[fc % 2][:], wout_d[fc * 128:(fc + 1) * 128, :], w=["wost%d" % (fc % 2)])
                self.cp("dve" if fc % 2 else "act", wout[:, fc, :], wst[fc % 2][:], r=["wost%d" % (fc % 2)], w=["wout"])
            pg = T("pg", [128, D])
            c.dma(pg[:], pg_rep_d, w=["pg"])
            NXR = 4
            xr = [T("xr%d" % i, [128, D]) for i in range(NXR)]
            yo = [T("yo%d" % i, [128, D]) for i in range(2)]
            junk = T("ojunk", [128, BR])
            ss2 = [T("oss%d" % i, [128, 2]) for i in range(2)]; rstd2 = [T("orstd%d" % i, [128, 1]) for i in range(2)]
            pref, prov = lhs_provider(st)
            nblk = NT // 128

            def prefetch(b):
                if b < nblk:
                    c.dma(xr[b % NXR][:], res_rows[b * 128:(b + 1) * 128, :], r=[res_key % b], w=["xr%d" % (b % NXR)])
                    pref(b)
            prefetch(0)
            prefetch(1)
            lhs_next = prov(0)
            for blk in range(nblk):
                rows = slice(blk * 128, (blk + 1) * 128)
                xk = "xr%d" % (blk % NXR)
                prefetch(blk + 2)
                lhs = lhs_next
                ss = ss2[blk % 2]; rstd = rstd2[blk % 2]
                ssk = "oss%d" % (blk % 2); rsk = "orstd%d" % (blk % 2)
                for half in range(2):
                    pb = 4 + half + 2 * (blk % 2)
                    pk = "ps%d" % pb
                    for fc in range(8):
                        ap, key = lhs[fc]
                        c.op("pe", lambda e: e.matmul(out=self.ps[pb][:], lhsT=ap, rhs=wout[:, fc, half * BR:(half + 1) * BR],
                                                      start=(fc == 0), stop=(fc == 7)), r=[key, "wout"], w=[pk])
                if blk + 1 < nblk:
                    lhs_next = prov(blk + 1)
                for half in range(2):
                    pb = 4 + half + 2 * (blk % 2)
                    pk = "ps%d" % pb
                    self.actf(junk[:], self.ps[pb][:], AF.Square, r=[pk], w=["ojunk", ssk], accum_out=ss[:, half:half + 1])
                self.tt("dve", rstd[:], ss[:, 0:1], ss[:, 1:2], ALU.add, r=[ssk], w=[rsk])
                self.actf(rstd[:], rstd[:], AF.Ln, r=[rsk], w=[rsk], scale=1.0 / D, bias=NORM_EPS)
                self.actf(rstd[:], rstd[:], AF.Exp, r=[rsk], w=[rsk], scale=-0.5)
                yk = "yo%d" % (blk % 2)
                for half in range(2):
                    pb = 4 + half + 2 * (blk % 2)
                    hs = slice(half * BR, (half + 1) * BR)
                    self.tt("dve", yo[blk % 2][:, hs], self.ps[pb][:], pg[:, hs], ALU.mult, r=["ps%d" % pb, "pg"], w=[yk])
                self.stt("dve", yo[blk % 2][:], yo[blk % 2][:], rstd[:, 0:1], xr[blk % NXR][:], ALU.mult, ALU.add, r=[yk, rsk, xk], w=[yk])
                c.dma(dst_rows[rows, :], yo[blk % 2][:], r=[yk], w=[dst_key % blk])

    def phase_l0_out(self):
        nc, c = self.nc, self.c
        bT_v = self.bT.rearrange("(c p) t -> p c t", p=128)

        def provider(st):
            T = lambda name, shape, dt=F32: st.enter_context(self.sbt(name, shape, dt))
            at = [T("at%d" % i, [128, BR], BF16) for i in range(4)]
            aT = [T("aT%d" % i, [128, 4, 128], BF16) for i in range(2)]
            bt = [T("bt%d" % i, [128, 4, 512], BF16) for i in range(2)]

            def pref(blk):
                i4 = (blk // 4) % 2
                c.dma(at[blk % 4][:], self.a_tok[blk * 128:(blk + 1) * 128, :], r=["a_tok"], w=["at%d" % (blk % 4)])
                if blk % 4 == 0:
                    c.dma(bt[i4][:], bT_v[:, :, blk * 128:blk * 128 + 512], r=["bT"], w=["bt%d" % i4])

            def prov(blk):
                i = blk % 2
                i4 = (blk // 4) % 2
                pk = "ps%d" % i
                ptv = self.ps[i][:].bitcast(BF16).rearrange("p (a b) -> p a b", a=8)
                for fc in range(4):
                    c.op("pe", lambda e: e.transpose(out=ptv[:, fc, :], in_=at[blk % 4][:, fc * 128:(fc + 1) * 128], identity=self.ident[:]),
                         r=["at%d" % (blk % 4), "ident"], w=[pk])
                self.cp("act", aT[i][:], ptv[:, 0:4, :], r=[pk], w=["aT%d" % i])
                off = (blk % 4) * 128
                return [(aT[i][:, fc, :], "aT%d" % i) for fc in range(4)] + \
                       [(bt[i4][:, h, off:off + 128], "bt%d" % i4) for h in range(4)]
            return pref, prov
        self.out_proj_norm_res(0, self.w_out_ab, self.post0_rep, provider, self.x, self.out, "h1_%d", "x%.0d")

    def phase_l1_proj(self):
        nc, c = self.nc, self.c
        NT = self.NT
        S_ = self.S
        ngrp = NT // 512
        nblk = NT // 128
        with self.scope() as st:
            T = lambda name, shape, dt=F32: st.enter_context(self.sbt(name, shape, dt))
            win = T("win1", [128, 8, 8 * BR], BF16)
            g1 = T("g1n", [128, 8])
            stage = [T("w1st%d" % i, [128, 4 * BR]) for i in range(2)]
            c.dma(g1[:], self.pre1, w=["g1n"])
            for dc in range(8):
                for hf in range(2):
                    i = (dc * 2 + hf) % 2
                    sk = "w1st%d" % i
                    c.dma(stage[i][:], self.w_in_cd[dc * 128:(dc + 1) * 128, hf * 2048:(hf + 1) * 2048], w=[sk])
                    if hf == 0:
                        c.op("act", lambda e: e.activation(out=win[:, dc, 0:2048], in_=stage[i][:], func=AF.Copy, scale=g1[:, dc:dc + 1]),
                             r=[sk, "g1n"], w=["win1"])
                    else:
                        self.ts("dve", win[:, dc, 2048:4096], stage[i][:], g1[:, dc:dc + 1], ALU.mult, r=[sk, "g1n"], w=["win1"])
            cosT = T("cosT", [128, S_]); sinT = T("sinT", [128, S_]); Rm = T("Rm", [128, 128], BF16)
            c.dma(cosT[:], self.rope_cos, w=["cosT"])
            c.dma(sinT[:], self.rope_sin, w=["sinT"])
            c.dma(Rm[:], self.rope_rm, w=["Rm"])
            NXB = 6
            xt = [T("x1t%d" % i, [128, D]) for i in range(NXB)]
            junk = T("junk1", [128, D])
            ss2 = [T("ss1_%d" % i, [128, 4]) for i in range(2)]; rstd2 = [T("rstd1_%d" % i, [128, 4]) for i in range(2)]
            zb = [T("z1b%d" % i, [128, D], BF16) for i in range(2)]
            zT = [T("z1T%d" % i, [128, 8, 512], BF16) for i in range(2)]
            vo = [T("vo%d" % i, [128, BR], BF16) for i in range(4)]
            xb = [T("xb%d" % i, [128, 512], BF16) for i in range(2)]
            r1 = [T("r1_%d" % i, [128, 512]) for i in range(2)]
            r2 = [T("r2_%d" % i, [128, 512]) for i in range(2)]
            fo = [T("fo%d" % i, [128, 4, 512], BF16) for i in range(2)]

            def load_x(b):
                if b < nblk:
                    c.dma(xt[b % NXB][:], self.h1src[b * 128:(b + 1) * 128, :], r=["h1_%d" % b], w=["x1t%d" % (b % NXB)])
            for b in range(4):
                load_x(b)
            fm_groups = [(0, self.cqT, "cqT", "rope"), (512, self.ckT, "ckT", "rope"), (2048, self.dqT, "dqT", "rope"),
                         (2560, self.dkT, "dkT", "rope"), (1536, self.czT, "czT", "silu"), (3584, self.dzT, "dzT", "silu")]
            cnt = 0
            for g in range(ngrp):
                zk = "z1T%d" % (g % 2)
                ss = ss2[g % 2]; rstd = rstd2[g % 2]
                ssk = "ss1_%d" % (g % 2); rsk = "rstd1_%d" % (g % 2)
                pos0 = (g * 512) % S_
                for j in range(4):
                    b = g * 4 + j
                    xk = "x1t%d" % (b % NXB)
                    c.op("act", lambda e: e.activation(out=junk[:], in_=xt[b % NXB][:], func=AF.Square, accum_out=ss[:, j:j + 1]),
                         r=[xk], w=["junk1", ssk])
                self.actf(rstd[:], ss[:], AF.Ln, r=[ssk], w=[rsk], scale=1.0 / D, bias=NORM_EPS)
                self.actf(rstd[:], rstd[:], AF.Exp, r=[rsk], w=[rsk], scale=-0.5)
                for j in range(4):
                    b = g * 4 + j
                    xk = "x1t%d" % (b % NXB)
                    zbk = "z1b%d" % (b % 2)
                    pk = "ps%d" % (b % 2)
                    self.ts("dve", zb[b % 2][:], xt[b % NXB][:], rstd[:, j:j + 1], ALU.mult, r=[xk, rsk], w=[zbk])
                    load_x(b + 4)
                    ptv = self.ps[b % 2][:].bitcast(BF16).rearrange("p (a b) -> p a b", a=8)
                    for dc in range(8):
                        c.op("pe", lambda e: e.transpose(out=ptv[:, dc, :], in_=zb[b % 2][:, dc * 128:(dc + 1) * 128], identity=self.ident[:]),
                             r=[zbk, "ident"], w=[pk])
                    self.cp("dve", zT[g % 2][:, :, j * 128:(j + 1) * 128], ptv, r=[pk], w=[zk])
                    for vi, (col0, dst, dkey) in enumerate(((1024, self.cv_tok, "cv_tok"), (3072, self.dv_tok, "dv_tok"))):
                        pb = 2 + vi
                        vk = "vo%d" % ((b % 2) * 2 + vi)
                        for dc in range(8):
                            c.op("pe", lambda e: e.matmul(out=self.ps[pb][:], lhsT=zT[g % 2][:, dc, j * 128:(j + 1) * 128],
                                                          rhs=win[:, dc, col0:col0 + BR], start=(dc == 0), stop=(dc == 7)), r=[zk, "win1"], w=["ps%d" % pb])
                        self.cp("act", vo[(b % 2) * 2 + vi][:], self.ps[pb][:], r=["ps%d" % pb], w=[vk])
                        c.dma(dst[b * 128:(b + 1) * 128, :], vo[(b % 2) * 2 + vi][:], r=[vk], w=[dkey])
                pend = []

                def flush_pend():
                    while pend:
                        (fot_, fc_, fk_, i2_, pb_, dst_, dkey_, lastfc) = pend.pop(0)
                        pr = 6 + i2_
                        c.op("pe", lambda e: e.matmul(out=self.ps[pr][:], lhsT=Rm[:], rhs=xb[i2_][:], start=True, stop=True),
                             r=["Rm", "xb%d" % i2_], w=["ps%d" % pr])
                        self.tt("dve", r1[i2_][:], self.ps[pb_][:], cosT[:, pos0:pos0 + 512], ALU.mult, r=["ps%d" % pb_, "cosT"], w=["r1_%d" % i2_])
                        self.tt("dve", r2[i2_][:], self.ps[pr][:], sinT[:, pos0:pos0 + 512], ALU.mult, r=["ps%d" % pr, "sinT"], w=["r2_%d" % i2_])
                        self.tt("pool", fot_[:, fc_, :], r1[i2_][:], r2[i2_][:], ALU.add, r=["r1_%d" % i2_, "r2_%d" % i2_], w=[fk_])
                        if lastfc:
                            c.dma(dst_.rearrange("(c p) t -> p c t", p=128)[:, :, g * 512:(g + 1) * 512], fot_[:], r=[fk_], w=[dkey_])
                for (col0, dst, dkey, kind) in fm_groups:
                    fk = "fo%d" % (cnt % 2)
                    fot = fo[cnt % 2]
                    cnt += 1
                    for fc in range(4):
                        pb = 4 + fc % 2
                        pk = "ps%d" % pb
                        for dc in range(8):
                            c.op("pe", lambda e: e.matmul(out=self.ps[pb][:], lhsT=win[:, dc, col0 + fc * 128:col0 + (fc + 1) * 128],
                                                          rhs=zT[g % 2][:, dc, :], start=(dc == 0), stop=(dc == 7)), r=[zk, "win1"], w=[pk])
                        flush_pend()
                        if kind == "silu":
                            self.actf(fot[:, fc, :], self.ps[pb][:], AF.Silu, r=[pk], w=[fk])
                            if fc == 3:
                                c.dma(dst.rearrange("(c p) t -> p c t", p=128)[:, :, g * 512:(g + 1) * 512], fot[:], r=[fk], w=[dkey])
                        else:
                            i2 = fc % 2
                            self.cp("act", xb[i2][:], self.ps[pb][:], r=[pk], w=["xb%d" % i2])
                            pend.append((fot, fc, fk, i2, pb, dst, dkey, fc == 3))
                flush_pend()

    def phase_attn(self, kind):
        nc, c = self.nc, self.c
        S_ = self.S
        NB = S_ // 128
        NQT = S_ // 512
        dil = (kind == "dil")
        qT_d, kT_d, v_d, o_d = (self.cqT, self.ckT, self.cv_tok, self.oc_tok) if dil else (self.dqT, self.dkT, self.dv_tok, self.od_tok)
        okey = "oc_tok" if dil else "od_tok"
        NH = 8 if dil else 4
        VW = 64 if dil else 128
        nmask = 9 if dil else 4
        lam_init = 0.8 - 0.6 * math.exp(-0.3 * 1)
        with self.scope() as st:
            T = lambda name, shape, dt=F32: st.enter_context(self.sbt(name, shape, dt))
            masks = T("amask", [128, nmask, 512], BF16)
            c.dma(masks[:], self.dil_masks if dil else self.diff_masks, w=["amask"])
            if not dil:
                lqk = T("lqk", [1, 4, 64]); pr = T("lpr", [1, 2, 64]); sm = T("lsm", [1, 2]); nl = T("nl", [1, 1])
                ones1 = T("ones1", [1, 128]); nlam = T("nlam", [128, 1]); gdn = T("gdn", [128, 128])
                c.dma(lqk[:], self.diff_lqk, w=["lqk"])
                c.dma(gdn[:], self.diffnorm_rep, w=["gdn"])
                c.op("dve", lambda e: e.memset(ones1[:], 1.0), w=["ones1"])
                self.tt("dve", pr[:, 0, :], lqk[:, 0, :], lqk[:, 1, :], ALU.mult, r=["lqk"], w=["lpr"])
                self.tt("dve", pr[:, 1, :], lqk[:, 2, :], lqk[:, 3, :], ALU.mult, r=["lqk", "lpr"], w=["lpr"])
                c.op("dve", lambda e: e.reduce_sum(out=sm[:], in_=pr[:], axis=AX.X), r=["lpr"], w=["lsm"])
                self.actf(sm[:], sm[:], AF.Exp, r=["lsm"], w=["lsm"])
                self.tt("dve", nl[:], sm[:, 1:2], sm[:, 0:1], ALU.subtract, r=["lsm"], w=["nl"])
                self.ts("dve", nl[:], nl[:], -lam_init, ALU.add, r=["nl"], w=["nl"])
                c.op("pe", lambda e: e.matmul(out=self.ps[0][:, 0:1], lhsT=ones1[:], rhs=nl[:], start=True, stop=True), r=["ones1", "nl"], w=["ps0"])
                self.cp("dve", nlam[:], self.ps[0][:, 0:1], r=["ps0"], w=["nlam"])
                self.ts("dve", gdn[:], gdn[:], 1.0 - lam_init, ALU.mult, r=["gdn"], w=["gdn"])
            qT_v = qT_d.rearrange("(c p) t -> p c t", p=128)
            kT_v = kT_d.rearrange("(c p) t -> p c t", p=128)
            sets = []
            for q in range(self.nseq):
                tb = q * S_
                qz_ = [T("aqz%d_%d" % (q, i), [128, 4, S_], BF16) for i in range(2)]
                kT_ = T("akT_%d" % q, [128, 4, S_], BF16)
                V1_ = T("aV1_%d" % q, [128, NB, NH, VW + 1], BF16)
                kq, kk, kv = "aqT%d" % q, "akT%d" % q, "aV1%d" % q
                c.op("pool", lambda e: e.memset(qz_[0][64:128, :, :], 0.0), w=[kq])
                c.op("pool", lambda e: e.memset(qz_[1][0:64, :, :], 0.0), w=[kq])
                c.dma(qz_[0][0:64, :, :], qT_v[0:64, :, tb:tb + S_], r=[("cqT" if dil else "dqT")], w=[kq])
                c.dma(qz_[1][64:128, :, :], qT_v[64:128, :, tb:tb + S_], r=[("cqT" if dil else "dqT")], w=[kq])
                c.dma(kT_[:], kT_v[:, :, tb:tb + S_], r=[("ckT" if dil else "dkT")], w=[kk])
                c.op("pool", lambda e: e.memset(V1_[:, :, :, VW:VW + 1], 1.0), w=[kv])
                for blk in range(NB):
                    c.dma(V1_[:, blk, :, 0:VW], v_d[tb + blk * 128:tb + (blk + 1) * 128, :].rearrange("p (h d) -> p h d", h=NH),
                          r=[("cv_tok" if dil else "dv_tok")], w=[kv])
                sets.append((qz_, kT_, V1_, kq, kk, kv))
            for q in range(self.nseq):
                tb = q * S_
                with self.scope() as sq:
                    TQ = lambda name, shape, dt=F32: sq.enter_context(self.sbt(name, shape, dt))
                    qz, kT, V1, kq, kk, kv = sets[q]
                    osb = TQ("aosb", [128, NB, BR], BF16)
                    NEP = 3
                    E = [TQ("aE%d" % i, [128, 512], BF16) for i in range(NEP)]
                    P = [TQ("aP%d" % i, [128, 512], BF16) for i in range(NEP)]
                    rd = [TQ("ard%d" % i, [128, 1]) for i in range(4)]
                    if not dil:
                        o01 = [TQ("ao%d" % i, [128, NB, 128]) for i in range(2)]
                        sqj = TQ("asq", [128, 128]); ssn = TQ("assn", [128, NB]); t3 = TQ("at3", [128, 128])
                    tiles = []
                    for h in range(NH):
                        for m in range(1 if dil else 2):
                            for qt in range(NQT):
                                nkb = 4 * qt + 4
                                for kb in range(nkb):
                                    tiles.append((h, m, qt, kb, kb == nkb - 1))
                    LA = 3
                    NSB = 4

                    def emit_S(i):
                        h, m, qt, kb, _ = tiles[i]
                        ch = h // 2 if dil else h
                        rb = 64 * (h % 2) if dil else 64 * m
                        sb = i % NSB
                        c0 = 128 * max(0, kb - 4 * qt)
                        c.op("pe", lambda e: e.matmul(out=self.ps[sb][:, c0:512], lhsT=kT[:, ch, kb * 128:(kb + 1) * 128],
                                                      rhs=qz[rb // 64][:, ch, qt * 512 + c0:(qt + 1) * 512], start=True, stop=True),
                             r=[kk, kq], w=["ps%d" % sb])
                    for i in range(min(LA, len(tiles))):
                        emit_S(i)
                    for i, (h, m, qt, kb, last) in enumerate(tiles):
                        if i + LA < len(tiles):
                            emit_S(i + LA)
                        sb = i % NSB
                        i2 = i % NEP
                        pS = self.ps[sb]
                        kS = "ps%d" % sb
                        d0 = 4 * qt - kb
                        if dil:
                            mi = 8 if d0 >= 5 else d0 + 3
                        else:
                            mi = d0 + 3 if d0 <= 0 else None
                        c0 = 128 * max(0, kb - 4 * qt)
                        if mi is None:
                            self.actf(P[i2][:, c0:512], pS[:, c0:512], AF.Exp, r=[kS], w=["aP%d" % i2], scale=0.125)
                        else:
                            self.actf(E[i2][:, c0:512], pS[:, c0:512], AF.Exp, r=[kS], w=["aE%d" % i2], scale=0.125)
                            self.tt("dve", P[i2][:, c0:512], E[i2][:, c0:512], masks[:, mi, c0:512], ALU.mult,
                                    r=["aE%d" % i2, "amask"], w=["aP%d" % i2])
                        for j in range(4):
                            Q = 4 * qt + j
                            if kb > Q:
                                continue
                            c.op("pe", lambda e: e.matmul(out=self.ps[4 + j][:, 0:VW + 1], lhsT=P[i2][:, j * 128:(j + 1) * 128],
                                                          rhs=V1[:, kb, h, :], start=(kb == 0), stop=(kb == Q)),
                                 r=["aP%d" % i2, kv], w=["ps%d" % (4 + j)])
                        if last:
                            for j in range(4):
                                c.op("dve", lambda e: e.reciprocal(out=rd[j][:], in_=self.ps[4 + j][:, VW:VW + 1]), r=["ps%d" % (4 + j)], w=["ard%d" % j])
                            for j in range(4):
                                Q = 4 * qt + j
                                pO = self.ps[4 + j]
                                kO = "ps%d" % (4 + j)
                                if dil:
                                    self.ts("dve", osb[:, Q, h * 64:(h + 1) * 64], pO[:, 0:VW], rd[j][:, 0:1], ALU.mult,
                                            r=[kO, "ard%d" % j], w=["aosb%d" % j])
                                else:
                                    self.ts("dve", o01[m][:, Q, :], pO[:, 0:VW], rd[j][:, 0:1], ALU.mult,
                                            r=[kO, "ard%d" % j], w=["ao%d_%d" % (m, j)])
                            if (not dil) and m == 1 and qt == NQT - 1:
                                aok = ["ao%d_%d" % (mm_, jj_) for mm_ in range(2) for jj_ in range(4)]
                                self.stt("dve", o01[0][:], o01[1][:], nlam[:, 0:1], o01[0][:], ALU.mult, ALU.add, r=aok + ["nlam"], w=aok + ["ao0"])
                                for Q in range(NB):
                                    self.actf(sqj[:], o01[0][:, Q, :], AF.Square, r=aok, w=["asq", "assn"], accum_out=ssn[:, Q:Q + 1])
                                self.actf(ssn[:], ssn[:], AF.Ln, r=["assn"], w=["assn"], scale=1.0 / 128, bias=HEAD_EPS)
                                self.actf(ssn[:], ssn[:], AF.Exp, r=["assn"], w=["assn"], scale=-0.5)
                                for Q in range(NB):
                                    self.ts("dve", t3[:], o01[0][:, Q, :], ssn[:, Q:Q + 1], ALU.mult, r=aok + ["assn"], w=["at3"])
                                    self.tt("pool", osb[:, Q, h * 128:(h + 1) * 128], t3[:], gdn[:], ALU.mult, r=["at3", "gdn"], w=["aosb0"])
                    c.dma(o_d[tb:tb + S_, :].rearrange("(b p) f -> p b f", p=128), osb[:], r=["aosb%d" % jj_ for jj_ in range(4)], w=[okey])

    def phase_l1_out(self):
        nc, c = self.nc, self.c
        czT_v = self.czT.rearrange("(c p) t -> p c t", p=128)
        dzT_v = self.dzT.rearrange("(c p) t -> p c t", p=128)

        def provider(st):
            T = lambda name, shape, dt=F32: st.enter_context(self.sbt(name, shape, dt))
            ot = [T("ot%d" % i, [128, 2, BR], BF16) for i in range(4)]
            gT = [T("gT1_%d" % i, [128, 8, 128], BF16) for i in range(2)]
            gz = [T("gz%d" % i, [128, 8, 512], BF16) for i in range(2)]

            def pref(blk):
                i = blk % 4
                i4 = (blk // 4) % 2
                c.dma(ot[i][:, 0, :], self.oc_tok[blk * 128:(blk + 1) * 128, :], r=["oc_tok"], w=["ot%d" % i])
                c.dma(ot[i][:, 1, :], self.od_tok[blk * 128:(blk + 1) * 128, :], r=["od_tok"], w=["ot%d" % i])
                if blk % 4 == 0:
                    c.dma(gz[i4][:, 0:4, :], czT_v[:, :, blk * 128:blk * 128 + 512], r=["czT"], w=["gz%d" % i4])
                    c.dma(gz[i4][:, 4:8, :], dzT_v[:, :, blk * 128:blk * 128 + 512], r=["dzT"], w=["gz%d" % i4])

            def prov(blk):
                i = blk % 2
                i4 = (blk // 4) % 2
                pk = "ps%d" % i
                ptv = self.ps[i][:].bitcast(BF16).rearrange("p (a b) -> p a b", a=8)
                for fc in range(8):
                    c.op("pe", lambda e: e.transpose(out=ptv[:, fc, :], in_=ot[blk % 4][:, fc // 4, (fc % 4) * 128:(fc % 4 + 1) * 128],
                                                     identity=self.ident[:]), r=["ot%d" % (blk % 4), "ident"], w=[pk])
                off = (blk % 4) * 128
                self.tt("dve", gT[i][:], ptv, gz[i4][:, :, off:off + 128], ALU.mult, r=[pk, "gz%d" % i4], w=["gT1_%d" % i])
                return [(gT[i][:, fc, :], "gT1_%d" % i) for fc in range(8)]
            return pref, prov
        self.out_proj_norm_res(1, self.w_out_cd, self.post1_rep, provider, self.h1src, self.out, "h1_%d", "h1_%d")


def core_inputs(inp, x_rows):
    m = {}
    m["x"] = np.ascontiguousarray(x_rows, dtype=np.float32)
    m["ident"] = np.eye(128, dtype=np.float32).astype(ml_dtypes.bfloat16)
    m["pre0"] = np.ascontiguousarray(inp["pre_norm"][0].reshape(8, 128).T)
    m["w_in_ab"] = inp["w_in_ab"][0]
    m["s5_lr"] = inp["s5_lambda_re"][0].T
    m["s5_li"] = inp["s5_lambda_im"][0].T
    m["s5_ldt"] = np.broadcast_to(inp["s5_log_dt"][0][None, :], (64, 32))
    m["s5_br"] = inp["s5_b_re"][0].transpose(1, 0, 2)
    m["s5_bi"] = inp["s5_b_im"][0].transpose(1, 0, 2)
    m["s5_cr"] = inp["s5_c_re"][0].transpose(2, 0, 1)
    m["s5_ci"] = inp["s5_c_im"][0].transpose(2, 0, 1)
    tv = np.array([0, -1, -2, -3, -4, -5, -6, -7, 1, 2, 3, 4, 5, 6, 7, 8], dtype=np.float32)
    m["tv"] = np.broadcast_to(tv[None, :], (64, 16))
    m["kv"] = np.broadcast_to(np.arange(256, dtype=np.float32)[None, :], (64, 256))
    sidx = np.arange(128) // 16
    m["toepmask"] = (sidx[None, :] >= sidx[:, None]).astype(np.float32)
    m["identf"] = np.eye(128, dtype=np.float32)
    m["s5d_rep"] = np.broadcast_to(inp["s5_d"][0][None, :], (128, 512))
    m["glub_rep"] = np.broadcast_to(inp["s5_glu_b"][0][None, :], (128, 512))
    m["glu_w"] = inp["s5_glu_w"][0]
    m["ml_cw"] = inp["ml_conv_w"][0].reshape(4, 4, 128).transpose(2, 1, 0)
    m["ml_cb"] = inp["ml_conv_b"][0].reshape(4, 128).T
    m["ml_mn"] = inp["ml_norm"][0].reshape(4, 128).T
    m["ml_msk"] = inp["ml_skip"][0].reshape(4, 128).T
    m["ml_gbi"] = inp["ml_gate_b"][0][0:4].reshape(4, 1)
    m["ml_gbf"] = inp["ml_gate_b"][0][4:8].reshape(4, 1)
    si = np.arange(128)
    m["ml_maskS"] = ((si[:, None] <= si[None, :]) * (128 ** -0.5)).astype(np.float32)
    m["ml_gw"] = inp["ml_gate_w"][0].reshape(12, 128, 8).transpose(1, 0, 2)
    m["ml_wq"] = inp["ml_wq"][0].transpose(1, 0, 2)
    m["ml_wk"] = inp["ml_wk"][0].transpose(1, 0, 2)
    m["ml_wv"] = inp["ml_wv"][0].transpose(1, 0, 2)
    m["w_out_ab"] = inp["w_out_ab"][0]
    m["pre1"] = np.ascontiguousarray(inp["pre_norm"][1].reshape(8, 128).T)
    m["w_in_cd"] = inp["w_in_cd"][0]
    pos = np.arange(S, dtype=np.float32)
    inv = (10000.0 ** (-np.arange(0, 64, 2, dtype=np.float32) / 64)).astype(np.float32)
    ang = pos[None, :] * inv[np.arange(128) % 32][:, None]
    m["rope_cos"] = np.cos(ang).astype(np.float32)
    m["rope_sin"] = np.sin(ang).astype(np.float32)
    rm = np.zeros((128, 128), dtype=np.float32)
    for mm in range(128):
        if mm % 64 < 32:
            rm[mm + 32, mm] = -1.0
        else:
            rm[mm - 32, mm] = 1.0
    m["rope_rm"] = rm.astype(ml_dtypes.bfloat16)
    kk = np.arange(128)[:, None]
    qq = np.arange(512)[None, :]
    dm = np.zeros((128, 9, 512), dtype=np.float32)
    fm = np.zeros((128, 4, 512), dtype=np.float32)
    for mi in range(9):
        d0 = mi - 3 if mi < 8 else 5
        dl = 128 * d0 + qq - kk
        mult = ((dl >= 0) & (dl <= 128)).astype(np.float32) + ((dl >= 0) & (dl % 4 == 0) & (dl <= 512)) + ((dl >= 0) & (dl % 16 == 0) & (dl <= 2048))
        dm[:, mi, :] = mult
        if mi < 4:
            fm[:, mi, :] = (dl >= 0)
    m["dil_masks"] = dm.astype(ml_dtypes.bfloat16)
    m["diff_masks"] = fm.astype(ml_dtypes.bfloat16)
    m["diff_lqk"] = np.stack([inp["diff_lq1"][0], inp["diff_lk1"][0], inp["diff_lq2"][0], inp["diff_lk2"][0]])[None]
    m["diffnorm_rep"] = np.broadcast_to(inp["diff_norm"][0][None, :], (128, 128))
    m["w_out_cd"] = inp["w_out_cd"][0]
    m["post1_rep"] = np.broadcast_to(inp["post_norm"][1][None, :], (128, 1024))
    m["post0_rep"] = np.broadcast_to(inp["post_norm"][0][None, :], (128, 1024))
    return m


_CACHE = {}


def kernel(**inputs):
    inp = {k_: np.asarray(v) for k_, v in inputs.items()}
    x = inp["x"]
    B = x.shape[0]
    nseq = B // NCORES
    if "prog" not in _CACHE:
        kb = K(nseq=nseq)
        kb.build()
        _CACHE["prog"] = kb
    kb = _CACHE["prog"]
    in_maps = []
    for ci in range(NCORES):
        m = core_inputs(inp, x[ci * nseq:(ci + 1) * nseq].reshape(-1, D))
        in_maps.append({n: np.ascontiguousarray(m[n]) for n in kb.inputs})
    res = run_bass_kernel_spmd(kb.nc, in_maps, core_ids=list(range(NCORES)))
    out = np.stack([np.asarray(res.results[ci]["out"]).reshape(nseq, S, D) for ci in range(NCORES)], axis=0)
    return out.reshape(B, S, D).astype(np.float32)
```
